# Optimizing a Trainium2 kernel written in Bass

```python
import jax, jax.numpy as jnp
from jax import lax
import numpy as np


D_MODEL = 1024
BATCH = 4
SEQ = 8192
DEPTH = 1

GRID_W = 64
N_MEM = 256
EPS = 1e-6

ATT_HEADS = 8
ATT_KV_HEADS = 2
ATT_HEAD_DIM = 64
ATT_WIDTH = ATT_HEADS * ATT_HEAD_DIM
KV_WIDTH = ATT_KV_HEADS * ATT_HEAD_DIM
ROPE_THETA = 10000.0
Q_BLOCK = 128

SGU_GROUPS = 4
SGU_WIDTH = 512
SGU_GROUP_CH = SGU_WIDTH // SGU_GROUPS
SGU_CHUNK = 128

IN_COLS = ATT_WIDTH + 2 * KV_WIDTH + 2 * SGU_WIDTH + 2 * D_MODEL

X_HEADS = 4
X_HEAD_DIM = 128
X_WIDTH = X_HEADS * X_HEAD_DIM

N_GROUPS = 4
EXPERTS_PER_GROUP = 4
N_EXPERTS = N_GROUPS * EXPERTS_PER_GROUP
TOP_K = 2
D_EXPERT = 512

kernel_name = "hybrid_gated_gqa_gmlp_hmoe_encoder"


def rms_norm(x, g):
    xf = x.astype(jnp.float32)
    y = xf * lax.rsqrt(jnp.mean(xf * xf, axis=-1, keepdims=True) + EPS)
    return y.astype(x.dtype) * g


def layer_norm(x, g, b):
    xf = x.astype(jnp.float32)
    mu = jnp.mean(xf, axis=-1, keepdims=True)
    xc = xf - mu
    var = jnp.mean(xc * xc, axis=-1, keepdims=True)
    return (xc * lax.rsqrt(var + EPS)).astype(x.dtype) * g + b


def axial_rope_tables(seq_len):
    rows = seq_len // GRID_W
    row_ids = jnp.repeat(jnp.arange(rows), GRID_W).astype(jnp.float32)
    col_ids = jnp.tile(jnp.arange(GRID_W), rows).astype(jnp.float32)
    pairs_per_axis = ATT_HEAD_DIM // 4
    inv_freq = ROPE_THETA ** (-jnp.arange(pairs_per_axis, dtype=jnp.float32) / pairs_per_axis)
    ang = jnp.concatenate([row_ids[:, None] * inv_freq[None, :],
                           col_ids[:, None] * inv_freq[None, :]], axis=-1)
    return jnp.cos(ang), jnp.sin(ang)


def apply_rope(x, cos, sin):
    xf = x.astype(jnp.float32).reshape(*x.shape[:-1], x.shape[-1] // 2, 2)
    x0, x1 = xf[..., 0], xf[..., 1]
    c = cos[None, :, None, :]
    s = sin[None, :, None, :]
    out = jnp.stack([x0 * c - x1 * s, x0 * s + x1 * c], axis=-1).reshape(x.shape)
    return out.astype(x.dtype)


def gqa_attention(q, k, v):
    B, S, _, HD = q.shape
    G = ATT_HEADS // ATT_KV_HEADS
    nb = S // Q_BLOCK
    qb = q.reshape(B, nb, Q_BLOCK, ATT_KV_HEADS, G, HD).transpose(1, 0, 2, 3, 4, 5)
    scale = HD ** -0.5

    def block(q_blk):
        s = jnp.einsum('bqhgd,bkhd->bhgqk', q_blk, k, preferred_element_type=jnp.float32) * scale
        p = jax.nn.softmax(s, axis=-1).astype(v.dtype)
        return jnp.einsum('bhgqk,bkhd->bqhgd', p, v)

    o = lax.map(block, qb)
    return o.transpose(1, 0, 2, 3, 4, 5).reshape(B, S, ATT_WIDTH)


def spatial_gating(u, v, w_s, b_s, ln_g, ln_b):
    B, S, _ = v.shape
    nc = S // SGU_CHUNK
    vn = layer_norm(v, ln_g, ln_b).reshape(B, nc, SGU_CHUNK, SGU_GROUPS, SGU_GROUP_CH)
    vm = jnp.einsum('gpq,bnqgc->bnpgc', w_s, vn) + b_s.T[:, :, None]
    return u * vm.reshape(B, S, SGU_WIDTH)


def memory_cross_attention(h, mem_n, w_cq, w_ckv, w_co):
    B, S, _ = h.shape
    M = mem_n.shape[1]
    q = (h @ w_cq).reshape(B, S, X_HEADS, X_HEAD_DIM)
    kv = mem_n @ w_ckv
    k = kv[..., :X_WIDTH].reshape(B, M, X_HEADS, X_HEAD_DIM)
    v = kv[..., X_WIDTH:].reshape(B, M, X_HEADS, X_HEAD_DIM)
    s = jnp.einsum('bshd,bmhd->bhsm', q, k, preferred_element_type=jnp.float32) * (X_HEAD_DIM ** -0.5)
    p = jax.nn.softmax(s, axis=-1).astype(v.dtype)
    o = jnp.einsum('bhsm,bmhd->bshd', p, v).reshape(B, S, X_WIDTH)
    return o @ w_co


def hierarchical_moe(h, w_rg, b_rg, w_re, b_re, w_gate, w_up, w_down):
    B, S, D = h.shape
    t = h.reshape(B * S, D)
    glog = (t @ w_rg + b_rg).astype(jnp.float32)
    gprob = jax.nn.softmax(glog, axis=-1)
    gval, gidx = lax.top_k(gprob, 1)
    elog = (t @ w_re + b_re).astype(jnp.float32).reshape(-1, N_GROUPS, EXPERTS_PER_GROUP)
    elog_sel = jnp.take_along_axis(elog, gidx[:, :, None], axis=1)[:, 0]
    evals, eidx = lax.top_k(elog_sel, TOP_K)
    ew = jax.nn.softmax(evals, axis=-1) * gval
    expert_id = gidx * EXPERTS_PER_GROUP + eidx
    combine = jnp.sum(jax.nn.one_hot(expert_id, N_EXPERTS, dtype=jnp.float32) * ew[..., None], axis=1)
    combine = combine.astype(t.dtype)
    y = jnp.zeros_like(t)
    for e in range(N_EXPERTS):
        he = jax.nn.silu(t @ w_gate[e]) * (t @ w_up[e])
        y = y + combine[:, e:e + 1] * (he @ w_down[e])
    return y.reshape(B, S, D)


def setup_inputs(seed: int = 0) -> dict:
    key = jax.random.key(seed)
    ks = iter(jax.random.split(key, 40))
    L = DEPTH
    f32 = jnp.float32

    def nrm(shape, scale):
        return jax.random.normal(next(ks), shape, f32) * scale

    def gain(shape):
        return 1.0 + 0.1 * jax.random.normal(next(ks), shape, f32)

    return {
        'x': nrm((BATCH, SEQ, D_MODEL), 1.0),
        'mem': nrm((BATCH, N_MEM, D_MODEL), 1.0),
        'g_mix': gain((L, D_MODEL)),
        'w_in': nrm((L, D_MODEL, IN_COLS), D_MODEL ** -0.5),
        'g_q': gain((L, ATT_HEAD_DIM)),
        'g_k': gain((L, ATT_HEAD_DIM)),
        'w_att_out': nrm((L, ATT_WIDTH, D_MODEL), ATT_WIDTH ** -0.5),
        'sgu_ln_g': gain((L, SGU_WIDTH)),
        'sgu_ln_b': nrm((L, SGU_WIDTH), 0.02),
        'w_s': nrm((L, SGU_GROUPS, SGU_CHUNK, SGU_CHUNK), 0.5 * SGU_CHUNK ** -0.5),
        'b_s': 1.0 + nrm((L, SGU_GROUPS, SGU_CHUNK), 0.1),
        'w_sgu_out': nrm((L, SGU_WIDTH, D_MODEL), SGU_WIDTH ** -0.5),
        'w_out': nrm((L, D_MODEL, D_MODEL), D_MODEL ** -0.5),
        'g_cross': gain((L, D_MODEL)),
        'g_mem': gain((L, D_MODEL)),
        'w_cq': nrm((L, D_MODEL, X_WIDTH), D_MODEL ** -0.5),
        'w_ckv': nrm((L, D_MODEL, 2 * X_WIDTH), D_MODEL ** -0.5),
        'w_co': nrm((L, X_WIDTH, D_MODEL), X_WIDTH ** -0.5),
        'g_moe': gain((L, D_MODEL)),
        'w_rg': nrm((L, D_MODEL, N_GROUPS), D_MODEL ** -0.5),
        'b_rg': nrm((L, N_GROUPS), 0.01),
        'w_re': nrm((L, D_MODEL, N_EXPERTS), D_MODEL ** -0.5),
        'b_re': nrm((L, N_EXPERTS), 0.01),
        'w_gate': nrm((L, N_EXPERTS, D_MODEL, D_EXPERT), D_MODEL ** -0.5),
        'w_up': nrm((L, N_EXPERTS, D_MODEL, D_EXPERT), D_MODEL ** -0.5),
        'w_down': nrm((L, N_EXPERTS, D_EXPERT, D_MODEL), D_EXPERT ** -0.5),
        'g_final': gain((D_MODEL,)),
    }


def reference(x, mem, g_mix, w_in, g_q, g_k, w_att_out, sgu_ln_g, sgu_ln_b, w_s, b_s,
              w_sgu_out, w_out, g_cross, g_mem, w_cq, w_ckv, w_co, g_moe, w_rg, b_rg,
              w_re, b_re, w_gate, w_up, w_down, g_final):
    B, S, _ = x.shape
    cos, sin = axial_rope_tables(S)
    splits = [ATT_WIDTH,
              ATT_WIDTH + KV_WIDTH,
              ATT_WIDTH + 2 * KV_WIDTH,
              ATT_WIDTH + 2 * KV_WIDTH + SGU_WIDTH,
              ATT_WIDTH + 2 * KV_WIDTH + 2 * SGU_WIDTH,
              ATT_WIDTH + 2 * KV_WIDTH + 2 * SGU_WIDTH + D_MODEL]
    for l in range(DEPTH):
        h = rms_norm(x, g_mix[l])
        z = h @ w_in[l]
        q, k, v, su, sv, ga, gb = jnp.split(z, splits, axis=-1)
        q = rms_norm(q.reshape(B, S, ATT_HEADS, ATT_HEAD_DIM), g_q[l])
        k = rms_norm(k.reshape(B, S, ATT_KV_HEADS, ATT_HEAD_DIM), g_k[l])
        q = apply_rope(q, cos, sin)
        k = apply_rope(k, cos, sin)
        v = v.reshape(B, S, ATT_KV_HEADS, ATT_HEAD_DIM)
        y_att = gqa_attention(q, k, v) @ w_att_out[l]
        y_sgu = spatial_gating(jax.nn.gelu(su), jax.nn.gelu(sv), w_s[l], b_s[l],
                               sgu_ln_g[l], sgu_ln_b[l]) @ w_sgu_out[l]
        merged = jax.nn.sigmoid(ga) * y_att + jax.nn.sigmoid(gb) * y_sgu
        x = x + merged @ w_out[l]
        x = x + memory_cross_attention(rms_norm(x, g_cross[l]), rms_norm(mem, g_mem[l]),
                                       w_cq[l], w_ckv[l], w_co[l])
        x = x + hierarchical_moe(rms_norm(x, g_moe[l]), w_rg[l], b_rg[l], w_re[l], b_re[l],
                                 w_gate[l], w_up[l], w_down[l])
    return rms_norm(x, g_final)
```

```python
import contextlib
import numpy as np
import concourse.bass as bass
import concourse.mybir as mybir
from concourse.bass_utils import run_bass_kernel_spmd

F32 = mybir.dt.float32
BF16 = mybir.dt.bfloat16
AF = mybir.ActivationFunctionType
ALU = mybir.AluOpType
AX = mybir.AxisListType

D = 1024
SEQ = 8192
OWN = 4096
NMEM = 256
EPS = 1e-6
NE = 16
BIG = 1.0e30


class Buf:
    __slots__ = ("name", "w", "r", "psum")

    def __init__(self, name="", psum=False):
        self.name = name
        self.psum = psum
        self.w = None
        self.r = {}


class Eng:
    def __init__(self, name):
        self.name = name
        self.sem = None
        self.count = 0
        self.seen = {}
        self.prog = []
        self.chsems = []
        self.dma_i = 0


class Prog:
    NCH = 8

    def __init__(self, nc, stack):
        self.nc = nc
        self.E = {}
        for n in ("pe", "act", "dve", "pool", "sp"):
            e = Eng(n)
            e.sem = stack.enter_context(nc.semaphore("sem_" + n))
            self.E[n] = e
        for n in ("sp", "pool", "act"):
            e = self.E[n]
            for i in range(self.NCH):
                e.chsems.append(stack.enter_context(nc.semaphore("ch_%s_%d" % (n, i))))
        self.chan_issued = {}
        import os
        self.limit = int(os.environ.get("OPLIMIT", 10 ** 9))
        self.nops = 0

    def _need(self, need, ev):
        sem, val, src = ev
        k = id(sem)
        if k not in need or need[k][1] < val:
            need[k] = (sem, val, src)

    def _deps(self, eng, reads, writes, extra=()):
        E = self.E[eng]
        need = {}
        for b in reads:
            if b.w is not None:
                self._need(need, b.w)
        for b in writes:
            if b.w is not None:
                self._need(need, b.w)
            for k, (sem, val, src) in b.r.items():
                self._need(need, (sem, val, src))
        for ev in extra:
            self._need(need, ev)
        for k, (sem, val, src) in need.items():
            if src == "pe" and eng == "pe":
                continue
            if E.seen.get(k, 0) >= val:
                continue
            E.seen[k] = val
            E.prog.append(("wait", sem, val))

    def _commit(self, ev, reads, writes):
        sem, val, src = ev
        for b in writes:
            b.w = ev
            b.r = {}
        for b in reads:
            if b not in writes:
                b.r[id(sem)] = (sem, val, src)

    def op(self, eng, fns, reads=(), writes=()):
        if not isinstance(fns, (list, tuple)):
            fns = [fns]
        self.nops += 1
        if self.nops > self.limit:
            return
        pr = [b for b in reads if b.psum]
        if pr:
            reads = [b for b in reads if not b.psum]
            writes = list(writes) + [b for b in pr if b not in writes]
        E = self.E[eng]
        self._deps(eng, reads, writes)
        E.count += 1
        for f in fns[:-1]:
            E.prog.append(("raw", f))
        E.prog.append(("op", fns[-1], E.sem))
        self._commit((E.sem, E.count, eng), reads, writes)

    def dma(self, q, fn, reads=(), writes=()):
        self.nops += 1
        if self.nops > self.limit:
            return
        E = self.E[q]
        ch = E.dma_i % self.NCH
        rnd = E.dma_i // self.NCH
        E.dma_i += 1
        chsem = E.chsems[ch]
        extra = []
        if rnd > 0:
            extra.append((chsem, 16 * rnd, "dma"))
        self._deps(q, reads, writes, extra)
        E.prog.append(("dma", fn, chsem))
        self.chan_issued[id(chsem)] = (chsem, 16 * (rnd + 1))
        self._commit((chsem, 16 * (rnd + 1), "dma"), reads, writes)

    def barrier(self):
        for n, E in self.E.items():
            for m, F in self.E.items():
                if m == n or F.count == 0:
                    continue
                k = id(F.sem)
                if E.seen.get(k, 0) < F.count:
                    E.seen[k] = F.count
                    E.prog.append(("wait", F.sem, F.count))
            for k, (sem, val) in self.chan_issued.items():
                if E.seen.get(k, 0) < val:
                    E.seen[k] = val
                    E.prog.append(("wait", sem, val))

    def flush(self):
        self.barrier()
        nc = self.nc
        progs = {n: E.prog for n, E in self.E.items()}
        for E in self.E.values():
            E.prog = []

        def replay(eng, prog):
            for it in prog:
                if it[0] == "wait":
                    eng.wait_ge(it[1], it[2])
                elif it[0] == "raw":
                    it[1](eng)
                elif it[0] == "op":
                    it[1](eng).then_inc(it[2], 1)
                else:
                    it[1](eng).then_inc(it[2], 16)

        with nc.Block() as block:
            @block.tensor
            def _(e):
                replay(e, progs["pe"])

            @block.scalar
            def _(e):
                replay(e, progs["act"])

            @block.vector
            def _(e):
                replay(e, progs["dve"])

            @block.gpsimd
            def _(e):
                replay(e, progs["pool"])

            @block.sync
            def _(e):
                replay(e, progs["sp"])


class Rot:
    def __init__(self, items):
        self.items = items
        self.i = 0

    def next(self):
        it = self.items[self.i % len(self.items)]
        self.i += 1
        return it


def build(dbg=False):
    nc = bass.Bass("TRN2", target_bir_lowering=False)

    def din(name, shape):
        return nc.dram_tensor(name, list(shape), F32, kind="ExternalInput").ap()

    xseq = din("xseq", [SEQ, D])
    xown = din("xown", [OWN, D])
    memd = din("mem", [NMEM, D])
    cos_seq = din("cos_seq", [128, 64, 32])
    sin_seq = din("sin_seq", [128, 64, 32])
    cos_own = din("cos_own", [128, 32, 32])
    sin_own = din("sin_own", [128, 32, 32])
    identd = din("ident", [128, 128])
    g_mix = din("g_mix", [D])
    w_in = din("w_in", [D, 3840])
    g_q = din("g_q", [64])
    g_k = din("g_k", [64])
    w_att_out = din("w_att_out", [512, D])
    sgu_ln_g = din("sgu_ln_g", [512])
    sgu_ln_b = din("sgu_ln_b", [512])
    w_s = din("w_s", [4, 128, 128])
    b_s = din("b_s", [4, 128])
    w_sgu_out = din("w_sgu_out", [512, D])
    w_out = din("w_out", [D, D])
    g_cross = din("g_cross", [D])
    g_mem = din("g_mem", [D])
    w_cq = din("w_cq", [D, 512])
    w_ckv = din("w_ckv", [D, D])
    w_co = din("w_co", [512, D])
    g_moe = din("g_moe", [D])
    w_rg = din("w_rg", [D, 4])
    b_rg = din("b_rg", [4])
    w_re = din("w_re", [D, 16])
    b_re = din("b_re", [16])
    w_gate = din("w_gate", [NE, D, 512])
    w_up = din("w_up", [NE, D, 512])
    w_down = din("w_down", [NE, 512, D])
    g_final = din("g_final", [D])
    out = nc.dram_tensor("out", [OWN, D], F32, kind="ExternalOutput").ap()
    yatt_d = nc.dram_tensor("yatt_scr", [8, 128, 8, 512], F32, kind="Internal").ap()
    x2_d = nc.dram_tensor("x2_scr", [OWN, D], F32, kind="Internal").ap()

    def wv(w, c0, c1):
        return w.rearrange("(c p) n -> p c n", p=128)[:, :, c0:c1]

    def gT_view(g):
        return g.rearrange("(c p) -> p c", p=128)

    with contextlib.ExitStack() as top:
        P = Prog(nc, top)
        print("sbuf bytes remaining at start:", nc.sbuf_bytes_remaining)

        def sb(stack, name, shape, dt=F32):
            return stack.enter_context(nc.sbuf_tensor("s_" + name, list(shape), dt))

        def ps(stack, name, shape, dt=F32):
            return stack.enter_context(nc.psum_tensor("p_" + name, list(shape), dt))

        ident = sb(top, "ident", [128, 128], BF16)
        kcT = sb(top, "kcT", [128, 4, NMEM], BF16)
        Vc = sb(top, "Vc", [128, 2, 512], BF16)
        gmixT = sb(top, "gmixT", [128, 8])
        attn_stack = contextlib.ExitStack()
        kT_all = sb(attn_stack, "kT_all", [128, SEQ], BF16)
        V_all = sb(attn_stack, "V_all", [128, 64, 2, 65], BF16)
        B_const = Buf("const")

        P.dma("pool", lambda e: e.dma_start(out=ident[:], in_=identd[:, :]), [], [B_const])
        P.dma("sp", lambda e: e.dma_start(out=gmixT[:], in_=gT_view(g_mix), allow_slow_non_contiguous=True), [], [B_const])
        P.op("pool", lambda e: e.memset(V_all[:], 1.0), [], [B_const])

        def norm_T(st, xt_ap, xbuf, gT, out_ap3, outbuf, tps, tpsb, act_copy_eng="act"):
            P.op("act", lambda e: e.activation(out=st["junk"][:], in_=xt_ap, func=AF.Square, scale=1.0 / 32,
                                               accum_out=st["ss"][:, 0:1]), [xbuf], [st["bjunk"], st["bss"]])
            P.op("dve", lambda e: e.tensor_scalar(out=st["ss"][:, 1:2], in0=st["ss"][:, 0:1], scalar1=EPS, scalar2=None,
                                                  op0=ALU.add), [st["bss"]], [st["bss1"]])
            P.op("act", lambda e: e.activation(out=st["ss"][:, 2:3], in_=st["ss"][:, 1:2], func=AF.Ln), [st["bss1"]], [st["bss2"]])
            P.op("act", lambda e: e.activation(out=st["ss"][:, 3:4], in_=st["ss"][:, 2:3], func=AF.Exp, scale=-0.5),
                 [st["bss2"]], [st["bss3"]])
            xb, bxb = st["xb"].next()
            P.op("act", lambda e: e.activation(out=xb[:], in_=xt_ap, func=AF.Copy, scale=st["ss"][:, 3:4]),
                 [xbuf, st["bss3"]], [bxb])
            P.op("pe", [(lambda e, c=c: e.transpose(tps[:, c, :], xb[:, c * 128:(c + 1) * 128], ident[:])) for c in range(8)],
                 [bxb], [tpsb])
            P.op("dve", lambda e: e.tensor_tensor(out=out_ap3, in0=tps[:], in1=gT[:, 0:8].unsqueeze(2).broadcast_to([128, 8, 128]),
                                                  op=ALU.mult), [tpsb], [outbuf])

        def norm_state(stack, pfx):
            st = {}
            st["junk"] = sb(stack, pfx + "junk", [128, 1024], BF16)
            st["ss"] = sb(stack, pfx + "ss", [128, 4])
            for k in ("bjunk", "bss", "bss1", "bss2", "bss3"):
                st[k] = Buf(k)
            st["xb"] = Rot([(sb(stack, pfx + "xb%d" % i, [128, 1024], BF16), Buf("xb")) for i in range(2)])
            return st

        def head_norm_rope(stack_t, src_ps, bsrc, H, g_bc, cosb, sinb, dst, bdst, t):
            w = stack_t
            n = H * 64
            src3 = src_ps.rearrange("p (h d) -> p h d", h=H)
            P.op("act", lambda e: e.activation(out=w["sq"][:, 0:n], in_=src_ps, func=AF.Square, scale=0.125), [bsrc], [w["bsq"]])
            P.op("dve", lambda e: e.tensor_reduce(out=w["hs"][:, 0:H], in_=w["sq"][:, 0:n].rearrange("p (h d) -> p h d", h=H),
                                                  axis=AX.X, op=ALU.add), [w["bsq"]], [w["bhs"]])
            P.op("dve", lambda e: e.tensor_scalar(out=w["hs"][:, 8:8 + H], in0=w["hs"][:, 0:H], scalar1=EPS, scalar2=None, op0=ALU.add),
                 [w["bhs"]], [w["bhs1"]])
            P.op("act", lambda e: e.activation(out=w["hs"][:, 16:16 + H], in_=w["hs"][:, 8:8 + H], func=AF.Ln), [w["bhs1"]], [w["bhs2"]])
            P.op("act", lambda e: e.activation(out=w["hs"][:, 24:24 + H], in_=w["hs"][:, 16:16 + H], func=AF.Exp, scale=-0.5),
                 [w["bhs2"]], [w["bhs3"]])
            qn3 = w["qn"][:, 0:n].rearrange("p (h d) -> p h d", h=H)
            P.op("dve", lambda e: e.tensor_tensor(out=qn3, in0=src3, in1=w["hs"][:, 24:24 + H].unsqueeze(2).broadcast_to([128, H, 64]),
                                                  op=ALU.mult), [bsrc, w["bhs3"]], [w["bqn"]])
            P.op("dve", lambda e: e.tensor_tensor(out=qn3, in0=qn3, in1=g_bc[:, :].unsqueeze(1).broadcast_to([128, H, 64]),
                                                  op=ALU.mult), [w["bqn"]], [w["bqn"]])
            qn4 = w["qn"][:, 0:n].rearrange("p (h i t) -> p h i t", h=H, t=2)
            A4 = w["A"][:, 0:n].rearrange("p (h i t) -> p h i t", h=H, t=2)
            B4 = w["B"][:, 0:n].rearrange("p (h i t) -> p h i t", h=H, t=2)
            c4 = cosb[:, t, :].unsqueeze(1).unsqueeze(3).broadcast_to([128, H, 32, 2])
            s4 = sinb[:, t, :].unsqueeze(1).unsqueeze(3).broadcast_to([128, H, 32, 2])
            P.op("dve", lambda e: e.tensor_tensor(out=A4, in0=qn4, in1=c4, op=ALU.mult), [w["bqn"]], [w["bA"]])
            P.op("dve", lambda e: e.tensor_tensor(out=B4, in0=qn4, in1=s4, op=ALU.mult), [w["bqn"]], [w["bB"]])
            P.op("dve", lambda e: e.tensor_tensor(out=dst[:, :, :, 0], in0=A4[:, :, :, 0], in1=B4[:, :, :, 1], op=ALU.subtract),
                 [w["bA"], w["bB"]], [bdst])
            P.op("dve", lambda e: e.tensor_tensor(out=dst[:, :, :, 1], in0=B4[:, :, :, 0], in1=A4[:, :, :, 1], op=ALU.add),
                 [w["bA"], w["bB"]], [bdst])

        def rope_state(stack, pfx, n):
            w = {}
            for k in ("sq", "qn", "A", "B"):
                w[k] = sb(stack, pfx + k, [128, n])
            w["hs"] = sb(stack, pfx + "hs", [128, 32])
            for k in ("bsq", "bhs", "bhs1", "bhs2", "bhs3", "bqn", "bA", "bB"):
                w[k] = Buf(k)
            return w

        with contextlib.ExitStack() as ph:
            wkv = sb(ph, "wkv", [128, 8, 256], BF16)
            cosb = sb(ph, "cosb", [128, 64, 32])
            sinb = sb(ph, "sinb", [128, 64, 32])
            gk_bc = sb(ph, "gk_bc", [128, 64])
            P.dma("pool", lambda e: e.dma_start(out=wkv[:], in_=wv(w_in, 512, 768)), [], [B_const])
            P.dma("sp", lambda e: e.dma_start(out=cosb[:], in_=cos_seq[:, :, :]), [], [B_const])
            P.dma("sp", lambda e: e.dma_start(out=sinb[:], in_=sin_seq[:, :, :]), [], [B_const])
            P.dma("sp", lambda e: e.dma_start(out=gk_bc[:], in_=g_k.partition_broadcast(128)), [], [B_const])
            st = norm_state(ph, "p1")
            rw = rope_state(ph, "p1r", 128)
            xts = Rot([(sb(ph, "p1x%d" % i, [128, 1024]), Buf("x")) for i in range(3)])
            hTs = Rot([(sb(ph, "p1h%d" % i, [128, 8, 128], BF16), Buf("h")) for i in range(2)])
            krs = Rot([(sb(ph, "p1kr%d" % i, [128, 2, 32, 2], BF16), Buf("kr")) for i in range(2)])
            tpss = Rot([(ps(ph, "p1tp%d" % i, [128, 8, 128], BF16), Buf("tp", True)) for i in range(2)])
            kvps = Rot([(ps(ph, "p1kv%d" % i, [128, 512]), Buf("kv", True)) for i in range(2)])
            ktps = Rot([(ps(ph, "p1kt%d" % i, [128, 1024], BF16), Buf("kt", True)) for i in range(2)])
            import os as _os
            for t in range(int(_os.environ.get('NT1', 64))):
                xt, bx = xts.next()
                P.dma("sp", lambda e, xt=xt, t=t: e.dma_start(out=xt[:], in_=xseq[t * 128:(t + 1) * 128, :]), [], [bx])
                hT, bh = hTs.next()
                tp, btp = tpss.next()
                norm_T(st, xt[:], bx, gmixT, hT[:], bh, tp, btp)
                kv, bkv = kvps.next()
                P.op("pe", [(lambda e, c=c, kv=kv, hT=hT: e.matmul(kv[:, 0:256], hT[:, c, :], wkv[:, c, :], start=(c == 0), stop=(c == 7)))
                            for c in range(8)], [bh, B_const], [bkv])
                kr, bkr = krs.next()
                head_norm_rope(rw, kv[:, 0:128], bkv, 2, gk_bc, cosb, sinb, kr, bkr, t)
                P.op("act", lambda e, kv=kv, t=t: e.activation(out=V_all[:, t, :, 0:64], in_=kv[:, 128:256].rearrange("p (h d) -> p h d", h=2),
                                                               func=AF.Copy), [bkv], [])
                kt, bkt = ktps.next()
                P.op("pe", lambda e, kt=kt, kr=kr: e.transpose(kt[:, 0:128], kr[:].rearrange("p h i t -> p (h i t)"), ident[:]), [bkr], [bkt])
                P.op("act", lambda e, kt=kt, t=t: e.activation(out=kT_all[:, t * 128:(t + 1) * 128], in_=kt[:, 0:128], func=AF.Copy), [bkt], [])
            P.flush()

        if dbg == 1:
            P.limit = 10 ** 9
            P.limit = 10 ** 9
            print("nops", P.nops)
            dk = nc.dram_tensor("dbg_kT", [128, SEQ], BF16, kind="ExternalOutput").ap()
            dv = nc.dram_tensor("dbg_V", [128, 64 * 2 * 65], BF16, kind="ExternalOutput").ap()
            P.dma("sp", lambda e: e.dma_start(out=dk[:, :], in_=kT_all[:]), [], [])
            P.dma("sp", lambda e: e.dma_start(out=dv[:, :], in_=V_all[:].rearrange("p a b c -> p (a b c)")), [], [])
            P.flush()
            attn_stack.close()
            return nc

        with contextlib.ExitStack() as ph:
            wq = sb(ph, "wq", [128, 8, 512], BF16)
            wao = sb(ph, "wao", [64, 8, 1024], BF16)
            cosb = sb(ph, "cosb2", [128, 32, 32])
            sinb = sb(ph, "sinb2", [128, 32, 32])
            gq_bc = sb(ph, "gq_bc", [128, 64])
            ones65 = sb(ph, "ones65", [65, 64])
            B_c2 = Buf("c2")
            for hd_ in range(8):
                pos_ = (hd_ % 4) * 2 + hd_ // 4
                P.dma("pool", lambda e, hd_=hd_, pos_=pos_: e.dma_start(out=wq[:, :, pos_ * 64:(pos_ + 1) * 64], in_=wv(w_in, hd_ * 64, hd_ * 64 + 64)), [], [B_c2])
            P.dma("pool", lambda e: e.dma_start(out=wao[:], in_=w_att_out.rearrange("(h p) n -> p h n", p=64)), [], [B_c2])
            P.dma("sp", lambda e: e.dma_start(out=cosb[:], in_=cos_own[:, :, :]), [], [B_c2])
            P.dma("sp", lambda e: e.dma_start(out=sinb[:], in_=sin_own[:, :, :]), [], [B_c2])
            P.dma("sp", lambda e: e.dma_start(out=gq_bc[:], in_=g_q.partition_broadcast(128)), [], [B_c2])
            P.op("dve", lambda e: e.tensor_scalar(out=gq_bc[:], in0=gq_bc[:], scalar1=0.125, scalar2=None, op0=ALU.mult), [B_c2], [B_c2])
            P.op("pool", lambda e: e.memset(ones65[:], 1.0), [], [B_c2])
            st = norm_state(ph, "p2")
            rw = rope_state(ph, "p2r", 512)
            xts = Rot([(sb(ph, "p2x%d" % i, [128, 1024]), Buf("x")) for i in range(2)])
            hTs = Rot([(sb(ph, "p2h%d" % i, [128, 8, 128], BF16), Buf("h")) for i in range(2)])
            qrs = Rot([(sb(ph, "p2qr%d" % i, [128, 8, 32, 2], BF16), Buf("qr")) for i in range(2)])
            qTs = Rot([(sb(ph, "p2qT%d" % i, [128, 4, 512], BF16), [Buf("qT") for _ in range(4)]) for i in range(2)])
            Ps = Rot([(sb(ph, "p2P%d" % i, [128, 2, 512], BF16), Buf("P")) for i in range(3)])
            Osbs = Rot([(sb(ph, "p2Osb%d" % i, [65, 512]), Buf("Osb")) for i in range(2)])
            rrs = Rot([(sb(ph, "p2rr%d" % i, [65, 512]), Buf("rr")) for i in range(2)])
            OnTs = Rot([(sb(ph, "p2OnT%d" % i, [64, 8, 512], BF16), [Buf("OnT") for _ in range(8)]) for i in range(2)])
            ysbs = Rot([(sb(ph, "p2ysb%d" % i, [128, 8, 512]), [Buf("ysb") for _ in range(8)]) for i in range(1)])
            Sps = Rot([(ps(ph, "p2S%d" % i, [128, 2, 512]), Buf("S", True)) for i in range(2)])
            Ops, bO = ps(ph, "p2O", [128, 2, 512]), [Buf("O0", True), Buf("O1", True)]
            miscA, bmA = ps(ph, "p2mA", [128, 512]), Buf("mA", True)
            miscB, bmB = ps(ph, "p2mB", [128, 8, 128], BF16), Buf("mB", True)
            for gi in range(8):
                qT, bqT = qTs.next()
                for j in range(4):
                    tile = gi * 4 + j
                    xt, bx = xts.next()
                    P.dma("sp", lambda e, xt=xt, tile=tile: e.dma_start(out=xt[:], in_=xown[tile * 128:(tile + 1) * 128, :]), [], [bx])
                    hT, bh = hTs.next()
                    norm_T(st, xt[:], bx, gmixT, hT[:], bh, miscB, bmB)
                    P.op("pe", [(lambda e, c=c, hT=hT: e.matmul(miscA[:], hT[:, c, :], wq[:, c, :], start=(c == 0), stop=(c == 7)))
                                for c in range(8)], [bh, B_c2], [bmA])
                    qr, bqr = qrs.next()
                    head_norm_rope(rw, miscA[:], bmA, 8, gq_bc, cosb, sinb, qr, bqr, tile)
                    qr3 = qr[:].rearrange("p h i t -> p (h i t)")
                    P.op("pe", [(lambda e, g=g, qr3=qr3: e.transpose(miscB[:, g, :], qr3[:, g * 128:(g + 1) * 128], ident[:])) for g in range(4)],
                         [bqr], [bmB])
                    P.op("act", lambda e, qT=qT, j=j: e.activation(out=qT[:, :, j * 128:(j + 1) * 128], in_=miscB[:, 0:4, :], func=AF.Copy),
                         [bmB], [bqT[j]])
                OnT, bOn = OnTs.next()
                for g in range(4):
                    def issue_S(kt, g=g, qT=qT):
                        S, bS = Sps.next()
                        P.op("pe", [lambda e, S=S, kt=kt: e.matmul(S[:, 0, :], kT_all[0:64, kt * 128:(kt + 1) * 128], qT[0:64, g, :], start=True, stop=True),
                                    lambda e, S=S, kt=kt: e.matmul(S[:, 1, :], kT_all[64:128, kt * 128:(kt + 1) * 128], qT[64:128, g, :], start=True, stop=True)],
                             bqT, [bS])
                        return S, bS
                    cur = issue_S(0)
                    for kt in range(64):
                        nxt = issue_S(kt + 1) if kt < 63 else None
                        S, bS = cur
                        Pt, bP = Ps.next()
                        P.op("act", lambda e, S=S, Pt=Pt: e.activation(out=Pt[:], in_=S[:], func=AF.Exp), [bS], [bP])
                        P.op("pe", [lambda e, Pt=Pt, kt=kt: e.matmul(Ops[0:65, 0, :], V_all[:, kt, 0, :], Pt[:, 0, :], start=(kt == 0), stop=(kt == 63)),
                                    lambda e, Pt=Pt, kt=kt: e.matmul(Ops[0:65, 1, :], V_all[:, kt, 1, :], Pt[:, 1, :], start=(kt == 0), stop=(kt == 63))],
                             [bP], bO)
                        cur = nxt
                    for hh in range(2):
                        head = g + 4 * hh
                        Osb, bOsb = Osbs.next()
                        rr, brr = rrs.next()
                        P.op("act", lambda e, Osb=Osb, hh=hh: e.activation(out=Osb[:], in_=Ops[0:65, hh, :], func=AF.Copy), [bO[hh]], [bOsb])
                        P.op("dve", lambda e, Osb=Osb, rr=rr: e.reciprocal(out=rr[64:65, :], in_=Osb[64:65, :]), [bOsb], [brr])
                        P.op("pe", lambda e, rr=rr: e.matmul(miscA[0:64, :], ones65[64:65, 0:64], rr[64:65, :], start=True, stop=True), [brr, B_c2], [bmA])
                        P.op("dve", lambda e, Osb=Osb, OnT=OnT, head=head: e.tensor_tensor(out=OnT[:, head, :], in0=Osb[0:64, :], in1=miscA[0:64, :], op=ALU.mult),
                             [bOsb, bmA], [bOn[head]])
                ysb, bys = ysbs.next()
                for dc in range(8):
                    P.op("pe", [(lambda e, h=h, dc=dc, OnT=OnT: e.matmul(miscA[:], wao[:, h, dc * 128:(dc + 1) * 128], OnT[:, h, :], start=(h == 0), stop=(h == 7)))
                                for h in range(8)], bOn + [B_c2], [bmA])
                    P.op("act", lambda e, ysb=ysb, dc=dc: e.activation(out=ysb[:, dc, :], in_=miscA[:], func=AF.Copy), [bmA], [bys[dc]])
                P.dma("sp", lambda e, ysb=ysb, gi=gi: e.dma_start(out=yatt_d[gi], in_=ysb[:]), bys, [])
            P.flush()

        attn_stack.close()
        if dbg == 2:
            dy = nc.dram_tensor("dbg_yatt", [8, 128, 8, 512], F32, kind="ExternalOutput").ap()
            P.dma("sp", lambda e: e.dma_start(out=dy.rearrange("a p c t -> (a p) (c t)"), in_=yatt_d.rearrange("a p c t -> (a p) (c t)")), [], [])
            P.flush()
            return nc

        with contextlib.ExitStack() as ph:
            wckv = sb(ph, "wckv", [128, 8, 1024], BF16)
            gmemT = sb(ph, "gmemT", [128, 8])
            memT = sb(ph, "memT", [128, 8, 256], BF16)
            B_cm = Buf("cm")
            P.dma("pool", lambda e: e.dma_start(out=wckv[:], in_=wv(w_ckv, 0, 1024)), [], [B_cm])
            P.dma("sp", lambda e: e.dma_start(out=gmemT[:], in_=gT_view(g_mem), allow_slow_non_contiguous=True), [], [B_cm])
            st = norm_state(ph, "pm")
            xts = Rot([(sb(ph, "pmx%d" % i, [128, 1024]), Buf("x")) for i in range(2)])
            tp, btp = ps(ph, "pmtp", [128, 8, 128], BF16), Buf("tp", True)
            mps = Rot([(ps(ph, "pmps%d" % i, [128, 512]), Buf("mps", True)) for i in range(2)])
            bmemT = [Buf("memT0"), Buf("memT1")]
            for mt in range(2):
                xt, bx = xts.next()
                P.dma("sp", lambda e, xt=xt, mt=mt: e.dma_start(out=xt[:], in_=memd[mt * 128:(mt + 1) * 128, :]), [], [bx])
                norm_T(st, xt[:], bx, gmemT, memT[:, :, mt * 128:(mt + 1) * 128], bmemT[mt], tp, btp)
            for h in range(4):
                mp, bmp = mps.next()
                P.op("pe", [(lambda e, c=c, h=h, mp=mp: e.matmul(mp[:, 0:256], wckv[:, c, h * 128:(h + 1) * 128], memT[:, c, :], start=(c == 0), stop=(c == 7)))
                            for c in range(8)], bmemT + [B_cm], [bmp])
                P.op("act", lambda e, h=h, mp=mp: e.activation(out=kcT[:, h, :], in_=mp[:, 0:256], func=AF.Copy), [bmp], [])
            for mt in range(2):
                mp, bmp = mps.next()
                P.op("pe", [(lambda e, c=c, mt=mt, mp=mp: e.matmul(mp[:], memT[:, c, mt * 128:(mt + 1) * 128], wckv[:, c, 512:1024], start=(c == 0), stop=(c == 7)))
                            for c in range(8)], bmemT + [B_cm], [bmp])
                P.op("act", lambda e, mt=mt, mp=mp: e.activation(out=Vc[:, mt, :], in_=mp[:], func=AF.Copy), [bmp], [])
            P.flush()

        with contextlib.ExitStack() as ph:
            wsv = sb(ph, "wsv", [128, 8, 512], BF16)
            wsu = sb(ph, "wsu", [128, 8, 512], BF16)
            wga = sb(ph, "wga", [128, 8, 1024], BF16)
            wgb = sb(ph, "wgb", [128, 8, 1024], BF16)
            wso = sb(ph, "wso", [128, 4, 1024], BF16)
            wo = sb(ph, "wo", [128, 8, 1024], BF16)
            wcq = sb(ph, "wcq", [128, 8, 512], BF16)
            wco = sb(ph, "wco", [128, 4, 1024], BF16)
            gcrT = sb(ph, "gcrT", [128, 8])
            lng_bc = sb(ph, "lng_bc", [128, 512])
            lnb_bc = sb(ph, "lnb_bc", [128, 512])
            bs_bc = sb(ph, "bs_bc", [128, 4, 128])
            wsraw = sb(ph, "wsraw", [128, 4, 128], BF16)
            WsT = sb(ph, "WsT", [128, 4, 128], BF16)
            B_c = Buf("c2b")
            B_ws = Buf("ws")
            P.dma("pool", lambda e: e.dma_start(out=wsu[:], in_=wv(w_in, 768, 1280)), [], [B_c])
            P.dma("pool", lambda e: e.dma_start(out=wsv[:], in_=wv(w_in, 1280, 1792)), [], [B_c])
            P.dma("pool", lambda e: e.dma_start(out=wga[:], in_=wv(w_in, 1792, 2816)), [], [B_c])
            P.dma("pool", lambda e: e.dma_start(out=wgb[:], in_=wv(w_in, 2816, 3840)), [], [B_c])
            P.dma("pool", lambda e: e.dma_start(out=wso[:], in_=wv(w_sgu_out, 0, 1024)), [], [B_c])
            P.dma("pool", lambda e: e.dma_start(out=wo[:], in_=wv(w_out, 0, 1024)), [], [B_c])
            P.dma("pool", lambda e: e.dma_start(out=wcq[:], in_=wv(w_cq, 0, 512)), [], [B_c])
            P.dma("pool", lambda e: e.dma_start(out=wco[:], in_=wv(w_co, 0, 1024)), [], [B_c])
            P.dma("pool", lambda e: e.dma_start(out=wsraw[:], in_=w_s.rearrange("g p q -> p g q")), [], [B_ws])
            P.dma("sp", lambda e: e.dma_start(out=gcrT[:], in_=gT_view(g_cross), allow_slow_non_contiguous=True), [], [B_c])
            P.dma("sp", lambda e: e.dma_start(out=lng_bc[:], in_=sgu_ln_g.partition_broadcast(128)), [], [B_c])
            P.dma("sp", lambda e: e.dma_start(out=lnb_bc[:], in_=sgu_ln_b.partition_broadcast(128)), [], [B_c])
            P.dma("sp", lambda e: e.dma_start(out=bs_bc[:].rearrange("p g q -> p (g q)"), in_=b_s.rearrange("g q -> (g q)").partition_broadcast(128)), [], [B_c])
            st = norm_state(ph, "p3")
            tpb, btpb = ps(ph, "p3tp", [128, 8, 128], BF16), Buf("tp", True)
            gen = Rot([(ps(ph, "p3g%d" % i, [128, 512]), Buf("g", True)) for i in range(5)])
            scps, bsc = ps(ph, "p3sc", [128, 4, 256]), Buf("sc", True)
            P.op("pe", [(lambda e, g=g: e.transpose(tpb[:, g, :], wsraw[:, g, :], ident[:])) for g in range(4)], [B_ws], [btpb])
            P.op("act", lambda e: e.activation(out=WsT[:], in_=tpb[:, 0:4, :], func=AF.Copy), [btpb], [B_c])

            xg = [(sb(ph, "p3x%d" % i, [128, 1024]), Buf("x")) for i in range(4)]
            hTg, bhT = sb(ph, "p3hT", [128, 8, 512], BF16), [Buf("hT") for _ in range(4)]
            hcT, bhc = hTg, bhT
            gv, bgv = sb(ph, "p3gv", [128, 512]), Buf("gv")
            lst, blst = sb(ph, "p3lst", [128, 16]), [Buf("l%d" % i) for i in range(8)]
            vn, bvn = sb(ph, "p3vn", [128, 512], BF16), Buf("vn")
            vnf, bvnf = sb(ph, "p3vnf", [128, 512]), Buf("vnf")
            vmb, bvmb = sb(ph, "p3vmb", [128, 4, 512]), [Buf("vmb") for _ in range(4)]
            gu, bgu = sb(ph, "p3gu", [128, 512]), Buf("gu")
            sguT, bsg = sb(ph, "p3sguT", [128, 4, 512], BF16), [Buf("sg") for _ in range(4)]
            yatts = Rot([(sb(ph, "p3yatt%d" % i, [128, 512]), Buf("ya")) for i in range(2)])
            sa, bsa = sb(ph, "p3sa", [128, 512]), Buf("sa")
            sbt, bsb = sb(ph, "p3sb", [128, 512]), Buf("sb")
            m1, bm1 = sb(ph, "p3m1", [128, 512]), Buf("m1")
            m2, bm2 = sb(ph, "p3m2", [128, 512]), Buf("m2")
            mrg, bmrg = sb(ph, "p3mrg", [128, 8, 512], BF16), [Buf("mrg") for _ in range(8)]
            qcT, bqc = sb(ph, "p3qcT", [128, 4, 512], BF16), [Buf("qc") for _ in range(4)]
            cst, bcst = sb(ph, "p3cst", [128, 16]), [Buf("c%d" % i) for i in range(4)]
            pc, bpc = sb(ph, "p3pc", [128, 4, 256]), Buf("pc")
            pn, bpn = sb(ph, "p3pn", [128, 4, 256], BF16), Buf("pn")
            pnT, bpnT = sb(ph, "p3pnT", [128, 8, 128], BF16), Buf("pnT")
            ocT, boc = sb(ph, "p3ocT", [128, 4, 128], BF16), Buf("oc")

            for gi in range(8):
                for j in range(4):
                    tile = gi * 4 + j
                    xt, bx = xg[j]
                    P.dma("sp", lambda e, xt=xt, tile=tile: e.dma_start(out=xt[:], in_=xown[tile * 128:(tile + 1) * 128, :]), [], [bx])
                    norm_T(st, xt[:], bx, gmixT, hTg[:, :, j * 128:(j + 1) * 128], bhT[j], tpb, btpb)
                    g0, bg0 = gen.next()
                    P.op("pe", [(lambda e, c=c, j=j, g0=g0: e.matmul(g0[:], hTg[:, c, j * 128:(j + 1) * 128], wsv[:, c, :], start=(c == 0), stop=(c == 7)))
                                for c in range(8)], [bhT[j], B_c], [bg0])
                    P.op("act", lambda e, g0=g0: e.activation(out=gv[:], in_=g0[:], func=AF.Gelu_apprx_tanh), [bg0], [bgv])
                    P.op("dve", lambda e: e.tensor_reduce(out=lst[:, 0:1], in_=gv[:], axis=AX.X, op=ALU.add), [bgv], [blst[0]])
                    P.op("act", lambda e: e.activation(out=vnf[:], in_=gv[:], func=AF.Square, accum_out=lst[:, 1:2]), [bgv], [bvnf, blst[1]])
                    P.op("dve", lambda e: e.tensor_scalar(out=lst[:, 2:3], in0=lst[:, 0:1], scalar1=1.0 / 512, scalar2=None, op0=ALU.mult), [blst[0]], [blst[2]])
                    P.op("dve", lambda e: e.tensor_tensor(out=lst[:, 3:4], in0=lst[:, 2:3], in1=lst[:, 2:3], op=ALU.mult), [blst[2]], [blst[3]])
                    P.op("dve", lambda e: e.scalar_tensor_tensor(out=lst[:, 4:5], in0=lst[:, 1:2], scalar=1.0 / 512, in1=lst[:, 3:4], op0=ALU.mult, op1=ALU.subtract),
                         [blst[1], blst[3]], [blst[4]])
                    P.op("dve", lambda e: e.tensor_scalar(out=lst[:, 5:6], in0=lst[:, 4:5], scalar1=EPS, scalar2=None, op0=ALU.add), [blst[4]], [blst[5]])
                    P.op("act", lambda e: e.activation(out=lst[:, 6:7], in_=lst[:, 5:6], func=AF.Ln), [blst[5]], [blst[6]])
                    P.op("act", lambda e: e.activation(out=lst[:, 7:8], in_=lst[:, 6:7], func=AF.Exp, scale=-0.5), [blst[6]], [blst[7]])
                    P.op("dve", lambda e: e.tensor_scalar(out=vnf[:], in0=gv[:], scalar1=lst[:, 2:3], scalar2=lst[:, 7:8], op0=ALU.subtract, op1=ALU.mult),
                         [bgv, blst[2], blst[7]], [bvnf])
                    P.op("dve", lambda e: e.tensor_tensor(out=vnf[:], in0=vnf[:], in1=lng_bc[:], op=ALU.mult), [bvnf, B_c], [bvnf])
                    P.op("dve", lambda e: e.tensor_tensor(out=vn[:], in0=vnf[:], in1=lnb_bc[:], op=ALU.add), [bvnf, B_c], [bvn])
                    g1, bg1 = gen.next()
                    P.op("pe", [(lambda e, g=g, g1=g1: e.matmul(g1[:, g * 128:(g + 1) * 128], vn[:, g * 128:(g + 1) * 128], WsT[:, g, :], start=True, stop=True))
                                for g in range(4)], [bvn, B_c], [bg1])
                    P.op("dve", lambda e, g1=g1, j=j: e.tensor_tensor(out=vmb[:, :, j * 128:(j + 1) * 128], in0=g1[:].rearrange("p (g q) -> p g q", g=4),
                                                                     in1=bs_bc[:], op=ALU.add), [bg1, B_c], [bvmb[j]])
                for g in range(4):
                    g0, bg0 = gen.next()
                    P.op("pe", [(lambda e, c=c, g=g, g0=g0: e.matmul(g0[:], wsu[:, c, g * 128:(g + 1) * 128], hTg[:, c, :], start=(c == 0), stop=(c == 7)))
                                for c in range(8)], bhT + [B_c], [bg0])
                    P.op("act", lambda e, g0=g0: e.activation(out=gu[:], in_=g0[:], func=AF.Gelu_apprx_tanh), [bg0], [bgu])
                    P.op("dve", lambda e, g=g: e.tensor_tensor(out=sguT[:, g, :], in0=gu[:], in1=vmb[:, g, :], op=ALU.mult), [bgu] + bvmb, [bsg[g]])
                for dc in range(8):
                    ga_ps, bga = gen.next()
                    P.op("pe", [(lambda e, c=c, dc=dc, ga_ps=ga_ps: e.matmul(ga_ps[:], wga[:, c, dc * 128:(dc + 1) * 128], hTg[:, c, :], start=(c == 0), stop=(c == 7)))
                                for c in range(8)], bhT + [B_c], [bga])
                    P.op("act", lambda e, ga_ps=ga_ps: e.activation(out=sa[:], in_=ga_ps[:], func=AF.Sigmoid), [bga], [bsa])
                    gb_ps, bgb = gen.next()
                    P.op("pe", [(lambda e, c=c, dc=dc, gb_ps=gb_ps: e.matmul(gb_ps[:], wgb[:, c, dc * 128:(dc + 1) * 128], hTg[:, c, :], start=(c == 0), stop=(c == 7)))
                                for c in range(8)], bhT + [B_c], [bgb])
                    P.op("act", lambda e, gb_ps=gb_ps: e.activation(out=sbt[:], in_=gb_ps[:], func=AF.Sigmoid), [bgb], [bsb])
                    ys_ps, bysp = gen.next()
                    P.op("pe", [(lambda e, g=g, dc=dc, ys_ps=ys_ps: e.matmul(ys_ps[:], wso[:, g, dc * 128:(dc + 1) * 128], sguT[:, g, :], start=(g == 0), stop=(g == 3)))
                                for g in range(4)], bsg + [B_c], [bysp])
                    yatt, bya = yatts.next()
                    P.dma("sp", lambda e, gi=gi, dc=dc, yatt=yatt: e.dma_start(out=yatt[:], in_=yatt_d[gi][:, dc, :]), [], [bya])
                    P.op("dve", lambda e, yatt=yatt: e.tensor_tensor(out=m1[:], in0=sa[:], in1=yatt[:], op=ALU.mult), [bsa, bya], [bm1])
                    P.op("dve", lambda e, ys_ps=ys_ps: e.tensor_tensor(out=m2[:], in0=sbt[:], in1=ys_ps[:], op=ALU.mult), [bsb, bysp], [bm2])
                    P.op("dve", lambda e, dc=dc: e.tensor_tensor(out=mrg[:, dc, :], in0=m1[:], in1=m2[:], op=ALU.add), [bm1, bm2], [bmrg[dc]])
                for j in range(4):
                    xt, bx = xg[j]
                    for nb in range(2):
                        o_ps, bo = gen.next()
                        P.op("pe", [(lambda e, dc=dc, j=j, nb=nb, o_ps=o_ps: e.matmul(o_ps[:], mrg[:, dc, j * 128:(j + 1) * 128], wo[:, dc, nb * 512:(nb + 1) * 512],
                                                                                     start=(dc == 0), stop=(dc == 7))) for dc in range(8)], bmrg + [B_c], [bo])
                        P.op("dve", lambda e, xt=xt, nb=nb, o_ps=o_ps: e.tensor_tensor(out=xt[:, nb * 512:(nb + 1) * 512], in0=xt[:, nb * 512:(nb + 1) * 512], in1=o_ps[:], op=ALU.add),
                             [bo, bx], [bx])
                for j in range(4):
                    xt, bx = xg[j]
                    norm_T(st, xt[:], bx, gcrT, hcT[:, :, j * 128:(j + 1) * 128], bhc[j], tpb, btpb)
                for h in range(4):
                    q_ps, bq = gen.next()
                    P.op("pe", [(lambda e, c=c, h=h, q_ps=q_ps: e.matmul(q_ps[:], wcq[:, c, h * 128:(h + 1) * 128], hcT[:, c, :], start=(c == 0), stop=(c == 7)))
                                for c in range(8)], bhc + [B_c], [bq])
                    P.op("act", lambda e, h=h, q_ps=q_ps: e.activation(out=qcT[:, h, :], in_=q_ps[:], func=AF.Copy, scale=float(128 ** -0.5)), [bq], [bqc[h]])
                for j in range(4):
                    tile = gi * 4 + j
                    xt, bx = xg[j]
                    P.op("pe", [(lambda e, h=h, j=j: e.matmul(scps[:, h, :], qcT[:, h, j * 128:(j + 1) * 128], kcT[:, h, :], start=True, stop=True)) for h in range(4)],
                         bqc, [bsc])
                    P.op("dve", lambda e: e.tensor_reduce(out=cst[:, 0:4], in_=scps[:], axis=AX.X, op=ALU.max), [bsc], [bcst[0]])
                    P.op("dve", lambda e: e.tensor_scalar(out=cst[:, 4:8], in0=cst[:, 0:4], scalar1=-1.0, scalar2=None, op0=ALU.mult), [bcst[0]], [bcst[1]])
                    P.op("act", [(lambda e, h=h: e.activation(out=pc[:, h, :], in_=scps[:, h, :], func=AF.Exp, bias=cst[:, 4 + h:5 + h], accum_out=cst[:, 8 + h:9 + h]))
                                 for h in range(4)], [bsc, bcst[1]], [bpc, bcst[2]])
                    P.op("dve", lambda e: e.reciprocal(out=cst[:, 12:16], in_=cst[:, 8:12]), [bcst[2]], [bcst[3]])
                    P.op("dve", lambda e: e.tensor_tensor(out=pn[:], in0=pc[:], in1=cst[:, 12:16].unsqueeze(2).broadcast_to([128, 4, 256]), op=ALU.mult),
                         [bpc, bcst[3]], [bpn])
                    P.op("pe", [(lambda e, k=k: e.transpose(tpb[:, k, :], pn[:, k // 2, (k % 2) * 128:(k % 2 + 1) * 128], ident[:])) for k in range(8)], [bpn], [btpb])
                    P.op("act", lambda e: e.activation(out=pnT[:], in_=tpb[:], func=AF.Copy), [btpb], [bpnT])
                    oc_ps, bocp = gen.next()
                    fl = []
                    for h in range(4):
                        for mt in range(2):
                            fl.append(lambda e, h=h, mt=mt, oc_ps=oc_ps: e.matmul(oc_ps[:, h * 128:(h + 1) * 128], Vc[:, mt, h * 128:(h + 1) * 128], pnT[:, h * 2 + mt, :],
                                                                                  start=(mt == 0), stop=(mt == 1)))
                    P.op("pe", fl, [bpnT], [bocp])
                    P.op("act", lambda e, oc_ps=oc_ps: e.activation(out=ocT[:], in_=oc_ps[:].rearrange("p (h t) -> p h t", h=4), func=AF.Copy), [bocp], [boc])
                    for nb in range(2):
                        y_ps, byp = gen.next()
                        P.op("pe", [(lambda e, h=h, nb=nb, y_ps=y_ps: e.matmul(y_ps[:], ocT[:, h, :], wco[:, h, nb * 512:(nb + 1) * 512], start=(h == 0), stop=(h == 3)))
                                    for h in range(4)], [boc, B_c], [byp])
                        P.op("dve", lambda e, xt=xt, nb=nb, y_ps=y_ps: e.tensor_tensor(out=xt[:, nb * 512:(nb + 1) * 512], in0=xt[:, nb * 512:(nb + 1) * 512], in1=y_ps[:], op=ALU.add),
                             [byp, bx], [bx])
                    P.dma("sp", lambda e, xt=xt, tile=tile: e.dma_start(out=x2_d[tile * 128:(tile + 1) * 128, :], in_=xt[:]), [bx], [])
            P.flush()

        if dbg == 3:
            dx = nc.dram_tensor("dbg_x2", [OWN, D], F32, kind="ExternalOutput").ap()
            P.dma("sp", lambda e: e.dma_start(out=dx[:, :], in_=x2_d[:, :]), [], [])
            P.flush()
            return nc

        with contextlib.ExitStack() as ph:
            gmoT = sb(ph, "gmoT", [128, 8])
            wr = sb(ph, "wr", [128, 8, 20], BF16)
            br_bc = sb(ph, "br_bc", [128, 20])
            gfin_bc = sb(ph, "gfin_bc", [128, 1024])
            B_c = Buf("c3")
            P.dma("sp", lambda e: e.dma_start(out=gmoT[:], in_=gT_view(g_moe), allow_slow_non_contiguous=True), [], [B_c])
            P.dma("pool", lambda e: e.dma_start(out=wr[:, :, 0:4], in_=wv(w_rg, 0, 4)), [], [B_c])
            P.dma("pool", lambda e: e.dma_start(out=wr[:, :, 4:20], in_=wv(w_re, 0, 16)), [], [B_c])
            P.dma("sp", lambda e: e.dma_start(out=br_bc[:, 0:4], in_=b_rg.partition_broadcast(128)), [], [B_c])
            P.dma("sp", lambda e: e.dma_start(out=br_bc[:, 4:20], in_=b_re.partition_broadcast(128)), [], [B_c])
            P.dma("sp", lambda e: e.dma_start(out=gfin_bc[:], in_=g_final.partition_broadcast(128)), [], [B_c])
            st = norm_state(ph, "p4")
            xres, bxr = sb(ph, "xres", [128, 16, 1024]), [Buf("xr%d" % i) for i in range(16)]
            hmT, bhm = sb(ph, "hmT", [128, 8, 2048], BF16), [Buf("hm%d" % i) for i in range(16)]
            comb, bcomb = sb(ph, "comb", [128, 16, 16]), [Buf("cb%d" % i) for i in range(16)]
            Wg = [(sb(ph, "Wg%d" % i, [128, 8, 512], BF16), Buf("Wg")) for i in range(2)]
            Wu = [(sb(ph, "Wu%d" % i, [128, 8, 512], BF16), Buf("Wu")) for i in range(2)]
            Wd = [(sb(ph, "Wd%d" % i, [128, 4, 1024], BF16), Buf("Wd")) for i in range(2)]
            sgs = Rot([(sb(ph, "sg%d" % i, [128, 512]), Buf("sg")) for i in range(2)])
            heTs = Rot([(sb(ph, "heT%d" % i, [128, 4, 512], BF16), [Buf("he") for _ in range(4)]) for i in range(2)])
            ots = Rot([(sb(ph, "ot%d" % i, [128, 1024]), Buf("ot")) for i in range(2)])
            rt = sb(ph, "rt", [128, 160])
            brt = [Buf("rt%d" % i) for i in range(24)]
            tpb, btpb = ps(ph, "p4tp", [128, 8, 128], BF16), Buf("tp", True)
            gen = Rot([(ps(ph, "p4g%d" % i, [128, 512]), Buf("g", True)) for i in range(7)])

            for sgi in range(2):
                for tl in range(16):
                    tile = sgi * 16 + tl
                    P.dma("sp", lambda e, tl=tl, tile=tile: e.dma_start(out=xres[:, tl, :], in_=x2_d[tile * 128:(tile + 1) * 128, :]), [], [bxr[tl]])
                    norm_T(st, xres[:, tl, :], bxr[tl], gmoT, hmT[:, :, tl * 128:(tl + 1) * 128], bhm[tl], tpb, btpb)
                    r_ps, brp = gen.next()
                    P.op("pe", [(lambda e, c=c, tl=tl, r_ps=r_ps: e.matmul(r_ps[:, 0:20], hmT[:, c, tl * 128:(tl + 1) * 128], wr[:, c, :], start=(c == 0), stop=(c == 7)))
                                for c in range(8)], [bhm[tl], B_c], [brp])
                    R = rt
                    P.op("dve", lambda e, r_ps=r_ps: e.tensor_tensor(out=R[:, 0:20], in0=r_ps[:, 0:20], in1=br_bc[:], op=ALU.add), [brp, B_c], [brt[0]])
                    P.op("dve", lambda e: e.tensor_reduce(out=R[:, 20:21], in_=R[:, 0:4], axis=AX.X, op=ALU.max), [brt[0]], [brt[1]])
                    P.op("dve", lambda e: e.tensor_scalar(out=R[:, 21:22], in0=R[:, 20:21], scalar1=-1.0, scalar2=None, op0=ALU.mult), [brt[1]], [brt[2]])
                    P.op("act", lambda e: e.activation(out=R[:, 22:26], in_=R[:, 0:4], func=AF.Exp, bias=R[:, 21:22], accum_out=R[:, 26:27]), [brt[0], brt[2]], [brt[3]])
                    P.op("dve", lambda e: e.reciprocal(out=R[:, 27:28], in_=R[:, 26:27]), [brt[3]], [brt[4]])
                    P.op("dve", lambda e: e.tensor_scalar(out=R[:, 28:32], in0=R[:, 0:4], scalar1=R[:, 20:21], scalar2=None, op0=ALU.is_ge), [brt[0], brt[1]], [brt[5]])
                    P.op("dve", lambda e: e.tensor_scalar(out=R[:, 32:36], in0=R[:, 28:32], scalar1=BIG, scalar2=-BIG, op0=ALU.mult, op1=ALU.add), [brt[5]], [brt[6]])
                    P.op("dve", lambda e: e.tensor_tensor(out=R[:, 40:56].rearrange("p (g k) -> p g k", g=4), in0=R[:, 4:20].rearrange("p (g k) -> p g k", g=4),
                                                          in1=R[:, 32:36].unsqueeze(2).broadcast_to([128, 4, 4]), op=ALU.add), [brt[0], brt[6]], [brt[7]])
                    P.op("dve", lambda e: e.tensor_reduce(out=R[:, 56:57], in_=R[:, 40:56], axis=AX.X, op=ALU.max), [brt[7]], [brt[8]])
                    P.op("dve", lambda e: e.tensor_scalar(out=R[:, 60:76], in0=R[:, 40:56], scalar1=R[:, 56:57], scalar2=None, op0=ALU.is_ge), [brt[7], brt[8]], [brt[9]])
                    P.op("dve", lambda e: e.scalar_tensor_tensor(out=R[:, 80:96], in0=R[:, 60:76], scalar=-BIG, in1=R[:, 40:56], op0=ALU.mult, op1=ALU.add),
                         [brt[9], brt[7]], [brt[10]])
                    P.op("dve", lambda e: e.tensor_reduce(out=R[:, 96:97], in_=R[:, 80:96], axis=AX.X, op=ALU.max), [brt[10]], [brt[11]])
                    P.op("dve", lambda e: e.tensor_scalar(out=R[:, 100:116], in0=R[:, 80:96], scalar1=R[:, 96:97], scalar2=None, op0=ALU.is_ge), [brt[10], brt[11]], [brt[12]])
                    P.op("dve", lambda e: e.tensor_tensor(out=R[:, 116:117], in0=R[:, 96:97], in1=R[:, 56:57], op=ALU.subtract), [brt[11], brt[8]], [brt[13]])
                    P.op("act", lambda e: e.activation(out=R[:, 117:118], in_=R[:, 116:117], func=AF.Exp), [brt[13]], [brt[14]])
                    P.op("dve", lambda e: e.tensor_scalar(out=R[:, 118:119], in0=R[:, 117:118], scalar1=1.0, scalar2=None, op0=ALU.add), [brt[14]], [brt[15]])
                    P.op("dve", lambda e: e.reciprocal(out=R[:, 119:120], in_=R[:, 118:119]), [brt[15]], [brt[16]])
                    P.op("dve", lambda e: e.tensor_tensor(out=R[:, 120:121], in0=R[:, 119:120], in1=R[:, 27:28], op=ALU.mult), [brt[16], brt[4]], [brt[17]])
                    P.op("dve", lambda e: e.tensor_tensor(out=R[:, 121:122], in0=R[:, 120:121], in1=R[:, 117:118], op=ALU.mult), [brt[17], brt[14]], [brt[18]])
                    P.op("dve", lambda e, tl=tl: e.tensor_scalar(out=comb[:, tl, :], in0=R[:, 60:76], scalar1=R[:, 120:121], scalar2=None, op0=ALU.mult),
                         [brt[9], brt[17]], [bcomb[tl]])
                    P.op("dve", lambda e, tl=tl: e.scalar_tensor_tensor(out=comb[:, tl, :], in0=R[:, 100:116], scalar=R[:, 121:122], in1=comb[:, tl, :], op0=ALU.mult, op1=ALU.add),
                         [brt[12], brt[18], bcomb[tl]], [bcomb[tl]])

                def load_w(e_):
                    (wg, bwg), (wu, bwu), (wd, bwd) = Wg[e_ % 2], Wu[e_ % 2], Wd[e_ % 2]
                    P.dma("pool", lambda e: e.dma_start(out=wg[:], in_=wv(w_gate[e_], 0, 512)), [], [bwg])
                    P.dma("pool", lambda e: e.dma_start(out=wu[:], in_=wv(w_up[e_], 0, 512)), [], [bwu])
                    P.dma("pool", lambda e: e.dma_start(out=wd[:], in_=wv(w_down[e_], 0, 1024)), [], [bwd])

                load_w(0)
                for ex in range(NE):
                    if ex + 1 < NE:
                        load_w(ex + 1)
                    (wg, bwg), (wu, bwu), (wd, bwd) = Wg[ex % 2], Wu[ex % 2], Wd[ex % 2]
                    for tg in range(4):
                        heT, bhe = heTs.next()
                        for fc in range(4):
                            g_ps, bgp = gen.next()
                            u_ps, bup = gen.next()
                            P.op("pe", [(lambda e, c=c, fc=fc, tg=tg, g_ps=g_ps, wg=wg: e.matmul(g_ps[:], wg[:, c, fc * 128:(fc + 1) * 128], hmT[:, c, tg * 512:(tg + 1) * 512],
                                                                                                 start=(c == 0), stop=(c == 7))) for c in range(8)],
                                 bhm[tg * 4:tg * 4 + 4] + [bwg], [bgp])
                            P.op("pe", [(lambda e, c=c, fc=fc, tg=tg, u_ps=u_ps, wu=wu: e.matmul(u_ps[:], wu[:, c, fc * 128:(fc + 1) * 128], hmT[:, c, tg * 512:(tg + 1) * 512],
                                                                                                 start=(c == 0), stop=(c == 7))) for c in range(8)],
                                 bhm[tg * 4:tg * 4 + 4] + [bwu], [bup])
                            sg, bsg_ = sgs.next()
                            P.op("act", lambda e, sg=sg, g_ps=g_ps: e.activation(out=sg[:], in_=g_ps[:], func=AF.Silu), [bgp], [bsg_])
                            P.op("dve", lambda e, sg=sg, u_ps=u_ps, heT=heT, fc=fc: e.tensor_tensor(out=heT[:, fc, :], in0=sg[:], in1=u_ps[:], op=ALU.mult),
                                 [bsg_, bup], [bhe[fc]])
                        for j in range(4):
                            tl = tg * 4 + j
                            for nb in range(2):
                                y_ps, byp = gen.next()
                                P.op("pe", [(lambda e, fc=fc, j=j, nb=nb, y_ps=y_ps, heT=heT, wd=wd: e.matmul(y_ps[:], heT[:, fc, j * 128:(j + 1) * 128], wd[:, fc, nb * 512:(nb + 1) * 512],
                                                                                                             start=(fc == 0), stop=(fc == 3))) for fc in range(4)],
                                     bhe + [bwd], [byp])
                                P.op("dve", lambda e, tl=tl, nb=nb, y_ps=y_ps, ex=ex: e.scalar_tensor_tensor(
                                    out=xres[:, tl, nb * 512:(nb + 1) * 512], in0=y_ps[:], scalar=comb[:, tl, ex:ex + 1], in1=xres[:, tl, nb * 512:(nb + 1) * 512],
                                    op0=ALU.mult, op1=ALU.add), [byp, bcomb[tl], bxr[tl]], [bxr[tl]])
                for tl in range(16):
                    tile = sgi * 16 + tl
                    P.op("act", lambda e, tl=tl: e.activation(out=st["junk"][:], in_=xres[:, tl, :], func=AF.Square, scale=1.0 / 32, accum_out=st["ss"][:, 0:1]),
                         [bxr[tl]], [st["bjunk"], st["bss"]])
                    P.op("dve", lambda e: e.tensor_scalar(out=st["ss"][:, 1:2], in0=st["ss"][:, 0:1], scalar1=EPS, scalar2=None, op0=ALU.add), [st["bss"]], [st["bss1"]])
                    P.op("act", lambda e: e.activation(out=st["ss"][:, 2:3], in_=st["ss"][:, 1:2], func=AF.Ln), [st["bss1"]], [st["bss2"]])
                    P.op("act", lambda e: e.activation(out=st["ss"][:, 3:4], in_=st["ss"][:, 2:3], func=AF.Exp, scale=-0.5), [st["bss2"]], [st["bss3"]])
                    ot, bot = ots.next()
                    P.op("dve", lambda e, tl=tl, ot=ot: e.scalar_tensor_tensor(out=ot[:], in0=xres[:, tl, :], scalar=st["ss"][:, 3:4], in1=gfin_bc[:], op0=ALU.mult, op1=ALU.mult),
                         [bxr[tl], st["bss3"], B_c], [bot])
                    P.dma("sp", lambda e, ot=ot, tile=tile: e.dma_start(out=out[tile * 128:(tile + 1) * 128, :], in_=ot[:]), [bot], [])
            P.flush()
    return nc


def _rope_tables():
    rows = SEQ // 64
    row_ids = np.repeat(np.arange(rows), 64).astype(np.float32)
    col_ids = np.tile(np.arange(64), rows).astype(np.float32)
    inv = (10000.0 ** (-np.arange(16, dtype=np.float32) / 16)).astype(np.float32)
    ang = np.concatenate([row_ids[:, None] * inv[None, :], col_ids[:, None] * inv[None, :]], axis=-1).astype(np.float32)
    return np.cos(ang).astype(np.float32), np.sin(ang).astype(np.float32)


def make_in_maps(inputs):
    f = lambda a: np.ascontiguousarray(np.asarray(a, dtype=np.float32))
    x = f(inputs["x"])
    mem = f(inputs["mem"])
    cos, sin = _rope_tables()

    def lay(t, n):
        return np.ascontiguousarray(t.reshape(n, 128, 32).transpose(1, 0, 2))

    shared = {
        "ident": np.eye(128, dtype=np.float32),
        "g_mix": f(inputs["g_mix"][0]), "w_in": f(inputs["w_in"][0]), "g_q": f(inputs["g_q"][0]), "g_k": f(inputs["g_k"][0]),
        "w_att_out": f(inputs["w_att_out"][0]), "sgu_ln_g": f(inputs["sgu_ln_g"][0]), "sgu_ln_b": f(inputs["sgu_ln_b"][0]),
        "w_s": f(inputs["w_s"][0]), "b_s": f(inputs["b_s"][0]), "w_sgu_out": f(inputs["w_sgu_out"][0]), "w_out": f(inputs["w_out"][0]),
        "g_cross": f(inputs["g_cross"][0]), "g_mem": f(inputs["g_mem"][0]), "w_cq": f(inputs["w_cq"][0]), "w_ckv": f(inputs["w_ckv"][0]),
        "w_co": f(inputs["w_co"][0]), "g_moe": f(inputs["g_moe"][0]), "w_rg": f(inputs["w_rg"][0]), "b_rg": f(inputs["b_rg"][0]),
        "w_re": f(inputs["w_re"][0]), "b_re": f(inputs["b_re"][0]), "w_gate": f(inputs["w_gate"][0]), "w_up": f(inputs["w_up"][0]),
        "w_down": f(inputs["w_down"][0]), "g_final": f(inputs["g_final"]),
        "cos_seq": lay(cos, 64), "sin_seq": lay(sin, 64),
    }
    maps = []
    for c in range(8):
        b, hf = c // 2, c % 2
        m = dict(shared)
        m["xseq"] = x[b]
        m["xown"] = np.ascontiguousarray(x[b, hf * OWN:(hf + 1) * OWN])
        m["mem"] = mem[b]
        m["cos_own"] = lay(cos[hf * OWN:(hf + 1) * OWN], 32)
        m["sin_own"] = lay(sin[hf * OWN:(hf + 1) * OWN], 32)
        maps.append(m)
    return maps


def kernel(**inputs):
    nc = build()
    maps = make_in_maps(inputs)
    res = run_bass_kernel_spmd(nc, maps, core_ids=list(range(8)))
    outp = np.empty((4, SEQ, D), np.float32)
    for c in range(8):
        b, hf = c // 2, c % 2
        outp[b, hf * OWN:(hf + 1) * OWN] = np.asarray(res.results[c]["out"], dtype=np.float32)
    return outp
```

```python
import contextlib
import os as _os0
import numpy as np
import concourse.bass as bass
import concourse.mybir as mybir
from concourse.bass_utils import run_bass_kernel_spmd

F32 = mybir.dt.float32
BF16 = mybir.dt.bfloat16
AF = mybir.ActivationFunctionType
ALU = mybir.AluOpType
AX = mybir.AxisListType

D = 1024
SEQ = 8192
OWN = 4096
NMEM = 256
EPS = 1e-6
NE = 16
BIG = 1.0e30


class Buf:
    __slots__ = ("name", "w", "r", "psum")

    def __init__(self, name="", psum=False):
        self.name = name
        self.psum = psum
        self.w = None
        self.r = {}


class Eng:
    def __init__(self, name):
        self.name = name
        self.sem = None
        self.count = 0
        self.seen = {}
        self.prog = []
        self.chsems = []
        self.dma_i = 0


class Prog:
    NCH = int(_os0.environ.get('NCH', 8))

    def __init__(self, nc, stack):
        self.nc = nc
        self.E = {}
        for n in ("pe", "act", "dve", "pool", "sp"):
            e = Eng(n)
            e.sem = stack.enter_context(nc.semaphore("sem_" + n))
            self.E[n] = e
        for n in ("sp", "pool", "act"):
            e = self.E[n]
            for i in range(self.NCH):
                e.chsems.append(stack.enter_context(nc.semaphore("ch_%s_%d" % (n, i))))
        self.chan_issued = {}
        import os
        self.limit = int(os.environ.get("OPLIMIT", 10 ** 9))
        self.nops = 0

    def _need(self, need, ev):
        sem, val, src = ev
        k = id(sem)
        if k not in need or need[k][1] < val:
            need[k] = (sem, val, src)

    def _deps(self, eng, reads, writes, extra=()):
        E = self.E[eng]
        need = {}
        for b in reads:
            if b.w is not None:
                self._need(need, b.w)
        for b in writes:
            if b.w is not None:
                self._need(need, b.w)
            for k, (sem, val, src) in b.r.items():
                self._need(need, (sem, val, src))
        for ev in extra:
            self._need(need, ev)
        for k, (sem, val, src) in need.items():
            if src == "pe" and eng == "pe":
                continue
            if E.seen.get(k, 0) >= val:
                continue
            E.seen[k] = val
            E.prog.append(("wait", sem, val))

    def _commit(self, ev, reads, writes):
        sem, val, src = ev
        for b in writes:
            b.w = ev
            b.r = {}
        for b in reads:
            if b not in writes:
                b.r[id(sem)] = (sem, val, src)

    def op(self, eng, fns, reads=(), writes=()):
        if not isinstance(fns, (list, tuple)):
            fns = [fns]
        self.nops += 1
        if self.nops > self.limit:
            return
        pr = [b for b in reads if b.psum]
        if pr:
            reads = [b for b in reads if not b.psum]
            writes = list(writes) + [b for b in pr if b not in writes]
        E = self.E[eng]
        self._deps(eng, reads, writes)
        E.count += 1
        for f in fns[:-1]:
            E.prog.append(("raw", f))
        E.prog.append(("op", fns[-1], E.sem))
        self._commit((E.sem, E.count, eng), reads, writes)

    def dma(self, q, fn, reads=(), writes=()):
        self.nops += 1
        if self.nops > self.limit:
            return
        E = self.E[q]
        ch = E.dma_i % self.NCH
        rnd = E.dma_i // self.NCH
        E.dma_i += 1
        chsem = E.chsems[ch]
        extra = []
        if rnd > 0:
            extra.append((chsem, 16 * rnd, "dma"))
        self._deps(q, reads, writes, extra)
        E.prog.append(("dma", fn, chsem))
        self.chan_issued[id(chsem)] = (chsem, 16 * (rnd + 1))
        self._commit((chsem, 16 * (rnd + 1), "dma"), reads, writes)

    def barrier(self):
        for n, E in self.E.items():
            for m, F in self.E.items():
                if m == n or F.count == 0:
                    continue
                k = id(F.sem)
                if E.seen.get(k, 0) < F.count:
                    E.seen[k] = F.count
                    E.prog.append(("wait", F.sem, F.count))
            for k, (sem, val) in self.chan_issued.items():
                if E.seen.get(k, 0) < val:
                    E.seen[k] = val
                    E.prog.append(("wait", sem, val))

    def flush(self):
        self.barrier()
        nc = self.nc
        progs = {n: E.prog for n, E in self.E.items()}
        for E in self.E.values():
            E.prog = []

        def replay(eng, prog):
            for it in prog:
                if it[0] == "wait":
                    eng.wait_ge(it[1], it[2])
                elif it[0] == "raw":
                    it[1](eng)
                elif it[0] == "op":
                    it[1](eng).then_inc(it[2], 1)
                else:
                    it[1](eng).then_inc(it[2], 16)

        with nc.Block() as block:
            @block.tensor
            def _(e):
                replay(e, progs["pe"])

            @block.scalar
            def _(e):
                replay(e, progs["act"])

            @block.vector
            def _(e):
                replay(e, progs["dve"])

            @block.gpsimd
            def _(e):
                replay(e, progs["pool"])

            @block.sync
            def _(e):
                replay(e, progs["sp"])


class Rot:
    def __init__(self, items):
        self.items = items
        self.i = 0

    def next(self):
        it = self.items[self.i % len(self.items)]
        self.i += 1
        return it


class Pool:
    def __init__(self, items):
        self.free = list(items)

    def acquire(self):
        return self.free.pop(0) if self.free else None

    def release(self, it):
        self.free.append(it)


def build(dbg=False):
    nc = bass.Bass("TRN2", target_bir_lowering=False)

    def din(name, shape):
        return nc.dram_tensor(name, list(shape), F32, kind="ExternalInput").ap()

    xseq = din("xseq", [SEQ, D])
    xown = din("xown", [OWN, D])
    memd = din("mem", [NMEM, D])
    cos_seq = din("cos_seq", [128, 64, 32])
    sin_seq = din("sin_seq", [128, 64, 32])
    cos_own = din("cos_own", [128, 32, 32])
    sin_own = din("sin_own", [128, 32, 32])
    identd = din("ident", [128, 128])
    g_mix = din("g_mix", [D])
    w_in = din("w_in", [D, 3840])
    g_q = din("g_q", [64])
    g_k = din("g_k", [64])
    w_att_out = din("w_att_out", [512, D])
    sgu_ln_g = din("sgu_ln_g", [512])
    sgu_ln_b = din("sgu_ln_b", [512])
    w_s = din("w_s", [4, 128, 128])
    b_s = din("b_s", [4, 128])
    w_sgu_out = din("w_sgu_out", [512, D])
    w_out = din("w_out", [D, D])
    g_cross = din("g_cross", [D])
    g_mem = din("g_mem", [D])
    w_cq = din("w_cq", [D, 512])
    w_ckv = din("w_ckv", [D, D])
    w_co = din("w_co", [512, D])
    g_moe = din("g_moe", [D])
    w_rg = din("w_rg", [D, 4])
    b_rg = din("b_rg", [4])
    w_re = din("w_re", [D, 16])
    b_re = din("b_re", [16])
    w_gate = din("w_gate", [NE, D, 512])
    w_up = din("w_up", [NE, D, 512])
    w_down = din("w_down", [NE, 512, D])
    g_final = din("g_final", [D])
    out = nc.dram_tensor("out", [OWN, D], F32, kind="ExternalOutput").ap()
    yatt_d = nc.dram_tensor("yatt_scr", [8, 128, 8, 512], F32, kind="Internal").ap()
    x2_d = nc.dram_tensor("x2_scr", [OWN, D], F32, kind="Internal").ap()

    def wv(w, c0, c1):
        return w.rearrange("(c p) n -> p c n", p=128)[:, :, c0:c1]

    def gT_view(g):
        return g.rearrange("(c p) -> p c", p=128)

    with contextlib.ExitStack() as top:
        P = Prog(nc, top)
        print("sbuf bytes remaining at start:", nc.sbuf_bytes_remaining)

        def sb(stack, name, shape, dt=F32):
            return stack.enter_context(nc.sbuf_tensor("s_" + name, list(shape), dt))

        def ps(stack, name, shape, dt=F32):
            return stack.enter_context(nc.psum_tensor("p_" + name, list(shape), dt))

        ident = sb(top, "ident", [128, 128], BF16)
        kcT = sb(top, "kcT", [128, 4, NMEM], BF16)
        Vc = sb(top, "Vc", [128, 2, 512], BF16)
        gmixT = sb(top, "gmixT", [128, 8])
        attn_stack = contextlib.ExitStack()
        kT_all = sb(attn_stack, "kT_all", [128, SEQ], BF16)
        V_all = sb(attn_stack, "V_all", [128, 64, 2, 65], BF16)
        B_const = Buf("const")

        P.dma("pool", lambda e: e.dma_start(out=ident[:], in_=identd[:, :]), [], [B_const])
        P.dma("sp", lambda e: e.dma_start(out=gmixT[:], in_=gT_view(g_mix), allow_slow_non_contiguous=True), [], [B_const])
        P.op("pool", lambda e: e.memset(V_all[:], 1.0), [], [B_const])

        def run_pipeline(gens, depth):
            it = iter(gens)
            active = []
            more = True
            while True:
                if more and len(active) < depth:
                    try:
                        active.append(next(it))
                    except StopIteration:
                        more = False
                if not active:
                    break
                for g in list(active):
                    try:
                        next(g)
                    except StopIteration:
                        active.remove(g)

        def drain(g):
            for _ in g:
                pass

        def bfview(bank_ap):
            return bank_ap.bitcast(BF16).rearrange("p (c t) -> p c t", c=8)

        def norm_T_gen(st, xt_ap, xbuf, gT, out_ap3, outbuf, tps, tpsb, pool=None):
            ss, b, junk = st["sets"].next()
            P.op("act", lambda e: e.activation(out=junk[:], in_=xt_ap, func=AF.Square, scale=1.0 / 32,
                                               accum_out=ss[:, 0:1]), [xbuf], [b[0]])
            yield
            P.op("dve", lambda e: e.tensor_scalar(out=ss[:, 1:2], in0=ss[:, 0:1], scalar1=EPS, scalar2=None,
                                                  op0=ALU.add), [b[0]], [b[1]])
            yield
            P.op("act", lambda e: e.activation(out=ss[:, 2:3], in_=ss[:, 1:2], func=AF.Ln), [b[1]], [b[2]])
            yield
            P.op("act", lambda e: e.activation(out=ss[:, 3:4], in_=ss[:, 2:3], func=AF.Exp, scale=-0.5), [b[2]], [b[3]])
            yield
            xb, bxb = st["xb"].next()
            P.op("act", lambda e: e.activation(out=xb[:], in_=xt_ap, func=AF.Copy, scale=ss[:, 3:4]), [xbuf, b[3]], [bxb])
            yield
            held = None
            if pool is not None:
                while held is None:
                    held = pool.acquire()
                    if held is None:
                        yield
                tps, tpsb = bfview(held[0][:]), held[1]
            P.op("pe", [(lambda e, c=c: e.transpose(tps[:, c, :], xb[:, c * 128:(c + 1) * 128], ident[:])) for c in range(8)],
                 [bxb], [tpsb])
            yield
            P.op("dve", lambda e: e.tensor_tensor(out=out_ap3, in0=tps, in1=gT[:, 0:8].unsqueeze(2).broadcast_to([128, 8, 128]),
                                                  op=ALU.mult), [tpsb], [outbuf])
            if held is not None:
                pool.release(held)
            yield

        def norm_T(st, xt_ap, xbuf, gT, out_ap3, outbuf, tps, tpsb):
            drain(norm_T_gen(st, xt_ap, xbuf, gT, out_ap3, outbuf, tps[:], tpsb))

        def norm_state(stack, pfx, nsets=2, nxb=2):
            st = {}
            st["sets"] = Rot([(sb(stack, pfx + "ss%d" % i, [128, 4]), [Buf("ss") for _ in range(4)],
                               sb(stack, pfx + "junk%d" % i, [128, 1024], BF16)) for i in range(nsets)])
            st["xb"] = Rot([(sb(stack, pfx + "xb%d" % i, [128, 1024], BF16), Buf("xb")) for i in range(nxb)])
            return st

        def head_norm_rope_gen(ws, src_ps, bsrc, H, g_bc, cosb, sinb, dst, bdst, t):
            w = ws.next()
            n = H * 64
            src3 = src_ps.rearrange("p (h d) -> p h d", h=H)
            P.op("act", lambda e: e.activation(out=w["sq"][:, 0:n], in_=src_ps, func=AF.Square, scale=0.125), [bsrc], [w["bsq"]])
            yield
            P.op("dve", lambda e: e.tensor_reduce(out=w["hs"][:, 0:H], in_=w["sq"][:, 0:n].rearrange("p (h d) -> p h d", h=H),
                                                  axis=AX.X, op=ALU.add), [w["bsq"]], [w["bhs"]])
            yield
            P.op("dve", lambda e: e.tensor_scalar(out=w["hs"][:, 8:8 + H], in0=w["hs"][:, 0:H], scalar1=EPS, scalar2=None, op0=ALU.add),
                 [w["bhs"]], [w["bhs1"]])
            yield
            P.op("act", lambda e: e.activation(out=w["hs"][:, 16:16 + H], in_=w["hs"][:, 8:8 + H], func=AF.Ln), [w["bhs1"]], [w["bhs2"]])
            yield
            P.op("act", lambda e: e.activation(out=w["hs"][:, 24:24 + H], in_=w["hs"][:, 16:16 + H], func=AF.Exp, scale=-0.5),
                 [w["bhs2"]], [w["bhs3"]])
            yield
            qn3 = w["qn"][:, 0:n].rearrange("p (h d) -> p h d", h=H)
            P.op("dve", lambda e: e.tensor_tensor(out=qn3, in0=src3, in1=w["hs"][:, 24:24 + H].unsqueeze(2).broadcast_to([128, H, 64]),
                                                  op=ALU.mult), [bsrc, w["bhs3"]], [w["bqn"]])
            yield
            P.op("dve", lambda e: e.tensor_tensor(out=qn3, in0=qn3, in1=g_bc[:, :].unsqueeze(1).broadcast_to([128, H, 64]),
                                                  op=ALU.mult), [w["bqn"]], [w["bqn"]])
            yield
            qn4 = w["qn"][:, 0:n].rearrange("p (h i t) -> p h i t", h=H, t=2)
            A4 = w["A"][:, 0:n].rearrange("p (h i t) -> p h i t", h=H, t=2)
            B4 = w["B"][:, 0:n].rearrange("p (h i t) -> p h i t", h=H, t=2)
            c4 = cosb[:, t, :].unsqueeze(1).unsqueeze(3).broadcast_to([128, H, 32, 2])
            s4 = sinb[:, t, :].unsqueeze(1).unsqueeze(3).broadcast_to([128, H, 32, 2])
            P.op("dve", lambda e: e.tensor_tensor(out=A4, in0=qn4, in1=c4, op=ALU.mult), [w["bqn"]], [w["bA"]])
            yield
            P.op("dve", lambda e: e.tensor_tensor(out=B4, in0=qn4, in1=s4, op=ALU.mult), [w["bqn"]], [w["bB"]])
            yield
            P.op("dve", lambda e: e.tensor_tensor(out=dst[:, :, :, 0], in0=A4[:, :, :, 0], in1=B4[:, :, :, 1], op=ALU.subtract),
                 [w["bA"], w["bB"]], [bdst])
            yield
            P.op("dve", lambda e: e.tensor_tensor(out=dst[:, :, :, 1], in0=B4[:, :, :, 0], in1=A4[:, :, :, 1], op=ALU.add),
                 [w["bA"], w["bB"]], [bdst])
            yield

        def rope_state(stack, pfx, n, nsets=1):
            sets = []
            for i in range(nsets):
                w = {}
                for k in ("sq", "qn", "A", "B"):
                    w[k] = sb(stack, "%s%s%d" % (pfx, k, i), [128, n])
                w["hs"] = sb(stack, "%shs%d" % (pfx, i), [128, 32])
                for k in ("bsq", "bhs", "bhs1", "bhs2", "bhs3", "bqn", "bA", "bB"):
                    w[k] = Buf(k)
                sets.append(w)
            return Rot(sets)

        with contextlib.ExitStack() as ph:
            wkv = sb(ph, "wkv", [128, 8, 256], BF16)
            cosb = sb(ph, "cosb", [128, 64, 32])
            sinb = sb(ph, "sinb", [128, 64, 32])
            gk_bc = sb(ph, "gk_bc", [128, 64])
            P.dma("pool", lambda e: e.dma_start(out=wkv[:], in_=wv(w_in, 512, 768)), [], [B_const])
            P.dma("sp", lambda e: e.dma_start(out=cosb[:], in_=cos_seq[:, :, :]), [], [B_const])
            P.dma("sp", lambda e: e.dma_start(out=sinb[:], in_=sin_seq[:, :, :]), [], [B_const])
            P.dma("sp", lambda e: e.dma_start(out=gk_bc[:], in_=g_k.partition_broadcast(128)), [], [B_const])
            NP1 = 8
            P.barrier()
            st = norm_state(ph, "p1", nsets=NP1, nxb=NP1)
            rws = rope_state(ph, "p1r", 128, nsets=NP1)
            xts = Rot([(sb(ph, "p1x%d" % i, [128, 1024]), Buf("x")) for i in range(NP1)])
            hTs = Rot([(sb(ph, "p1h%d" % i, [128, 8, 128], BF16), Buf("h")) for i in range(NP1)])
            krs = Rot([(sb(ph, "p1kr%d" % i, [128, 2, 32, 2], BF16), Buf("kr")) for i in range(NP1)])
            banks = Pool([(ps(ph, "p1b%d" % i, [128, 512]), Buf("bank", True)) for i in range(8)])

            def acq(pool):
                while True:
                    it = pool.acquire()
                    if it is not None:
                        return it
                    yield
            import os as _os

            def p1_tile(t):
                xt, bx = xts.next()
                P.dma("sp", lambda e: e.dma_start(out=xt[:], in_=xseq[t * 128:(t + 1) * 128, :]), [], [bx])
                yield
                hT, bh = hTs.next()
                yield from norm_T_gen(st, xt[:], bx, gmixT, hT[:], bh, None, None, pool=banks)
                hkv = yield from acq(banks)
                kv, bkv = hkv
                P.op("pe", [(lambda e, c=c: e.matmul(kv[:, 0:256], hT[:, c, :], wkv[:, c, :], start=(c == 0), stop=(c == 7)))
                            for c in range(8)], [bh, B_const], [bkv])
                yield
                P.op("act", lambda e: e.activation(out=V_all[:, t, :, 0:64], in_=kv[:, 128:256].rearrange("p (h d) -> p h d", h=2),
                                                   func=AF.Copy), [bkv], [])
                yield
                kr, bkr = krs.next()
                yield from head_norm_rope_gen(rws, kv[:, 0:128], bkv, 2, gk_bc, cosb, sinb, kr, bkr, t)
                banks.release(hkv)
                hkt = yield from acq(banks)
                kt, bkt = hkt
                ktv = kt[:].bitcast(BF16)
                P.op("pe", lambda e: e.transpose(ktv[:, 0:128], kr[:].rearrange("p h i t -> p (h i t)"), ident[:]), [bkr], [bkt])
                yield
                P.op("act", lambda e: e.activation(out=kT_all[:, t * 128:(t + 1) * 128], in_=ktv[:, 0:128], func=AF.Copy), [bkt], [])
                banks.release(hkt)
                yield

            run_pipeline((p1_tile(t) for t in range(int(_os.environ.get('NT1', 64)))), int(_os.environ.get('DEPTH1', 8)))
            P.flush()

        if dbg == 1:
            P.limit = 10 ** 9
            P.limit = 10 ** 9
            print("nops", P.nops)
            dk = nc.dram_tensor("dbg_kT", [128, SEQ], BF16, kind="ExternalOutput").ap()
            dv = nc.dram_tensor("dbg_V", [128, 64 * 2 * 65], BF16, kind="ExternalOutput").ap()
            P.dma("sp", lambda e: e.dma_start(out=dk[:, :], in_=kT_all[:]), [], [])
            P.dma("sp", lambda e: e.dma_start(out=dv[:, :], in_=V_all[:].rearrange("p a b c -> p (a b c)")), [], [])
            P.flush()
            attn_stack.close()
            return nc

        with contextlib.ExitStack() as ph:
            wq = sb(ph, "wq", [128, 8, 512], BF16)
            wao = sb(ph, "wao", [64, 8, 1024], BF16)
            cosb = sb(ph, "cosb2", [128, 32, 32])
            sinb = sb(ph, "sinb2", [128, 32, 32])
            gq_bc = sb(ph, "gq_bc", [128, 64])
            ones65 = sb(ph, "ones65", [65, 64])
            B_c2 = Buf("c2")
            for hd_ in range(8):
                pos_ = (hd_ % 4) * 2 + hd_ // 4
                P.dma("pool", lambda e, hd_=hd_, pos_=pos_: e.dma_start(out=wq[:, :, pos_ * 64:(pos_ + 1) * 64], in_=wv(w_in, hd_ * 64, hd_ * 64 + 64)), [], [B_c2])
            P.dma("pool", lambda e: e.dma_start(out=wao[:], in_=w_att_out.rearrange("(h p) n -> p h n", p=64)), [], [B_c2])
            P.dma("sp", lambda e: e.dma_start(out=cosb[:], in_=cos_own[:, :, :]), [], [B_c2])
            P.dma("sp", lambda e: e.dma_start(out=sinb[:], in_=sin_own[:, :, :]), [], [B_c2])
            P.dma("sp", lambda e: e.dma_start(out=gq_bc[:], in_=g_q.partition_broadcast(128)), [], [B_c2])
            P.op("dve", lambda e: e.tensor_scalar(out=gq_bc[:], in0=gq_bc[:], scalar1=0.125, scalar2=None, op0=ALU.mult), [B_c2], [B_c2])
            P.op("pool", lambda e: e.memset(ones65[:], 1.0), [], [B_c2])
            P.barrier()
            st = norm_state(ph, "p2", nsets=4, nxb=4)
            rws = rope_state(ph, "p2r", 512, nsets=3)
            xts = Rot([(sb(ph, "p2x%d" % i, [128, 1024]), Buf("x")) for i in range(4)])
            hTs = Rot([(sb(ph, "p2h%d" % i, [128, 8, 128], BF16), Buf("h")) for i in range(4)])
            qrs = Rot([(sb(ph, "p2qr%d" % i, [128, 8, 32, 2], BF16), Buf("qr")) for i in range(4)])
            qTs = Rot([(sb(ph, "p2qT%d" % i, [128, 4, 512], BF16), [Buf("qT") for _ in range(4)]) for i in range(2)])
            Ps = Rot([(sb(ph, "p2P%d" % i, [128, 2, 512], BF16), Buf("P")) for i in range(4)])
            Osbs = Rot([(sb(ph, "p2Osb%d" % i, [65, 512]), Buf("Osb")) for i in range(2)])
            rrs = Rot([(sb(ph, "p2rr%d" % i, [65, 512]), Buf("rr")) for i in range(2)])
            OnTs = Rot([(sb(ph, "p2OnT%d" % i, [64, 8, 512], BF16), [Buf("OnT") for _ in range(8)]) for i in range(2)])
            ysbs = Rot([(sb(ph, "p2ysb%d" % i, [128, 8, 512]), [Buf("ysb") for _ in range(8)]) for i in range(1)])
            Sps = Rot([(ps(ph, "p2S%d" % i, [128, 2, 512]), Buf("S", True)) for i in range(3)])
            Ops, bO = ps(ph, "p2O", [128, 2, 512]), [Buf("O0", True), Buf("O1", True)]
            LA = int(_os0.environ.get('LA', 2))

            def q_tile(gi, j, qT, bqT):
                tile = gi * 4 + j
                xt, bx = xts.next()
                P.dma("sp", lambda e: e.dma_start(out=xt[:], in_=xown[tile * 128:(tile + 1) * 128, :]), [], [bx])
                yield
                hT, bh = hTs.next()
                S, bS = Sps.next()
                tpv = bfview(S[:, 1, :])
                yield from norm_T_gen(st, xt[:], bx, gmixT, hT[:], bh, tpv, bS)
                P.op("pe", [(lambda e, c=c: e.matmul(S[:, 0, :], hT[:, c, :], wq[:, c, :], start=(c == 0), stop=(c == 7)))
                            for c in range(8)], [bh, B_c2], [bS])
                yield
                qr, bqr = qrs.next()
                yield from head_norm_rope_gen(rws, S[:, 0, :], bS, 8, gq_bc, cosb, sinb, qr, bqr, tile)
                qr3 = qr[:].rearrange("p h i t -> p (h i t)")
                P.op("pe", [(lambda e, g=g: e.transpose(tpv[:, g, :], qr3[:, g * 128:(g + 1) * 128], ident[:])) for g in range(4)],
                     [bqr], [bS])
                yield
                P.op("act", lambda e: e.activation(out=qT[:, :, j * 128:(j + 1) * 128], in_=tpv[:, 0:4, :], func=AF.Copy),
                     [bS], [bqT[j]])
                yield

            for gi in range(8):
                qT, bqT = qTs.next()
                run_pipeline((q_tile(gi, j, qT, bqT) for j in range(4)), int(_os0.environ.get('QD', 3)))
                OnT, bOn = OnTs.next()
                for g in range(4):
                    def issue_S(kt, g=g, qT=qT):
                        S, bS = Sps.next()
                        P.op("pe", [lambda e, S=S, kt=kt: e.matmul(S[:, 0, :], kT_all[0:64, kt * 128:(kt + 1) * 128], qT[0:64, g, :], start=True, stop=True),
                                    lambda e, S=S, kt=kt: e.matmul(S[:, 1, :], kT_all[64:128, kt * 128:(kt + 1) * 128], qT[64:128, g, :], start=True, stop=True)],
                             bqT, [bS])
                        return S, bS
                    pend = [issue_S(kt) for kt in range(LA)]
                    for kt in range(64):
                        if kt + LA < 64:
                            pend.append(issue_S(kt + LA))
                        S, bS = pend.pop(0)
                        Pt, bP = Ps.next()
                        P.op("act", lambda e, S=S, Pt=Pt: e.activation(out=Pt[:], in_=S[:], func=AF.Exp), [bS], [bP])
                        P.op("pe", [lambda e, Pt=Pt, kt=kt: e.matmul(Ops[0:65, 0, :], V_all[:, kt, 0, :], Pt[:, 0, :], start=(kt == 0), stop=(kt == 63)),
                                    lambda e, Pt=Pt, kt=kt: e.matmul(Ops[0:65, 1, :], V_all[:, kt, 1, :], Pt[:, 1, :], start=(kt == 0), stop=(kt == 63))],
                             [bP], bO)
                    for hh in range(2):
                        head = g + 4 * hh
                        Osb, bOsb = Osbs.next()
                        rr, brr = rrs.next()
                        mA, bmA = Sps.next()
                        P.op("act", lambda e, Osb=Osb, hh=hh: e.activation(out=Osb[:], in_=Ops[0:65, hh, :], func=AF.Copy), [bO[hh]], [bOsb])
                        P.op("dve", lambda e, Osb=Osb, rr=rr: e.reciprocal(out=rr[64:65, :], in_=Osb[64:65, :]), [bOsb], [brr])
                        P.op("pe", lambda e, rr=rr, mA=mA: e.matmul(mA[0:64, 0, :], ones65[64:65, 0:64], rr[64:65, :], start=True, stop=True), [brr, B_c2], [bmA])
                        P.op("dve", lambda e, Osb=Osb, OnT=OnT, head=head, mA=mA: e.tensor_tensor(out=OnT[:, head, :], in0=Osb[0:64, :], in1=mA[0:64, 0, :], op=ALU.mult),
                             [bOsb, bmA], [bOn[head]])
                ysb, bys = ysbs.next()
                for dc in range(8):
                    mA, bmA = Sps.next()
                    P.op("pe", [(lambda e, h=h, dc=dc, OnT=OnT, mA=mA: e.matmul(mA[:, 0, :], wao[:, h, dc * 128:(dc + 1) * 128], OnT[:, h, :], start=(h == 0), stop=(h == 7)))
                                for h in range(8)], bOn + [B_c2], [bmA])
                    P.op("act", lambda e, ysb=ysb, dc=dc, mA=mA: e.activation(out=ysb[:, dc, :], in_=mA[:, 0, :], func=AF.Copy), [bmA], [bys[dc]])
                P.dma("sp", lambda e, ysb=ysb, gi=gi: e.dma_start(out=yatt_d[gi], in_=ysb[:]), bys, [])
            P.flush()

        attn_stack.close()
        if dbg == 2:
            dy = nc.dram_tensor("dbg_yatt", [8, 128, 8, 512], F32, kind="ExternalOutput").ap()
            P.dma("sp", lambda e: e.dma_start(out=dy.rearrange("a p c t -> (a p) (c t)"), in_=yatt_d.rearrange("a p c t -> (a p) (c t)")), [], [])
            P.flush()
            return nc

        with contextlib.ExitStack() as ph:
            wckv = sb(ph, "wckv", [128, 8, 1024], BF16)
            gmemT = sb(ph, "gmemT", [128, 8])
            memT = sb(ph, "memT", [128, 8, 256], BF16)
            B_cm = Buf("cm")
            P.dma("pool", lambda e: e.dma_start(out=wckv[:], in_=wv(w_ckv, 0, 1024)), [], [B_cm])
            P.dma("sp", lambda e: e.dma_start(out=gmemT[:], in_=gT_view(g_mem), allow_slow_non_contiguous=True), [], [B_cm])
            st = norm_state(ph, "pm")
            xts = Rot([(sb(ph, "pmx%d" % i, [128, 1024]), Buf("x")) for i in range(2)])
            tp, btp = ps(ph, "pmtp", [128, 8, 128], BF16), Buf("tp", True)
            mps = Rot([(ps(ph, "pmps%d" % i, [128, 512]), Buf("mps", True)) for i in range(2)])
            bmemT = [Buf("memT0"), Buf("memT1")]
            for mt in range(2):
                xt, bx = xts.next()
                P.dma("sp", lambda e, xt=xt, mt=mt: e.dma_start(out=xt[:], in_=memd[mt * 128:(mt + 1) * 128, :]), [], [bx])
                norm_T(st, xt[:], bx, gmemT, memT[:, :, mt * 128:(mt + 1) * 128], bmemT[mt], tp, btp)
            for h in range(4):
                mp, bmp = mps.next()
                P.op("pe", [(lambda e, c=c, h=h, mp=mp: e.matmul(mp[:, 0:256], wckv[:, c, h * 128:(h + 1) * 128], memT[:, c, :], start=(c == 0), stop=(c == 7)))
                            for c in range(8)], bmemT + [B_cm], [bmp])
                P.op("act", lambda e, h=h, mp=mp: e.activation(out=kcT[:, h, :], in_=mp[:, 0:256], func=AF.Copy), [bmp], [])
            for mt in range(2):
                mp, bmp = mps.next()
                P.op("pe", [(lambda e, c=c, mt=mt, mp=mp: e.matmul(mp[:], memT[:, c, mt * 128:(mt + 1) * 128], wckv[:, c, 512:1024], start=(c == 0), stop=(c == 7)))
                            for c in range(8)], bmemT + [B_cm], [bmp])
                P.op("act", lambda e, mt=mt, mp=mp: e.activation(out=Vc[:, mt, :], in_=mp[:], func=AF.Copy), [bmp], [])
            P.flush()

        with contextlib.ExitStack() as ph:
            wsv = sb(ph, "wsv", [128, 8, 512], BF16)
            wsu = sb(ph, "wsu", [128, 8, 512], BF16)
            wga = sb(ph, "wga", [128, 8, 1024], BF16)
            wgb = sb(ph, "wgb", [128, 8, 1024], BF16)
            wso = sb(ph, "wso", [128, 4, 1024], BF16)
            wo = sb(ph, "wo", [128, 8, 1024], BF16)
            wcq = sb(ph, "wcq", [128, 8, 512], BF16)
            wco = sb(ph, "wco", [128, 4, 1024], BF16)
            gcrT = sb(ph, "gcrT", [128, 8])
            lng_bc = sb(ph, "lng_bc", [128, 512])
            lnb_bc = sb(ph, "lnb_bc", [128, 512])
            bs_bc = sb(ph, "bs_bc", [128, 4, 128])
            wsraw = sb(ph, "wsraw", [128, 4, 128], BF16)
            WsT = sb(ph, "WsT", [128, 4, 128], BF16)
            B_c = Buf("c2b")
            B_ws = Buf("ws")
            P.dma("pool", lambda e: e.dma_start(out=wsu[:], in_=wv(w_in, 768, 1280)), [], [B_c])
            P.dma("pool", lambda e: e.dma_start(out=wsv[:], in_=wv(w_in, 1280, 1792)), [], [B_c])
            P.dma("pool", lambda e: e.dma_start(out=wga[:], in_=wv(w_in, 1792, 2816)), [], [B_c])
            P.dma("pool", lambda e: e.dma_start(out=wgb[:], in_=wv(w_in, 2816, 3840)), [], [B_c])
            P.dma("pool", lambda e: e.dma_start(out=wso[:], in_=wv(w_sgu_out, 0, 1024)), [], [B_c])
            P.dma("pool", lambda e: e.dma_start(out=wo[:], in_=wv(w_out, 0, 1024)), [], [B_c])
            P.dma("pool", lambda e: e.dma_start(out=wcq[:], in_=wv(w_cq, 0, 512)), [], [B_c])
            P.dma("pool", lambda e: e.dma_start(out=wco[:], in_=wv(w_co, 0, 1024)), [], [B_c])
            P.dma("pool", lambda e: e.dma_start(out=wsraw[:], in_=w_s.rearrange("g p q -> p g q")), [], [B_ws])
            P.dma("sp", lambda e: e.dma_start(out=gcrT[:], in_=gT_view(g_cross), allow_slow_non_contiguous=True), [], [B_c])
            P.dma("sp", lambda e: e.dma_start(out=lng_bc[:], in_=sgu_ln_g.partition_broadcast(128)), [], [B_c])
            P.dma("sp", lambda e: e.dma_start(out=lnb_bc[:], in_=sgu_ln_b.partition_broadcast(128)), [], [B_c])
            P.dma("sp", lambda e: e.dma_start(out=bs_bc[:].rearrange("p g q -> p (g q)"), in_=b_s.rearrange("g q -> (g q)").partition_broadcast(128)), [], [B_c])
            st = norm_state(ph, "p3")
            tpb, btpb = ps(ph, "p3tp", [128, 8, 128], BF16), Buf("tp", True)
            gen = Rot([(ps(ph, "p3g%d" % i, [128, 512]), Buf("g", True)) for i in range(5)])
            scps, bsc = ps(ph, "p3sc", [128, 4, 256]), Buf("sc", True)
            P.op("pe", [(lambda e, g=g: e.transpose(tpb[:, g, :], wsraw[:, g, :], ident[:])) for g in range(4)], [B_ws], [btpb])
            P.op("act", lambda e: e.activation(out=WsT[:], in_=tpb[:, 0:4, :], func=AF.Copy), [btpb], [B_c])

            xg = [(sb(ph, "p3x%d" % i, [128, 1024]), Buf("x")) for i in range(4)]
            hTg, bhT = sb(ph, "p3hT", [128, 8, 512], BF16), [Buf("hT") for _ in range(4)]
            hcT, bhc = hTg, bhT
            gv, bgv = sb(ph, "p3gv", [128, 512]), Buf("gv")
            lst, blst = sb(ph, "p3lst", [128, 16]), [Buf("l%d" % i) for i in range(8)]
            vn, bvn = sb(ph, "p3vn", [128, 512], BF16), Buf("vn")
            vnf, bvnf = sb(ph, "p3vnf", [128, 512]), Buf("vnf")
            vmb, bvmb = sb(ph, "p3vmb", [128, 4, 512]), [Buf("vmb") for _ in range(4)]
            gu, bgu = sb(ph, "p3gu", [128, 512]), Buf("gu")
            sguT, bsg = sb(ph, "p3sguT", [128, 4, 512], BF16), [Buf("sg") for _ in range(4)]
            yatts = Rot([(sb(ph, "p3yatt%d" % i, [128, 512]), Buf("ya")) for i in range(2)])
            sa, bsa = sb(ph, "p3sa", [128, 512]), Buf("sa")
            sbt, bsb = sb(ph, "p3sb", [128, 512]), Buf("sb")
            m1, bm1 = sb(ph, "p3m1", [128, 512]), Buf("m1")
            m2, bm2 = sb(ph, "p3m2", [128, 512]), Buf("m2")
            mrg, bmrg = sb(ph, "p3mrg", [128, 8, 512], BF16), [Buf("mrg") for _ in range(8)]
            qcT, bqc = sb(ph, "p3qcT", [128, 4, 512], BF16), [Buf("qc") for _ in range(4)]
            cst, bcst = sb(ph, "p3cst", [128, 16]), [Buf("c%d" % i) for i in range(4)]
            pc, bpc = sb(ph, "p3pc", [128, 4, 256]), Buf("pc")
            pn, bpn = sb(ph, "p3pn", [128, 4, 256], BF16), Buf("pn")
            pnT, bpnT = sb(ph, "p3pnT", [128, 8, 128], BF16), Buf("pnT")
            ocT, boc = sb(ph, "p3ocT", [128, 4, 128], BF16), Buf("oc")

            for gi in range(8):
                for j in range(4):
                    tile = gi * 4 + j
                    xt, bx = xg[j]
                    P.dma("sp", lambda e, xt=xt, tile=tile: e.dma_start(out=xt[:], in_=xown[tile * 128:(tile + 1) * 128, :]), [], [bx])
                    norm_T(st, xt[:], bx, gmixT, hTg[:, :, j * 128:(j + 1) * 128], bhT[j], tpb, btpb)
                    g0, bg0 = gen.next()
                    P.op("pe", [(lambda e, c=c, j=j, g0=g0: e.matmul(g0[:], hTg[:, c, j * 128:(j + 1) * 128], wsv[:, c, :], start=(c == 0), stop=(c == 7)))
                                for c in range(8)], [bhT[j], B_c], [bg0])
                    P.op("act", lambda e, g0=g0: e.activation(out=gv[:], in_=g0[:], func=AF.Gelu_apprx_tanh), [bg0], [bgv])
                    P.op("dve", lambda e: e.tensor_reduce(out=lst[:, 0:1], in_=gv[:], axis=AX.X, op=ALU.add), [bgv], [blst[0]])
                    P.op("act", lambda e: e.activation(out=vnf[:], in_=gv[:], func=AF.Square, accum_out=lst[:, 1:2]), [bgv], [bvnf, blst[1]])
                    P.op("dve", lambda e: e.tensor_scalar(out=lst[:, 2:3], in0=lst[:, 0:1], scalar1=1.0 / 512, scalar2=None, op0=ALU.mult), [blst[0]], [blst[2]])
                    P.op("dve", lambda e: e.tensor_tensor(out=lst[:, 3:4], in0=lst[:, 2:3], in1=lst[:, 2:3], op=ALU.mult), [blst[2]], [blst[3]])
                    P.op("dve", lambda e: e.scalar_tensor_tensor(out=lst[:, 4:5], in0=lst[:, 1:2], scalar=1.0 / 512, in1=lst[:, 3:4], op0=ALU.mult, op1=ALU.subtract),
                         [blst[1], blst[3]], [blst[4]])
                    P.op("dve", lambda e: e.tensor_scalar(out=lst[:, 5:6], in0=lst[:, 4:5], scalar1=EPS, scalar2=None, op0=ALU.add), [blst[4]], [blst[5]])
                    P.op("act", lambda e: e.activation(out=lst[:, 6:7], in_=lst[:, 5:6], func=AF.Ln), [blst[5]], [blst[6]])
                    P.op("act", lambda e: e.activation(out=lst[:, 7:8], in_=lst[:, 6:7], func=AF.Exp, scale=-0.5), [blst[6]], [blst[7]])
                    P.op("dve", lambda e: e.tensor_scalar(out=vnf[:], in0=gv[:], scalar1=lst[:, 2:3], scalar2=lst[:, 7:8], op0=ALU.subtract, op1=ALU.mult),
                         [bgv, blst[2], blst[7]], [bvnf])
                    P.op("dve", lambda e: e.tensor_tensor(out=vnf[:], in0=vnf[:], in1=lng_bc[:], op=ALU.mult), [bvnf, B_c], [bvnf])
                    P.op("dve", lambda e: e.tensor_tensor(out=vn[:], in0=vnf[:], in1=lnb_bc[:], op=ALU.add), [bvnf, B_c], [bvn])
                    g1, bg1 = gen.next()
                    P.op("pe", [(lambda e, g=g, g1=g1: e.matmul(g1[:, g * 128:(g + 1) * 128], vn[:, g * 128:(g + 1) * 128], WsT[:, g, :], start=True, stop=True))
                                for g in range(4)], [bvn, B_c], [bg1])
                    P.op("dve", lambda e, g1=g1, j=j: e.tensor_tensor(out=vmb[:, :, j * 128:(j + 1) * 128], in0=g1[:].rearrange("p (g q) -> p g q", g=4),
                                                                     in1=bs_bc[:], op=ALU.add), [bg1, B_c], [bvmb[j]])
                for g in range(4):
                    g0, bg0 = gen.next()
                    P.op("pe", [(lambda e, c=c, g=g, g0=g0: e.matmul(g0[:], wsu[:, c, g * 128:(g + 1) * 128], hTg[:, c, :], start=(c == 0), stop=(c == 7)))
                                for c in range(8)], bhT + [B_c], [bg0])
                    P.op("act", lambda e, g0=g0: e.activation(out=gu[:], in_=g0[:], func=AF.Gelu_apprx_tanh), [bg0], [bgu])
                    P.op("dve", lambda e, g=g: e.tensor_tensor(out=sguT[:, g, :], in0=gu[:], in1=vmb[:, g, :], op=ALU.mult), [bgu] + bvmb, [bsg[g]])
                for dc in range(8):
                    ga_ps, bga = gen.next()
                    P.op("pe", [(lambda e, c=c, dc=dc, ga_ps=ga_ps: e.matmul(ga_ps[:], wga[:, c, dc * 128:(dc + 1) * 128], hTg[:, c, :], start=(c == 0), stop=(c == 7)))
                                for c in range(8)], bhT + [B_c], [bga])
                    P.op("act", lambda e, ga_ps=ga_ps: e.activation(out=sa[:], in_=ga_ps[:], func=AF.Sigmoid), [bga], [bsa])
                    gb_ps, bgb = gen.next()
                    P.op("pe", [(lambda e, c=c, dc=dc, gb_ps=gb_ps: e.matmul(gb_ps[:], wgb[:, c, dc * 128:(dc + 1) * 128], hTg[:, c, :], start=(c == 0), stop=(c == 7)))
                                for c in range(8)], bhT + [B_c], [bgb])
                    P.op("act", lambda e, gb_ps=gb_ps: e.activation(out=sbt[:], in_=gb_ps[:], func=AF.Sigmoid), [bgb], [bsb])
                    ys_ps, bysp = gen.next()
                    P.op("pe", [(lambda e, g=g, dc=dc, ys_ps=ys_ps: e.matmul(ys_ps[:], wso[:, g, dc * 128:(dc + 1) * 128], sguT[:, g, :], start=(g == 0), stop=(g == 3)))
                                for g in range(4)], bsg + [B_c], [bysp])
                    yatt, bya = yatts.next()
                    P.dma("sp", lambda e, gi=gi, dc=dc, yatt=yatt: e.dma_start(out=yatt[:], in_=yatt_d[gi][:, dc, :]), [], [bya])
                    P.op("dve", lambda e, yatt=yatt: e.tensor_tensor(out=m1[:], in0=sa[:], in1=yatt[:], op=ALU.mult), [bsa, bya], [bm1])
                    P.op("dve", lambda e, ys_ps=ys_ps: e.tensor_tensor(out=m2[:], in0=sbt[:], in1=ys_ps[:], op=ALU.mult), [bsb, bysp], [bm2])
                    P.op("dve", lambda e, dc=dc: e.tensor_tensor(out=mrg[:, dc, :], in0=m1[:], in1=m2[:], op=ALU.add), [bm1, bm2], [bmrg[dc]])
                for j in range(4):
                    xt, bx = xg[j]
                    for nb in range(2):
                        o_ps, bo = gen.next()
                        P.op("pe", [(lambda e, dc=dc, j=j, nb=nb, o_ps=o_ps: e.matmul(o_ps[:], mrg[:, dc, j * 128:(j + 1) * 128], wo[:, dc, nb * 512:(nb + 1) * 512],
                                                                                     start=(dc == 0), stop=(dc == 7))) for dc in range(8)], bmrg + [B_c], [bo])
                        P.op("dve", lambda e, xt=xt, nb=nb, o_ps=o_ps: e.tensor_tensor(out=xt[:, nb * 512:(nb + 1) * 512], in0=xt[:, nb * 512:(nb + 1) * 512], in1=o_ps[:], op=ALU.add),
                             [bo, bx], [bx])
                for j in range(4):
                    xt, bx = xg[j]
                    norm_T(st, xt[:], bx, gcrT, hcT[:, :, j * 128:(j + 1) * 128], bhc[j], tpb, btpb)
                for h in range(4):
                    q_ps, bq = gen.next()
                    P.op("pe", [(lambda e, c=c, h=h, q_ps=q_ps: e.matmul(q_ps[:], wcq[:, c, h * 128:(h + 1) * 128], hcT[:, c, :], start=(c == 0), stop=(c == 7)))
                                for c in range(8)], bhc + [B_c], [bq])
                    P.op("act", lambda e, h=h, q_ps=q_ps: e.activation(out=qcT[:, h, :], in_=q_ps[:], func=AF.Copy, scale=float(128 ** -0.5)), [bq], [bqc[h]])
                for j in range(4):
                    tile = gi * 4 + j
                    xt, bx = xg[j]
                    P.op("pe", [(lambda e, h=h, j=j: e.matmul(scps[:, h, :], qcT[:, h, j * 128:(j + 1) * 128], kcT[:, h, :], start=True, stop=True)) for h in range(4)],
                         bqc, [bsc])
                    P.op("dve", lambda e: e.tensor_reduce(out=cst[:, 0:4], in_=scps[:], axis=AX.X, op=ALU.max), [bsc], [bcst[0]])
                    P.op("dve", lambda e: e.tensor_scalar(out=cst[:, 4:8], in0=cst[:, 0:4], scalar1=-1.0, scalar2=None, op0=ALU.mult), [bcst[0]], [bcst[1]])
                    P.op("act", [(lambda e, h=h: e.activation(out=pc[:, h, :], in_=scps[:, h, :], func=AF.Exp, bias=cst[:, 4 + h:5 + h], accum_out=cst[:, 8 + h:9 + h]))
                                 for h in range(4)], [bsc, bcst[1]], [bpc, bcst[2]])
                    P.op("dve", lambda e: e.reciprocal(out=cst[:, 12:16], in_=cst[:, 8:12]), [bcst[2]], [bcst[3]])
                    P.op("dve", lambda e: e.tensor_tensor(out=pn[:], in0=pc[:], in1=cst[:, 12:16].unsqueeze(2).broadcast_to([128, 4, 256]), op=ALU.mult),
                         [bpc, bcst[3]], [bpn])
                    P.op("pe", [(lambda e, k=k: e.transpose(tpb[:, k, :], pn[:, k // 2, (k % 2) * 128:(k % 2 + 1) * 128], ident[:])) for k in range(8)], [bpn], [btpb])
                    P.op("act", lambda e: e.activation(out=pnT[:], in_=tpb[:], func=AF.Copy), [btpb], [bpnT])
                    oc_ps, bocp = gen.next()
                    fl = []
                    for h in range(4):
                        for mt in range(2):
                            fl.append(lambda e, h=h, mt=mt, oc_ps=oc_ps: e.matmul(oc_ps[:, h * 128:(h + 1) * 128], Vc[:, mt, h * 128:(h + 1) * 128], pnT[:, h * 2 + mt, :],
                                                                                  start=(mt == 0), stop=(mt == 1)))
                    P.op("pe", fl, [bpnT], [bocp])
                    P.op("act", lambda e, oc_ps=oc_ps: e.activation(out=ocT[:], in_=oc_ps[:].rearrange("p (h t) -> p h t", h=4), func=AF.Copy), [bocp], [boc])
                    for nb in range(2):
                        y_ps, byp = gen.next()
                        P.op("pe", [(lambda e, h=h, nb=nb, y_ps=y_ps: e.matmul(y_ps[:], ocT[:, h, :], wco[:, h, nb * 512:(nb + 1) * 512], start=(h == 0), stop=(h == 3)))
                                    for h in range(4)], [boc, B_c], [byp])
                        P.op("dve", lambda e, xt=xt, nb=nb, y_ps=y_ps: e.tensor_tensor(out=xt[:, nb * 512:(nb + 1) * 512], in0=xt[:, nb * 512:(nb + 1) * 512], in1=y_ps[:], op=ALU.add),
                             [byp, bx], [bx])
                    P.dma("sp", lambda e, xt=xt, tile=tile: e.dma_start(out=x2_d[tile * 128:(tile + 1) * 128, :], in_=xt[:]), [bx], [])
            P.flush()

        if dbg == 3:
            dx = nc.dram_tensor("dbg_x2", [OWN, D], F32, kind="ExternalOutput").ap()
            P.dma("sp", lambda e: e.dma_start(out=dx[:, :], in_=x2_d[:, :]), [], [])
            P.flush()
            return nc

        with contextlib.ExitStack() as ph:
            gmoT = sb(ph, "gmoT", [128, 8])
            wr = sb(ph, "wr", [128, 8, 20], BF16)
            br_bc = sb(ph, "br_bc", [128, 20])
            gfin_bc = sb(ph, "gfin_bc", [128, 1024])
            B_c = Buf("c3")
            P.dma("sp", lambda e: e.dma_start(out=gmoT[:], in_=gT_view(g_moe), allow_slow_non_contiguous=True), [], [B_c])
            P.dma("pool", lambda e: e.dma_start(out=wr[:, :, 0:4], in_=wv(w_rg, 0, 4)), [], [B_c])
            P.dma("pool", lambda e: e.dma_start(out=wr[:, :, 4:20], in_=wv(w_re, 0, 16)), [], [B_c])
            P.dma("sp", lambda e: e.dma_start(out=br_bc[:, 0:4], in_=b_rg.partition_broadcast(128)), [], [B_c])
            P.dma("sp", lambda e: e.dma_start(out=br_bc[:, 4:20], in_=b_re.partition_broadcast(128)), [], [B_c])
            P.dma("sp", lambda e: e.dma_start(out=gfin_bc[:], in_=g_final.partition_broadcast(128)), [], [B_c])
            st = norm_state(ph, "p4")
            xres, bxr = sb(ph, "xres", [128, 16, 1024]), [Buf("xr%d" % i) for i in range(16)]
            hmT, bhm = sb(ph, "hmT", [128, 8, 2048], BF16), [Buf("hm%d" % i) for i in range(16)]
            comb, bcomb = sb(ph, "comb", [128, 16, 16]), [Buf("cb%d" % i) for i in range(16)]
            Wg = [(sb(ph, "Wg%d" % i, [128, 8, 512], BF16), Buf("Wg")) for i in range(2)]
            Wu = [(sb(ph, "Wu%d" % i, [128, 8, 512], BF16), Buf("Wu")) for i in range(2)]
            Wd = [(sb(ph, "Wd%d" % i, [128, 4, 1024], BF16), Buf("Wd")) for i in range(2)]
            sgs = Rot([(sb(ph, "sg%d" % i, [128, 512]), Buf("sg")) for i in range(2)])
            heTs = Rot([(sb(ph, "heT%d" % i, [128, 4, 512], BF16), [Buf("he") for _ in range(4)]) for i in range(2)])
            ots = Rot([(sb(ph, "ot%d" % i, [128, 1024]), Buf("ot")) for i in range(2)])
            rt = sb(ph, "rt", [128, 160])
            brt = [Buf("rt%d" % i) for i in range(24)]
            tpb, btpb = ps(ph, "p4tp", [128, 8, 128], BF16), Buf("tp", True)
            gen = Rot([(ps(ph, "p4g%d" % i, [128, 512]), Buf("g", True)) for i in range(7)])

            for sgi in range(2):
                for tl in range(16):
                    tile = sgi * 16 + tl
                    P.dma("sp", lambda e, tl=tl, tile=tile: e.dma_start(out=xres[:, tl, :], in_=x2_d[tile * 128:(tile + 1) * 128, :]), [], [bxr[tl]])
                    norm_T(st, xres[:, tl, :], bxr[tl], gmoT, hmT[:, :, tl * 128:(tl + 1) * 128], bhm[tl], tpb, btpb)
                    r_ps, brp = gen.next()
                    P.op("pe", [(lambda e, c=c, tl=tl, r_ps=r_ps: e.matmul(r_ps[:, 0:20], hmT[:, c, tl * 128:(tl + 1) * 128], wr[:, c, :], start=(c == 0), stop=(c == 7)))
                                for c in range(8)], [bhm[tl], B_c], [brp])
                    R = rt
                    P.op("dve", lambda e, r_ps=r_ps: e.tensor_tensor(out=R[:, 0:20], in0=r_ps[:, 0:20], in1=br_bc[:], op=ALU.add), [brp, B_c], [brt[0]])
                    P.op("dve", lambda e: e.tensor_reduce(out=R[:, 20:21], in_=R[:, 0:4], axis=AX.X, op=ALU.max), [brt[0]], [brt[1]])
                    P.op("dve", lambda e: e.tensor_scalar(out=R[:, 21:22], in0=R[:, 20:21], scalar1=-1.0, scalar2=None, op0=ALU.mult), [brt[1]], [brt[2]])
                    P.op("act", lambda e: e.activation(out=R[:, 22:26], in_=R[:, 0:4], func=AF.Exp, bias=R[:, 21:22], accum_out=R[:, 26:27]), [brt[0], brt[2]], [brt[3]])
                    P.op("dve", lambda e: e.reciprocal(out=R[:, 27:28], in_=R[:, 26:27]), [brt[3]], [brt[4]])
                    P.op("dve", lambda e: e.tensor_scalar(out=R[:, 28:32], in0=R[:, 0:4], scalar1=R[:, 20:21], scalar2=None, op0=ALU.is_ge), [brt[0], brt[1]], [brt[5]])
                    P.op("dve", lambda e: e.tensor_scalar(out=R[:, 32:36], in0=R[:, 28:32], scalar1=BIG, scalar2=-BIG, op0=ALU.mult, op1=ALU.add), [brt[5]], [brt[6]])
                    P.op("dve", lambda e: e.tensor_tensor(out=R[:, 40:56].rearrange("p (g k) -> p g k", g=4), in0=R[:, 4:20].rearrange("p (g k) -> p g k", g=4),
                                                          in1=R[:, 32:36].unsqueeze(2).broadcast_to([128, 4, 4]), op=ALU.add), [brt[0], brt[6]], [brt[7]])
                    P.op("dve", lambda e: e.tensor_reduce(out=R[:, 56:57], in_=R[:, 40:56], axis=AX.X, op=ALU.max), [brt[7]], [brt[8]])
                    P.op("dve", lambda e: e.tensor_scalar(out=R[:, 60:76], in0=R[:, 40:56], scalar1=R[:, 56:57], scalar2=None, op0=ALU.is_ge), [brt[7], brt[8]], [brt[9]])
                    P.op("dve", lambda e: e.scalar_tensor_tensor(out=R[:, 80:96], in0=R[:, 60:76], scalar=-BIG, in1=R[:, 40:56], op0=ALU.mult, op1=ALU.add),
                         [brt[9], brt[7]], [brt[10]])
                    P.op("dve", lambda e: e.tensor_reduce(out=R[:, 96:97], in_=R[:, 80:96], axis=AX.X, op=ALU.max), [brt[10]], [brt[11]])
                    P.op("dve", lambda e: e.tensor_scalar(out=R[:, 100:116], in0=R[:, 80:96], scalar1=R[:, 96:97], scalar2=None, op0=ALU.is_ge), [brt[10], brt[11]], [brt[12]])
                    P.op("dve", lambda e: e.tensor_tensor(out=R[:, 116:117], in0=R[:, 96:97], in1=R[:, 56:57], op=ALU.subtract), [brt[11], brt[8]], [brt[13]])
                    P.op("act", lambda e: e.activation(out=R[:, 117:118], in_=R[:, 116:117], func=AF.Exp), [brt[13]], [brt[14]])
                    P.op("dve", lambda e: e.tensor_scalar(out=R[:, 118:119], in0=R[:, 117:118], scalar1=1.0, scalar2=None, op0=ALU.add), [brt[14]], [brt[15]])
                    P.op("dve", lambda e: e.reciprocal(out=R[:, 119:120], in_=R[:, 118:119]), [brt[15]], [brt[16]])
                    P.op("dve", lambda e: e.tensor_tensor(out=R[:, 120:121], in0=R[:, 119:120], in1=R[:, 27:28], op=ALU.mult), [brt[16], brt[4]], [brt[17]])
                    P.op("dve", lambda e: e.tensor_tensor(out=R[:, 121:122], in0=R[:, 120:121], in1=R[:, 117:118], op=ALU.mult), [brt[17], brt[14]], [brt[18]])
                    P.op("dve", lambda e, tl=tl: e.tensor_scalar(out=comb[:, tl, :], in0=R[:, 60:76], scalar1=R[:, 120:121], scalar2=None, op0=ALU.mult),
                         [brt[9], brt[17]], [bcomb[tl]])
                    P.op("dve", lambda e, tl=tl: e.scalar_tensor_tensor(out=comb[:, tl, :], in0=R[:, 100:116], scalar=R[:, 121:122], in1=comb[:, tl, :], op0=ALU.mult, op1=ALU.add),
                         [brt[12], brt[18], bcomb[tl]], [bcomb[tl]])

                def load_w(e_):
                    (wg, bwg), (wu, bwu), (wd, bwd) = Wg[e_ % 2], Wu[e_ % 2], Wd[e_ % 2]
                    P.dma("pool", lambda e: e.dma_start(out=wg[:], in_=wv(w_gate[e_], 0, 512)), [], [bwg])
                    P.dma("pool", lambda e: e.dma_start(out=wu[:], in_=wv(w_up[e_], 0, 512)), [], [bwu])
                    P.dma("pool", lambda e: e.dma_start(out=wd[:], in_=wv(w_down[e_], 0, 1024)), [], [bwd])

                load_w(0)
                for ex in range(NE):
                    if ex + 1 < NE:
                        load_w(ex + 1)
                    (wg, bwg), (wu, bwu), (wd, bwd) = Wg[ex % 2], Wu[ex % 2], Wd[ex % 2]
                    for tg in range(4):
                        heT, bhe = heTs.next()
                        for fc in range(4):
                            g_ps, bgp = gen.next()
                            u_ps, bup = gen.next()
                            P.op("pe", [(lambda e, c=c, fc=fc, tg=tg, g_ps=g_ps, wg=wg: e.matmul(g_ps[:], wg[:, c, fc * 128:(fc + 1) * 128], hmT[:, c, tg * 512:(tg + 1) * 512],
                                                                                                 start=(c == 0), stop=(c == 7))) for c in range(8)],
                                 bhm[tg * 4:tg * 4 + 4] + [bwg], [bgp])
                            P.op("pe", [(lambda e, c=c, fc=fc, tg=tg, u_ps=u_ps, wu=wu: e.matmul(u_ps[:], wu[:, c, fc * 128:(fc + 1) * 128], hmT[:, c, tg * 512:(tg + 1) * 512],
                                                                                                 start=(c == 0), stop=(c == 7))) for c in range(8)],
                                 bhm[tg * 4:tg * 4 + 4] + [bwu], [bup])
                            sg, bsg_ = sgs.next()
                            P.op("act", lambda e, sg=sg, g_ps=g_ps: e.activation(out=sg[:], in_=g_ps[:], func=AF.Silu), [bgp], [bsg_])
                            P.op("dve", lambda e, sg=sg, u_ps=u_ps, heT=heT, fc=fc: e.tensor_tensor(out=heT[:, fc, :], in0=sg[:], in1=u_ps[:], op=ALU.mult),
                                 [bsg_, bup], [bhe[fc]])
                        for j in range(4):
                            tl = tg * 4 + j
                            for nb in range(2):
                                y_ps, byp = gen.next()
                                P.op("pe", [(lambda e, fc=fc, j=j, nb=nb, y_ps=y_ps, heT=heT, wd=wd: e.matmul(y_ps[:], heT[:, fc, j * 128:(j + 1) * 128], wd[:, fc, nb * 512:(nb + 1) * 512],
                                                                                                             start=(fc == 0), stop=(fc == 3))) for fc in range(4)],
                                     bhe + [bwd], [byp])
                                P.op("dve", lambda e, tl=tl, nb=nb, y_ps=y_ps, ex=ex: e.scalar_tensor_tensor(
                                    out=xres[:, tl, nb * 512:(nb + 1) * 512], in0=y_ps[:], scalar=comb[:, tl, ex:ex + 1], in1=xres[:, tl, nb * 512:(nb + 1) * 512],
                                    op0=ALU.mult, op1=ALU.add), [byp, bcomb[tl], bxr[tl]], [bxr[tl]])
                for tl in range(16):
                    tile = sgi * 16 + tl
                    fss, fb, fjunk = st["sets"].next()
                    P.op("act", lambda e, tl=tl, fss=fss, fjunk=fjunk: e.activation(out=fjunk[:], in_=xres[:, tl, :], func=AF.Square, scale=1.0 / 32, accum_out=fss[:, 0:1]),
                         [bxr[tl]], [fb[0]])
                    P.op("dve", lambda e, fss=fss: e.tensor_scalar(out=fss[:, 1:2], in0=fss[:, 0:1], scalar1=EPS, scalar2=None, op0=ALU.add), [fb[0]], [fb[1]])
                    P.op("act", lambda e, fss=fss: e.activation(out=fss[:, 2:3], in_=fss[:, 1:2], func=AF.Ln), [fb[1]], [fb[2]])
                    P.op("act", lambda e, fss=fss: e.activation(out=fss[:, 3:4], in_=fss[:, 2:3], func=AF.Exp, scale=-0.5), [fb[2]], [fb[3]])
                    ot, bot = ots.next()
                    P.op("dve", lambda e, tl=tl, ot=ot, fss=fss: e.scalar_tensor_tensor(out=ot[:], in0=xres[:, tl, :], scalar=fss[:, 3:4], in1=gfin_bc[:], op0=ALU.mult, op1=ALU.mult),
                         [bxr[tl], fb[3], B_c], [bot])
                    P.dma("sp", lambda e, ot=ot, tile=tile: e.dma_start(out=out[tile * 128:(tile + 1) * 128, :], in_=ot[:]), [bot], [])
            P.flush()
    return nc


def _rope_tables():
    rows = SEQ // 64
    row_ids = np.repeat(np.arange(rows), 64).astype(np.float32)
    col_ids = np.tile(np.arange(64), rows).astype(np.float32)
    inv = (10000.0 ** (-np.arange(16, dtype=np.float32) / 16)).astype(np.float32)
    ang = np.concatenate([row_ids[:, None] * inv[None, :], col_ids[:, None] * inv[None, :]], axis=-1).astype(np.float32)
    return np.cos(ang).astype(np.float32), np.sin(ang).astype(np.float32)


def make_in_maps(inputs):
    f = lambda a: np.ascontiguousarray(np.asarray(a, dtype=np.float32))
    x = f(inputs["x"])
    mem = f(inputs["mem"])
    cos, sin = _rope_tables()

    def lay(t, n):
        return np.ascontiguousarray(t.reshape(n, 128, 32).transpose(1, 0, 2))

    shared = {
        "ident": np.eye(128, dtype=np.float32),
        "g_mix": f(inputs["g_mix"][0]), "w_in": f(inputs["w_in"][0]), "g_q": f(inputs["g_q"][0]), "g_k": f(inputs["g_k"][0]),
        "w_att_out": f(inputs["w_att_out"][0]), "sgu_ln_g": f(inputs["sgu_ln_g"][0]), "sgu_ln_b": f(inputs["sgu_ln_b"][0]),
        "w_s": f(inputs["w_s"][0]), "b_s": f(inputs["b_s"][0]), "w_sgu_out": f(inputs["w_sgu_out"][0]), "w_out": f(inputs["w_out"][0]),
        "g_cross": f(inputs["g_cross"][0]), "g_mem": f(inputs["g_mem"][0]), "w_cq": f(inputs["w_cq"][0]), "w_ckv": f(inputs["w_ckv"][0]),
        "w_co": f(inputs["w_co"][0]), "g_moe": f(inputs["g_moe"][0]), "w_rg": f(inputs["w_rg"][0]), "b_rg": f(inputs["b_rg"][0]),
        "w_re": f(inputs["w_re"][0]), "b_re": f(inputs["b_re"][0]), "w_gate": f(inputs["w_gate"][0]), "w_up": f(inputs["w_up"][0]),
        "w_down": f(inputs["w_down"][0]), "g_final": f(inputs["g_final"]),
        "cos_seq": lay(cos, 64), "sin_seq": lay(sin, 64),
    }
    maps = []
    for c in range(8):
        b, hf = c // 2, c % 2
        m = dict(shared)
        m["xseq"] = x[b]
        m["xown"] = np.ascontiguousarray(x[b, hf * OWN:(hf + 1) * OWN])
        m["mem"] = mem[b]
        m["cos_own"] = lay(cos[hf * OWN:(hf + 1) * OWN], 32)
        m["sin_own"] = lay(sin[hf * OWN:(hf + 1) * OWN], 32)
        maps.append(m)
    return maps


def kernel(**inputs):
    nc = build()
    maps = make_in_maps(inputs)
    res = run_bass_kernel_spmd(nc, maps, core_ids=list(range(8)))
    outp = np.empty((4, SEQ, D), np.float32)
    for c in range(8):
        b, hf = c // 2, c % 2
        outp[b, hf * OWN:(hf + 1) * OWN] = np.asarray(res.results[c]["out"], dtype=np.float32)
    return outp
```

```python
import contextlib
import os as _os0
import numpy as np
import concourse.bass as bass
import concourse.mybir as mybir
from concourse.bass_utils import run_bass_kernel_spmd

F32 = mybir.dt.float32
BF16 = mybir.dt.bfloat16
AF = mybir.ActivationFunctionType
ALU = mybir.AluOpType
AX = mybir.AxisListType

D = 1024
SEQ = 8192
OWN = 4096
NMEM = 256
EPS = 1e-6
NE = 16
BIG = 1.0e30


class Buf:
    __slots__ = ("name", "w", "r", "psum")

    def __init__(self, name="", psum=False):
        self.name = name
        self.psum = psum
        self.w = None
        self.r = {}


class Eng:
    def __init__(self, name):
        self.name = name
        self.sem = None
        self.count = 0
        self.seen = {}
        self.prog = []
        self.chsems = []
        self.dma_i = 0


class Prog:
    NCH = int(_os0.environ.get('NCH', 8))

    def __init__(self, nc, stack):
        self.nc = nc
        self.E = {}
        for n in ("pe", "act", "dve", "pool", "sp"):
            e = Eng(n)
            e.sem = stack.enter_context(nc.semaphore("sem_" + n))
            self.E[n] = e
        for n in ("sp", "pool", "act"):
            e = self.E[n]
            for i in range(self.NCH):
                e.chsems.append(stack.enter_context(nc.semaphore("ch_%s_%d" % (n, i))))
        self.chan_issued = {}
        import os
        self.limit = int(os.environ.get("OPLIMIT", 10 ** 9))
        self.nops = 0

    def _need(self, need, ev):
        sem, val, src = ev
        k = id(sem)
        if k not in need or need[k][1] < val:
            need[k] = (sem, val, src)

    def _deps(self, eng, reads, writes, extra=()):
        E = self.E[eng]
        need = {}
        for b in reads:
            if b.w is not None:
                self._need(need, b.w)
        for b in writes:
            if b.w is not None:
                self._need(need, b.w)
            for k, (sem, val, src) in b.r.items():
                self._need(need, (sem, val, src))
        for ev in extra:
            self._need(need, ev)
        for k, (sem, val, src) in need.items():
            if src == "pe" and eng == "pe":
                continue
            if E.seen.get(k, 0) >= val:
                continue
            E.seen[k] = val
            E.prog.append(("wait", sem, val))

    def _commit(self, ev, reads, writes):
        sem, val, src = ev
        for b in writes:
            b.w = ev
            b.r = {}
        for b in reads:
            if b not in writes:
                b.r[id(sem)] = (sem, val, src)

    def op(self, eng, fns, reads=(), writes=()):
        if not isinstance(fns, (list, tuple)):
            fns = [fns]
        self.nops += 1
        if self.nops > self.limit:
            return
        pr = [b for b in reads if b.psum]
        if pr:
            reads = [b for b in reads if not b.psum]
            writes = list(writes) + [b for b in pr if b not in writes]
        E = self.E[eng]
        self._deps(eng, reads, writes)
        E.count += 1
        for f in fns[:-1]:
            E.prog.append(("raw", f))
        E.prog.append(("op", fns[-1], E.sem))
        self._commit((E.sem, E.count, eng), reads, writes)

    def dma(self, q, fn, reads=(), writes=()):
        self.nops += 1
        if self.nops > self.limit:
            return
        E = self.E[q]
        ch = E.dma_i % self.NCH
        rnd = E.dma_i // self.NCH
        E.dma_i += 1
        chsem = E.chsems[ch]
        extra = []
        if rnd > 0:
            extra.append((chsem, 16 * rnd, "dma"))
        self._deps(q, reads, writes, extra)
        E.prog.append(("dma", fn, chsem))
        self.chan_issued[id(chsem)] = (chsem, 16 * (rnd + 1))
        self._commit((chsem, 16 * (rnd + 1), "dma"), reads, writes)

    def barrier(self):
        for n, E in self.E.items():
            for m, F in self.E.items():
                if m == n or F.count == 0:
                    continue
                k = id(F.sem)
                if E.seen.get(k, 0) < F.count:
                    E.seen[k] = F.count
                    E.prog.append(("wait", F.sem, F.count))
            for k, (sem, val) in self.chan_issued.items():
                if E.seen.get(k, 0) < val:
                    E.seen[k] = val
                    E.prog.append(("wait", sem, val))

    def flush(self):
        self.barrier()
        nc = self.nc
        progs = {n: E.prog for n, E in self.E.items()}
        for E in self.E.values():
            E.prog = []

        def replay(eng, prog):
            for it in prog:
                if it[0] == "wait":
                    eng.wait_ge(it[1], it[2])
                elif it[0] == "raw":
                    it[1](eng)
                elif it[0] == "op":
                    it[1](eng).then_inc(it[2], 1)
                else:
                    it[1](eng).then_inc(it[2], 16)

        with nc.Block() as block:
            @block.tensor
            def _(e):
                replay(e, progs["pe"])

            @block.scalar
            def _(e):
                replay(e, progs["act"])

            @block.vector
            def _(e):
                replay(e, progs["dve"])

            @block.gpsimd
            def _(e):
                replay(e, progs["pool"])

            @block.sync
            def _(e):
                replay(e, progs["sp"])


class Rot:
    def __init__(self, items):
        self.items = items
        self.i = 0

    def next(self):
        it = self.items[self.i % len(self.items)]
        self.i += 1
        return it


class Pool:
    def __init__(self, items):
        self.free = list(items)

    def acquire(self):
        return self.free.pop(0) if self.free else None

    def release(self, it):
        self.free.append(it)


def build(dbg=False):
    nc = bass.Bass("TRN2", target_bir_lowering=False)

    def din(name, shape):
        return nc.dram_tensor(name, list(shape), F32, kind="ExternalInput").ap()

    xseq = din("xseq", [SEQ, D])
    xown = din("xown", [OWN, D])
    memd = din("mem", [NMEM, D])
    cos_seq = din("cos_seq", [128, 64, 32])
    sin_seq = din("sin_seq", [128, 64, 32])
    cos_own = din("cos_own", [128, 32, 32])
    sin_own = din("sin_own", [128, 32, 32])
    identd = din("ident", [128, 128])
    g_mix = din("g_mix", [D])
    w_in = din("w_in", [D, 3840])
    g_q = din("g_q", [64])
    g_k = din("g_k", [64])
    w_att_out = din("w_att_out", [512, D])
    sgu_ln_g = din("sgu_ln_g", [512])
    sgu_ln_b = din("sgu_ln_b", [512])
    w_s = din("w_s", [4, 128, 128])
    b_s = din("b_s", [4, 128])
    w_sgu_out = din("w_sgu_out", [512, D])
    w_out = din("w_out", [D, D])
    g_cross = din("g_cross", [D])
    g_mem = din("g_mem", [D])
    w_cq = din("w_cq", [D, 512])
    w_ckv = din("w_ckv", [D, D])
    w_co = din("w_co", [512, D])
    g_moe = din("g_moe", [D])
    w_rg = din("w_rg", [D, 4])
    b_rg = din("b_rg", [4])
    w_re = din("w_re", [D, 16])
    b_re = din("b_re", [16])
    w_gate = din("w_gate", [NE, D, 512])
    w_up = din("w_up", [NE, D, 512])
    w_down = din("w_down", [NE, 512, D])
    g_final = din("g_final", [D])
    out = nc.dram_tensor("out", [OWN, D], F32, kind="ExternalOutput").ap()
    yatt_d = nc.dram_tensor("yatt_scr", [8, 128, 8, 512], F32, kind="Internal").ap()
    x2_d = nc.dram_tensor("x2_scr", [OWN, D], F32, kind="Internal").ap()

    def wv(w, c0, c1):
        return w.rearrange("(c p) n -> p c n", p=128)[:, :, c0:c1]

    def gT_view(g):
        return g.rearrange("(c p) -> p c", p=128)

    with contextlib.ExitStack() as top:
        P = Prog(nc, top)
        print("sbuf bytes remaining at start:", nc.sbuf_bytes_remaining)

        def sb(stack, name, shape, dt=F32):
            return stack.enter_context(nc.sbuf_tensor("s_" + name, list(shape), dt))

        def ps(stack, name, shape, dt=F32):
            return stack.enter_context(nc.psum_tensor("p_" + name, list(shape), dt))

        ident = sb(top, "ident", [128, 128], BF16)
        kcT = sb(top, "kcT", [128, 4, NMEM], BF16)
        Vc = sb(top, "Vc", [128, 2, 512], BF16)
        gmixT = sb(top, "gmixT", [128, 8])
        attn_stack = contextlib.ExitStack()
        kT_all = sb(attn_stack, "kT_all", [128, SEQ], BF16)
        V_all = sb(attn_stack, "V_all", [128, 64, 2, 65], BF16)
        B_const = Buf("const")

        P.dma("pool", lambda e: e.dma_start(out=ident[:], in_=identd[:, :]), [], [B_const])
        P.dma("sp", lambda e: e.dma_start(out=gmixT[:], in_=gT_view(g_mix), allow_slow_non_contiguous=True), [], [B_const])
        P.op("pool", lambda e: e.memset(V_all[:], 1.0), [], [B_const])

        def run_pipeline(gens, depth):
            it = iter(gens)
            active = []
            more = True
            while True:
                if more and len(active) < depth:
                    try:
                        active.append(next(it))
                    except StopIteration:
                        more = False
                if not active:
                    break
                for g in list(active):
                    try:
                        next(g)
                    except StopIteration:
                        active.remove(g)

        def drain(g):
            for _ in g:
                pass

        def acq(pool):
            while True:
                it = pool.acquire()
                if it is not None:
                    return it
                yield

        def bfview(bank_ap):
            return bank_ap.bitcast(BF16).rearrange("p (c t) -> p c t", c=8)

        def norm_T_gen(st, xt_ap, xbuf, gT, out_ap3, outbuf, tps, tpsb, pool=None):
            ss, b, junk = st["sets"].next()
            P.op("act", lambda e: e.activation(out=junk[:], in_=xt_ap, func=AF.Square, scale=1.0 / 32,
                                               accum_out=ss[:, 0:1]), [xbuf], [b[0]])
            yield
            P.op("dve", lambda e: e.tensor_scalar(out=ss[:, 1:2], in0=ss[:, 0:1], scalar1=EPS, scalar2=None,
                                                  op0=ALU.add), [b[0]], [b[1]])
            yield
            P.op("act", lambda e: e.activation(out=ss[:, 2:3], in_=ss[:, 1:2], func=AF.Ln), [b[1]], [b[2]])
            yield
            P.op("act", lambda e: e.activation(out=ss[:, 3:4], in_=ss[:, 2:3], func=AF.Exp, scale=-0.5), [b[2]], [b[3]])
            yield
            xb, bxb = st["xb"].next()
            P.op("act", lambda e: e.activation(out=xb[:], in_=xt_ap, func=AF.Copy, scale=ss[:, 3:4]), [xbuf, b[3]], [bxb])
            yield
            held = None
            if pool is not None:
                while held is None:
                    held = pool.acquire()
                    if held is None:
                        yield
                tps, tpsb = bfview(held[0][:]), held[1]
            P.op("pe", [(lambda e, c=c: e.transpose(tps[:, c, :], xb[:, c * 128:(c + 1) * 128], ident[:])) for c in range(8)],
                 [bxb], [tpsb])
            yield
            P.op("dve", lambda e: e.tensor_tensor(out=out_ap3, in0=tps, in1=gT[:, 0:8].unsqueeze(2).broadcast_to([128, 8, 128]),
                                                  op=ALU.mult), [tpsb], [outbuf])
            if held is not None:
                pool.release(held)
            yield

        def norm_T(st, xt_ap, xbuf, gT, out_ap3, outbuf, tps, tpsb):
            drain(norm_T_gen(st, xt_ap, xbuf, gT, out_ap3, outbuf, tps[:], tpsb))

        def norm_state(stack, pfx, nsets=2, nxb=2):
            st = {}
            st["sets"] = Rot([(sb(stack, pfx + "ss%d" % i, [128, 4]), [Buf("ss") for _ in range(4)],
                               sb(stack, pfx + "junk%d" % i, [128, 1024], BF16)) for i in range(nsets)])
            st["xb"] = Rot([(sb(stack, pfx + "xb%d" % i, [128, 1024], BF16), Buf("xb")) for i in range(nxb)])
            return st

        def head_norm_rope_gen(ws, src_ps, bsrc, H, g_bc, cosb, sinb, dst, bdst, t):
            w = ws.next()
            n = H * 64
            src3 = src_ps.rearrange("p (h d) -> p h d", h=H)
            P.op("act", lambda e: e.activation(out=w["sq"][:, 0:n], in_=src_ps, func=AF.Square, scale=0.125), [bsrc], [w["bsq"]])
            yield
            P.op("dve", lambda e: e.tensor_reduce(out=w["hs"][:, 0:H], in_=w["sq"][:, 0:n].rearrange("p (h d) -> p h d", h=H),
                                                  axis=AX.X, op=ALU.add), [w["bsq"]], [w["bhs"]])
            yield
            P.op("dve", lambda e: e.tensor_scalar(out=w["hs"][:, 8:8 + H], in0=w["hs"][:, 0:H], scalar1=EPS, scalar2=None, op0=ALU.add),
                 [w["bhs"]], [w["bhs1"]])
            yield
            P.op("act", lambda e: e.activation(out=w["hs"][:, 16:16 + H], in_=w["hs"][:, 8:8 + H], func=AF.Ln), [w["bhs1"]], [w["bhs2"]])
            yield
            P.op("act", lambda e: e.activation(out=w["hs"][:, 24:24 + H], in_=w["hs"][:, 16:16 + H], func=AF.Exp, scale=-0.5),
                 [w["bhs2"]], [w["bhs3"]])
            yield
            qn3 = w["qn"][:, 0:n].rearrange("p (h d) -> p h d", h=H)
            P.op("dve", lambda e: e.tensor_tensor(out=qn3, in0=src3, in1=w["hs"][:, 24:24 + H].unsqueeze(2).broadcast_to([128, H, 64]),
                                                  op=ALU.mult), [bsrc, w["bhs3"]], [w["bqn"]])
            yield
            P.op("dve", lambda e: e.tensor_tensor(out=qn3, in0=qn3, in1=g_bc[:, :].unsqueeze(1).broadcast_to([128, H, 64]),
                                                  op=ALU.mult), [w["bqn"]], [w["bqn"]])
            yield
            qn4 = w["qn"][:, 0:n].rearrange("p (h i t) -> p h i t", h=H, t=2)
            A4 = w["A"][:, 0:n].rearrange("p (h i t) -> p h i t", h=H, t=2)
            B4 = w["B"][:, 0:n].rearrange("p (h i t) -> p h i t", h=H, t=2)
            c4 = cosb[:, t, :].unsqueeze(1).unsqueeze(3).broadcast_to([128, H, 32, 2])
            s4 = sinb[:, t, :].unsqueeze(1).unsqueeze(3).broadcast_to([128, H, 32, 2])
            P.op("dve", lambda e: e.tensor_tensor(out=A4, in0=qn4, in1=c4, op=ALU.mult), [w["bqn"]], [w["bA"]])
            yield
            P.op("dve", lambda e: e.tensor_tensor(out=B4, in0=qn4, in1=s4, op=ALU.mult), [w["bqn"]], [w["bB"]])
            yield
            P.op("dve", lambda e: e.tensor_tensor(out=dst[:, :, :, 0], in0=A4[:, :, :, 0], in1=B4[:, :, :, 1], op=ALU.subtract),
                 [w["bA"], w["bB"]], [bdst])
            yield
            P.op("dve", lambda e: e.tensor_tensor(out=dst[:, :, :, 1], in0=B4[:, :, :, 0], in1=A4[:, :, :, 1], op=ALU.add),
                 [w["bA"], w["bB"]], [bdst])
            yield

        def rope_state(stack, pfx, n, nsets=1):
            sets = []
            for i in range(nsets):
                w = {}
                for k in ("sq", "qn", "A", "B"):
                    w[k] = sb(stack, "%s%s%d" % (pfx, k, i), [128, n])
                w["hs"] = sb(stack, "%shs%d" % (pfx, i), [128, 32])
                for k in ("bsq", "bhs", "bhs1", "bhs2", "bhs3", "bqn", "bA", "bB"):
                    w[k] = Buf(k)
                sets.append(w)
            return Rot(sets)

        with contextlib.ExitStack() as ph:
            wkv = sb(ph, "wkv", [128, 8, 256], BF16)
            cosb = sb(ph, "cosb", [128, 64, 32])
            sinb = sb(ph, "sinb", [128, 64, 32])
            gk_bc = sb(ph, "gk_bc", [128, 64])
            P.dma("pool", lambda e: e.dma_start(out=wkv[:], in_=wv(w_in, 512, 768)), [], [B_const])
            P.dma("sp", lambda e: e.dma_start(out=cosb[:], in_=cos_seq[:, :, :]), [], [B_const])
            P.dma("sp", lambda e: e.dma_start(out=sinb[:], in_=sin_seq[:, :, :]), [], [B_const])
            P.dma("sp", lambda e: e.dma_start(out=gk_bc[:], in_=g_k.partition_broadcast(128)), [], [B_const])
            NP1 = 8
            P.barrier()
            st = norm_state(ph, "p1", nsets=NP1, nxb=NP1)
            rws = rope_state(ph, "p1r", 128, nsets=NP1)
            xts = Rot([(sb(ph, "p1x%d" % i, [128, 1024]), Buf("x")) for i in range(NP1)])
            hTs = Rot([(sb(ph, "p1h%d" % i, [128, 8, 128], BF16), Buf("h")) for i in range(NP1)])
            krs = Rot([(sb(ph, "p1kr%d" % i, [128, 2, 32, 2], BF16), Buf("kr")) for i in range(NP1)])
            banks = Pool([(ps(ph, "p1b%d" % i, [128, 512]), Buf("bank", True)) for i in range(8)])

            import os as _os

            def p1_tile(t):
                xt, bx = xts.next()
                P.dma("sp", lambda e: e.dma_start(out=xt[:], in_=xseq[t * 128:(t + 1) * 128, :]), [], [bx])
                yield
                hT, bh = hTs.next()
                yield from norm_T_gen(st, xt[:], bx, gmixT, hT[:], bh, None, None, pool=banks)
                hkv = yield from acq(banks)
                kv, bkv = hkv
                P.op("pe", [(lambda e, c=c: e.matmul(kv[:, 0:256], hT[:, c, :], wkv[:, c, :], start=(c == 0), stop=(c == 7)))
                            for c in range(8)], [bh, B_const], [bkv])
                yield
                P.op("act", lambda e: e.activation(out=V_all[:, t, :, 0:64], in_=kv[:, 128:256].rearrange("p (h d) -> p h d", h=2),
                                                   func=AF.Copy), [bkv], [])
                yield
                kr, bkr = krs.next()
                yield from head_norm_rope_gen(rws, kv[:, 0:128], bkv, 2, gk_bc, cosb, sinb, kr, bkr, t)
                banks.release(hkv)
                hkt = yield from acq(banks)
                kt, bkt = hkt
                ktv = kt[:].bitcast(BF16)
                P.op("pe", lambda e: e.transpose(ktv[:, 0:128], kr[:].rearrange("p h i t -> p (h i t)"), ident[:]), [bkr], [bkt])
                yield
                P.op("act", lambda e: e.activation(out=kT_all[:, t * 128:(t + 1) * 128], in_=ktv[:, 0:128], func=AF.Copy), [bkt], [])
                banks.release(hkt)
                yield

            run_pipeline((p1_tile(t) for t in range(int(_os.environ.get('NT1', 64)))), int(_os.environ.get('DEPTH1', 8)))
            P.flush()

        if dbg == 1:
            P.limit = 10 ** 9
            P.limit = 10 ** 9
            print("nops", P.nops)
            dk = nc.dram_tensor("dbg_kT", [128, SEQ], BF16, kind="ExternalOutput").ap()
            dv = nc.dram_tensor("dbg_V", [128, 64 * 2 * 65], BF16, kind="ExternalOutput").ap()
            P.dma("sp", lambda e: e.dma_start(out=dk[:, :], in_=kT_all[:]), [], [])
            P.dma("sp", lambda e: e.dma_start(out=dv[:, :], in_=V_all[:].rearrange("p a b c -> p (a b c)")), [], [])
            P.flush()
            attn_stack.close()
            return nc

        with contextlib.ExitStack() as ph:
            wq = sb(ph, "wq", [128, 8, 512], BF16)
            wao = sb(ph, "wao", [64, 8, 1024], BF16)
            cosb = sb(ph, "cosb2", [128, 32, 32])
            sinb = sb(ph, "sinb2", [128, 32, 32])
            gq_bc = sb(ph, "gq_bc", [128, 64])
            ones65 = sb(ph, "ones65", [65, 64])
            B_c2 = Buf("c2")
            for hd_ in range(8):
                pos_ = (hd_ % 4) * 2 + hd_ // 4
                P.dma("pool", lambda e, hd_=hd_, pos_=pos_: e.dma_start(out=wq[:, :, pos_ * 64:(pos_ + 1) * 64], in_=wv(w_in, hd_ * 64, hd_ * 64 + 64)), [], [B_c2])
            P.dma("pool", lambda e: e.dma_start(out=wao[:], in_=w_att_out.rearrange("(h p) n -> p h n", p=64)), [], [B_c2])
            P.dma("sp", lambda e: e.dma_start(out=cosb[:], in_=cos_own[:, :, :]), [], [B_c2])
            P.dma("sp", lambda e: e.dma_start(out=sinb[:], in_=sin_own[:, :, :]), [], [B_c2])
            P.dma("sp", lambda e: e.dma_start(out=gq_bc[:], in_=g_q.partition_broadcast(128)), [], [B_c2])
            P.op("dve", lambda e: e.tensor_scalar(out=gq_bc[:], in0=gq_bc[:], scalar1=0.125, scalar2=None, op0=ALU.mult), [B_c2], [B_c2])
            P.op("pool", lambda e: e.memset(ones65[:], 1.0), [], [B_c2])
            P.barrier()
            st = norm_state(ph, "p2", nsets=4, nxb=4)
            rws = rope_state(ph, "p2r", 512, nsets=3)
            xts = Rot([(sb(ph, "p2x%d" % i, [128, 1024]), Buf("x")) for i in range(4)])
            hTs = Rot([(sb(ph, "p2h%d" % i, [128, 8, 128], BF16), Buf("h")) for i in range(4)])
            qrs = Rot([(sb(ph, "p2qr%d" % i, [128, 8, 32, 2], BF16), Buf("qr")) for i in range(4)])
            qTs = Rot([(sb(ph, "p2qT%d" % i, [128, 4, 512], BF16), [Buf("qT") for _ in range(4)]) for i in range(2)])
            Ps = Rot([(sb(ph, "p2P%d" % i, [128, 2, 512], BF16), Buf("P")) for i in range(4)])
            Osbs = Rot([(sb(ph, "p2Osb%d" % i, [65, 512]), Buf("Osb")) for i in range(2)])
            rrs = Rot([(sb(ph, "p2rr%d" % i, [65, 512]), Buf("rr")) for i in range(2)])
            OnTs = Rot([(sb(ph, "p2OnT%d" % i, [64, 8, 512], BF16), [Buf("OnT") for _ in range(8)]) for i in range(2)])
            ysbs = Rot([(sb(ph, "p2ysb%d" % i, [128, 8, 512]), [Buf("ysb") for _ in range(8)]) for i in range(1)])
            Sps = Rot([(ps(ph, "p2S%d" % i, [128, 2, 512]), Buf("S", True)) for i in range(3)])
            Ops, bO = ps(ph, "p2O", [128, 2, 512]), [Buf("O0", True), Buf("O1", True)]
            LA = int(_os0.environ.get('LA', 2))

            def q_tile(gi, j, qT, bqT):
                tile = gi * 4 + j
                xt, bx = xts.next()
                P.dma("sp", lambda e: e.dma_start(out=xt[:], in_=xown[tile * 128:(tile + 1) * 128, :]), [], [bx])
                yield
                hT, bh = hTs.next()
                S, bS = Sps.next()
                tpv = bfview(S[:, 1, :])
                yield from norm_T_gen(st, xt[:], bx, gmixT, hT[:], bh, tpv, bS)
                P.op("pe", [(lambda e, c=c: e.matmul(S[:, 0, :], hT[:, c, :], wq[:, c, :], start=(c == 0), stop=(c == 7)))
                            for c in range(8)], [bh, B_c2], [bS])
                yield
                qr, bqr = qrs.next()
                yield from head_norm_rope_gen(rws, S[:, 0, :], bS, 8, gq_bc, cosb, sinb, qr, bqr, tile)
                qr3 = qr[:].rearrange("p h i t -> p (h i t)")
                P.op("pe", [(lambda e, g=g: e.transpose(tpv[:, g, :], qr3[:, g * 128:(g + 1) * 128], ident[:])) for g in range(4)],
                     [bqr], [bS])
                yield
                P.op("act", lambda e: e.activation(out=qT[:, :, j * 128:(j + 1) * 128], in_=tpv[:, 0:4, :], func=AF.Copy),
                     [bS], [bqT[j]])
                yield

            for gi in range(8):
                qT, bqT = qTs.next()
                run_pipeline((q_tile(gi, j, qT, bqT) for j in range(4)), int(_os0.environ.get('QD', 3)))
                OnT, bOn = OnTs.next()
                for g in range(4):
                    def issue_S(kt, g=g, qT=qT):
                        S, bS = Sps.next()
                        P.op("pe", [lambda e, S=S, kt=kt: e.matmul(S[:, 0, :], kT_all[0:64, kt * 128:(kt + 1) * 128], qT[0:64, g, :], start=True, stop=True),
                                    lambda e, S=S, kt=kt: e.matmul(S[:, 1, :], kT_all[64:128, kt * 128:(kt + 1) * 128], qT[64:128, g, :], start=True, stop=True)],
                             bqT, [bS])
                        return S, bS
                    pend = [issue_S(kt) for kt in range(LA)]
                    for kt in range(64):
                        if kt + LA < 64:
                            pend.append(issue_S(kt + LA))
                        S, bS = pend.pop(0)
                        Pt, bP = Ps.next()
                        P.op("act", lambda e, S=S, Pt=Pt: e.activation(out=Pt[:], in_=S[:], func=AF.Exp), [bS], [bP])
                        P.op("pe", [lambda e, Pt=Pt, kt=kt: e.matmul(Ops[0:65, 0, :], V_all[:, kt, 0, :], Pt[:, 0, :], start=(kt == 0), stop=(kt == 63)),
                                    lambda e, Pt=Pt, kt=kt: e.matmul(Ops[0:65, 1, :], V_all[:, kt, 1, :], Pt[:, 1, :], start=(kt == 0), stop=(kt == 63))],
                             [bP], bO)
                    for hh in range(2):
                        head = g + 4 * hh
                        Osb, bOsb = Osbs.next()
                        rr, brr = rrs.next()
                        mA, bmA = Sps.next()
                        P.op("act", lambda e, Osb=Osb, hh=hh: e.activation(out=Osb[:], in_=Ops[0:65, hh, :], func=AF.Copy), [bO[hh]], [bOsb])
                        P.op("dve", lambda e, Osb=Osb, rr=rr: e.reciprocal(out=rr[64:65, :], in_=Osb[64:65, :]), [bOsb], [brr])
                        P.op("pe", lambda e, rr=rr, mA=mA: e.matmul(mA[0:64, 0, :], ones65[64:65, 0:64], rr[64:65, :], start=True, stop=True), [brr, B_c2], [bmA])
                        P.op("dve", lambda e, Osb=Osb, OnT=OnT, head=head, mA=mA: e.tensor_tensor(out=OnT[:, head, :], in0=Osb[0:64, :], in1=mA[0:64, 0, :], op=ALU.mult),
                             [bOsb, bmA], [bOn[head]])
                ysb, bys = ysbs.next()
                for dc in range(8):
                    mA, bmA = Sps.next()
                    P.op("pe", [(lambda e, h=h, dc=dc, OnT=OnT, mA=mA: e.matmul(mA[:, 0, :], wao[:, h, dc * 128:(dc + 1) * 128], OnT[:, h, :], start=(h == 0), stop=(h == 7)))
                                for h in range(8)], bOn + [B_c2], [bmA])
                    P.op("act", lambda e, ysb=ysb, dc=dc, mA=mA: e.activation(out=ysb[:, dc, :], in_=mA[:, 0, :], func=AF.Copy), [bmA], [bys[dc]])
                P.dma("sp", lambda e, ysb=ysb, gi=gi: e.dma_start(out=yatt_d[gi], in_=ysb[:]), bys, [])
            P.flush()

        attn_stack.close()
        if dbg == 2:
            dy = nc.dram_tensor("dbg_yatt", [8, 128, 8, 512], F32, kind="ExternalOutput").ap()
            P.dma("sp", lambda e: e.dma_start(out=dy.rearrange("a p c t -> (a p) (c t)"), in_=yatt_d.rearrange("a p c t -> (a p) (c t)")), [], [])
            P.flush()
            return nc

        with contextlib.ExitStack() as ph:
            wckv = sb(ph, "wckv", [128, 8, 1024], BF16)
            gmemT = sb(ph, "gmemT", [128, 8])
            memT = sb(ph, "memT", [128, 8, 256], BF16)
            B_cm = Buf("cm")
            P.dma("pool", lambda e: e.dma_start(out=wckv[:], in_=wv(w_ckv, 0, 1024)), [], [B_cm])
            P.dma("sp", lambda e: e.dma_start(out=gmemT[:], in_=gT_view(g_mem), allow_slow_non_contiguous=True), [], [B_cm])
            st = norm_state(ph, "pm")
            xts = Rot([(sb(ph, "pmx%d" % i, [128, 1024]), Buf("x")) for i in range(2)])
            tp, btp = ps(ph, "pmtp", [128, 8, 128], BF16), Buf("tp", True)
            mps = Rot([(ps(ph, "pmps%d" % i, [128, 512]), Buf("mps", True)) for i in range(2)])
            bmemT = [Buf("memT0"), Buf("memT1")]
            for mt in range(2):
                xt, bx = xts.next()
                P.dma("sp", lambda e, xt=xt, mt=mt: e.dma_start(out=xt[:], in_=memd[mt * 128:(mt + 1) * 128, :]), [], [bx])
                norm_T(st, xt[:], bx, gmemT, memT[:, :, mt * 128:(mt + 1) * 128], bmemT[mt], tp, btp)
            for h in range(4):
                mp, bmp = mps.next()
                P.op("pe", [(lambda e, c=c, h=h, mp=mp: e.matmul(mp[:, 0:256], wckv[:, c, h * 128:(h + 1) * 128], memT[:, c, :], start=(c == 0), stop=(c == 7)))
                            for c in range(8)], bmemT + [B_cm], [bmp])
                P.op("act", lambda e, h=h, mp=mp: e.activation(out=kcT[:, h, :], in_=mp[:, 0:256], func=AF.Copy), [bmp], [])
            for mt in range(2):
                mp, bmp = mps.next()
                P.op("pe", [(lambda e, c=c, mt=mt, mp=mp: e.matmul(mp[:], memT[:, c, mt * 128:(mt + 1) * 128], wckv[:, c, 512:1024], start=(c == 0), stop=(c == 7)))
                            for c in range(8)], bmemT + [B_cm], [bmp])
                P.op("act", lambda e, mt=mt, mp=mp: e.activation(out=Vc[:, mt, :], in_=mp[:], func=AF.Copy), [bmp], [])
            P.flush()

        with contextlib.ExitStack() as ph:
            wsv = sb(ph, "wsv", [128, 8, 512], BF16)
            wsu = sb(ph, "wsu", [128, 8, 512], BF16)
            wga = sb(ph, "wga", [128, 8, 1024], BF16)
            wgb = sb(ph, "wgb", [128, 8, 1024], BF16)
            wso = sb(ph, "wso", [128, 4, 1024], BF16)
            wo = sb(ph, "wo", [128, 8, 1024], BF16)
            wcq = sb(ph, "wcq", [128, 8, 512], BF16)
            wco = sb(ph, "wco", [128, 4, 1024], BF16)
            gcrT = sb(ph, "gcrT", [128, 8])
            lng_bc = sb(ph, "lng_bc", [128, 512])
            lnb_bc = sb(ph, "lnb_bc", [128, 512])
            bs_bc = sb(ph, "bs_bc", [128, 4, 128])
            wsraw = sb(ph, "wsraw", [128, 4, 128], BF16)
            WsT = sb(ph, "WsT", [128, 4, 128], BF16)
            B_c = Buf("c2b")
            B_ws = Buf("ws")
            P.dma("pool", lambda e: e.dma_start(out=wsu[:], in_=wv(w_in, 768, 1280)), [], [B_c])
            P.dma("pool", lambda e: e.dma_start(out=wsv[:], in_=wv(w_in, 1280, 1792)), [], [B_c])
            P.dma("pool", lambda e: e.dma_start(out=wga[:], in_=wv(w_in, 1792, 2816)), [], [B_c])
            P.dma("pool", lambda e: e.dma_start(out=wgb[:], in_=wv(w_in, 2816, 3840)), [], [B_c])
            P.dma("pool", lambda e: e.dma_start(out=wso[:], in_=wv(w_sgu_out, 0, 1024)), [], [B_c])
            P.dma("pool", lambda e: e.dma_start(out=wo[:], in_=wv(w_out, 0, 1024)), [], [B_c])
            P.dma("pool", lambda e: e.dma_start(out=wcq[:], in_=wv(w_cq, 0, 512)), [], [B_c])
            P.dma("pool", lambda e: e.dma_start(out=wco[:], in_=wv(w_co, 0, 1024)), [], [B_c])
            P.dma("pool", lambda e: e.dma_start(out=wsraw[:], in_=w_s.rearrange("g p q -> p g q")), [], [B_ws])
            P.dma("sp", lambda e: e.dma_start(out=gcrT[:], in_=gT_view(g_cross), allow_slow_non_contiguous=True), [], [B_c])
            P.dma("sp", lambda e: e.dma_start(out=lng_bc[:], in_=sgu_ln_g.partition_broadcast(128)), [], [B_c])
            P.dma("sp", lambda e: e.dma_start(out=lnb_bc[:], in_=sgu_ln_b.partition_broadcast(128)), [], [B_c])
            P.dma("sp", lambda e: e.dma_start(out=bs_bc[:].rearrange("p g q -> p (g q)"), in_=b_s.rearrange("g q -> (g q)").partition_broadcast(128)), [], [B_c])
            st = norm_state(ph, "p3")
            tpb, btpb = ps(ph, "p3tp", [128, 8, 128], BF16), Buf("tp", True)
            gen = Rot([(ps(ph, "p3g%d" % i, [128, 512]), Buf("g", True)) for i in range(5)])
            scps, bsc = ps(ph, "p3sc", [128, 4, 256]), Buf("sc", True)
            P.op("pe", [(lambda e, g=g: e.transpose(tpb[:, g, :], wsraw[:, g, :], ident[:])) for g in range(4)], [B_ws], [btpb])
            P.op("act", lambda e: e.activation(out=WsT[:], in_=tpb[:, 0:4, :], func=AF.Copy), [btpb], [B_c])

            xg = [(sb(ph, "p3x%d" % i, [128, 1024]), Buf("x")) for i in range(4)]
            hTg, bhT = sb(ph, "p3hT", [128, 8, 512], BF16), [Buf("hT") for _ in range(4)]
            hcT, bhc = hTg, bhT
            gv, bgv = sb(ph, "p3gv", [128, 512]), Buf("gv")
            lst, blst = sb(ph, "p3lst", [128, 16]), [Buf("l%d" % i) for i in range(8)]
            vn, bvn = sb(ph, "p3vn", [128, 512], BF16), Buf("vn")
            vnf, bvnf = sb(ph, "p3vnf", [128, 512]), Buf("vnf")
            vmb, bvmb = sb(ph, "p3vmb", [128, 4, 512]), [Buf("vmb") for _ in range(4)]
            gu, bgu = sb(ph, "p3gu", [128, 512]), Buf("gu")
            sguT, bsg = sb(ph, "p3sguT", [128, 4, 512], BF16), [Buf("sg") for _ in range(4)]
            yatts = Rot([(sb(ph, "p3yatt%d" % i, [128, 512]), Buf("ya")) for i in range(2)])
            sa, bsa = sb(ph, "p3sa", [128, 512]), Buf("sa")
            sbt, bsb = sb(ph, "p3sb", [128, 512]), Buf("sb")
            m1, bm1 = sb(ph, "p3m1", [128, 512]), Buf("m1")
            m2, bm2 = sb(ph, "p3m2", [128, 512]), Buf("m2")
            mrg, bmrg = sb(ph, "p3mrg", [128, 8, 512], BF16), [Buf("mrg") for _ in range(8)]
            qcT, bqc = sb(ph, "p3qcT", [128, 4, 512], BF16), [Buf("qc") for _ in range(4)]
            cst, bcst = sb(ph, "p3cst", [128, 16]), [Buf("c%d" % i) for i in range(4)]
            pc, bpc = sb(ph, "p3pc", [128, 4, 256]), Buf("pc")
            pn, bpn = sb(ph, "p3pn", [128, 4, 256], BF16), Buf("pn")
            pnT, bpnT = sb(ph, "p3pnT", [128, 8, 128], BF16), Buf("pnT")
            ocT, boc = sb(ph, "p3ocT", [128, 4, 128], BF16), Buf("oc")

            for gi in range(8):
                for j in range(4):
                    tile = gi * 4 + j
                    xt, bx = xg[j]
                    P.dma("sp", lambda e, xt=xt, tile=tile: e.dma_start(out=xt[:], in_=xown[tile * 128:(tile + 1) * 128, :]), [], [bx])
                    norm_T(st, xt[:], bx, gmixT, hTg[:, :, j * 128:(j + 1) * 128], bhT[j], tpb, btpb)
                    g0, bg0 = gen.next()
                    P.op("pe", [(lambda e, c=c, j=j, g0=g0: e.matmul(g0[:], hTg[:, c, j * 128:(j + 1) * 128], wsv[:, c, :], start=(c == 0), stop=(c == 7)))
                                for c in range(8)], [bhT[j], B_c], [bg0])
                    P.op("act", lambda e, g0=g0: e.activation(out=gv[:], in_=g0[:], func=AF.Gelu_apprx_tanh), [bg0], [bgv])
                    P.op("dve", lambda e: e.tensor_reduce(out=lst[:, 0:1], in_=gv[:], axis=AX.X, op=ALU.add), [bgv], [blst[0]])
                    P.op("act", lambda e: e.activation(out=vnf[:], in_=gv[:], func=AF.Square, accum_out=lst[:, 1:2]), [bgv], [bvnf, blst[1]])
                    P.op("dve", lambda e: e.tensor_scalar(out=lst[:, 2:3], in0=lst[:, 0:1], scalar1=1.0 / 512, scalar2=None, op0=ALU.mult), [blst[0]], [blst[2]])
                    P.op("dve", lambda e: e.tensor_tensor(out=lst[:, 3:4], in0=lst[:, 2:3], in1=lst[:, 2:3], op=ALU.mult), [blst[2]], [blst[3]])
                    P.op("dve", lambda e: e.scalar_tensor_tensor(out=lst[:, 4:5], in0=lst[:, 1:2], scalar=1.0 / 512, in1=lst[:, 3:4], op0=ALU.mult, op1=ALU.subtract),
                         [blst[1], blst[3]], [blst[4]])
                    P.op("dve", lambda e: e.tensor_scalar(out=lst[:, 5:6], in0=lst[:, 4:5], scalar1=EPS, scalar2=None, op0=ALU.add), [blst[4]], [blst[5]])
                    P.op("act", lambda e: e.activation(out=lst[:, 6:7], in_=lst[:, 5:6], func=AF.Ln), [blst[5]], [blst[6]])
                    P.op("act", lambda e: e.activation(out=lst[:, 7:8], in_=lst[:, 6:7], func=AF.Exp, scale=-0.5), [blst[6]], [blst[7]])
                    P.op("dve", lambda e: e.tensor_scalar(out=vnf[:], in0=gv[:], scalar1=lst[:, 2:3], scalar2=lst[:, 7:8], op0=ALU.subtract, op1=ALU.mult),
                         [bgv, blst[2], blst[7]], [bvnf])
                    P.op("dve", lambda e: e.tensor_tensor(out=vnf[:], in0=vnf[:], in1=lng_bc[:], op=ALU.mult), [bvnf, B_c], [bvnf])
                    P.op("dve", lambda e: e.tensor_tensor(out=vn[:], in0=vnf[:], in1=lnb_bc[:], op=ALU.add), [bvnf, B_c], [bvn])
                    g1, bg1 = gen.next()
                    P.op("pe", [(lambda e, g=g, g1=g1: e.matmul(g1[:, g * 128:(g + 1) * 128], vn[:, g * 128:(g + 1) * 128], WsT[:, g, :], start=True, stop=True))
                                for g in range(4)], [bvn, B_c], [bg1])
                    P.op("dve", lambda e, g1=g1, j=j: e.tensor_tensor(out=vmb[:, :, j * 128:(j + 1) * 128], in0=g1[:].rearrange("p (g q) -> p g q", g=4),
                                                                     in1=bs_bc[:], op=ALU.add), [bg1, B_c], [bvmb[j]])
                for g in range(4):
                    g0, bg0 = gen.next()
                    P.op("pe", [(lambda e, c=c, g=g, g0=g0: e.matmul(g0[:], wsu[:, c, g * 128:(g + 1) * 128], hTg[:, c, :], start=(c == 0), stop=(c == 7)))
                                for c in range(8)], bhT + [B_c], [bg0])
                    P.op("act", lambda e, g0=g0: e.activation(out=gu[:], in_=g0[:], func=AF.Gelu_apprx_tanh), [bg0], [bgu])
                    P.op("dve", lambda e, g=g: e.tensor_tensor(out=sguT[:, g, :], in0=gu[:], in1=vmb[:, g, :], op=ALU.mult), [bgu] + bvmb, [bsg[g]])
                for dc in range(8):
                    ga_ps, bga = gen.next()
                    P.op("pe", [(lambda e, c=c, dc=dc, ga_ps=ga_ps: e.matmul(ga_ps[:], wga[:, c, dc * 128:(dc + 1) * 128], hTg[:, c, :], start=(c == 0), stop=(c == 7)))
                                for c in range(8)], bhT + [B_c], [bga])
                    P.op("act", lambda e, ga_ps=ga_ps: e.activation(out=sa[:], in_=ga_ps[:], func=AF.Sigmoid), [bga], [bsa])
                    gb_ps, bgb = gen.next()
                    P.op("pe", [(lambda e, c=c, dc=dc, gb_ps=gb_ps: e.matmul(gb_ps[:], wgb[:, c, dc * 128:(dc + 1) * 128], hTg[:, c, :], start=(c == 0), stop=(c == 7)))
                                for c in range(8)], bhT + [B_c], [bgb])
                    P.op("act", lambda e, gb_ps=gb_ps: e.activation(out=sbt[:], in_=gb_ps[:], func=AF.Sigmoid), [bgb], [bsb])
                    ys_ps, bysp = gen.next()
                    P.op("pe", [(lambda e, g=g, dc=dc, ys_ps=ys_ps: e.matmul(ys_ps[:], wso[:, g, dc * 128:(dc + 1) * 128], sguT[:, g, :], start=(g == 0), stop=(g == 3)))
                                for g in range(4)], bsg + [B_c], [bysp])
                    yatt, bya = yatts.next()
                    P.dma("sp", lambda e, gi=gi, dc=dc, yatt=yatt: e.dma_start(out=yatt[:], in_=yatt_d[gi][:, dc, :]), [], [bya])
                    P.op("dve", lambda e, yatt=yatt: e.tensor_tensor(out=m1[:], in0=sa[:], in1=yatt[:], op=ALU.mult), [bsa, bya], [bm1])
                    P.op("dve", lambda e, ys_ps=ys_ps: e.tensor_tensor(out=m2[:], in0=sbt[:], in1=ys_ps[:], op=ALU.mult), [bsb, bysp], [bm2])
                    P.op("dve", lambda e, dc=dc: e.tensor_tensor(out=mrg[:, dc, :], in0=m1[:], in1=m2[:], op=ALU.add), [bm1, bm2], [bmrg[dc]])
                for j in range(4):
                    xt, bx = xg[j]
                    for nb in range(2):
                        o_ps, bo = gen.next()
                        P.op("pe", [(lambda e, dc=dc, j=j, nb=nb, o_ps=o_ps: e.matmul(o_ps[:], mrg[:, dc, j * 128:(j + 1) * 128], wo[:, dc, nb * 512:(nb + 1) * 512],
                                                                                     start=(dc == 0), stop=(dc == 7))) for dc in range(8)], bmrg + [B_c], [bo])
                        P.op("dve", lambda e, xt=xt, nb=nb, o_ps=o_ps: e.tensor_tensor(out=xt[:, nb * 512:(nb + 1) * 512], in0=xt[:, nb * 512:(nb + 1) * 512], in1=o_ps[:], op=ALU.add),
                             [bo, bx], [bx])
                for j in range(4):
                    xt, bx = xg[j]
                    norm_T(st, xt[:], bx, gcrT, hcT[:, :, j * 128:(j + 1) * 128], bhc[j], tpb, btpb)
                for h in range(4):
                    q_ps, bq = gen.next()
                    P.op("pe", [(lambda e, c=c, h=h, q_ps=q_ps: e.matmul(q_ps[:], wcq[:, c, h * 128:(h + 1) * 128], hcT[:, c, :], start=(c == 0), stop=(c == 7)))
                                for c in range(8)], bhc + [B_c], [bq])
                    P.op("act", lambda e, h=h, q_ps=q_ps: e.activation(out=qcT[:, h, :], in_=q_ps[:], func=AF.Copy, scale=float(128 ** -0.5)), [bq], [bqc[h]])
                for j in range(4):
                    tile = gi * 4 + j
                    xt, bx = xg[j]
                    P.op("pe", [(lambda e, h=h, j=j: e.matmul(scps[:, h, :], qcT[:, h, j * 128:(j + 1) * 128], kcT[:, h, :], start=True, stop=True)) for h in range(4)],
                         bqc, [bsc])
                    P.op("dve", lambda e: e.tensor_reduce(out=cst[:, 0:4], in_=scps[:], axis=AX.X, op=ALU.max), [bsc], [bcst[0]])
                    P.op("dve", lambda e: e.tensor_scalar(out=cst[:, 4:8], in0=cst[:, 0:4], scalar1=-1.0, scalar2=None, op0=ALU.mult), [bcst[0]], [bcst[1]])
                    P.op("act", [(lambda e, h=h: e.activation(out=pc[:, h, :], in_=scps[:, h, :], func=AF.Exp, bias=cst[:, 4 + h:5 + h], accum_out=cst[:, 8 + h:9 + h]))
                                 for h in range(4)], [bsc, bcst[1]], [bpc, bcst[2]])
                    P.op("dve", lambda e: e.reciprocal(out=cst[:, 12:16], in_=cst[:, 8:12]), [bcst[2]], [bcst[3]])
                    P.op("dve", lambda e: e.tensor_tensor(out=pn[:], in0=pc[:], in1=cst[:, 12:16].unsqueeze(2).broadcast_to([128, 4, 256]), op=ALU.mult),
                         [bpc, bcst[3]], [bpn])
                    P.op("pe", [(lambda e, k=k: e.transpose(tpb[:, k, :], pn[:, k // 2, (k % 2) * 128:(k % 2 + 1) * 128], ident[:])) for k in range(8)], [bpn], [btpb])
                    P.op("act", lambda e: e.activation(out=pnT[:], in_=tpb[:], func=AF.Copy), [btpb], [bpnT])
                    oc_ps, bocp = gen.next()
                    fl = []
                    for h in range(4):
                        for mt in range(2):
                            fl.append(lambda e, h=h, mt=mt, oc_ps=oc_ps: e.matmul(oc_ps[:, h * 128:(h + 1) * 128], Vc[:, mt, h * 128:(h + 1) * 128], pnT[:, h * 2 + mt, :],
                                                                                  start=(mt == 0), stop=(mt == 1)))
                    P.op("pe", fl, [bpnT], [bocp])
                    P.op("act", lambda e, oc_ps=oc_ps: e.activation(out=ocT[:], in_=oc_ps[:].rearrange("p (h t) -> p h t", h=4), func=AF.Copy), [bocp], [boc])
                    for nb in range(2):
                        y_ps, byp = gen.next()
                        P.op("pe", [(lambda e, h=h, nb=nb, y_ps=y_ps: e.matmul(y_ps[:], ocT[:, h, :], wco[:, h, nb * 512:(nb + 1) * 512], start=(h == 0), stop=(h == 3)))
                                    for h in range(4)], [boc, B_c], [byp])
                        P.op("dve", lambda e, xt=xt, nb=nb, y_ps=y_ps: e.tensor_tensor(out=xt[:, nb * 512:(nb + 1) * 512], in0=xt[:, nb * 512:(nb + 1) * 512], in1=y_ps[:], op=ALU.add),
                             [byp, bx], [bx])
                    P.dma("sp", lambda e, xt=xt, tile=tile: e.dma_start(out=x2_d[tile * 128:(tile + 1) * 128, :], in_=xt[:]), [bx], [])
            P.flush()

        if dbg == 3:
            dx = nc.dram_tensor("dbg_x2", [OWN, D], F32, kind="ExternalOutput").ap()
            P.dma("sp", lambda e: e.dma_start(out=dx[:, :], in_=x2_d[:, :]), [], [])
            P.flush()
            return nc

        with contextlib.ExitStack() as ph:
            gmoT = sb(ph, "gmoT", [128, 8])
            wr = sb(ph, "wr", [128, 8, 20], BF16)
            br_bc = sb(ph, "br_bc", [128, 20])
            gfin_bc = sb(ph, "gfin_bc", [128, 1024])
            B_c = Buf("c3")
            P.dma("sp", lambda e: e.dma_start(out=gmoT[:], in_=gT_view(g_moe), allow_slow_non_contiguous=True), [], [B_c])
            P.dma("pool", lambda e: e.dma_start(out=wr[:, :, 0:4], in_=wv(w_rg, 0, 4)), [], [B_c])
            P.dma("pool", lambda e: e.dma_start(out=wr[:, :, 4:20], in_=wv(w_re, 0, 16)), [], [B_c])
            P.dma("sp", lambda e: e.dma_start(out=br_bc[:, 0:4], in_=b_rg.partition_broadcast(128)), [], [B_c])
            P.dma("sp", lambda e: e.dma_start(out=br_bc[:, 4:20], in_=b_re.partition_broadcast(128)), [], [B_c])
            P.dma("sp", lambda e: e.dma_start(out=gfin_bc[:], in_=g_final.partition_broadcast(128)), [], [B_c])
            P.barrier()
            NP3 = 6
            st = norm_state(ph, "p4", nsets=NP3, nxb=NP3)
            xres, bxr = sb(ph, "xres", [128, 16, 1024]), [Buf("xr%d" % i) for i in range(16)]
            hmT, bhm = sb(ph, "hmT", [128, 8, 2048], BF16), [Buf("hm%d" % i) for i in range(16)]
            comb, bcomb = sb(ph, "comb", [128, 16, 16]), [Buf("cb%d" % i) for i in range(16)]
            Wg = [(sb(ph, "Wg%d" % i, [128, 8, 512], BF16), Buf("Wg")) for i in range(2)]
            Wu = [(sb(ph, "Wu%d" % i, [128, 8, 512], BF16), Buf("Wu")) for i in range(2)]
            Wd = [(sb(ph, "Wd%d" % i, [128, 4, 1024], BF16), Buf("Wd")) for i in range(2)]
            sgs = Rot([(sb(ph, "sg%d" % i, [128, 512]), Buf("sg")) for i in range(2)])
            heTs = Rot([(sb(ph, "heT%d" % i, [128, 4, 512], BF16), [Buf("he") for _ in range(4)]) for i in range(2)])
            ots = Rot([(sb(ph, "ot%d" % i, [128, 1024]), Buf("ot")) for i in range(2)])
            rts = Rot([(sb(ph, "rt%d" % i, [128, 160]), [Buf("rt") for _ in range(24)]) for i in range(NP3)])
            allbanks = [(ps(ph, "p4g%d" % i, [128, 512]), Buf("g", True)) for i in range(8)]
            bpool = Pool(allbanks)
            gen = Rot(allbanks)

            def moe_tile(sgi, tl):
                tile = sgi * 16 + tl
                P.dma("sp", lambda e: e.dma_start(out=xres[:, tl, :], in_=x2_d[tile * 128:(tile + 1) * 128, :]), [], [bxr[tl]])
                yield
                yield from norm_T_gen(st, xres[:, tl, :], bxr[tl], gmoT, hmT[:, :, tl * 128:(tl + 1) * 128], bhm[tl], None, None, pool=bpool)
                hb = yield from acq(bpool)
                r_ps, brp = hb
                P.op("pe", [(lambda e, c=c: e.matmul(r_ps[:, 0:20], hmT[:, c, tl * 128:(tl + 1) * 128], wr[:, c, :], start=(c == 0), stop=(c == 7)))
                            for c in range(8)], [bhm[tl], B_c], [brp])
                yield
                R, brt = rts.next()
                P.op("dve", lambda e: e.tensor_tensor(out=R[:, 0:20], in0=r_ps[:, 0:20], in1=br_bc[:], op=ALU.add), [brp, B_c], [brt[0]])
                bpool.release(hb)
                yield
                P.op("dve", lambda e: e.tensor_reduce(out=R[:, 20:21], in_=R[:, 0:4], axis=AX.X, op=ALU.max), [brt[0]], [brt[1]])
                yield
                P.op("dve", lambda e: e.tensor_scalar(out=R[:, 21:22], in0=R[:, 20:21], scalar1=-1.0, scalar2=None, op0=ALU.mult), [brt[1]], [brt[2]])
                yield
                P.op("act", lambda e: e.activation(out=R[:, 22:26], in_=R[:, 0:4], func=AF.Exp, bias=R[:, 21:22], accum_out=R[:, 26:27]), [brt[0], brt[2]], [brt[3]])
                yield
                P.op("dve", lambda e: e.reciprocal(out=R[:, 27:28], in_=R[:, 26:27]), [brt[3]], [brt[4]])
                yield
                P.op("dve", lambda e: e.tensor_scalar(out=R[:, 28:32], in0=R[:, 0:4], scalar1=R[:, 20:21], scalar2=None, op0=ALU.is_ge), [brt[0], brt[1]], [brt[5]])
                yield
                P.op("dve", lambda e: e.tensor_scalar(out=R[:, 32:36], in0=R[:, 28:32], scalar1=BIG, scalar2=-BIG, op0=ALU.mult, op1=ALU.add), [brt[5]], [brt[6]])
                yield
                P.op("dve", lambda e: e.tensor_tensor(out=R[:, 40:56].rearrange("p (g k) -> p g k", g=4), in0=R[:, 4:20].rearrange("p (g k) -> p g k", g=4),
                                                      in1=R[:, 32:36].unsqueeze(2).broadcast_to([128, 4, 4]), op=ALU.add), [brt[0], brt[6]], [brt[7]])
                yield
                P.op("dve", lambda e: e.tensor_reduce(out=R[:, 56:57], in_=R[:, 40:56], axis=AX.X, op=ALU.max), [brt[7]], [brt[8]])
                yield
                P.op("dve", lambda e: e.tensor_scalar(out=R[:, 60:76], in0=R[:, 40:56], scalar1=R[:, 56:57], scalar2=None, op0=ALU.is_ge), [brt[7], brt[8]], [brt[9]])
                yield
                P.op("dve", lambda e: e.scalar_tensor_tensor(out=R[:, 80:96], in0=R[:, 60:76], scalar=-BIG, in1=R[:, 40:56], op0=ALU.mult, op1=ALU.add),
                     [brt[9], brt[7]], [brt[10]])
                yield
                P.op("dve", lambda e: e.tensor_reduce(out=R[:, 96:97], in_=R[:, 80:96], axis=AX.X, op=ALU.max), [brt[10]], [brt[11]])
                yield
                P.op("dve", lambda e: e.tensor_scalar(out=R[:, 100:116], in0=R[:, 80:96], scalar1=R[:, 96:97], scalar2=None, op0=ALU.is_ge), [brt[10], brt[11]], [brt[12]])
                yield
                P.op("dve", lambda e: e.tensor_tensor(out=R[:, 116:117], in0=R[:, 96:97], in1=R[:, 56:57], op=ALU.subtract), [brt[11], brt[8]], [brt[13]])
                yield
                P.op("act", lambda e: e.activation(out=R[:, 117:118], in_=R[:, 116:117], func=AF.Exp), [brt[13]], [brt[14]])
                yield
                P.op("dve", lambda e: e.tensor_scalar(out=R[:, 118:119], in0=R[:, 117:118], scalar1=1.0, scalar2=None, op0=ALU.add), [brt[14]], [brt[15]])
                yield
                P.op("dve", lambda e: e.reciprocal(out=R[:, 119:120], in_=R[:, 118:119]), [brt[15]], [brt[16]])
                yield
                P.op("dve", lambda e: e.tensor_tensor(out=R[:, 120:121], in0=R[:, 119:120], in1=R[:, 27:28], op=ALU.mult), [brt[16], brt[4]], [brt[17]])
                yield
                P.op("dve", lambda e: e.tensor_tensor(out=R[:, 121:122], in0=R[:, 120:121], in1=R[:, 117:118], op=ALU.mult), [brt[17], brt[14]], [brt[18]])
                yield
                P.op("dve", lambda e: e.tensor_scalar(out=comb[:, tl, :], in0=R[:, 60:76], scalar1=R[:, 120:121], scalar2=None, op0=ALU.mult),
                     [brt[9], brt[17]], [bcomb[tl]])
                yield
                P.op("dve", lambda e: e.scalar_tensor_tensor(out=comb[:, tl, :], in0=R[:, 100:116], scalar=R[:, 121:122], in1=comb[:, tl, :], op0=ALU.mult, op1=ALU.add),
                     [brt[12], brt[18], bcomb[tl]], [bcomb[tl]])
                yield

            def fin_tile(sgi, tl):
                tile = sgi * 16 + tl
                fss, fb, fjunk = st["sets"].next()
                P.op("act", lambda e: e.activation(out=fjunk[:], in_=xres[:, tl, :], func=AF.Square, scale=1.0 / 32, accum_out=fss[:, 0:1]), [bxr[tl]], [fb[0]])
                yield
                P.op("dve", lambda e: e.tensor_scalar(out=fss[:, 1:2], in0=fss[:, 0:1], scalar1=EPS, scalar2=None, op0=ALU.add), [fb[0]], [fb[1]])
                yield
                P.op("act", lambda e: e.activation(out=fss[:, 2:3], in_=fss[:, 1:2], func=AF.Ln), [fb[1]], [fb[2]])
                yield
                P.op("act", lambda e: e.activation(out=fss[:, 3:4], in_=fss[:, 2:3], func=AF.Exp, scale=-0.5), [fb[2]], [fb[3]])
                yield
                ot, bot = ots.next()
                P.op("dve", lambda e: e.scalar_tensor_tensor(out=ot[:], in0=xres[:, tl, :], scalar=fss[:, 3:4], in1=gfin_bc[:], op0=ALU.mult, op1=ALU.mult),
                     [bxr[tl], fb[3], B_c], [bot])
                yield
                P.dma("sp", lambda e: e.dma_start(out=out[tile * 128:(tile + 1) * 128, :], in_=ot[:]), [bot], [])
                yield

            def load_w(e_):
                (wg, bwg), (wu, bwu), (wd, bwd) = Wg[e_ % 2], Wu[e_ % 2], Wd[e_ % 2]
                P.dma("pool", lambda e: e.dma_start(out=wg[:], in_=wv(w_gate[e_], 0, 512)), [], [bwg])
                P.dma("pool", lambda e: e.dma_start(out=wu[:], in_=wv(w_up[e_], 0, 512)), [], [bwu])
                P.dma("pool", lambda e: e.dma_start(out=wd[:], in_=wv(w_down[e_], 0, 1024)), [], [bwd])

            for sgi in range(2):
                load_w(0)
                run_pipeline((moe_tile(sgi, tl) for tl in range(16)), NP3)
                for ex in range(NE):
                    if ex + 1 < NE:
                        load_w(ex + 1)
                    (wg, bwg), (wu, bwu), (wd, bwd) = Wg[ex % 2], Wu[ex % 2], Wd[ex % 2]
                    for tg in range(4):
                        heT, bhe = heTs.next()
                        for fc in range(4):
                            g_ps, bgp = gen.next()
                            u_ps, bup = gen.next()
                            P.op("pe", [(lambda e, c=c, fc=fc, tg=tg, g_ps=g_ps, wg=wg: e.matmul(g_ps[:], wg[:, c, fc * 128:(fc + 1) * 128], hmT[:, c, tg * 512:(tg + 1) * 512],
                                                                                                 start=(c == 0), stop=(c == 7))) for c in range(8)],
                                 bhm[tg * 4:tg * 4 + 4] + [bwg], [bgp])
                            P.op("pe", [(lambda e, c=c, fc=fc, tg=tg, u_ps=u_ps, wu=wu: e.matmul(u_ps[:], wu[:, c, fc * 128:(fc + 1) * 128], hmT[:, c, tg * 512:(tg + 1) * 512],
                                                                                                 start=(c == 0), stop=(c == 7))) for c in range(8)],
                                 bhm[tg * 4:tg * 4 + 4] + [bwu], [bup])
                            sg, bsg_ = sgs.next()
                            P.op("act", lambda e, sg=sg, g_ps=g_ps: e.activation(out=sg[:], in_=g_ps[:], func=AF.Silu), [bgp], [bsg_])
                            P.op("dve", lambda e, sg=sg, u_ps=u_ps, heT=heT, fc=fc: e.tensor_tensor(out=heT[:, fc, :], in0=sg[:], in1=u_ps[:], op=ALU.mult),
                                 [bsg_, bup], [bhe[fc]])
                        for j in range(4):
                            tl = tg * 4 + j
                            for nb in range(2):
                                y_ps, byp = gen.next()
                                P.op("pe", [(lambda e, fc=fc, j=j, nb=nb, y_ps=y_ps, heT=heT, wd=wd: e.matmul(y_ps[:], heT[:, fc, j * 128:(j + 1) * 128], wd[:, fc, nb * 512:(nb + 1) * 512],
                                                                                                             start=(fc == 0), stop=(fc == 3))) for fc in range(4)],
                                     bhe + [bwd], [byp])
                                P.op("dve", lambda e, tl=tl, nb=nb, y_ps=y_ps, ex=ex: e.scalar_tensor_tensor(
                                    out=xres[:, tl, nb * 512:(nb + 1) * 512], in0=y_ps[:], scalar=comb[:, tl, ex:ex + 1], in1=xres[:, tl, nb * 512:(nb + 1) * 512],
                                    op0=ALU.mult, op1=ALU.add), [byp, bcomb[tl], bxr[tl]], [bxr[tl]])
                run_pipeline((fin_tile(sgi, tl) for tl in range(16)), 2)
            P.flush()
    return nc


def _rope_tables():
    rows = SEQ // 64
    row_ids = np.repeat(np.arange(rows), 64).astype(np.float32)
    col_ids = np.tile(np.arange(64), rows).astype(np.float32)
    inv = (10000.0 ** (-np.arange(16, dtype=np.float32) / 16)).astype(np.float32)
    ang = np.concatenate([row_ids[:, None] * inv[None, :], col_ids[:, None] * inv[None, :]], axis=-1).astype(np.float32)
    return np.cos(ang).astype(np.float32), np.sin(ang).astype(np.float32)


def make_in_maps(inputs):
    f = lambda a: np.ascontiguousarray(np.asarray(a, dtype=np.float32))
    x = f(inputs["x"])
    mem = f(inputs["mem"])
    cos, sin = _rope_tables()

    def lay(t, n):
        return np.ascontiguousarray(t.reshape(n, 128, 32).transpose(1, 0, 2))

    shared = {
        "ident": np.eye(128, dtype=np.float32),
        "g_mix": f(inputs["g_mix"][0]), "w_in": f(inputs["w_in"][0]), "g_q": f(inputs["g_q"][0]), "g_k": f(inputs["g_k"][0]),
        "w_att_out": f(inputs["w_att_out"][0]), "sgu_ln_g": f(inputs["sgu_ln_g"][0]), "sgu_ln_b": f(inputs["sgu_ln_b"][0]),
        "w_s": f(inputs["w_s"][0]), "b_s": f(inputs["b_s"][0]), "w_sgu_out": f(inputs["w_sgu_out"][0]), "w_out": f(inputs["w_out"][0]),
        "g_cross": f(inputs["g_cross"][0]), "g_mem": f(inputs["g_mem"][0]), "w_cq": f(inputs["w_cq"][0]), "w_ckv": f(inputs["w_ckv"][0]),
        "w_co": f(inputs["w_co"][0]), "g_moe": f(inputs["g_moe"][0]), "w_rg": f(inputs["w_rg"][0]), "b_rg": f(inputs["b_rg"][0]),
        "w_re": f(inputs["w_re"][0]), "b_re": f(inputs["b_re"][0]), "w_gate": f(inputs["w_gate"][0]), "w_up": f(inputs["w_up"][0]),
        "w_down": f(inputs["w_down"][0]), "g_final": f(inputs["g_final"]),
        "cos_seq": lay(cos, 64), "sin_seq": lay(sin, 64),
    }
    maps = []
    for c in range(8):
        b, hf = c // 2, c % 2
        m = dict(shared)
        m["xseq"] = x[b]
        m["xown"] = np.ascontiguousarray(x[b, hf * OWN:(hf + 1) * OWN])
        m["mem"] = mem[b]
        m["cos_own"] = lay(cos[hf * OWN:(hf + 1) * OWN], 32)
        m["sin_own"] = lay(sin[hf * OWN:(hf + 1) * OWN], 32)
        maps.append(m)
    return maps


def kernel(**inputs):
    nc = build()
    maps = make_in_maps(inputs)
    res = run_bass_kernel_spmd(nc, maps, core_ids=list(range(8)))
    outp = np.empty((4, SEQ, D), np.float32)
    for c in range(8):
        b, hf = c // 2, c % 2
        outp[b, hf * OWN:(hf + 1) * OWN] = np.asarray(res.results[c]["out"], dtype=np.float32)
    return outp
```

```python
import contextlib
import os as _os0
import numpy as np
import concourse.bass as bass
import concourse.mybir as mybir
from concourse.bass_utils import run_bass_kernel_spmd

F32 = mybir.dt.float32
BF16 = mybir.dt.bfloat16
AF = mybir.ActivationFunctionType
ALU = mybir.AluOpType
AX = mybir.AxisListType

D = 1024
SEQ = 8192
OWN = 4096
NMEM = 256
EPS = 1e-6
NE = 16
BIG = 1.0e30


class Buf:
    __slots__ = ("name", "w", "r", "psum")

    def __init__(self, name="", psum=False):
        self.name = name
        self.psum = psum
        self.w = None
        self.r = {}


class Eng:
    def __init__(self, name):
        self.name = name
        self.sem = None
        self.count = 0
        self.seen = {}
        self.prog = []
        self.chsems = []
        self.dma_i = 0


class Prog:
    NCH = int(_os0.environ.get('NCH', 8))

    def __init__(self, nc, stack):
        self.nc = nc
        self.E = {}
        for n in ("pe", "act", "dve", "pool", "sp"):
            e = Eng(n)
            e.sem = stack.enter_context(nc.semaphore("sem_" + n))
            self.E[n] = e
        for n in ("sp", "pool", "act"):
            e = self.E[n]
            for i in range(self.NCH):
                e.chsems.append(stack.enter_context(nc.semaphore("ch_%s_%d" % (n, i))))
        self.chan_issued = {}
        import os
        self.limit = int(os.environ.get("OPLIMIT", 10 ** 9))
        self.nops = 0

    def _need(self, need, ev):
        sem, val, src = ev
        k = id(sem)
        if k not in need or need[k][1] < val:
            need[k] = (sem, val, src)

    def _deps(self, eng, reads, writes, extra=()):
        E = self.E[eng]
        need = {}
        for b in reads:
            if b.w is not None:
                self._need(need, b.w)
        for b in writes:
            if b.w is not None:
                self._need(need, b.w)
            for k, (sem, val, src) in b.r.items():
                self._need(need, (sem, val, src))
        for ev in extra:
            self._need(need, ev)
        for k, (sem, val, src) in need.items():
            if src == "pe" and eng == "pe":
                continue
            if E.seen.get(k, 0) >= val:
                continue
            E.seen[k] = val
            E.prog.append(("wait", sem, val))

    def _commit(self, ev, reads, writes):
        sem, val, src = ev
        for b in writes:
            b.w = ev
            b.r = {}
        for b in reads:
            if b not in writes:
                b.r[id(sem)] = (sem, val, src)

    def op(self, eng, fns, reads=(), writes=()):
        if not isinstance(fns, (list, tuple)):
            fns = [fns]
        self.nops += 1
        if self.nops > self.limit:
            return
        pr = [b for b in reads if b.psum]
        if pr:
            reads = [b for b in reads if not b.psum]
            writes = list(writes) + [b for b in pr if b not in writes]
        E = self.E[eng]
        self._deps(eng, reads, writes)
        E.count += 1
        for f in fns[:-1]:
            E.prog.append(("raw", f))
        E.prog.append(("op", fns[-1], E.sem))
        self._commit((E.sem, E.count, eng), reads, writes)

    def dma(self, q, fn, reads=(), writes=()):
        self.nops += 1
        if self.nops > self.limit:
            return
        E = self.E[q]
        ch = E.dma_i % self.NCH
        rnd = E.dma_i // self.NCH
        E.dma_i += 1
        chsem = E.chsems[ch]
        extra = []
        if rnd > 0:
            extra.append((chsem, 16 * rnd, "dma"))
        self._deps(q, reads, writes, extra)
        E.prog.append(("dma", fn, chsem))
        self.chan_issued[id(chsem)] = (chsem, 16 * (rnd + 1))
        self._commit((chsem, 16 * (rnd + 1), "dma"), reads, writes)

    def barrier(self):
        for n, E in self.E.items():
            for m, F in self.E.items():
                if m == n or F.count == 0:
                    continue
                k = id(F.sem)
                if E.seen.get(k, 0) < F.count:
                    E.seen[k] = F.count
                    E.prog.append(("wait", F.sem, F.count))
            for k, (sem, val) in self.chan_issued.items():
                if E.seen.get(k, 0) < val:
                    E.seen[k] = val
                    E.prog.append(("wait", sem, val))

    def flush(self):
        self.barrier()
        nc = self.nc
        progs = {n: E.prog for n, E in self.E.items()}
        for E in self.E.values():
            E.prog = []

        def replay(eng, prog):
            for it in prog:
                if it[0] == "wait":
                    eng.wait_ge(it[1], it[2])
                elif it[0] == "raw":
                    it[1](eng)
                elif it[0] == "op":
                    it[1](eng).then_inc(it[2], 1)
                else:
                    it[1](eng).then_inc(it[2], 16)

        with nc.Block() as block:
            @block.tensor
            def _(e):
                replay(e, progs["pe"])

            @block.scalar
            def _(e):
                replay(e, progs["act"])

            @block.vector
            def _(e):
                replay(e, progs["dve"])

            @block.gpsimd
            def _(e):
                replay(e, progs["pool"])

            @block.sync
            def _(e):
                replay(e, progs["sp"])


class Rot:
    def __init__(self, items):
        self.items = items
        self.i = 0

    def next(self):
        it = self.items[self.i % len(self.items)]
        self.i += 1
        return it


class Pool:
    def __init__(self, items):
        self.free = list(items)

    def acquire(self):
        return self.free.pop(0) if self.free else None

    def release(self, it):
        self.free.append(it)


def build(dbg=False):
    nc = bass.Bass("TRN2", target_bir_lowering=False)

    def din(name, shape):
        return nc.dram_tensor(name, list(shape), F32, kind="ExternalInput").ap()

    xseq = din("xseq", [SEQ, D])
    xown = din("xown", [OWN, D])
    memd = din("mem", [NMEM, D])
    cos_seq = din("cos_seq", [128, 64, 32])
    sin_seq = din("sin_seq", [128, 64, 32])
    cos_own = din("cos_own", [128, 32, 32])
    sin_own = din("sin_own", [128, 32, 32])
    identd = din("ident", [128, 128])
    g_mix = din("g_mix", [D])
    w_in = din("w_in", [D, 3840])
    g_q = din("g_q", [64])
    g_k = din("g_k", [64])
    w_att_out = din("w_att_out", [512, D])
    sgu_ln_g = din("sgu_ln_g", [512])
    sgu_ln_b = din("sgu_ln_b", [512])
    w_s = din("w_s", [4, 128, 128])
    b_s = din("b_s", [4, 128])
    w_sgu_out = din("w_sgu_out", [512, D])
    w_out = din("w_out", [D, D])
    g_cross = din("g_cross", [D])
    g_mem = din("g_mem", [D])
    w_cq = din("w_cq", [D, 512])
    w_ckv = din("w_ckv", [D, D])
    w_co = din("w_co", [512, D])
    g_moe = din("g_moe", [D])
    w_rg = din("w_rg", [D, 4])
    b_rg = din("b_rg", [4])
    w_re = din("w_re", [D, 16])
    b_re = din("b_re", [16])
    w_gate = din("w_gate", [NE, D, 512])
    w_up = din("w_up", [NE, D, 512])
    w_down = din("w_down", [NE, 512, D])
    g_final = din("g_final", [D])
    out = nc.dram_tensor("out", [OWN, D], F32, kind="ExternalOutput").ap()
    yatt_d = nc.dram_tensor("yatt_scr", [8, 128, 8, 512], F32, kind="Internal").ap()
    x2_d = nc.dram_tensor("x2_scr", [OWN, D], F32, kind="Internal").ap()

    def wv(w, c0, c1):
        return w.rearrange("(c p) n -> p c n", p=128)[:, :, c0:c1]

    def gT_view(g):
        return g.rearrange("(c p) -> p c", p=128)

    with contextlib.ExitStack() as top:
        P = Prog(nc, top)
        print("sbuf bytes remaining at start:", nc.sbuf_bytes_remaining)

        def sb(stack, name, shape, dt=F32):
            return stack.enter_context(nc.sbuf_tensor("s_" + name, list(shape), dt))

        def ps(stack, name, shape, dt=F32):
            return stack.enter_context(nc.psum_tensor("p_" + name, list(shape), dt))

        ident = sb(top, "ident", [128, 128], BF16)
        kcT = sb(top, "kcT", [128, 4, NMEM], BF16)
        Vc = sb(top, "Vc", [128, 2, 512], BF16)
        gmixT = sb(top, "gmixT", [128, 8])
        attn_stack = contextlib.ExitStack()
        kT_all = sb(attn_stack, "kT_all", [128, SEQ], BF16)
        V_all = sb(attn_stack, "V_all", [128, 64, 2, 65], BF16)
        B_const = Buf("const")

        P.dma("pool", lambda e: e.dma_start(out=ident[:], in_=identd[:, :]), [], [B_const])
        P.dma("sp", lambda e: e.dma_start(out=gmixT[:], in_=gT_view(g_mix), allow_slow_non_contiguous=True), [], [B_const])
        P.op("pool", lambda e: e.memset(V_all[:], 1.0), [], [B_const])

        def run_pipeline(gens, depth):
            it = iter(gens)
            active = []
            more = True
            while True:
                if more and len(active) < depth:
                    try:
                        active.append(next(it))
                    except StopIteration:
                        more = False
                if not active:
                    break
                for g in list(active):
                    try:
                        next(g)
                    except StopIteration:
                        active.remove(g)

        def drain(g):
            for _ in g:
                pass

        def acq(pool):
            while True:
                it = pool.acquire()
                if it is not None:
                    return it
                yield

        def bfview(bank_ap):
            return bank_ap.bitcast(BF16).rearrange("p (c t) -> p c t", c=8)

        def norm_T_gen(st, xt_ap, xbuf, gT, out_ap3, outbuf, tps, tpsb, pool=None):
            ss, b, _unused = st["sets"].next()
            xb, bxb = st["xb"].next()
            P.op("act", lambda e: e.activation(out=xb[:], in_=xt_ap, func=AF.Square, scale=1.0 / 32,
                                               accum_out=ss[:, 0:1]), [xbuf], [b[0], bxb])
            yield
            P.op("dve", lambda e: e.tensor_scalar(out=ss[:, 1:2], in0=ss[:, 0:1], scalar1=EPS, scalar2=None,
                                                  op0=ALU.add), [b[0]], [b[1]])
            yield
            P.op("act", lambda e: e.activation(out=ss[:, 2:3], in_=ss[:, 1:2], func=AF.Ln), [b[1]], [b[2]])
            yield
            P.op("act", lambda e: e.activation(out=ss[:, 3:4], in_=ss[:, 2:3], func=AF.Exp, scale=-0.5), [b[2]], [b[3]])
            yield
            P.op("act", lambda e: e.activation(out=xb[:], in_=xt_ap, func=AF.Copy, scale=ss[:, 3:4]), [xbuf, b[3]], [bxb])
            yield
            held = None
            if pool is not None:
                while held is None:
                    held = pool.acquire()
                    if held is None:
                        yield
                tps, tpsb = bfview(held[0][:]), held[1]
            P.op("pe", [(lambda e, c=c: e.transpose(tps[:, c, :], xb[:, c * 128:(c + 1) * 128], ident[:])) for c in range(8)],
                 [bxb], [tpsb])
            yield
            P.op("dve", lambda e: e.tensor_tensor(out=out_ap3, in0=tps, in1=gT[:, 0:8].unsqueeze(2).broadcast_to([128, 8, 128]),
                                                  op=ALU.mult), [tpsb], [outbuf])
            if held is not None:
                pool.release(held)
            yield

        def norm_T(st, xt_ap, xbuf, gT, out_ap3, outbuf, tps, tpsb):
            drain(norm_T_gen(st, xt_ap, xbuf, gT, out_ap3, outbuf, tps[:], tpsb))

        def norm_state(stack, pfx, nsets=2, nxb=2):
            st = {}
            st["sets"] = Rot([(sb(stack, pfx + "ss%d" % i, [128, 4]), [Buf("ss") for _ in range(4)], None) for i in range(nsets)])
            st["xb"] = Rot([(sb(stack, pfx + "xb%d" % i, [128, 1024], BF16), Buf("xb")) for i in range(nxb)])
            return st

        def head_norm_rope_gen(ws, src_ps, bsrc, H, g_bc, cosb, sinb, dst, bdst, t):
            w = ws.next()
            n = H * 64
            src3 = src_ps.rearrange("p (h d) -> p h d", h=H)
            P.op("act", lambda e: e.activation(out=w["sq"][:, 0:n], in_=src_ps, func=AF.Square, scale=0.125), [bsrc], [w["bsq"]])
            yield
            P.op("dve", lambda e: e.tensor_reduce(out=w["hs"][:, 0:H], in_=w["sq"][:, 0:n].rearrange("p (h d) -> p h d", h=H),
                                                  axis=AX.X, op=ALU.add), [w["bsq"]], [w["bhs"]])
            yield
            P.op("dve", lambda e: e.tensor_scalar(out=w["hs"][:, 8:8 + H], in0=w["hs"][:, 0:H], scalar1=EPS, scalar2=None, op0=ALU.add),
                 [w["bhs"]], [w["bhs1"]])
            yield
            P.op("act", lambda e: e.activation(out=w["hs"][:, 16:16 + H], in_=w["hs"][:, 8:8 + H], func=AF.Ln), [w["bhs1"]], [w["bhs2"]])
            yield
            P.op("act", lambda e: e.activation(out=w["hs"][:, 24:24 + H], in_=w["hs"][:, 16:16 + H], func=AF.Exp, scale=-0.5),
                 [w["bhs2"]], [w["bhs3"]])
            yield
            qn3 = w["qn"][:, 0:n].rearrange("p (h d) -> p h d", h=H)
            P.op("dve", lambda e: e.tensor_tensor(out=qn3, in0=src3, in1=w["hs"][:, 24:24 + H].unsqueeze(2).broadcast_to([128, H, 64]),
                                                  op=ALU.mult), [bsrc, w["bhs3"]], [w["bqn"]])
            yield
            P.op("dve", lambda e: e.tensor_tensor(out=qn3, in0=qn3, in1=g_bc[:, :].unsqueeze(1).broadcast_to([128, H, 64]),
                                                  op=ALU.mult), [w["bqn"]], [w["bqn"]])
            yield
            qn4 = w["qn"][:, 0:n].rearrange("p (h i t) -> p h i t", h=H, t=2)
            A4 = w["A"][:, 0:n].rearrange("p (h i t) -> p h i t", h=H, t=2)
            B4 = w["B"][:, 0:n].rearrange("p (h i t) -> p h i t", h=H, t=2)
            c4 = cosb[:, t, :].unsqueeze(1).unsqueeze(3).broadcast_to([128, H, 32, 2])
            s4 = sinb[:, t, :].unsqueeze(1).unsqueeze(3).broadcast_to([128, H, 32, 2])
            P.op("dve", lambda e: e.tensor_tensor(out=A4, in0=qn4, in1=c4, op=ALU.mult), [w["bqn"]], [w["bA"]])
            yield
            P.op("dve", lambda e: e.tensor_tensor(out=B4, in0=qn4, in1=s4, op=ALU.mult), [w["bqn"]], [w["bB"]])
            yield
            P.op("dve", lambda e: e.tensor_tensor(out=dst[:, :, :, 0], in0=A4[:, :, :, 0], in1=B4[:, :, :, 1], op=ALU.subtract),
                 [w["bA"], w["bB"]], [bdst])
            yield
            P.op("dve", lambda e: e.tensor_tensor(out=dst[:, :, :, 1], in0=B4[:, :, :, 0], in1=A4[:, :, :, 1], op=ALU.add),
                 [w["bA"], w["bB"]], [bdst])
            yield

        def rope_state(stack, pfx, n, nsets=1):
            sets = []
            for i in range(nsets):
                w = {}
                for k in ("sq", "qn", "A", "B"):
                    w[k] = sb(stack, "%s%s%d" % (pfx, k, i), [128, n])
                w["hs"] = sb(stack, "%shs%d" % (pfx, i), [128, 32])
                for k in ("bsq", "bhs", "bhs1", "bhs2", "bhs3", "bqn", "bA", "bB"):
                    w[k] = Buf(k)
                sets.append(w)
            return Rot(sets)

        with contextlib.ExitStack() as ph:
            wkv = sb(ph, "wkv", [128, 8, 256], BF16)
            cosb = sb(ph, "cosb", [128, 64, 32])
            sinb = sb(ph, "sinb", [128, 64, 32])
            gk_bc = sb(ph, "gk_bc", [128, 64])
            P.dma("pool", lambda e: e.dma_start(out=wkv[:], in_=wv(w_in, 512, 768)), [], [B_const])
            P.dma("sp", lambda e: e.dma_start(out=cosb[:], in_=cos_seq[:, :, :]), [], [B_const])
            P.dma("sp", lambda e: e.dma_start(out=sinb[:], in_=sin_seq[:, :, :]), [], [B_const])
            P.dma("sp", lambda e: e.dma_start(out=gk_bc[:], in_=g_k.partition_broadcast(128)), [], [B_const])
            NP1 = 8
            P.barrier()
            st = norm_state(ph, "p1", nsets=NP1, nxb=NP1)
            rws = rope_state(ph, "p1r", 128, nsets=NP1)
            xts = Rot([(sb(ph, "p1x%d" % i, [128, 1024]), Buf("x")) for i in range(NP1)])
            hTs = Rot([(sb(ph, "p1h%d" % i, [128, 8, 128], BF16), Buf("h")) for i in range(NP1)])
            krs = Rot([(sb(ph, "p1kr%d" % i, [128, 2, 32, 2], BF16), Buf("kr")) for i in range(NP1)])
            banks = Pool([(ps(ph, "p1b%d" % i, [128, 512]), Buf("bank", True)) for i in range(8)])

            import os as _os

            def p1_tile(t):
                xt, bx = xts.next()
                P.dma("sp", lambda e: e.dma_start(out=xt[:], in_=xseq[t * 128:(t + 1) * 128, :]), [], [bx])
                yield
                hT, bh = hTs.next()
                yield from norm_T_gen(st, xt[:], bx, gmixT, hT[:], bh, None, None, pool=banks)
                hkv = yield from acq(banks)
                kv, bkv = hkv
                P.op("pe", [(lambda e, c=c: e.matmul(kv[:, 0:256], hT[:, c, :], wkv[:, c, :], start=(c == 0), stop=(c == 7)))
                            for c in range(8)], [bh, B_const], [bkv])
                yield
                P.op("act", lambda e: e.activation(out=V_all[:, t, :, 0:64], in_=kv[:, 128:256].rearrange("p (h d) -> p h d", h=2),
                                                   func=AF.Copy), [bkv], [])
                yield
                kr, bkr = krs.next()
                yield from head_norm_rope_gen(rws, kv[:, 0:128], bkv, 2, gk_bc, cosb, sinb, kr, bkr, t)
                banks.release(hkv)
                hkt = yield from acq(banks)
                kt, bkt = hkt
                ktv = kt[:].bitcast(BF16)
                P.op("pe", lambda e: e.transpose(ktv[:, 0:128], kr[:].rearrange("p h i t -> p (h i t)"), ident[:]), [bkr], [bkt])
                yield
                P.op("act", lambda e: e.activation(out=kT_all[:, t * 128:(t + 1) * 128], in_=ktv[:, 0:128], func=AF.Copy), [bkt], [])
                banks.release(hkt)
                yield

            run_pipeline((p1_tile(t) for t in range(int(_os.environ.get('NT1', 64)))), int(_os.environ.get('DEPTH1', 8)))
            P.flush()

        if dbg == 1:
            P.limit = 10 ** 9
            P.limit = 10 ** 9
            print("nops", P.nops)
            dk = nc.dram_tensor("dbg_kT", [128, SEQ], BF16, kind="ExternalOutput").ap()
            dv = nc.dram_tensor("dbg_V", [128, 64 * 2 * 65], BF16, kind="ExternalOutput").ap()
            P.dma("sp", lambda e: e.dma_start(out=dk[:, :], in_=kT_all[:]), [], [])
            P.dma("sp", lambda e: e.dma_start(out=dv[:, :], in_=V_all[:].rearrange("p a b c -> p (a b c)")), [], [])
            P.flush()
            attn_stack.close()
            return nc

        with contextlib.ExitStack() as ph:
            wq = sb(ph, "wq", [128, 8, 512], BF16)
            wao = sb(ph, "wao", [64, 8, 1024], BF16)
            cosb = sb(ph, "cosb2", [128, 32, 32])
            sinb = sb(ph, "sinb2", [128, 32, 32])
            gq_bc = sb(ph, "gq_bc", [128, 64])
            ones65 = sb(ph, "ones65", [65, 64])
            B_c2 = Buf("c2")
            for hd_ in range(8):
                pos_ = (hd_ % 4) * 2 + hd_ // 4
                P.dma("pool", lambda e, hd_=hd_, pos_=pos_: e.dma_start(out=wq[:, :, pos_ * 64:(pos_ + 1) * 64], in_=wv(w_in, hd_ * 64, hd_ * 64 + 64)), [], [B_c2])
            P.dma("pool", lambda e: e.dma_start(out=wao[:], in_=w_att_out.rearrange("(h p) n -> p h n", p=64)), [], [B_c2])
            P.dma("sp", lambda e: e.dma_start(out=cosb[:], in_=cos_own[:, :, :]), [], [B_c2])
            P.dma("sp", lambda e: e.dma_start(out=sinb[:], in_=sin_own[:, :, :]), [], [B_c2])
            P.dma("sp", lambda e: e.dma_start(out=gq_bc[:], in_=g_q.partition_broadcast(128)), [], [B_c2])
            P.op("dve", lambda e: e.tensor_scalar(out=gq_bc[:], in0=gq_bc[:], scalar1=0.125, scalar2=None, op0=ALU.mult), [B_c2], [B_c2])
            P.op("pool", lambda e: e.memset(ones65[:], 1.0), [], [B_c2])
            P.barrier()
            st = norm_state(ph, "p2", nsets=4, nxb=4)
            rws = rope_state(ph, "p2r", 512, nsets=3)
            xts = Rot([(sb(ph, "p2x%d" % i, [128, 1024]), Buf("x")) for i in range(4)])
            hTs = Rot([(sb(ph, "p2h%d" % i, [128, 8, 128], BF16), Buf("h")) for i in range(4)])
            qrs = Rot([(sb(ph, "p2qr%d" % i, [128, 8, 32, 2], BF16), Buf("qr")) for i in range(4)])
            qTs = Rot([(sb(ph, "p2qT%d" % i, [128, 4, 512], BF16), [Buf("qT") for _ in range(4)]) for i in range(2)])
            Ps = Rot([(sb(ph, "p2P%d" % i, [128, 2, 512], BF16), Buf("P")) for i in range(4)])
            Osbs = Rot([(sb(ph, "p2Osb%d" % i, [65, 512]), Buf("Osb")) for i in range(2)])
            rrs = Rot([(sb(ph, "p2rr%d" % i, [65, 512]), Buf("rr")) for i in range(2)])
            OnTs = Rot([(sb(ph, "p2OnT%d" % i, [64, 8, 512], BF16), [Buf("OnT") for _ in range(8)]) for i in range(2)])
            ysbs = Rot([(sb(ph, "p2ysb%d" % i, [128, 8, 512]), [Buf("ysb") for _ in range(8)]) for i in range(1)])
            Sps = Rot([(ps(ph, "p2S%d" % i, [128, 2, 512]), Buf("S", True)) for i in range(3)])
            Ops, bO = ps(ph, "p2O", [128, 2, 512]), [Buf("O0", True), Buf("O1", True)]
            LA = int(_os0.environ.get('LA', 2))

            def q_tile(gi, j, qT, bqT):
                tile = gi * 4 + j
                xt, bx = xts.next()
                P.dma("sp", lambda e: e.dma_start(out=xt[:], in_=xown[tile * 128:(tile + 1) * 128, :]), [], [bx])
                yield
                hT, bh = hTs.next()
                S, bS = Sps.next()
                tpv = bfview(S[:, 1, :])
                yield from norm_T_gen(st, xt[:], bx, gmixT, hT[:], bh, tpv, bS)
                P.op("pe", [(lambda e, c=c: e.matmul(S[:, 0, :], hT[:, c, :], wq[:, c, :], start=(c == 0), stop=(c == 7)))
                            for c in range(8)], [bh, B_c2], [bS])
                yield
                qr, bqr = qrs.next()
                yield from head_norm_rope_gen(rws, S[:, 0, :], bS, 8, gq_bc, cosb, sinb, qr, bqr, tile)
                qr3 = qr[:].rearrange("p h i t -> p (h i t)")
                P.op("pe", [(lambda e, g=g: e.transpose(tpv[:, g, :], qr3[:, g * 128:(g + 1) * 128], ident[:])) for g in range(4)],
                     [bqr], [bS])
                yield
                P.op("act", lambda e: e.activation(out=qT[:, :, j * 128:(j + 1) * 128], in_=tpv[:, 0:4, :], func=AF.Copy),
                     [bS], [bqT[j]])
                yield

            for gi in range(8):
                qT, bqT = qTs.next()
                run_pipeline((q_tile(gi, j, qT, bqT) for j in range(4)), int(_os0.environ.get('QD', 3)))
                OnT, bOn = OnTs.next()
                for g in range(4):
                    def issue_S(kt, g=g, qT=qT):
                        S, bS = Sps.next()
                        P.op("pe", [lambda e, S=S, kt=kt: e.matmul(S[:, 0, :], kT_all[0:64, kt * 128:(kt + 1) * 128], qT[0:64, g, :], start=True, stop=True),
                                    lambda e, S=S, kt=kt: e.matmul(S[:, 1, :], kT_all[64:128, kt * 128:(kt + 1) * 128], qT[64:128, g, :], start=True, stop=True)],
                             bqT, [bS])
                        return S, bS
                    pend = [issue_S(kt) for kt in range(LA)]
                    for kt in range(64):
                        if kt + LA < 64:
                            pend.append(issue_S(kt + LA))
                        S, bS = pend.pop(0)
                        Pt, bP = Ps.next()
                        P.op("act", lambda e, S=S, Pt=Pt: e.activation(out=Pt[:], in_=S[:], func=AF.Exp), [bS], [bP])
                        P.op("pe", [lambda e, Pt=Pt, kt=kt: e.matmul(Ops[0:65, 0, :], V_all[:, kt, 0, :], Pt[:, 0, :], start=(kt == 0), stop=(kt == 63)),
                                    lambda e, Pt=Pt, kt=kt: e.matmul(Ops[0:65, 1, :], V_all[:, kt, 1, :], Pt[:, 1, :], start=(kt == 0), stop=(kt == 63))],
                             [bP], bO)
                    for hh in range(2):
                        head = g + 4 * hh
                        Osb, bOsb = Osbs.next()
                        rr, brr = rrs.next()
                        mA, bmA = Sps.next()
                        P.op("act", lambda e, Osb=Osb, hh=hh: e.activation(out=Osb[:], in_=Ops[0:65, hh, :], func=AF.Copy), [bO[hh]], [bOsb])
                        P.op("dve", lambda e, Osb=Osb, rr=rr: e.reciprocal(out=rr[64:65, :], in_=Osb[64:65, :]), [bOsb], [brr])
                        P.op("pe", lambda e, rr=rr, mA=mA: e.matmul(mA[0:64, 0, :], ones65[64:65, 0:64], rr[64:65, :], start=True, stop=True), [brr, B_c2], [bmA])
                        P.op("dve", lambda e, Osb=Osb, OnT=OnT, head=head, mA=mA: e.tensor_tensor(out=OnT[:, head, :], in0=Osb[0:64, :], in1=mA[0:64, 0, :], op=ALU.mult),
                             [bOsb, bmA], [bOn[head]])
                ysb, bys = ysbs.next()
                for dc in range(8):
                    mA, bmA = Sps.next()
                    P.op("pe", [(lambda e, h=h, dc=dc, OnT=OnT, mA=mA: e.matmul(mA[:, 0, :], wao[:, h, dc * 128:(dc + 1) * 128], OnT[:, h, :], start=(h == 0), stop=(h == 7)))
                                for h in range(8)], bOn + [B_c2], [bmA])
                    P.op("act", lambda e, ysb=ysb, dc=dc, mA=mA: e.activation(out=ysb[:, dc, :], in_=mA[:, 0, :], func=AF.Copy), [bmA], [bys[dc]])
                P.dma("sp", lambda e, ysb=ysb, gi=gi: e.dma_start(out=yatt_d[gi], in_=ysb[:]), bys, [])
            P.flush()

        attn_stack.close()
        if dbg == 2:
            dy = nc.dram_tensor("dbg_yatt", [8, 128, 8, 512], F32, kind="ExternalOutput").ap()
            P.dma("sp", lambda e: e.dma_start(out=dy.rearrange("a p c t -> (a p) (c t)"), in_=yatt_d.rearrange("a p c t -> (a p) (c t)")), [], [])
            P.flush()
            return nc

        with contextlib.ExitStack() as ph:
            wckv = sb(ph, "wckv", [128, 8, 1024], BF16)
            gmemT = sb(ph, "gmemT", [128, 8])
            memT = sb(ph, "memT", [128, 8, 256], BF16)
            B_cm = Buf("cm")
            P.dma("pool", lambda e: e.dma_start(out=wckv[:], in_=wv(w_ckv, 0, 1024)), [], [B_cm])
            P.dma("sp", lambda e: e.dma_start(out=gmemT[:], in_=gT_view(g_mem), allow_slow_non_contiguous=True), [], [B_cm])
            st = norm_state(ph, "pm")
            xts = Rot([(sb(ph, "pmx%d" % i, [128, 1024]), Buf("x")) for i in range(2)])
            tp, btp = ps(ph, "pmtp", [128, 8, 128], BF16), Buf("tp", True)
            mps = Rot([(ps(ph, "pmps%d" % i, [128, 512]), Buf("mps", True)) for i in range(2)])
            bmemT = [Buf("memT0"), Buf("memT1")]
            for mt in range(2):
                xt, bx = xts.next()
                P.dma("sp", lambda e, xt=xt, mt=mt: e.dma_start(out=xt[:], in_=memd[mt * 128:(mt + 1) * 128, :]), [], [bx])
                norm_T(st, xt[:], bx, gmemT, memT[:, :, mt * 128:(mt + 1) * 128], bmemT[mt], tp, btp)
            for h in range(4):
                mp, bmp = mps.next()
                P.op("pe", [(lambda e, c=c, h=h, mp=mp: e.matmul(mp[:, 0:256], wckv[:, c, h * 128:(h + 1) * 128], memT[:, c, :], start=(c == 0), stop=(c == 7)))
                            for c in range(8)], bmemT + [B_cm], [bmp])
                P.op("act", lambda e, h=h, mp=mp: e.activation(out=kcT[:, h, :], in_=mp[:, 0:256], func=AF.Copy), [bmp], [])
            for mt in range(2):
                mp, bmp = mps.next()
                P.op("pe", [(lambda e, c=c, mt=mt, mp=mp: e.matmul(mp[:], memT[:, c, mt * 128:(mt + 1) * 128], wckv[:, c, 512:1024], start=(c == 0), stop=(c == 7)))
                            for c in range(8)], bmemT + [B_cm], [bmp])
                P.op("act", lambda e, mt=mt, mp=mp: e.activation(out=Vc[:, mt, :], in_=mp[:], func=AF.Copy), [bmp], [])
            P.flush()

        with contextlib.ExitStack() as ph:
            wsv = sb(ph, "wsv", [128, 8, 512], BF16)
            wsu = sb(ph, "wsu", [128, 8, 512], BF16)
            wga = sb(ph, "wga", [128, 8, 1024], BF16)
            wgb = sb(ph, "wgb", [128, 8, 1024], BF16)
            wso = sb(ph, "wso", [128, 4, 1024], BF16)
            wo = sb(ph, "wo", [128, 8, 1024], BF16)
            wcq = sb(ph, "wcq", [128, 8, 512], BF16)
            wco = sb(ph, "wco", [128, 4, 1024], BF16)
            gcrT = sb(ph, "gcrT", [128, 8])
            lng_bc = sb(ph, "lng_bc", [128, 512])
            lnb_bc = sb(ph, "lnb_bc", [128, 512])
            bs_bc = sb(ph, "bs_bc", [128, 4, 128])
            wsraw = sb(ph, "wsraw", [128, 4, 128], BF16)
            WsT = sb(ph, "WsT", [128, 4, 128], BF16)
            B_c = Buf("c2b")
            B_ws = Buf("ws")
            P.dma("pool", lambda e: e.dma_start(out=wsu[:], in_=wv(w_in, 768, 1280)), [], [B_c])
            P.dma("pool", lambda e: e.dma_start(out=wsv[:], in_=wv(w_in, 1280, 1792)), [], [B_c])
            P.dma("pool", lambda e: e.dma_start(out=wga[:], in_=wv(w_in, 1792, 2816)), [], [B_c])
            P.dma("pool", lambda e: e.dma_start(out=wgb[:], in_=wv(w_in, 2816, 3840)), [], [B_c])
            P.dma("pool", lambda e: e.dma_start(out=wso[:], in_=wv(w_sgu_out, 0, 1024)), [], [B_c])
            P.dma("pool", lambda e: e.dma_start(out=wo[:], in_=wv(w_out, 0, 1024)), [], [B_c])
            P.dma("pool", lambda e: e.dma_start(out=wcq[:], in_=wv(w_cq, 0, 512)), [], [B_c])
            P.dma("pool", lambda e: e.dma_start(out=wco[:], in_=wv(w_co, 0, 1024)), [], [B_c])
            P.dma("pool", lambda e: e.dma_start(out=wsraw[:], in_=w_s.rearrange("g p q -> p g q")), [], [B_ws])
            P.dma("sp", lambda e: e.dma_start(out=gcrT[:], in_=gT_view(g_cross), allow_slow_non_contiguous=True), [], [B_c])
            P.dma("sp", lambda e: e.dma_start(out=lng_bc[:], in_=sgu_ln_g.partition_broadcast(128)), [], [B_c])
            P.dma("sp", lambda e: e.dma_start(out=lnb_bc[:], in_=sgu_ln_b.partition_broadcast(128)), [], [B_c])
            P.dma("sp", lambda e: e.dma_start(out=bs_bc[:].rearrange("p g q -> p (g q)"), in_=b_s.rearrange("g q -> (g q)").partition_broadcast(128)), [], [B_c])
            P.barrier()
            st = norm_state(ph, "p3", nsets=4, nxb=4)
            bpool = Pool([(ps(ph, "p3g%d" % i, [128, 512]), Buf("g", True)) for i in range(8)])
            hb0 = bpool.acquire()
            tpb0 = bfview(hb0[0][:])
            P.op("pe", [(lambda e, g=g: e.transpose(tpb0[:, g, :], wsraw[:, g, :], ident[:])) for g in range(4)], [B_ws], [hb0[1]])
            P.op("act", lambda e: e.activation(out=WsT[:], in_=tpb0[:, 0:4, :], func=AF.Copy), [hb0[1]], [B_c])
            bpool.release(hb0)
            P.barrier()

            xg = [(sb(ph, "p3x%d" % i, [128, 1024]), Buf("x")) for i in range(4)]
            hTg, bhT = sb(ph, "p3hT", [128, 8, 512], BF16), [Buf("hT") for _ in range(4)]
            hcT, bhc = hTg, bhT
            NA = 3
            asets = Pool([dict(gv=sb(ph, "p3gv%d" % i, [128, 512]), vn=sb(ph, "p3vn%d" % i, [128, 512], BF16), lst=sb(ph, "p3lst%d" % i, [128, 16]),
                               bgv=Buf("gv"), bvn=Buf("vn"), bl=[Buf("l") for _ in range(8)]) for i in range(NA)])
            vmb, bvmb = sb(ph, "p3vmb", [128, 4, 512]), [Buf("vmb") for _ in range(4)]
            gus = Pool([(sb(ph, "p3gu%d" % i, [128, 512]), Buf("gu")) for i in range(2)])
            sguT, bsg = sb(ph, "p3sguT", [128, 4, 512], BF16), [Buf("sg") for _ in range(4)]
            csets = Pool([dict(ya=sb(ph, "p3ya%d" % i, [128, 512]), sa=sb(ph, "p3sa%d" % i, [128, 512]), sb=sb(ph, "p3sb%d" % i, [128, 512]),
                               bya=Buf("ya"), bsa=Buf("sa"), bsb=Buf("sb")) for i in range(2)])
            mrg, bmrg = sb(ph, "p3mrg", [128, 8, 512], BF16), [Buf("mrg") for _ in range(8)]
            qcT, bqc = sb(ph, "p3qcT", [128, 4, 512], BF16), [Buf("qc") for _ in range(4)]
            gsets = Pool([dict(cst=sb(ph, "p3cst%d" % i, [128, 16]), pc=sb(ph, "p3pc%d" % i, [128, 4, 256]), pn=sb(ph, "p3pn%d" % i, [128, 4, 256], BF16),
                               pnT=sb(ph, "p3pnT%d" % i, [128, 8, 128], BF16), ocT=sb(ph, "p3ocT%d" % i, [128, 4, 128], BF16),
                               bc=[Buf("c") for _ in range(4)], bpc=Buf("pc"), bpn=Buf("pn"), bpnT=Buf("pnT"), boc=Buf("oc")) for i in range(2)])
            print("sbuf bytes remaining in 2b:", nc.sbuf_bytes_remaining)

            def a_tile(gi, j):
                tile = gi * 4 + j
                xt, bx = xg[j]
                P.dma("sp", lambda e: e.dma_start(out=xt[:], in_=xown[tile * 128:(tile + 1) * 128, :]), [], [bx])
                yield
                yield from norm_T_gen(st, xt[:], bx, gmixT, hTg[:, :, j * 128:(j + 1) * 128], bhT[j], None, None, pool=bpool)
                A = yield from acq(asets)
                gv, vn, lst, bgv, bvn, bl = A["gv"], A["vn"], A["lst"], A["bgv"], A["bvn"], A["bl"]
                h0 = yield from acq(bpool)
                g0, bg0 = h0
                P.op("pe", [(lambda e, c=c: e.matmul(g0[:], hTg[:, c, j * 128:(j + 1) * 128], wsv[:, c, :], start=(c == 0), stop=(c == 7)))
                            for c in range(8)], [bhT[j], B_c], [bg0])
                yield
                P.op("act", lambda e: e.activation(out=gv[:], in_=g0[:], func=AF.Gelu_apprx_tanh), [bg0], [bgv])
                bpool.release(h0)
                yield
                P.op("dve", lambda e: e.tensor_reduce(out=lst[:, 0:1], in_=gv[:], axis=AX.X, op=ALU.add), [bgv], [bl[0]])
                yield
                P.op("act", lambda e: e.activation(out=vn[:], in_=gv[:], func=AF.Square, accum_out=lst[:, 1:2]), [bgv], [bvn, bl[1]])
                yield
                P.op("dve", lambda e: e.tensor_scalar(out=lst[:, 2:3], in0=lst[:, 0:1], scalar1=1.0 / 512, scalar2=None, op0=ALU.mult), [bl[0]], [bl[2]])
                yield
                P.op("dve", lambda e: e.tensor_tensor(out=lst[:, 3:4], in0=lst[:, 2:3], in1=lst[:, 2:3], op=ALU.mult), [bl[2]], [bl[3]])
                yield
                P.op("dve", lambda e: e.scalar_tensor_tensor(out=lst[:, 4:5], in0=lst[:, 1:2], scalar=1.0 / 512, in1=lst[:, 3:4], op0=ALU.mult, op1=ALU.subtract),
                     [bl[1], bl[3]], [bl[4]])
                yield
                P.op("dve", lambda e: e.tensor_scalar(out=lst[:, 5:6], in0=lst[:, 4:5], scalar1=EPS, scalar2=None, op0=ALU.add), [bl[4]], [bl[5]])
                yield
                P.op("act", lambda e: e.activation(out=lst[:, 6:7], in_=lst[:, 5:6], func=AF.Ln), [bl[5]], [bl[6]])
                yield
                P.op("act", lambda e: e.activation(out=lst[:, 7:8], in_=lst[:, 6:7], func=AF.Exp, scale=-0.5), [bl[6]], [bl[7]])
                yield
                P.op("dve", lambda e: e.tensor_scalar(out=gv[:], in0=gv[:], scalar1=lst[:, 2:3], scalar2=lst[:, 7:8], op0=ALU.subtract, op1=ALU.mult),
                     [bgv, bl[2], bl[7]], [bgv])
                yield
                P.op("dve", lambda e: e.tensor_tensor(out=gv[:], in0=gv[:], in1=lng_bc[:], op=ALU.mult), [bgv, B_c], [bgv])
                yield
                P.op("dve", lambda e: e.tensor_tensor(out=vn[:], in0=gv[:], in1=lnb_bc[:], op=ALU.add), [bgv, B_c], [bvn])
                yield
                h1 = yield from acq(bpool)
                g1, bg1 = h1
                P.op("pe", [(lambda e, g=g: e.matmul(g1[:, g * 128:(g + 1) * 128], vn[:, g * 128:(g + 1) * 128], WsT[:, g, :], start=True, stop=True))
                            for g in range(4)], [bvn, B_c], [bg1])
                yield
                P.op("dve", lambda e: e.tensor_tensor(out=vmb[:, :, j * 128:(j + 1) * 128], in0=g1[:].rearrange("p (g q) -> p g q", g=4),
                                                      in1=bs_bc[:], op=ALU.add), [bg1, B_c], [bvmb[j]])
                bpool.release(h1)
                asets.release(A)
                yield

            def b_chunk(g):
                h0 = yield from acq(bpool)
                g0, bg0 = h0
                P.op("pe", [(lambda e, c=c: e.matmul(g0[:], wsu[:, c, g * 128:(g + 1) * 128], hTg[:, c, :], start=(c == 0), stop=(c == 7)))
                            for c in range(8)], bhT + [B_c], [bg0])
                yield
                G = yield from acq(gus)
                gu, bgu = G
                P.op("act", lambda e: e.activation(out=gu[:], in_=g0[:], func=AF.Gelu_apprx_tanh), [bg0], [bgu])
                bpool.release(h0)
                yield
                P.op("dve", lambda e: e.tensor_tensor(out=sguT[:, g, :], in0=gu[:], in1=vmb[:, g, :], op=ALU.mult), [bgu] + bvmb, [bsg[g]])
                gus.release(G)
                yield

            def c_chunk(gi, dc):
                C = yield from acq(csets)
                ya, sa, sbt, bya, bsa, bsb = C["ya"], C["sa"], C["sb"], C["bya"], C["bsa"], C["bsb"]
                P.dma("sp", lambda e: e.dma_start(out=ya[:], in_=yatt_d[gi][:, dc, :]), [], [bya])
                yield
                h0 = yield from acq(bpool)
                ga_ps, bga = h0
                P.op("pe", [(lambda e, c=c: e.matmul(ga_ps[:], wga[:, c, dc * 128:(dc + 1) * 128], hTg[:, c, :], start=(c == 0), stop=(c == 7)))
                            for c in range(8)], bhT + [B_c], [bga])
                yield
                P.op("act", lambda e: e.activation(out=sa[:], in_=ga_ps[:], func=AF.Sigmoid), [bga], [bsa])
                bpool.release(h0)
                yield
                h1 = yield from acq(bpool)
                gb_ps, bgb = h1
                P.op("pe", [(lambda e, c=c: e.matmul(gb_ps[:], wgb[:, c, dc * 128:(dc + 1) * 128], hTg[:, c, :], start=(c == 0), stop=(c == 7)))
                            for c in range(8)], bhT + [B_c], [bgb])
                yield
                P.op("act", lambda e: e.activation(out=sbt[:], in_=gb_ps[:], func=AF.Sigmoid), [bgb], [bsb])
                bpool.release(h1)
                yield
                h2 = yield from acq(bpool)
                ys_ps, bysp = h2
                P.op("pe", [(lambda e, g=g: e.matmul(ys_ps[:], wso[:, g, dc * 128:(dc + 1) * 128], sguT[:, g, :], start=(g == 0), stop=(g == 3)))
                            for g in range(4)], bsg + [B_c], [bysp])
                yield
                P.op("dve", lambda e: e.tensor_tensor(out=sa[:], in0=sa[:], in1=ya[:], op=ALU.mult), [bsa, bya], [bsa])
                yield
                P.op("dve", lambda e: e.tensor_tensor(out=sbt[:], in0=sbt[:], in1=ys_ps[:], op=ALU.mult), [bsb, bysp], [bsb])
                bpool.release(h2)
                yield
                P.op("dve", lambda e: e.tensor_tensor(out=mrg[:, dc, :], in0=sa[:], in1=sbt[:], op=ALU.add), [bsa, bsb], [bmrg[dc]])
                csets.release(C)
                yield

            def d_part(j, nb):
                xt, bx = xg[j]
                h0 = yield from acq(bpool)
                o_ps, bo = h0
                P.op("pe", [(lambda e, dc=dc: e.matmul(o_ps[:], mrg[:, dc, j * 128:(j + 1) * 128], wo[:, dc, nb * 512:(nb + 1) * 512],
                                                      start=(dc == 0), stop=(dc == 7))) for dc in range(8)], bmrg + [B_c], [bo])
                yield
                P.op("dve", lambda e: e.tensor_tensor(out=xt[:, nb * 512:(nb + 1) * 512], in0=xt[:, nb * 512:(nb + 1) * 512], in1=o_ps[:], op=ALU.add),
                     [bo, bx], [bx])
                bpool.release(h0)
                yield

            def e_tile(j):
                xt, bx = xg[j]
                yield from norm_T_gen(st, xt[:], bx, gcrT, hcT[:, :, j * 128:(j + 1) * 128], bhc[j], None, None, pool=bpool)

            def f_head(h):
                h0 = yield from acq(bpool)
                q_ps, bq = h0
                P.op("pe", [(lambda e, c=c: e.matmul(q_ps[:], wcq[:, c, h * 128:(h + 1) * 128], hcT[:, c, :], start=(c == 0), stop=(c == 7)))
                            for c in range(8)], bhc + [B_c], [bq])
                yield
                P.op("act", lambda e: e.activation(out=qcT[:, h, :], in_=q_ps[:], func=AF.Copy, scale=float(128 ** -0.5)), [bq], [bqc[h]])
                bpool.release(h0)
                yield

            def g_tile(gi, j):
                tile = gi * 4 + j
                xt, bx = xg[j]
                Gs = yield from acq(gsets)
                cst, pc, pn, pnT, ocT = Gs["cst"], Gs["pc"], Gs["pn"], Gs["pnT"], Gs["ocT"]
                bc, bpc, bpn, bpnT, boc = Gs["bc"], Gs["bpc"], Gs["bpn"], Gs["bpnT"], Gs["boc"]
                hs0 = yield from acq(bpool)
                hs1 = yield from acq(bpool)
                scs = [hs0[0], hs1[0]]
                bscs = [hs0[1], hs1[1]]
                for half in range(2):
                    P.op("pe", [(lambda e, hh=hh, half=half: e.matmul(scs[half][:, hh * 256:(hh + 1) * 256], qcT[:, half * 2 + hh, j * 128:(j + 1) * 128], kcT[:, half * 2 + hh, :],
                                                                       start=True, stop=True)) for hh in range(2)], bqc, [bscs[half]])
                    yield
                for half in range(2):
                    P.op("dve", lambda e, half=half: e.tensor_reduce(out=cst[:, half * 2:half * 2 + 2], in_=scs[half][:].rearrange("p (h m) -> p h m", h=2), axis=AX.X, op=ALU.max),
                         [bscs[half]], [bc[0]])
                    yield
                P.op("dve", lambda e: e.tensor_scalar(out=cst[:, 4:8], in0=cst[:, 0:4], scalar1=-1.0, scalar2=None, op0=ALU.mult), [bc[0]], [bc[1]])
                yield
                for half in range(2):
                    P.op("act", [(lambda e, hh=hh, half=half: e.activation(out=pc[:, half * 2 + hh, :], in_=scs[half][:, hh * 256:(hh + 1) * 256], func=AF.Exp,
                                                                          bias=cst[:, 4 + half * 2 + hh:5 + half * 2 + hh], accum_out=cst[:, 8 + half * 2 + hh:9 + half * 2 + hh]))
                                 for hh in range(2)], [bscs[half], bc[1]], [bpc, bc[2]])
                    yield
                bpool.release(hs0)
                bpool.release(hs1)
                P.op("dve", lambda e: e.reciprocal(out=cst[:, 12:16], in_=cst[:, 8:12]), [bc[2]], [bc[3]])
                yield
                P.op("dve", lambda e: e.tensor_tensor(out=pn[:], in0=pc[:], in1=cst[:, 12:16].unsqueeze(2).broadcast_to([128, 4, 256]), op=ALU.mult),
                     [bpc, bc[3]], [bpn])
                yield
                ht = yield from acq(bpool)
                tpv = bfview(ht[0][:])
                P.op("pe", [(lambda e, k=k: e.transpose(tpv[:, k, :], pn[:, k // 2, (k % 2) * 128:(k % 2 + 1) * 128], ident[:])) for k in range(8)], [bpn], [ht[1]])
                yield
                P.op("act", lambda e: e.activation(out=pnT[:], in_=tpv, func=AF.Copy), [ht[1]], [bpnT])
                bpool.release(ht)
                yield
                ho = yield from acq(bpool)
                oc_ps, bocp = ho
                fl = []
                for h in range(4):
                    for mt in range(2):
                        fl.append(lambda e, h=h, mt=mt: e.matmul(oc_ps[:, h * 128:(h + 1) * 128], Vc[:, mt, h * 128:(h + 1) * 128], pnT[:, h * 2 + mt, :],
                                                                 start=(mt == 0), stop=(mt == 1)))
                P.op("pe", fl, [bpnT], [bocp])
                yield
                P.op("act", lambda e: e.activation(out=ocT[:], in_=oc_ps[:].rearrange("p (h t) -> p h t", h=4), func=AF.Copy), [bocp], [boc])
                bpool.release(ho)
                yield
                for nb in range(2):
                    hy = yield from acq(bpool)
                    y_ps, byp = hy
                    P.op("pe", [(lambda e, h=h, nb=nb, y_ps=y_ps: e.matmul(y_ps[:], ocT[:, h, :], wco[:, h, nb * 512:(nb + 1) * 512], start=(h == 0), stop=(h == 3)))
                                for h in range(4)], [boc, B_c], [byp])
                    yield
                    P.op("dve", lambda e, nb=nb, y_ps=y_ps: e.tensor_tensor(out=xt[:, nb * 512:(nb + 1) * 512], in0=xt[:, nb * 512:(nb + 1) * 512], in1=y_ps[:], op=ALU.add),
                         [byp, bx], [bx])
                    bpool.release(hy)
                    yield
                gsets.release(Gs)
                P.dma("sp", lambda e: e.dma_start(out=x2_d[tile * 128:(tile + 1) * 128, :], in_=xt[:]), [bx], [])
                yield

            for gi in range(8):
                run_pipeline((a_tile(gi, j) for j in range(4)), 4)
                run_pipeline((b_chunk(g) for g in range(4)), 3)
                run_pipeline((c_chunk(gi, dc) for dc in range(8)), 3)
                run_pipeline((d_part(j, nb) for j in range(4) for nb in range(2)), 4)
                run_pipeline((e_tile(j) for j in range(4)), 4)
                run_pipeline((f_head(h) for h in range(4)), 4)
                run_pipeline((g_tile(gi, j) for j in range(4)), 3)
            P.flush()

        if dbg == 3:
            dx = nc.dram_tensor("dbg_x2", [OWN, D], F32, kind="ExternalOutput").ap()
            P.dma("sp", lambda e: e.dma_start(out=dx[:, :], in_=x2_d[:, :]), [], [])
            P.flush()
            return nc

        with contextlib.ExitStack() as ph:
            gmoT = sb(ph, "gmoT", [128, 8])
            wr = sb(ph, "wr", [128, 8, 20], BF16)
            br_bc = sb(ph, "br_bc", [128, 20])
            gfin_bc = sb(ph, "gfin_bc", [128, 1024])
            B_c = Buf("c3")
            P.dma("sp", lambda e: e.dma_start(out=gmoT[:], in_=gT_view(g_moe), allow_slow_non_contiguous=True), [], [B_c])
            P.dma("pool", lambda e: e.dma_start(out=wr[:, :, 0:4], in_=wv(w_rg, 0, 4)), [], [B_c])
            P.dma("pool", lambda e: e.dma_start(out=wr[:, :, 4:20], in_=wv(w_re, 0, 16)), [], [B_c])
            P.dma("sp", lambda e: e.dma_start(out=br_bc[:, 0:4], in_=b_rg.partition_broadcast(128)), [], [B_c])
            P.dma("sp", lambda e: e.dma_start(out=br_bc[:, 4:20], in_=b_re.partition_broadcast(128)), [], [B_c])
            P.dma("sp", lambda e: e.dma_start(out=gfin_bc[:], in_=g_final.partition_broadcast(128)), [], [B_c])
            P.barrier()
            NP3 = 6
            st = norm_state(ph, "p4", nsets=NP3, nxb=NP3)
            xres, bxr = sb(ph, "xres", [128, 16, 1024]), [Buf("xr%d" % i) for i in range(16)]
            hmT, bhm = sb(ph, "hmT", [128, 8, 2048], BF16), [Buf("hm%d" % i) for i in range(16)]
            comb, bcomb = sb(ph, "comb", [128, 16, 16]), [Buf("cb%d" % i) for i in range(16)]
            Wg = [(sb(ph, "Wg%d" % i, [128, 8, 512], BF16), Buf("Wg")) for i in range(2)]
            Wu = [(sb(ph, "Wu%d" % i, [128, 8, 512], BF16), Buf("Wu")) for i in range(2)]
            Wd = [(sb(ph, "Wd%d" % i, [128, 4, 1024], BF16), Buf("Wd")) for i in range(2)]
            sgs = Rot([(sb(ph, "sg%d" % i, [128, 512]), Buf("sg")) for i in range(2)])
            heTs = Rot([(sb(ph, "heT%d" % i, [128, 4, 512], BF16), [Buf("he") for _ in range(4)]) for i in range(2)])
            ots = Rot([(sb(ph, "ot%d" % i, [128, 1024]), Buf("ot")) for i in range(2)])
            rts = Rot([(sb(ph, "rt%d" % i, [128, 160]), [Buf("rt") for _ in range(24)]) for i in range(NP3)])
            allbanks = [(ps(ph, "p4g%d" % i, [128, 512]), Buf("g", True)) for i in range(8)]
            bpool = Pool(allbanks)
            gen = Rot(allbanks)

            def moe_tile(sgi, tl):
                tile = sgi * 16 + tl
                P.dma("sp", lambda e: e.dma_start(out=xres[:, tl, :], in_=x2_d[tile * 128:(tile + 1) * 128, :]), [], [bxr[tl]])
                yield
                yield from norm_T_gen(st, xres[:, tl, :], bxr[tl], gmoT, hmT[:, :, tl * 128:(tl + 1) * 128], bhm[tl], None, None, pool=bpool)
                hb = yield from acq(bpool)
                r_ps, brp = hb
                P.op("pe", [(lambda e, c=c: e.matmul(r_ps[:, 0:20], hmT[:, c, tl * 128:(tl + 1) * 128], wr[:, c, :], start=(c == 0), stop=(c == 7)))
                            for c in range(8)], [bhm[tl], B_c], [brp])
                yield
                R, brt = rts.next()
                P.op("dve", lambda e: e.tensor_tensor(out=R[:, 0:20], in0=r_ps[:, 0:20], in1=br_bc[:], op=ALU.add), [brp, B_c], [brt[0]])
                bpool.release(hb)
                yield
                P.op("dve", lambda e: e.tensor_reduce(out=R[:, 20:21], in_=R[:, 0:4], axis=AX.X, op=ALU.max), [brt[0]], [brt[1]])
                yield
                P.op("dve", lambda e: e.tensor_scalar(out=R[:, 21:22], in0=R[:, 20:21], scalar1=-1.0, scalar2=None, op0=ALU.mult), [brt[1]], [brt[2]])
                yield
                P.op("act", lambda e: e.activation(out=R[:, 22:26], in_=R[:, 0:4], func=AF.Exp, bias=R[:, 21:22], accum_out=R[:, 26:27]), [brt[0], brt[2]], [brt[3]])
                yield
                P.op("dve", lambda e: e.reciprocal(out=R[:, 27:28], in_=R[:, 26:27]), [brt[3]], [brt[4]])
                yield
                P.op("dve", lambda e: e.tensor_scalar(out=R[:, 28:32], in0=R[:, 0:4], scalar1=R[:, 20:21], scalar2=None, op0=ALU.is_ge), [brt[0], brt[1]], [brt[5]])
                yield
                P.op("dve", lambda e: e.tensor_scalar(out=R[:, 32:36], in0=R[:, 28:32], scalar1=BIG, scalar2=-BIG, op0=ALU.mult, op1=ALU.add), [brt[5]], [brt[6]])
                yield
                P.op("dve", lambda e: e.tensor_tensor(out=R[:, 40:56].rearrange("p (g k) -> p g k", g=4), in0=R[:, 4:20].rearrange("p (g k) -> p g k", g=4),
                                                      in1=R[:, 32:36].unsqueeze(2).broadcast_to([128, 4, 4]), op=ALU.add), [brt[0], brt[6]], [brt[7]])
                yield
                P.op("dve", lambda e: e.tensor_reduce(out=R[:, 56:57], in_=R[:, 40:56], axis=AX.X, op=ALU.max), [brt[7]], [brt[8]])
                yield
                P.op("dve", lambda e: e.tensor_scalar(out=R[:, 60:76], in0=R[:, 40:56], scalar1=R[:, 56:57], scalar2=None, op0=ALU.is_ge), [brt[7], brt[8]], [brt[9]])
                yield
                P.op("dve", lambda e: e.scalar_tensor_tensor(out=R[:, 80:96], in0=R[:, 60:76], scalar=-BIG, in1=R[:, 40:56], op0=ALU.mult, op1=ALU.add),
                     [brt[9], brt[7]], [brt[10]])
                yield
                P.op("dve", lambda e: e.tensor_reduce(out=R[:, 96:97], in_=R[:, 80:96], axis=AX.X, op=ALU.max), [brt[10]], [brt[11]])
                yield
                P.op("dve", lambda e: e.tensor_scalar(out=R[:, 100:116], in0=R[:, 80:96], scalar1=R[:, 96:97], scalar2=None, op0=ALU.is_ge), [brt[10], brt[11]], [brt[12]])
                yield
                P.op("dve", lambda e: e.tensor_tensor(out=R[:, 116:117], in0=R[:, 96:97], in1=R[:, 56:57], op=ALU.subtract), [brt[11], brt[8]], [brt[13]])
                yield
                P.op("act", lambda e: e.activation(out=R[:, 117:118], in_=R[:, 116:117], func=AF.Exp), [brt[13]], [brt[14]])
                yield
                P.op("dve", lambda e: e.tensor_scalar(out=R[:, 118:119], in0=R[:, 117:118], scalar1=1.0, scalar2=None, op0=ALU.add), [brt[14]], [brt[15]])
                yield
                P.op("dve", lambda e: e.reciprocal(out=R[:, 119:120], in_=R[:, 118:119]), [brt[15]], [brt[16]])
                yield
                P.op("dve", lambda e: e.tensor_tensor(out=R[:, 120:121], in0=R[:, 119:120], in1=R[:, 27:28], op=ALU.mult), [brt[16], brt[4]], [brt[17]])
                yield
                P.op("dve", lambda e: e.tensor_tensor(out=R[:, 121:122], in0=R[:, 120:121], in1=R[:, 117:118], op=ALU.mult), [brt[17], brt[14]], [brt[18]])
                yield
                P.op("dve", lambda e: e.tensor_scalar(out=comb[:, tl, :], in0=R[:, 60:76], scalar1=R[:, 120:121], scalar2=None, op0=ALU.mult),
                     [brt[9], brt[17]], [bcomb[tl]])
                yield
                P.op("dve", lambda e: e.scalar_tensor_tensor(out=comb[:, tl, :], in0=R[:, 100:116], scalar=R[:, 121:122], in1=comb[:, tl, :], op0=ALU.mult, op1=ALU.add),
                     [brt[12], brt[18], bcomb[tl]], [bcomb[tl]])
                yield

            def fin_tile(sgi, tl):
                tile = sgi * 16 + tl
                fss, fb, _u = st["sets"].next()
                ot, bot = ots.next()
                P.op("act", lambda e: e.activation(out=ot[:], in_=xres[:, tl, :], func=AF.Square, scale=1.0 / 32, accum_out=fss[:, 0:1]), [bxr[tl]], [fb[0], bot])
                yield
                P.op("dve", lambda e: e.tensor_scalar(out=fss[:, 1:2], in0=fss[:, 0:1], scalar1=EPS, scalar2=None, op0=ALU.add), [fb[0]], [fb[1]])
                yield
                P.op("act", lambda e: e.activation(out=fss[:, 2:3], in_=fss[:, 1:2], func=AF.Ln), [fb[1]], [fb[2]])
                yield
                P.op("act", lambda e: e.activation(out=fss[:, 3:4], in_=fss[:, 2:3], func=AF.Exp, scale=-0.5), [fb[2]], [fb[3]])
                yield
                P.op("dve", lambda e: e.scalar_tensor_tensor(out=ot[:], in0=xres[:, tl, :], scalar=fss[:, 3:4], in1=gfin_bc[:], op0=ALU.mult, op1=ALU.mult),
                     [bxr[tl], fb[3], B_c], [bot])
                yield
                P.dma("sp", lambda e: e.dma_start(out=out[tile * 128:(tile + 1) * 128, :], in_=ot[:]), [bot], [])
                yield

            def load_w(e_):
                (wg, bwg), (wu, bwu), (wd, bwd) = Wg[e_ % 2], Wu[e_ % 2], Wd[e_ % 2]
                P.dma("pool", lambda e: e.dma_start(out=wg[:], in_=wv(w_gate[e_], 0, 512)), [], [bwg])
                P.dma("pool", lambda e: e.dma_start(out=wu[:], in_=wv(w_up[e_], 0, 512)), [], [bwu])
                P.dma("pool", lambda e: e.dma_start(out=wd[:], in_=wv(w_down[e_], 0, 1024)), [], [bwd])

            for sgi in range(2):
                load_w(0)
                run_pipeline((moe_tile(sgi, tl) for tl in range(16)), NP3)
                for ex in range(NE):
                    if ex + 1 < NE:
                        load_w(ex + 1)
                    (wg, bwg), (wu, bwu), (wd, bwd) = Wg[ex % 2], Wu[ex % 2], Wd[ex % 2]
                    for tg in range(4):
                        heT, bhe = heTs.next()
                        for fc in range(4):
                            g_ps, bgp = gen.next()
                            u_ps, bup = gen.next()
                            P.op("pe", [(lambda e, c=c, fc=fc, tg=tg, g_ps=g_ps, wg=wg: e.matmul(g_ps[:], wg[:, c, fc * 128:(fc + 1) * 128], hmT[:, c, tg * 512:(tg + 1) * 512],
                                                                                                 start=(c == 0), stop=(c == 7))) for c in range(8)],
                                 bhm[tg * 4:tg * 4 + 4] + [bwg], [bgp])
                            P.op("pe", [(lambda e, c=c, fc=fc, tg=tg, u_ps=u_ps, wu=wu: e.matmul(u_ps[:], wu[:, c, fc * 128:(fc + 1) * 128], hmT[:, c, tg * 512:(tg + 1) * 512],
                                                                                                 start=(c == 0), stop=(c == 7))) for c in range(8)],
                                 bhm[tg * 4:tg * 4 + 4] + [bwu], [bup])
                            sg, bsg_ = sgs.next()
                            P.op("act", lambda e, sg=sg, g_ps=g_ps: e.activation(out=sg[:], in_=g_ps[:], func=AF.Silu), [bgp], [bsg_])
                            P.op("dve", lambda e, sg=sg, u_ps=u_ps, heT=heT, fc=fc: e.tensor_tensor(out=heT[:, fc, :], in0=sg[:], in1=u_ps[:], op=ALU.mult),
                                 [bsg_, bup], [bhe[fc]])
                        for j in range(4):
                            tl = tg * 4 + j
                            for nb in range(2):
                                y_ps, byp = gen.next()
                                P.op("pe", [(lambda e, fc=fc, j=j, nb=nb, y_ps=y_ps, heT=heT, wd=wd: e.matmul(y_ps[:], heT[:, fc, j * 128:(j + 1) * 128], wd[:, fc, nb * 512:(nb + 1) * 512],
                                                                                                             start=(fc == 0), stop=(fc == 3))) for fc in range(4)],
                                     bhe + [bwd], [byp])
                                P.op("dve", lambda e, tl=tl, nb=nb, y_ps=y_ps, ex=ex: e.scalar_tensor_tensor(
                                    out=xres[:, tl, nb * 512:(nb + 1) * 512], in0=y_ps[:], scalar=comb[:, tl, ex:ex + 1], in1=xres[:, tl, nb * 512:(nb + 1) * 512],
                                    op0=ALU.mult, op1=ALU.add), [byp, bcomb[tl], bxr[tl]], [bxr[tl]])
                run_pipeline((fin_tile(sgi, tl) for tl in range(16)), 2)
            P.flush()
    return nc


def _rope_tables():
    rows = SEQ // 64
    row_ids = np.repeat(np.arange(rows), 64).astype(np.float32)
    col_ids = np.tile(np.arange(64), rows).astype(np.float32)
    inv = (10000.0 ** (-np.arange(16, dtype=np.float32) / 16)).astype(np.float32)
    ang = np.concatenate([row_ids[:, None] * inv[None, :], col_ids[:, None] * inv[None, :]], axis=-1).astype(np.float32)
    return np.cos(ang).astype(np.float32), np.sin(ang).astype(np.float32)


def make_in_maps(inputs):
    f = lambda a: np.ascontiguousarray(np.asarray(a, dtype=np.float32))
    x = f(inputs["x"])
    mem = f(inputs["mem"])
    cos, sin = _rope_tables()

    def lay(t, n):
        return np.ascontiguousarray(t.reshape(n, 128, 32).transpose(1, 0, 2))

    shared = {
        "ident": np.eye(128, dtype=np.float32),
        "g_mix": f(inputs["g_mix"][0]), "w_in": f(inputs["w_in"][0]), "g_q": f(inputs["g_q"][0]), "g_k": f(inputs["g_k"][0]),
        "w_att_out": f(inputs["w_att_out"][0]), "sgu_ln_g": f(inputs["sgu_ln_g"][0]), "sgu_ln_b": f(inputs["sgu_ln_b"][0]),
        "w_s": f(inputs["w_s"][0]), "b_s": f(inputs["b_s"][0]), "w_sgu_out": f(inputs["w_sgu_out"][0]), "w_out": f(inputs["w_out"][0]),
        "g_cross": f(inputs["g_cross"][0]), "g_mem": f(inputs["g_mem"][0]), "w_cq": f(inputs["w_cq"][0]), "w_ckv": f(inputs["w_ckv"][0]),
        "w_co": f(inputs["w_co"][0]), "g_moe": f(inputs["g_moe"][0]), "w_rg": f(inputs["w_rg"][0]), "b_rg": f(inputs["b_rg"][0]),
        "w_re": f(inputs["w_re"][0]), "b_re": f(inputs["b_re"][0]), "w_gate": f(inputs["w_gate"][0]), "w_up": f(inputs["w_up"][0]),
        "w_down": f(inputs["w_down"][0]), "g_final": f(inputs["g_final"]),
        "cos_seq": lay(cos, 64), "sin_seq": lay(sin, 64),
    }
    maps = []
    for c in range(8):
        b, hf = c // 2, c % 2
        m = dict(shared)
        m["xseq"] = x[b]
        m["xown"] = np.ascontiguousarray(x[b, hf * OWN:(hf + 1) * OWN])
        m["mem"] = mem[b]
        m["cos_own"] = lay(cos[hf * OWN:(hf + 1) * OWN], 32)
        m["sin_own"] = lay(sin[hf * OWN:(hf + 1) * OWN], 32)
        maps.append(m)
    return maps


def kernel(**inputs):
    nc = build()
    maps = make_in_maps(inputs)
    res = run_bass_kernel_spmd(nc, maps, core_ids=list(range(8)))
    outp = np.empty((4, SEQ, D), np.float32)
    for c in range(8):
        b, hf = c // 2, c % 2
        outp[b, hf * OWN:(hf + 1) * OWN] = np.asarray(res.results[c]["out"], dtype=np.float32)
    return outp
```

```python
import contextlib
import os as _os0
import numpy as np
import concourse.bass as bass
import concourse.mybir as mybir
from concourse.bass_utils import run_bass_kernel_spmd

F32 = mybir.dt.float32
BF16 = mybir.dt.bfloat16
AF = mybir.ActivationFunctionType
ALU = mybir.AluOpType
AX = mybir.AxisListType

D = 1024
SEQ = 8192
OWN = 4096
NMEM = 256
EPS = 1e-6
NE = 16
BIG = 1.0e30


class Buf:
    __slots__ = ("name", "w", "r", "psum")

    def __init__(self, name="", psum=False):
        self.name = name
        self.psum = psum
        self.w = None
        self.r = {}


class Eng:
    def __init__(self, name):
        self.name = name
        self.sem = None
        self.count = 0
        self.seen = {}
        self.prog = []
        self.chsems = []
        self.dma_i = 0


class Prog:
    NCH = int(_os0.environ.get('NCH', 8))

    def __init__(self, nc, stack):
        self.nc = nc
        self.E = {}
        for n in ("pe", "act", "dve", "pool", "sp"):
            e = Eng(n)
            e.sem = stack.enter_context(nc.semaphore("sem_" + n))
            self.E[n] = e
        for n in ("sp", "pool", "act"):
            e = self.E[n]
            for i in range(self.NCH):
                e.chsems.append(stack.enter_context(nc.semaphore("ch_%s_%d" % (n, i))))
        self.chan_issued = {}
        import os
        self.limit = int(os.environ.get("OPLIMIT", 10 ** 9))
        self.nops = 0

    def _need(self, need, ev):
        sem, val, src = ev
        k = id(sem)
        if k not in need or need[k][1] < val:
            need[k] = (sem, val, src)

    def _deps(self, eng, reads, writes, extra=()):
        E = self.E[eng]
        need = {}
        for b in reads:
            if b.w is not None:
                self._need(need, b.w)
        for b in writes:
            if b.w is not None:
                self._need(need, b.w)
            for k, (sem, val, src) in b.r.items():
                self._need(need, (sem, val, src))
        for ev in extra:
            self._need(need, ev)
        for k, (sem, val, src) in need.items():
            if src == "pe" and eng == "pe":
                continue
            if E.seen.get(k, 0) >= val:
                continue
            E.seen[k] = val
            E.prog.append(("wait", sem, val))

    def _commit(self, ev, reads, writes):
        sem, val, src = ev
        for b in writes:
            b.w = ev
            b.r = {}
        for b in reads:
            if b not in writes:
                b.r[id(sem)] = (sem, val, src)

    def op(self, eng, fns, reads=(), writes=()):
        if not isinstance(fns, (list, tuple)):
            fns = [fns]
        self.nops += 1
        if self.nops > self.limit:
            return
        pr = [b for b in reads if b.psum]
        if pr:
            reads = [b for b in reads if not b.psum]
            writes = list(writes) + [b for b in pr if b not in writes]
        E = self.E[eng]
        self._deps(eng, reads, writes)
        E.count += 1
        for f in fns[:-1]:
            E.prog.append(("raw", f))
        E.prog.append(("op", fns[-1], E.sem))
        self._commit((E.sem, E.count, eng), reads, writes)

    def dma(self, q, fn, reads=(), writes=()):
        self.nops += 1
        if self.nops > self.limit:
            return
        E = self.E[q]
        ch = E.dma_i % self.NCH
        rnd = E.dma_i // self.NCH
        E.dma_i += 1
        chsem = E.chsems[ch]
        extra = []
        if rnd > 0:
            extra.append((chsem, 16 * rnd, "dma"))
        self._deps(q, reads, writes, extra)
        E.prog.append(("dma", fn, chsem))
        self.chan_issued[id(chsem)] = (chsem, 16 * (rnd + 1))
        self._commit((chsem, 16 * (rnd + 1), "dma"), reads, writes)

    def barrier(self):
        for n, E in self.E.items():
            for m, F in self.E.items():
                if m == n or F.count == 0:
                    continue
                k = id(F.sem)
                if E.seen.get(k, 0) < F.count:
                    E.seen[k] = F.count
                    E.prog.append(("wait", F.sem, F.count))
            for k, (sem, val) in self.chan_issued.items():
                if E.seen.get(k, 0) < val:
                    E.seen[k] = val
                    E.prog.append(("wait", sem, val))

    def flush(self):
        self.barrier()
        nc = self.nc
        progs = {n: E.prog for n, E in self.E.items()}
        for E in self.E.values():
            E.prog = []

        def replay(eng, prog):
            for it in prog:
                if it[0] == "wait":
                    eng.wait_ge(it[1], it[2])
                elif it[0] == "raw":
                    it[1](eng)
                elif it[0] == "op":
                    it[1](eng).then_inc(it[2], 1)
                else:
                    it[1](eng).then_inc(it[2], 16)

        with nc.Block() as block:
            @block.tensor
            def _(e):
                replay(e, progs["pe"])

            @block.scalar
            def _(e):
                replay(e, progs["act"])

            @block.vector
            def _(e):
                replay(e, progs["dve"])

            @block.gpsimd
            def _(e):
                replay(e, progs["pool"])

            @block.sync
            def _(e):
                replay(e, progs["sp"])


class Rot:
    def __init__(self, items):
        self.items = items
        self.i = 0

    def next(self):
        it = self.items[self.i % len(self.items)]
        self.i += 1
        return it


class Pool:
    def __init__(self, items):
        self.free = list(items)

    def acquire(self):
        return self.free.pop(0) if self.free else None

    def release(self, it):
        self.free.append(it)


def build(dbg=False):
    nc = bass.Bass("TRN2", target_bir_lowering=False)

    def din(name, shape):
        return nc.dram_tensor(name, list(shape), F32, kind="ExternalInput").ap()

    xseq = din("xseq", [SEQ, D])
    xown = din("xown", [OWN, D])
    memd = din("mem", [NMEM, D])
    cos_seq = din("cos_seq", [128, 64, 32])
    sin_seq = din("sin_seq", [128, 64, 32])
    cos_own = din("cos_own", [128, 32, 32])
    sin_own = din("sin_own", [128, 32, 32])
    identd = din("ident", [128, 128])
    g_mix = din("g_mix", [D])
    w_in = din("w_in", [D, 3840])
    g_q = din("g_q", [64])
    g_k = din("g_k", [64])
    w_att_out = din("w_att_out", [512, D])
    sgu_ln_g = din("sgu_ln_g", [512])
    sgu_ln_b = din("sgu_ln_b", [512])
    w_s = din("w_s", [4, 128, 128])
    b_s = din("b_s", [4, 128])
    w_sgu_out = din("w_sgu_out", [512, D])
    w_out = din("w_out", [D, D])
    g_cross = din("g_cross", [D])
    g_mem = din("g_mem", [D])
    w_cq = din("w_cq", [D, 512])
    w_ckv = din("w_ckv", [D, D])
    w_co = din("w_co", [512, D])
    g_moe = din("g_moe", [D])
    w_rg = din("w_rg", [D, 4])
    b_rg = din("b_rg", [4])
    w_re = din("w_re", [D, 16])
    b_re = din("b_re", [16])
    w_gate = din("w_gate", [NE, D, 512])
    w_up = din("w_up", [NE, D, 512])
    w_down = din("w_down", [NE, 512, D])
    g_final = din("g_final", [D])
    out = nc.dram_tensor("out", [OWN, D], F32, kind="ExternalOutput").ap()
    yatt_d = nc.dram_tensor("yatt_scr", [8, 128, 8, 512], F32, kind="Internal").ap()
    x2_d = nc.dram_tensor("x2_scr", [OWN, D], F32, kind="Internal").ap()

    def wv(w, c0, c1):
        return w.rearrange("(c p) n -> p c n", p=128)[:, :, c0:c1]

    def gT_view(g):
        return g.rearrange("(c p) -> p c", p=128)

    with contextlib.ExitStack() as top:
        P = Prog(nc, top)
        print("sbuf bytes remaining at start:", nc.sbuf_bytes_remaining)

        def sb(stack, name, shape, dt=F32):
            return stack.enter_context(nc.sbuf_tensor("s_" + name, list(shape), dt))

        def ps(stack, name, shape, dt=F32):
            return stack.enter_context(nc.psum_tensor("p_" + name, list(shape), dt))

        ident = sb(top, "ident", [128, 128], BF16)
        kcT = sb(top, "kcT", [128, 4, NMEM], BF16)
        Vc = sb(top, "Vc", [128, 2, 512], BF16)
        gmixT = sb(top, "gmixT", [128, 8])
        attn_stack = contextlib.ExitStack()
        kT_all = sb(attn_stack, "kT_all", [128, SEQ], BF16)
        V_all = sb(attn_stack, "V_all", [128, 64, 2, 65], BF16)
        B_const = Buf("const")

        P.dma("pool", lambda e: e.dma_start(out=ident[:], in_=identd[:, :]), [], [B_const])
        P.dma("sp", lambda e: e.dma_start(out=gmixT[:], in_=gT_view(g_mix), allow_slow_non_contiguous=True), [], [B_const])
        P.op("pool", lambda e: e.memset(V_all[:], 1.0), [], [B_const])

        def run_pipeline(gens, depth):
            it = iter(gens)
            active = []
            more = True
            while True:
                if more and len(active) < depth:
                    try:
                        active.append(next(it))
                    except StopIteration:
                        more = False
                if not active:
                    break
                for g in list(active):
                    try:
                        next(g)
                    except StopIteration:
                        active.remove(g)

        def drain(g):
            for _ in g:
                pass

        def acq(pool):
            while True:
                it = pool.acquire()
                if it is not None:
                    return it
                yield

        def bfview(bank_ap):
            return bank_ap.bitcast(BF16).rearrange("p (c t) -> p c t", c=8)

        def norm_T_gen(st, xt_ap, xbuf, gT, out_ap3, outbuf, tps, tpsb, pool=None):
            ss, b, _unused = st["sets"].next()
            xb, bxb = st["xb"].next()
            P.op("act", lambda e: e.activation(out=xb[:], in_=xt_ap, func=AF.Square, scale=1.0 / 32,
                                               accum_out=ss[:, 0:1]), [xbuf], [b[0], bxb])
            yield
            P.op("dve", lambda e: e.tensor_scalar(out=ss[:, 1:2], in0=ss[:, 0:1], scalar1=EPS, scalar2=None,
                                                  op0=ALU.add), [b[0]], [b[1]])
            yield
            P.op("act", lambda e: e.activation(out=ss[:, 2:3], in_=ss[:, 1:2], func=AF.Ln), [b[1]], [b[2]])
            yield
            P.op("act", lambda e: e.activation(out=ss[:, 3:4], in_=ss[:, 2:3], func=AF.Exp, scale=-0.5), [b[2]], [b[3]])
            yield
            P.op("act", lambda e: e.activation(out=xb[:], in_=xt_ap, func=AF.Copy, scale=ss[:, 3:4]), [xbuf, b[3]], [bxb])
            yield
            held = None
            if pool is not None:
                while held is None:
                    held = pool.acquire()
                    if held is None:
                        yield
                tps, tpsb = bfview(held[0][:]), held[1]
            P.op("pe", [(lambda e, c=c: e.transpose(tps[:, c, :], xb[:, c * 128:(c + 1) * 128], ident[:])) for c in range(8)],
                 [bxb], [tpsb])
            yield
            P.op("dve", lambda e: e.tensor_tensor(out=out_ap3, in0=tps, in1=gT[:, 0:8].unsqueeze(2).broadcast_to([128, 8, 128]),
                                                  op=ALU.mult), [tpsb], [outbuf])
            if held is not None:
                pool.release(held)
            yield

        def norm_T(st, xt_ap, xbuf, gT, out_ap3, outbuf, tps, tpsb):
            drain(norm_T_gen(st, xt_ap, xbuf, gT, out_ap3, outbuf, tps[:], tpsb))

        def norm_state(stack, pfx, nsets=2, nxb=2):
            st = {}
            st["sets"] = Rot([(sb(stack, pfx + "ss%d" % i, [128, 4]), [Buf("ss") for _ in range(4)], None) for i in range(nsets)])
            st["xb"] = Rot([(sb(stack, pfx + "xb%d" % i, [128, 1024], BF16), Buf("xb")) for i in range(nxb)])
            return st

        def head_norm_rope_gen(ws, src_ps, bsrc, H, g_bc, cosb, sinb, dst, bdst, t):
            w = ws.next()
            n = H * 64
            src3 = src_ps.rearrange("p (h d) -> p h d", h=H)
            P.op("act", lambda e: e.activation(out=w["sq"][:, 0:n], in_=src_ps, func=AF.Square, scale=0.125), [bsrc], [w["bsq"]])
            yield
            P.op("dve", lambda e: e.tensor_reduce(out=w["hs"][:, 0:H], in_=w["sq"][:, 0:n].rearrange("p (h d) -> p h d", h=H),
                                                  axis=AX.X, op=ALU.add), [w["bsq"]], [w["bhs"]])
            yield
            P.op("dve", lambda e: e.tensor_scalar(out=w["hs"][:, 8:8 + H], in0=w["hs"][:, 0:H], scalar1=EPS, scalar2=None, op0=ALU.add),
                 [w["bhs"]], [w["bhs1"]])
            yield
            P.op("act", lambda e: e.activation(out=w["hs"][:, 16:16 + H], in_=w["hs"][:, 8:8 + H], func=AF.Ln), [w["bhs1"]], [w["bhs2"]])
            yield
            P.op("act", lambda e: e.activation(out=w["hs"][:, 24:24 + H], in_=w["hs"][:, 16:16 + H], func=AF.Exp, scale=-0.5),
                 [w["bhs2"]], [w["bhs3"]])
            yield
            qn3 = w["qn"][:, 0:n].rearrange("p (h d) -> p h d", h=H)
            P.op("dve", lambda e: e.tensor_tensor(out=qn3, in0=src3, in1=w["hs"][:, 24:24 + H].unsqueeze(2).broadcast_to([128, H, 64]),
                                                  op=ALU.mult), [bsrc, w["bhs3"]], [w["bqn"]])
            yield
            P.op("dve", lambda e: e.tensor_tensor(out=qn3, in0=qn3, in1=g_bc[:, :].unsqueeze(1).broadcast_to([128, H, 64]),
                                                  op=ALU.mult), [w["bqn"]], [w["bqn"]])
            yield
            qn4 = w["qn"][:, 0:n].rearrange("p (h i t) -> p h i t", h=H, t=2)
            A4 = w["A"][:, 0:n].rearrange("p (h i t) -> p h i t", h=H, t=2)
            B4 = w["B"][:, 0:n].rearrange("p (h i t) -> p h i t", h=H, t=2)
            c4 = cosb[:, t, :].unsqueeze(1).unsqueeze(3).broadcast_to([128, H, 32, 2])
            s4 = sinb[:, t, :].unsqueeze(1).unsqueeze(3).broadcast_to([128, H, 32, 2])
            P.op("dve", lambda e: e.tensor_tensor(out=A4, in0=qn4, in1=c4, op=ALU.mult), [w["bqn"]], [w["bA"]])
            yield
            P.op("dve", lambda e: e.tensor_tensor(out=B4, in0=qn4, in1=s4, op=ALU.mult), [w["bqn"]], [w["bB"]])
            yield
            P.op("dve", lambda e: e.tensor_tensor(out=dst[:, :, :, 0], in0=A4[:, :, :, 0], in1=B4[:, :, :, 1], op=ALU.subtract),
                 [w["bA"], w["bB"]], [bdst])
            yield
            P.op("dve", lambda e: e.tensor_tensor(out=dst[:, :, :, 1], in0=B4[:, :, :, 0], in1=A4[:, :, :, 1], op=ALU.add),
                 [w["bA"], w["bB"]], [bdst])
            yield

        def rope_state(stack, pfx, n, nsets=1):
            sets = []
            for i in range(nsets):
                w = {}
                for k in ("sq", "qn", "A", "B"):
                    w[k] = sb(stack, "%s%s%d" % (pfx, k, i), [128, n])
                w["hs"] = sb(stack, "%shs%d" % (pfx, i), [128, 32])
                for k in ("bsq", "bhs", "bhs1", "bhs2", "bhs3", "bqn", "bA", "bB"):
                    w[k] = Buf(k)
                sets.append(w)
            return Rot(sets)

        with contextlib.ExitStack() as ph:
            wkv = sb(ph, "wkv", [128, 8, 256], BF16)
            cosb = sb(ph, "cosb", [128, 64, 32])
            sinb = sb(ph, "sinb", [128, 64, 32])
            gk_bc = sb(ph, "gk_bc", [128, 64])
            P.dma("pool", lambda e: e.dma_start(out=wkv[:], in_=wv(w_in, 512, 768)), [], [B_const])
            P.dma("sp", lambda e: e.dma_start(out=cosb[:], in_=cos_seq[:, :, :]), [], [B_const])
            P.dma("sp", lambda e: e.dma_start(out=sinb[:], in_=sin_seq[:, :, :]), [], [B_const])
            P.dma("sp", lambda e: e.dma_start(out=gk_bc[:], in_=g_k.partition_broadcast(128)), [], [B_const])
            NP1 = 8
            P.barrier()
            st = norm_state(ph, "p1", nsets=NP1, nxb=NP1)
            rws = rope_state(ph, "p1r", 128, nsets=NP1)
            xts = Rot([(sb(ph, "p1x%d" % i, [128, 1024]), Buf("x")) for i in range(NP1)])
            hTs = Rot([(sb(ph, "p1h%d" % i, [128, 8, 128], BF16), Buf("h")) for i in range(NP1)])
            krs = Rot([(sb(ph, "p1kr%d" % i, [128, 2, 32, 2], BF16), Buf("kr")) for i in range(NP1)])
            banks = Pool([(ps(ph, "p1b%d" % i, [128, 512]), Buf("bank", True)) for i in range(8)])

            import os as _os

            def p1_tile(t):
                xt, bx = xts.next()
                P.dma("sp", lambda e: e.dma_start(out=xt[:], in_=xseq[t * 128:(t + 1) * 128, :]), [], [bx])
                yield
                hT, bh = hTs.next()
                yield from norm_T_gen(st, xt[:], bx, gmixT, hT[:], bh, None, None, pool=banks)
                hkv = yield from acq(banks)
                kv, bkv = hkv
                P.op("pe", [(lambda e, c=c: e.matmul(kv[:, 0:256], hT[:, c, :], wkv[:, c, :], start=(c == 0), stop=(c == 7)))
                            for c in range(8)], [bh, B_const], [bkv])
                yield
                P.op("act", lambda e: e.activation(out=V_all[:, t, :, 0:64], in_=kv[:, 128:256].rearrange("p (h d) -> p h d", h=2),
                                                   func=AF.Copy), [bkv], [])
                yield
                kr, bkr = krs.next()
                yield from head_norm_rope_gen(rws, kv[:, 0:128], bkv, 2, gk_bc, cosb, sinb, kr, bkr, t)
                banks.release(hkv)
                hkt = yield from acq(banks)
                kt, bkt = hkt
                ktv = kt[:].bitcast(BF16)
                P.op("pe", lambda e: e.transpose(ktv[:, 0:128], kr[:].rearrange("p h i t -> p (h i t)"), ident[:]), [bkr], [bkt])
                yield
                P.op("act", lambda e: e.activation(out=kT_all[:, t * 128:(t + 1) * 128], in_=ktv[:, 0:128], func=AF.Copy), [bkt], [])
                banks.release(hkt)
                yield

            run_pipeline((p1_tile(t) for t in range(int(_os.environ.get('NT1', 64)))), int(_os.environ.get('DEPTH1', 8)))
            P.flush()

        if dbg == 1:
            P.limit = 10 ** 9
            P.limit = 10 ** 9
            print("nops", P.nops)
            dk = nc.dram_tensor("dbg_kT", [128, SEQ], BF16, kind="ExternalOutput").ap()
            dv = nc.dram_tensor("dbg_V", [128, 64 * 2 * 65], BF16, kind="ExternalOutput").ap()
            P.dma("sp", lambda e: e.dma_start(out=dk[:, :], in_=kT_all[:]), [], [])
            P.dma("sp", lambda e: e.dma_start(out=dv[:, :], in_=V_all[:].rearrange("p a b c -> p (a b c)")), [], [])
            P.flush()
            attn_stack.close()
            return nc

        with contextlib.ExitStack() as ph:
            wq = sb(ph, "wq", [128, 8, 512], BF16)
            wao = sb(ph, "wao", [64, 8, 1024], BF16)
            cosb = sb(ph, "cosb2", [128, 32, 32])
            sinb = sb(ph, "sinb2", [128, 32, 32])
            gq_bc = sb(ph, "gq_bc", [128, 64])
            ones65 = sb(ph, "ones65", [65, 64])
            B_c2 = Buf("c2")
            for hd_ in range(8):
                pos_ = (hd_ % 4) * 2 + hd_ // 4
                P.dma("pool", lambda e, hd_=hd_, pos_=pos_: e.dma_start(out=wq[:, :, pos_ * 64:(pos_ + 1) * 64], in_=wv(w_in, hd_ * 64, hd_ * 64 + 64)), [], [B_c2])
            P.dma("pool", lambda e: e.dma_start(out=wao[:], in_=w_att_out.rearrange("(h p) n -> p h n", p=64)), [], [B_c2])
            P.dma("sp", lambda e: e.dma_start(out=cosb[:], in_=cos_own[:, :, :]), [], [B_c2])
            P.dma("sp", lambda e: e.dma_start(out=sinb[:], in_=sin_own[:, :, :]), [], [B_c2])
            P.dma("sp", lambda e: e.dma_start(out=gq_bc[:], in_=g_q.partition_broadcast(128)), [], [B_c2])
            P.op("dve", lambda e: e.tensor_scalar(out=gq_bc[:], in0=gq_bc[:], scalar1=0.125, scalar2=None, op0=ALU.mult), [B_c2], [B_c2])
            P.op("pool", lambda e: e.memset(ones65[:], 1.0), [], [B_c2])
            P.barrier()
            st = norm_state(ph, "p2", nsets=4, nxb=4)
            rws = rope_state(ph, "p2r", 512, nsets=3)
            xts = Rot([(sb(ph, "p2x%d" % i, [128, 1024]), Buf("x")) for i in range(4)])
            hTs = Rot([(sb(ph, "p2h%d" % i, [128, 8, 128], BF16), Buf("h")) for i in range(4)])
            qrs = Rot([(sb(ph, "p2qr%d" % i, [128, 8, 32, 2], BF16), Buf("qr")) for i in range(4)])
            qTs = Rot([(sb(ph, "p2qT%d" % i, [128, 4, 512], BF16), [Buf("qT") for _ in range(4)]) for i in range(2)])
            Ps = Rot([(sb(ph, "p2P%d" % i, [128, 2, 512], BF16), Buf("P")) for i in range(4)])
            OnTs = Rot([(sb(ph, "p2OnT%d" % i, [64, 8, 512], BF16), [Buf("OnT") for _ in range(8)]) for i in range(2)])
            ysbs = Rot([(sb(ph, "p2ysb%d" % i, [128, 8, 512]), [Buf("ysb") for _ in range(8)]) for i in range(1)])
            Sps = Rot([(ps(ph, "p2S%d" % i, [128, 2, 512]), Buf("S", True)) for i in range(3)])
            Ops, bO = ps(ph, "p2O", [128, 2, 512]), [Buf("O0", True), Buf("O1", True)]
            LA = int(_os0.environ.get('LA', 2))

            sbank = Pool([(Sps.items[i][0][:, bnk, :], Buf("sbank", True)) for i in range(3) for bnk in range(2)])
            Osbs = Rot([(sb(ph, "p2Osc%d" % i, [65, 512]), Buf("Osb")) for i in range(4)])
            rrs = Rot([(sb(ph, "p2rrc%d" % i, [65, 512]), Buf("rr")) for i in range(4)])

            def q_tile(gi, j, qT, bqT):
                tile = gi * 4 + j
                xt, bx = xts.next()
                P.dma("sp", lambda e: e.dma_start(out=xt[:], in_=xown[tile * 128:(tile + 1) * 128, :]), [], [bx])
                yield
                hT, bh = hTs.next()
                yield from norm_T_gen(st, xt[:], bx, gmixT, hT[:], bh, None, None, pool=sbank)
                hq = yield from acq(sbank)
                qps, bqp = hq
                P.op("pe", [(lambda e, c=c: e.matmul(qps, hT[:, c, :], wq[:, c, :], start=(c == 0), stop=(c == 7)))
                            for c in range(8)], [bh, B_c2], [bqp])
                yield
                qr, bqr = qrs.next()
                yield from head_norm_rope_gen(rws, qps, bqp, 8, gq_bc, cosb, sinb, qr, bqr, tile)
                sbank.release(hq)
                ht = yield from acq(sbank)
                tpv = bfview(ht[0])
                qr3 = qr[:].rearrange("p h i t -> p (h i t)")
                P.op("pe", [(lambda e, g=g: e.transpose(tpv[:, g, :], qr3[:, g * 128:(g + 1) * 128], ident[:])) for g in range(4)],
                     [bqr], [ht[1]])
                yield
                P.op("act", lambda e: e.activation(out=qT[:, :, j * 128:(j + 1) * 128], in_=tpv[:, 0:4, :], func=AF.Copy),
                     [ht[1]], [bqT[j]])
                sbank.release(ht)
                yield

            def y_chunk(gi, dc, OnT, bOn, ysb, bys):
                hm_ = yield from acq(sbank)
                mA, bmA = hm_
                P.op("pe", [(lambda e, h=h: e.matmul(mA, wao[:, h, dc * 128:(dc + 1) * 128], OnT[:, h, :], start=(h == 0), stop=(h == 7)))
                            for h in range(8)], bOn + [B_c2], [bmA])
                yield
                P.op("dve", lambda e: e.tensor_copy(out=ysb[:, dc, :], in_=mA), [bmA], [bys[dc]])
                sbank.release(hm_)
                yield
                if dc == 7:
                    P.dma("sp", lambda e: e.dma_start(out=yatt_d[gi], in_=ysb[:]), bys, [])
                    yield

            spool = Pool(list(Sps.items))

            def epi_rest(items):
                hS = spool.acquire()
                assert hS is not None
                mS, bmS = hS
                for bnk, (Osb, bOsb, rr, brr, OnT, bOn, head) in enumerate(items):
                    P.op("dve", lambda e, Osb=Osb, rr=rr: e.reciprocal(out=rr[64:65, :], in_=Osb[64:65, :]), [bOsb], [brr])
                    P.op("pe", lambda e, rr=rr, bnk=bnk: e.matmul(mS[0:64, bnk, :], ones65[64:65, 0:64], rr[64:65, :], start=True, stop=True), [brr, B_c2], [bmS])
                    P.op("dve", lambda e, Osb=Osb, OnT=OnT, head=head, bnk=bnk: e.tensor_tensor(out=OnT[:, head, :], in0=Osb[0:64, :], in1=mS[0:64, bnk, :], op=ALU.mult),
                         [bOsb, bmS], [bOn[head]])
                spool.release(hS)

            qT, bqT = qTs.next()
            run_pipeline((q_tile(0, j, qT, bqT) for j in range(4)), 4)
            P.barrier()
            for gi in range(8):
                OnT, bOn = OnTs.next()
                pending_epi = None
                for g in range(4):
                    pend = []

                    def try_issue(kt, g=g, qT=qT, bqT=bqT, pend=pend):
                        hS = spool.acquire()
                        if hS is None:
                            return False
                        S, bS = hS
                        P.op("pe", [lambda e: e.matmul(S[:, 0, :], kT_all[0:64, kt * 128:(kt + 1) * 128], qT[0:64, g, :], start=True, stop=True),
                                    lambda e: e.matmul(S[:, 1, :], kT_all[64:128, kt * 128:(kt + 1) * 128], qT[64:128, g, :], start=True, stop=True)],
                             bqT, [bS])
                        pend.append(hS)
                        return True
                    nxt = 0
                    while nxt < LA and try_issue(nxt):
                        nxt += 1
                    if pending_epi is not None:
                        epi_rest(pending_epi)
                        pending_epi = None
                    for kt in range(64):
                        while nxt < 64 and nxt <= kt + LA and try_issue(nxt):
                            nxt += 1
                        hS = pend.pop(0)
                        S, bS = hS
                        Pt, bP = Ps.next()
                        P.op("act", lambda e, S=S, Pt=Pt: e.activation(out=Pt[:], in_=S[:], func=AF.Exp), [bS], [bP])
                        spool.release(hS)
                        P.op("pe", [lambda e, Pt=Pt, kt=kt: e.matmul(Ops[0:65, 0, :], V_all[:, kt, 0, :], Pt[:, 0, :], start=(kt == 0), stop=(kt == 63)),
                                    lambda e, Pt=Pt, kt=kt: e.matmul(Ops[0:65, 1, :], V_all[:, kt, 1, :], Pt[:, 1, :], start=(kt == 0), stop=(kt == 63))],
                             [bP], bO)
                    items = []
                    for hh in range(2):
                        head = g + 4 * hh
                        Osb, bOsb = Osbs.next()
                        rr, brr = rrs.next()
                        P.op("act", lambda e, Osb=Osb, hh=hh: e.activation(out=Osb[:], in_=Ops[0:65, hh, :], func=AF.Copy), [bO[hh]], [bOsb])
                        items.append((Osb, bOsb, rr, brr, OnT, bOn, head))
                    pending_epi = items
                epi_rest(pending_epi)
                P.barrier()
                ysb, bys = ysbs.next()
                gens = [y_chunk(gi, dc, OnT, bOn, ysb, bys) for dc in range(8)]
                if gi + 1 < 8:
                    qT, bqT = qTs.next()
                    gens = [q_tile(gi + 1, j, qT, bqT) for j in range(4)] + gens
                run_pipeline(iter(gens), 6)
                P.barrier()
            P.flush()

        attn_stack.close()
        if dbg == 2:
            dy = nc.dram_tensor("dbg_yatt", [8, 128, 8, 512], F32, kind="ExternalOutput").ap()
            P.dma("sp", lambda e: e.dma_start(out=dy.rearrange("a p c t -> (a p) (c t)"), in_=yatt_d.rearrange("a p c t -> (a p) (c t)")), [], [])
            P.flush()
            return nc

        with contextlib.ExitStack() as ph:
            wckv = sb(ph, "wckv", [128, 8, 1024], BF16)
            gmemT = sb(ph, "gmemT", [128, 8])
            memT = sb(ph, "memT", [128, 8, 256], BF16)
            B_cm = Buf("cm")
            P.dma("pool", lambda e: e.dma_start(out=wckv[:], in_=wv(w_ckv, 0, 1024)), [], [B_cm])
            P.dma("sp", lambda e: e.dma_start(out=gmemT[:], in_=gT_view(g_mem), allow_slow_non_contiguous=True), [], [B_cm])
            st = norm_state(ph, "pm")
            xts = Rot([(sb(ph, "pmx%d" % i, [128, 1024]), Buf("x")) for i in range(2)])
            tp, btp = ps(ph, "pmtp", [128, 8, 128], BF16), Buf("tp", True)
            mps = Rot([(ps(ph, "pmps%d" % i, [128, 512]), Buf("mps", True)) for i in range(2)])
            bmemT = [Buf("memT0"), Buf("memT1")]
            for mt in range(2):
                xt, bx = xts.next()
                P.dma("sp", lambda e, xt=xt, mt=mt: e.dma_start(out=xt[:], in_=memd[mt * 128:(mt + 1) * 128, :]), [], [bx])
                norm_T(st, xt[:], bx, gmemT, memT[:, :, mt * 128:(mt + 1) * 128], bmemT[mt], tp, btp)
            for h in range(4):
                mp, bmp = mps.next()
                P.op("pe", [(lambda e, c=c, h=h, mp=mp: e.matmul(mp[:, 0:256], wckv[:, c, h * 128:(h + 1) * 128], memT[:, c, :], start=(c == 0), stop=(c == 7)))
                            for c in range(8)], bmemT + [B_cm], [bmp])
                P.op("act", lambda e, h=h, mp=mp: e.activation(out=kcT[:, h, :], in_=mp[:, 0:256], func=AF.Copy), [bmp], [])
            for mt in range(2):
                mp, bmp = mps.next()
                P.op("pe", [(lambda e, c=c, mt=mt, mp=mp: e.matmul(mp[:], memT[:, c, mt * 128:(mt + 1) * 128], wckv[:, c, 512:1024], start=(c == 0), stop=(c == 7)))
                            for c in range(8)], bmemT + [B_cm], [bmp])
                P.op("act", lambda e, mt=mt, mp=mp: e.activation(out=Vc[:, mt, :], in_=mp[:], func=AF.Copy), [bmp], [])
            P.flush()

        with contextlib.ExitStack() as ph:
            wsv = sb(ph, "wsv", [128, 8, 512], BF16)
            wsu = sb(ph, "wsu", [128, 8, 512], BF16)
            wga = sb(ph, "wga", [128, 8, 1024], BF16)
            wgb = sb(ph, "wgb", [128, 8, 1024], BF16)
            wso = sb(ph, "wso", [128, 4, 1024], BF16)
            wo = sb(ph, "wo", [128, 8, 1024], BF16)
            wcq = sb(ph, "wcq", [128, 8, 512], BF16)
            wco = sb(ph, "wco", [128, 4, 1024], BF16)
            gcrT = sb(ph, "gcrT", [128, 8])
            lng_bc = sb(ph, "lng_bc", [128, 512])
            lnb_bc = sb(ph, "lnb_bc", [128, 512])
            bs_bc = sb(ph, "bs_bc", [128, 4, 128])
            wsraw = sb(ph, "wsraw", [128, 4, 128], BF16)
            WsT = sb(ph, "WsT", [128, 4, 128], BF16)
            B_c = Buf("c2b")
            B_ws = Buf("ws")
            P.dma("pool", lambda e: e.dma_start(out=wsu[:], in_=wv(w_in, 768, 1280)), [], [B_c])
            P.dma("pool", lambda e: e.dma_start(out=wsv[:], in_=wv(w_in, 1280, 1792)), [], [B_c])
            P.dma("pool", lambda e: e.dma_start(out=wga[:], in_=wv(w_in, 1792, 2816)), [], [B_c])
            P.dma("pool", lambda e: e.dma_start(out=wgb[:], in_=wv(w_in, 2816, 3840)), [], [B_c])
            P.dma("pool", lambda e: e.dma_start(out=wso[:], in_=wv(w_sgu_out, 0, 1024)), [], [B_c])
            P.dma("pool", lambda e: e.dma_start(out=wo[:], in_=wv(w_out, 0, 1024)), [], [B_c])
            P.dma("pool", lambda e: e.dma_start(out=wcq[:], in_=wv(w_cq, 0, 512)), [], [B_c])
            P.dma("pool", lambda e: e.dma_start(out=wco[:], in_=wv(w_co, 0, 1024)), [], [B_c])
            P.dma("pool", lambda e: e.dma_start(out=wsraw[:], in_=w_s.rearrange("g p q -> p g q")), [], [B_ws])
            P.dma("sp", lambda e: e.dma_start(out=gcrT[:], in_=gT_view(g_cross), allow_slow_non_contiguous=True), [], [B_c])
            P.dma("sp", lambda e: e.dma_start(out=lng_bc[:], in_=sgu_ln_g.partition_broadcast(128)), [], [B_c])
            P.dma("sp", lambda e: e.dma_start(out=lnb_bc[:], in_=sgu_ln_b.partition_broadcast(128)), [], [B_c])
            P.dma("sp", lambda e: e.dma_start(out=bs_bc[:].rearrange("p g q -> p (g q)"), in_=b_s.rearrange("g q -> (g q)").partition_broadcast(128)), [], [B_c])
            P.barrier()
            st = norm_state(ph, "p3", nsets=4, nxb=4)
            bpool = Pool([(ps(ph, "p3g%d" % i, [128, 512]), Buf("g", True)) for i in range(8)])
            hb0 = bpool.acquire()
            tpb0 = bfview(hb0[0][:])
            P.op("pe", [(lambda e, g=g: e.transpose(tpb0[:, g, :], wsraw[:, g, :], ident[:])) for g in range(4)], [B_ws], [hb0[1]])
            P.op("act", lambda e: e.activation(out=WsT[:], in_=tpb0[:, 0:4, :], func=AF.Copy), [hb0[1]], [B_c])
            bpool.release(hb0)
            P.barrier()

            xg = [(sb(ph, "p3x%d" % i, [128, 1024]), Buf("x")) for i in range(4)]
            hTg, bhT = sb(ph, "p3hT", [128, 8, 512], BF16), [Buf("hT") for _ in range(4)]
            hcT, bhc = hTg, bhT
            NA = 3
            asets = Pool([dict(gv=sb(ph, "p3gv%d" % i, [128, 512]), vn=sb(ph, "p3vn%d" % i, [128, 512], BF16), lst=sb(ph, "p3lst%d" % i, [128, 16]),
                               bgv=Buf("gv"), bvn=Buf("vn"), bl=[Buf("l") for _ in range(8)]) for i in range(NA)])
            vmb, bvmb = sb(ph, "p3vmb", [128, 4, 512]), [Buf("vmb") for _ in range(4)]
            gus = Pool([(sb(ph, "p3gu%d" % i, [128, 512]), Buf("gu")) for i in range(2)])
            sguT, bsg = sb(ph, "p3sguT", [128, 4, 512], BF16), [Buf("sg") for _ in range(4)]
            csets = Pool([dict(ya=sb(ph, "p3ya%d" % i, [128, 512]), sa=sb(ph, "p3sa%d" % i, [128, 512]), sb=sb(ph, "p3sb%d" % i, [128, 512]),
                               bya=Buf("ya"), bsa=Buf("sa"), bsb=Buf("sb")) for i in range(2)])
            mrg, bmrg = sb(ph, "p3mrg", [128, 8, 512], BF16), [Buf("mrg") for _ in range(8)]
            qcT, bqc = sb(ph, "p3qcT", [128, 4, 512], BF16), [Buf("qc") for _ in range(4)]
            gsets = Pool([dict(cst=sb(ph, "p3cst%d" % i, [128, 16]), pc=sb(ph, "p3pc%d" % i, [128, 4, 256]), pn=sb(ph, "p3pn%d" % i, [128, 4, 256], BF16),
                               pnT=sb(ph, "p3pnT%d" % i, [128, 8, 128], BF16), ocT=sb(ph, "p3ocT%d" % i, [128, 4, 128], BF16),
                               bc=[Buf("c") for _ in range(4)], bpc=Buf("pc"), bpn=Buf("pn"), bpnT=Buf("pnT"), boc=Buf("oc")) for i in range(2)])
            print("sbuf bytes remaining in 2b:", nc.sbuf_bytes_remaining)

            def a_tile(gi, j):
                tile = gi * 4 + j
                xt, bx = xg[j]
                P.dma("sp", lambda e: e.dma_start(out=xt[:], in_=xown[tile * 128:(tile + 1) * 128, :]), [], [bx])
                yield
                yield from norm_T_gen(st, xt[:], bx, gmixT, hTg[:, :, j * 128:(j + 1) * 128], bhT[j], None, None, pool=bpool)
                A = yield from acq(asets)
                gv, vn, lst, bgv, bvn, bl = A["gv"], A["vn"], A["lst"], A["bgv"], A["bvn"], A["bl"]
                h0 = yield from acq(bpool)
                g0, bg0 = h0
                P.op("pe", [(lambda e, c=c: e.matmul(g0[:], hTg[:, c, j * 128:(j + 1) * 128], wsv[:, c, :], start=(c == 0), stop=(c == 7)))
                            for c in range(8)], [bhT[j], B_c], [bg0])
                yield
                P.op("act", lambda e: e.activation(out=gv[:], in_=g0[:], func=AF.Gelu_apprx_tanh), [bg0], [bgv])
                bpool.release(h0)
                yield
                P.op("dve", lambda e: e.tensor_reduce(out=lst[:, 0:1], in_=gv[:], axis=AX.X, op=ALU.add), [bgv], [bl[0]])
                yield
                P.op("act", lambda e: e.activation(out=vn[:], in_=gv[:], func=AF.Square, accum_out=lst[:, 1:2]), [bgv], [bvn, bl[1]])
                yield
                P.op("dve", lambda e: e.tensor_scalar(out=lst[:, 2:3], in0=lst[:, 0:1], scalar1=1.0 / 512, scalar2=None, op0=ALU.mult), [bl[0]], [bl[2]])
                yield
                P.op("dve", lambda e: e.tensor_tensor(out=lst[:, 3:4], in0=lst[:, 2:3], in1=lst[:, 2:3], op=ALU.mult), [bl[2]], [bl[3]])
                yield
                P.op("dve", lambda e: e.scalar_tensor_tensor(out=lst[:, 4:5], in0=lst[:, 1:2], scalar=1.0 / 512, in1=lst[:, 3:4], op0=ALU.mult, op1=ALU.subtract),
                     [bl[1], bl[3]], [bl[4]])
                yield
                P.op("dve", lambda e: e.tensor_scalar(out=lst[:, 5:6], in0=lst[:, 4:5], scalar1=EPS, scalar2=None, op0=ALU.add), [bl[4]], [bl[5]])
                yield
                P.op("act", lambda e: e.activation(out=lst[:, 6:7], in_=lst[:, 5:6], func=AF.Ln), [bl[5]], [bl[6]])
                yield
                P.op("act", lambda e: e.activation(out=lst[:, 7:8], in_=lst[:, 6:7], func=AF.Exp, scale=-0.5), [bl[6]], [bl[7]])
                yield
                P.op("dve", lambda e: e.tensor_scalar(out=gv[:], in0=gv[:], scalar1=lst[:, 2:3], scalar2=lst[:, 7:8], op0=ALU.subtract, op1=ALU.mult),
                     [bgv, bl[2], bl[7]], [bgv])
                yield
                P.op("dve", lambda e: e.tensor_tensor(out=gv[:], in0=gv[:], in1=lng_bc[:], op=ALU.mult), [bgv, B_c], [bgv])
                yield
                P.op("dve", lambda e: e.tensor_tensor(out=vn[:], in0=gv[:], in1=lnb_bc[:], op=ALU.add), [bgv, B_c], [bvn])
                yield
                h1 = yield from acq(bpool)
                g1, bg1 = h1
                P.op("pe", [(lambda e, g=g: e.matmul(g1[:, g * 128:(g + 1) * 128], vn[:, g * 128:(g + 1) * 128], WsT[:, g, :], start=True, stop=True))
                            for g in range(4)], [bvn, B_c], [bg1])
                yield
                P.op("dve", lambda e: e.tensor_tensor(out=vmb[:, :, j * 128:(j + 1) * 128], in0=g1[:].rearrange("p (g q) -> p g q", g=4),
                                                      in1=bs_bc[:], op=ALU.add), [bg1, B_c], [bvmb[j]])
                bpool.release(h1)
                asets.release(A)
                yield

            def b_chunk(g):
                h0 = yield from acq(bpool)
                g0, bg0 = h0
                P.op("pe", [(lambda e, c=c: e.matmul(g0[:], wsu[:, c, g * 128:(g + 1) * 128], hTg[:, c, :], start=(c == 0), stop=(c == 7)))
                            for c in range(8)], bhT + [B_c], [bg0])
                yield
                G = yield from acq(gus)
                gu, bgu = G
                P.op("act", lambda e: e.activation(out=gu[:], in_=g0[:], func=AF.Gelu_apprx_tanh), [bg0], [bgu])
                bpool.release(h0)
                yield
                P.op("dve", lambda e: e.tensor_tensor(out=sguT[:, g, :], in0=gu[:], in1=vmb[:, g, :], op=ALU.mult), [bgu] + bvmb, [bsg[g]])
                gus.release(G)
                yield

            def c_chunk(gi, dc):
                C = yield from acq(csets)
                ya, sa, sbt, bya, bsa, bsb = C["ya"], C["sa"], C["sb"], C["bya"], C["bsa"], C["bsb"]
                P.dma("sp", lambda e: e.dma_start(out=ya[:], in_=yatt_d[gi][:, dc, :]), [], [bya])
                yield
                h0 = yield from acq(bpool)
                ga_ps, bga = h0
                P.op("pe", [(lambda e, c=c: e.matmul(ga_ps[:], wga[:, c, dc * 128:(dc + 1) * 128], hTg[:, c, :], start=(c == 0), stop=(c == 7)))
                            for c in range(8)], bhT + [B_c], [bga])
                yield
                P.op("act", lambda e: e.activation(out=sa[:], in_=ga_ps[:], func=AF.Sigmoid), [bga], [bsa])
                bpool.release(h0)
                yield
                h1 = yield from acq(bpool)
                gb_ps, bgb = h1
                P.op("pe", [(lambda e, c=c: e.matmul(gb_ps[:], wgb[:, c, dc * 128:(dc + 1) * 128], hTg[:, c, :], start=(c == 0), stop=(c == 7)))
                            for c in range(8)], bhT + [B_c], [bgb])
                yield
                P.op("act", lambda e: e.activation(out=sbt[:], in_=gb_ps[:], func=AF.Sigmoid), [bgb], [bsb])
                bpool.release(h1)
                yield
                h2 = yield from acq(bpool)
                ys_ps, bysp = h2
                P.op("pe", [(lambda e, g=g: e.matmul(ys_ps[:], wso[:, g, dc * 128:(dc + 1) * 128], sguT[:, g, :], start=(g == 0), stop=(g == 3)))
                            for g in range(4)], bsg + [B_c], [bysp])
                yield
                P.op("dve", lambda e: e.tensor_tensor(out=sa[:], in0=sa[:], in1=ya[:], op=ALU.mult), [bsa, bya], [bsa])
                yield
                P.op("dve", lambda e: e.tensor_tensor(out=sbt[:], in0=sbt[:], in1=ys_ps[:], op=ALU.mult), [bsb, bysp], [bsb])
                bpool.release(h2)
                yield
                P.op("dve", lambda e: e.tensor_tensor(out=mrg[:, dc, :], in0=sa[:], in1=sbt[:], op=ALU.add), [bsa, bsb], [bmrg[dc]])
                csets.release(C)
                yield

            def d_part(j, nb):
                xt, bx = xg[j]
                h0 = yield from acq(bpool)
                o_ps, bo = h0
                P.op("pe", [(lambda e, dc=dc: e.matmul(o_ps[:], mrg[:, dc, j * 128:(j + 1) * 128], wo[:, dc, nb * 512:(nb + 1) * 512],
                                                      start=(dc == 0), stop=(dc == 7))) for dc in range(8)], bmrg + [B_c], [bo])
                yield
                P.op("dve", lambda e: e.tensor_tensor(out=xt[:, nb * 512:(nb + 1) * 512], in0=xt[:, nb * 512:(nb + 1) * 512], in1=o_ps[:], op=ALU.add),
                     [bo, bx], [bx])
                bpool.release(h0)
                yield

            def e_tile(j):
                xt, bx = xg[j]
                yield from norm_T_gen(st, xt[:], bx, gcrT, hcT[:, :, j * 128:(j + 1) * 128], bhc[j], None, None, pool=bpool)

            def f_head(h):
                h0 = yield from acq(bpool)
                q_ps, bq = h0
                P.op("pe", [(lambda e, c=c: e.matmul(q_ps[:], wcq[:, c, h * 128:(h + 1) * 128], hcT[:, c, :], start=(c == 0), stop=(c == 7)))
                            for c in range(8)], bhc + [B_c], [bq])
                yield
                P.op("act", lambda e: e.activation(out=qcT[:, h, :], in_=q_ps[:], func=AF.Copy, scale=float(128 ** -0.5)), [bq], [bqc[h]])
                bpool.release(h0)
                yield

            def g_tile(gi, j):
                tile = gi * 4 + j
                xt, bx = xg[j]
                Gs = yield from acq(gsets)
                cst, pc, pn, pnT, ocT = Gs["cst"], Gs["pc"], Gs["pn"], Gs["pnT"], Gs["ocT"]
                bc, bpc, bpn, bpnT, boc = Gs["bc"], Gs["bpc"], Gs["bpn"], Gs["bpnT"], Gs["boc"]
                hs0 = yield from acq(bpool)
                hs1 = yield from acq(bpool)
                scs = [hs0[0], hs1[0]]
                bscs = [hs0[1], hs1[1]]
                for half in range(2):
                    P.op("pe", [(lambda e, hh=hh, half=half: e.matmul(scs[half][:, hh * 256:(hh + 1) * 256], qcT[:, half * 2 + hh, j * 128:(j + 1) * 128], kcT[:, half * 2 + hh, :],
                                                                       start=True, stop=True)) for hh in range(2)], bqc, [bscs[half]])
                    yield
                for half in range(2):
                    P.op("dve", lambda e, half=half: e.tensor_reduce(out=cst[:, half * 2:half * 2 + 2], in_=scs[half][:].rearrange("p (h m) -> p h m", h=2), axis=AX.X, op=ALU.max),
                         [bscs[half]], [bc[0]])
                    yield
                P.op("dve", lambda e: e.tensor_scalar(out=cst[:, 4:8], in0=cst[:, 0:4], scalar1=-1.0, scalar2=None, op0=ALU.mult), [bc[0]], [bc[1]])
                yield
                for half in range(2):
                    P.op("act", [(lambda e, hh=hh, half=half: e.activation(out=pc[:, half * 2 + hh, :], in_=scs[half][:, hh * 256:(hh + 1) * 256], func=AF.Exp,
                                                                          bias=cst[:, 4 + half * 2 + hh:5 + half * 2 + hh], accum_out=cst[:, 8 + half * 2 + hh:9 + half * 2 + hh]))
                                 for hh in range(2)], [bscs[half], bc[1]], [bpc, bc[2]])
                    yield
                bpool.release(hs0)
                bpool.release(hs1)
                P.op("dve", lambda e: e.reciprocal(out=cst[:, 12:16], in_=cst[:, 8:12]), [bc[2]], [bc[3]])
                yield
                P.op("dve", lambda e: e.tensor_tensor(out=pn[:], in0=pc[:], in1=cst[:, 12:16].unsqueeze(2).broadcast_to([128, 4, 256]), op=ALU.mult),
                     [bpc, bc[3]], [bpn])
                yield
                ht = yield from acq(bpool)
                tpv = bfview(ht[0][:])
                P.op("pe", [(lambda e, k=k: e.transpose(tpv[:, k, :], pn[:, k // 2, (k % 2) * 128:(k % 2 + 1) * 128], ident[:])) for k in range(8)], [bpn], [ht[1]])
                yield
                P.op("act", lambda e: e.activation(out=pnT[:], in_=tpv, func=AF.Copy), [ht[1]], [bpnT])
                bpool.release(ht)
                yield
                ho = yield from acq(bpool)
                oc_ps, bocp = ho
                fl = []
                for h in range(4):
                    for mt in range(2):
                        fl.append(lambda e, h=h, mt=mt: e.matmul(oc_ps[:, h * 128:(h + 1) * 128], Vc[:, mt, h * 128:(h + 1) * 128], pnT[:, h * 2 + mt, :],
                                                                 start=(mt == 0), stop=(mt == 1)))
                P.op("pe", fl, [bpnT], [bocp])
                yield
                P.op("act", lambda e: e.activation(out=ocT[:], in_=oc_ps[:].rearrange("p (h t) -> p h t", h=4), func=AF.Copy), [bocp], [boc])
                bpool.release(ho)
                yield
                for nb in range(2):
                    hy = yield from acq(bpool)
                    y_ps, byp = hy
                    P.op("pe", [(lambda e, h=h, nb=nb, y_ps=y_ps: e.matmul(y_ps[:], ocT[:, h, :], wco[:, h, nb * 512:(nb + 1) * 512], start=(h == 0), stop=(h == 3)))
                                for h in range(4)], [boc, B_c], [byp])
                    yield
                    P.op("dve", lambda e, nb=nb, y_ps=y_ps: e.tensor_tensor(out=xt[:, nb * 512:(nb + 1) * 512], in0=xt[:, nb * 512:(nb + 1) * 512], in1=y_ps[:], op=ALU.add),
                         [byp, bx], [bx])
                    bpool.release(hy)
                    yield
                gsets.release(Gs)
                P.dma("sp", lambda e: e.dma_start(out=x2_d[tile * 128:(tile + 1) * 128, :], in_=xt[:]), [bx], [])
                yield

            for gi in range(8):
                run_pipeline((a_tile(gi, j) for j in range(4)), 4)
                run_pipeline((b_chunk(g) for g in range(4)), 3)
                run_pipeline((c_chunk(gi, dc) for dc in range(8)), 3)
                run_pipeline((d_part(j, nb) for j in range(4) for nb in range(2)), 4)
                run_pipeline((e_tile(j) for j in range(4)), 4)
                run_pipeline((f_head(h) for h in range(4)), 4)
                run_pipeline((g_tile(gi, j) for j in range(4)), 3)
            P.flush()

        if dbg == 3:
            dx = nc.dram_tensor("dbg_x2", [OWN, D], F32, kind="ExternalOutput").ap()
            P.dma("sp", lambda e: e.dma_start(out=dx[:, :], in_=x2_d[:, :]), [], [])
            P.flush()
            return nc

        with contextlib.ExitStack() as ph:
            gmoT = sb(ph, "gmoT", [128, 8])
            wr = sb(ph, "wr", [128, 8, 20], BF16)
            br_bc = sb(ph, "br_bc", [128, 20])
            gfin_bc = sb(ph, "gfin_bc", [128, 1024])
            B_c = Buf("c3")
            P.dma("sp", lambda e: e.dma_start(out=gmoT[:], in_=gT_view(g_moe), allow_slow_non_contiguous=True), [], [B_c])
            P.dma("pool", lambda e: e.dma_start(out=wr[:, :, 0:4], in_=wv(w_rg, 0, 4)), [], [B_c])
            P.dma("pool", lambda e: e.dma_start(out=wr[:, :, 4:20], in_=wv(w_re, 0, 16)), [], [B_c])
            P.dma("sp", lambda e: e.dma_start(out=br_bc[:, 0:4], in_=b_rg.partition_broadcast(128)), [], [B_c])
            P.dma("sp", lambda e: e.dma_start(out=br_bc[:, 4:20], in_=b_re.partition_broadcast(128)), [], [B_c])
            P.dma("sp", lambda e: e.dma_start(out=gfin_bc[:], in_=g_final.partition_broadcast(128)), [], [B_c])
            P.barrier()
            NP3 = 6
            st = norm_state(ph, "p4", nsets=NP3, nxb=NP3)
            xres, bxr = sb(ph, "xres", [128, 16, 1024]), [Buf("xr%d" % i) for i in range(16)]
            hmT, bhm = sb(ph, "hmT", [128, 8, 2048], BF16), [Buf("hm%d" % i) for i in range(16)]
            comb, bcomb = sb(ph, "comb", [128, 16, 16]), [Buf("cb%d" % i) for i in range(16)]
            Wg = [(sb(ph, "Wg%d" % i, [128, 8, 512], BF16), Buf("Wg")) for i in range(2)]
            Wu = [(sb(ph, "Wu%d" % i, [128, 8, 512], BF16), Buf("Wu")) for i in range(2)]
            Wd = [(sb(ph, "Wd%d" % i, [128, 4, 1024], BF16), Buf("Wd")) for i in range(2)]
            sgs = Rot([(sb(ph, "sg%d" % i, [128, 512]), Buf("sg")) for i in range(2)])
            heTs = Rot([(sb(ph, "heT%d" % i, [128, 4, 512], BF16), [Buf("he") for _ in range(4)]) for i in range(2)])
            ots = Rot([(sb(ph, "ot%d" % i, [128, 1024]), Buf("ot")) for i in range(2)])
            rts = Rot([(sb(ph, "rt%d" % i, [128, 160]), [Buf("rt") for _ in range(24)]) for i in range(NP3)])
            allbanks = [(ps(ph, "p4g%d" % i, [128, 512]), Buf("g", True)) for i in range(8)]
            bpool = Pool(allbanks)
            gen = Rot(allbanks)

            def moe_tile(sgi, tl):
                tile = sgi * 16 + tl
                P.dma("sp", lambda e: e.dma_start(out=xres[:, tl, :], in_=x2_d[tile * 128:(tile + 1) * 128, :]), [], [bxr[tl]])
                yield
                yield from norm_T_gen(st, xres[:, tl, :], bxr[tl], gmoT, hmT[:, :, tl * 128:(tl + 1) * 128], bhm[tl], None, None, pool=bpool)
                hb = yield from acq(bpool)
                r_ps, brp = hb
                P.op("pe", [(lambda e, c=c: e.matmul(r_ps[:, 0:20], hmT[:, c, tl * 128:(tl + 1) * 128], wr[:, c, :], start=(c == 0), stop=(c == 7)))
                            for c in range(8)], [bhm[tl], B_c], [brp])
                yield
                R, brt = rts.next()
                P.op("dve", lambda e: e.tensor_tensor(out=R[:, 0:20], in0=r_ps[:, 0:20], in1=br_bc[:], op=ALU.add), [brp, B_c], [brt[0]])
                bpool.release(hb)
                yield
                P.op("dve", lambda e: e.tensor_reduce(out=R[:, 20:21], in_=R[:, 0:4], axis=AX.X, op=ALU.max), [brt[0]], [brt[1]])
                yield
                P.op("dve", lambda e: e.tensor_scalar(out=R[:, 21:22], in0=R[:, 20:21], scalar1=-1.0, scalar2=None, op0=ALU.mult), [brt[1]], [brt[2]])
                yield
                P.op("act", lambda e: e.activation(out=R[:, 22:26], in_=R[:, 0:4], func=AF.Exp, bias=R[:, 21:22], accum_out=R[:, 26:27]), [brt[0], brt[2]], [brt[3]])
                yield
                P.op("dve", lambda e: e.reciprocal(out=R[:, 27:28], in_=R[:, 26:27]), [brt[3]], [brt[4]])
                yield
                P.op("dve", lambda e: e.tensor_scalar(out=R[:, 28:32], in0=R[:, 0:4], scalar1=R[:, 20:21], scalar2=None, op0=ALU.is_ge), [brt[0], brt[1]], [brt[5]])
                yield
                P.op("dve", lambda e: e.tensor_scalar(out=R[:, 32:36], in0=R[:, 28:32], scalar1=BIG, scalar2=-BIG, op0=ALU.mult, op1=ALU.add), [brt[5]], [brt[6]])
                yield
                P.op("dve", lambda e: e.tensor_tensor(out=R[:, 40:56].rearrange("p (g k) -> p g k", g=4), in0=R[:, 4:20].rearrange("p (g k) -> p g k", g=4),
                                                      in1=R[:, 32:36].unsqueeze(2).broadcast_to([128, 4, 4]), op=ALU.add), [brt[0], brt[6]], [brt[7]])
                yield
                P.op("dve", lambda e: e.tensor_reduce(out=R[:, 56:57], in_=R[:, 40:56], axis=AX.X, op=ALU.max), [brt[7]], [brt[8]])
                yield
                P.op("dve", lambda e: e.tensor_scalar(out=R[:, 60:76], in0=R[:, 40:56], scalar1=R[:, 56:57], scalar2=None, op0=ALU.is_ge), [brt[7], brt[8]], [brt[9]])
                yield
                P.op("dve", lambda e: e.scalar_tensor_tensor(out=R[:, 80:96], in0=R[:, 60:76], scalar=-BIG, in1=R[:, 40:56], op0=ALU.mult, op1=ALU.add),
                     [brt[9], brt[7]], [brt[10]])
                yield
                P.op("dve", lambda e: e.tensor_reduce(out=R[:, 96:97], in_=R[:, 80:96], axis=AX.X, op=ALU.max), [brt[10]], [brt[11]])
                yield
                P.op("dve", lambda e: e.tensor_scalar(out=R[:, 100:116], in0=R[:, 80:96], scalar1=R[:, 96:97], scalar2=None, op0=ALU.is_ge), [brt[10], brt[11]], [brt[12]])
                yield
                P.op("dve", lambda e: e.tensor_tensor(out=R[:, 116:117], in0=R[:, 96:97], in1=R[:, 56:57], op=ALU.subtract), [brt[11], brt[8]], [brt[13]])
                yield
                P.op("act", lambda e: e.activation(out=R[:, 117:118], in_=R[:, 116:117], func=AF.Exp), [brt[13]], [brt[14]])
                yield
                P.op("dve", lambda e: e.tensor_scalar(out=R[:, 118:119], in0=R[:, 117:118], scalar1=1.0, scalar2=None, op0=ALU.add), [brt[14]], [brt[15]])
                yield
                P.op("dve", lambda e: e.reciprocal(out=R[:, 119:120], in_=R[:, 118:119]), [brt[15]], [brt[16]])
                yield
                P.op("dve", lambda e: e.tensor_tensor(out=R[:, 120:121], in0=R[:, 119:120], in1=R[:, 27:28], op=ALU.mult), [brt[16], brt[4]], [brt[17]])
                yield
                P.op("dve", lambda e: e.tensor_tensor(out=R[:, 121:122], in0=R[:, 120:121], in1=R[:, 117:118], op=ALU.mult), [brt[17], brt[14]], [brt[18]])
                yield
                P.op("dve", lambda e: e.tensor_scalar(out=comb[:, tl, :], in0=R[:, 60:76], scalar1=R[:, 120:121], scalar2=None, op0=ALU.mult),
                     [brt[9], brt[17]], [bcomb[tl]])
                yield
                P.op("dve", lambda e: e.scalar_tensor_tensor(out=comb[:, tl, :], in0=R[:, 100:116], scalar=R[:, 121:122], in1=comb[:, tl, :], op0=ALU.mult, op1=ALU.add),
                     [brt[12], brt[18], bcomb[tl]], [bcomb[tl]])
                yield

            def fin_tile(sgi, tl):
                tile = sgi * 16 + tl
                fss, fb, _u = st["sets"].next()
                ot, bot = ots.next()
                P.op("act", lambda e: e.activation(out=ot[:], in_=xres[:, tl, :], func=AF.Square, scale=1.0 / 32, accum_out=fss[:, 0:1]), [bxr[tl]], [fb[0], bot])
                yield
                P.op("dve", lambda e: e.tensor_scalar(out=fss[:, 1:2], in0=fss[:, 0:1], scalar1=EPS, scalar2=None, op0=ALU.add), [fb[0]], [fb[1]])
                yield
                P.op("act", lambda e: e.activation(out=fss[:, 2:3], in_=fss[:, 1:2], func=AF.Ln), [fb[1]], [fb[2]])
                yield
                P.op("act", lambda e: e.activation(out=fss[:, 3:4], in_=fss[:, 2:3], func=AF.Exp, scale=-0.5), [fb[2]], [fb[3]])
                yield
                P.op("dve", lambda e: e.scalar_tensor_tensor(out=ot[:], in0=xres[:, tl, :], scalar=fss[:, 3:4], in1=gfin_bc[:], op0=ALU.mult, op1=ALU.mult),
                     [bxr[tl], fb[3], B_c], [bot])
                yield
                P.dma("sp", lambda e: e.dma_start(out=out[tile * 128:(tile + 1) * 128, :], in_=ot[:]), [bot], [])
                yield

            def load_w(e_):
                (wg, bwg), (wu, bwu), (wd, bwd) = Wg[e_ % 2], Wu[e_ % 2], Wd[e_ % 2]
                P.dma("pool", lambda e: e.dma_start(out=wg[:], in_=wv(w_gate[e_], 0, 512)), [], [bwg])
                P.dma("pool", lambda e: e.dma_start(out=wu[:], in_=wv(w_up[e_], 0, 512)), [], [bwu])
                P.dma("pool", lambda e: e.dma_start(out=wd[:], in_=wv(w_down[e_], 0, 1024)), [], [bwd])

            for sgi in range(2):
                load_w(0)
                run_pipeline((moe_tile(sgi, tl) for tl in range(16)), NP3)
                for ex in range(NE):
                    if ex + 1 < NE:
                        load_w(ex + 1)
                    (wg, bwg), (wu, bwu), (wd, bwd) = Wg[ex % 2], Wu[ex % 2], Wd[ex % 2]
                    for tg in range(4):
                        heT, bhe = heTs.next()
                        for fc in range(4):
                            g_ps, bgp = gen.next()
                            u_ps, bup = gen.next()
                            P.op("pe", [(lambda e, c=c, fc=fc, tg=tg, g_ps=g_ps, wg=wg: e.matmul(g_ps[:], wg[:, c, fc * 128:(fc + 1) * 128], hmT[:, c, tg * 512:(tg + 1) * 512],
                                                                                                 start=(c == 0), stop=(c == 7))) for c in range(8)],
                                 bhm[tg * 4:tg * 4 + 4] + [bwg], [bgp])
                            P.op("pe", [(lambda e, c=c, fc=fc, tg=tg, u_ps=u_ps, wu=wu: e.matmul(u_ps[:], wu[:, c, fc * 128:(fc + 1) * 128], hmT[:, c, tg * 512:(tg + 1) * 512],
                                                                                                 start=(c == 0), stop=(c == 7))) for c in range(8)],
                                 bhm[tg * 4:tg * 4 + 4] + [bwu], [bup])
                            sg, bsg_ = sgs.next()
                            P.op("act", lambda e, sg=sg, g_ps=g_ps: e.activation(out=sg[:], in_=g_ps[:], func=AF.Silu), [bgp], [bsg_])
                            P.op("dve", lambda e, sg=sg, u_ps=u_ps, heT=heT, fc=fc: e.tensor_tensor(out=heT[:, fc, :], in0=sg[:], in1=u_ps[:], op=ALU.mult),
                                 [bsg_, bup], [bhe[fc]])
                        for j in range(4):
                            tl = tg * 4 + j
                            for nb in range(2):
                                y_ps, byp = gen.next()
                                P.op("pe", [(lambda e, fc=fc, j=j, nb=nb, y_ps=y_ps, heT=heT, wd=wd: e.matmul(y_ps[:], heT[:, fc, j * 128:(j + 1) * 128], wd[:, fc, nb * 512:(nb + 1) * 512],
                                                                                                             start=(fc == 0), stop=(fc == 3))) for fc in range(4)],
                                     bhe + [bwd], [byp])
                                P.op("dve", lambda e, tl=tl, nb=nb, y_ps=y_ps, ex=ex: e.scalar_tensor_tensor(
                                    out=xres[:, tl, nb * 512:(nb + 1) * 512], in0=y_ps[:], scalar=comb[:, tl, ex:ex + 1], in1=xres[:, tl, nb * 512:(nb + 1) * 512],
                                    op0=ALU.mult, op1=ALU.add), [byp, bcomb[tl], bxr[tl]], [bxr[tl]])
                run_pipeline((fin_tile(sgi, tl) for tl in range(16)), 2)
            P.flush()
    return nc


def _rope_tables():
    rows = SEQ // 64
    row_ids = np.repeat(np.arange(rows), 64).astype(np.float32)
    col_ids = np.tile(np.arange(64), rows).astype(np.float32)
    inv = (10000.0 ** (-np.arange(16, dtype=np.float32) / 16)).astype(np.float32)
    ang = np.concatenate([row_ids[:, None] * inv[None, :], col_ids[:, None] * inv[None, :]], axis=-1).astype(np.float32)
    return np.cos(ang).astype(np.float32), np.sin(ang).astype(np.float32)


def make_in_maps(inputs):
    f = lambda a: np.ascontiguousarray(np.asarray(a, dtype=np.float32))
    x = f(inputs["x"])
    mem = f(inputs["mem"])
    cos, sin = _rope_tables()

    def lay(t, n):
        return np.ascontiguousarray(t.reshape(n, 128, 32).transpose(1, 0, 2))

    shared = {
        "ident": np.eye(128, dtype=np.float32),
        "g_mix": f(inputs["g_mix"][0]), "w_in": f(inputs["w_in"][0]), "g_q": f(inputs["g_q"][0]), "g_k": f(inputs["g_k"][0]),
        "w_att_out": f(inputs["w_att_out"][0]), "sgu_ln_g": f(inputs["sgu_ln_g"][0]), "sgu_ln_b": f(inputs["sgu_ln_b"][0]),
        "w_s": f(inputs["w_s"][0]), "b_s": f(inputs["b_s"][0]), "w_sgu_out": f(inputs["w_sgu_out"][0]), "w_out": f(inputs["w_out"][0]),
        "g_cross": f(inputs["g_cross"][0]), "g_mem": f(inputs["g_mem"][0]), "w_cq": f(inputs["w_cq"][0]), "w_ckv": f(inputs["w_ckv"][0]),
        "w_co": f(inputs["w_co"][0]), "g_moe": f(inputs["g_moe"][0]), "w_rg": f(inputs["w_rg"][0]), "b_rg": f(inputs["b_rg"][0]),
        "w_re": f(inputs["w_re"][0]), "b_re": f(inputs["b_re"][0]), "w_gate": f(inputs["w_gate"][0]), "w_up": f(inputs["w_up"][0]),
        "w_down": f(inputs["w_down"][0]), "g_final": f(inputs["g_final"]),
        "cos_seq": lay(cos, 64), "sin_seq": lay(sin, 64),
    }
    maps = []
    for c in range(8):
        b, hf = c // 2, c % 2
        m = dict(shared)
        m["xseq"] = x[b]
        m["xown"] = np.ascontiguousarray(x[b, hf * OWN:(hf + 1) * OWN])
        m["mem"] = mem[b]
        m["cos_own"] = lay(cos[hf * OWN:(hf + 1) * OWN], 32)
        m["sin_own"] = lay(sin[hf * OWN:(hf + 1) * OWN], 32)
        maps.append(m)
    return maps


def kernel(**inputs):
    nc = build()
    maps = make_in_maps(inputs)
    res = run_bass_kernel_spmd(nc, maps, core_ids=list(range(8)))
    outp = np.empty((4, SEQ, D), np.float32)
    for c in range(8):
        b, hf = c // 2, c % 2
        outp[b, hf * OWN:(hf + 1) * OWN] = np.asarray(res.results[c]["out"], dtype=np.float32)
    return outp
```

```python
import contextlib
import os as _os0
import numpy as np
import concourse.bass as bass
import concourse.mybir as mybir
from concourse.bass_utils import run_bass_kernel_spmd

F32 = mybir.dt.float32
BF16 = mybir.dt.bfloat16
AF = mybir.ActivationFunctionType
ALU = mybir.AluOpType
AX = mybir.AxisListType

D = 1024
SEQ = 8192
OWN = 4096
NMEM = 256
EPS = 1e-6
NE = 16
BIG = 1.0e30


class Buf:
    __slots__ = ("name", "w", "r", "psum")

    def __init__(self, name="", psum=False):
        self.name = name
        self.psum = psum
        self.w = None
        self.r = {}


class Eng:
    def __init__(self, name):
        self.name = name
        self.sem = None
        self.count = 0
        self.seen = {}
        self.prog = []
        self.chsems = []
        self.dma_i = 0


class Prog:
    NCH = int(_os0.environ.get('NCH', 8))

    def __init__(self, nc, stack):
        self.nc = nc
        self.E = {}
        for n in ("pe", "act", "dve", "pool", "sp"):
            e = Eng(n)
            e.sem = stack.enter_context(nc.semaphore("sem_" + n))
            self.E[n] = e
        for n in ("sp", "pool", "act"):
            e = self.E[n]
            for i in range(self.NCH):
                e.chsems.append(stack.enter_context(nc.semaphore("ch_%s_%d" % (n, i))))
        self.chan_issued = {}
        import os
        self.limit = int(os.environ.get("OPLIMIT", 10 ** 9))
        self.nops = 0

    def _need(self, need, ev):
        sem, val, src = ev
        k = id(sem)
        if k not in need or need[k][1] < val:
            need[k] = (sem, val, src)

    def _deps(self, eng, reads, writes, extra=()):
        E = self.E[eng]
        need = {}
        for b in reads:
            if b.w is not None:
                self._need(need, b.w)
        for b in writes:
            if b.w is not None:
                self._need(need, b.w)
            for k, (sem, val, src) in b.r.items():
                self._need(need, (sem, val, src))
        for ev in extra:
            self._need(need, ev)
        for k, (sem, val, src) in need.items():
            if src == "pe" and eng == "pe":
                continue
            if E.seen.get(k, 0) >= val:
                continue
            E.seen[k] = val
            E.prog.append(("wait", sem, val))

    def _commit(self, ev, reads, writes):
        sem, val, src = ev
        for b in writes:
            b.w = ev
            b.r = {}
        for b in reads:
            if b not in writes:
                b.r[id(sem)] = (sem, val, src)

    def op(self, eng, fns, reads=(), writes=()):
        if not isinstance(fns, (list, tuple)):
            fns = [fns]
        self.nops += 1
        if self.nops > self.limit:
            return
        pr = [b for b in reads if b.psum]
        if pr:
            reads = [b for b in reads if not b.psum]
            writes = list(writes) + [b for b in pr if b not in writes]
        E = self.E[eng]
        self._deps(eng, reads, writes)
        E.count += 1
        for f in fns[:-1]:
            E.prog.append(("raw", f))
        E.prog.append(("op", fns[-1], E.sem))
        self._commit((E.sem, E.count, eng), reads, writes)

    def dma(self, q, fn, reads=(), writes=()):
        self.nops += 1
        if self.nops > self.limit:
            return
        E = self.E[q]
        ch = E.dma_i % self.NCH
        rnd = E.dma_i // self.NCH
        E.dma_i += 1
        chsem = E.chsems[ch]
        extra = []
        if rnd > 0:
            extra.append((chsem, 16 * rnd, "dma"))
        self._deps(q, reads, writes, extra)
        E.prog.append(("dma", fn, chsem))
        self.chan_issued[id(chsem)] = (chsem, 16 * (rnd + 1))
        self._commit((chsem, 16 * (rnd + 1), "dma"), reads, writes)

    def barrier(self):
        for n, E in self.E.items():
            for m, F in self.E.items():
                if m == n or F.count == 0:
                    continue
                k = id(F.sem)
                if E.seen.get(k, 0) < F.count:
                    E.seen[k] = F.count
                    E.prog.append(("wait", F.sem, F.count))
            for k, (sem, val) in self.chan_issued.items():
                if E.seen.get(k, 0) < val:
                    E.seen[k] = val
                    E.prog.append(("wait", sem, val))

    def flush(self):
        self.barrier()
        nc = self.nc
        progs = {n: E.prog for n, E in self.E.items()}
        for E in self.E.values():
            E.prog = []

        def replay(eng, prog):
            for it in prog:
                if it[0] == "wait":
                    eng.wait_ge(it[1], it[2])
                elif it[0] == "raw":
                    it[1](eng)
                elif it[0] == "op":
                    it[1](eng).then_inc(it[2], 1)
                else:
                    it[1](eng).then_inc(it[2], 16)

        with nc.Block() as block:
            @block.tensor
            def _(e):
                replay(e, progs["pe"])

            @block.scalar
            def _(e):
                replay(e, progs["act"])

            @block.vector
            def _(e):
                replay(e, progs["dve"])

            @block.gpsimd
            def _(e):
                replay(e, progs["pool"])

            @block.sync
            def _(e):
                replay(e, progs["sp"])


class Rot:
    def __init__(self, items):
        self.items = items
        self.i = 0

    def next(self):
        it = self.items[self.i % len(self.items)]
        self.i += 1
        return it


class Pool:
    def __init__(self, items):
        self.free = list(items)

    def acquire(self):
        return self.free.pop(0) if self.free else None

    def release(self, it):
        self.free.append(it)


def build(dbg=False):
    nc = bass.Bass("TRN2", target_bir_lowering=False)

    def din(name, shape):
        return nc.dram_tensor(name, list(shape), F32, kind="ExternalInput").ap()

    xseq = din("xseq", [SEQ, D])
    xown = din("xown", [OWN, D])
    memd = din("mem", [NMEM, D])
    cos_seq = din("cos_seq", [128, 64, 32])
    sin_seq = din("sin_seq", [128, 64, 32])
    cos_own = din("cos_own", [128, 32, 32])
    sin_own = din("sin_own", [128, 32, 32])
    identd = din("ident", [128, 128])
    g_mix = din("g_mix", [D])
    w_in = din("w_in", [D, 3840])
    g_q = din("g_q", [64])
    g_k = din("g_k", [64])
    w_att_out = din("w_att_out", [512, D])
    sgu_ln_g = din("sgu_ln_g", [512])
    sgu_ln_b = din("sgu_ln_b", [512])
    w_s = din("w_s", [4, 128, 128])
    b_s = din("b_s", [4, 128])
    w_sgu_out = din("w_sgu_out", [512, D])
    w_out = din("w_out", [D, D])
    g_cross = din("g_cross", [D])
    g_mem = din("g_mem", [D])
    w_cq = din("w_cq", [D, 512])
    w_ckv = din("w_ckv", [D, D])
    w_co = din("w_co", [512, D])
    g_moe = din("g_moe", [D])
    w_rg = din("w_rg", [D, 4])
    b_rg = din("b_rg", [4])
    w_re = din("w_re", [D, 16])
    b_re = din("b_re", [16])
    w_gate = din("w_gate", [NE, D, 512])
    w_up = din("w_up", [NE, D, 512])
    w_down = din("w_down", [NE, 512, D])
    g_final = din("g_final", [D])
    out = nc.dram_tensor("out", [OWN, D], F32, kind="ExternalOutput").ap()
    yatt_d = nc.dram_tensor("yatt_scr", [8, 128, 8, 512], F32, kind="Internal").ap()
    x2_d = nc.dram_tensor("x2_scr", [OWN, D], F32, kind="Internal").ap()

    def wv(w, c0, c1):
        return w.rearrange("(c p) n -> p c n", p=128)[:, :, c0:c1]

    def gT_view(g):
        return g.rearrange("(c p) -> p c", p=128)

    with contextlib.ExitStack() as top:
        P = Prog(nc, top)
        print("sbuf bytes remaining at start:", nc.sbuf_bytes_remaining)

        def sb(stack, name, shape, dt=F32):
            return stack.enter_context(nc.sbuf_tensor("s_" + name, list(shape), dt))

        def ps(stack, name, shape, dt=F32):
            return stack.enter_context(nc.psum_tensor("p_" + name, list(shape), dt))

        ident = sb(top, "ident", [128, 128], BF16)
        kcT = sb(top, "kcT", [128, 4, NMEM], BF16)
        Vc = sb(top, "Vc", [128, 2, 512], BF16)
        gmixT = sb(top, "gmixT", [128, 8])
        attn_stack = contextlib.ExitStack()
        kT_all = sb(attn_stack, "kT_all", [128, SEQ], BF16)
        V_all = sb(attn_stack, "V_all", [128, 64, 2, 65], BF16)
        B_const = Buf("const")

        P.dma("pool", lambda e: e.dma_start(out=ident[:], in_=identd[:, :]), [], [B_const])
        P.dma("sp", lambda e: e.dma_start(out=gmixT[:], in_=gT_view(g_mix), allow_slow_non_contiguous=True), [], [B_const])
        P.op("pool", lambda e: e.memset(V_all[:], 1.0), [], [B_const])

        def run_pipeline(gens, depth):
            it = iter(gens)
            active = []
            more = True
            while True:
                if more and len(active) < depth:
                    try:
                        active.append(next(it))
                    except StopIteration:
                        more = False
                if not active:
                    break
                for g in list(active):
                    try:
                        next(g)
                    except StopIteration:
                        active.remove(g)

        def drain(g):
            for _ in g:
                pass

        def acq(pool):
            while True:
                it = pool.acquire()
                if it is not None:
                    return it
                yield

        def bfview(bank_ap):
            return bank_ap.bitcast(BF16).rearrange("p (c t) -> p c t", c=8)

        def norm_T_gen(st, xt_ap, xbuf, gT, out_ap3, outbuf, tps, tpsb, pool=None):
            ss, b, _unused = st["sets"].next()
            xb, bxb = st["xb"].next()
            P.op("act", lambda e: e.activation(out=xb[:], in_=xt_ap, func=AF.Square, scale=1.0 / 32,
                                               accum_out=ss[:, 0:1]), [xbuf], [b[0], bxb])
            yield
            P.op("dve", lambda e: e.tensor_scalar(out=ss[:, 1:2], in0=ss[:, 0:1], scalar1=EPS, scalar2=None,
                                                  op0=ALU.add), [b[0]], [b[1]])
            yield
            P.op("act", lambda e: e.activation(out=ss[:, 2:3], in_=ss[:, 1:2], func=AF.Ln), [b[1]], [b[2]])
            yield
            P.op("act", lambda e: e.activation(out=ss[:, 3:4], in_=ss[:, 2:3], func=AF.Exp, scale=-0.5), [b[2]], [b[3]])
            yield
            P.op("act", lambda e: e.activation(out=xb[:], in_=xt_ap, func=AF.Copy, scale=ss[:, 3:4]), [xbuf, b[3]], [bxb])
            yield
            held = None
            if pool is not None:
                while held is None:
                    held = pool.acquire()
                    if held is None:
                        yield
                tps, tpsb = bfview(held[0][:]), held[1]
            P.op("pe", [(lambda e, c=c: e.transpose(tps[:, c, :], xb[:, c * 128:(c + 1) * 128], ident[:])) for c in range(8)],
                 [bxb], [tpsb])
            yield
            P.op("dve", lambda e: e.tensor_tensor(out=out_ap3, in0=tps, in1=gT[:, 0:8].unsqueeze(2).broadcast_to([128, 8, 128]),
                                                  op=ALU.mult), [tpsb], [outbuf])
            if held is not None:
                pool.release(held)
            yield

        def norm_T(st, xt_ap, xbuf, gT, out_ap3, outbuf, tps, tpsb):
            drain(norm_T_gen(st, xt_ap, xbuf, gT, out_ap3, outbuf, tps[:], tpsb))

        def norm_state(stack, pfx, nsets=2, nxb=2):
            st = {}
            st["sets"] = Rot([(sb(stack, pfx + "ss%d" % i, [128, 4]), [Buf("ss") for _ in range(4)], None) for i in range(nsets)])
            st["xb"] = Rot([(sb(stack, pfx + "xb%d" % i, [128, 1024], BF16), Buf("xb")) for i in range(nxb)])
            return st

        def head_norm_rope_gen(ws, src_ps, bsrc, H, g_bc, cosb, sinb, dst, bdst, t):
            if isinstance(ws, Pool):
                w = yield from acq(ws)
            else:
                w = ws.next()
            n = H * 64
            src3 = src_ps.rearrange("p (h d) -> p h d", h=H)
            P.op("act", lambda e: e.activation(out=w["sq"][:, 0:n], in_=src_ps, func=AF.Square, scale=0.125), [bsrc], [w["bsq"]])
            yield
            P.op("dve", lambda e: e.tensor_reduce(out=w["hs"][:, 0:H], in_=w["sq"][:, 0:n].rearrange("p (h d) -> p h d", h=H),
                                                  axis=AX.X, op=ALU.add), [w["bsq"]], [w["bhs"]])
            yield
            P.op("dve", lambda e: e.tensor_scalar(out=w["hs"][:, 8:8 + H], in0=w["hs"][:, 0:H], scalar1=EPS, scalar2=None, op0=ALU.add),
                 [w["bhs"]], [w["bhs1"]])
            yield
            P.op("act", lambda e: e.activation(out=w["hs"][:, 16:16 + H], in_=w["hs"][:, 8:8 + H], func=AF.Ln), [w["bhs1"]], [w["bhs2"]])
            yield
            P.op("act", lambda e: e.activation(out=w["hs"][:, 24:24 + H], in_=w["hs"][:, 16:16 + H], func=AF.Exp, scale=-0.5),
                 [w["bhs2"]], [w["bhs3"]])
            yield
            qn3 = w["qn"][:, 0:n].rearrange("p (h d) -> p h d", h=H)
            P.op("dve", lambda e: e.tensor_tensor(out=qn3, in0=src3, in1=w["hs"][:, 24:24 + H].unsqueeze(2).broadcast_to([128, H, 64]),
                                                  op=ALU.mult), [bsrc, w["bhs3"]], [w["bqn"]])
            yield
            P.op("dve", lambda e: e.tensor_tensor(out=qn3, in0=qn3, in1=g_bc[:, :].unsqueeze(1).broadcast_to([128, H, 64]),
                                                  op=ALU.mult), [w["bqn"]], [w["bqn"]])
            yield
            qn4 = w["qn"][:, 0:n].rearrange("p (h i t) -> p h i t", h=H, t=2)
            A4 = w["A"][:, 0:n].rearrange("p (h i t) -> p h i t", h=H, t=2)
            B4 = w["B"][:, 0:n].rearrange("p (h i t) -> p h i t", h=H, t=2)
            c4 = cosb[:, t, :].unsqueeze(1).unsqueeze(3).broadcast_to([128, H, 32, 2])
            s4 = sinb[:, t, :].unsqueeze(1).unsqueeze(3).broadcast_to([128, H, 32, 2])
            P.op("dve", lambda e: e.tensor_tensor(out=A4, in0=qn4, in1=c4, op=ALU.mult), [w["bqn"]], [w["bA"]])
            yield
            P.op("dve", lambda e: e.tensor_tensor(out=B4, in0=qn4, in1=s4, op=ALU.mult), [w["bqn"]], [w["bB"]])
            yield
            P.op("dve", lambda e: e.tensor_tensor(out=dst[:, :, :, 0], in0=A4[:, :, :, 0], in1=B4[:, :, :, 1], op=ALU.subtract),
                 [w["bA"], w["bB"]], [bdst])
            yield
            P.op("dve", lambda e: e.tensor_tensor(out=dst[:, :, :, 1], in0=B4[:, :, :, 0], in1=A4[:, :, :, 1], op=ALU.add),
                 [w["bA"], w["bB"]], [bdst])
            if isinstance(ws, Pool):
                ws.release(w)
            yield

        def rope_state(stack, pfx, n, nsets=1):
            sets = []
            for i in range(nsets):
                w = {}
                for k in ("sq", "qn", "A", "B"):
                    w[k] = sb(stack, "%s%s%d" % (pfx, k, i), [128, n])
                w["hs"] = sb(stack, "%shs%d" % (pfx, i), [128, 32])
                for k in ("bsq", "bhs", "bhs1", "bhs2", "bhs3", "bqn", "bA", "bB"):
                    w[k] = Buf(k)
                sets.append(w)
            return Rot(sets)

        with contextlib.ExitStack() as ph:
            wkv = sb(ph, "wkv", [128, 8, 256], BF16)
            cosb = sb(ph, "cosb", [128, 64, 32])
            sinb = sb(ph, "sinb", [128, 64, 32])
            gk_bc = sb(ph, "gk_bc", [128, 64])
            P.dma("pool", lambda e: e.dma_start(out=wkv[:], in_=wv(w_in, 512, 768)), [], [B_const])
            P.dma("sp", lambda e: e.dma_start(out=cosb[:], in_=cos_seq[:, :, :]), [], [B_const])
            P.dma("sp", lambda e: e.dma_start(out=sinb[:], in_=sin_seq[:, :, :]), [], [B_const])
            P.dma("sp", lambda e: e.dma_start(out=gk_bc[:], in_=g_k.partition_broadcast(128)), [], [B_const])
            NP1 = 8
            P.barrier()
            st = norm_state(ph, "p1", nsets=NP1, nxb=NP1)
            rws = rope_state(ph, "p1r", 128, nsets=NP1)
            xts = Rot([(sb(ph, "p1x%d" % i, [128, 1024]), Buf("x")) for i in range(NP1)])
            hTs = Rot([(sb(ph, "p1h%d" % i, [128, 8, 128], BF16), Buf("h")) for i in range(NP1)])
            krs = Rot([(sb(ph, "p1kr%d" % i, [128, 2, 32, 2], BF16), Buf("kr")) for i in range(NP1)])
            banks = Pool([(ps(ph, "p1b%d" % i, [128, 512]), Buf("bank", True)) for i in range(8)])

            import os as _os

            def p1_tile(t):
                xt, bx = xts.next()
                P.dma("sp", lambda e: e.dma_start(out=xt[:], in_=xseq[t * 128:(t + 1) * 128, :]), [], [bx])
                yield
                hT, bh = hTs.next()
                yield from norm_T_gen(st, xt[:], bx, gmixT, hT[:], bh, None, None, pool=banks)
                hkv = yield from acq(banks)
                kv, bkv = hkv
                P.op("pe", [(lambda e, c=c: e.matmul(kv[:, 0:256], hT[:, c, :], wkv[:, c, :], start=(c == 0), stop=(c == 7)))
                            for c in range(8)], [bh, B_const], [bkv])
                yield
                P.op("act", lambda e: e.activation(out=V_all[:, t, :, 0:64], in_=kv[:, 128:256].rearrange("p (h d) -> p h d", h=2),
                                                   func=AF.Copy), [bkv], [])
                yield
                kr, bkr = krs.next()
                yield from head_norm_rope_gen(rws, kv[:, 0:128], bkv, 2, gk_bc, cosb, sinb, kr, bkr, t)
                banks.release(hkv)
                hkt = yield from acq(banks)
                kt, bkt = hkt
                ktv = kt[:].bitcast(BF16)
                P.op("pe", lambda e: e.transpose(ktv[:, 0:128], kr[:].rearrange("p h i t -> p (h i t)"), ident[:]), [bkr], [bkt])
                yield
                P.op("act", lambda e: e.activation(out=kT_all[:, t * 128:(t + 1) * 128], in_=ktv[:, 0:128], func=AF.Copy), [bkt], [])
                banks.release(hkt)
                yield

            run_pipeline((p1_tile(t) for t in range(int(_os.environ.get('NT1', 64)))), int(_os.environ.get('DEPTH1', 8)))
            P.flush()

        if dbg == 1:
            P.limit = 10 ** 9
            P.limit = 10 ** 9
            print("nops", P.nops)
            dk = nc.dram_tensor("dbg_kT", [128, SEQ], BF16, kind="ExternalOutput").ap()
            dv = nc.dram_tensor("dbg_V", [128, 64 * 2 * 65], BF16, kind="ExternalOutput").ap()
            P.dma("sp", lambda e: e.dma_start(out=dk[:, :], in_=kT_all[:]), [], [])
            P.dma("sp", lambda e: e.dma_start(out=dv[:, :], in_=V_all[:].rearrange("p a b c -> p (a b c)")), [], [])
            P.flush()
            attn_stack.close()
            return nc

        with contextlib.ExitStack() as ph:
            wq = sb(ph, "wq", [128, 8, 512], BF16)
            wao = sb(ph, "wao", [64, 8, 1024], BF16)
            cosb = sb(ph, "cosb2", [128, 32, 32])
            sinb = sb(ph, "sinb2", [128, 32, 32])
            gq_bc = sb(ph, "gq_bc", [128, 64])
            ones65 = sb(ph, "ones65", [65, 64])
            B_c2 = Buf("c2")
            for hd_ in range(8):
                pos_ = (hd_ % 4) * 2 + hd_ // 4
                P.dma("pool", lambda e, hd_=hd_, pos_=pos_: e.dma_start(out=wq[:, :, pos_ * 64:(pos_ + 1) * 64], in_=wv(w_in, hd_ * 64, hd_ * 64 + 64)), [], [B_c2])
            P.dma("pool", lambda e: e.dma_start(out=wao[:], in_=w_att_out.rearrange("(h p) n -> p h n", p=64)), [], [B_c2])
            P.dma("sp", lambda e: e.dma_start(out=cosb[:], in_=cos_own[:, :, :]), [], [B_c2])
            P.dma("sp", lambda e: e.dma_start(out=sinb[:], in_=sin_own[:, :, :]), [], [B_c2])
            P.dma("sp", lambda e: e.dma_start(out=gq_bc[:], in_=g_q.partition_broadcast(128)), [], [B_c2])
            P.op("dve", lambda e: e.tensor_scalar(out=gq_bc[:], in0=gq_bc[:], scalar1=0.125, scalar2=None, op0=ALU.mult), [B_c2], [B_c2])
            P.op("pool", lambda e: e.memset(ones65[:], 1.0), [], [B_c2])
            P.barrier()
            st = norm_state(ph, "p2", nsets=4, nxb=4)
            rws = Pool(rope_state(ph, "p2r", 512, nsets=3).items)
            xts = Rot([(sb(ph, "p2x%d" % i, [128, 1024]), Buf("x")) for i in range(4)])
            hTs = Rot([(sb(ph, "p2h%d" % i, [128, 8, 128], BF16), Buf("h")) for i in range(4)])
            qrs = Rot([(sb(ph, "p2qr%d" % i, [128, 8, 32, 2], BF16), Buf("qr")) for i in range(4)])
            Ps = Rot([(sb(ph, "p2P%d" % i, [128, 2, 512], BF16), Buf("P")) for i in range(3)])
            OnTs = Rot([(sb(ph, "p2OnT%d" % i, [64, 8, 512], BF16), [Buf("OnT") for _ in range(8)]) for i in range(2)])
            ysbs = Rot([(sb(ph, "p2ysb%d" % i, [128, 4, 512]), [Buf("ysb") for _ in range(4)]) for i in range(1)])
            Sps = Rot([(ps(ph, "p2S%d" % i, [128, 2, 512]), Buf("S", True)) for i in range(3)])
            Ops, bO = ps(ph, "p2O", [128, 2, 512]), [Buf("O0", True), Buf("O1", True)]
            LA = int(_os0.environ.get('LA', 2))

            sbank = Pool([(Sps.items[i][0][:, bnk, :], Buf("sbank", True)) for i in range(3) for bnk in range(2)])
            Osbs = Rot([(sb(ph, "p2Osc%d" % i, [65, 512]), Buf("Osb")) for i in range(4)])
            rrs = Rot([(sb(ph, "p2rrc%d" % i, [65, 512]), Buf("rr")) for i in range(4)])

            qT_all = sb(ph, "qT_all", [128, 4, OWN], BF16)
            bqTt = [Buf("qTt") for _ in range(32)]
            print("sbuf bytes remaining in 2a:", nc.sbuf_bytes_remaining)

            def q_tile(tile):
                xt, bx = xts.next()
                P.dma("sp", lambda e: e.dma_start(out=xt[:], in_=xown[tile * 128:(tile + 1) * 128, :]), [], [bx])
                yield
                hT, bh = hTs.next()
                yield from norm_T_gen(st, xt[:], bx, gmixT, hT[:], bh, None, None, pool=sbank)
                hq = yield from acq(sbank)
                qps, bqp = hq
                P.op("pe", [(lambda e, c=c: e.matmul(qps, hT[:, c, :], wq[:, c, :], start=(c == 0), stop=(c == 7)))
                            for c in range(8)], [bh, B_c2], [bqp])
                yield
                qr, bqr = qrs.next()
                yield from head_norm_rope_gen(rws, qps, bqp, 8, gq_bc, cosb, sinb, qr, bqr, tile)
                sbank.release(hq)
                ht = yield from acq(sbank)
                tpv = bfview(ht[0])
                qr3 = qr[:].rearrange("p h i t -> p (h i t)")
                P.op("pe", [(lambda e, g=g: e.transpose(tpv[:, g, :], qr3[:, g * 128:(g + 1) * 128], ident[:])) for g in range(4)],
                     [bqr], [ht[1]])
                yield
                P.op("act", lambda e: e.activation(out=qT_all[:, :, tile * 128:(tile + 1) * 128], in_=tpv[:, 0:4, :], func=AF.Copy),
                     [ht[1]], [bqTt[tile]])
                sbank.release(ht)
                yield

            spool = Pool(list(Sps.items))

            def epi_unit(item, bnk):
                (Osb, bOsb, rr, brr, OnT, bOn, head) = item
                hS = spool.acquire()
                assert hS is not None
                mS, bmS = hS
                P.op("pe", lambda e: e.matmul(mS[0:64, bnk, :], ones65[64:65, 0:64], rr[64:65, :], start=True, stop=True), [brr, B_c2], [bmS])
                P.op("dve", lambda e: e.tensor_tensor(out=OnT[:, head, :], in0=Osb[0:64, :], in1=mS[0:64, bnk, :], op=ALU.mult),
                     [bOsb, bmS], [bOn[head]])
                spool.release(hS)

            def y_unit(gi, dc, OnT, bOn, ysb, bys):
                hS = spool.acquire()
                assert hS is not None
                mS, bmS = hS
                P.op("pe", [(lambda e, h=h: e.matmul(mS[:, 0, :], wao[:, h, dc * 128:(dc + 1) * 128], OnT[:, h, :], start=(h == 0), stop=(h == 7)))
                            for h in range(8)], bOn + [B_c2], [bmS])
                P.op("dve", lambda e: e.tensor_copy(out=ysb[:, dc % 4, :], in_=mS[:, 0, :]), [bmS], [bys[dc % 4]])
                spool.release(hS)
                if dc % 4 == 3:
                    P.dma("sp", lambda e: e.dma_start(out=yatt_d[gi][:, dc - 3:dc + 1, :], in_=ysb[:]), bys, [])

            run_pipeline((q_tile(t) for t in range(32)), 4)
            P.barrier()
            units = []
            for gi in range(8):
                OnT, bOn = OnTs.next()
                bqT = bqTt[gi * 4:gi * 4 + 4]
                for g in range(4):
                    pend = []

                    def try_issue(kt, g=g, gi=gi, bqT=bqT, pend=pend):
                        hS = spool.acquire()
                        if hS is None:
                            return False
                        S, bS = hS
                        P.op("pe", [lambda e: e.matmul(S[:, 0, :], kT_all[0:64, kt * 128:(kt + 1) * 128], qT_all[0:64, g, gi * 512:(gi + 1) * 512], start=True, stop=True),
                                    lambda e: e.matmul(S[:, 1, :], kT_all[64:128, kt * 128:(kt + 1) * 128], qT_all[64:128, g, gi * 512:(gi + 1) * 512], start=True, stop=True)],
                             bqT, [bS])
                        pend.append(hS)
                        return True
                    nxt = 0
                    for kt in range(64):
                        while nxt < 64 and nxt <= kt + LA and try_issue(nxt):
                            nxt += 1
                        hS = pend.pop(0)
                        S, bS = hS
                        Pt, bP = Ps.next()
                        P.op("act", lambda e, S=S, Pt=Pt: e.activation(out=Pt[:], in_=S[:], func=AF.Exp), [bS], [bP])
                        spool.release(hS)
                        if units and kt >= 3 and kt % 3 == 0:
                            units.pop(0)()
                        P.op("pe", [lambda e, Pt=Pt, kt=kt: e.matmul(Ops[0:65, 0, :], V_all[:, kt, 0, :], Pt[:, 0, :], start=(kt == 0), stop=(kt == 63)),
                                    lambda e, Pt=Pt, kt=kt: e.matmul(Ops[0:65, 1, :], V_all[:, kt, 1, :], Pt[:, 1, :], start=(kt == 0), stop=(kt == 63))],
                             [bP], bO)
                    for hh in range(2):
                        head = g + 4 * hh
                        Osb, bOsb = Osbs.next()
                        rr, brr = rrs.next()
                        P.op("act", lambda e, Osb=Osb, hh=hh: e.activation(out=Osb[:], in_=Ops[0:65, hh, :], func=AF.Copy), [bO[hh]], [bOsb])
                        P.op("dve", lambda e, Osb=Osb, rr=rr: e.reciprocal(out=rr[64:65, :], in_=Osb[64:65, :]), [bOsb], [brr])
                        item = (Osb, bOsb, rr, brr, OnT, bOn, head)
                        units.append(lambda item=item, hh=hh: epi_unit(item, hh))
                ysb, bys = ysbs.next()
                for dc in range(8):
                    units.append(lambda gi=gi, dc=dc, OnT=OnT, bOn=bOn, ysb=ysb, bys=bys: y_unit(gi, dc, OnT, bOn, ysb, bys))
            while units:
                units.pop(0)()
            P.flush()

        attn_stack.close()
        if dbg == 2:
            dy = nc.dram_tensor("dbg_yatt", [8, 128, 8, 512], F32, kind="ExternalOutput").ap()
            P.dma("sp", lambda e: e.dma_start(out=dy.rearrange("a p c t -> (a p) (c t)"), in_=yatt_d.rearrange("a p c t -> (a p) (c t)")), [], [])
            P.flush()
            return nc

        with contextlib.ExitStack() as ph:
            wckv = sb(ph, "wckv", [128, 8, 1024], BF16)
            gmemT = sb(ph, "gmemT", [128, 8])
            memT = sb(ph, "memT", [128, 8, 256], BF16)
            B_cm = Buf("cm")
            P.dma("pool", lambda e: e.dma_start(out=wckv[:], in_=wv(w_ckv, 0, 1024)), [], [B_cm])
            P.dma("sp", lambda e: e.dma_start(out=gmemT[:], in_=gT_view(g_mem), allow_slow_non_contiguous=True), [], [B_cm])
            st = norm_state(ph, "pm")
            xts = Rot([(sb(ph, "pmx%d" % i, [128, 1024]), Buf("x")) for i in range(2)])
            tp, btp = ps(ph, "pmtp", [128, 8, 128], BF16), Buf("tp", True)
            mps = Rot([(ps(ph, "pmps%d" % i, [128, 512]), Buf("mps", True)) for i in range(2)])
            bmemT = [Buf("memT0"), Buf("memT1")]
            for mt in range(2):
                xt, bx = xts.next()
                P.dma("sp", lambda e, xt=xt, mt=mt: e.dma_start(out=xt[:], in_=memd[mt * 128:(mt + 1) * 128, :]), [], [bx])
                norm_T(st, xt[:], bx, gmemT, memT[:, :, mt * 128:(mt + 1) * 128], bmemT[mt], tp, btp)
            for h in range(4):
                mp, bmp = mps.next()
                P.op("pe", [(lambda e, c=c, h=h, mp=mp: e.matmul(mp[:, 0:256], wckv[:, c, h * 128:(h + 1) * 128], memT[:, c, :], start=(c == 0), stop=(c == 7)))
                            for c in range(8)], bmemT + [B_cm], [bmp])
                P.op("act", lambda e, h=h, mp=mp: e.activation(out=kcT[:, h, :], in_=mp[:, 0:256], func=AF.Copy), [bmp], [])
            for mt in range(2):
                mp, bmp = mps.next()
                P.op("pe", [(lambda e, c=c, mt=mt, mp=mp: e.matmul(mp[:], memT[:, c, mt * 128:(mt + 1) * 128], wckv[:, c, 512:1024], start=(c == 0), stop=(c == 7)))
                            for c in range(8)], bmemT + [B_cm], [bmp])
                P.op("act", lambda e, mt=mt, mp=mp: e.activation(out=Vc[:, mt, :], in_=mp[:], func=AF.Copy), [bmp], [])
            P.flush()

        with contextlib.ExitStack() as ph:
            wsv = sb(ph, "wsv", [128, 8, 512], BF16)
            wsu = sb(ph, "wsu", [128, 8, 512], BF16)
            wga = sb(ph, "wga", [128, 8, 1024], BF16)
            wgb = sb(ph, "wgb", [128, 8, 1024], BF16)
            wso = sb(ph, "wso", [128, 4, 1024], BF16)
            wo = sb(ph, "wo", [128, 8, 1024], BF16)
            wcq = sb(ph, "wcq", [128, 8, 512], BF16)
            wco = sb(ph, "wco", [128, 4, 1024], BF16)
            gcrT = sb(ph, "gcrT", [128, 8])
            lng_bc = sb(ph, "lng_bc", [128, 512])
            lnb_bc = sb(ph, "lnb_bc", [128, 512])
            bs_bc = sb(ph, "bs_bc", [128, 4, 128])
            wsraw = sb(ph, "wsraw", [128, 4, 128], BF16)
            WsT = sb(ph, "WsT", [128, 4, 128], BF16)
            B_c = Buf("c2b")
            B_ws = Buf("ws")
            P.dma("pool", lambda e: e.dma_start(out=wsu[:], in_=wv(w_in, 768, 1280)), [], [B_c])
            P.dma("pool", lambda e: e.dma_start(out=wsv[:], in_=wv(w_in, 1280, 1792)), [], [B_c])
            P.dma("pool", lambda e: e.dma_start(out=wga[:], in_=wv(w_in, 1792, 2816)), [], [B_c])
            P.dma("pool", lambda e: e.dma_start(out=wgb[:], in_=wv(w_in, 2816, 3840)), [], [B_c])
            P.dma("pool", lambda e: e.dma_start(out=wso[:], in_=wv(w_sgu_out, 0, 1024)), [], [B_c])
            P.dma("pool", lambda e: e.dma_start(out=wo[:], in_=wv(w_out, 0, 1024)), [], [B_c])
            P.dma("pool", lambda e: e.dma_start(out=wcq[:], in_=wv(w_cq, 0, 512)), [], [B_c])
            P.dma("pool", lambda e: e.dma_start(out=wco[:], in_=wv(w_co, 0, 1024)), [], [B_c])
            P.dma("pool", lambda e: e.dma_start(out=wsraw[:], in_=w_s.rearrange("g p q -> p g q")), [], [B_ws])
            P.dma("sp", lambda e: e.dma_start(out=gcrT[:], in_=gT_view(g_cross), allow_slow_non_contiguous=True), [], [B_c])
            P.dma("sp", lambda e: e.dma_start(out=lng_bc[:], in_=sgu_ln_g.partition_broadcast(128)), [], [B_c])
            P.dma("sp", lambda e: e.dma_start(out=lnb_bc[:], in_=sgu_ln_b.partition_broadcast(128)), [], [B_c])
            P.dma("sp", lambda e: e.dma_start(out=bs_bc[:].rearrange("p g q -> p (g q)"), in_=b_s.rearrange("g q -> (g q)").partition_broadcast(128)), [], [B_c])
            P.barrier()
            st = norm_state(ph, "p3", nsets=4, nxb=4)
            bpool = Pool([(ps(ph, "p3g%d" % i, [128, 512]), Buf("g", True)) for i in range(8)])
            hb0 = bpool.acquire()
            tpb0 = bfview(hb0[0][:])
            P.op("pe", [(lambda e, g=g: e.transpose(tpb0[:, g, :], wsraw[:, g, :], ident[:])) for g in range(4)], [B_ws], [hb0[1]])
            P.op("act", lambda e: e.activation(out=WsT[:], in_=tpb0[:, 0:4, :], func=AF.Copy), [hb0[1]], [B_c])
            bpool.release(hb0)
            P.barrier()

            xg = [(sb(ph, "p3x%d" % i, [128, 1024]), Buf("x")) for i in range(4)]
            hTg, bhT = sb(ph, "p3hT", [128, 8, 512], BF16), [Buf("hT") for _ in range(4)]
            hcT, bhc = hTg, bhT
            NA = 3
            asets = Pool([dict(gv=sb(ph, "p3gv%d" % i, [128, 512]), vn=sb(ph, "p3vn%d" % i, [128, 512], BF16), lst=sb(ph, "p3lst%d" % i, [128, 16]),
                               bgv=Buf("gv"), bvn=Buf("vn"), bl=[Buf("l") for _ in range(8)]) for i in range(NA)])
            vmb, bvmb = sb(ph, "p3vmb", [128, 4, 512]), [Buf("vmb") for _ in range(4)]
            gus = Pool([(sb(ph, "p3gu%d" % i, [128, 512]), Buf("gu")) for i in range(2)])
            sguT, bsg = sb(ph, "p3sguT", [128, 4, 512], BF16), [Buf("sg") for _ in range(4)]
            csets = Pool([dict(ya=sb(ph, "p3ya%d" % i, [128, 512]), sa=sb(ph, "p3sa%d" % i, [128, 512]), sb=sb(ph, "p3sb%d" % i, [128, 512]),
                               bya=Buf("ya"), bsa=Buf("sa"), bsb=Buf("sb")) for i in range(2)])
            mrg, bmrg = sb(ph, "p3mrg", [128, 8, 512], BF16), [Buf("mrg") for _ in range(8)]
            qcT, bqc = sb(ph, "p3qcT", [128, 4, 512], BF16), [Buf("qc") for _ in range(4)]
            gsets = Pool([dict(cst=sb(ph, "p3cst%d" % i, [128, 16]), pc=sb(ph, "p3pc%d" % i, [128, 4, 256]), pn=sb(ph, "p3pn%d" % i, [128, 4, 256], BF16),
                               pnT=sb(ph, "p3pnT%d" % i, [128, 8, 128], BF16), ocT=sb(ph, "p3ocT%d" % i, [128, 4, 128], BF16),
                               bc=[Buf("c") for _ in range(4)], bpc=Buf("pc"), bpn=Buf("pn"), bpnT=Buf("pnT"), boc=Buf("oc")) for i in range(2)])
            print("sbuf bytes remaining in 2b:", nc.sbuf_bytes_remaining)

            def a_tile(gi, j):
                tile = gi * 4 + j
                xt, bx = xg[j]
                P.dma("sp", lambda e: e.dma_start(out=xt[:], in_=xown[tile * 128:(tile + 1) * 128, :]), [], [bx])
                yield
                yield from norm_T_gen(st, xt[:], bx, gmixT, hTg[:, :, j * 128:(j + 1) * 128], bhT[j], None, None, pool=bpool)
                A = yield from acq(asets)
                gv, vn, lst, bgv, bvn, bl = A["gv"], A["vn"], A["lst"], A["bgv"], A["bvn"], A["bl"]
                h0 = yield from acq(bpool)
                g0, bg0 = h0
                P.op("pe", [(lambda e, c=c: e.matmul(g0[:], hTg[:, c, j * 128:(j + 1) * 128], wsv[:, c, :], start=(c == 0), stop=(c == 7)))
                            for c in range(8)], [bhT[j], B_c], [bg0])
                yield
                P.op("act", lambda e: e.activation(out=gv[:], in_=g0[:], func=AF.Gelu_apprx_tanh), [bg0], [bgv])
                bpool.release(h0)
                yield
                P.op("dve", lambda e: e.tensor_reduce(out=lst[:, 0:1], in_=gv[:], axis=AX.X, op=ALU.add), [bgv], [bl[0]])
                yield
                P.op("act", lambda e: e.activation(out=vn[:], in_=gv[:], func=AF.Square, accum_out=lst[:, 1:2]), [bgv], [bvn, bl[1]])
                yield
                P.op("dve", lambda e: e.tensor_scalar(out=lst[:, 2:3], in0=lst[:, 0:1], scalar1=1.0 / 512, scalar2=None, op0=ALU.mult), [bl[0]], [bl[2]])
                yield
                P.op("dve", lambda e: e.tensor_tensor(out=lst[:, 3:4], in0=lst[:, 2:3], in1=lst[:, 2:3], op=ALU.mult), [bl[2]], [bl[3]])
                yield
                P.op("dve", lambda e: e.scalar_tensor_tensor(out=lst[:, 4:5], in0=lst[:, 1:2], scalar=1.0 / 512, in1=lst[:, 3:4], op0=ALU.mult, op1=ALU.subtract),
                     [bl[1], bl[3]], [bl[4]])
                yield
                P.op("dve", lambda e: e.tensor_scalar(out=lst[:, 5:6], in0=lst[:, 4:5], scalar1=EPS, scalar2=None, op0=ALU.add), [bl[4]], [bl[5]])
                yield
                P.op("act", lambda e: e.activation(out=lst[:, 6:7], in_=lst[:, 5:6], func=AF.Ln), [bl[5]], [bl[6]])
                yield
                P.op("act", lambda e: e.activation(out=lst[:, 7:8], in_=lst[:, 6:7], func=AF.Exp, scale=-0.5), [bl[6]], [bl[7]])
                yield
                P.op("dve", lambda e: e.tensor_scalar(out=gv[:], in0=gv[:], scalar1=lst[:, 2:3], scalar2=lst[:, 7:8], op0=ALU.subtract, op1=ALU.mult),
                     [bgv, bl[2], bl[7]], [bgv])
                yield
                P.op("dve", lambda e: e.tensor_tensor(out=gv[:], in0=gv[:], in1=lng_bc[:], op=ALU.mult), [bgv, B_c], [bgv])
                yield
                P.op("dve", lambda e: e.tensor_tensor(out=vn[:], in0=gv[:], in1=lnb_bc[:], op=ALU.add), [bgv, B_c], [bvn])
                yield
                h1 = yield from acq(bpool)
                g1, bg1 = h1
                P.op("pe", [(lambda e, g=g: e.matmul(g1[:, g * 128:(g + 1) * 128], vn[:, g * 128:(g + 1) * 128], WsT[:, g, :], start=True, stop=True))
                            for g in range(4)], [bvn, B_c], [bg1])
                yield
                P.op("dve", lambda e: e.tensor_tensor(out=vmb[:, :, j * 128:(j + 1) * 128], in0=g1[:].rearrange("p (g q) -> p g q", g=4),
                                                      in1=bs_bc[:], op=ALU.add), [bg1, B_c], [bvmb[j]])
                bpool.release(h1)
                asets.release(A)
                yield

            def b_chunk(g):
                h0 = yield from acq(bpool)
                g0, bg0 = h0
                P.op("pe", [(lambda e, c=c: e.matmul(g0[:], wsu[:, c, g * 128:(g + 1) * 128], hTg[:, c, :], start=(c == 0), stop=(c == 7)))
                            for c in range(8)], bhT + [B_c], [bg0])
                yield
                G = yield from acq(gus)
                gu, bgu = G
                P.op("act", lambda e: e.activation(out=gu[:], in_=g0[:], func=AF.Gelu_apprx_tanh), [bg0], [bgu])
                bpool.release(h0)
                yield
                P.op("dve", lambda e: e.tensor_tensor(out=sguT[:, g, :], in0=gu[:], in1=vmb[:, g, :], op=ALU.mult), [bgu] + bvmb, [bsg[g]])
                gus.release(G)
                yield

            def c_chunk(gi, dc):
                C = yield from acq(csets)
                ya, sa, sbt, bya, bsa, bsb = C["ya"], C["sa"], C["sb"], C["bya"], C["bsa"], C["bsb"]
                P.dma("sp", lambda e: e.dma_start(out=ya[:], in_=yatt_d[gi][:, dc, :]), [], [bya])
                yield
                h0 = yield from acq(bpool)
                ga_ps, bga = h0
                P.op("pe", [(lambda e, c=c: e.matmul(ga_ps[:], wga[:, c, dc * 128:(dc + 1) * 128], hTg[:, c, :], start=(c == 0), stop=(c == 7)))
                            for c in range(8)], bhT + [B_c], [bga])
                yield
                P.op("act", lambda e: e.activation(out=sa[:], in_=ga_ps[:], func=AF.Sigmoid), [bga], [bsa])
                bpool.release(h0)
                yield
                h1 = yield from acq(bpool)
                gb_ps, bgb = h1
                P.op("pe", [(lambda e, c=c: e.matmul(gb_ps[:], wgb[:, c, dc * 128:(dc + 1) * 128], hTg[:, c, :], start=(c == 0), stop=(c == 7)))
                            for c in range(8)], bhT + [B_c], [bgb])
                yield
                P.op("act", lambda e: e.activation(out=sbt[:], in_=gb_ps[:], func=AF.Sigmoid), [bgb], [bsb])
                bpool.release(h1)
                yield
                h2 = yield from acq(bpool)
                ys_ps, bysp = h2
                P.op("pe", [(lambda e, g=g: e.matmul(ys_ps[:], wso[:, g, dc * 128:(dc + 1) * 128], sguT[:, g, :], start=(g == 0), stop=(g == 3)))
                            for g in range(4)], bsg + [B_c], [bysp])
                yield
                P.op("dve", lambda e: e.tensor_tensor(out=sa[:], in0=sa[:], in1=ya[:], op=ALU.mult), [bsa, bya], [bsa])
                yield
                P.op("dve", lambda e: e.tensor_tensor(out=sbt[:], in0=sbt[:], in1=ys_ps[:], op=ALU.mult), [bsb, bysp], [bsb])
                bpool.release(h2)
                yield
                P.op("dve", lambda e: e.tensor_tensor(out=mrg[:, dc, :], in0=sa[:], in1=sbt[:], op=ALU.add), [bsa, bsb], [bmrg[dc]])
                csets.release(C)
                yield

            def d_part(j, nb):
                xt, bx = xg[j]
                h0 = yield from acq(bpool)
                o_ps, bo = h0
                P.op("pe", [(lambda e, dc=dc: e.matmul(o_ps[:], mrg[:, dc, j * 128:(j + 1) * 128], wo[:, dc, nb * 512:(nb + 1) * 512],
                                                      start=(dc == 0), stop=(dc == 7))) for dc in range(8)], bmrg + [B_c], [bo])
                yield
                P.op("dve", lambda e: e.tensor_tensor(out=xt[:, nb * 512:(nb + 1) * 512], in0=xt[:, nb * 512:(nb + 1) * 512], in1=o_ps[:], op=ALU.add),
                     [bo, bx], [bx])
                bpool.release(h0)
                yield

            def e_tile(j):
                xt, bx = xg[j]
                yield from norm_T_gen(st, xt[:], bx, gcrT, hcT[:, :, j * 128:(j + 1) * 128], bhc[j], None, None, pool=bpool)

            def f_head(h):
                h0 = yield from acq(bpool)
                q_ps, bq = h0
                P.op("pe", [(lambda e, c=c: e.matmul(q_ps[:], wcq[:, c, h * 128:(h + 1) * 128], hcT[:, c, :], start=(c == 0), stop=(c == 7)))
                            for c in range(8)], bhc + [B_c], [bq])
                yield
                P.op("act", lambda e: e.activation(out=qcT[:, h, :], in_=q_ps[:], func=AF.Copy, scale=float(128 ** -0.5)), [bq], [bqc[h]])
                bpool.release(h0)
                yield

            def g_tile(gi, j):
                tile = gi * 4 + j
                xt, bx = xg[j]
                Gs = yield from acq(gsets)
                cst, pc, pn, pnT, ocT = Gs["cst"], Gs["pc"], Gs["pn"], Gs["pnT"], Gs["ocT"]
                bc, bpc, bpn, bpnT, boc = Gs["bc"], Gs["bpc"], Gs["bpn"], Gs["bpnT"], Gs["boc"]
                hs0 = yield from acq(bpool)
                hs1 = yield from acq(bpool)
                scs = [hs0[0], hs1[0]]
                bscs = [hs0[1], hs1[1]]
                for half in range(2):
                    P.op("pe", [(lambda e, hh=hh, half=half: e.matmul(scs[half][:, hh * 256:(hh + 1) * 256], qcT[:, half * 2 + hh, j * 128:(j + 1) * 128], kcT[:, half * 2 + hh, :],
                                                                       start=True, stop=True)) for hh in range(2)], bqc, [bscs[half]])
                    yield
                for half in range(2):
                    P.op("dve", lambda e, half=half: e.tensor_reduce(out=cst[:, half * 2:half * 2 + 2], in_=scs[half][:].rearrange("p (h m) -> p h m", h=2), axis=AX.X, op=ALU.max),
                         [bscs[half]], [bc[0]])
                    yield
                P.op("dve", lambda e: e.tensor_scalar(out=cst[:, 4:8], in0=cst[:, 0:4], scalar1=-1.0, scalar2=None, op0=ALU.mult), [bc[0]], [bc[1]])
                yield
                for half in range(2):
                    P.op("act", [(lambda e, hh=hh, half=half: e.activation(out=pc[:, half * 2 + hh, :], in_=scs[half][:, hh * 256:(hh + 1) * 256], func=AF.Exp,
                                                                          bias=cst[:, 4 + half * 2 + hh:5 + half * 2 + hh], accum_out=cst[:, 8 + half * 2 + hh:9 + half * 2 + hh]))
                                 for hh in range(2)], [bscs[half], bc[1]], [bpc, bc[2]])
                    yield
                bpool.release(hs0)
                bpool.release(hs1)
                P.op("dve", lambda e: e.reciprocal(out=cst[:, 12:16], in_=cst[:, 8:12]), [bc[2]], [bc[3]])
                yield
                P.op("dve", lambda e: e.tensor_tensor(out=pn[:], in0=pc[:], in1=cst[:, 12:16].unsqueeze(2).broadcast_to([128, 4, 256]), op=ALU.mult),
                     [bpc, bc[3]], [bpn])
                yield
                ht = yield from acq(bpool)
                tpv = bfview(ht[0][:])
                P.op("pe", [(lambda e, k=k: e.transpose(tpv[:, k, :], pn[:, k // 2, (k % 2) * 128:(k % 2 + 1) * 128], ident[:])) for k in range(8)], [bpn], [ht[1]])
                yield
                P.op("act", lambda e: e.activation(out=pnT[:], in_=tpv, func=AF.Copy), [ht[1]], [bpnT])
                bpool.release(ht)
                yield
                ho = yield from acq(bpool)
                oc_ps, bocp = ho
                fl = []
                for h in range(4):
                    for mt in range(2):
                        fl.append(lambda e, h=h, mt=mt: e.matmul(oc_ps[:, h * 128:(h + 1) * 128], Vc[:, mt, h * 128:(h + 1) * 128], pnT[:, h * 2 + mt, :],
                                                                 start=(mt == 0), stop=(mt == 1)))
                P.op("pe", fl, [bpnT], [bocp])
                yield
                P.op("act", lambda e: e.activation(out=ocT[:], in_=oc_ps[:].rearrange("p (h t) -> p h t", h=4), func=AF.Copy), [bocp], [boc])
                bpool.release(ho)
                yield
                for nb in range(2):
                    hy = yield from acq(bpool)
                    y_ps, byp = hy
                    P.op("pe", [(lambda e, h=h, nb=nb, y_ps=y_ps: e.matmul(y_ps[:], ocT[:, h, :], wco[:, h, nb * 512:(nb + 1) * 512], start=(h == 0), stop=(h == 3)))
                                for h in range(4)], [boc, B_c], [byp])
                    yield
                    P.op("dve", lambda e, nb=nb, y_ps=y_ps: e.tensor_tensor(out=xt[:, nb * 512:(nb + 1) * 512], in0=xt[:, nb * 512:(nb + 1) * 512], in1=y_ps[:], op=ALU.add),
                         [byp, bx], [bx])
                    bpool.release(hy)
                    yield
                gsets.release(Gs)
                P.dma("sp", lambda e: e.dma_start(out=x2_d[tile * 128:(tile + 1) * 128, :], in_=xt[:]), [bx], [])
                yield

            for gi in range(8):
                run_pipeline((a_tile(gi, j) for j in range(4)), 4)
                run_pipeline((b_chunk(g) for g in range(4)), 3)
                run_pipeline((c_chunk(gi, dc) for dc in range(8)), 3)
                run_pipeline((d_part(j, nb) for j in range(4) for nb in range(2)), 4)
                run_pipeline((e_tile(j) for j in range(4)), 4)
                run_pipeline((f_head(h) for h in range(4)), 4)
                run_pipeline((g_tile(gi, j) for j in range(4)), 3)
            P.flush()

        if dbg == 3:
            dx = nc.dram_tensor("dbg_x2", [OWN, D], F32, kind="ExternalOutput").ap()
            P.dma("sp", lambda e: e.dma_start(out=dx[:, :], in_=x2_d[:, :]), [], [])
            P.flush()
            return nc

        with contextlib.ExitStack() as ph:
            gmoT = sb(ph, "gmoT", [128, 8])
            wr = sb(ph, "wr", [128, 8, 20], BF16)
            br_bc = sb(ph, "br_bc", [128, 20])
            gfin_bc = sb(ph, "gfin_bc", [128, 1024])
            B_c = Buf("c3")
            P.dma("sp", lambda e: e.dma_start(out=gmoT[:], in_=gT_view(g_moe), allow_slow_non_contiguous=True), [], [B_c])
            P.dma("pool", lambda e: e.dma_start(out=wr[:, :, 0:4], in_=wv(w_rg, 0, 4)), [], [B_c])
            P.dma("pool", lambda e: e.dma_start(out=wr[:, :, 4:20], in_=wv(w_re, 0, 16)), [], [B_c])
            P.dma("sp", lambda e: e.dma_start(out=br_bc[:, 0:4], in_=b_rg.partition_broadcast(128)), [], [B_c])
            P.dma("sp", lambda e: e.dma_start(out=br_bc[:, 4:20], in_=b_re.partition_broadcast(128)), [], [B_c])
            P.dma("sp", lambda e: e.dma_start(out=gfin_bc[:], in_=g_final.partition_broadcast(128)), [], [B_c])
            P.barrier()
            NP3 = 6
            st = norm_state(ph, "p4", nsets=NP3, nxb=NP3)
            xres, bxr = sb(ph, "xres", [128, 16, 1024]), [Buf("xr%d" % i) for i in range(16)]
            hmT, bhm = sb(ph, "hmT", [128, 8, 2048], BF16), [Buf("hm%d" % i) for i in range(16)]
            comb, bcomb = sb(ph, "comb", [128, 16, 16]), [Buf("cb%d" % i) for i in range(16)]
            Wg = [(sb(ph, "Wg%d" % i, [128, 8, 512], BF16), Buf("Wg")) for i in range(2)]
            Wu = [(sb(ph, "Wu%d" % i, [128, 8, 512], BF16), Buf("Wu")) for i in range(2)]
            Wd = [(sb(ph, "Wd%d" % i, [128, 4, 1024], BF16), Buf("Wd")) for i in range(2)]
            sgs = Rot([(sb(ph, "sg%d" % i, [128, 512]), Buf("sg")) for i in range(2)])
            heTs = Rot([(sb(ph, "heT%d" % i, [128, 4, 512], BF16), [Buf("he") for _ in range(4)]) for i in range(2)])
            ots = Rot([(sb(ph, "ot%d" % i, [128, 1024]), Buf("ot")) for i in range(2)])
            rts = Rot([(sb(ph, "rt%d" % i, [128, 160]), [Buf("rt") for _ in range(24)]) for i in range(NP3)])
            allbanks = [(ps(ph, "p4g%d" % i, [128, 512]), Buf("g", True)) for i in range(8)]
            bpool = Pool(allbanks)
            gen = Rot(allbanks)

            def moe_tile(sgi, tl):
                tile = sgi * 16 + tl
                P.dma("sp", lambda e: e.dma_start(out=xres[:, tl, :], in_=x2_d[tile * 128:(tile + 1) * 128, :]), [], [bxr[tl]])
                yield
                yield from norm_T_gen(st, xres[:, tl, :], bxr[tl], gmoT, hmT[:, :, tl * 128:(tl + 1) * 128], bhm[tl], None, None, pool=bpool)
                hb = yield from acq(bpool)
                r_ps, brp = hb
                P.op("pe", [(lambda e, c=c: e.matmul(r_ps[:, 0:20], hmT[:, c, tl * 128:(tl + 1) * 128], wr[:, c, :], start=(c == 0), stop=(c == 7)))
                            for c in range(8)], [bhm[tl], B_c], [brp])
                yield
                R, brt = rts.next()
                P.op("dve", lambda e: e.tensor_tensor(out=R[:, 0:20], in0=r_ps[:, 0:20], in1=br_bc[:], op=ALU.add), [brp, B_c], [brt[0]])
                bpool.release(hb)
                yield
                P.op("dve", lambda e: e.tensor_reduce(out=R[:, 20:21], in_=R[:, 0:4], axis=AX.X, op=ALU.max), [brt[0]], [brt[1]])
                yield
                P.op("dve", lambda e: e.tensor_scalar(out=R[:, 21:22], in0=R[:, 20:21], scalar1=-1.0, scalar2=None, op0=ALU.mult), [brt[1]], [brt[2]])
                yield
                P.op("act", lambda e: e.activation(out=R[:, 22:26], in_=R[:, 0:4], func=AF.Exp, bias=R[:, 21:22], accum_out=R[:, 26:27]), [brt[0], brt[2]], [brt[3]])
                yield
                P.op("dve", lambda e: e.reciprocal(out=R[:, 27:28], in_=R[:, 26:27]), [brt[3]], [brt[4]])
                yield
                P.op("dve", lambda e: e.tensor_scalar(out=R[:, 28:32], in0=R[:, 0:4], scalar1=R[:, 20:21], scalar2=None, op0=ALU.is_ge), [brt[0], brt[1]], [brt[5]])
                yield
                P.op("dve", lambda e: e.tensor_scalar(out=R[:, 32:36], in0=R[:, 28:32], scalar1=BIG, scalar2=-BIG, op0=ALU.mult, op1=ALU.add), [brt[5]], [brt[6]])
                yield
                P.op("dve", lambda e: e.tensor_tensor(out=R[:, 40:56].rearrange("p (g k) -> p g k", g=4), in0=R[:, 4:20].rearrange("p (g k) -> p g k", g=4),
                                                      in1=R[:, 32:36].unsqueeze(2).broadcast_to([128, 4, 4]), op=ALU.add), [brt[0], brt[6]], [brt[7]])
                yield
                P.op("dve", lambda e: e.tensor_reduce(out=R[:, 56:57], in_=R[:, 40:56], axis=AX.X, op=ALU.max), [brt[7]], [brt[8]])
                yield
                P.op("dve", lambda e: e.tensor_scalar(out=R[:, 60:76], in0=R[:, 40:56], scalar1=R[:, 56:57], scalar2=None, op0=ALU.is_ge), [brt[7], brt[8]], [brt[9]])
                yield
                P.op("dve", lambda e: e.scalar_tensor_tensor(out=R[:, 80:96], in0=R[:, 60:76], scalar=-BIG, in1=R[:, 40:56], op0=ALU.mult, op1=ALU.add),
                     [brt[9], brt[7]], [brt[10]])
                yield
                P.op("dve", lambda e: e.tensor_reduce(out=R[:, 96:97], in_=R[:, 80:96], axis=AX.X, op=ALU.max), [brt[10]], [brt[11]])
                yield
                P.op("dve", lambda e: e.tensor_scalar(out=R[:, 100:116], in0=R[:, 80:96], scalar1=R[:, 96:97], scalar2=None, op0=ALU.is_ge), [brt[10], brt[11]], [brt[12]])
                yield
                P.op("dve", lambda e: e.tensor_tensor(out=R[:, 116:117], in0=R[:, 96:97], in1=R[:, 56:57], op=ALU.subtract), [brt[11], brt[8]], [brt[13]])
                yield
                P.op("act", lambda e: e.activation(out=R[:, 117:118], in_=R[:, 116:117], func=AF.Exp), [brt[13]], [brt[14]])
                yield
                P.op("dve", lambda e: e.tensor_scalar(out=R[:, 118:119], in0=R[:, 117:118], scalar1=1.0, scalar2=None, op0=ALU.add), [brt[14]], [brt[15]])
                yield
                P.op("dve", lambda e: e.reciprocal(out=R[:, 119:120], in_=R[:, 118:119]), [brt[15]], [brt[16]])
                yield
                P.op("dve", lambda e: e.tensor_tensor(out=R[:, 120:121], in0=R[:, 119:120], in1=R[:, 27:28], op=ALU.mult), [brt[16], brt[4]], [brt[17]])
                yield
                P.op("dve", lambda e: e.tensor_tensor(out=R[:, 121:122], in0=R[:, 120:121], in1=R[:, 117:118], op=ALU.mult), [brt[17], brt[14]], [brt[18]])
                yield
                P.op("dve", lambda e: e.tensor_scalar(out=comb[:, tl, :], in0=R[:, 60:76], scalar1=R[:, 120:121], scalar2=None, op0=ALU.mult),
                     [brt[9], brt[17]], [bcomb[tl]])
                yield
                P.op("dve", lambda e: e.scalar_tensor_tensor(out=comb[:, tl, :], in0=R[:, 100:116], scalar=R[:, 121:122], in1=comb[:, tl, :], op0=ALU.mult, op1=ALU.add),
                     [brt[12], brt[18], bcomb[tl]], [bcomb[tl]])
                yield

            def fin_tile(sgi, tl):
                tile = sgi * 16 + tl
                fss, fb, _u = st["sets"].next()
                ot, bot = ots.next()
                P.op("act", lambda e: e.activation(out=ot[:], in_=xres[:, tl, :], func=AF.Square, scale=1.0 / 32, accum_out=fss[:, 0:1]), [bxr[tl]], [fb[0], bot])
                yield
                P.op("dve", lambda e: e.tensor_scalar(out=fss[:, 1:2], in0=fss[:, 0:1], scalar1=EPS, scalar2=None, op0=ALU.add), [fb[0]], [fb[1]])
                yield
                P.op("act", lambda e: e.activation(out=fss[:, 2:3], in_=fss[:, 1:2], func=AF.Ln), [fb[1]], [fb[2]])
                yield
                P.op("act", lambda e: e.activation(out=fss[:, 3:4], in_=fss[:, 2:3], func=AF.Exp, scale=-0.5), [fb[2]], [fb[3]])
                yield
                P.op("dve", lambda e: e.scalar_tensor_tensor(out=ot[:], in0=xres[:, tl, :], scalar=fss[:, 3:4], in1=gfin_bc[:], op0=ALU.mult, op1=ALU.mult),
                     [bxr[tl], fb[3], B_c], [bot])
                yield
                P.dma("sp", lambda e: e.dma_start(out=out[tile * 128:(tile + 1) * 128, :], in_=ot[:]), [bot], [])
                yield

            def load_w(e_):
                (wg, bwg), (wu, bwu), (wd, bwd) = Wg[e_ % 2], Wu[e_ % 2], Wd[e_ % 2]
                P.dma("pool", lambda e: e.dma_start(out=wg[:], in_=wv(w_gate[e_], 0, 512)), [], [bwg])
                P.dma("pool", lambda e: e.dma_start(out=wu[:], in_=wv(w_up[e_], 0, 512)), [], [bwu])
                P.dma("pool", lambda e: e.dma_start(out=wd[:], in_=wv(w_down[e_], 0, 1024)), [], [bwd])

            for sgi in range(2):
                load_w(0)
                run_pipeline((moe_tile(sgi, tl) for tl in range(16)), NP3)
                for ex in range(NE):
                    if ex + 1 < NE:
                        load_w(ex + 1)
                    (wg, bwg), (wu, bwu), (wd, bwd) = Wg[ex % 2], Wu[ex % 2], Wd[ex % 2]
                    for tg in range(4):
                        heT, bhe = heTs.next()
                        for fc in range(4):
                            g_ps, bgp = gen.next()
                            u_ps, bup = gen.next()
                            P.op("pe", [(lambda e, c=c, fc=fc, tg=tg, g_ps=g_ps, wg=wg: e.matmul(g_ps[:], wg[:, c, fc * 128:(fc + 1) * 128], hmT[:, c, tg * 512:(tg + 1) * 512],
                                                                                                 start=(c == 0), stop=(c == 7))) for c in range(8)],
                                 bhm[tg * 4:tg * 4 + 4] + [bwg], [bgp])
                            P.op("pe", [(lambda e, c=c, fc=fc, tg=tg, u_ps=u_ps, wu=wu: e.matmul(u_ps[:], wu[:, c, fc * 128:(fc + 1) * 128], hmT[:, c, tg * 512:(tg + 1) * 512],
                                                                                                 start=(c == 0), stop=(c == 7))) for c in range(8)],
                                 bhm[tg * 4:tg * 4 + 4] + [bwu], [bup])
                            sg, bsg_ = sgs.next()
                            P.op("act", lambda e, sg=sg, g_ps=g_ps: e.activation(out=sg[:], in_=g_ps[:], func=AF.Silu), [bgp], [bsg_])
                            P.op("dve", lambda e, sg=sg, u_ps=u_ps, heT=heT, fc=fc: e.tensor_tensor(out=heT[:, fc, :], in0=sg[:], in1=u_ps[:], op=ALU.mult),
                                 [bsg_, bup], [bhe[fc]])
                        for j in range(4):
                            tl = tg * 4 + j
                            for nb in range(2):
                                y_ps, byp = gen.next()
                                P.op("pe", [(lambda e, fc=fc, j=j, nb=nb, y_ps=y_ps, heT=heT, wd=wd: e.matmul(y_ps[:], heT[:, fc, j * 128:(j + 1) * 128], wd[:, fc, nb * 512:(nb + 1) * 512],
                                                                                                             start=(fc == 0), stop=(fc == 3))) for fc in range(4)],
                                     bhe + [bwd], [byp])
                                P.op("dve", lambda e, tl=tl, nb=nb, y_ps=y_ps, ex=ex: e.scalar_tensor_tensor(
                                    out=xres[:, tl, nb * 512:(nb + 1) * 512], in0=y_ps[:], scalar=comb[:, tl, ex:ex + 1], in1=xres[:, tl, nb * 512:(nb + 1) * 512],
                                    op0=ALU.mult, op1=ALU.add), [byp, bcomb[tl], bxr[tl]], [bxr[tl]])
                run_pipeline((fin_tile(sgi, tl) for tl in range(16)), 2)
            P.flush()
    return nc


def _rope_tables():
    rows = SEQ // 64
    row_ids = np.repeat(np.arange(rows), 64).astype(np.float32)
    col_ids = np.tile(np.arange(64), rows).astype(np.float32)
    inv = (10000.0 ** (-np.arange(16, dtype=np.float32) / 16)).astype(np.float32)
    ang = np.concatenate([row_ids[:, None] * inv[None, :], col_ids[:, None] * inv[None, :]], axis=-1).astype(np.float32)
    return np.cos(ang).astype(np.float32), np.sin(ang).astype(np.float32)


def make_in_maps(inputs):
    f = lambda a: np.ascontiguousarray(np.asarray(a, dtype=np.float32))
    x = f(inputs["x"])
    mem = f(inputs["mem"])
    cos, sin = _rope_tables()

    def lay(t, n):
        return np.ascontiguousarray(t.reshape(n, 128, 32).transpose(1, 0, 2))

    shared = {
        "ident": np.eye(128, dtype=np.float32),
        "g_mix": f(inputs["g_mix"][0]), "w_in": f(inputs["w_in"][0]), "g_q": f(inputs["g_q"][0]), "g_k": f(inputs["g_k"][0]),
        "w_att_out": f(inputs["w_att_out"][0]), "sgu_ln_g": f(inputs["sgu_ln_g"][0]), "sgu_ln_b": f(inputs["sgu_ln_b"][0]),
        "w_s": f(inputs["w_s"][0]), "b_s": f(inputs["b_s"][0]), "w_sgu_out": f(inputs["w_sgu_out"][0]), "w_out": f(inputs["w_out"][0]),
        "g_cross": f(inputs["g_cross"][0]), "g_mem": f(inputs["g_mem"][0]), "w_cq": f(inputs["w_cq"][0]), "w_ckv": f(inputs["w_ckv"][0]),
        "w_co": f(inputs["w_co"][0]), "g_moe": f(inputs["g_moe"][0]), "w_rg": f(inputs["w_rg"][0]), "b_rg": f(inputs["b_rg"][0]),
        "w_re": f(inputs["w_re"][0]), "b_re": f(inputs["b_re"][0]), "w_gate": f(inputs["w_gate"][0]), "w_up": f(inputs["w_up"][0]),
        "w_down": f(inputs["w_down"][0]), "g_final": f(inputs["g_final"]),
        "cos_seq": lay(cos, 64), "sin_seq": lay(sin, 64),
    }
    maps = []
    for c in range(8):
        b, hf = c // 2, c % 2
        m = dict(shared)
        m["xseq"] = x[b]
        m["xown"] = np.ascontiguousarray(x[b, hf * OWN:(hf + 1) * OWN])
        m["mem"] = mem[b]
        m["cos_own"] = lay(cos[hf * OWN:(hf + 1) * OWN], 32)
        m["sin_own"] = lay(sin[hf * OWN:(hf + 1) * OWN], 32)
        maps.append(m)
    return maps


def kernel(**inputs):
    nc = build()
    maps = make_in_maps(inputs)
    res = run_bass_kernel_spmd(nc, maps, core_ids=list(range(8)))
    outp = np.empty((4, SEQ, D), np.float32)
    for c in range(8):
        b, hf = c // 2, c % 2
        outp[b, hf * OWN:(hf + 1) * OWN] = np.asarray(res.results[c]["out"], dtype=np.float32)
    return outp
```

```python
import contextlib
import os as _os0
import numpy as np
import concourse.bass as bass
import concourse.mybir as mybir
from concourse.bass_utils import run_bass_kernel_spmd

F32 = mybir.dt.float32
BF16 = mybir.dt.bfloat16
AF = mybir.ActivationFunctionType
ALU = mybir.AluOpType
AX = mybir.AxisListType

D = 1024
SEQ = 8192
OWN = 4096
NMEM = 256
EPS = 1e-6
NE = 16
BIG = 1.0e30


class Buf:
    __slots__ = ("name", "w", "r", "psum")

    def __init__(self, name="", psum=False):
        self.name = name
        self.psum = psum
        self.w = None
        self.r = {}


class Eng:
    def __init__(self, name):
        self.name = name
        self.sem = None
        self.count = 0
        self.seen = {}
        self.prog = []
        self.chsems = []
        self.dma_i = 0


class Prog:
    NCH = int(_os0.environ.get('NCH', 8))

    def __init__(self, nc, stack):
        self.nc = nc
        self.E = {}
        for n in ("pe", "act", "dve", "pool", "sp"):
            e = Eng(n)
            e.sem = stack.enter_context(nc.semaphore("sem_" + n))
            self.E[n] = e
        for n in ("sp", "pool", "act"):
            e = self.E[n]
            for i in range(self.NCH):
                e.chsems.append(stack.enter_context(nc.semaphore("ch_%s_%d" % (n, i))))
        self.chan_issued = {}
        import os
        self.limit = int(os.environ.get("OPLIMIT", 10 ** 9))
        self.nops = 0

    def _need(self, need, ev):
        sem, val, src = ev
        k = id(sem)
        if k not in need or need[k][1] < val:
            need[k] = (sem, val, src)

    def _deps(self, eng, reads, writes, extra=()):
        E = self.E[eng]
        need = {}
        for b in reads:
            if b.w is not None:
                self._need(need, b.w)
        for b in writes:
            if b.w is not None:
                self._need(need, b.w)
            for k, (sem, val, src) in b.r.items():
                self._need(need, (sem, val, src))
        for ev in extra:
            self._need(need, ev)
        for k, (sem, val, src) in need.items():
            if src == "pe" and eng == "pe":
                continue
            if E.seen.get(k, 0) >= val:
                continue
            E.seen[k] = val
            E.prog.append(("wait", sem, val))

    def _commit(self, ev, reads, writes):
        sem, val, src = ev
        for b in writes:
            b.w = ev
            b.r = {}
        for b in reads:
            if b not in writes:
                b.r[id(sem)] = (sem, val, src)

    def op(self, eng, fns, reads=(), writes=()):
        if not isinstance(fns, (list, tuple)):
            fns = [fns]
        self.nops += 1
        if self.nops > self.limit:
            return
        pr = [b for b in reads if b.psum]
        if pr:
            reads = [b for b in reads if not b.psum]
            writes = list(writes) + [b for b in pr if b not in writes]
        E = self.E[eng]
        self._deps(eng, reads, writes)
        E.count += 1
        for f in fns[:-1]:
            E.prog.append(("raw", f))
        E.prog.append(("op", fns[-1], E.sem))
        self._commit((E.sem, E.count, eng), reads, writes)

    def dma(self, q, fn, reads=(), writes=()):
        self.nops += 1
        if self.nops > self.limit:
            return
        E = self.E[q]
        ch = E.dma_i % self.NCH
        rnd = E.dma_i // self.NCH
        E.dma_i += 1
        chsem = E.chsems[ch]
        extra = []
        if rnd > 0:
            extra.append((chsem, 16 * rnd, "dma"))
        self._deps(q, reads, writes, extra)
        E.prog.append(("dma", fn, chsem))
        self.chan_issued[id(chsem)] = (chsem, 16 * (rnd + 1))
        self._commit((chsem, 16 * (rnd + 1), "dma"), reads, writes)

    def barrier(self):
        for n, E in self.E.items():
            for m, F in self.E.items():
                if m == n or F.count == 0:
                    continue
                k = id(F.sem)
                if E.seen.get(k, 0) < F.count:
                    E.seen[k] = F.count
                    E.prog.append(("wait", F.sem, F.count))
            for k, (sem, val) in self.chan_issued.items():
                if E.seen.get(k, 0) < val:
                    E.seen[k] = val
                    E.prog.append(("wait", sem, val))

    def flush(self):
        self.barrier()
        nc = self.nc
        progs = {n: E.prog for n, E in self.E.items()}
        for E in self.E.values():
            E.prog = []

        def replay(eng, prog):
            for it in prog:
                if it[0] == "wait":
                    eng.wait_ge(it[1], it[2])
                elif it[0] == "raw":
                    it[1](eng)
                elif it[0] == "op":
                    it[1](eng).then_inc(it[2], 1)
                else:
                    it[1](eng).then_inc(it[2], 16)

        with nc.Block() as block:
            @block.tensor
            def _(e):
                replay(e, progs["pe"])

            @block.scalar
            def _(e):
                replay(e, progs["act"])

            @block.vector
            def _(e):
                replay(e, progs["dve"])

            @block.gpsimd
            def _(e):
                replay(e, progs["pool"])

            @block.sync
            def _(e):
                replay(e, progs["sp"])


class Rot:
    def __init__(self, items):
        self.items = items
        self.i = 0

    def next(self):
        it = self.items[self.i % len(self.items)]
        self.i += 1
        return it


class Pool:
    def __init__(self, items):
        self.free = list(items)

    def acquire(self):
        return self.free.pop(0) if self.free else None

    def release(self, it):
        self.free.append(it)


def build(dbg=False):
    nc = bass.Bass("TRN2", target_bir_lowering=False)

    def din(name, shape):
        return nc.dram_tensor(name, list(shape), F32, kind="ExternalInput").ap()

    xseq = din("xseq", [SEQ, D])
    xown = din("xown", [OWN, D])
    memd = din("mem", [NMEM, D])
    cos_seq = din("cos_seq", [128, 64, 32])
    sin_seq = din("sin_seq", [128, 64, 32])
    cos_own = din("cos_own", [128, 32, 32])
    sin_own = din("sin_own", [128, 32, 32])
    identd = din("ident", [128, 128])
    g_mix = din("g_mix", [D])
    w_in = din("w_in", [D, 3840])
    g_q = din("g_q", [64])
    g_k = din("g_k", [64])
    w_att_out = din("w_att_out", [512, D])
    sgu_ln_g = din("sgu_ln_g", [512])
    sgu_ln_b = din("sgu_ln_b", [512])
    w_s = din("w_s", [4, 128, 128])
    b_s = din("b_s", [4, 128])
    w_sgu_out = din("w_sgu_out", [512, D])
    w_out = din("w_out", [D, D])
    g_cross = din("g_cross", [D])
    g_mem = din("g_mem", [D])
    w_cq = din("w_cq", [D, 512])
    w_ckv = din("w_ckv", [D, D])
    w_co = din("w_co", [512, D])
    g_moe = din("g_moe", [D])
    w_rg = din("w_rg", [D, 4])
    b_rg = din("b_rg", [4])
    w_re = din("w_re", [D, 16])
    b_re = din("b_re", [16])
    wcat = din("wcat", [NE * 128, 12288])
    pidxd = din("pidx", [128, 1])
    g_final = din("g_final", [D])
    out = nc.dram_tensor("out", [OWN, D], F32, kind="ExternalOutput").ap()
    yatt_d = nc.dram_tensor("yatt_scr", [8, 128, 8, 512], F32, kind="Internal").ap()
    x2_d = nc.dram_tensor("x2_scr", [OWN, D], F32, kind="Internal").ap()

    def wv(w, c0, c1):
        return w.rearrange("(c p) n -> p c n", p=128)[:, :, c0:c1]

    def gT_view(g):
        return g.rearrange("(c p) -> p c", p=128)

    with contextlib.ExitStack() as top:
        P = Prog(nc, top)
        print("sbuf bytes remaining at start:", nc.sbuf_bytes_remaining)

        def sb(stack, name, shape, dt=F32):
            return stack.enter_context(nc.sbuf_tensor("s_" + name, list(shape), dt))

        def ps(stack, name, shape, dt=F32):
            return stack.enter_context(nc.psum_tensor("p_" + name, list(shape), dt))

        ident = sb(top, "ident", [128, 128], BF16)
        kcT = sb(top, "kcT", [128, 4, NMEM], BF16)
        Vc = sb(top, "Vc", [128, 2, 512], BF16)
        gmixT = sb(top, "gmixT", [128, 8])
        attn_stack = contextlib.ExitStack()
        kT_all = sb(attn_stack, "kT_all", [128, SEQ], BF16)
        V_all = sb(attn_stack, "V_all", [128, 64, 2, 65], BF16)
        B_const = Buf("const")

        P.dma("pool", lambda e: e.dma_start(out=ident[:], in_=identd[:, :]), [], [B_const])
        P.dma("sp", lambda e: e.dma_start(out=gmixT[:], in_=gT_view(g_mix), allow_slow_non_contiguous=True), [], [B_const])
        P.op("pool", lambda e: e.memset(V_all[:], 1.0), [], [B_const])

        def run_pipeline(gens, depth):
            it = iter(gens)
            active = []
            more = True
            while True:
                if more and len(active) < depth:
                    try:
                        active.append(next(it))
                    except StopIteration:
                        more = False
                if not active:
                    break
                for g in list(active):
                    try:
                        next(g)
                    except StopIteration:
                        active.remove(g)

        def drain(g):
            for _ in g:
                pass

        def acq(pool):
            while True:
                it = pool.acquire()
                if it is not None:
                    return it
                yield

        def bfview(bank_ap):
            return bank_ap.bitcast(BF16).rearrange("p (c t) -> p c t", c=8)

        def norm_T_gen(st, xt_ap, xbuf, gT, out_ap3, outbuf, tps, tpsb, pool=None, tok=None):
            ss, b, _unused = st["sets"].next()
            xb, bxb = st["xb"].next()
            P.op("act", lambda e: e.activation(out=xb[:], in_=xt_ap, func=AF.Square, scale=1.0 / 32,
                                               accum_out=ss[:, 0:1]), [xbuf], [b[0], bxb])
            yield
            P.op("dve", lambda e: e.tensor_scalar(out=ss[:, 1:2], in0=ss[:, 0:1], scalar1=EPS, scalar2=None,
                                                  op0=ALU.add), [b[0]], [b[1]])
            yield
            P.op("act", lambda e: e.activation(out=ss[:, 2:3], in_=ss[:, 1:2], func=AF.Ln), [b[1]], [b[2]])
            yield
            P.op("act", lambda e: e.activation(out=ss[:, 3:4], in_=ss[:, 2:3], func=AF.Exp, scale=-0.5), [b[2]], [b[3]])
            yield
            P.op("act", lambda e: e.activation(out=xb[:], in_=xt_ap, func=AF.Copy, scale=ss[:, 3:4]), [xbuf, b[3]], [bxb])
            yield
            if tok is not None:
                P.op("dve", lambda e: e.tensor_tensor(out=tok[0], in0=xb[:], in1=tok[1], op=ALU.mult), [bxb, tok[2]], [tok[3]])
                yield
            held = None
            if pool is not None:
                while held is None:
                    held = pool.acquire()
                    if held is None:
                        yield
                tps, tpsb = bfview(held[0][:]), held[1]
            P.op("pe", [(lambda e, c=c: e.transpose(tps[:, c, :], xb[:, c * 128:(c + 1) * 128], ident[:])) for c in range(8)],
                 [bxb], [tpsb])
            yield
            P.op("dve", lambda e: e.tensor_tensor(out=out_ap3, in0=tps, in1=gT[:, 0:8].unsqueeze(2).broadcast_to([128, 8, 128]),
                                                  op=ALU.mult), [tpsb], [outbuf])
            if held is not None:
                pool.release(held)
            yield

        def norm_T(st, xt_ap, xbuf, gT, out_ap3, outbuf, tps, tpsb):
            drain(norm_T_gen(st, xt_ap, xbuf, gT, out_ap3, outbuf, tps[:], tpsb))

        def norm_state(stack, pfx, nsets=2, nxb=2):
            st = {}
            st["sets"] = Rot([(sb(stack, pfx + "ss%d" % i, [128, 4]), [Buf("ss") for _ in range(4)], None) for i in range(nsets)])
            st["xb"] = Rot([(sb(stack, pfx + "xb%d" % i, [128, 1024], BF16), Buf("xb")) for i in range(nxb)])
            return st

        def head_norm_rope_gen(ws, src_ps, bsrc, H, g_bc, cosb, sinb, dst, bdst, t):
            if isinstance(ws, Pool):
                w = yield from acq(ws)
            else:
                w = ws.next()
            n = H * 64
            src3 = src_ps.rearrange("p (h d) -> p h d", h=H)
            P.op("act", lambda e: e.activation(out=w["sq"][:, 0:n], in_=src_ps, func=AF.Square, scale=0.125), [bsrc], [w["bsq"]])
            yield
            P.op("dve", lambda e: e.tensor_reduce(out=w["hs"][:, 0:H], in_=w["sq"][:, 0:n].rearrange("p (h d) -> p h d", h=H),
                                                  axis=AX.X, op=ALU.add), [w["bsq"]], [w["bhs"]])
            yield
            P.op("dve", lambda e: e.tensor_scalar(out=w["hs"][:, 8:8 + H], in0=w["hs"][:, 0:H], scalar1=EPS, scalar2=None, op0=ALU.add),
                 [w["bhs"]], [w["bhs1"]])
            yield
            P.op("act", lambda e: e.activation(out=w["hs"][:, 16:16 + H], in_=w["hs"][:, 8:8 + H], func=AF.Ln), [w["bhs1"]], [w["bhs2"]])
            yield
            P.op("act", lambda e: e.activation(out=w["hs"][:, 24:24 + H], in_=w["hs"][:, 16:16 + H], func=AF.Exp, scale=-0.5),
                 [w["bhs2"]], [w["bhs3"]])
            yield
            qn3 = w["qn"][:, 0:n].rearrange("p (h d) -> p h d", h=H)
            P.op("dve", lambda e: e.tensor_tensor(out=qn3, in0=src3, in1=w["hs"][:, 24:24 + H].unsqueeze(2).broadcast_to([128, H, 64]),
                                                  op=ALU.mult), [bsrc, w["bhs3"]], [w["bqn"]])
            yield
            P.op("dve", lambda e: e.tensor_tensor(out=qn3, in0=qn3, in1=g_bc[:, :].unsqueeze(1).broadcast_to([128, H, 64]),
                                                  op=ALU.mult), [w["bqn"]], [w["bqn"]])
            yield
            qn4 = w["qn"][:, 0:n].rearrange("p (h i t) -> p h i t", h=H, t=2)
            A4 = w["A"][:, 0:n].rearrange("p (h i t) -> p h i t", h=H, t=2)
            B4 = w["B"][:, 0:n].rearrange("p (h i t) -> p h i t", h=H, t=2)
            c4 = cosb[:, t, :].unsqueeze(1).unsqueeze(3).broadcast_to([128, H, 32, 2])
            s4 = sinb[:, t, :].unsqueeze(1).unsqueeze(3).broadcast_to([128, H, 32, 2])
            P.op("dve", lambda e: e.tensor_tensor(out=A4, in0=qn4, in1=c4, op=ALU.mult), [w["bqn"]], [w["bA"]])
            yield
            P.op("dve", lambda e: e.tensor_tensor(out=B4, in0=qn4, in1=s4, op=ALU.mult), [w["bqn"]], [w["bB"]])
            yield
            P.op("dve", lambda e: e.tensor_tensor(out=dst[:, :, :, 0], in0=A4[:, :, :, 0], in1=B4[:, :, :, 1], op=ALU.subtract),
                 [w["bA"], w["bB"]], [bdst])
            yield
            P.op("dve", lambda e: e.tensor_tensor(out=dst[:, :, :, 1], in0=B4[:, :, :, 0], in1=A4[:, :, :, 1], op=ALU.add),
                 [w["bA"], w["bB"]], [bdst])
            if isinstance(ws, Pool):
                ws.release(w)
            yield

        def rope_state(stack, pfx, n, nsets=1):
            sets = []
            for i in range(nsets):
                w = {}
                for k in ("sq", "qn", "A", "B"):
                    w[k] = sb(stack, "%s%s%d" % (pfx, k, i), [128, n])
                w["hs"] = sb(stack, "%shs%d" % (pfx, i), [128, 32])
                for k in ("bsq", "bhs", "bhs1", "bhs2", "bhs3", "bqn", "bA", "bB"):
                    w[k] = Buf(k)
                sets.append(w)
            return Rot(sets)

        with contextlib.ExitStack() as ph:
            wkv = sb(ph, "wkv", [128, 8, 256], BF16)
            cosb = sb(ph, "cosb", [128, 64, 32])
            sinb = sb(ph, "sinb", [128, 64, 32])
            gk_bc = sb(ph, "gk_bc", [128, 64])
            P.dma("pool", lambda e: e.dma_start(out=wkv[:], in_=wv(w_in, 512, 768)), [], [B_const])
            P.dma("sp", lambda e: e.dma_start(out=cosb[:], in_=cos_seq[:, :, :]), [], [B_const])
            P.dma("sp", lambda e: e.dma_start(out=sinb[:], in_=sin_seq[:, :, :]), [], [B_const])
            P.dma("sp", lambda e: e.dma_start(out=gk_bc[:], in_=g_k.partition_broadcast(128)), [], [B_const])
            NP1 = 8
            P.barrier()
            st = norm_state(ph, "p1", nsets=NP1, nxb=NP1)
            rws = rope_state(ph, "p1r", 128, nsets=NP1)
            xts = Rot([(sb(ph, "p1x%d" % i, [128, 1024]), Buf("x")) for i in range(NP1)])
            hTs = Rot([(sb(ph, "p1h%d" % i, [128, 8, 128], BF16), Buf("h")) for i in range(NP1)])
            krs = Rot([(sb(ph, "p1kr%d" % i, [128, 2, 32, 2], BF16), Buf("kr")) for i in range(NP1)])
            banks = Pool([(ps(ph, "p1b%d" % i, [128, 512]), Buf("bank", True)) for i in range(8)])

            import os as _os

            def p1_tile(t):
                xt, bx = xts.next()
                P.dma("sp", lambda e: e.dma_start(out=xt[:], in_=xseq[t * 128:(t + 1) * 128, :]), [], [bx])
                yield
                hT, bh = hTs.next()
                yield from norm_T_gen(st, xt[:], bx, gmixT, hT[:], bh, None, None, pool=banks)
                hkv = yield from acq(banks)
                kv, bkv = hkv
                P.op("pe", [(lambda e, c=c: e.matmul(kv[:, 0:256], hT[:, c, :], wkv[:, c, :], start=(c == 0), stop=(c == 7)))
                            for c in range(8)], [bh, B_const], [bkv])
                yield
                P.op("act", lambda e: e.activation(out=V_all[:, t, :, 0:64], in_=kv[:, 128:256].rearrange("p (h d) -> p h d", h=2),
                                                   func=AF.Copy), [bkv], [])
                yield
                kr, bkr = krs.next()
                yield from head_norm_rope_gen(rws, kv[:, 0:128], bkv, 2, gk_bc, cosb, sinb, kr, bkr, t)
                banks.release(hkv)
                hkt = yield from acq(banks)
                kt, bkt = hkt
                ktv = kt[:].bitcast(BF16)
                P.op("pe", lambda e: e.transpose(ktv[:, 0:128], kr[:].rearrange("p h i t -> p (h i t)"), ident[:]), [bkr], [bkt])
                yield
                P.op("act", lambda e: e.activation(out=kT_all[:, t * 128:(t + 1) * 128], in_=ktv[:, 0:128], func=AF.Copy), [bkt], [])
                banks.release(hkt)
                yield

            run_pipeline((p1_tile(t) for t in range(int(_os.environ.get('NT1', 64)))), int(_os.environ.get('DEPTH1', 8)))
            P.flush()

        if dbg == 1:
            P.limit = 10 ** 9
            P.limit = 10 ** 9
            print("nops", P.nops)
            dk = nc.dram_tensor("dbg_kT", [128, SEQ], BF16, kind="ExternalOutput").ap()
            dv = nc.dram_tensor("dbg_V", [128, 64 * 2 * 65], BF16, kind="ExternalOutput").ap()
            P.dma("sp", lambda e: e.dma_start(out=dk[:, :], in_=kT_all[:]), [], [])
            P.dma("sp", lambda e: e.dma_start(out=dv[:, :], in_=V_all[:].rearrange("p a b c -> p (a b c)")), [], [])
            P.flush()
            attn_stack.close()
            return nc

        with contextlib.ExitStack() as ph:
            wq = sb(ph, "wq", [128, 8, 512], BF16)
            wao = sb(ph, "wao", [64, 8, 1024], BF16)
            cosb = sb(ph, "cosb2", [128, 32, 32])
            sinb = sb(ph, "sinb2", [128, 32, 32])
            gq_bc = sb(ph, "gq_bc", [128, 64])
            ones65 = sb(ph, "ones65", [65, 64])
            B_c2 = Buf("c2")
            for hd_ in range(8):
                pos_ = (hd_ % 4) * 2 + hd_ // 4
                P.dma("pool", lambda e, hd_=hd_, pos_=pos_: e.dma_start(out=wq[:, :, pos_ * 64:(pos_ + 1) * 64], in_=wv(w_in, hd_ * 64, hd_ * 64 + 64)), [], [B_c2])
            P.dma("pool", lambda e: e.dma_start(out=wao[:], in_=w_att_out.rearrange("(h p) n -> p h n", p=64)), [], [B_c2])
            P.dma("sp", lambda e: e.dma_start(out=cosb[:], in_=cos_own[:, :, :]), [], [B_c2])
            P.dma("sp", lambda e: e.dma_start(out=sinb[:], in_=sin_own[:, :, :]), [], [B_c2])
            P.dma("sp", lambda e: e.dma_start(out=gq_bc[:], in_=g_q.partition_broadcast(128)), [], [B_c2])
            P.op("dve", lambda e: e.tensor_scalar(out=gq_bc[:], in0=gq_bc[:], scalar1=0.125, scalar2=None, op0=ALU.mult), [B_c2], [B_c2])
            P.op("pool", lambda e: e.memset(ones65[:], 1.0), [], [B_c2])
            P.barrier()
            st = norm_state(ph, "p2", nsets=4, nxb=4)
            rws = Pool(rope_state(ph, "p2r", 512, nsets=3).items)
            xts = Rot([(sb(ph, "p2x%d" % i, [128, 1024]), Buf("x")) for i in range(4)])
            hTs = Rot([(sb(ph, "p2h%d" % i, [128, 8, 128], BF16), Buf("h")) for i in range(4)])
            qrs = Rot([(sb(ph, "p2qr%d" % i, [128, 8, 32, 2], BF16), Buf("qr")) for i in range(4)])
            Ps = Rot([(sb(ph, "p2P%d" % i, [128, 2, 512], BF16), Buf("P")) for i in range(3)])
            OnTs = Rot([(sb(ph, "p2OnT%d" % i, [64, 8, 512], BF16), [Buf("OnT") for _ in range(8)]) for i in range(2)])
            ysbs = Rot([(sb(ph, "p2ysb%d" % i, [128, 4, 512]), [Buf("ysb") for _ in range(4)]) for i in range(1)])
            Sps = Rot([(ps(ph, "p2S%d" % i, [128, 2, 512]), Buf("S", True)) for i in range(3)])
            Ops, bO = ps(ph, "p2O", [128, 2, 512]), [Buf("O0", True), Buf("O1", True)]
            LA = int(_os0.environ.get('LA', 2))

            sbank = Pool([(Sps.items[i][0][:, bnk, :], Buf("sbank", True)) for i in range(3) for bnk in range(2)])
            Osbs = Rot([(sb(ph, "p2Osc%d" % i, [65, 512]), Buf("Osb")) for i in range(4)])
            rrs = Rot([(sb(ph, "p2rrc%d" % i, [65, 512]), Buf("rr")) for i in range(4)])

            qT_all = sb(ph, "qT_all", [128, 4, OWN], BF16)
            bqTt = [Buf("qTt") for _ in range(32)]
            print("sbuf bytes remaining in 2a:", nc.sbuf_bytes_remaining)

            def q_tile(tile):
                xt, bx = xts.next()
                P.dma("sp", lambda e: e.dma_start(out=xt[:], in_=xown[tile * 128:(tile + 1) * 128, :]), [], [bx])
                yield
                hT, bh = hTs.next()
                yield from norm_T_gen(st, xt[:], bx, gmixT, hT[:], bh, None, None, pool=sbank)
                hq = yield from acq(sbank)
                qps, bqp = hq
                P.op("pe", [(lambda e, c=c: e.matmul(qps, hT[:, c, :], wq[:, c, :], start=(c == 0), stop=(c == 7)))
                            for c in range(8)], [bh, B_c2], [bqp])
                yield
                qr, bqr = qrs.next()
                yield from head_norm_rope_gen(rws, qps, bqp, 8, gq_bc, cosb, sinb, qr, bqr, tile)
                sbank.release(hq)
                ht = yield from acq(sbank)
                tpv = bfview(ht[0])
                qr3 = qr[:].rearrange("p h i t -> p (h i t)")
                P.op("pe", [(lambda e, g=g: e.transpose(tpv[:, g, :], qr3[:, g * 128:(g + 1) * 128], ident[:])) for g in range(4)],
                     [bqr], [ht[1]])
                yield
                P.op("act", lambda e: e.activation(out=qT_all[:, :, tile * 128:(tile + 1) * 128], in_=tpv[:, 0:4, :], func=AF.Copy),
                     [ht[1]], [bqTt[tile]])
                sbank.release(ht)
                yield

            spool = Pool(list(Sps.items))

            def epi_unit(item, bnk):
                (Osb, bOsb, rr, brr, OnT, bOn, head) = item
                hS = spool.acquire()
                assert hS is not None
                mS, bmS = hS
                P.op("pe", lambda e: e.matmul(mS[0:64, bnk, :], ones65[64:65, 0:64], rr[64:65, :], start=True, stop=True), [brr, B_c2], [bmS])
                P.op("dve", lambda e: e.tensor_tensor(out=OnT[:, head, :], in0=Osb[0:64, :], in1=mS[0:64, bnk, :], op=ALU.mult),
                     [bOsb, bmS], [bOn[head]])
                spool.release(hS)

            def y_unit(gi, dc, OnT, bOn, ysb, bys):
                hS = spool.acquire()
                assert hS is not None
                mS, bmS = hS
                P.op("pe", [(lambda e, h=h: e.matmul(mS[:, 0, :], wao[:, h, dc * 128:(dc + 1) * 128], OnT[:, h, :], start=(h == 0), stop=(h == 7)))
                            for h in range(8)], bOn + [B_c2], [bmS])
                P.op("dve", lambda e: e.tensor_copy(out=ysb[:, dc % 4, :], in_=mS[:, 0, :]), [bmS], [bys[dc % 4]])
                spool.release(hS)
                if dc % 4 == 3:
                    P.dma("sp", lambda e: e.dma_start(out=yatt_d[gi][:, dc - 3:dc + 1, :], in_=ysb[:]), bys, [])

            run_pipeline((q_tile(t) for t in range(32)), 4)
            P.barrier()
            units = []
            for gi in range(8):
                OnT, bOn = OnTs.next()
                bqT = bqTt[gi * 4:gi * 4 + 4]
                for g in range(4):
                    pend = []

                    def try_issue(kt, g=g, gi=gi, bqT=bqT, pend=pend):
                        hS = spool.acquire()
                        if hS is None:
                            return False
                        S, bS = hS
                        P.op("pe", [lambda e: e.matmul(S[:, 0, :], kT_all[0:64, kt * 128:(kt + 1) * 128], qT_all[0:64, g, gi * 512:(gi + 1) * 512], start=True, stop=True),
                                    lambda e: e.matmul(S[:, 1, :], kT_all[64:128, kt * 128:(kt + 1) * 128], qT_all[64:128, g, gi * 512:(gi + 1) * 512], start=True, stop=True)],
                             bqT, [bS])
                        pend.append(hS)
                        return True
                    nxt = 0
                    for kt in range(64):
                        while nxt < 64 and nxt <= kt + LA and try_issue(nxt):
                            nxt += 1
                        hS = pend.pop(0)
                        S, bS = hS
                        Pt, bP = Ps.next()
                        P.op("act", lambda e, S=S, Pt=Pt: e.activation(out=Pt[:], in_=S[:], func=AF.Exp), [bS], [bP])
                        spool.release(hS)
                        if units and kt >= 3 and kt % 3 == 0:
                            units.pop(0)()
                        P.op("pe", [lambda e, Pt=Pt, kt=kt: e.matmul(Ops[0:65, 0, :], V_all[:, kt, 0, :], Pt[:, 0, :], start=(kt == 0), stop=(kt == 63)),
                                    lambda e, Pt=Pt, kt=kt: e.matmul(Ops[0:65, 1, :], V_all[:, kt, 1, :], Pt[:, 1, :], start=(kt == 0), stop=(kt == 63))],
                             [bP], bO)
                    for hh in range(2):
                        head = g + 4 * hh
                        Osb, bOsb = Osbs.next()
                        rr, brr = rrs.next()
                        P.op("act", lambda e, Osb=Osb, hh=hh: e.activation(out=Osb[:], in_=Ops[0:65, hh, :], func=AF.Copy), [bO[hh]], [bOsb])
                        P.op("dve", lambda e, Osb=Osb, rr=rr: e.reciprocal(out=rr[64:65, :], in_=Osb[64:65, :]), [bOsb], [brr])
                        item = (Osb, bOsb, rr, brr, OnT, bOn, head)
                        units.append(lambda item=item, hh=hh: epi_unit(item, hh))
                ysb, bys = ysbs.next()
                for dc in range(8):
                    units.append(lambda gi=gi, dc=dc, OnT=OnT, bOn=bOn, ysb=ysb, bys=bys: y_unit(gi, dc, OnT, bOn, ysb, bys))
            while units:
                units.pop(0)()
            P.flush()

        attn_stack.close()
        if dbg == 2:
            dy = nc.dram_tensor("dbg_yatt", [8, 128, 8, 512], F32, kind="ExternalOutput").ap()
            P.dma("sp", lambda e: e.dma_start(out=dy.rearrange("a p c t -> (a p) (c t)"), in_=yatt_d.rearrange("a p c t -> (a p) (c t)")), [], [])
            P.flush()
            return nc

        with contextlib.ExitStack() as ph:
            wckv = sb(ph, "wckv", [128, 8, 1024], BF16)
            gmemT = sb(ph, "gmemT", [128, 8])
            memT = sb(ph, "memT", [128, 8, 256], BF16)
            B_cm = Buf("cm")
            P.dma("pool", lambda e: e.dma_start(out=wckv[:], in_=wv(w_ckv, 0, 1024)), [], [B_cm])
            P.dma("sp", lambda e: e.dma_start(out=gmemT[:], in_=gT_view(g_mem), allow_slow_non_contiguous=True), [], [B_cm])
            st = norm_state(ph, "pm")
            xts = Rot([(sb(ph, "pmx%d" % i, [128, 1024]), Buf("x")) for i in range(2)])
            tp, btp = ps(ph, "pmtp", [128, 8, 128], BF16), Buf("tp", True)
            mps = Rot([(ps(ph, "pmps%d" % i, [128, 512]), Buf("mps", True)) for i in range(2)])
            bmemT = [Buf("memT0"), Buf("memT1")]
            for mt in range(2):
                xt, bx = xts.next()
                P.dma("sp", lambda e, xt=xt, mt=mt: e.dma_start(out=xt[:], in_=memd[mt * 128:(mt + 1) * 128, :]), [], [bx])
                norm_T(st, xt[:], bx, gmemT, memT[:, :, mt * 128:(mt + 1) * 128], bmemT[mt], tp, btp)
            for h in range(4):
                mp, bmp = mps.next()
                P.op("pe", [(lambda e, c=c, h=h, mp=mp: e.matmul(mp[:, 0:256], wckv[:, c, h * 128:(h + 1) * 128], memT[:, c, :], start=(c == 0), stop=(c == 7)))
                            for c in range(8)], bmemT + [B_cm], [bmp])
                P.op("act", lambda e, h=h, mp=mp: e.activation(out=kcT[:, h, :], in_=mp[:, 0:256], func=AF.Copy), [bmp], [])
            for mt in range(2):
                mp, bmp = mps.next()
                P.op("pe", [(lambda e, c=c, mt=mt, mp=mp: e.matmul(mp[:], memT[:, c, mt * 128:(mt + 1) * 128], wckv[:, c, 512:1024], start=(c == 0), stop=(c == 7)))
                            for c in range(8)], bmemT + [B_cm], [bmp])
                P.op("act", lambda e, mt=mt, mp=mp: e.activation(out=Vc[:, mt, :], in_=mp[:], func=AF.Copy), [bmp], [])
            P.flush()

        with contextlib.ExitStack() as ph:
            wsv = sb(ph, "wsv", [128, 8, 512], BF16)
            wsu = sb(ph, "wsu", [128, 8, 512], BF16)
            wga = sb(ph, "wga", [128, 8, 1024], BF16)
            wgb = sb(ph, "wgb", [128, 8, 1024], BF16)
            wso = sb(ph, "wso", [128, 4, 1024], BF16)
            wo = sb(ph, "wo", [128, 8, 1024], BF16)
            wcq = sb(ph, "wcq", [128, 8, 512], BF16)
            wco = sb(ph, "wco", [128, 4, 1024], BF16)
            gcrT = sb(ph, "gcrT", [128, 8])
            lng_bc = sb(ph, "lng_bc", [128, 512])
            lnb_bc = sb(ph, "lnb_bc", [128, 512])
            bs_bc = sb(ph, "bs_bc", [128, 4, 128])
            wsraw = sb(ph, "wsraw", [128, 4, 128], BF16)
            WsT = sb(ph, "WsT", [128, 4, 128], BF16)
            B_c = Buf("c2b")
            B_ws = Buf("ws")
            P.dma("pool", lambda e: e.dma_start(out=wsu[:], in_=wv(w_in, 768, 1280)), [], [B_c])
            P.dma("pool", lambda e: e.dma_start(out=wsv[:], in_=wv(w_in, 1280, 1792)), [], [B_c])
            P.dma("pool", lambda e: e.dma_start(out=wga[:], in_=wv(w_in, 1792, 2816)), [], [B_c])
            P.dma("pool", lambda e: e.dma_start(out=wgb[:], in_=wv(w_in, 2816, 3840)), [], [B_c])
            P.dma("pool", lambda e: e.dma_start(out=wso[:], in_=wv(w_sgu_out, 0, 1024)), [], [B_c])
            P.dma("pool", lambda e: e.dma_start(out=wo[:], in_=wv(w_out, 0, 1024)), [], [B_c])
            P.dma("pool", lambda e: e.dma_start(out=wcq[:], in_=wv(w_cq, 0, 512)), [], [B_c])
            P.dma("pool", lambda e: e.dma_start(out=wco[:], in_=wv(w_co, 0, 1024)), [], [B_c])
            P.dma("pool", lambda e: e.dma_start(out=wsraw[:], in_=w_s.rearrange("g p q -> p g q")), [], [B_ws])
            P.dma("sp", lambda e: e.dma_start(out=gcrT[:], in_=gT_view(g_cross), allow_slow_non_contiguous=True), [], [B_c])
            P.dma("sp", lambda e: e.dma_start(out=lng_bc[:], in_=sgu_ln_g.partition_broadcast(128)), [], [B_c])
            P.dma("sp", lambda e: e.dma_start(out=lnb_bc[:], in_=sgu_ln_b.partition_broadcast(128)), [], [B_c])
            P.dma("sp", lambda e: e.dma_start(out=bs_bc[:].rearrange("p g q -> p (g q)"), in_=b_s.rearrange("g q -> (g q)").partition_broadcast(128)), [], [B_c])
            P.barrier()
            st = norm_state(ph, "p3", nsets=4, nxb=4)
            bpool = Pool([(ps(ph, "p3g%d" % i, [128, 512]), Buf("g", True)) for i in range(8)])
            hb0 = bpool.acquire()
            tpb0 = bfview(hb0[0][:])
            P.op("pe", [(lambda e, g=g: e.transpose(tpb0[:, g, :], wsraw[:, g, :], ident[:])) for g in range(4)], [B_ws], [hb0[1]])
            P.op("act", lambda e: e.activation(out=WsT[:], in_=tpb0[:, 0:4, :], func=AF.Copy), [hb0[1]], [B_c])
            bpool.release(hb0)
            P.barrier()

            xg = [(sb(ph, "p3x%d" % i, [128, 1024]), Buf("x")) for i in range(4)]
            hTg, bhT = sb(ph, "p3hT", [128, 8, 512], BF16), [Buf("hT") for _ in range(4)]
            hcT, bhc = hTg, bhT
            NA = 3
            asets = Pool([dict(gv=sb(ph, "p3gv%d" % i, [128, 512]), vn=sb(ph, "p3vn%d" % i, [128, 512], BF16), lst=sb(ph, "p3lst%d" % i, [128, 16]),
                               bgv=Buf("gv"), bvn=Buf("vn"), bl=[Buf("l") for _ in range(8)]) for i in range(NA)])
            vmb, bvmb = sb(ph, "p3vmb", [128, 4, 512]), [Buf("vmb") for _ in range(4)]
            gus = Pool([(sb(ph, "p3gu%d" % i, [128, 512]), Buf("gu")) for i in range(2)])
            sguT, bsg = sb(ph, "p3sguT", [128, 4, 512], BF16), [Buf("sg") for _ in range(4)]
            csets = Pool([dict(ya=sb(ph, "p3ya%d" % i, [128, 512]), sa=sb(ph, "p3sa%d" % i, [128, 512]), sb=sb(ph, "p3sb%d" % i, [128, 512]),
                               bya=Buf("ya"), bsa=Buf("sa"), bsb=Buf("sb")) for i in range(2)])
            mrg, bmrg = sb(ph, "p3mrg", [128, 8, 512], BF16), [Buf("mrg") for _ in range(8)]
            qcT, bqc = sb(ph, "p3qcT", [128, 4, 512], BF16), [Buf("qc") for _ in range(4)]
            gsets = Pool([dict(cst=sb(ph, "p3cst%d" % i, [128, 16]), pc=sb(ph, "p3pc%d" % i, [128, 4, 256]), pn=sb(ph, "p3pn%d" % i, [128, 4, 256], BF16),
                               pnT=sb(ph, "p3pnT%d" % i, [128, 8, 128], BF16), ocT=sb(ph, "p3ocT%d" % i, [128, 4, 128], BF16),
                               bc=[Buf("c") for _ in range(4)], bpc=Buf("pc"), bpn=Buf("pn"), bpnT=Buf("pnT"), boc=Buf("oc")) for i in range(2)])
            print("sbuf bytes remaining in 2b:", nc.sbuf_bytes_remaining)

            def a_tile(gi, j):
                tile = gi * 4 + j
                xt, bx = xg[j]
                P.dma("sp", lambda e: e.dma_start(out=xt[:], in_=xown[tile * 128:(tile + 1) * 128, :]), [], [bx])
                yield
                yield from norm_T_gen(st, xt[:], bx, gmixT, hTg[:, :, j * 128:(j + 1) * 128], bhT[j], None, None, pool=bpool)
                A = yield from acq(asets)
                gv, vn, lst, bgv, bvn, bl = A["gv"], A["vn"], A["lst"], A["bgv"], A["bvn"], A["bl"]
                h0 = yield from acq(bpool)
                g0, bg0 = h0
                P.op("pe", [(lambda e, c=c: e.matmul(g0[:], hTg[:, c, j * 128:(j + 1) * 128], wsv[:, c, :], start=(c == 0), stop=(c == 7)))
                            for c in range(8)], [bhT[j], B_c], [bg0])
                yield
                P.op("act", lambda e: e.activation(out=gv[:], in_=g0[:], func=AF.Gelu_apprx_tanh), [bg0], [bgv])
                bpool.release(h0)
                yield
                P.op("dve", lambda e: e.tensor_reduce(out=lst[:, 0:1], in_=gv[:], axis=AX.X, op=ALU.add), [bgv], [bl[0]])
                yield
                P.op("act", lambda e: e.activation(out=vn[:], in_=gv[:], func=AF.Square, accum_out=lst[:, 1:2]), [bgv], [bvn, bl[1]])
                yield
                P.op("dve", lambda e: e.tensor_scalar(out=lst[:, 2:3], in0=lst[:, 0:1], scalar1=1.0 / 512, scalar2=None, op0=ALU.mult), [bl[0]], [bl[2]])
                yield
                P.op("dve", lambda e: e.tensor_tensor(out=lst[:, 3:4], in0=lst[:, 2:3], in1=lst[:, 2:3], op=ALU.mult), [bl[2]], [bl[3]])
                yield
                P.op("dve", lambda e: e.scalar_tensor_tensor(out=lst[:, 4:5], in0=lst[:, 1:2], scalar=1.0 / 512, in1=lst[:, 3:4], op0=ALU.mult, op1=ALU.subtract),
                     [bl[1], bl[3]], [bl[4]])
                yield
                P.op("dve", lambda e: e.tensor_scalar(out=lst[:, 5:6], in0=lst[:, 4:5], scalar1=EPS, scalar2=None, op0=ALU.add), [bl[4]], [bl[5]])
                yield
                P.op("act", lambda e: e.activation(out=lst[:, 6:7], in_=lst[:, 5:6], func=AF.Ln), [bl[5]], [bl[6]])
                yield
                P.op("act", lambda e: e.activation(out=lst[:, 7:8], in_=lst[:, 6:7], func=AF.Exp, scale=-0.5), [bl[6]], [bl[7]])
                yield
                P.op("dve", lambda e: e.tensor_scalar(out=gv[:], in0=gv[:], scalar1=lst[:, 2:3], scalar2=lst[:, 7:8], op0=ALU.subtract, op1=ALU.mult),
                     [bgv, bl[2], bl[7]], [bgv])
                yield
                P.op("dve", lambda e: e.tensor_tensor(out=gv[:], in0=gv[:], in1=lng_bc[:], op=ALU.mult), [bgv, B_c], [bgv])
                yield
                P.op("dve", lambda e: e.tensor_tensor(out=vn[:], in0=gv[:], in1=lnb_bc[:], op=ALU.add), [bgv, B_c], [bvn])
                yield
                h1 = yield from acq(bpool)
                g1, bg1 = h1
                P.op("pe", [(lambda e, g=g: e.matmul(g1[:, g * 128:(g + 1) * 128], vn[:, g * 128:(g + 1) * 128], WsT[:, g, :], start=True, stop=True))
                            for g in range(4)], [bvn, B_c], [bg1])
                yield
                P.op("dve", lambda e: e.tensor_tensor(out=vmb[:, :, j * 128:(j + 1) * 128], in0=g1[:].rearrange("p (g q) -> p g q", g=4),
                                                      in1=bs_bc[:], op=ALU.add), [bg1, B_c], [bvmb[j]])
                bpool.release(h1)
                asets.release(A)
                yield

            def b_chunk(g):
                h0 = yield from acq(bpool)
                g0, bg0 = h0
                P.op("pe", [(lambda e, c=c: e.matmul(g0[:], wsu[:, c, g * 128:(g + 1) * 128], hTg[:, c, :], start=(c == 0), stop=(c == 7)))
                            for c in range(8)], bhT + [B_c], [bg0])
                yield
                G = yield from acq(gus)
                gu, bgu = G
                P.op("act", lambda e: e.activation(out=gu[:], in_=g0[:], func=AF.Gelu_apprx_tanh), [bg0], [bgu])
                bpool.release(h0)
                yield
                P.op("dve", lambda e: e.tensor_tensor(out=sguT[:, g, :], in0=gu[:], in1=vmb[:, g, :], op=ALU.mult), [bgu] + bvmb, [bsg[g]])
                gus.release(G)
                yield

            def c_chunk(gi, dc):
                C = yield from acq(csets)
                ya, sa, sbt, bya, bsa, bsb = C["ya"], C["sa"], C["sb"], C["bya"], C["bsa"], C["bsb"]
                P.dma("sp", lambda e: e.dma_start(out=ya[:], in_=yatt_d[gi][:, dc, :]), [], [bya])
                yield
                h0 = yield from acq(bpool)
                ga_ps, bga = h0
                P.op("pe", [(lambda e, c=c: e.matmul(ga_ps[:], wga[:, c, dc * 128:(dc + 1) * 128], hTg[:, c, :], start=(c == 0), stop=(c == 7)))
                            for c in range(8)], bhT + [B_c], [bga])
                yield
                P.op("act", lambda e: e.activation(out=sa[:], in_=ga_ps[:], func=AF.Sigmoid), [bga], [bsa])
                bpool.release(h0)
                yield
                h1 = yield from acq(bpool)
                gb_ps, bgb = h1
                P.op("pe", [(lambda e, c=c: e.matmul(gb_ps[:], wgb[:, c, dc * 128:(dc + 1) * 128], hTg[:, c, :], start=(c == 0), stop=(c == 7)))
                            for c in range(8)], bhT + [B_c], [bgb])
                yield
                P.op("act", lambda e: e.activation(out=sbt[:], in_=gb_ps[:], func=AF.Sigmoid), [bgb], [bsb])
                bpool.release(h1)
                yield
                h2 = yield from acq(bpool)
                ys_ps, bysp = h2
                P.op("pe", [(lambda e, g=g: e.matmul(ys_ps[:], wso[:, g, dc * 128:(dc + 1) * 128], sguT[:, g, :], start=(g == 0), stop=(g == 3)))
                            for g in range(4)], bsg + [B_c], [bysp])
                yield
                P.op("dve", lambda e: e.tensor_tensor(out=sa[:], in0=sa[:], in1=ya[:], op=ALU.mult), [bsa, bya], [bsa])
                yield
                P.op("dve", lambda e: e.tensor_tensor(out=sbt[:], in0=sbt[:], in1=ys_ps[:], op=ALU.mult), [bsb, bysp], [bsb])
                bpool.release(h2)
                yield
                P.op("dve", lambda e: e.tensor_tensor(out=mrg[:, dc, :], in0=sa[:], in1=sbt[:], op=ALU.add), [bsa, bsb], [bmrg[dc]])
                csets.release(C)
                yield

            def d_part(j, nb):
                xt, bx = xg[j]
                h0 = yield from acq(bpool)
                o_ps, bo = h0
                P.op("pe", [(lambda e, dc=dc: e.matmul(o_ps[:], mrg[:, dc, j * 128:(j + 1) * 128], wo[:, dc, nb * 512:(nb + 1) * 512],
                                                      start=(dc == 0), stop=(dc == 7))) for dc in range(8)], bmrg + [B_c], [bo])
                yield
                P.op("dve", lambda e: e.tensor_tensor(out=xt[:, nb * 512:(nb + 1) * 512], in0=xt[:, nb * 512:(nb + 1) * 512], in1=o_ps[:], op=ALU.add),
                     [bo, bx], [bx])
                bpool.release(h0)
                yield

            def e_tile(j):
                xt, bx = xg[j]
                yield from norm_T_gen(st, xt[:], bx, gcrT, hcT[:, :, j * 128:(j + 1) * 128], bhc[j], None, None, pool=bpool)

            def f_head(h):
                h0 = yield from acq(bpool)
                q_ps, bq = h0
                P.op("pe", [(lambda e, c=c: e.matmul(q_ps[:], wcq[:, c, h * 128:(h + 1) * 128], hcT[:, c, :], start=(c == 0), stop=(c == 7)))
                            for c in range(8)], bhc + [B_c], [bq])
                yield
                P.op("act", lambda e: e.activation(out=qcT[:, h, :], in_=q_ps[:], func=AF.Copy, scale=float(128 ** -0.5)), [bq], [bqc[h]])
                bpool.release(h0)
                yield

            def g_tile(gi, j):
                tile = gi * 4 + j
                xt, bx = xg[j]
                Gs = yield from acq(gsets)
                cst, pc, pn, pnT, ocT = Gs["cst"], Gs["pc"], Gs["pn"], Gs["pnT"], Gs["ocT"]
                bc, bpc, bpn, bpnT, boc = Gs["bc"], Gs["bpc"], Gs["bpn"], Gs["bpnT"], Gs["boc"]
                hs0 = yield from acq(bpool)
                hs1 = yield from acq(bpool)
                scs = [hs0[0], hs1[0]]
                bscs = [hs0[1], hs1[1]]
                for half in range(2):
                    P.op("pe", [(lambda e, hh=hh, half=half: e.matmul(scs[half][:, hh * 256:(hh + 1) * 256], qcT[:, half * 2 + hh, j * 128:(j + 1) * 128], kcT[:, half * 2 + hh, :],
                                                                       start=True, stop=True)) for hh in range(2)], bqc, [bscs[half]])
                    yield
                for half in range(2):
                    P.op("dve", lambda e, half=half: e.tensor_reduce(out=cst[:, half * 2:half * 2 + 2], in_=scs[half][:].rearrange("p (h m) -> p h m", h=2), axis=AX.X, op=ALU.max),
                         [bscs[half]], [bc[0]])
                    yield
                P.op("dve", lambda e: e.tensor_scalar(out=cst[:, 4:8], in0=cst[:, 0:4], scalar1=-1.0, scalar2=None, op0=ALU.mult), [bc[0]], [bc[1]])
                yield
                for half in range(2):
                    P.op("act", [(lambda e, hh=hh, half=half: e.activation(out=pc[:, half * 2 + hh, :], in_=scs[half][:, hh * 256:(hh + 1) * 256], func=AF.Exp,
                                                                          bias=cst[:, 4 + half * 2 + hh:5 + half * 2 + hh], accum_out=cst[:, 8 + half * 2 + hh:9 + half * 2 + hh]))
                                 for hh in range(2)], [bscs[half], bc[1]], [bpc, bc[2]])
                    yield
                bpool.release(hs0)
                bpool.release(hs1)
                P.op("dve", lambda e: e.reciprocal(out=cst[:, 12:16], in_=cst[:, 8:12]), [bc[2]], [bc[3]])
                yield
                P.op("dve", lambda e: e.tensor_tensor(out=pn[:], in0=pc[:], in1=cst[:, 12:16].unsqueeze(2).broadcast_to([128, 4, 256]), op=ALU.mult),
                     [bpc, bc[3]], [bpn])
                yield
                ht = yield from acq(bpool)
                tpv = bfview(ht[0][:])
                P.op("pe", [(lambda e, k=k: e.transpose(tpv[:, k, :], pn[:, k // 2, (k % 2) * 128:(k % 2 + 1) * 128], ident[:])) for k in range(8)], [bpn], [ht[1]])
                yield
                P.op("act", lambda e: e.activation(out=pnT[:], in_=tpv, func=AF.Copy), [ht[1]], [bpnT])
                bpool.release(ht)
                yield
                ho = yield from acq(bpool)
                oc_ps, bocp = ho
                fl = []
                for h in range(4):
                    for mt in range(2):
                        fl.append(lambda e, h=h, mt=mt: e.matmul(oc_ps[:, h * 128:(h + 1) * 128], Vc[:, mt, h * 128:(h + 1) * 128], pnT[:, h * 2 + mt, :],
                                                                 start=(mt == 0), stop=(mt == 1)))
                P.op("pe", fl, [bpnT], [bocp])
                yield
                P.op("act", lambda e: e.activation(out=ocT[:], in_=oc_ps[:].rearrange("p (h t) -> p h t", h=4), func=AF.Copy), [bocp], [boc])
                bpool.release(ho)
                yield
                for nb in range(2):
                    hy = yield from acq(bpool)
                    y_ps, byp = hy
                    P.op("pe", [(lambda e, h=h, nb=nb, y_ps=y_ps: e.matmul(y_ps[:], ocT[:, h, :], wco[:, h, nb * 512:(nb + 1) * 512], start=(h == 0), stop=(h == 3)))
                                for h in range(4)], [boc, B_c], [byp])
                    yield
                    P.op("dve", lambda e, nb=nb, y_ps=y_ps: e.tensor_tensor(out=xt[:, nb * 512:(nb + 1) * 512], in0=xt[:, nb * 512:(nb + 1) * 512], in1=y_ps[:], op=ALU.add),
                         [byp, bx], [bx])
                    bpool.release(hy)
                    yield
                gsets.release(Gs)
                P.dma("sp", lambda e: e.dma_start(out=x2_d[tile * 128:(tile + 1) * 128, :], in_=xt[:]), [bx], [])
                yield

            for gi in range(8):
                run_pipeline((a_tile(gi, j) for j in range(4)), 4)
                run_pipeline((b_chunk(g) for g in range(4)), 3)
                run_pipeline((c_chunk(gi, dc) for dc in range(8)), 3)
                run_pipeline((d_part(j, nb) for j in range(4) for nb in range(2)), 4)
                run_pipeline((e_tile(j) for j in range(4)), 4)
                run_pipeline((f_head(h) for h in range(4)), 4)
                run_pipeline((g_tile(gi, j) for j in range(4)), 3)
            P.flush()

        if dbg == 3:
            dx = nc.dram_tensor("dbg_x2", [OWN, D], F32, kind="ExternalOutput").ap()
            P.dma("sp", lambda e: e.dma_start(out=dx[:, :], in_=x2_d[:, :]), [], [])
            P.flush()
            return nc

        NI = 32
        IT = 512
        NS = NI * IT
        hs_d = nc.dram_tensor("hs_scr", [NS, D], BF16, kind="Internal").ap()
        ys_d = nc.dram_tensor("ys_scr", [NS, D], F32, kind="Internal").ap()
        trid = din("tri", [128, 128])
        iotad = din("iota32", [128, 32])
        thrd = din("thr8", [128, 8])
        with contextlib.ExitStack() as ph:
            gmoT = sb(ph, "gmoT", [128, 8])
            wr = sb(ph, "wr", [128, 8, 20], BF16)
            br_bc = sb(ph, "br_bc", [128, 20])
            gfin_bc = sb(ph, "gfin_bc", [128, 1024])
            oh1 = sb(ph, "oh1", [128, 32, 16])
            oh2 = sb(ph, "oh2", [128, 32, 16])
            wts = sb(ph, "wts", [128, 32, 2])
            idx1 = sb(ph, "idx1", [128, 32], mybir.dt.int32)
            idx2 = sb(ph, "idx2", [128, 32], mybir.dt.int32)
            Ei = sb(ph, "Ei", [128, 32], mybir.dt.int32)
            Widx = sb(ph, "Widx", [128, 32], mybir.dt.int32)
            pidx = sb(ph, "pidx", [128, 1])
            P.dma("sp", lambda e: e.dma_start(out=pidx[:], in_=pidxd[:, :]), [], [])
            B_c = Buf("c3")
            P.dma("sp", lambda e: e.dma_start(out=gmoT[:], in_=gT_view(g_moe), allow_slow_non_contiguous=True), [], [B_c])
            P.dma("pool", lambda e: e.dma_start(out=wr[:, :, 0:4], in_=wv(w_rg, 0, 4)), [], [B_c])
            P.dma("pool", lambda e: e.dma_start(out=wr[:, :, 4:20], in_=wv(w_re, 0, 16)), [], [B_c])
            P.dma("sp", lambda e: e.dma_start(out=br_bc[:, 0:4], in_=b_rg.partition_broadcast(128)), [], [B_c])
            P.dma("sp", lambda e: e.dma_start(out=br_bc[:, 4:20], in_=b_re.partition_broadcast(128)), [], [B_c])
            P.dma("sp", lambda e: e.dma_start(out=gfin_bc[:], in_=g_final.partition_broadcast(128)), [], [B_c])
            allbanks = [(ps(ph, "p4g%d" % i, [128, 512]), Buf("g", True)) for i in range(8)]
            bpool = Pool(allbanks)
            gen = Rot(allbanks)

            with contextlib.ExitStack() as sp1:
                hm_all = sb(sp1, "hm_all", [128, 32, 1024], BF16)
                gmo_bc = sb(sp1, "gmo_bc", [128, 1024])
                tri = sb(sp1, "tri", [128, 128], BF16)
                onesb = sb(sp1, "onesb", [128, 128], BF16)
                iota32 = sb(sp1, "iota32", [128, 32])
                thr8 = sb(sp1, "thr8", [128, 8])
                Mb = sb(sp1, "Mb", [128, 32, 16], BF16)
                pos_all = sb(sp1, "pos_all", [128, 32, 16])
                base = sb(sp1, "base", [128, 16])
                P.dma("sp", lambda e: e.dma_start(out=gmo_bc[:], in_=g_moe.partition_broadcast(128)), [], [B_c])
                P.dma("pool", lambda e: e.dma_start(out=tri[:], in_=trid[:, :]), [], [B_c])
                P.dma("sp", lambda e: e.dma_start(out=iota32[:], in_=iotad[:, :]), [], [B_c])
                P.dma("sp", lambda e: e.dma_start(out=thr8[:], in_=thrd[:, :]), [], [B_c])
                P.op("pool", lambda e: e.memset(onesb[:], 1.0), [], [B_c])
                P.op("pool", lambda e: e.memset(base[:], 0.0), [], [B_c])
                P.barrier()
                NP3 = 6
                st = norm_state(sp1, "p4", nsets=NP3, nxb=NP3)
                xts = Rot([(sb(sp1, "p4x%d" % i, [128, 1024]), Buf("x")) for i in range(NP3)])
                hTt = Rot([(sb(sp1, "p4h%d" % i, [128, 8, 128], BF16), Buf("h")) for i in range(NP3)])
                rts = Rot([(sb(sp1, "rt%d" % i, [128, 160]), [Buf("rt") for _ in range(24)]) for i in range(NP3)])
                boh = [Buf("oh%d" % i) for i in range(32)]
                bhm = [Buf("hm%d" % i) for i in range(32)]

                def moe_tile(i):
                    xt, bx = xts.next()
                    P.dma("sp", lambda e: e.dma_start(out=xt[:], in_=x2_d[i * 128:(i + 1) * 128, :]), [], [bx])
                    yield
                    hT, bh = hTt.next()
                    yield from norm_T_gen(st, xt[:], bx, gmoT, hT[:], bh, None, None, pool=bpool, tok=(hm_all[:, i, :], gmo_bc[:], B_c, bhm[i]))
                    hb = yield from acq(bpool)
                    r_ps, brp = hb
                    P.op("pe", [(lambda e, c=c: e.matmul(r_ps[:, 0:20], hT[:, c, :], wr[:, c, :], start=(c == 0), stop=(c == 7)))
                                for c in range(8)], [bh, B_c], [brp])
                    yield
                    R, brt = rts.next()
                    o1 = oh1[:, i, :]
                    o2 = oh2[:, i, :]
                    P.op("dve", lambda e: e.tensor_tensor(out=R[:, 0:20], in0=r_ps[:, 0:20], in1=br_bc[:], op=ALU.add), [brp, B_c], [brt[0]])
                    bpool.release(hb)
                    yield
                    P.op("dve", lambda e: e.tensor_reduce(out=R[:, 20:21], in_=R[:, 0:4], axis=AX.X, op=ALU.max), [brt[0]], [brt[1]])
                    yield
                    P.op("dve", lambda e: e.tensor_scalar(out=R[:, 21:22], in0=R[:, 20:21], scalar1=-1.0, scalar2=None, op0=ALU.mult), [brt[1]], [brt[2]])
                    yield
                    P.op("act", lambda e: e.activation(out=R[:, 22:26], in_=R[:, 0:4], func=AF.Exp, bias=R[:, 21:22], accum_out=R[:, 26:27]), [brt[0], brt[2]], [brt[3]])
                    yield
                    P.op("dve", lambda e: e.reciprocal(out=R[:, 27:28], in_=R[:, 26:27]), [brt[3]], [brt[4]])
                    yield
                    P.op("dve", lambda e: e.tensor_scalar(out=R[:, 28:32], in0=R[:, 0:4], scalar1=R[:, 20:21], scalar2=None, op0=ALU.is_ge), [brt[0], brt[1]], [brt[5]])
                    yield
                    P.op("dve", lambda e: e.tensor_scalar(out=R[:, 32:36], in0=R[:, 28:32], scalar1=BIG, scalar2=-BIG, op0=ALU.mult, op1=ALU.add), [brt[5]], [brt[6]])
                    yield
                    P.op("dve", lambda e: e.tensor_tensor(out=R[:, 40:56].rearrange("p (g k) -> p g k", g=4), in0=R[:, 4:20].rearrange("p (g k) -> p g k", g=4),
                                                          in1=R[:, 32:36].unsqueeze(2).broadcast_to([128, 4, 4]), op=ALU.add), [brt[0], brt[6]], [brt[7]])
                    yield
                    P.op("dve", lambda e: e.tensor_reduce(out=R[:, 56:57], in_=R[:, 40:56], axis=AX.X, op=ALU.max), [brt[7]], [brt[8]])
                    yield
                    P.op("dve", lambda e: e.tensor_scalar(out=o1, in0=R[:, 40:56], scalar1=R[:, 56:57], scalar2=None, op0=ALU.is_ge), [brt[7], brt[8]], [brt[9]])
                    yield
                    P.op("dve", lambda e: e.scalar_tensor_tensor(out=R[:, 80:96], in0=o1, scalar=-BIG, in1=R[:, 40:56], op0=ALU.mult, op1=ALU.add),
                         [brt[9], brt[7]], [brt[10]])
                    yield
                    P.op("dve", lambda e: e.tensor_reduce(out=R[:, 96:97], in_=R[:, 80:96], axis=AX.X, op=ALU.max), [brt[10]], [brt[11]])
                    yield
                    P.op("dve", lambda e: e.tensor_scalar(out=o2, in0=R[:, 80:96], scalar1=R[:, 96:97], scalar2=None, op0=ALU.is_ge), [brt[10], brt[11]], [brt[12]])
                    yield
                    P.op("dve", lambda e: e.tensor_tensor(out=R[:, 116:117], in0=R[:, 96:97], in1=R[:, 56:57], op=ALU.subtract), [brt[11], brt[8]], [brt[13]])
                    yield
                    P.op("act", lambda e: e.activation(out=R[:, 117:118], in_=R[:, 116:117], func=AF.Exp), [brt[13]], [brt[14]])
                    yield
                    P.op("dve", lambda e: e.tensor_scalar(out=R[:, 118:119], in0=R[:, 117:118], scalar1=1.0, scalar2=None, op0=ALU.add), [brt[14]], [brt[15]])
                    yield
                    P.op("dve", lambda e: e.reciprocal(out=R[:, 119:120], in_=R[:, 118:119]), [brt[15]], [brt[16]])
                    yield
                    P.op("dve", lambda e: e.tensor_tensor(out=wts[:, i, 0:1], in0=R[:, 119:120], in1=R[:, 27:28], op=ALU.mult), [brt[16], brt[4]], [brt[17]])
                    yield
                    P.op("dve", lambda e: e.tensor_tensor(out=wts[:, i, 1:2], in0=wts[:, i, 0:1], in1=R[:, 117:118], op=ALU.mult), [brt[17], brt[14]], [brt[18]])
                    yield
                    P.op("dve", lambda e: e.tensor_tensor(out=Mb[:, i, :], in0=o1, in1=o2, op=ALU.add), [brt[9], brt[12]], [boh[i]])
                    yield

                run_pipeline((moe_tile(i) for i in range(32)), NP3)
                P.barrier()
                prefb = Rot([bpool.acquire(), bpool.acquire()])
                bbase = Buf("base")
                bpos = Buf("pos")
                for i in range(32):
                    pb, bpb = prefb.next()
                    P.op("pe", [lambda e, pb=pb, i=i: e.matmul(pb[:, 0:16], tri[:], Mb[:, i, :], start=True, stop=True),
                                lambda e, pb=pb, i=i: e.matmul(pb[:, 16:32], onesb[:], Mb[:, i, :], start=True, stop=True)], [], [bpb])
                    P.op("dve", lambda e, pb=pb, i=i: e.tensor_tensor(out=pos_all[:, i, :], in0=pb[:, 0:16], in1=base[:], op=ALU.add), [bpb, bbase], [bpos])
                    P.op("dve", lambda e, pb=pb: e.tensor_tensor(out=base[:], in0=pb[:, 16:32], in1=base[:], op=ALU.add), [bpb, bbase], [bbase])
                for it_ in prefb.items:
                    bpool.release(it_)
                P.barrier()
                sm = sb(sp1, "sm", [128, 256])
                big = sb(sp1, "bigt", [128, 32, 16])
                big2 = sb(sp1, "bigt2", [128, 32, 16])
                sf = sb(sp1, "sf", [128, 64])
                bs_ = Buf("sm")

                def S1(fn, eng="dve"):
                    P.op(eng, fn, [bs_], [bs_])
                S1(lambda e: e.tensor_tensor(out=sm[:, 0:128].rearrange("p (a k) -> p a k", k=8), in0=base[:].unsqueeze(2).broadcast_to([128, 16, 8]),
                                             in1=thr8[:].unsqueeze(1).broadcast_to([128, 16, 8]), op=ALU.is_gt))
                S1(lambda e: e.tensor_reduce(out=sm[:, 128:144], in_=sm[:, 0:128].rearrange("p (a k) -> p a k", k=8), axis=AX.X, op=ALU.add))
                S1(lambda e: e.tensor_copy(out=sm[:, 144:160], in_=sm[:, 128:144]))
                cur, oth = 144, 160
                for k in (1, 2, 4, 8):
                    S1(lambda e, cur=cur, oth=oth, k=k: e.tensor_copy(out=sm[:, oth:oth + k], in_=sm[:, cur:cur + k]))
                    S1(lambda e, cur=cur, oth=oth, k=k: e.tensor_tensor(out=sm[:, oth + k:oth + 16], in0=sm[:, cur + k:cur + 16], in1=sm[:, cur:cur + 16 - k], op=ALU.add))
                    cur, oth = oth, cur
                S1(lambda e, cur=cur: e.tensor_copy(out=sm[:, 176:192], in_=sm[:, cur:cur + 16]))
                S1(lambda e: e.tensor_tensor(out=sm[:, 192:208], in0=sm[:, 176:192], in1=sm[:, 128:144], op=ALU.subtract))
                S1(lambda e: e.tensor_scalar(out=sm[:, 192:208], in0=sm[:, 192:208], scalar1=float(IT), scalar2=None, op0=ALU.mult))
                S1(lambda e: e.memset(sm[:, 208:240], 0.0))
                for ex in range(NE):
                    S1(lambda e, ex=ex: e.scalar_tensor_tensor(out=sm[:, 208:240], in0=iota32[:], scalar=sm[:, 176 + ex:177 + ex], in1=sm[:, 208:240], op0=ALU.is_ge, op1=ALU.add))
                S1(lambda e: e.tensor_scalar(out=sm[:, 208:240], in0=sm[:, 208:240], scalar1=float(NE - 1), scalar2=None, op0=ALU.min))
                S1(lambda e: e.tensor_copy(out=Ei[:], in_=sm[:, 208:240]))
                S1(lambda e: e.tensor_scalar(out=sf[:, 0:32], in0=sm[:, 208:240], scalar1=128.0, scalar2=pidx[:, 0:1], op0=ALU.mult, op1=ALU.add))
                S1(lambda e: e.tensor_copy(out=Widx[:], in_=sf[:, 0:32]))
                S1(lambda e: e.tensor_tensor(out=big[:], in0=pos_all[:], in1=sm[:, 192:208].unsqueeze(1).broadcast_to([128, 32, 16]), op=ALU.add))
                S1(lambda e: e.tensor_tensor(out=big2[:], in0=big[:], in1=oh1[:], op=ALU.mult))
                S1(lambda e: e.tensor_reduce(out=sf[:, 0:32], in_=big2[:], axis=AX.X, op=ALU.add))
                S1(lambda e: e.tensor_tensor(out=big2[:], in0=big[:], in1=oh2[:], op=ALU.mult))
                S1(lambda e: e.tensor_reduce(out=sf[:, 32:64], in_=big2[:], axis=AX.X, op=ALU.add))
                S1(lambda e: e.tensor_copy(out=idx1[:], in_=sf[:, 0:32]))
                S1(lambda e: e.tensor_copy(out=idx2[:], in_=sf[:, 32:64]))
                P.barrier()
                for i in range(int(_os0.environ.get("NSCAT", 32))):
                    for ixt in (idx1, idx2):
                        P.dma("pool", lambda e, i=i, ixt=ixt: e.indirect_dma_start(out=hs_d[:, :], out_offset=bass.IndirectOffsetOnAxis(ap=ixt[:, i:i + 1], axis=0),
                                                                                   in_=hm_all[:, i, :], in_offset=None), [], [])
                P.barrier()

            if dbg == 4:
                d1 = nc.dram_tensor("dbg_idx", [128, 64], mybir.dt.int32, kind="ExternalOutput").ap()
                d2 = nc.dram_tensor("dbg_E", [128, 32], mybir.dt.int32, kind="ExternalOutput").ap()
                d3 = nc.dram_tensor("dbg_wts", [128, 64], F32, kind="ExternalOutput").ap()
                P.dma("sp", lambda e: e.dma_start(out=d1[:, 0:32], in_=idx1[:]), [], [])
                P.dma("sp", lambda e: e.dma_start(out=d1[:, 32:64], in_=idx2[:]), [], [])
                P.dma("sp", lambda e: e.dma_start(out=d2[:, :], in_=Ei[:]), [], [])
                P.dma("sp", lambda e: e.dma_start(out=d3[:, :], in_=wts[:].rearrange("p a b -> p (a b)")), [], [])
                P.flush()
                return nc

            with contextlib.ExitStack() as sp2:
                Wb = [(sb(sp2, "Wb%d" % i, [128, 12288], BF16), Buf("Wb")) for i in range(2)]
                hss = Rot([(sb(sp2, "hs%d" % i, [128, 4, 1024], BF16), Buf("hs")) for i in range(2)])
                hsTs = Rot([(sb(sp2, "hsT%d" % i, [128, 8, 512], BF16), [Buf("hsT") for _ in range(4)]) for i in range(2)])
                sgs = Rot([(sb(sp2, "sg%d" % i, [128, 512]), Buf("sg")) for i in range(2)])
                heTs = Rot([(sb(sp2, "heT%d" % i, [128, 4, 512], BF16), [Buf("he") for _ in range(4)]) for i in range(2)])
                yss = Rot([(sb(sp2, "ys%d" % i, [128, 1024]), Buf("ys")) for i in range(3)])

                def load_w(w):
                    wb, bwb = Wb[w % 2]
                    P.dma("pool", lambda e: e.indirect_dma_start(out=wb[:, :], out_offset=None, in_=wcat[:, :],
                                                                 in_offset=bass.IndirectOffsetOnAxis(ap=Widx[:, w:w + 1], axis=0)), [], [bwb])

                load_w(0)
                for w in range(NI):
                    if w + 1 < NI:
                        load_w(w + 1)
                    wb, bwb = Wb[w % 2]
                    wg = wb[:, 0:4096].rearrange("p (c f) -> p c f", c=8)
                    wu = wb[:, 4096:8192].rearrange("p (c f) -> p c f", c=8)
                    wd = wb[:, 8192:12288].rearrange("p (c f) -> p c f", c=4)
                    bwg = bwu = bwd = bwb
                    hs, bhs = hss.next()
                    P.dma("sp", lambda e, hs=hs, w=w: e.dma_start(out=hs[:], in_=hs_d[w * IT:(w + 1) * IT, :].rearrange("(s p) d -> p s d", p=128)), [], [bhs])
                    hsT, bhsT = hsTs.next()
                    for sub in range(4):
                        tb, btb = gen.next()
                        tpv = bfview(tb[:])
                        P.op("pe", [(lambda e, c=c, sub=sub, tpv=tpv, hs=hs: e.transpose(tpv[:, c, :], hs[:, sub, c * 128:(c + 1) * 128], ident[:])) for c in range(8)],
                             [bhs], [btb])
                        if sub % 2 == 0:
                            P.op("act", lambda e, sub=sub, tpv=tpv, hsT=hsT: e.activation(out=hsT[:, :, sub * 128:(sub + 1) * 128], in_=tpv, func=AF.Copy), [btb], [bhsT[sub]])
                        else:
                            P.op("dve", lambda e, sub=sub, tpv=tpv, hsT=hsT: e.tensor_copy(out=hsT[:, :, sub * 128:(sub + 1) * 128], in_=tpv), [btb], [bhsT[sub]])
                    heT, bhe = heTs.next()
                    for fc in range(4):
                        g_ps, bgp = gen.next()
                        u_ps, bup = gen.next()
                        P.op("pe", [(lambda e, c=c, fc=fc, g_ps=g_ps, wg=wg, hsT=hsT: e.matmul(g_ps[:], wg[:, c, fc * 128:(fc + 1) * 128], hsT[:, c, :],
                                                                                          start=(c == 0), stop=(c == 7))) for c in range(8)], bhsT + [bwg], [bgp])
                        P.op("pe", [(lambda e, c=c, fc=fc, u_ps=u_ps, wu=wu, hsT=hsT: e.matmul(u_ps[:], wu[:, c, fc * 128:(fc + 1) * 128], hsT[:, c, :],
                                                                                          start=(c == 0), stop=(c == 7))) for c in range(8)], bhsT + [bwu], [bup])
                        sg, bsg_ = sgs.next()
                        P.op("act", lambda e, sg=sg, g_ps=g_ps: e.activation(out=sg[:], in_=g_ps[:], func=AF.Silu), [bgp], [bsg_])
                        P.op("dve", lambda e, sg=sg, u_ps=u_ps, heT=heT, fc=fc: e.tensor_tensor(out=heT[:, fc, :], in0=sg[:], in1=u_ps[:], op=ALU.mult),
                             [bsg_, bup], [bhe[fc]])
                    for sub in range(4):
                        ys, bys = yss.next()
                        for nb in range(2):
                            y_ps, byp = gen.next()
                            P.op("pe", [(lambda e, fc=fc, sub=sub, nb=nb, y_ps=y_ps, heT=heT, wd=wd: e.matmul(y_ps[:], heT[:, fc, sub * 128:(sub + 1) * 128], wd[:, fc, nb * 512:(nb + 1) * 512],
                                                                                                             start=(fc == 0), stop=(fc == 3))) for fc in range(4)], bhe + [bwd], [byp])
                            if nb == 0:
                                P.op("act", lambda e, ys=ys, y_ps=y_ps: e.activation(out=ys[:, 0:512], in_=y_ps[:], func=AF.Copy), [byp], [bys])
                            else:
                                P.op("dve", lambda e, ys=ys, y_ps=y_ps: e.tensor_copy(out=ys[:, 512:1024], in_=y_ps[:]), [byp], [bys])
                        P.dma("sp", lambda e, ys=ys, w=w, sub=sub: e.dma_start(out=ys_d[w * IT + sub * 128:w * IT + (sub + 1) * 128, :], in_=ys[:]), [bys], [])
                P.barrier()

            with contextlib.ExitStack() as sp3:
                NC3 = 3
                st = norm_state(sp3, "p5", nsets=4, nxb=1)
                xts = Rot([(sb(sp3, "p5x%d" % i, [128, 1024]), Buf("x")) for i in range(NC3)])
                y0s = Rot([(sb(sp3, "p5y0%d" % i, [128, 1024]), Buf("y0")) for i in range(NC3)])
                y1s = Rot([(sb(sp3, "p5y1%d" % i, [128, 1024]), Buf("y1")) for i in range(NC3)])
                ots = Rot([(sb(sp3, "p5o%d" % i, [128, 1024]), Buf("ot")) for i in range(NC3)])

                def fin_tile(i):
                    xt, bx = xts.next()
                    y0, by0 = y0s.next()
                    y1, by1 = y1s.next()
                    P.dma("sp", lambda e: e.dma_start(out=xt[:], in_=x2_d[i * 128:(i + 1) * 128, :]), [], [bx])
                    yield
                    P.dma("pool", lambda e: e.indirect_dma_start(out=y0[:, :], out_offset=None, in_=ys_d[:, :],
                                                                 in_offset=bass.IndirectOffsetOnAxis(ap=idx1[:, i:i + 1], axis=0)), [], [by0])
                    yield
                    P.dma("pool", lambda e: e.indirect_dma_start(out=y1[:, :], out_offset=None, in_=ys_d[:, :],
                                                                 in_offset=bass.IndirectOffsetOnAxis(ap=idx2[:, i:i + 1], axis=0)), [], [by1])
                    yield
                    P.op("dve", lambda e: e.scalar_tensor_tensor(out=xt[:], in0=y0[:], scalar=wts[:, i, 0:1], in1=xt[:], op0=ALU.mult, op1=ALU.add), [by0, bx], [bx])
                    yield
                    P.op("dve", lambda e: e.scalar_tensor_tensor(out=xt[:], in0=y1[:], scalar=wts[:, i, 1:2], in1=xt[:], op0=ALU.mult, op1=ALU.add), [by1, bx], [bx])
                    yield
                    fss, fb, _u = st["sets"].next()
                    ot, bot = ots.next()
                    P.op("act", lambda e: e.activation(out=ot[:], in_=xt[:], func=AF.Square, scale=1.0 / 32, accum_out=fss[:, 0:1]), [bx], [fb[0], bot])
                    yield
                    P.op("dve", lambda e: e.tensor_scalar(out=fss[:, 1:2], in0=fss[:, 0:1], scalar1=EPS, scalar2=None, op0=ALU.add), [fb[0]], [fb[1]])
                    yield
                    P.op("act", lambda e: e.activation(out=fss[:, 2:3], in_=fss[:, 1:2], func=AF.Ln), [fb[1]], [fb[2]])
                    yield
                    P.op("act", lambda e: e.activation(out=fss[:, 3:4], in_=fss[:, 2:3], func=AF.Exp, scale=-0.5), [fb[2]], [fb[3]])
                    yield
                    P.op("dve", lambda e: e.scalar_tensor_tensor(out=ot[:], in0=xt[:], scalar=fss[:, 3:4], in1=gfin_bc[:], op0=ALU.mult, op1=ALU.mult),
                         [bx, fb[3], B_c], [bot])
                    yield
                    P.dma("sp", lambda e: e.dma_start(out=out[i * 128:(i + 1) * 128, :], in_=ot[:]), [bot], [])
                    yield

                run_pipeline((fin_tile(i) for i in range(32)), NC3)
                P.barrier()
            P.flush()
    return nc


def _rope_tables():
    rows = SEQ // 64
    row_ids = np.repeat(np.arange(rows), 64).astype(np.float32)
    col_ids = np.tile(np.arange(64), rows).astype(np.float32)
    inv = (10000.0 ** (-np.arange(16, dtype=np.float32) / 16)).astype(np.float32)
    ang = np.concatenate([row_ids[:, None] * inv[None, :], col_ids[:, None] * inv[None, :]], axis=-1).astype(np.float32)
    return np.cos(ang).astype(np.float32), np.sin(ang).astype(np.float32)


def _wcat(wg, wu, wd):
    g = wg.reshape(NE, 8, 128, 512).transpose(0, 2, 1, 3).reshape(NE, 128, 4096)
    u = wu.reshape(NE, 8, 128, 512).transpose(0, 2, 1, 3).reshape(NE, 128, 4096)
    d_ = wd.reshape(NE, 4, 128, 1024).transpose(0, 2, 1, 3).reshape(NE, 128, 4096)
    return np.ascontiguousarray(np.concatenate([g, u, d_], axis=2).reshape(NE * 128, 12288))


def make_in_maps(inputs):
    f = lambda a: np.ascontiguousarray(np.asarray(a, dtype=np.float32))
    x = f(inputs["x"])
    mem = f(inputs["mem"])
    cos, sin = _rope_tables()

    def lay(t, n):
        return np.ascontiguousarray(t.reshape(n, 128, 32).transpose(1, 0, 2))

    shared = {
        "ident": np.eye(128, dtype=np.float32),
        "g_mix": f(inputs["g_mix"][0]), "w_in": f(inputs["w_in"][0]), "g_q": f(inputs["g_q"][0]), "g_k": f(inputs["g_k"][0]),
        "w_att_out": f(inputs["w_att_out"][0]), "sgu_ln_g": f(inputs["sgu_ln_g"][0]), "sgu_ln_b": f(inputs["sgu_ln_b"][0]),
        "w_s": f(inputs["w_s"][0]), "b_s": f(inputs["b_s"][0]), "w_sgu_out": f(inputs["w_sgu_out"][0]), "w_out": f(inputs["w_out"][0]),
        "g_cross": f(inputs["g_cross"][0]), "g_mem": f(inputs["g_mem"][0]), "w_cq": f(inputs["w_cq"][0]), "w_ckv": f(inputs["w_ckv"][0]),
        "w_co": f(inputs["w_co"][0]), "g_moe": f(inputs["g_moe"][0]), "w_rg": f(inputs["w_rg"][0]), "b_rg": f(inputs["b_rg"][0]),
        "w_re": f(inputs["w_re"][0]), "b_re": f(inputs["b_re"][0]), "g_final": f(inputs["g_final"]),
        "wcat": _wcat(f(inputs["w_gate"][0]), f(inputs["w_up"][0]), f(inputs["w_down"][0])),
        "pidx": np.arange(128, dtype=np.float32).reshape(128, 1),
        "cos_seq": lay(cos, 64), "sin_seq": lay(sin, 64),
        "tri": np.triu(np.ones((128, 128), np.float32), 1),
        "iota32": np.tile(np.arange(32, dtype=np.float32), (128, 1)),
        "thr8": np.tile(np.arange(8, dtype=np.float32) * 512.0, (128, 1)),
    }
    maps = []
    for c in range(8):
        b, hf = c // 2, c % 2
        m = dict(shared)
        m["xseq"] = x[b]
        m["xown"] = np.ascontiguousarray(x[b, hf * OWN:(hf + 1) * OWN])
        m["mem"] = mem[b]
        m["cos_own"] = lay(cos[hf * OWN:(hf + 1) * OWN], 32)
        m["sin_own"] = lay(sin[hf * OWN:(hf + 1) * OWN], 32)
        maps.append(m)
    return maps


def kernel(**inputs):
    nc = build()
    maps = make_in_maps(inputs)
    res = run_bass_kernel_spmd(nc, maps, core_ids=list(range(8)))
    outp = np.empty((4, SEQ, D), np.float32)
    for c in range(8):
        b, hf = c // 2, c % 2
        outp[b, hf * OWN:(hf + 1) * OWN] = np.asarray(res.results[c]["out"], dtype=np.float32)
    return outp
```

```python
import contextlib
import os as _os0
import numpy as np
import concourse.bass as bass
import concourse.mybir as mybir
from concourse.bass_utils import run_bass_kernel_spmd

F32 = mybir.dt.float32
BF16 = mybir.dt.bfloat16
AF = mybir.ActivationFunctionType
ALU = mybir.AluOpType
AX = mybir.AxisListType

D = 1024
SEQ = 8192
OWN = 4096
NMEM = 256
EPS = 1e-6
NE = 16
BIG = 1.0e30


class Buf:
    __slots__ = ("name", "w", "r", "psum")

    def __init__(self, name="", psum=False):
        self.name = name
        self.psum = psum
        self.w = None
        self.r = {}


class Eng:
    def __init__(self, name):
        self.name = name
        self.sem = None
        self.count = 0
        self.seen = {}
        self.prog = []
        self.chsems = []
        self.dma_i = 0


class Prog:
    NCH = int(_os0.environ.get('NCH', 8))

    def __init__(self, nc, stack):
        self.nc = nc
        self.E = {}
        for n in ("pe", "act", "dve", "pool", "sp"):
            e = Eng(n)
            e.sem = stack.enter_context(nc.semaphore("sem_" + n))
            self.E[n] = e
        for n in ("sp", "pool", "act"):
            e = self.E[n]
            for i in range(self.NCH):
                e.chsems.append(stack.enter_context(nc.semaphore("ch_%s_%d" % (n, i))))
        self.chan_issued = {}
        import os
        self.limit = int(os.environ.get("OPLIMIT", 10 ** 9))
        self.nops = 0

    def _need(self, need, ev):
        sem, val, src = ev
        k = id(sem)
        if k not in need or need[k][1] < val:
            need[k] = (sem, val, src)

    def _deps(self, eng, reads, writes, extra=()):
        E = self.E[eng]
        need = {}
        for b in reads:
            if b.w is not None:
                self._need(need, b.w)
        for b in writes:
            if b.w is not None:
                self._need(need, b.w)
            for k, (sem, val, src) in b.r.items():
                self._need(need, (sem, val, src))
        for ev in extra:
            self._need(need, ev)
        for k, (sem, val, src) in need.items():
            if src == "pe" and eng == "pe":
                continue
            if E.seen.get(k, 0) >= val:
                continue
            E.seen[k] = val
            E.prog.append(("wait", sem, val))

    def _commit(self, ev, reads, writes):
        sem, val, src = ev
        for b in writes:
            b.w = ev
            b.r = {}
        for b in reads:
            if b not in writes:
                b.r[id(sem)] = (sem, val, src)

    def op(self, eng, fns, reads=(), writes=()):
        if not isinstance(fns, (list, tuple)):
            fns = [fns]
        self.nops += 1
        if self.nops > self.limit:
            return
        pr = [b for b in reads if b.psum]
        if pr:
            reads = [b for b in reads if not b.psum]
            writes = list(writes) + [b for b in pr if b not in writes]
        E = self.E[eng]
        self._deps(eng, reads, writes)
        E.count += 1
        for f in fns[:-1]:
            E.prog.append(("raw", f))
        E.prog.append(("op", fns[-1], E.sem))
        self._commit((E.sem, E.count, eng), reads, writes)

    def dma(self, q, fn, reads=(), writes=()):
        self.nops += 1
        if self.nops > self.limit:
            return
        E = self.E[q]
        ch = E.dma_i % self.NCH
        rnd = E.dma_i // self.NCH
        E.dma_i += 1
        chsem = E.chsems[ch]
        extra = []
        if rnd > 0:
            extra.append((chsem, 16 * rnd, "dma"))
        self._deps(q, reads, writes, extra)
        E.prog.append(("dma", fn, chsem))
        self.chan_issued[id(chsem)] = (chsem, 16 * (rnd + 1))
        self._commit((chsem, 16 * (rnd + 1), "dma"), reads, writes)

    def dma_bg(self, q, fn, sem, total, buf):
        E = self.E[q]
        E.prog.append(("dma", fn, sem))
        buf.w = (sem, total, "dma")

    def barrier(self):
        for n, E in self.E.items():
            for m, F in self.E.items():
                if m == n or F.count == 0:
                    continue
                k = id(F.sem)
                if E.seen.get(k, 0) < F.count:
                    E.seen[k] = F.count
                    E.prog.append(("wait", F.sem, F.count))
            for k, (sem, val) in self.chan_issued.items():
                if E.seen.get(k, 0) < val:
                    E.seen[k] = val
                    E.prog.append(("wait", sem, val))

    def flush(self):
        self.barrier()
        nc = self.nc
        progs = {n: E.prog for n, E in self.E.items()}
        for E in self.E.values():
            E.prog = []

        def replay(eng, prog):
            for it in prog:
                if it[0] == "wait":
                    eng.wait_ge(it[1], it[2])
                elif it[0] == "raw":
                    it[1](eng)
                elif it[0] == "op":
                    it[1](eng).then_inc(it[2], 1)
                else:
                    it[1](eng).then_inc(it[2], 16)

        with nc.Block() as block:
            @block.tensor
            def _(e):
                replay(e, progs["pe"])

            @block.scalar
            def _(e):
                replay(e, progs["act"])

            @block.vector
            def _(e):
                replay(e, progs["dve"])

            @block.gpsimd
            def _(e):
                replay(e, progs["pool"])

            @block.sync
            def _(e):
                replay(e, progs["sp"])


class Rot:
    def __init__(self, items):
        self.items = items
        self.i = 0

    def next(self):
        it = self.items[self.i % len(self.items)]
        self.i += 1
        return it


class Pool:
    def __init__(self, items):
        self.free = list(items)

    def acquire(self):
        return self.free.pop(0) if self.free else None

    def release(self, it):
        self.free.append(it)


def build(dbg=False):
    nc = bass.Bass("TRN2", target_bir_lowering=False)

    def din(name, shape):
        return nc.dram_tensor(name, list(shape), F32, kind="ExternalInput").ap()

    xseq = din("xseq", [SEQ, D])
    xown = din("xown", [OWN, D])
    memd = din("mem", [NMEM, D])
    cos_seq = din("cos_seq", [128, 64, 32])
    sin_seq = din("sin_seq", [128, 64, 32])
    cos_own = din("cos_own", [128, 32, 32])
    sin_own = din("sin_own", [128, 32, 32])
    identd = din("ident", [128, 128])
    g_mix = din("g_mix", [D])
    w_in = din("w_in", [D, 3840])
    g_q = din("g_q", [64])
    g_k = din("g_k", [64])
    w_att_out = din("w_att_out", [512, D])
    sgu_ln_g = din("sgu_ln_g", [512])
    sgu_ln_b = din("sgu_ln_b", [512])
    w_s = din("w_s", [4, 128, 128])
    b_s = din("b_s", [4, 128])
    w_sgu_out = din("w_sgu_out", [512, D])
    w_out = din("w_out", [D, D])
    g_cross = din("g_cross", [D])
    g_mem = din("g_mem", [D])
    w_cq = din("w_cq", [D, 512])
    w_ckv = din("w_ckv", [D, D])
    w_co = din("w_co", [512, D])
    g_moe = din("g_moe", [D])
    w_rg = din("w_rg", [D, 4])
    b_rg = din("b_rg", [4])
    w_re = din("w_re", [D, 16])
    b_re = din("b_re", [16])
    wcat = din("wcat", [NE * 128, 12288])
    pidxd = din("pidx", [128, 1])
    wcat_bf = nc.dram_tensor("wcat_bf", [NE * 128, 12288], BF16, kind="Internal").ap()
    g_final = din("g_final", [D])
    out = nc.dram_tensor("out", [OWN, D], F32, kind="ExternalOutput").ap()
    yatt_d = nc.dram_tensor("yatt_scr", [8, 128, 8, 512], F32, kind="Internal").ap()
    x2_d = nc.dram_tensor("x2_scr", [OWN, D], F32, kind="Internal").ap()

    def wv(w, c0, c1):
        return w.rearrange("(c p) n -> p c n", p=128)[:, :, c0:c1]

    def gT_view(g):
        return g.rearrange("(c p) -> p c", p=128)

    with contextlib.ExitStack() as top:
        P = Prog(nc, top)
        bgsem = top.enter_context(nc.semaphore("bgsem"))
        B_wbf = Buf("wcat_bf")
        print("sbuf bytes remaining at start:", nc.sbuf_bytes_remaining)

        def sb(stack, name, shape, dt=F32):
            return stack.enter_context(nc.sbuf_tensor("s_" + name, list(shape), dt))

        def ps(stack, name, shape, dt=F32):
            return stack.enter_context(nc.psum_tensor("p_" + name, list(shape), dt))

        ident = sb(top, "ident", [128, 128], BF16)
        kcT = sb(top, "kcT", [128, 4, NMEM], BF16)
        Vc = sb(top, "Vc", [128, 2, 512], BF16)
        gmixT = sb(top, "gmixT", [128, 8])
        attn_stack = contextlib.ExitStack()
        kT_all = sb(attn_stack, "kT_all", [128, SEQ], BF16)
        V_all = sb(attn_stack, "V_all", [128, 64, 2, 65], BF16)
        B_const = Buf("const")

        P.dma("pool", lambda e: e.dma_start(out=ident[:], in_=identd[:, :]), [], [B_const])
        P.dma("sp", lambda e: e.dma_start(out=gmixT[:], in_=gT_view(g_mix), allow_slow_non_contiguous=True), [], [B_const])
        P.op("pool", lambda e: e.memset(V_all[:], 1.0), [], [B_const])

        def run_pipeline(gens, depth):
            it = iter(gens)
            active = []
            more = True
            while True:
                if more and len(active) < depth:
                    try:
                        active.append(next(it))
                    except StopIteration:
                        more = False
                if not active:
                    break
                for g in list(active):
                    try:
                        next(g)
                    except StopIteration:
                        active.remove(g)

        def drain(g):
            for _ in g:
                pass

        def acq(pool):
            while True:
                it = pool.acquire()
                if it is not None:
                    return it
                yield

        def bfview(bank_ap):
            return bank_ap.bitcast(BF16).rearrange("p (c t) -> p c t", c=8)

        def norm_T_gen(st, xt_ap, xbuf, gT, out_ap3, outbuf, tps, tpsb, pool=None, tok=None):
            ss, b, _unused = st["sets"].next()
            xb, bxb = st["xb"].next()
            P.op("act", lambda e: e.activation(out=xb[:], in_=xt_ap, func=AF.Square, scale=1.0 / 32,
                                               accum_out=ss[:, 0:1]), [xbuf], [b[0], bxb])
            yield
            P.op("dve", lambda e: e.tensor_scalar(out=ss[:, 1:2], in0=ss[:, 0:1], scalar1=EPS, scalar2=None,
                                                  op0=ALU.add), [b[0]], [b[1]])
            yield
            P.op("act", lambda e: e.activation(out=ss[:, 2:3], in_=ss[:, 1:2], func=AF.Ln), [b[1]], [b[2]])
            yield
            P.op("act", lambda e: e.activation(out=ss[:, 3:4], in_=ss[:, 2:3], func=AF.Exp, scale=-0.5), [b[2]], [b[3]])
            yield
            P.op("act", lambda e: e.activation(out=xb[:], in_=xt_ap, func=AF.Copy, scale=ss[:, 3:4]), [xbuf, b[3]], [bxb])
            yield
            if tok is not None:
                P.op("dve", lambda e: e.tensor_tensor(out=tok[0], in0=xb[:], in1=tok[1], op=ALU.mult), [bxb, tok[2]], [tok[3]])
                yield
            held = None
            if pool is not None:
                while held is None:
                    held = pool.acquire()
                    if held is None:
                        yield
                tps, tpsb = bfview(held[0][:]), held[1]
            P.op("pe", [(lambda e, c=c: e.transpose(tps[:, c, :], xb[:, c * 128:(c + 1) * 128], ident[:])) for c in range(8)],
                 [bxb], [tpsb])
            yield
            P.op("dve", lambda e: e.tensor_tensor(out=out_ap3, in0=tps, in1=gT[:, 0:8].unsqueeze(2).broadcast_to([128, 8, 128]),
                                                  op=ALU.mult), [tpsb], [outbuf])
            if held is not None:
                pool.release(held)
            yield

        def norm_T(st, xt_ap, xbuf, gT, out_ap3, outbuf, tps, tpsb):
            drain(norm_T_gen(st, xt_ap, xbuf, gT, out_ap3, outbuf, tps[:], tpsb))

        def norm_state(stack, pfx, nsets=2, nxb=2):
            st = {}
            st["sets"] = Rot([(sb(stack, pfx + "ss%d" % i, [128, 4]), [Buf("ss") for _ in range(4)], None) for i in range(nsets)])
            st["xb"] = Rot([(sb(stack, pfx + "xb%d" % i, [128, 1024], BF16), Buf("xb")) for i in range(nxb)])
            return st

        def head_norm_rope_gen(ws, src_ps, bsrc, H, g_bc, cosb, sinb, dst, bdst, t):
            if isinstance(ws, Pool):
                w = yield from acq(ws)
            else:
                w = ws.next()
            n = H * 64
            src3 = src_ps.rearrange("p (h d) -> p h d", h=H)
            P.op("act", lambda e: e.activation(out=w["sq"][:, 0:n], in_=src_ps, func=AF.Square, scale=0.125), [bsrc], [w["bsq"]])
            yield
            P.op("dve", lambda e: e.tensor_reduce(out=w["hs"][:, 0:H], in_=w["sq"][:, 0:n].rearrange("p (h d) -> p h d", h=H),
                                                  axis=AX.X, op=ALU.add), [w["bsq"]], [w["bhs"]])
            yield
            P.op("dve", lambda e: e.tensor_scalar(out=w["hs"][:, 8:8 + H], in0=w["hs"][:, 0:H], scalar1=EPS, scalar2=None, op0=ALU.add),
                 [w["bhs"]], [w["bhs1"]])
            yield
            P.op("act", lambda e: e.activation(out=w["hs"][:, 16:16 + H], in_=w["hs"][:, 8:8 + H], func=AF.Ln), [w["bhs1"]], [w["bhs2"]])
            yield
            P.op("act", lambda e: e.activation(out=w["hs"][:, 24:24 + H], in_=w["hs"][:, 16:16 + H], func=AF.Exp, scale=-0.5),
                 [w["bhs2"]], [w["bhs3"]])
            yield
            qn3 = w["qn"][:, 0:n].rearrange("p (h d) -> p h d", h=H)
            P.op("dve", lambda e: e.tensor_tensor(out=qn3, in0=src3, in1=w["hs"][:, 24:24 + H].unsqueeze(2).broadcast_to([128, H, 64]),
                                                  op=ALU.mult), [bsrc, w["bhs3"]], [w["bqn"]])
            yield
            P.op("dve", lambda e: e.tensor_tensor(out=qn3, in0=qn3, in1=g_bc[:, :].unsqueeze(1).broadcast_to([128, H, 64]),
                                                  op=ALU.mult), [w["bqn"]], [w["bqn"]])
            yield
            qn4 = w["qn"][:, 0:n].rearrange("p (h i t) -> p h i t", h=H, t=2)
            A4 = w["A"][:, 0:n].rearrange("p (h i t) -> p h i t", h=H, t=2)
            B4 = w["B"][:, 0:n].rearrange("p (h i t) -> p h i t", h=H, t=2)
            c4 = cosb[:, t, :].unsqueeze(1).unsqueeze(3).broadcast_to([128, H, 32, 2])
            s4 = sinb[:, t, :].unsqueeze(1).unsqueeze(3).broadcast_to([128, H, 32, 2])
            P.op("dve", lambda e: e.tensor_tensor(out=A4, in0=qn4, in1=c4, op=ALU.mult), [w["bqn"]], [w["bA"]])
            yield
            P.op("dve", lambda e: e.tensor_tensor(out=B4, in0=qn4, in1=s4, op=ALU.mult), [w["bqn"]], [w["bB"]])
            yield
            P.op("dve", lambda e: e.tensor_tensor(out=dst[:, :, :, 0], in0=A4[:, :, :, 0], in1=B4[:, :, :, 1], op=ALU.subtract),
                 [w["bA"], w["bB"]], [bdst])
            yield
            P.op("dve", lambda e: e.tensor_tensor(out=dst[:, :, :, 1], in0=B4[:, :, :, 0], in1=A4[:, :, :, 1], op=ALU.add),
                 [w["bA"], w["bB"]], [bdst])
            if isinstance(ws, Pool):
                ws.release(w)
            yield

        def rope_state(stack, pfx, n, nsets=1):
            sets = []
            for i in range(nsets):
                w = {}
                for k in ("sq", "qn", "A", "B"):
                    w[k] = sb(stack, "%s%s%d" % (pfx, k, i), [128, n])
                w["hs"] = sb(stack, "%shs%d" % (pfx, i), [128, 32])
                for k in ("bsq", "bhs", "bhs1", "bhs2", "bhs3", "bqn", "bA", "bB"):
                    w[k] = Buf(k)
                sets.append(w)
            return Rot(sets)

        with contextlib.ExitStack() as ph:
            wkv = sb(ph, "wkv", [128, 8, 256], BF16)
            cosb = sb(ph, "cosb", [128, 64, 32])
            sinb = sb(ph, "sinb", [128, 64, 32])
            gk_bc = sb(ph, "gk_bc", [128, 64])
            P.dma("pool", lambda e: e.dma_start(out=wkv[:], in_=wv(w_in, 512, 768)), [], [B_const])
            P.dma("sp", lambda e: e.dma_start(out=cosb[:], in_=cos_seq[:, :, :]), [], [B_const])
            P.dma("sp", lambda e: e.dma_start(out=sinb[:], in_=sin_seq[:, :, :]), [], [B_const])
            P.dma("sp", lambda e: e.dma_start(out=gk_bc[:], in_=g_k.partition_broadcast(128)), [], [B_const])
            NP1 = 8
            P.barrier()
            st = norm_state(ph, "p1", nsets=NP1, nxb=NP1)
            rws = rope_state(ph, "p1r", 128, nsets=NP1)
            xts = Rot([(sb(ph, "p1x%d" % i, [128, 1024]), Buf("x")) for i in range(NP1)])
            hTs = Rot([(sb(ph, "p1h%d" % i, [128, 8, 128], BF16), Buf("h")) for i in range(NP1)])
            krs = Rot([(sb(ph, "p1kr%d" % i, [128, 2, 32, 2], BF16), Buf("kr")) for i in range(NP1)])
            banks = Pool([(ps(ph, "p1b%d" % i, [128, 512]), Buf("bank", True)) for i in range(8)])

            import os as _os

            def p1_tile(t):
                xt, bx = xts.next()
                P.dma("sp", lambda e: e.dma_start(out=xt[:], in_=xseq[t * 128:(t + 1) * 128, :]), [], [bx])
                yield
                hT, bh = hTs.next()
                yield from norm_T_gen(st, xt[:], bx, gmixT, hT[:], bh, None, None, pool=banks)
                hkv = yield from acq(banks)
                kv, bkv = hkv
                P.op("pe", [(lambda e, c=c: e.matmul(kv[:, 0:256], hT[:, c, :], wkv[:, c, :], start=(c == 0), stop=(c == 7)))
                            for c in range(8)], [bh, B_const], [bkv])
                yield
                P.op("act", lambda e: e.activation(out=V_all[:, t, :, 0:64], in_=kv[:, 128:256].rearrange("p (h d) -> p h d", h=2),
                                                   func=AF.Copy), [bkv], [])
                yield
                kr, bkr = krs.next()
                yield from head_norm_rope_gen(rws, kv[:, 0:128], bkv, 2, gk_bc, cosb, sinb, kr, bkr, t)
                banks.release(hkv)
                hkt = yield from acq(banks)
                kt, bkt = hkt
                ktv = kt[:].bitcast(BF16)
                P.op("pe", lambda e: e.transpose(ktv[:, 0:128], kr[:].rearrange("p h i t -> p (h i t)"), ident[:]), [bkr], [bkt])
                yield
                P.op("act", lambda e: e.activation(out=kT_all[:, t * 128:(t + 1) * 128], in_=ktv[:, 0:128], func=AF.Copy), [bkt], [])
                banks.release(hkt)
                yield

            run_pipeline((p1_tile(t) for t in range(int(_os.environ.get('NT1', 64)))), int(_os.environ.get('DEPTH1', 8)))
            P.flush()

        if dbg == 1:
            P.limit = 10 ** 9
            P.limit = 10 ** 9
            print("nops", P.nops)
            dk = nc.dram_tensor("dbg_kT", [128, SEQ], BF16, kind="ExternalOutput").ap()
            dv = nc.dram_tensor("dbg_V", [128, 64 * 2 * 65], BF16, kind="ExternalOutput").ap()
            P.dma("sp", lambda e: e.dma_start(out=dk[:, :], in_=kT_all[:]), [], [])
            P.dma("sp", lambda e: e.dma_start(out=dv[:, :], in_=V_all[:].rearrange("p a b c -> p (a b c)")), [], [])
            P.flush()
            attn_stack.close()
            return nc

        with contextlib.ExitStack() as ph:
            wq = sb(ph, "wq", [128, 8, 512], BF16)
            wao = sb(ph, "wao", [64, 8, 1024], BF16)
            cosb = sb(ph, "cosb2", [128, 32, 32])
            sinb = sb(ph, "sinb2", [128, 32, 32])
            gq_bc = sb(ph, "gq_bc", [128, 64])
            ones65 = sb(ph, "ones65", [65, 64])
            B_c2 = Buf("c2")
            for hd_ in range(8):
                pos_ = (hd_ % 4) * 2 + hd_ // 4
                P.dma("pool", lambda e, hd_=hd_, pos_=pos_: e.dma_start(out=wq[:, :, pos_ * 64:(pos_ + 1) * 64], in_=wv(w_in, hd_ * 64, hd_ * 64 + 64)), [], [B_c2])
            P.dma("pool", lambda e: e.dma_start(out=wao[:], in_=w_att_out.rearrange("(h p) n -> p h n", p=64)), [], [B_c2])
            P.dma("sp", lambda e: e.dma_start(out=cosb[:], in_=cos_own[:, :, :]), [], [B_c2])
            P.dma("sp", lambda e: e.dma_start(out=sinb[:], in_=sin_own[:, :, :]), [], [B_c2])
            P.dma("sp", lambda e: e.dma_start(out=gq_bc[:], in_=g_q.partition_broadcast(128)), [], [B_c2])
            P.op("dve", lambda e: e.tensor_scalar(out=gq_bc[:], in0=gq_bc[:], scalar1=0.125, scalar2=None, op0=ALU.mult), [B_c2], [B_c2])
            P.op("pool", lambda e: e.memset(ones65[:], 1.0), [], [B_c2])
            for ex_ in range(NE):
                P.dma_bg("pool", lambda e, ex_=ex_: e.dma_start(out=wcat_bf[ex_ * 128:(ex_ + 1) * 128, :], in_=wcat[ex_ * 128:(ex_ + 1) * 128, :]), bgsem, 16 * NE, B_wbf)
            P.barrier()
            st = norm_state(ph, "p2", nsets=4, nxb=4)
            rws = Pool(rope_state(ph, "p2r", 512, nsets=3).items)
            xts = Rot([(sb(ph, "p2x%d" % i, [128, 1024]), Buf("x")) for i in range(4)])
            hTs = Rot([(sb(ph, "p2h%d" % i, [128, 8, 128], BF16), Buf("h")) for i in range(4)])
            qrs = Rot([(sb(ph, "p2qr%d" % i, [128, 8, 32, 2], BF16), Buf("qr")) for i in range(4)])
            Ps = Rot([(sb(ph, "p2P%d" % i, [128, 2, 512], BF16), Buf("P")) for i in range(3)])
            OnTs = Rot([(sb(ph, "p2OnT%d" % i, [64, 8, 512], BF16), [Buf("OnT") for _ in range(8)]) for i in range(2)])
            ysbs = Rot([(sb(ph, "p2ysb%d" % i, [128, 4, 512]), [Buf("ysb") for _ in range(4)]) for i in range(1)])
            Sps = Rot([(ps(ph, "p2S%d" % i, [128, 2, 512]), Buf("S", True)) for i in range(3)])
            Ops, bO = ps(ph, "p2O", [128, 2, 512]), [Buf("O0", True), Buf("O1", True)]
            LA = int(_os0.environ.get('LA', 2))

            sbank = Pool([(Sps.items[i][0][:, bnk, :], Buf("sbank", True)) for i in range(3) for bnk in range(2)])
            Osbs = Rot([(sb(ph, "p2Osc%d" % i, [65, 512]), Buf("Osb")) for i in range(4)])
            rrs = Rot([(sb(ph, "p2rrc%d" % i, [65, 512]), Buf("rr")) for i in range(4)])

            qT_all = sb(ph, "qT_all", [128, 4, OWN], BF16)
            bqTt = [Buf("qTt") for _ in range(32)]
            print("sbuf bytes remaining in 2a:", nc.sbuf_bytes_remaining)

            def q_tile(tile):
                xt, bx = xts.next()
                P.dma("sp", lambda e: e.dma_start(out=xt[:], in_=xown[tile * 128:(tile + 1) * 128, :]), [], [bx])
                yield
                hT, bh = hTs.next()
                yield from norm_T_gen(st, xt[:], bx, gmixT, hT[:], bh, None, None, pool=sbank)
                hq = yield from acq(sbank)
                qps, bqp = hq
                P.op("pe", [(lambda e, c=c: e.matmul(qps, hT[:, c, :], wq[:, c, :], start=(c == 0), stop=(c == 7)))
                            for c in range(8)], [bh, B_c2], [bqp])
                yield
                qr, bqr = qrs.next()
                yield from head_norm_rope_gen(rws, qps, bqp, 8, gq_bc, cosb, sinb, qr, bqr, tile)
                sbank.release(hq)
                ht = yield from acq(sbank)
                tpv = bfview(ht[0])
                qr3 = qr[:].rearrange("p h i t -> p (h i t)")
                P.op("pe", [(lambda e, g=g: e.transpose(tpv[:, g, :], qr3[:, g * 128:(g + 1) * 128], ident[:])) for g in range(4)],
                     [bqr], [ht[1]])
                yield
                P.op("act", lambda e: e.activation(out=qT_all[:, :, tile * 128:(tile + 1) * 128], in_=tpv[:, 0:4, :], func=AF.Copy),
                     [ht[1]], [bqTt[tile]])
                sbank.release(ht)
                yield

            spool = Pool(list(Sps.items))

            def epi_unit(item, bnk):
                (Osb, bOsb, rr, brr, OnT, bOn, head) = item
                hS = spool.acquire()
                assert hS is not None
                mS, bmS = hS
                P.op("pe", lambda e: e.matmul(mS[0:64, bnk, :], ones65[64:65, 0:64], rr[64:65, :], start=True, stop=True), [brr, B_c2], [bmS])
                P.op("dve", lambda e: e.tensor_tensor(out=OnT[:, head, :], in0=Osb[0:64, :], in1=mS[0:64, bnk, :], op=ALU.mult),
                     [bOsb, bmS], [bOn[head]])
                spool.release(hS)

            def y_unit(gi, dc, OnT, bOn, ysb, bys):
                hS = spool.acquire()
                assert hS is not None
                mS, bmS = hS
                P.op("pe", [(lambda e, h=h: e.matmul(mS[:, 0, :], wao[:, h, dc * 128:(dc + 1) * 128], OnT[:, h, :], start=(h == 0), stop=(h == 7)))
                            for h in range(8)], bOn + [B_c2], [bmS])
                P.op("dve", lambda e: e.tensor_copy(out=ysb[:, dc % 4, :], in_=mS[:, 0, :]), [bmS], [bys[dc % 4]])
                spool.release(hS)
                if dc % 4 == 3:
                    P.dma("sp", lambda e: e.dma_start(out=yatt_d[gi][:, dc - 3:dc + 1, :], in_=ysb[:]), bys, [])

            run_pipeline((q_tile(t) for t in range(32)), 4)
            P.barrier()
            units = []
            for gi in range(8):
                OnT, bOn = OnTs.next()
                bqT = bqTt[gi * 4:gi * 4 + 4]
                for g in range(4):
                    pend = []

                    def try_issue(kt, g=g, gi=gi, bqT=bqT, pend=pend):
                        hS = spool.acquire()
                        if hS is None:
                            return False
                        S, bS = hS
                        P.op("pe", [lambda e: e.matmul(S[:, 0, :], kT_all[0:64, kt * 128:(kt + 1) * 128], qT_all[0:64, g, gi * 512:(gi + 1) * 512], start=True, stop=True),
                                    lambda e: e.matmul(S[:, 1, :], kT_all[64:128, kt * 128:(kt + 1) * 128], qT_all[64:128, g, gi * 512:(gi + 1) * 512], start=True, stop=True)],
                             bqT, [bS])
                        pend.append(hS)
                        return True
                    nxt = 0
                    for kt in range(64):
                        while nxt < 64 and nxt <= kt + LA and try_issue(nxt):
                            nxt += 1
                        hS = pend.pop(0)
                        S, bS = hS
                        Pt, bP = Ps.next()
                        P.op("act", lambda e, S=S, Pt=Pt: e.activation(out=Pt[:], in_=S[:], func=AF.Exp), [bS], [bP])
                        spool.release(hS)
                        if units and kt >= 3 and kt % 3 == 0:
                            units.pop(0)()
                        P.op("pe", [lambda e, Pt=Pt, kt=kt: e.matmul(Ops[0:65, 0, :], V_all[:, kt, 0, :], Pt[:, 0, :], start=(kt == 0), stop=(kt == 63)),
                                    lambda e, Pt=Pt, kt=kt: e.matmul(Ops[0:65, 1, :], V_all[:, kt, 1, :], Pt[:, 1, :], start=(kt == 0), stop=(kt == 63))],
                             [bP], bO)
                    for hh in range(2):
                        head = g + 4 * hh
                        Osb, bOsb = Osbs.next()
                        rr, brr = rrs.next()
                        P.op("act", lambda e, Osb=Osb, hh=hh: e.activation(out=Osb[:], in_=Ops[0:65, hh, :], func=AF.Copy), [bO[hh]], [bOsb])
                        P.op("dve", lambda e, Osb=Osb, rr=rr: e.reciprocal(out=rr[64:65, :], in_=Osb[64:65, :]), [bOsb], [brr])
                        item = (Osb, bOsb, rr, brr, OnT, bOn, head)
                        units.append(lambda item=item, hh=hh: epi_unit(item, hh))
                ysb, bys = ysbs.next()
                for dc in range(8):
                    units.append(lambda gi=gi, dc=dc, OnT=OnT, bOn=bOn, ysb=ysb, bys=bys: y_unit(gi, dc, OnT, bOn, ysb, bys))
            while units:
                units.pop(0)()
            P.flush()

        attn_stack.close()
        if dbg == 2:
            dy = nc.dram_tensor("dbg_yatt", [8, 128, 8, 512], F32, kind="ExternalOutput").ap()
            P.dma("sp", lambda e: e.dma_start(out=dy.rearrange("a p c t -> (a p) (c t)"), in_=yatt_d.rearrange("a p c t -> (a p) (c t)")), [], [])
            P.flush()
            return nc

        with contextlib.ExitStack() as ph:
            wckv = sb(ph, "wckv", [128, 8, 1024], BF16)
            gmemT = sb(ph, "gmemT", [128, 8])
            memT = sb(ph, "memT", [128, 8, 256], BF16)
            B_cm = Buf("cm")
            P.dma("pool", lambda e: e.dma_start(out=wckv[:], in_=wv(w_ckv, 0, 1024)), [], [B_cm])
            P.dma("sp", lambda e: e.dma_start(out=gmemT[:], in_=gT_view(g_mem), allow_slow_non_contiguous=True), [], [B_cm])
            st = norm_state(ph, "pm")
            xts = Rot([(sb(ph, "pmx%d" % i, [128, 1024]), Buf("x")) for i in range(2)])
            tp, btp = ps(ph, "pmtp", [128, 8, 128], BF16), Buf("tp", True)
            mps = Rot([(ps(ph, "pmps%d" % i, [128, 512]), Buf("mps", True)) for i in range(2)])
            bmemT = [Buf("memT0"), Buf("memT1")]
            for mt in range(2):
                xt, bx = xts.next()
                P.dma("sp", lambda e, xt=xt, mt=mt: e.dma_start(out=xt[:], in_=memd[mt * 128:(mt + 1) * 128, :]), [], [bx])
                norm_T(st, xt[:], bx, gmemT, memT[:, :, mt * 128:(mt + 1) * 128], bmemT[mt], tp, btp)
            for h in range(4):
                mp, bmp = mps.next()
                P.op("pe", [(lambda e, c=c, h=h, mp=mp: e.matmul(mp[:, 0:256], wckv[:, c, h * 128:(h + 1) * 128], memT[:, c, :], start=(c == 0), stop=(c == 7)))
                            for c in range(8)], bmemT + [B_cm], [bmp])
                P.op("act", lambda e, h=h, mp=mp: e.activation(out=kcT[:, h, :], in_=mp[:, 0:256], func=AF.Copy), [bmp], [])
            for mt in range(2):
                mp, bmp = mps.next()
                P.op("pe", [(lambda e, c=c, mt=mt, mp=mp: e.matmul(mp[:], memT[:, c, mt * 128:(mt + 1) * 128], wckv[:, c, 512:1024], start=(c == 0), stop=(c == 7)))
                            for c in range(8)], bmemT + [B_cm], [bmp])
                P.op("act", lambda e, mt=mt, mp=mp: e.activation(out=Vc[:, mt, :], in_=mp[:], func=AF.Copy), [bmp], [])
            P.flush()

        with contextlib.ExitStack() as ph:
            wsv = sb(ph, "wsv", [128, 8, 512], BF16)
            wsu = sb(ph, "wsu", [128, 8, 512], BF16)
            wga = sb(ph, "wga", [128, 8, 1024], BF16)
            wgb = sb(ph, "wgb", [128, 8, 1024], BF16)
            wso = sb(ph, "wso", [128, 4, 1024], BF16)
            wo = sb(ph, "wo", [128, 8, 1024], BF16)
            wcq = sb(ph, "wcq", [128, 8, 512], BF16)
            wco = sb(ph, "wco", [128, 4, 1024], BF16)
            gcrT = sb(ph, "gcrT", [128, 8])
            lng_bc = sb(ph, "lng_bc", [128, 512])
            lnb_bc = sb(ph, "lnb_bc", [128, 512])
            bs_bc = sb(ph, "bs_bc", [128, 4, 128])
            wsraw = sb(ph, "wsraw", [128, 4, 128], BF16)
            WsT = sb(ph, "WsT", [128, 4, 128], BF16)
            B_c = Buf("c2b")
            B_ws = Buf("ws")
            P.dma("pool", lambda e: e.dma_start(out=wsu[:], in_=wv(w_in, 768, 1280)), [], [B_c])
            P.dma("pool", lambda e: e.dma_start(out=wsv[:], in_=wv(w_in, 1280, 1792)), [], [B_c])
            P.dma("pool", lambda e: e.dma_start(out=wga[:], in_=wv(w_in, 1792, 2816)), [], [B_c])
            P.dma("pool", lambda e: e.dma_start(out=wgb[:], in_=wv(w_in, 2816, 3840)), [], [B_c])
            P.dma("pool", lambda e: e.dma_start(out=wso[:], in_=wv(w_sgu_out, 0, 1024)), [], [B_c])
            P.dma("pool", lambda e: e.dma_start(out=wo[:], in_=wv(w_out, 0, 1024)), [], [B_c])
            P.dma("pool", lambda e: e.dma_start(out=wcq[:], in_=wv(w_cq, 0, 512)), [], [B_c])
            P.dma("pool", lambda e: e.dma_start(out=wco[:], in_=wv(w_co, 0, 1024)), [], [B_c])
            P.dma("pool", lambda e: e.dma_start(out=wsraw[:], in_=w_s.rearrange("g p q -> p g q")), [], [B_ws])
            P.dma("sp", lambda e: e.dma_start(out=gcrT[:], in_=gT_view(g_cross), allow_slow_non_contiguous=True), [], [B_c])
            P.dma("sp", lambda e: e.dma_start(out=lng_bc[:], in_=sgu_ln_g.partition_broadcast(128)), [], [B_c])
            P.dma("sp", lambda e: e.dma_start(out=lnb_bc[:], in_=sgu_ln_b.partition_broadcast(128)), [], [B_c])
            P.dma("sp", lambda e: e.dma_start(out=bs_bc[:].rearrange("p g q -> p (g q)"), in_=b_s.rearrange("g q -> (g q)").partition_broadcast(128)), [], [B_c])
            P.barrier()
            st = norm_state(ph, "p3", nsets=4, nxb=4)
            bpool = Pool([(ps(ph, "p3g%d" % i, [128, 512]), Buf("g", True)) for i in range(8)])
            hb0 = bpool.acquire()
            tpb0 = bfview(hb0[0][:])
            P.op("pe", [(lambda e, g=g: e.transpose(tpb0[:, g, :], wsraw[:, g, :], ident[:])) for g in range(4)], [B_ws], [hb0[1]])
            P.op("act", lambda e: e.activation(out=WsT[:], in_=tpb0[:, 0:4, :], func=AF.Copy), [hb0[1]], [B_c])
            bpool.release(hb0)
            P.barrier()

            xg = [(sb(ph, "p3x%d" % i, [128, 1024]), Buf("x")) for i in range(4)]
            hTg, bhT = sb(ph, "p3hT", [128, 8, 512], BF16), [Buf("hT") for _ in range(4)]
            hcT, bhc = hTg, bhT
            NA = 3
            asets = Pool([dict(gv=sb(ph, "p3gv%d" % i, [128, 512]), vn=sb(ph, "p3vn%d" % i, [128, 512], BF16), lst=sb(ph, "p3lst%d" % i, [128, 16]),
                               bgv=Buf("gv"), bvn=Buf("vn"), bl=[Buf("l") for _ in range(8)]) for i in range(NA)])
            vmb, bvmb = sb(ph, "p3vmb", [128, 4, 512]), [Buf("vmb") for _ in range(4)]
            gus = Pool([(sb(ph, "p3gu%d" % i, [128, 512]), Buf("gu")) for i in range(2)])
            sguT, bsg = sb(ph, "p3sguT", [128, 4, 512], BF16), [Buf("sg") for _ in range(4)]
            csets = Pool([dict(ya=sb(ph, "p3ya%d" % i, [128, 512]), sa=sb(ph, "p3sa%d" % i, [128, 512]), sb=sb(ph, "p3sb%d" % i, [128, 512]),
                               bya=Buf("ya"), bsa=Buf("sa"), bsb=Buf("sb")) for i in range(2)])
            mrg, bmrg = sb(ph, "p3mrg", [128, 8, 512], BF16), [Buf("mrg") for _ in range(8)]
            qcT, bqc = sb(ph, "p3qcT", [128, 4, 512], BF16), [Buf("qc") for _ in range(4)]
            gsets = Pool([dict(cst=sb(ph, "p3cst%d" % i, [128, 16]), pc=sb(ph, "p3pc%d" % i, [128, 4, 256]), pn=sb(ph, "p3pn%d" % i, [128, 4, 256], BF16),
                               pnT=sb(ph, "p3pnT%d" % i, [128, 8, 128], BF16), ocT=sb(ph, "p3ocT%d" % i, [128, 4, 128], BF16),
                               bc=[Buf("c") for _ in range(4)], bpc=Buf("pc"), bpn=Buf("pn"), bpnT=Buf("pnT"), boc=Buf("oc")) for i in range(2)])
            print("sbuf bytes remaining in 2b:", nc.sbuf_bytes_remaining)

            def a_tile(gi, j):
                tile = gi * 4 + j
                xt, bx = xg[j]
                P.dma("sp", lambda e: e.dma_start(out=xt[:], in_=xown[tile * 128:(tile + 1) * 128, :]), [], [bx])
                yield
                yield from norm_T_gen(st, xt[:], bx, gmixT, hTg[:, :, j * 128:(j + 1) * 128], bhT[j], None, None, pool=bpool)
                A = yield from acq(asets)
                gv, vn, lst, bgv, bvn, bl = A["gv"], A["vn"], A["lst"], A["bgv"], A["bvn"], A["bl"]
                h0 = yield from acq(bpool)
                g0, bg0 = h0
                P.op("pe", [(lambda e, c=c: e.matmul(g0[:], hTg[:, c, j * 128:(j + 1) * 128], wsv[:, c, :], start=(c == 0), stop=(c == 7)))
                            for c in range(8)], [bhT[j], B_c], [bg0])
                yield
                P.op("act", lambda e: e.activation(out=gv[:], in_=g0[:], func=AF.Gelu_apprx_tanh), [bg0], [bgv])
                bpool.release(h0)
                yield
                P.op("dve", lambda e: e.tensor_reduce(out=lst[:, 0:1], in_=gv[:], axis=AX.X, op=ALU.add), [bgv], [bl[0]])
                yield
                P.op("act", lambda e: e.activation(out=vn[:], in_=gv[:], func=AF.Square, accum_out=lst[:, 1:2]), [bgv], [bvn, bl[1]])
                yield
                P.op("dve", lambda e: e.tensor_scalar(out=lst[:, 2:3], in0=lst[:, 0:1], scalar1=1.0 / 512, scalar2=None, op0=ALU.mult), [bl[0]], [bl[2]])
                yield
                P.op("dve", lambda e: e.tensor_tensor(out=lst[:, 3:4], in0=lst[:, 2:3], in1=lst[:, 2:3], op=ALU.mult), [bl[2]], [bl[3]])
                yield
                P.op("dve", lambda e: e.scalar_tensor_tensor(out=lst[:, 4:5], in0=lst[:, 1:2], scalar=1.0 / 512, in1=lst[:, 3:4], op0=ALU.mult, op1=ALU.subtract),
                     [bl[1], bl[3]], [bl[4]])
                yield
                P.op("dve", lambda e: e.tensor_scalar(out=lst[:, 5:6], in0=lst[:, 4:5], scalar1=EPS, scalar2=None, op0=ALU.add), [bl[4]], [bl[5]])
                yield
                P.op("act", lambda e: e.activation(out=lst[:, 6:7], in_=lst[:, 5:6], func=AF.Ln), [bl[5]], [bl[6]])
                yield
                P.op("act", lambda e: e.activation(out=lst[:, 7:8], in_=lst[:, 6:7], func=AF.Exp, scale=-0.5), [bl[6]], [bl[7]])
                yield
                P.op("dve", lambda e: e.tensor_scalar(out=gv[:], in0=gv[:], scalar1=lst[:, 2:3], scalar2=lst[:, 7:8], op0=ALU.subtract, op1=ALU.mult),
                     [bgv, bl[2], bl[7]], [bgv])
                yield
                P.op("dve", lambda e: e.tensor_tensor(out=gv[:], in0=gv[:], in1=lng_bc[:], op=ALU.mult), [bgv, B_c], [bgv])
                yield
                P.op("dve", lambda e: e.tensor_tensor(out=vn[:], in0=gv[:], in1=lnb_bc[:], op=ALU.add), [bgv, B_c], [bvn])
                yield
                h1 = yield from acq(bpool)
                g1, bg1 = h1
                P.op("pe", [(lambda e, g=g: e.matmul(g1[:, g * 128:(g + 1) * 128], vn[:, g * 128:(g + 1) * 128], WsT[:, g, :], start=True, stop=True))
                            for g in range(4)], [bvn, B_c], [bg1])
                yield
                P.op("dve", lambda e: e.tensor_tensor(out=vmb[:, :, j * 128:(j + 1) * 128], in0=g1[:].rearrange("p (g q) -> p g q", g=4),
                                                      in1=bs_bc[:], op=ALU.add), [bg1, B_c], [bvmb[j]])
                bpool.release(h1)
                asets.release(A)
                yield

            def b_chunk(g):
                h0 = yield from acq(bpool)
                g0, bg0 = h0
                P.op("pe", [(lambda e, c=c: e.matmul(g0[:], wsu[:, c, g * 128:(g + 1) * 128], hTg[:, c, :], start=(c == 0), stop=(c == 7)))
                            for c in range(8)], bhT + [B_c], [bg0])
                yield
                G = yield from acq(gus)
                gu, bgu = G
                P.op("act", lambda e: e.activation(out=gu[:], in_=g0[:], func=AF.Gelu_apprx_tanh), [bg0], [bgu])
                bpool.release(h0)
                yield
                P.op("dve", lambda e: e.tensor_tensor(out=sguT[:, g, :], in0=gu[:], in1=vmb[:, g, :], op=ALU.mult), [bgu] + bvmb, [bsg[g]])
                gus.release(G)
                yield

            def c_chunk(gi, dc):
                C = yield from acq(csets)
                ya, sa, sbt, bya, bsa, bsb = C["ya"], C["sa"], C["sb"], C["bya"], C["bsa"], C["bsb"]
                P.dma("sp", lambda e: e.dma_start(out=ya[:], in_=yatt_d[gi][:, dc, :]), [], [bya])
                yield
                h0 = yield from acq(bpool)
                ga_ps, bga = h0
                P.op("pe", [(lambda e, c=c: e.matmul(ga_ps[:], wga[:, c, dc * 128:(dc + 1) * 128], hTg[:, c, :], start=(c == 0), stop=(c == 7)))
                            for c in range(8)], bhT + [B_c], [bga])
                yield
                P.op("act", lambda e: e.activation(out=sa[:], in_=ga_ps[:], func=AF.Sigmoid), [bga], [bsa])
                bpool.release(h0)
                yield
                h1 = yield from acq(bpool)
                gb_ps, bgb = h1
                P.op("pe", [(lambda e, c=c: e.matmul(gb_ps[:], wgb[:, c, dc * 128:(dc + 1) * 128], hTg[:, c, :], start=(c == 0), stop=(c == 7)))
                            for c in range(8)], bhT + [B_c], [bgb])
                yield
                P.op("act", lambda e: e.activation(out=sbt[:], in_=gb_ps[:], func=AF.Sigmoid), [bgb], [bsb])
                bpool.release(h1)
                yield
                h2 = yield from acq(bpool)
                ys_ps, bysp = h2
                P.op("pe", [(lambda e, g=g: e.matmul(ys_ps[:], wso[:, g, dc * 128:(dc + 1) * 128], sguT[:, g, :], start=(g == 0), stop=(g == 3)))
                            for g in range(4)], bsg + [B_c], [bysp])
                yield
                P.op("dve", lambda e: e.tensor_tensor(out=sa[:], in0=sa[:], in1=ya[:], op=ALU.mult), [bsa, bya], [bsa])
                yield
                P.op("dve", lambda e: e.tensor_tensor(out=sbt[:], in0=sbt[:], in1=ys_ps[:], op=ALU.mult), [bsb, bysp], [bsb])
                bpool.release(h2)
                yield
                P.op("dve", lambda e: e.tensor_tensor(out=mrg[:, dc, :], in0=sa[:], in1=sbt[:], op=ALU.add), [bsa, bsb], [bmrg[dc]])
                csets.release(C)
                yield

            def d_part(j, nb):
                xt, bx = xg[j]
                h0 = yield from acq(bpool)
                o_ps, bo = h0
                P.op("pe", [(lambda e, dc=dc: e.matmul(o_ps[:], mrg[:, dc, j * 128:(j + 1) * 128], wo[:, dc, nb * 512:(nb + 1) * 512],
                                                      start=(dc == 0), stop=(dc == 7))) for dc in range(8)], bmrg + [B_c], [bo])
                yield
                P.op("dve", lambda e: e.tensor_tensor(out=xt[:, nb * 512:(nb + 1) * 512], in0=xt[:, nb * 512:(nb + 1) * 512], in1=o_ps[:], op=ALU.add),
                     [bo, bx], [bx])
                bpool.release(h0)
                yield

            def e_tile(j):
                xt, bx = xg[j]
                yield from norm_T_gen(st, xt[:], bx, gcrT, hcT[:, :, j * 128:(j + 1) * 128], bhc[j], None, None, pool=bpool)

            def f_head(h):
                h0 = yield from acq(bpool)
                q_ps, bq = h0
                P.op("pe", [(lambda e, c=c: e.matmul(q_ps[:], wcq[:, c, h * 128:(h + 1) * 128], hcT[:, c, :], start=(c == 0), stop=(c == 7)))
                            for c in range(8)], bhc + [B_c], [bq])
                yield
                P.op("act", lambda e: e.activation(out=qcT[:, h, :], in_=q_ps[:], func=AF.Copy, scale=float(128 ** -0.5)), [bq], [bqc[h]])
                bpool.release(h0)
                yield

            def g_tile(gi, j):
                tile = gi * 4 + j
                xt, bx = xg[j]
                Gs = yield from acq(gsets)
                cst, pc, pn, pnT, ocT = Gs["cst"], Gs["pc"], Gs["pn"], Gs["pnT"], Gs["ocT"]
                bc, bpc, bpn, bpnT, boc = Gs["bc"], Gs["bpc"], Gs["bpn"], Gs["bpnT"], Gs["boc"]
                hs0 = yield from acq(bpool)
                hs1 = yield from acq(bpool)
                scs = [hs0[0], hs1[0]]
                bscs = [hs0[1], hs1[1]]
                for half in range(2):
                    P.op("pe", [(lambda e, hh=hh, half=half: e.matmul(scs[half][:, hh * 256:(hh + 1) * 256], qcT[:, half * 2 + hh, j * 128:(j + 1) * 128], kcT[:, half * 2 + hh, :],
                                                                       start=True, stop=True)) for hh in range(2)], bqc, [bscs[half]])
                    yield
                for half in range(2):
                    P.op("dve", lambda e, half=half: e.tensor_reduce(out=cst[:, half * 2:half * 2 + 2], in_=scs[half][:].rearrange("p (h m) -> p h m", h=2), axis=AX.X, op=ALU.max),
                         [bscs[half]], [bc[0]])
                    yield
                P.op("dve", lambda e: e.tensor_scalar(out=cst[:, 4:8], in0=cst[:, 0:4], scalar1=-1.0, scalar2=None, op0=ALU.mult), [bc[0]], [bc[1]])
                yield
                for half in range(2):
                    P.op("act", [(lambda e, hh=hh, half=half: e.activation(out=pc[:, half * 2 + hh, :], in_=scs[half][:, hh * 256:(hh + 1) * 256], func=AF.Exp,
                                                                          bias=cst[:, 4 + half * 2 + hh:5 + half * 2 + hh], accum_out=cst[:, 8 + half * 2 + hh:9 + half * 2 + hh]))
                                 for hh in range(2)], [bscs[half], bc[1]], [bpc, bc[2]])
                    yield
                bpool.release(hs0)
                bpool.release(hs1)
                P.op("dve", lambda e: e.reciprocal(out=cst[:, 12:16], in_=cst[:, 8:12]), [bc[2]], [bc[3]])
                yield
                P.op("dve", lambda e: e.tensor_tensor(out=pn[:], in0=pc[:], in1=cst[:, 12:16].unsqueeze(2).broadcast_to([128, 4, 256]), op=ALU.mult),
                     [bpc, bc[3]], [bpn])
                yield
                ht = yield from acq(bpool)
                tpv = bfview(ht[0][:])
                P.op("pe", [(lambda e, k=k: e.transpose(tpv[:, k, :], pn[:, k // 2, (k % 2) * 128:(k % 2 + 1) * 128], ident[:])) for k in range(8)], [bpn], [ht[1]])
                yield
                P.op("act", lambda e: e.activation(out=pnT[:], in_=tpv, func=AF.Copy), [ht[1]], [bpnT])
                bpool.release(ht)
                yield
                ho = yield from acq(bpool)
                oc_ps, bocp = ho
                fl = []
                for h in range(4):
                    for mt in range(2):
                        fl.append(lambda e, h=h, mt=mt: e.matmul(oc_ps[:, h * 128:(h + 1) * 128], Vc[:, mt, h * 128:(h + 1) * 128], pnT[:, h * 2 + mt, :],
                                                                 start=(mt == 0), stop=(mt == 1)))
                P.op("pe", fl, [bpnT], [bocp])
                yield
                P.op("act", lambda e: e.activation(out=ocT[:], in_=oc_ps[:].rearrange("p (h t) -> p h t", h=4), func=AF.Copy), [bocp], [boc])
                bpool.release(ho)
                yield
                for nb in range(2):
                    hy = yield from acq(bpool)
                    y_ps, byp = hy
                    P.op("pe", [(lambda e, h=h, nb=nb, y_ps=y_ps: e.matmul(y_ps[:], ocT[:, h, :], wco[:, h, nb * 512:(nb + 1) * 512], start=(h == 0), stop=(h == 3)))
                                for h in range(4)], [boc, B_c], [byp])
                    yield
                    P.op("dve", lambda e, nb=nb, y_ps=y_ps: e.tensor_tensor(out=xt[:, nb * 512:(nb + 1) * 512], in0=xt[:, nb * 512:(nb + 1) * 512], in1=y_ps[:], op=ALU.add),
                         [byp, bx], [bx])
                    bpool.release(hy)
                    yield
                gsets.release(Gs)
                P.dma("sp", lambda e: e.dma_start(out=x2_d[tile * 128:(tile + 1) * 128, :], in_=xt[:]), [bx], [])
                yield

            for gi in range(8):
                run_pipeline((a_tile(gi, j) for j in range(4)), 4)
                run_pipeline((b_chunk(g) for g in range(4)), 3)
                run_pipeline((c_chunk(gi, dc) for dc in range(8)), 3)
                run_pipeline((d_part(j, nb) for j in range(4) for nb in range(2)), 4)
                run_pipeline((e_tile(j) for j in range(4)), 4)
                run_pipeline((f_head(h) for h in range(4)), 4)
                run_pipeline((g_tile(gi, j) for j in range(4)), 3)
            P.flush()

        if dbg == 3:
            dx = nc.dram_tensor("dbg_x2", [OWN, D], F32, kind="ExternalOutput").ap()
            P.dma("sp", lambda e: e.dma_start(out=dx[:, :], in_=x2_d[:, :]), [], [])
            P.flush()
            return nc

        NI = 32
        IT = 512
        NS = NI * IT
        hs_d = nc.dram_tensor("hs_scr", [NS, D], BF16, kind="Internal").ap()
        ys_d = nc.dram_tensor("ys_scr", [NS, D], F32, kind="Internal").ap()
        trid = din("tri", [128, 128])
        iotad = din("iota32", [128, 32])
        thrd = din("thr8", [128, 8])
        with contextlib.ExitStack() as ph:
            gmoT = sb(ph, "gmoT", [128, 8])
            wr = sb(ph, "wr", [128, 8, 20], BF16)
            br_bc = sb(ph, "br_bc", [128, 20])
            gfin_bc = sb(ph, "gfin_bc", [128, 1024])
            oh1 = sb(ph, "oh1", [128, 32, 16])
            oh2 = sb(ph, "oh2", [128, 32, 16])
            wts = sb(ph, "wts", [128, 32, 2])
            idx1 = sb(ph, "idx1", [128, 32], mybir.dt.int32)
            idx2 = sb(ph, "idx2", [128, 32], mybir.dt.int32)
            Ei = sb(ph, "Ei", [128, 32], mybir.dt.int32)
            Widx = sb(ph, "Widx", [128, 32], mybir.dt.int32)
            pidx = sb(ph, "pidx", [128, 1])
            P.dma("sp", lambda e: e.dma_start(out=pidx[:], in_=pidxd[:, :]), [], [])
            B_c = Buf("c3")
            P.dma("sp", lambda e: e.dma_start(out=gmoT[:], in_=gT_view(g_moe), allow_slow_non_contiguous=True), [], [B_c])
            P.dma("pool", lambda e: e.dma_start(out=wr[:, :, 0:4], in_=wv(w_rg, 0, 4)), [], [B_c])
            P.dma("pool", lambda e: e.dma_start(out=wr[:, :, 4:20], in_=wv(w_re, 0, 16)), [], [B_c])
            P.dma("sp", lambda e: e.dma_start(out=br_bc[:, 0:4], in_=b_rg.partition_broadcast(128)), [], [B_c])
            P.dma("sp", lambda e: e.dma_start(out=br_bc[:, 4:20], in_=b_re.partition_broadcast(128)), [], [B_c])
            P.dma("sp", lambda e: e.dma_start(out=gfin_bc[:], in_=g_final.partition_broadcast(128)), [], [B_c])
            allbanks = [(ps(ph, "p4g%d" % i, [128, 512]), Buf("g", True)) for i in range(8)]
            bpool = Pool(allbanks)
            gen = Rot(allbanks)

            with contextlib.ExitStack() as sp1:
                hm_all = sb(sp1, "hm_all", [128, 32, 1024], BF16)
                gmo_bc = sb(sp1, "gmo_bc", [128, 1024])
                tri = sb(sp1, "tri", [128, 128], BF16)
                onesb = sb(sp1, "onesb", [128, 128], BF16)
                iota32 = sb(sp1, "iota32", [128, 32])
                thr8 = sb(sp1, "thr8", [128, 8])
                Mb = sb(sp1, "Mb", [128, 32, 16], BF16)
                pos_all = sb(sp1, "pos_all", [128, 32, 16])
                base = sb(sp1, "base", [128, 16])
                P.dma("sp", lambda e: e.dma_start(out=gmo_bc[:], in_=g_moe.partition_broadcast(128)), [], [B_c])
                P.dma("pool", lambda e: e.dma_start(out=tri[:], in_=trid[:, :]), [], [B_c])
                P.dma("sp", lambda e: e.dma_start(out=iota32[:], in_=iotad[:, :]), [], [B_c])
                P.dma("sp", lambda e: e.dma_start(out=thr8[:], in_=thrd[:, :]), [], [B_c])
                P.op("pool", lambda e: e.memset(onesb[:], 1.0), [], [B_c])
                P.op("pool", lambda e: e.memset(base[:], 0.0), [], [B_c])
                P.barrier()
                NP3 = 6
                st = norm_state(sp1, "p4", nsets=NP3, nxb=NP3)
                xts = Rot([(sb(sp1, "p4x%d" % i, [128, 1024]), Buf("x")) for i in range(NP3)])
                hTt = Rot([(sb(sp1, "p4h%d" % i, [128, 8, 128], BF16), Buf("h")) for i in range(NP3)])
                rts = Rot([(sb(sp1, "rt%d" % i, [128, 160]), [Buf("rt") for _ in range(24)]) for i in range(NP3)])
                boh = [Buf("oh%d" % i) for i in range(32)]
                bhm = [Buf("hm%d" % i) for i in range(32)]

                def moe_tile(i):
                    xt, bx = xts.next()
                    P.dma("sp", lambda e: e.dma_start(out=xt[:], in_=x2_d[i * 128:(i + 1) * 128, :]), [], [bx])
                    yield
                    hT, bh = hTt.next()
                    yield from norm_T_gen(st, xt[:], bx, gmoT, hT[:], bh, None, None, pool=bpool, tok=(hm_all[:, i, :], gmo_bc[:], B_c, bhm[i]))
                    hb = yield from acq(bpool)
                    r_ps, brp = hb
                    P.op("pe", [(lambda e, c=c: e.matmul(r_ps[:, 0:20], hT[:, c, :], wr[:, c, :], start=(c == 0), stop=(c == 7)))
                                for c in range(8)], [bh, B_c], [brp])
                    yield
                    R, brt = rts.next()
                    o1 = oh1[:, i, :]
                    o2 = oh2[:, i, :]
                    P.op("dve", lambda e: e.tensor_tensor(out=R[:, 0:20], in0=r_ps[:, 0:20], in1=br_bc[:], op=ALU.add), [brp, B_c], [brt[0]])
                    bpool.release(hb)
                    yield
                    P.op("dve", lambda e: e.tensor_reduce(out=R[:, 20:21], in_=R[:, 0:4], axis=AX.X, op=ALU.max), [brt[0]], [brt[1]])
                    yield
                    P.op("dve", lambda e: e.tensor_scalar(out=R[:, 21:22], in0=R[:, 20:21], scalar1=-1.0, scalar2=None, op0=ALU.mult), [brt[1]], [brt[2]])
                    yield
                    P.op("act", lambda e: e.activation(out=R[:, 22:26], in_=R[:, 0:4], func=AF.Exp, bias=R[:, 21:22], accum_out=R[:, 26:27]), [brt[0], brt[2]], [brt[3]])
                    yield
                    P.op("dve", lambda e: e.reciprocal(out=R[:, 27:28], in_=R[:, 26:27]), [brt[3]], [brt[4]])
                    yield
                    P.op("dve", lambda e: e.tensor_scalar(out=R[:, 28:32], in0=R[:, 0:4], scalar1=R[:, 20:21], scalar2=None, op0=ALU.is_ge), [brt[0], brt[1]], [brt[5]])
                    yield
                    P.op("dve", lambda e: e.tensor_scalar(out=R[:, 32:36], in0=R[:, 28:32], scalar1=BIG, scalar2=-BIG, op0=ALU.mult, op1=ALU.add), [brt[5]], [brt[6]])
                    yield
                    P.op("dve", lambda e: e.tensor_tensor(out=R[:, 40:56].rearrange("p (g k) -> p g k", g=4), in0=R[:, 4:20].rearrange("p (g k) -> p g k", g=4),
                                                          in1=R[:, 32:36].unsqueeze(2).broadcast_to([128, 4, 4]), op=ALU.add), [brt[0], brt[6]], [brt[7]])
                    yield
                    P.op("dve", lambda e: e.tensor_reduce(out=R[:, 56:57], in_=R[:, 40:56], axis=AX.X, op=ALU.max), [brt[7]], [brt[8]])
                    yield
                    P.op("dve", lambda e: e.tensor_scalar(out=o1, in0=R[:, 40:56], scalar1=R[:, 56:57], scalar2=None, op0=ALU.is_ge), [brt[7], brt[8]], [brt[9]])
                    yield
                    P.op("dve", lambda e: e.scalar_tensor_tensor(out=R[:, 80:96], in0=o1, scalar=-BIG, in1=R[:, 40:56], op0=ALU.mult, op1=ALU.add),
                         [brt[9], brt[7]], [brt[10]])
                    yield
                    P.op("dve", lambda e: e.tensor_reduce(out=R[:, 96:97], in_=R[:, 80:96], axis=AX.X, op=ALU.max), [brt[10]], [brt[11]])
                    yield
                    P.op("dve", lambda e: e.tensor_scalar(out=o2, in0=R[:, 80:96], scalar1=R[:, 96:97], scalar2=None, op0=ALU.is_ge), [brt[10], brt[11]], [brt[12]])
                    yield
                    P.op("dve", lambda e: e.tensor_tensor(out=R[:, 116:117], in0=R[:, 96:97], in1=R[:, 56:57], op=ALU.subtract), [brt[11], brt[8]], [brt[13]])
                    yield
                    P.op("act", lambda e: e.activation(out=R[:, 117:118], in_=R[:, 116:117], func=AF.Exp), [brt[13]], [brt[14]])
                    yield
                    P.op("dve", lambda e: e.tensor_scalar(out=R[:, 118:119], in0=R[:, 117:118], scalar1=1.0, scalar2=None, op0=ALU.add), [brt[14]], [brt[15]])
                    yield
                    P.op("dve", lambda e: e.reciprocal(out=R[:, 119:120], in_=R[:, 118:119]), [brt[15]], [brt[16]])
                    yield
                    P.op("dve", lambda e: e.tensor_tensor(out=wts[:, i, 0:1], in0=R[:, 119:120], in1=R[:, 27:28], op=ALU.mult), [brt[16], brt[4]], [brt[17]])
                    yield
                    P.op("dve", lambda e: e.tensor_tensor(out=wts[:, i, 1:2], in0=wts[:, i, 0:1], in1=R[:, 117:118], op=ALU.mult), [brt[17], brt[14]], [brt[18]])
                    yield
                    P.op("dve", lambda e: e.tensor_tensor(out=Mb[:, i, :], in0=o1, in1=o2, op=ALU.add), [brt[9], brt[12]], [boh[i]])
                    yield

                run_pipeline((moe_tile(i) for i in range(32)), NP3)
                P.barrier()
                prefb = Rot([bpool.acquire(), bpool.acquire()])
                bbase = Buf("base")
                bpos = Buf("pos")
                for i in range(32):
                    pb, bpb = prefb.next()
                    P.op("pe", [lambda e, pb=pb, i=i: e.matmul(pb[:, 0:16], tri[:], Mb[:, i, :], start=True, stop=True),
                                lambda e, pb=pb, i=i: e.matmul(pb[:, 16:32], onesb[:], Mb[:, i, :], start=True, stop=True)], [], [bpb])
                    P.op("dve", lambda e, pb=pb, i=i: e.tensor_tensor(out=pos_all[:, i, :], in0=pb[:, 0:16], in1=base[:], op=ALU.add), [bpb, bbase], [bpos])
                    P.op("dve", lambda e, pb=pb: e.tensor_tensor(out=base[:], in0=pb[:, 16:32], in1=base[:], op=ALU.add), [bpb, bbase], [bbase])
                for it_ in prefb.items:
                    bpool.release(it_)
                P.barrier()
                sm = sb(sp1, "sm", [128, 256])
                big = sb(sp1, "bigt", [128, 32, 16])
                big2 = sb(sp1, "bigt2", [128, 32, 16])
                sf = sb(sp1, "sf", [128, 64])
                bs_ = Buf("sm")

                def S1(fn, eng="dve"):
                    P.op(eng, fn, [bs_], [bs_])
                S1(lambda e: e.tensor_tensor(out=sm[:, 0:128].rearrange("p (a k) -> p a k", k=8), in0=base[:].unsqueeze(2).broadcast_to([128, 16, 8]),
                                             in1=thr8[:].unsqueeze(1).broadcast_to([128, 16, 8]), op=ALU.is_gt))
                S1(lambda e: e.tensor_reduce(out=sm[:, 128:144], in_=sm[:, 0:128].rearrange("p (a k) -> p a k", k=8), axis=AX.X, op=ALU.add))
                S1(lambda e: e.tensor_copy(out=sm[:, 144:160], in_=sm[:, 128:144]))
                cur, oth = 144, 160
                for k in (1, 2, 4, 8):
                    S1(lambda e, cur=cur, oth=oth, k=k: e.tensor_copy(out=sm[:, oth:oth + k], in_=sm[:, cur:cur + k]))
                    S1(lambda e, cur=cur, oth=oth, k=k: e.tensor_tensor(out=sm[:, oth + k:oth + 16], in0=sm[:, cur + k:cur + 16], in1=sm[:, cur:cur + 16 - k], op=ALU.add))
                    cur, oth = oth, cur
                S1(lambda e, cur=cur: e.tensor_copy(out=sm[:, 176:192], in_=sm[:, cur:cur + 16]))
                S1(lambda e: e.tensor_tensor(out=sm[:, 192:208], in0=sm[:, 176:192], in1=sm[:, 128:144], op=ALU.subtract))
                S1(lambda e: e.tensor_scalar(out=sm[:, 192:208], in0=sm[:, 192:208], scalar1=float(IT), scalar2=None, op0=ALU.mult))
                S1(lambda e: e.memset(sm[:, 208:240], 0.0))
                for ex in range(NE):
                    S1(lambda e, ex=ex: e.scalar_tensor_tensor(out=sm[:, 208:240], in0=iota32[:], scalar=sm[:, 176 + ex:177 + ex], in1=sm[:, 208:240], op0=ALU.is_ge, op1=ALU.add))
                S1(lambda e: e.tensor_scalar(out=sm[:, 208:240], in0=sm[:, 208:240], scalar1=float(NE - 1), scalar2=None, op0=ALU.min))
                S1(lambda e: e.tensor_copy(out=Ei[:], in_=sm[:, 208:240]))
                S1(lambda e: e.tensor_scalar(out=sf[:, 0:32], in0=sm[:, 208:240], scalar1=128.0, scalar2=pidx[:, 0:1], op0=ALU.mult, op1=ALU.add))
                S1(lambda e: e.tensor_copy(out=Widx[:], in_=sf[:, 0:32]))
                S1(lambda e: e.tensor_tensor(out=big[:], in0=pos_all[:], in1=sm[:, 192:208].unsqueeze(1).broadcast_to([128, 32, 16]), op=ALU.add))
                S1(lambda e: e.tensor_tensor(out=big2[:], in0=big[:], in1=oh1[:], op=ALU.mult))
                S1(lambda e: e.tensor_reduce(out=sf[:, 0:32], in_=big2[:], axis=AX.X, op=ALU.add))
                S1(lambda e: e.tensor_tensor(out=big2[:], in0=big[:], in1=oh2[:], op=ALU.mult))
                S1(lambda e: e.tensor_reduce(out=sf[:, 32:64], in_=big2[:], axis=AX.X, op=ALU.add))
                S1(lambda e: e.tensor_copy(out=idx1[:], in_=sf[:, 0:32]))
                S1(lambda e: e.tensor_copy(out=idx2[:], in_=sf[:, 32:64]))
                P.barrier()
                for i in range(int(_os0.environ.get("NSCAT", 32))):
                    for ixt in (idx1, idx2):
                        P.dma("pool", lambda e, i=i, ixt=ixt: e.indirect_dma_start(out=hs_d[:, :], out_offset=bass.IndirectOffsetOnAxis(ap=ixt[:, i:i + 1], axis=0),
                                                                                   in_=hm_all[:, i, :], in_offset=None), [], [])
                P.barrier()

            if dbg == 4:
                d1 = nc.dram_tensor("dbg_idx", [128, 64], mybir.dt.int32, kind="ExternalOutput").ap()
                d2 = nc.dram_tensor("dbg_E", [128, 32], mybir.dt.int32, kind="ExternalOutput").ap()
                d3 = nc.dram_tensor("dbg_wts", [128, 64], F32, kind="ExternalOutput").ap()
                P.dma("sp", lambda e: e.dma_start(out=d1[:, 0:32], in_=idx1[:]), [], [])
                P.dma("sp", lambda e: e.dma_start(out=d1[:, 32:64], in_=idx2[:]), [], [])
                P.dma("sp", lambda e: e.dma_start(out=d2[:, :], in_=Ei[:]), [], [])
                P.dma("sp", lambda e: e.dma_start(out=d3[:, :], in_=wts[:].rearrange("p a b -> p (a b)")), [], [])
                P.flush()
                return nc

            with contextlib.ExitStack() as sp2:
                Wb = [(sb(sp2, "Wb%d" % i, [128, 12288], BF16), Buf("Wb")) for i in range(2)]
                hss = Rot([(sb(sp2, "hs%d" % i, [128, 4, 1024], BF16), Buf("hs")) for i in range(2)])
                hsTs = Rot([(sb(sp2, "hsT%d" % i, [128, 8, 512], BF16), [Buf("hsT") for _ in range(4)]) for i in range(2)])
                sgs = Rot([(sb(sp2, "sg%d" % i, [128, 512]), Buf("sg")) for i in range(2)])
                heTs = Rot([(sb(sp2, "heT%d" % i, [128, 4, 512], BF16), [Buf("he") for _ in range(4)]) for i in range(2)])
                yss = Rot([(sb(sp2, "ys%d" % i, [128, 1024]), Buf("ys")) for i in range(3)])

                def load_w(w):
                    wb, bwb = Wb[w % 2]
                    P.dma("pool", lambda e: e.indirect_dma_start(out=wb[:, :], out_offset=None, in_=wcat_bf[:, :],
                                                                 in_offset=bass.IndirectOffsetOnAxis(ap=Widx[:, w:w + 1], axis=0)), [B_wbf], [bwb])

                load_w(0)
                for w in range(NI):
                    if w + 1 < NI:
                        load_w(w + 1)
                    wb, bwb = Wb[w % 2]
                    wg = wb[:, 0:4096].rearrange("p (c f) -> p c f", c=8)
                    wu = wb[:, 4096:8192].rearrange("p (c f) -> p c f", c=8)
                    wd = wb[:, 8192:12288].rearrange("p (c f) -> p c f", c=4)
                    bwg = bwu = bwd = bwb
                    hs, bhs = hss.next()
                    P.dma("sp", lambda e, hs=hs, w=w: e.dma_start(out=hs[:], in_=hs_d[w * IT:(w + 1) * IT, :].rearrange("(s p) d -> p s d", p=128)), [], [bhs])
                    hsT, bhsT = hsTs.next()
                    for sub in range(4):
                        tb, btb = gen.next()
                        tpv = bfview(tb[:])
                        P.op("pe", [(lambda e, c=c, sub=sub, tpv=tpv, hs=hs: e.transpose(tpv[:, c, :], hs[:, sub, c * 128:(c + 1) * 128], ident[:])) for c in range(8)],
                             [bhs], [btb])
                        if sub % 2 == 0:
                            P.op("act", lambda e, sub=sub, tpv=tpv, hsT=hsT: e.activation(out=hsT[:, :, sub * 128:(sub + 1) * 128], in_=tpv, func=AF.Copy), [btb], [bhsT[sub]])
                        else:
                            P.op("dve", lambda e, sub=sub, tpv=tpv, hsT=hsT: e.tensor_copy(out=hsT[:, :, sub * 128:(sub + 1) * 128], in_=tpv), [btb], [bhsT[sub]])
                    heT, bhe = heTs.next()
                    for fc in range(4):
                        g_ps, bgp = gen.next()
                        u_ps, bup = gen.next()
                        P.op("pe", [(lambda e, c=c, fc=fc, g_ps=g_ps, wg=wg, hsT=hsT: e.matmul(g_ps[:], wg[:, c, fc * 128:(fc + 1) * 128], hsT[:, c, :],
                                                                                          start=(c == 0), stop=(c == 7))) for c in range(8)], bhsT + [bwg], [bgp])
                        P.op("pe", [(lambda e, c=c, fc=fc, u_ps=u_ps, wu=wu, hsT=hsT: e.matmul(u_ps[:], wu[:, c, fc * 128:(fc + 1) * 128], hsT[:, c, :],
                                                                                          start=(c == 0), stop=(c == 7))) for c in range(8)], bhsT + [bwu], [bup])
                        sg, bsg_ = sgs.next()
                        P.op("act", lambda e, sg=sg, g_ps=g_ps: e.activation(out=sg[:], in_=g_ps[:], func=AF.Silu), [bgp], [bsg_])
                        P.op("dve", lambda e, sg=sg, u_ps=u_ps, heT=heT, fc=fc: e.tensor_tensor(out=heT[:, fc, :], in0=sg[:], in1=u_ps[:], op=ALU.mult),
                             [bsg_, bup], [bhe[fc]])
                    for sub in range(4):
                        ys, bys = yss.next()
                        for nb in range(2):
                            y_ps, byp = gen.next()
                            P.op("pe", [(lambda e, fc=fc, sub=sub, nb=nb, y_ps=y_ps, heT=heT, wd=wd: e.matmul(y_ps[:], heT[:, fc, sub * 128:(sub + 1) * 128], wd[:, fc, nb * 512:(nb + 1) * 512],
                                                                                                             start=(fc == 0), stop=(fc == 3))) for fc in range(4)], bhe + [bwd], [byp])
                            if nb == 0:
                                P.op("act", lambda e, ys=ys, y_ps=y_ps: e.activation(out=ys[:, 0:512], in_=y_ps[:], func=AF.Copy), [byp], [bys])
                            else:
                                P.op("dve", lambda e, ys=ys, y_ps=y_ps: e.tensor_copy(out=ys[:, 512:1024], in_=y_ps[:]), [byp], [bys])
                        P.dma("sp", lambda e, ys=ys, w=w, sub=sub: e.dma_start(out=ys_d[w * IT + sub * 128:w * IT + (sub + 1) * 128, :], in_=ys[:]), [bys], [])
                P.barrier()

            with contextlib.ExitStack() as sp3:
                NC3 = 3
                st = norm_state(sp3, "p5", nsets=4, nxb=1)
                xts = Rot([(sb(sp3, "p5x%d" % i, [128, 1024]), Buf("x")) for i in range(NC3)])
                y0s = Rot([(sb(sp3, "p5y0%d" % i, [128, 1024]), Buf("y0")) for i in range(NC3)])
                y1s = Rot([(sb(sp3, "p5y1%d" % i, [128, 1024]), Buf("y1")) for i in range(NC3)])
                ots = Rot([(sb(sp3, "p5o%d" % i, [128, 1024]), Buf("ot")) for i in range(NC3)])

                def fin_tile(i):
                    xt, bx = xts.next()
                    y0, by0 = y0s.next()
                    y1, by1 = y1s.next()
                    P.dma("sp", lambda e: e.dma_start(out=xt[:], in_=x2_d[i * 128:(i + 1) * 128, :]), [], [bx])
                    yield
                    P.dma("pool", lambda e: e.indirect_dma_start(out=y0[:, :], out_offset=None, in_=ys_d[:, :],
                                                                 in_offset=bass.IndirectOffsetOnAxis(ap=idx1[:, i:i + 1], axis=0)), [], [by0])
                    yield
                    P.dma("pool", lambda e: e.indirect_dma_start(out=y1[:, :], out_offset=None, in_=ys_d[:, :],
                                                                 in_offset=bass.IndirectOffsetOnAxis(ap=idx2[:, i:i + 1], axis=0)), [], [by1])
                    yield
                    P.op("dve", lambda e: e.scalar_tensor_tensor(out=xt[:], in0=y0[:], scalar=wts[:, i, 0:1], in1=xt[:], op0=ALU.mult, op1=ALU.add), [by0, bx], [bx])
                    yield
                    P.op("dve", lambda e: e.scalar_tensor_tensor(out=xt[:], in0=y1[:], scalar=wts[:, i, 1:2], in1=xt[:], op0=ALU.mult, op1=ALU.add), [by1, bx], [bx])
                    yield
                    fss, fb, _u = st["sets"].next()
                    ot, bot = ots.next()
                    P.op("act", lambda e: e.activation(out=ot[:], in_=xt[:], func=AF.Square, scale=1.0 / 32, accum_out=fss[:, 0:1]), [bx], [fb[0], bot])
                    yield
                    P.op("dve", lambda e: e.tensor_scalar(out=fss[:, 1:2], in0=fss[:, 0:1], scalar1=EPS, scalar2=None, op0=ALU.add), [fb[0]], [fb[1]])
                    yield
                    P.op("act", lambda e: e.activation(out=fss[:, 2:3], in_=fss[:, 1:2], func=AF.Ln), [fb[1]], [fb[2]])
                    yield
                    P.op("act", lambda e: e.activation(out=fss[:, 3:4], in_=fss[:, 2:3], func=AF.Exp, scale=-0.5), [fb[2]], [fb[3]])
                    yield
                    P.op("dve", lambda e: e.scalar_tensor_tensor(out=ot[:], in0=xt[:], scalar=fss[:, 3:4], in1=gfin_bc[:], op0=ALU.mult, op1=ALU.mult),
                         [bx, fb[3], B_c], [bot])
                    yield
                    P.dma("sp", lambda e: e.dma_start(out=out[i * 128:(i + 1) * 128, :], in_=ot[:]), [bot], [])
                    yield

                run_pipeline((fin_tile(i) for i in range(32)), NC3)
                P.barrier()
            P.flush()
    return nc


def _rope_tables():
    rows = SEQ // 64
    row_ids = np.repeat(np.arange(rows), 64).astype(np.float32)
    col_ids = np.tile(np.arange(64), rows).astype(np.float32)
    inv = (10000.0 ** (-np.arange(16, dtype=np.float32) / 16)).astype(np.float32)
    ang = np.concatenate([row_ids[:, None] * inv[None, :], col_ids[:, None] * inv[None, :]], axis=-1).astype(np.float32)
    return np.cos(ang).astype(np.float32), np.sin(ang).astype(np.float32)


def _wcat(wg, wu, wd):
    g = wg.reshape(NE, 8, 128, 512).transpose(0, 2, 1, 3).reshape(NE, 128, 4096)
    u = wu.reshape(NE, 8, 128, 512).transpose(0, 2, 1, 3).reshape(NE, 128, 4096)
    d_ = wd.reshape(NE, 4, 128, 1024).transpose(0, 2, 1, 3).reshape(NE, 128, 4096)
    return np.ascontiguousarray(np.concatenate([g, u, d_], axis=2).reshape(NE * 128, 12288))


def make_in_maps(inputs):
    f = lambda a: np.ascontiguousarray(np.asarray(a, dtype=np.float32))
    x = f(inputs["x"])
    mem = f(inputs["mem"])
    cos, sin = _rope_tables()

    def lay(t, n):
        return np.ascontiguousarray(t.reshape(n, 128, 32).transpose(1, 0, 2))

    shared = {
        "ident": np.eye(128, dtype=np.float32),
        "g_mix": f(inputs["g_mix"][0]), "w_in": f(inputs["w_in"][0]), "g_q": f(inputs["g_q"][0]), "g_k": f(inputs["g_k"][0]),
        "w_att_out": f(inputs["w_att_out"][0]), "sgu_ln_g": f(inputs["sgu_ln_g"][0]), "sgu_ln_b": f(inputs["sgu_ln_b"][0]),
        "w_s": f(inputs["w_s"][0]), "b_s": f(inputs["b_s"][0]), "w_sgu_out": f(inputs["w_sgu_out"][0]), "w_out": f(inputs["w_out"][0]),
        "g_cross": f(inputs["g_cross"][0]), "g_mem": f(inputs["g_mem"][0]), "w_cq": f(inputs["w_cq"][0]), "w_ckv": f(inputs["w_ckv"][0]),
        "w_co": f(inputs["w_co"][0]), "g_moe": f(inputs["g_moe"][0]), "w_rg": f(inputs["w_rg"][0]), "b_rg": f(inputs["b_rg"][0]),
        "w_re": f(inputs["w_re"][0]), "b_re": f(inputs["b_re"][0]), "g_final": f(inputs["g_final"]),
        "wcat": _wcat(f(inputs["w_gate"][0]), f(inputs["w_up"][0]), f(inputs["w_down"][0])),
        "pidx": np.arange(128, dtype=np.float32).reshape(128, 1),
        "cos_seq": lay(cos, 64), "sin_seq": lay(sin, 64),
        "tri": np.triu(np.ones((128, 128), np.float32), 1),
        "iota32": np.tile(np.arange(32, dtype=np.float32), (128, 1)),
        "thr8": np.tile(np.arange(8, dtype=np.float32) * 512.0, (128, 1)),
    }
    maps = []
    for c in range(8):
        b, hf = c // 2, c % 2
        m = dict(shared)
        m["xseq"] = x[b]
        m["xown"] = np.ascontiguousarray(x[b, hf * OWN:(hf + 1) * OWN])
        m["mem"] = mem[b]
        m["cos_own"] = lay(cos[hf * OWN:(hf + 1) * OWN], 32)
        m["sin_own"] = lay(sin[hf * OWN:(hf + 1) * OWN], 32)
        maps.append(m)
    return maps


def kernel(**inputs):
    nc = build()
    maps = make_in_maps(inputs)
    res = run_bass_kernel_spmd(nc, maps, core_ids=list(range(8)))
    outp = np.empty((4, SEQ, D), np.float32)
    for c in range(8):
        b, hf = c // 2, c % 2
        outp[b, hf * OWN:(hf + 1) * OWN] = np.asarray(res.results[c]["out"], dtype=np.float32)
    return outp
```

```python
import contextlib
import os as _os0
import numpy as np
import concourse.bass as bass
import concourse.mybir as mybir
from concourse.bass_utils import run_bass_kernel_spmd

F32 = mybir.dt.float32
BF16 = mybir.dt.bfloat16
AF = mybir.ActivationFunctionType
ALU = mybir.AluOpType
AX = mybir.AxisListType

D = 1024
SEQ = 8192
OWN = 4096
NMEM = 256
EPS = 1e-6
NE = 16
BIG = 1.0e30


class Buf:
    __slots__ = ("name", "w", "r", "psum")

    def __init__(self, name="", psum=False):
        self.name = name
        self.psum = psum
        self.w = None
        self.r = {}


class Eng:
    def __init__(self, name):
        self.name = name
        self.sem = None
        self.count = 0
        self.seen = {}
        self.prog = []
        self.chsems = []
        self.dma_i = 0


class Prog:
    NCH = int(_os0.environ.get('NCH', 8))

    def __init__(self, nc, stack):
        self.nc = nc
        self.E = {}
        for n in ("pe", "act", "dve", "pool", "sp"):
            e = Eng(n)
            e.sem = stack.enter_context(nc.semaphore("sem_" + n))
            self.E[n] = e
        for n in ("sp", "pool", "act"):
            e = self.E[n]
            for i in range(self.NCH):
                e.chsems.append(stack.enter_context(nc.semaphore("ch_%s_%d" % (n, i))))
        self.chan_issued = {}
        import os
        self.limit = int(os.environ.get("OPLIMIT", 10 ** 9))
        self.nops = 0

    def _need(self, need, ev):
        sem, val, src = ev
        k = id(sem)
        if k not in need or need[k][1] < val:
            need[k] = (sem, val, src)

    def _deps(self, eng, reads, writes, extra=()):
        E = self.E[eng]
        need = {}
        for b in reads:
            if b.w is not None:
                self._need(need, b.w)
        for b in writes:
            if b.w is not None:
                self._need(need, b.w)
            for k, (sem, val, src) in b.r.items():
                self._need(need, (sem, val, src))
        for ev in extra:
            self._need(need, ev)
        for k, (sem, val, src) in need.items():
            if src == "pe" and eng == "pe":
                continue
            if E.seen.get(k, 0) >= val:
                continue
            E.seen[k] = val
            E.prog.append(("wait", sem, val))

    def _commit(self, ev, reads, writes):
        sem, val, src = ev
        for b in writes:
            b.w = ev
            b.r = {}
        for b in reads:
            if b not in writes:
                b.r[id(sem)] = (sem, val, src)

    def op(self, eng, fns, reads=(), writes=()):
        if not isinstance(fns, (list, tuple)):
            fns = [fns]
        self.nops += 1
        if self.nops > self.limit:
            return
        pr = [b for b in reads if b.psum]
        if pr:
            reads = [b for b in reads if not b.psum]
            writes = list(writes) + [b for b in pr if b not in writes]
        E = self.E[eng]
        self._deps(eng, reads, writes)
        E.count += 1
        for f in fns[:-1]:
            E.prog.append(("raw", f))
        E.prog.append(("op", fns[-1], E.sem))
        self._commit((E.sem, E.count, eng), reads, writes)

    def dma(self, q, fn, reads=(), writes=()):
        self.nops += 1
        if self.nops > self.limit:
            return
        E = self.E[q]
        ch = E.dma_i % self.NCH
        rnd = E.dma_i // self.NCH
        E.dma_i += 1
        chsem = E.chsems[ch]
        extra = []
        if rnd > 0:
            extra.append((chsem, 16 * rnd, "dma"))
        self._deps(q, reads, writes, extra)
        E.prog.append(("dma", fn, chsem))
        self.chan_issued[id(chsem)] = (chsem, 16 * (rnd + 1))
        self._commit((chsem, 16 * (rnd + 1), "dma"), reads, writes)

    def dma_bg(self, q, fn, sem, total, buf):
        E = self.E[q]
        E.prog.append(("dma", fn, sem))
        buf.w = (sem, total, "dma")

    def barrier(self):
        for n, E in self.E.items():
            for m, F in self.E.items():
                if m == n or F.count == 0:
                    continue
                k = id(F.sem)
                if E.seen.get(k, 0) < F.count:
                    E.seen[k] = F.count
                    E.prog.append(("wait", F.sem, F.count))
            for k, (sem, val) in self.chan_issued.items():
                if E.seen.get(k, 0) < val:
                    E.seen[k] = val
                    E.prog.append(("wait", sem, val))

    def flush(self):
        self.barrier()
        nc = self.nc
        progs = {n: E.prog for n, E in self.E.items()}
        for E in self.E.values():
            E.prog = []

        def replay(eng, prog):
            for it in prog:
                if it[0] == "wait":
                    eng.wait_ge(it[1], it[2])
                elif it[0] == "raw":
                    it[1](eng)
                elif it[0] == "op":
                    it[1](eng).then_inc(it[2], 1)
                else:
                    it[1](eng).then_inc(it[2], 16)

        with nc.Block() as block:
            @block.tensor
            def _(e):
                replay(e, progs["pe"])

            @block.scalar
            def _(e):
                replay(e, progs["act"])

            @block.vector
            def _(e):
                replay(e, progs["dve"])

            @block.gpsimd
            def _(e):
                replay(e, progs["pool"])

            @block.sync
            def _(e):
                replay(e, progs["sp"])


class Rot:
    def __init__(self, items):
        self.items = items
        self.i = 0

    def next(self):
        it = self.items[self.i % len(self.items)]
        self.i += 1
        return it


class Pool:
    def __init__(self, items):
        self.free = list(items)

    def acquire(self):
        return self.free.pop(0) if self.free else None

    def release(self, it):
        self.free.append(it)


def build(dbg=False):
    nc = bass.Bass("TRN2", target_bir_lowering=False)

    def din(name, shape):
        return nc.dram_tensor(name, list(shape), F32, kind="ExternalInput").ap()

    xseq = din("xseq", [SEQ, D])
    xown = din("xown", [OWN, D])
    memd = din("mem", [NMEM, D])
    cos_seq = din("cos_seq", [128, 64, 32])
    sin_seq = din("sin_seq", [128, 64, 32])
    cos_own = din("cos_own", [128, 32, 32])
    sin_own = din("sin_own", [128, 32, 32])
    identd = din("ident", [128, 128])
    g_mix = din("g_mix", [D])
    w_in = din("w_in", [D, 3840])
    g_q = din("g_q", [64])
    g_k = din("g_k", [64])
    w_att_out = din("w_att_out", [512, D])
    sgu_ln_g = din("sgu_ln_g", [512])
    sgu_ln_b = din("sgu_ln_b", [512])
    w_s = din("w_s", [4, 128, 128])
    b_s = din("b_s", [4, 128])
    w_sgu_out = din("w_sgu_out", [512, D])
    w_out = din("w_out", [D, D])
    g_cross = din("g_cross", [D])
    g_mem = din("g_mem", [D])
    w_cq = din("w_cq", [D, 512])
    w_ckv = din("w_ckv", [D, D])
    w_co = din("w_co", [512, D])
    g_moe = din("g_moe", [D])
    w_rg = din("w_rg", [D, 4])
    b_rg = din("b_rg", [4])
    w_re = din("w_re", [D, 16])
    b_re = din("b_re", [16])
    wcat = din("wcat", [NE * 128, 12288])
    pidxd = din("pidx", [128, 1])
    wcat_bf = nc.dram_tensor("wcat_bf", [NE * 128, 12288], BF16, kind="Internal").ap()
    g_final = din("g_final", [D])
    out = nc.dram_tensor("out", [OWN, D], F32, kind="ExternalOutput").ap()
    yatt_d = nc.dram_tensor("yatt_scr", [8, 128, 8, 512], F32, kind="Internal").ap()
    x2_d = nc.dram_tensor("x2_scr", [OWN, D], F32, kind="Internal").ap()

    def wv(w, c0, c1):
        return w.rearrange("(c p) n -> p c n", p=128)[:, :, c0:c1]

    def gT_view(g):
        return g.rearrange("(c p) -> p c", p=128)

    with contextlib.ExitStack() as top:
        P = Prog(nc, top)
        bgsem = top.enter_context(nc.semaphore("bgsem"))
        B_wbf = Buf("wcat_bf")
        print("sbuf bytes remaining at start:", nc.sbuf_bytes_remaining)

        def sb(stack, name, shape, dt=F32):
            return stack.enter_context(nc.sbuf_tensor("s_" + name, list(shape), dt))

        def ps(stack, name, shape, dt=F32):
            return stack.enter_context(nc.psum_tensor("p_" + name, list(shape), dt))

        ident = sb(top, "ident", [128, 128], BF16)
        kcT = sb(top, "kcT", [128, 4, NMEM], BF16)
        Vc = sb(top, "Vc", [128, 2, 512], BF16)
        gmixT = sb(top, "gmixT", [128, 8])
        attn_stack = contextlib.ExitStack()
        kT_all = sb(attn_stack, "kT_all", [128, SEQ], BF16)
        V_all = sb(attn_stack, "V_all", [128, 64, 2, 65], BF16)
        B_const = Buf("const")

        P.dma("pool", lambda e: e.dma_start(out=ident[:], in_=identd[:, :]), [], [B_const])
        P.dma("sp", lambda e: e.dma_start(out=gmixT[:], in_=gT_view(g_mix), allow_slow_non_contiguous=True), [], [B_const])
        P.op("pool", lambda e: e.memset(V_all[:], 1.0), [], [B_const])

        def run_pipeline(gens, depth):
            it = iter(gens)
            active = []
            more = True
            while True:
                if more and len(active) < depth:
                    try:
                        active.append(next(it))
                    except StopIteration:
                        more = False
                if not active:
                    break
                for g in list(active):
                    try:
                        next(g)
                    except StopIteration:
                        active.remove(g)

        def drain(g):
            for _ in g:
                pass

        def acq(pool):
            while True:
                it = pool.acquire()
                if it is not None:
                    return it
                yield

        def bfview(bank_ap):
            return bank_ap.bitcast(BF16).rearrange("p (c t) -> p c t", c=8)

        def norm_T_gen(st, xt_ap, xbuf, gT, out_ap3, outbuf, tps, tpsb, pool=None, tok=None):
            ss, b, _unused = st["sets"].next()
            xb, bxb = st["xb"].next()
            P.op("act", lambda e: e.activation(out=xb[:], in_=xt_ap, func=AF.Square, scale=1.0 / 32,
                                               accum_out=ss[:, 0:1]), [xbuf], [b[0], bxb])
            yield
            P.op("dve", lambda e: e.tensor_scalar(out=ss[:, 1:2], in0=ss[:, 0:1], scalar1=EPS, scalar2=None,
                                                  op0=ALU.add), [b[0]], [b[1]])
            yield
            P.op("act", lambda e: e.activation(out=ss[:, 2:3], in_=ss[:, 1:2], func=AF.Ln), [b[1]], [b[2]])
            yield
            P.op("act", lambda e: e.activation(out=ss[:, 3:4], in_=ss[:, 2:3], func=AF.Exp, scale=-0.5), [b[2]], [b[3]])
            yield
            P.op("act", lambda e: e.activation(out=xb[:], in_=xt_ap, func=AF.Copy, scale=ss[:, 3:4]), [xbuf, b[3]], [bxb])
            yield
            if tok is not None:
                P.op("dve", lambda e: e.tensor_tensor(out=tok[0], in0=xb[:], in1=tok[1], op=ALU.mult), [bxb, tok[2]], [tok[3]])
                yield
            held = None
            if pool is not None:
                while held is None:
                    held = pool.acquire()
                    if held is None:
                        yield
                tps, tpsb = bfview(held[0][:]), held[1]
            P.op("pe", [(lambda e, c=c: e.transpose(tps[:, c, :], xb[:, c * 128:(c + 1) * 128], ident[:])) for c in range(8)],
                 [bxb], [tpsb])
            yield
            P.op("dve", lambda e: e.tensor_tensor(out=out_ap3, in0=tps, in1=gT[:, 0:8].unsqueeze(2).broadcast_to([128, 8, 128]),
                                                  op=ALU.mult), [tpsb], [outbuf])
            if held is not None:
                pool.release(held)
            yield

        def norm_T(st, xt_ap, xbuf, gT, out_ap3, outbuf, tps, tpsb):
            drain(norm_T_gen(st, xt_ap, xbuf, gT, out_ap3, outbuf, tps[:], tpsb))

        def norm_state(stack, pfx, nsets=2, nxb=2):
            st = {}
            st["sets"] = Rot([(sb(stack, pfx + "ss%d" % i, [128, 4]), [Buf("ss") for _ in range(4)], None) for i in range(nsets)])
            st["xb"] = Rot([(sb(stack, pfx + "xb%d" % i, [128, 1024], BF16), Buf("xb")) for i in range(nxb)])
            return st

        def head_norm_rope_gen(ws, src_ps, bsrc, H, g_bc, cosb, sinb, dst, bdst, t):
            if isinstance(ws, Pool):
                w = yield from acq(ws)
            else:
                w = ws.next()
            n = H * 64
            src3 = src_ps.rearrange("p (h d) -> p h d", h=H)
            P.op("act", lambda e: e.activation(out=w["sq"][:, 0:n], in_=src_ps, func=AF.Square, scale=0.125), [bsrc], [w["bsq"]])
            yield
            P.op("dve", lambda e: e.tensor_reduce(out=w["hs"][:, 0:H], in_=w["sq"][:, 0:n].rearrange("p (h d) -> p h d", h=H),
                                                  axis=AX.X, op=ALU.add), [w["bsq"]], [w["bhs"]])
            yield
            P.op("dve", lambda e: e.tensor_scalar(out=w["hs"][:, 8:8 + H], in0=w["hs"][:, 0:H], scalar1=EPS, scalar2=None, op0=ALU.add),
                 [w["bhs"]], [w["bhs1"]])
            yield
            P.op("act", lambda e: e.activation(out=w["hs"][:, 16:16 + H], in_=w["hs"][:, 8:8 + H], func=AF.Ln), [w["bhs1"]], [w["bhs2"]])
            yield
            P.op("act", lambda e: e.activation(out=w["hs"][:, 24:24 + H], in_=w["hs"][:, 16:16 + H], func=AF.Exp, scale=-0.5),
                 [w["bhs2"]], [w["bhs3"]])
            yield
            qn3 = w["qn"][:, 0:n].rearrange("p (h d) -> p h d", h=H)
            P.op("dve", lambda e: e.tensor_tensor(out=qn3, in0=src3, in1=w["hs"][:, 24:24 + H].unsqueeze(2).broadcast_to([128, H, 64]),
                                                  op=ALU.mult), [bsrc, w["bhs3"]], [w["bqn"]])
            yield
            P.op("dve", lambda e: e.tensor_tensor(out=qn3, in0=qn3, in1=g_bc[:, :].unsqueeze(1).broadcast_to([128, H, 64]),
                                                  op=ALU.mult), [w["bqn"]], [w["bqn"]])
            yield
            qn4 = w["qn"][:, 0:n].rearrange("p (h i t) -> p h i t", h=H, t=2)
            A4 = w["A"][:, 0:n].rearrange("p (h i t) -> p h i t", h=H, t=2)
            B4 = w["B"][:, 0:n].rearrange("p (h i t) -> p h i t", h=H, t=2)
            c4 = cosb[:, t, :].unsqueeze(1).unsqueeze(3).broadcast_to([128, H, 32, 2])
            s4 = sinb[:, t, :].unsqueeze(1).unsqueeze(3).broadcast_to([128, H, 32, 2])
            P.op("dve", lambda e: e.tensor_tensor(out=A4, in0=qn4, in1=c4, op=ALU.mult), [w["bqn"]], [w["bA"]])
            yield
            P.op("dve", lambda e: e.tensor_tensor(out=B4, in0=qn4, in1=s4, op=ALU.mult), [w["bqn"]], [w["bB"]])
            yield
            P.op("dve", lambda e: e.tensor_tensor(out=dst[:, :, :, 0], in0=A4[:, :, :, 0], in1=B4[:, :, :, 1], op=ALU.subtract),
                 [w["bA"], w["bB"]], [bdst])
            yield
            P.op("dve", lambda e: e.tensor_tensor(out=dst[:, :, :, 1], in0=B4[:, :, :, 0], in1=A4[:, :, :, 1], op=ALU.add),
                 [w["bA"], w["bB"]], [bdst])
            if isinstance(ws, Pool):
                ws.release(w)
            yield

        def rope_state(stack, pfx, n, nsets=1):
            sets = []
            for i in range(nsets):
                w = {}
                for k in ("sq", "qn", "A", "B"):
                    w[k] = sb(stack, "%s%s%d" % (pfx, k, i), [128, n])
                w["hs"] = sb(stack, "%shs%d" % (pfx, i), [128, 32])
                for k in ("bsq", "bhs", "bhs1", "bhs2", "bhs3", "bqn", "bA", "bB"):
                    w[k] = Buf(k)
                sets.append(w)
            return Rot(sets)

        with contextlib.ExitStack() as ph:
            wkv = sb(ph, "wkv", [128, 8, 256], BF16)
            cosb = sb(ph, "cosb", [128, 64, 32])
            sinb = sb(ph, "sinb", [128, 64, 32])
            gk_bc = sb(ph, "gk_bc", [128, 64])
            P.dma("pool", lambda e: e.dma_start(out=wkv[:], in_=wv(w_in, 512, 768)), [], [B_const])
            P.dma("sp", lambda e: e.dma_start(out=cosb[:], in_=cos_seq[:, :, :]), [], [B_const])
            P.dma("sp", lambda e: e.dma_start(out=sinb[:], in_=sin_seq[:, :, :]), [], [B_const])
            P.dma("sp", lambda e: e.dma_start(out=gk_bc[:], in_=g_k.partition_broadcast(128)), [], [B_const])
            NP1 = 8
            P.barrier()
            st = norm_state(ph, "p1", nsets=NP1, nxb=NP1)
            rws = rope_state(ph, "p1r", 128, nsets=NP1)
            xts = Rot([(sb(ph, "p1x%d" % i, [128, 1024]), Buf("x")) for i in range(NP1)])
            hTs = Rot([(sb(ph, "p1h%d" % i, [128, 8, 128], BF16), Buf("h")) for i in range(NP1)])
            krs = Rot([(sb(ph, "p1kr%d" % i, [128, 2, 32, 2], BF16), Buf("kr")) for i in range(NP1)])
            banks = Pool([(ps(ph, "p1b%d" % i, [128, 512]), Buf("bank", True)) for i in range(8)])

            import os as _os

            def p1_tile(t):
                xt, bx = xts.next()
                P.dma("sp", lambda e: e.dma_start(out=xt[:], in_=xseq[t * 128:(t + 1) * 128, :]), [], [bx])
                yield
                hT, bh = hTs.next()
                yield from norm_T_gen(st, xt[:], bx, gmixT, hT[:], bh, None, None, pool=banks)
                hkv = yield from acq(banks)
                kv, bkv = hkv
                P.op("pe", [(lambda e, c=c: e.matmul(kv[:, 0:256], hT[:, c, :], wkv[:, c, :], start=(c == 0), stop=(c == 7)))
                            for c in range(8)], [bh, B_const], [bkv])
                yield
                P.op("act", lambda e: e.activation(out=V_all[:, t, :, 0:64], in_=kv[:, 128:256].rearrange("p (h d) -> p h d", h=2),
                                                   func=AF.Copy), [bkv], [])
                yield
                kr, bkr = krs.next()
                yield from head_norm_rope_gen(rws, kv[:, 0:128], bkv, 2, gk_bc, cosb, sinb, kr, bkr, t)
                banks.release(hkv)
                hkt = yield from acq(banks)
                kt, bkt = hkt
                ktv = kt[:].bitcast(BF16)
                P.op("pe", lambda e: e.transpose(ktv[:, 0:128], kr[:].rearrange("p h i t -> p (h i t)"), ident[:]), [bkr], [bkt])
                yield
                P.op("act", lambda e: e.activation(out=kT_all[:, t * 128:(t + 1) * 128], in_=ktv[:, 0:128], func=AF.Copy), [bkt], [])
                banks.release(hkt)
                yield

            run_pipeline((p1_tile(t) for t in range(int(_os.environ.get('NT1', 64)))), int(_os.environ.get('DEPTH1', 8)))
            P.flush()

        if dbg == 1:
            P.limit = 10 ** 9
            P.limit = 10 ** 9
            print("nops", P.nops)
            dk = nc.dram_tensor("dbg_kT", [128, SEQ], BF16, kind="ExternalOutput").ap()
            dv = nc.dram_tensor("dbg_V", [128, 64 * 2 * 65], BF16, kind="ExternalOutput").ap()
            P.dma("sp", lambda e: e.dma_start(out=dk[:, :], in_=kT_all[:]), [], [])
            P.dma("sp", lambda e: e.dma_start(out=dv[:, :], in_=V_all[:].rearrange("p a b c -> p (a b c)")), [], [])
            P.flush()
            attn_stack.close()
            return nc

        with contextlib.ExitStack() as ph:
            wq = sb(ph, "wq", [128, 8, 512], BF16)
            wao = sb(ph, "wao", [64, 8, 1024], BF16)
            cosb = sb(ph, "cosb2", [128, 32, 32])
            sinb = sb(ph, "sinb2", [128, 32, 32])
            gq_bc = sb(ph, "gq_bc", [128, 64])
            ones65 = sb(ph, "ones65", [65, 64])
            B_c2 = Buf("c2")
            for hd_ in range(8):
                pos_ = (hd_ % 4) * 2 + hd_ // 4
                P.dma("pool", lambda e, hd_=hd_, pos_=pos_: e.dma_start(out=wq[:, :, pos_ * 64:(pos_ + 1) * 64], in_=wv(w_in, hd_ * 64, hd_ * 64 + 64)), [], [B_c2])
            P.dma("pool", lambda e: e.dma_start(out=wao[:], in_=w_att_out.rearrange("(h p) n -> p h n", p=64)), [], [B_c2])
            P.dma("sp", lambda e: e.dma_start(out=cosb[:], in_=cos_own[:, :, :]), [], [B_c2])
            P.dma("sp", lambda e: e.dma_start(out=sinb[:], in_=sin_own[:, :, :]), [], [B_c2])
            P.dma("sp", lambda e: e.dma_start(out=gq_bc[:], in_=g_q.partition_broadcast(128)), [], [B_c2])
            P.op("dve", lambda e: e.tensor_scalar(out=gq_bc[:], in0=gq_bc[:], scalar1=0.125, scalar2=None, op0=ALU.mult), [B_c2], [B_c2])
            P.op("pool", lambda e: e.memset(ones65[:], 1.0), [], [B_c2])
            P.barrier()
            st = norm_state(ph, "p2", nsets=4, nxb=4)
            rws = Pool(rope_state(ph, "p2r", 512, nsets=3).items)
            xts = Rot([(sb(ph, "p2x%d" % i, [128, 1024]), Buf("x")) for i in range(4)])
            hTs = Rot([(sb(ph, "p2h%d" % i, [128, 8, 128], BF16), Buf("h")) for i in range(4)])
            qrs = Rot([(sb(ph, "p2qr%d" % i, [128, 8, 32, 2], BF16), Buf("qr")) for i in range(4)])
            Ps = Rot([(sb(ph, "p2P%d" % i, [128, 2, 512], BF16), Buf("P")) for i in range(3)])
            OnTs = Rot([(sb(ph, "p2OnT%d" % i, [64, 8, 512], BF16), [Buf("OnT") for _ in range(8)]) for i in range(2)])
            ysbs = Rot([(sb(ph, "p2ysb%d" % i, [128, 4, 512]), [Buf("ysb") for _ in range(4)]) for i in range(1)])
            Sps = Rot([(ps(ph, "p2S%d" % i, [128, 2, 512]), Buf("S", True)) for i in range(3)])
            Ops, bO = ps(ph, "p2O", [128, 2, 512]), [Buf("O0", True), Buf("O1", True)]
            LA = int(_os0.environ.get('LA', 2))

            sbank = Pool([(Sps.items[i][0][:, bnk, :], Buf("sbank", True)) for i in range(3) for bnk in range(2)])
            Osbs = Rot([(sb(ph, "p2Osc%d" % i, [65, 512]), Buf("Osb")) for i in range(4)])
            rrs = Rot([(sb(ph, "p2rrc%d" % i, [65, 512]), Buf("rr")) for i in range(4)])

            qT_all = sb(ph, "qT_all", [128, 4, OWN], BF16)
            bqTt = [Buf("qTt") for _ in range(32)]
            print("sbuf bytes remaining in 2a:", nc.sbuf_bytes_remaining)

            def q_tile(tile):
                xt, bx = xts.next()
                P.dma("sp", lambda e: e.dma_start(out=xt[:], in_=xown[tile * 128:(tile + 1) * 128, :]), [], [bx])
                yield
                hT, bh = hTs.next()
                yield from norm_T_gen(st, xt[:], bx, gmixT, hT[:], bh, None, None, pool=sbank)
                hq = yield from acq(sbank)
                qps, bqp = hq
                P.op("pe", [(lambda e, c=c: e.matmul(qps, hT[:, c, :], wq[:, c, :], start=(c == 0), stop=(c == 7)))
                            for c in range(8)], [bh, B_c2], [bqp])
                yield
                qr, bqr = qrs.next()
                yield from head_norm_rope_gen(rws, qps, bqp, 8, gq_bc, cosb, sinb, qr, bqr, tile)
                sbank.release(hq)
                ht = yield from acq(sbank)
                tpv = bfview(ht[0])
                qr3 = qr[:].rearrange("p h i t -> p (h i t)")
                P.op("pe", [(lambda e, g=g: e.transpose(tpv[:, g, :], qr3[:, g * 128:(g + 1) * 128], ident[:])) for g in range(4)],
                     [bqr], [ht[1]])
                yield
                P.op("act", lambda e: e.activation(out=qT_all[:, :, tile * 128:(tile + 1) * 128], in_=tpv[:, 0:4, :], func=AF.Copy),
                     [ht[1]], [bqTt[tile]])
                sbank.release(ht)
                yield

            spool = Pool(list(Sps.items))

            def epi_unit(item, bnk):
                (Osb, bOsb, rr, brr, OnT, bOn, head) = item
                hS = spool.acquire()
                assert hS is not None
                mS, bmS = hS
                P.op("pe", lambda e: e.matmul(mS[0:64, bnk, :], ones65[64:65, 0:64], rr[64:65, :], start=True, stop=True), [brr, B_c2], [bmS])
                P.op("dve", lambda e: e.tensor_tensor(out=OnT[:, head, :], in0=Osb[0:64, :], in1=mS[0:64, bnk, :], op=ALU.mult),
                     [bOsb, bmS], [bOn[head]])
                spool.release(hS)

            def y_unit(gi, dc, OnT, bOn, ysb, bys):
                hS = spool.acquire()
                assert hS is not None
                mS, bmS = hS
                P.op("pe", [(lambda e, h=h: e.matmul(mS[:, 0, :], wao[:, h, dc * 128:(dc + 1) * 128], OnT[:, h, :], start=(h == 0), stop=(h == 7)))
                            for h in range(8)], bOn + [B_c2], [bmS])
                P.op("dve", lambda e: e.tensor_copy(out=ysb[:, dc % 4, :], in_=mS[:, 0, :]), [bmS], [bys[dc % 4]])
                spool.release(hS)
                if dc % 4 == 3:
                    P.dma("sp", lambda e: e.dma_start(out=yatt_d[gi][:, dc - 3:dc + 1, :], in_=ysb[:]), bys, [])

            run_pipeline((q_tile(t) for t in range(32)), 4)
            P.barrier()
            for ex_ in range(NE):
                P.dma_bg("pool", lambda e, ex_=ex_: e.dma_start(out=wcat_bf[ex_ * 128:(ex_ + 1) * 128, :], in_=wcat[ex_ * 128:(ex_ + 1) * 128, :]), bgsem, 16 * NE, B_wbf)
            units = []
            for gi in range(8):
                OnT, bOn = OnTs.next()
                bqT = bqTt[gi * 4:gi * 4 + 4]
                for g in range(4):
                    pend = []

                    def try_issue(kt, g=g, gi=gi, bqT=bqT, pend=pend):
                        hS = spool.acquire()
                        if hS is None:
                            return False
                        S, bS = hS
                        P.op("pe", [lambda e: e.matmul(S[:, 0, :], kT_all[0:64, kt * 128:(kt + 1) * 128], qT_all[0:64, g, gi * 512:(gi + 1) * 512], start=True, stop=True),
                                    lambda e: e.matmul(S[:, 1, :], kT_all[64:128, kt * 128:(kt + 1) * 128], qT_all[64:128, g, gi * 512:(gi + 1) * 512], start=True, stop=True)],
                             bqT, [bS])
                        pend.append(hS)
                        return True
                    nxt = 0
                    for kt in range(64):
                        while nxt < 64 and nxt <= kt + LA and try_issue(nxt):
                            nxt += 1
                        hS = pend.pop(0)
                        S, bS = hS
                        Pt, bP = Ps.next()
                        P.op("act", lambda e, S=S, Pt=Pt: e.activation(out=Pt[:], in_=S[:], func=AF.Exp), [bS], [bP])
                        spool.release(hS)
                        if units and kt >= 3 and kt % 3 == 0:
                            units.pop(0)()
                        P.op("pe", [lambda e, Pt=Pt, kt=kt: e.matmul(Ops[0:65, 0, :], V_all[:, kt, 0, :], Pt[:, 0, :], start=(kt == 0), stop=(kt == 63)),
                                    lambda e, Pt=Pt, kt=kt: e.matmul(Ops[0:65, 1, :], V_all[:, kt, 1, :], Pt[:, 1, :], start=(kt == 0), stop=(kt == 63))],
                             [bP], bO)
                    for hh in range(2):
                        head = g + 4 * hh
                        Osb, bOsb = Osbs.next()
                        rr, brr = rrs.next()
                        P.op("act", lambda e, Osb=Osb, hh=hh: e.activation(out=Osb[:], in_=Ops[0:65, hh, :], func=AF.Copy), [bO[hh]], [bOsb])
                        P.op("dve", lambda e, Osb=Osb, rr=rr: e.reciprocal(out=rr[64:65, :], in_=Osb[64:65, :]), [bOsb], [brr])
                        item = (Osb, bOsb, rr, brr, OnT, bOn, head)
                        units.append(lambda item=item, hh=hh: epi_unit(item, hh))
                ysb, bys = ysbs.next()
                for dc in range(8):
                    units.append(lambda gi=gi, dc=dc, OnT=OnT, bOn=bOn, ysb=ysb, bys=bys: y_unit(gi, dc, OnT, bOn, ysb, bys))
            while units:
                units.pop(0)()
            P.flush()

        attn_stack.close()
        if dbg == 2:
            dy = nc.dram_tensor("dbg_yatt", [8, 128, 8, 512], F32, kind="ExternalOutput").ap()
            P.dma("sp", lambda e: e.dma_start(out=dy.rearrange("a p c t -> (a p) (c t)"), in_=yatt_d.rearrange("a p c t -> (a p) (c t)")), [], [])
            P.flush()
            return nc

        with contextlib.ExitStack() as ph:
            wckv = sb(ph, "wckv", [128, 8, 1024], BF16)
            gmemT = sb(ph, "gmemT", [128, 8])
            memT = sb(ph, "memT", [128, 8, 256], BF16)
            B_cm = Buf("cm")
            P.dma("pool", lambda e: e.dma_start(out=wckv[:], in_=wv(w_ckv, 0, 1024)), [], [B_cm])
            P.dma("sp", lambda e: e.dma_start(out=gmemT[:], in_=gT_view(g_mem), allow_slow_non_contiguous=True), [], [B_cm])
            st = norm_state(ph, "pm")
            xts = Rot([(sb(ph, "pmx%d" % i, [128, 1024]), Buf("x")) for i in range(2)])
            tp, btp = ps(ph, "pmtp", [128, 8, 128], BF16), Buf("tp", True)
            mps = Rot([(ps(ph, "pmps%d" % i, [128, 512]), Buf("mps", True)) for i in range(2)])
            bmemT = [Buf("memT0"), Buf("memT1")]
            for mt in range(2):
                xt, bx = xts.next()
                P.dma("sp", lambda e, xt=xt, mt=mt: e.dma_start(out=xt[:], in_=memd[mt * 128:(mt + 1) * 128, :]), [], [bx])
                norm_T(st, xt[:], bx, gmemT, memT[:, :, mt * 128:(mt + 1) * 128], bmemT[mt], tp, btp)
            for h in range(4):
                mp, bmp = mps.next()
                P.op("pe", [(lambda e, c=c, h=h, mp=mp: e.matmul(mp[:, 0:256], wckv[:, c, h * 128:(h + 1) * 128], memT[:, c, :], start=(c == 0), stop=(c == 7)))
                            for c in range(8)], bmemT + [B_cm], [bmp])
                P.op("act", lambda e, h=h, mp=mp: e.activation(out=kcT[:, h, :], in_=mp[:, 0:256], func=AF.Copy), [bmp], [])
            for mt in range(2):
                mp, bmp = mps.next()
                P.op("pe", [(lambda e, c=c, mt=mt, mp=mp: e.matmul(mp[:], memT[:, c, mt * 128:(mt + 1) * 128], wckv[:, c, 512:1024], start=(c == 0), stop=(c == 7)))
                            for c in range(8)], bmemT + [B_cm], [bmp])
                P.op("act", lambda e, mt=mt, mp=mp: e.activation(out=Vc[:, mt, :], in_=mp[:], func=AF.Copy), [bmp], [])
            P.flush()

        with contextlib.ExitStack() as ph:
            wsv = sb(ph, "wsv", [128, 8, 512], BF16)
            wsu = sb(ph, "wsu", [128, 8, 512], BF16)
            wga = sb(ph, "wga", [128, 8, 1024], BF16)
            wgb = sb(ph, "wgb", [128, 8, 1024], BF16)
            wso = sb(ph, "wso", [128, 4, 1024], BF16)
            wo = sb(ph, "wo", [128, 8, 1024], BF16)
            wcq = sb(ph, "wcq", [128, 8, 512], BF16)
            wco = sb(ph, "wco", [128, 4, 1024], BF16)
            gcrT = sb(ph, "gcrT", [128, 8])
            lng_bc = sb(ph, "lng_bc", [128, 512])
            lnb_bc = sb(ph, "lnb_bc", [128, 512])
            bs_bc = sb(ph, "bs_bc", [128, 4, 128])
            wsraw = sb(ph, "wsraw", [128, 4, 128], BF16)
            WsT = sb(ph, "WsT", [128, 4, 128], BF16)
            B_c = Buf("c2b")
            B_ws = Buf("ws")
            P.dma("pool", lambda e: e.dma_start(out=wsu[:], in_=wv(w_in, 768, 1280)), [], [B_c])
            P.dma("pool", lambda e: e.dma_start(out=wsv[:], in_=wv(w_in, 1280, 1792)), [], [B_c])
            P.dma("pool", lambda e: e.dma_start(out=wga[:], in_=wv(w_in, 1792, 2816)), [], [B_c])
            P.dma("pool", lambda e: e.dma_start(out=wgb[:], in_=wv(w_in, 2816, 3840)), [], [B_c])
            P.dma("pool", lambda e: e.dma_start(out=wso[:], in_=wv(w_sgu_out, 0, 1024)), [], [B_c])
            P.dma("pool", lambda e: e.dma_start(out=wo[:], in_=wv(w_out, 0, 1024)), [], [B_c])
            P.dma("pool", lambda e: e.dma_start(out=wcq[:], in_=wv(w_cq, 0, 512)), [], [B_c])
            P.dma("pool", lambda e: e.dma_start(out=wco[:], in_=wv(w_co, 0, 1024)), [], [B_c])
            P.dma("pool", lambda e: e.dma_start(out=wsraw[:], in_=w_s.rearrange("g p q -> p g q")), [], [B_ws])
            P.dma("sp", lambda e: e.dma_start(out=gcrT[:], in_=gT_view(g_cross), allow_slow_non_contiguous=True), [], [B_c])
            P.dma("sp", lambda e: e.dma_start(out=lng_bc[:], in_=sgu_ln_g.partition_broadcast(128)), [], [B_c])
            P.dma("sp", lambda e: e.dma_start(out=lnb_bc[:], in_=sgu_ln_b.partition_broadcast(128)), [], [B_c])
            P.dma("sp", lambda e: e.dma_start(out=bs_bc[:].rearrange("p g q -> p (g q)"), in_=b_s.rearrange("g q -> (g q)").partition_broadcast(128)), [], [B_c])
            P.barrier()
            st = norm_state(ph, "p3", nsets=4, nxb=4)
            bpool = Pool([(ps(ph, "p3g%d" % i, [128, 512]), Buf("g", True)) for i in range(8)])
            hb0 = bpool.acquire()
            tpb0 = bfview(hb0[0][:])
            P.op("pe", [(lambda e, g=g: e.transpose(tpb0[:, g, :], wsraw[:, g, :], ident[:])) for g in range(4)], [B_ws], [hb0[1]])
            P.op("act", lambda e: e.activation(out=WsT[:], in_=tpb0[:, 0:4, :], func=AF.Copy), [hb0[1]], [B_c])
            bpool.release(hb0)
            P.barrier()

            xg = [(sb(ph, "p3x%d" % i, [128, 1024]), Buf("x")) for i in range(4)]
            hTg, bhT = sb(ph, "p3hT", [128, 8, 512], BF16), [Buf("hT") for _ in range(4)]
            hcT, bhc = hTg, bhT
            NA = 3
            asets = Pool([dict(gv=sb(ph, "p3gv%d" % i, [128, 512]), vn=sb(ph, "p3vn%d" % i, [128, 512], BF16), lst=sb(ph, "p3lst%d" % i, [128, 16]),
                               bgv=Buf("gv"), bvn=Buf("vn"), bl=[Buf("l") for _ in range(8)]) for i in range(NA)])
            vmb, bvmb = sb(ph, "p3vmb", [128, 4, 512]), [Buf("vmb") for _ in range(4)]
            gus = Pool([(sb(ph, "p3gu%d" % i, [128, 512]), Buf("gu")) for i in range(2)])
            sguT, bsg = sb(ph, "p3sguT", [128, 4, 512], BF16), [Buf("sg") for _ in range(4)]
            csets = Pool([dict(ya=sb(ph, "p3ya%d" % i, [128, 512]), sa=sb(ph, "p3sa%d" % i, [128, 512]), sb=sb(ph, "p3sb%d" % i, [128, 512]),
                               bya=Buf("ya"), bsa=Buf("sa"), bsb=Buf("sb")) for i in range(2)])
            mrg, bmrg = sb(ph, "p3mrg", [128, 8, 512], BF16), [Buf("mrg") for _ in range(8)]
            qcT, bqc = sb(ph, "p3qcT", [128, 4, 512], BF16), [Buf("qc") for _ in range(4)]
            gsets = Pool([dict(cst=sb(ph, "p3cst%d" % i, [128, 16]), pc=sb(ph, "p3pc%d" % i, [128, 4, 256]), pn=sb(ph, "p3pn%d" % i, [128, 4, 256], BF16),
                               pnT=sb(ph, "p3pnT%d" % i, [128, 8, 128], BF16), ocT=sb(ph, "p3ocT%d" % i, [128, 4, 128], BF16),
                               bc=[Buf("c") for _ in range(4)], bpc=Buf("pc"), bpn=Buf("pn"), bpnT=Buf("pnT"), boc=Buf("oc")) for i in range(2)])
            print("sbuf bytes remaining in 2b:", nc.sbuf_bytes_remaining)

            def a_tile(gi, j):
                tile = gi * 4 + j
                xt, bx = xg[j]
                P.dma("sp", lambda e: e.dma_start(out=xt[:], in_=xown[tile * 128:(tile + 1) * 128, :]), [], [bx])
                yield
                yield from norm_T_gen(st, xt[:], bx, gmixT, hTg[:, :, j * 128:(j + 1) * 128], bhT[j], None, None, pool=bpool)
                A = yield from acq(asets)
                gv, vn, lst, bgv, bvn, bl = A["gv"], A["vn"], A["lst"], A["bgv"], A["bvn"], A["bl"]
                h0 = yield from acq(bpool)
                g0, bg0 = h0
                P.op("pe", [(lambda e, c=c: e.matmul(g0[:], hTg[:, c, j * 128:(j + 1) * 128], wsv[:, c, :], start=(c == 0), stop=(c == 7)))
                            for c in range(8)], [bhT[j], B_c], [bg0])
                yield
                P.op("act", lambda e: e.activation(out=gv[:], in_=g0[:], func=AF.Gelu_apprx_tanh), [bg0], [bgv])
                bpool.release(h0)
                yield
                P.op("dve", lambda e: e.tensor_reduce(out=lst[:, 0:1], in_=gv[:], axis=AX.X, op=ALU.add), [bgv], [bl[0]])
                yield
                P.op("act", lambda e: e.activation(out=vn[:], in_=gv[:], func=AF.Square, accum_out=lst[:, 1:2]), [bgv], [bvn, bl[1]])
                yield
                P.op("dve", lambda e: e.tensor_scalar(out=lst[:, 2:3], in0=lst[:, 0:1], scalar1=1.0 / 512, scalar2=None, op0=ALU.mult), [bl[0]], [bl[2]])
                yield
                P.op("dve", lambda e: e.tensor_tensor(out=lst[:, 3:4], in0=lst[:, 2:3], in1=lst[:, 2:3], op=ALU.mult), [bl[2]], [bl[3]])
                yield
                P.op("dve", lambda e: e.scalar_tensor_tensor(out=lst[:, 4:5], in0=lst[:, 1:2], scalar=1.0 / 512, in1=lst[:, 3:4], op0=ALU.mult, op1=ALU.subtract),
                     [bl[1], bl[3]], [bl[4]])
                yield
                P.op("dve", lambda e: e.tensor_scalar(out=lst[:, 5:6], in0=lst[:, 4:5], scalar1=EPS, scalar2=None, op0=ALU.add), [bl[4]], [bl[5]])
                yield
                P.op("act", lambda e: e.activation(out=lst[:, 6:7], in_=lst[:, 5:6], func=AF.Ln), [bl[5]], [bl[6]])
                yield
                P.op("act", lambda e: e.activation(out=lst[:, 7:8], in_=lst[:, 6:7], func=AF.Exp, scale=-0.5), [bl[6]], [bl[7]])
                yield
                P.op("dve", lambda e: e.tensor_scalar(out=gv[:], in0=gv[:], scalar1=lst[:, 2:3], scalar2=lst[:, 7:8], op0=ALU.subtract, op1=ALU.mult),
                     [bgv, bl[2], bl[7]], [bgv])
                yield
                P.op("dve", lambda e: e.tensor_tensor(out=gv[:], in0=gv[:], in1=lng_bc[:], op=ALU.mult), [bgv, B_c], [bgv])
                yield
                P.op("dve", lambda e: e.tensor_tensor(out=vn[:], in0=gv[:], in1=lnb_bc[:], op=ALU.add), [bgv, B_c], [bvn])
                yield
                h1 = yield from acq(bpool)
                g1, bg1 = h1
                P.op("pe", [(lambda e, g=g: e.matmul(g1[:, g * 128:(g + 1) * 128], vn[:, g * 128:(g + 1) * 128], WsT[:, g, :], start=True, stop=True))
                            for g in range(4)], [bvn, B_c], [bg1])
                yield
                P.op("dve", lambda e: e.tensor_tensor(out=vmb[:, :, j * 128:(j + 1) * 128], in0=g1[:].rearrange("p (g q) -> p g q", g=4),
                                                      in1=bs_bc[:], op=ALU.add), [bg1, B_c], [bvmb[j]])
                bpool.release(h1)
                asets.release(A)
                yield

            def b_chunk(g):
                h0 = yield from acq(bpool)
                g0, bg0 = h0
                P.op("pe", [(lambda e, c=c: e.matmul(g0[:], wsu[:, c, g * 128:(g + 1) * 128], hTg[:, c, :], start=(c == 0), stop=(c == 7)))
                            for c in range(8)], bhT + [B_c], [bg0])
                yield
                G = yield from acq(gus)
                gu, bgu = G
                P.op("act", lambda e: e.activation(out=gu[:], in_=g0[:], func=AF.Gelu_apprx_tanh), [bg0], [bgu])
                bpool.release(h0)
                yield
                P.op("dve", lambda e: e.tensor_tensor(out=sguT[:, g, :], in0=gu[:], in1=vmb[:, g, :], op=ALU.mult), [bgu] + bvmb, [bsg[g]])
                gus.release(G)
                yield

            def c_chunk(gi, dc):
                C = yield from acq(csets)
                ya, sa, sbt, bya, bsa, bsb = C["ya"], C["sa"], C["sb"], C["bya"], C["bsa"], C["bsb"]
                P.dma("sp", lambda e: e.dma_start(out=ya[:], in_=yatt_d[gi][:, dc, :]), [], [bya])
                yield
                h0 = yield from acq(bpool)
                ga_ps, bga = h0
                P.op("pe", [(lambda e, c=c: e.matmul(ga_ps[:], wga[:, c, dc * 128:(dc + 1) * 128], hTg[:, c, :], start=(c == 0), stop=(c == 7)))
                            for c in range(8)], bhT + [B_c], [bga])
                yield
                P.op("act", lambda e: e.activation(out=sa[:], in_=ga_ps[:], func=AF.Sigmoid), [bga], [bsa])
                bpool.release(h0)
                yield
                h1 = yield from acq(bpool)
                gb_ps, bgb = h1
                P.op("pe", [(lambda e, c=c: e.matmul(gb_ps[:], wgb[:, c, dc * 128:(dc + 1) * 128], hTg[:, c, :], start=(c == 0), stop=(c == 7)))
                            for c in range(8)], bhT + [B_c], [bgb])
                yield
                P.op("act", lambda e: e.activation(out=sbt[:], in_=gb_ps[:], func=AF.Sigmoid), [bgb], [bsb])
                bpool.release(h1)
                yield
                h2 = yield from acq(bpool)
                ys_ps, bysp = h2
                P.op("pe", [(lambda e, g=g: e.matmul(ys_ps[:], wso[:, g, dc * 128:(dc + 1) * 128], sguT[:, g, :], start=(g == 0), stop=(g == 3)))
                            for g in range(4)], bsg + [B_c], [bysp])
                yield
                P.op("dve", lambda e: e.tensor_tensor(out=sa[:], in0=sa[:], in1=ya[:], op=ALU.mult), [bsa, bya], [bsa])
                yield
                P.op("dve", lambda e: e.tensor_tensor(out=sbt[:], in0=sbt[:], in1=ys_ps[:], op=ALU.mult), [bsb, bysp], [bsb])
                bpool.release(h2)
                yield
                P.op("dve", lambda e: e.tensor_tensor(out=mrg[:, dc, :], in0=sa[:], in1=sbt[:], op=ALU.add), [bsa, bsb], [bmrg[dc]])
                csets.release(C)
                yield

            def d_part(j, nb):
                xt, bx = xg[j]
                h0 = yield from acq(bpool)
                o_ps, bo = h0
                P.op("pe", [(lambda e, dc=dc: e.matmul(o_ps[:], mrg[:, dc, j * 128:(j + 1) * 128], wo[:, dc, nb * 512:(nb + 1) * 512],
                                                      start=(dc == 0), stop=(dc == 7))) for dc in range(8)], bmrg + [B_c], [bo])
                yield
                P.op("dve", lambda e: e.tensor_tensor(out=xt[:, nb * 512:(nb + 1) * 512], in0=xt[:, nb * 512:(nb + 1) * 512], in1=o_ps[:], op=ALU.add),
                     [bo, bx], [bx])
                bpool.release(h0)
                yield

            def e_tile(j):
                xt, bx = xg[j]
                yield from norm_T_gen(st, xt[:], bx, gcrT, hcT[:, :, j * 128:(j + 1) * 128], bhc[j], None, None, pool=bpool)

            def f_head(h):
                h0 = yield from acq(bpool)
                q_ps, bq = h0
                P.op("pe", [(lambda e, c=c: e.matmul(q_ps[:], wcq[:, c, h * 128:(h + 1) * 128], hcT[:, c, :], start=(c == 0), stop=(c == 7)))
                            for c in range(8)], bhc + [B_c], [bq])
                yield
                P.op("act", lambda e: e.activation(out=qcT[:, h, :], in_=q_ps[:], func=AF.Copy, scale=float(128 ** -0.5)), [bq], [bqc[h]])
                bpool.release(h0)
                yield

            def g_tile(gi, j):
                tile = gi * 4 + j
                xt, bx = xg[j]
                Gs = yield from acq(gsets)
                cst, pc, pn, pnT, ocT = Gs["cst"], Gs["pc"], Gs["pn"], Gs["pnT"], Gs["ocT"]
                bc, bpc, bpn, bpnT, boc = Gs["bc"], Gs["bpc"], Gs["bpn"], Gs["bpnT"], Gs["boc"]
                hs0 = yield from acq(bpool)
                hs1 = yield from acq(bpool)
                scs = [hs0[0], hs1[0]]
                bscs = [hs0[1], hs1[1]]
                for half in range(2):
                    P.op("pe", [(lambda e, hh=hh, half=half: e.matmul(scs[half][:, hh * 256:(hh + 1) * 256], qcT[:, half * 2 + hh, j * 128:(j + 1) * 128], kcT[:, half * 2 + hh, :],
                                                                       start=True, stop=True)) for hh in range(2)], bqc, [bscs[half]])
                    yield
                for half in range(2):
                    P.op("dve", lambda e, half=half: e.tensor_reduce(out=cst[:, half * 2:half * 2 + 2], in_=scs[half][:].rearrange("p (h m) -> p h m", h=2), axis=AX.X, op=ALU.max),
                         [bscs[half]], [bc[0]])
                    yield
                P.op("dve", lambda e: e.tensor_scalar(out=cst[:, 4:8], in0=cst[:, 0:4], scalar1=-1.0, scalar2=None, op0=ALU.mult), [bc[0]], [bc[1]])
                yield
                for half in range(2):
                    P.op("act", [(lambda e, hh=hh, half=half: e.activation(out=pc[:, half * 2 + hh, :], in_=scs[half][:, hh * 256:(hh + 1) * 256], func=AF.Exp,
                                                                          bias=cst[:, 4 + half * 2 + hh:5 + half * 2 + hh], accum_out=cst[:, 8 + half * 2 + hh:9 + half * 2 + hh]))
                                 for hh in range(2)], [bscs[half], bc[1]], [bpc, bc[2]])
                    yield
                bpool.release(hs0)
                bpool.release(hs1)
                P.op("dve", lambda e: e.reciprocal(out=cst[:, 12:16], in_=cst[:, 8:12]), [bc[2]], [bc[3]])
                yield
                P.op("dve", lambda e: e.tensor_tensor(out=pn[:], in0=pc[:], in1=cst[:, 12:16].unsqueeze(2).broadcast_to([128, 4, 256]), op=ALU.mult),
                     [bpc, bc[3]], [bpn])
                yield
                ht = yield from acq(bpool)
                tpv = bfview(ht[0][:])
                P.op("pe", [(lambda e, k=k: e.transpose(tpv[:, k, :], pn[:, k // 2, (k % 2) * 128:(k % 2 + 1) * 128], ident[:])) for k in range(8)], [bpn], [ht[1]])
                yield
                P.op("act", lambda e: e.activation(out=pnT[:], in_=tpv, func=AF.Copy), [ht[1]], [bpnT])
                bpool.release(ht)
                yield
                ho = yield from acq(bpool)
                oc_ps, bocp = ho
                fl = []
                for h in range(4):
                    for mt in range(2):
                        fl.append(lambda e, h=h, mt=mt: e.matmul(oc_ps[:, h * 128:(h + 1) * 128], Vc[:, mt, h * 128:(h + 1) * 128], pnT[:, h * 2 + mt, :],
                                                                 start=(mt == 0), stop=(mt == 1)))
                P.op("pe", fl, [bpnT], [bocp])
                yield
                P.op("act", lambda e: e.activation(out=ocT[:], in_=oc_ps[:].rearrange("p (h t) -> p h t", h=4), func=AF.Copy), [bocp], [boc])
                bpool.release(ho)
                yield
                for nb in range(2):
                    hy = yield from acq(bpool)
                    y_ps, byp = hy
                    P.op("pe", [(lambda e, h=h, nb=nb, y_ps=y_ps: e.matmul(y_ps[:], ocT[:, h, :], wco[:, h, nb * 512:(nb + 1) * 512], start=(h == 0), stop=(h == 3)))
                                for h in range(4)], [boc, B_c], [byp])
                    yield
                    P.op("dve", lambda e, nb=nb, y_ps=y_ps: e.tensor_tensor(out=xt[:, nb * 512:(nb + 1) * 512], in0=xt[:, nb * 512:(nb + 1) * 512], in1=y_ps[:], op=ALU.add),
                         [byp, bx], [bx])
                    bpool.release(hy)
                    yield
                gsets.release(Gs)
                P.dma("sp", lambda e: e.dma_start(out=x2_d[tile * 128:(tile + 1) * 128, :], in_=xt[:]), [bx], [])
                yield

            for gi in range(8):
                run_pipeline((a_tile(gi, j) for j in range(4)), 4)
                run_pipeline((b_chunk(g) for g in range(4)), 3)
                run_pipeline((c_chunk(gi, dc) for dc in range(8)), 3)
                run_pipeline((d_part(j, nb) for j in range(4) for nb in range(2)), 4)
                run_pipeline((e_tile(j) for j in range(4)), 4)
                run_pipeline((f_head(h) for h in range(4)), 4)
                run_pipeline((g_tile(gi, j) for j in range(4)), 3)
            P.flush()

        if dbg == 3:
            dx = nc.dram_tensor("dbg_x2", [OWN, D], F32, kind="ExternalOutput").ap()
            P.dma("sp", lambda e: e.dma_start(out=dx[:, :], in_=x2_d[:, :]), [], [])
            P.flush()
            return nc

        NI = 32
        IT = 512
        NS = NI * IT
        hs_d = nc.dram_tensor("hs_scr", [NS, D], BF16, kind="Internal").ap()
        ys_d = nc.dram_tensor("ys_scr", [NS, D], F32, kind="Internal").ap()
        trid = din("tri", [128, 128])
        iotad = din("iota32", [128, 32])
        thrd = din("thr8", [128, 8])
        with contextlib.ExitStack() as ph:
            gmoT = sb(ph, "gmoT", [128, 8])
            wr = sb(ph, "wr", [128, 8, 20], BF16)
            br_bc = sb(ph, "br_bc", [128, 20])
            gfin_bc = sb(ph, "gfin_bc", [128, 1024])
            oh1 = sb(ph, "oh1", [128, 32, 16])
            oh2 = sb(ph, "oh2", [128, 32, 16])
            wts = sb(ph, "wts", [128, 32, 2])
            idx1 = sb(ph, "idx1", [128, 32], mybir.dt.int32)
            idx2 = sb(ph, "idx2", [128, 32], mybir.dt.int32)
            Ei = sb(ph, "Ei", [128, 32], mybir.dt.int32)
            Widx = sb(ph, "Widx", [128, 32], mybir.dt.int32)
            pidx = sb(ph, "pidx", [128, 1])
            P.dma("sp", lambda e: e.dma_start(out=pidx[:], in_=pidxd[:, :]), [], [])
            B_c = Buf("c3")
            P.dma("sp", lambda e: e.dma_start(out=gmoT[:], in_=gT_view(g_moe), allow_slow_non_contiguous=True), [], [B_c])
            P.dma("pool", lambda e: e.dma_start(out=wr[:, :, 0:4], in_=wv(w_rg, 0, 4)), [], [B_c])
            P.dma("pool", lambda e: e.dma_start(out=wr[:, :, 4:20], in_=wv(w_re, 0, 16)), [], [B_c])
            P.dma("sp", lambda e: e.dma_start(out=br_bc[:, 0:4], in_=b_rg.partition_broadcast(128)), [], [B_c])
            P.dma("sp", lambda e: e.dma_start(out=br_bc[:, 4:20], in_=b_re.partition_broadcast(128)), [], [B_c])
            P.dma("sp", lambda e: e.dma_start(out=gfin_bc[:], in_=g_final.partition_broadcast(128)), [], [B_c])
            allbanks = [(ps(ph, "p4g%d" % i, [128, 512]), Buf("g", True)) for i in range(8)]
            bpool = Pool(allbanks)
            gen = Rot(allbanks)

            with contextlib.ExitStack() as sp1:
                hm_all = sb(sp1, "hm_all", [128, 32, 1024], BF16)
                gmo_bc = sb(sp1, "gmo_bc", [128, 1024])
                tri = sb(sp1, "tri", [128, 128], BF16)
                onesb = sb(sp1, "onesb", [128, 128], BF16)
                iota32 = sb(sp1, "iota32", [128, 32])
                thr8 = sb(sp1, "thr8", [128, 8])
                Mb = sb(sp1, "Mb", [128, 32, 16], BF16)
                pos_all = sb(sp1, "pos_all", [128, 32, 16])
                base = sb(sp1, "base", [128, 16])
                P.dma("sp", lambda e: e.dma_start(out=gmo_bc[:], in_=g_moe.partition_broadcast(128)), [], [B_c])
                P.dma("pool", lambda e: e.dma_start(out=tri[:], in_=trid[:, :]), [], [B_c])
                P.dma("sp", lambda e: e.dma_start(out=iota32[:], in_=iotad[:, :]), [], [B_c])
                P.dma("sp", lambda e: e.dma_start(out=thr8[:], in_=thrd[:, :]), [], [B_c])
                P.op("pool", lambda e: e.memset(onesb[:], 1.0), [], [B_c])
                P.op("pool", lambda e: e.memset(base[:], 0.0), [], [B_c])
                P.barrier()
                NP3 = 6
                st = norm_state(sp1, "p4", nsets=NP3, nxb=NP3)
                xts = Rot([(sb(sp1, "p4x%d" % i, [128, 1024]), Buf("x")) for i in range(NP3)])
                hTt = Rot([(sb(sp1, "p4h%d" % i, [128, 8, 128], BF16), Buf("h")) for i in range(NP3)])
                rts = Rot([(sb(sp1, "rt%d" % i, [128, 160]), [Buf("rt") for _ in range(24)]) for i in range(NP3)])
                boh = [Buf("oh%d" % i) for i in range(32)]
                bhm = [Buf("hm%d" % i) for i in range(32)]

                def moe_tile(i):
                    xt, bx = xts.next()
                    P.dma("sp", lambda e: e.dma_start(out=xt[:], in_=x2_d[i * 128:(i + 1) * 128, :]), [], [bx])
                    yield
                    hT, bh = hTt.next()
                    yield from norm_T_gen(st, xt[:], bx, gmoT, hT[:], bh, None, None, pool=bpool, tok=(hm_all[:, i, :], gmo_bc[:], B_c, bhm[i]))
                    hb = yield from acq(bpool)
                    r_ps, brp = hb
                    P.op("pe", [(lambda e, c=c: e.matmul(r_ps[:, 0:20], hT[:, c, :], wr[:, c, :], start=(c == 0), stop=(c == 7)))
                                for c in range(8)], [bh, B_c], [brp])
                    yield
                    R, brt = rts.next()
                    o1 = oh1[:, i, :]
                    o2 = oh2[:, i, :]
                    P.op("dve", lambda e: e.tensor_tensor(out=R[:, 0:20], in0=r_ps[:, 0:20], in1=br_bc[:], op=ALU.add), [brp, B_c], [brt[0]])
                    bpool.release(hb)
                    yield
                    P.op("dve", lambda e: e.tensor_reduce(out=R[:, 20:21], in_=R[:, 0:4], axis=AX.X, op=ALU.max), [brt[0]], [brt[1]])
                    yield
                    P.op("dve", lambda e: e.tensor_scalar(out=R[:, 21:22], in0=R[:, 20:21], scalar1=-1.0, scalar2=None, op0=ALU.mult), [brt[1]], [brt[2]])
                    yield
                    P.op("act", lambda e: e.activation(out=R[:, 22:26], in_=R[:, 0:4], func=AF.Exp, bias=R[:, 21:22], accum_out=R[:, 26:27]), [brt[0], brt[2]], [brt[3]])
                    yield
                    P.op("dve", lambda e: e.reciprocal(out=R[:, 27:28], in_=R[:, 26:27]), [brt[3]], [brt[4]])
                    yield
                    P.op("dve", lambda e: e.tensor_scalar(out=R[:, 28:32], in0=R[:, 0:4], scalar1=R[:, 20:21], scalar2=None, op0=ALU.is_ge), [brt[0], brt[1]], [brt[5]])
                    yield
                    P.op("dve", lambda e: e.tensor_scalar(out=R[:, 32:36], in0=R[:, 28:32], scalar1=BIG, scalar2=-BIG, op0=ALU.mult, op1=ALU.add), [brt[5]], [brt[6]])
                    yield
                    P.op("dve", lambda e: e.tensor_tensor(out=R[:, 40:56].rearrange("p (g k) -> p g k", g=4), in0=R[:, 4:20].rearrange("p (g k) -> p g k", g=4),
                                                          in1=R[:, 32:36].unsqueeze(2).broadcast_to([128, 4, 4]), op=ALU.add), [brt[0], brt[6]], [brt[7]])
                    yield
                    P.op("dve", lambda e: e.tensor_reduce(out=R[:, 56:57], in_=R[:, 40:56], axis=AX.X, op=ALU.max), [brt[7]], [brt[8]])
                    yield
                    P.op("dve", lambda e: e.tensor_scalar(out=o1, in0=R[:, 40:56], scalar1=R[:, 56:57], scalar2=None, op0=ALU.is_ge), [brt[7], brt[8]], [brt[9]])
                    yield
                    P.op("dve", lambda e: e.scalar_tensor_tensor(out=R[:, 80:96], in0=o1, scalar=-BIG, in1=R[:, 40:56], op0=ALU.mult, op1=ALU.add),
                         [brt[9], brt[7]], [brt[10]])
                    yield
                    P.op("dve", lambda e: e.tensor_reduce(out=R[:, 96:97], in_=R[:, 80:96], axis=AX.X, op=ALU.max), [brt[10]], [brt[11]])
                    yield
                    P.op("dve", lambda e: e.tensor_scalar(out=o2, in0=R[:, 80:96], scalar1=R[:, 96:97], scalar2=None, op0=ALU.is_ge), [brt[10], brt[11]], [brt[12]])
                    yield
                    P.op("dve", lambda e: e.tensor_tensor(out=R[:, 116:117], in0=R[:, 96:97], in1=R[:, 56:57], op=ALU.subtract), [brt[11], brt[8]], [brt[13]])
                    yield
                    P.op("act", lambda e: e.activation(out=R[:, 117:118], in_=R[:, 116:117], func=AF.Exp), [brt[13]], [brt[14]])
                    yield
                    P.op("dve", lambda e: e.tensor_scalar(out=R[:, 118:119], in0=R[:, 117:118], scalar1=1.0, scalar2=None, op0=ALU.add), [brt[14]], [brt[15]])
                    yield
                    P.op("dve", lambda e: e.reciprocal(out=R[:, 119:120], in_=R[:, 118:119]), [brt[15]], [brt[16]])
                    yield
                    P.op("dve", lambda e: e.tensor_tensor(out=wts[:, i, 0:1], in0=R[:, 119:120], in1=R[:, 27:28], op=ALU.mult), [brt[16], brt[4]], [brt[17]])
                    yield
                    P.op("dve", lambda e: e.tensor_tensor(out=wts[:, i, 1:2], in0=wts[:, i, 0:1], in1=R[:, 117:118], op=ALU.mult), [brt[17], brt[14]], [brt[18]])
                    yield
                    P.op("dve", lambda e: e.tensor_tensor(out=Mb[:, i, :], in0=o1, in1=o2, op=ALU.add), [brt[9], brt[12]], [boh[i]])
                    yield

                run_pipeline((moe_tile(i) for i in range(32)), NP3)
                P.barrier()
                prefb = Rot([bpool.acquire(), bpool.acquire()])
                bbase = Buf("base")
                bpos = Buf("pos")
                for i in range(32):
                    pb, bpb = prefb.next()
                    P.op("pe", [lambda e, pb=pb, i=i: e.matmul(pb[:, 0:16], tri[:], Mb[:, i, :], start=True, stop=True),
                                lambda e, pb=pb, i=i: e.matmul(pb[:, 16:32], onesb[:], Mb[:, i, :], start=True, stop=True)], [], [bpb])
                    P.op("dve", lambda e, pb=pb, i=i: e.tensor_tensor(out=pos_all[:, i, :], in0=pb[:, 0:16], in1=base[:], op=ALU.add), [bpb, bbase], [bpos])
                    P.op("dve", lambda e, pb=pb: e.tensor_tensor(out=base[:], in0=pb[:, 16:32], in1=base[:], op=ALU.add), [bpb, bbase], [bbase])
                for it_ in prefb.items:
                    bpool.release(it_)
                P.barrier()
                sm = sb(sp1, "sm", [128, 256])
                big = sb(sp1, "bigt", [128, 32, 16])
                big2 = sb(sp1, "bigt2", [128, 32, 16])
                sf = sb(sp1, "sf", [128, 64])
                bs_ = Buf("sm")

                def S1(fn, eng="dve"):
                    P.op(eng, fn, [bs_], [bs_])
                S1(lambda e: e.tensor_tensor(out=sm[:, 0:128].rearrange("p (a k) -> p a k", k=8), in0=base[:].unsqueeze(2).broadcast_to([128, 16, 8]),
                                             in1=thr8[:].unsqueeze(1).broadcast_to([128, 16, 8]), op=ALU.is_gt))
                S1(lambda e: e.tensor_reduce(out=sm[:, 128:144], in_=sm[:, 0:128].rearrange("p (a k) -> p a k", k=8), axis=AX.X, op=ALU.add))
                S1(lambda e: e.tensor_copy(out=sm[:, 144:160], in_=sm[:, 128:144]))
                cur, oth = 144, 160
                for k in (1, 2, 4, 8):
                    S1(lambda e, cur=cur, oth=oth, k=k: e.tensor_copy(out=sm[:, oth:oth + k], in_=sm[:, cur:cur + k]))
                    S1(lambda e, cur=cur, oth=oth, k=k: e.tensor_tensor(out=sm[:, oth + k:oth + 16], in0=sm[:, cur + k:cur + 16], in1=sm[:, cur:cur + 16 - k], op=ALU.add))
                    cur, oth = oth, cur
                S1(lambda e, cur=cur: e.tensor_copy(out=sm[:, 176:192], in_=sm[:, cur:cur + 16]))
                S1(lambda e: e.tensor_tensor(out=sm[:, 192:208], in0=sm[:, 176:192], in1=sm[:, 128:144], op=ALU.subtract))
                S1(lambda e: e.tensor_scalar(out=sm[:, 192:208], in0=sm[:, 192:208], scalar1=float(IT), scalar2=None, op0=ALU.mult))
                S1(lambda e: e.memset(sm[:, 208:240], 0.0))
                for ex in range(NE):
                    S1(lambda e, ex=ex: e.scalar_tensor_tensor(out=sm[:, 208:240], in0=iota32[:], scalar=sm[:, 176 + ex:177 + ex], in1=sm[:, 208:240], op0=ALU.is_ge, op1=ALU.add))
                S1(lambda e: e.tensor_scalar(out=sm[:, 208:240], in0=sm[:, 208:240], scalar1=float(NE - 1), scalar2=None, op0=ALU.min))
                S1(lambda e: e.tensor_copy(out=Ei[:], in_=sm[:, 208:240]))
                S1(lambda e: e.tensor_scalar(out=sf[:, 0:32], in0=sm[:, 208:240], scalar1=128.0, scalar2=pidx[:, 0:1], op0=ALU.mult, op1=ALU.add))
                S1(lambda e: e.tensor_copy(out=Widx[:], in_=sf[:, 0:32]))
                S1(lambda e: e.tensor_tensor(out=big[:], in0=pos_all[:], in1=sm[:, 192:208].unsqueeze(1).broadcast_to([128, 32, 16]), op=ALU.add))
                S1(lambda e: e.tensor_tensor(out=big2[:], in0=big[:], in1=oh1[:], op=ALU.mult))
                S1(lambda e: e.tensor_reduce(out=sf[:, 0:32], in_=big2[:], axis=AX.X, op=ALU.add))
                S1(lambda e: e.tensor_tensor(out=big2[:], in0=big[:], in1=oh2[:], op=ALU.mult))
                S1(lambda e: e.tensor_reduce(out=sf[:, 32:64], in_=big2[:], axis=AX.X, op=ALU.add))
                S1(lambda e: e.tensor_copy(out=idx1[:], in_=sf[:, 0:32]))
                S1(lambda e: e.tensor_copy(out=idx2[:], in_=sf[:, 32:64]))
                P.barrier()
                for i in range(int(_os0.environ.get("NSCAT", 32))):
                    for ixt in (idx1, idx2):
                        P.dma("pool", lambda e, i=i, ixt=ixt: e.indirect_dma_start(out=hs_d[:, :], out_offset=bass.IndirectOffsetOnAxis(ap=ixt[:, i:i + 1], axis=0),
                                                                                   in_=hm_all[:, i, :], in_offset=None), [], [])
                P.barrier()

            if dbg == 4:
                d1 = nc.dram_tensor("dbg_idx", [128, 64], mybir.dt.int32, kind="ExternalOutput").ap()
                d2 = nc.dram_tensor("dbg_E", [128, 32], mybir.dt.int32, kind="ExternalOutput").ap()
                d3 = nc.dram_tensor("dbg_wts", [128, 64], F32, kind="ExternalOutput").ap()
                P.dma("sp", lambda e: e.dma_start(out=d1[:, 0:32], in_=idx1[:]), [], [])
                P.dma("sp", lambda e: e.dma_start(out=d1[:, 32:64], in_=idx2[:]), [], [])
                P.dma("sp", lambda e: e.dma_start(out=d2[:, :], in_=Ei[:]), [], [])
                P.dma("sp", lambda e: e.dma_start(out=d3[:, :], in_=wts[:].rearrange("p a b -> p (a b)")), [], [])
                P.flush()
                return nc

            with contextlib.ExitStack() as sp2:
                Wb = [(sb(sp2, "Wb%d" % i, [128, 12288], BF16), Buf("Wb")) for i in range(2)]
                hss = Rot([(sb(sp2, "hs%d" % i, [128, 4, 1024], BF16), Buf("hs")) for i in range(2)])
                hsTs = Rot([(sb(sp2, "hsT%d" % i, [128, 8, 512], BF16), [Buf("hsT") for _ in range(4)]) for i in range(2)])
                sgs = Rot([(sb(sp2, "sg%d" % i, [128, 512]), Buf("sg")) for i in range(2)])
                heTs = Rot([(sb(sp2, "heT%d" % i, [128, 4, 512], BF16), [Buf("he") for _ in range(4)]) for i in range(2)])
                yss = Rot([(sb(sp2, "ys%d" % i, [128, 1024]), Buf("ys")) for i in range(3)])

                def load_w(w):
                    wb, bwb = Wb[w % 2]
                    P.dma("pool", lambda e: e.indirect_dma_start(out=wb[:, :], out_offset=None, in_=wcat_bf[:, :],
                                                                 in_offset=bass.IndirectOffsetOnAxis(ap=Widx[:, w:w + 1], axis=0)), [B_wbf], [bwb])

                def item_T(w):
                    hs, bhs = hss.next()
                    P.dma("sp", lambda e, hs=hs, w=w: e.dma_start(out=hs[:], in_=hs_d[w * IT:(w + 1) * IT, :].rearrange("(s p) d -> p s d", p=128)), [], [bhs])
                    hsT, bhsT = hsTs.next()
                    for sub in range(4):
                        tb, btb = gen.next()
                        tpv = bfview(tb[:])
                        P.op("pe", [(lambda e, c=c, sub=sub, tpv=tpv, hs=hs: e.transpose(tpv[:, c, :], hs[:, sub, c * 128:(c + 1) * 128], ident[:])) for c in range(8)],
                             [bhs], [btb])
                        if sub % 2 == 0:
                            P.op("act", lambda e, sub=sub, tpv=tpv, hsT=hsT: e.activation(out=hsT[:, :, sub * 128:(sub + 1) * 128], in_=tpv, func=AF.Copy), [btb], [bhsT[sub]])
                        else:
                            P.op("dve", lambda e, sub=sub, tpv=tpv, hsT=hsT: e.tensor_copy(out=hsT[:, :, sub * 128:(sub + 1) * 128], in_=tpv), [btb], [bhsT[sub]])
                    return hsT, bhsT

                load_w(0)
                nxtT = item_T(0)
                for w in range(NI):
                    if w + 1 < NI:
                        load_w(w + 1)
                    wb, bwb = Wb[w % 2]
                    wg = wb[:, 0:4096].rearrange("p (c f) -> p c f", c=8)
                    wu = wb[:, 4096:8192].rearrange("p (c f) -> p c f", c=8)
                    wd = wb[:, 8192:12288].rearrange("p (c f) -> p c f", c=4)
                    bwg = bwu = bwd = bwb
                    hsT, bhsT = nxtT
                    heT, bhe = heTs.next()
                    for fc in range(4):
                        g_ps, bgp = gen.next()
                        u_ps, bup = gen.next()
                        P.op("pe", [(lambda e, c=c, fc=fc, g_ps=g_ps, wg=wg, hsT=hsT: e.matmul(g_ps[:], wg[:, c, fc * 128:(fc + 1) * 128], hsT[:, c, :],
                                                                                          start=(c == 0), stop=(c == 7))) for c in range(8)], bhsT + [bwg], [bgp])
                        P.op("pe", [(lambda e, c=c, fc=fc, u_ps=u_ps, wu=wu, hsT=hsT: e.matmul(u_ps[:], wu[:, c, fc * 128:(fc + 1) * 128], hsT[:, c, :],
                                                                                          start=(c == 0), stop=(c == 7))) for c in range(8)], bhsT + [bwu], [bup])
                        sg, bsg_ = sgs.next()
                        P.op("act", lambda e, sg=sg, g_ps=g_ps: e.activation(out=sg[:], in_=g_ps[:], func=AF.Silu), [bgp], [bsg_])
                        P.op("dve", lambda e, sg=sg, u_ps=u_ps, heT=heT, fc=fc: e.tensor_tensor(out=heT[:, fc, :], in0=sg[:], in1=u_ps[:], op=ALU.mult),
                             [bsg_, bup], [bhe[fc]])
                    if w + 1 < NI:
                        nxtT = item_T(w + 1)
                    for sub in range(4):
                        ys, bys = yss.next()
                        for nb in range(2):
                            y_ps, byp = gen.next()
                            P.op("pe", [(lambda e, fc=fc, sub=sub, nb=nb, y_ps=y_ps, heT=heT, wd=wd: e.matmul(y_ps[:], heT[:, fc, sub * 128:(sub + 1) * 128], wd[:, fc, nb * 512:(nb + 1) * 512],
                                                                                                             start=(fc == 0), stop=(fc == 3))) for fc in range(4)], bhe + [bwd], [byp])
                            if nb == 0:
                                P.op("act", lambda e, ys=ys, y_ps=y_ps: e.activation(out=ys[:, 0:512], in_=y_ps[:], func=AF.Copy), [byp], [bys])
                            else:
                                P.op("dve", lambda e, ys=ys, y_ps=y_ps: e.tensor_copy(out=ys[:, 512:1024], in_=y_ps[:]), [byp], [bys])
                        P.dma("sp", lambda e, ys=ys, w=w, sub=sub: e.dma_start(out=ys_d[w * IT + sub * 128:w * IT + (sub + 1) * 128, :], in_=ys[:]), [bys], [])
                P.barrier()

            with contextlib.ExitStack() as sp3:
                NC3 = 3
                st = norm_state(sp3, "p5", nsets=4, nxb=1)
                xts = Rot([(sb(sp3, "p5x%d" % i, [128, 1024]), Buf("x")) for i in range(NC3)])
                y0s = Rot([(sb(sp3, "p5y0%d" % i, [128, 1024]), Buf("y0")) for i in range(NC3)])
                y1s = Rot([(sb(sp3, "p5y1%d" % i, [128, 1024]), Buf("y1")) for i in range(NC3)])
                ots = Rot([(sb(sp3, "p5o%d" % i, [128, 1024]), Buf("ot")) for i in range(NC3)])

                def fin_tile(i):
                    xt, bx = xts.next()
                    y0, by0 = y0s.next()
                    y1, by1 = y1s.next()
                    P.dma("sp", lambda e: e.dma_start(out=xt[:], in_=x2_d[i * 128:(i + 1) * 128, :]), [], [bx])
                    yield
                    P.dma("pool", lambda e: e.indirect_dma_start(out=y0[:, :], out_offset=None, in_=ys_d[:, :],
                                                                 in_offset=bass.IndirectOffsetOnAxis(ap=idx1[:, i:i + 1], axis=0)), [], [by0])
                    yield
                    P.dma("pool", lambda e: e.indirect_dma_start(out=y1[:, :], out_offset=None, in_=ys_d[:, :],
                                                                 in_offset=bass.IndirectOffsetOnAxis(ap=idx2[:, i:i + 1], axis=0)), [], [by1])
                    yield
                    P.op("dve", lambda e: e.scalar_tensor_tensor(out=xt[:], in0=y0[:], scalar=wts[:, i, 0:1], in1=xt[:], op0=ALU.mult, op1=ALU.add), [by0, bx], [bx])
                    yield
                    P.op("dve", lambda e: e.scalar_tensor_tensor(out=xt[:], in0=y1[:], scalar=wts[:, i, 1:2], in1=xt[:], op0=ALU.mult, op1=ALU.add), [by1, bx], [bx])
                    yield
                    fss, fb, _u = st["sets"].next()
                    ot, bot = ots.next()
                    P.op("act", lambda e: e.activation(out=ot[:], in_=xt[:], func=AF.Square, scale=1.0 / 32, accum_out=fss[:, 0:1]), [bx], [fb[0], bot])
                    yield
                    P.op("dve", lambda e: e.tensor_scalar(out=fss[:, 1:2], in0=fss[:, 0:1], scalar1=EPS, scalar2=None, op0=ALU.add), [fb[0]], [fb[1]])
                    yield
                    P.op("act", lambda e: e.activation(out=fss[:, 2:3], in_=fss[:, 1:2], func=AF.Ln), [fb[1]], [fb[2]])
                    yield
                    P.op("act", lambda e: e.activation(out=fss[:, 3:4], in_=fss[:, 2:3], func=AF.Exp, scale=-0.5), [fb[2]], [fb[3]])
                    yield
                    P.op("dve", lambda e: e.scalar_tensor_tensor(out=ot[:], in0=xt[:], scalar=fss[:, 3:4], in1=gfin_bc[:], op0=ALU.mult, op1=ALU.mult),
                         [bx, fb[3], B_c], [bot])
                    yield
                    P.dma("sp", lambda e: e.dma_start(out=out[i * 128:(i + 1) * 128, :], in_=ot[:]), [bot], [])
                    yield

                run_pipeline((fin_tile(i) for i in range(32)), NC3)
                P.barrier()
            P.flush()
    return nc


def _rope_tables():
    rows = SEQ // 64
    row_ids = np.repeat(np.arange(rows), 64).astype(np.float32)
    col_ids = np.tile(np.arange(64), rows).astype(np.float32)
    inv = (10000.0 ** (-np.arange(16, dtype=np.float32) / 16)).astype(np.float32)
    ang = np.concatenate([row_ids[:, None] * inv[None, :], col_ids[:, None] * inv[None, :]], axis=-1).astype(np.float32)
    return np.cos(ang).astype(np.float32), np.sin(ang).astype(np.float32)


def _wcat(wg, wu, wd):
    g = wg.reshape(NE, 8, 128, 512).transpose(0, 2, 1, 3).reshape(NE, 128, 4096)
    u = wu.reshape(NE, 8, 128, 512).transpose(0, 2, 1, 3).reshape(NE, 128, 4096)
    d_ = wd.reshape(NE, 4, 128, 1024).transpose(0, 2, 1, 3).reshape(NE, 128, 4096)
    return np.ascontiguousarray(np.concatenate([g, u, d_], axis=2).reshape(NE * 128, 12288))


def make_in_maps(inputs):
    f = lambda a: np.ascontiguousarray(np.asarray(a, dtype=np.float32))
    x = f(inputs["x"])
    mem = f(inputs["mem"])
    cos, sin = _rope_tables()

    def lay(t, n):
        return np.ascontiguousarray(t.reshape(n, 128, 32).transpose(1, 0, 2))

    shared = {
        "ident": np.eye(128, dtype=np.float32),
        "g_mix": f(inputs["g_mix"][0]), "w_in": f(inputs["w_in"][0]), "g_q": f(inputs["g_q"][0]), "g_k": f(inputs["g_k"][0]),
        "w_att_out": f(inputs["w_att_out"][0]), "sgu_ln_g": f(inputs["sgu_ln_g"][0]), "sgu_ln_b": f(inputs["sgu_ln_b"][0]),
        "w_s": f(inputs["w_s"][0]), "b_s": f(inputs["b_s"][0]), "w_sgu_out": f(inputs["w_sgu_out"][0]), "w_out": f(inputs["w_out"][0]),
        "g_cross": f(inputs["g_cross"][0]), "g_mem": f(inputs["g_mem"][0]), "w_cq": f(inputs["w_cq"][0]), "w_ckv": f(inputs["w_ckv"][0]),
        "w_co": f(inputs["w_co"][0]), "g_moe": f(inputs["g_moe"][0]), "w_rg": f(inputs["w_rg"][0]), "b_rg": f(inputs["b_rg"][0]),
        "w_re": f(inputs["w_re"][0]), "b_re": f(inputs["b_re"][0]), "g_final": f(inputs["g_final"]),
        "wcat": _wcat(f(inputs["w_gate"][0]), f(inputs["w_up"][0]), f(inputs["w_down"][0])),
        "pidx": np.arange(128, dtype=np.float32).reshape(128, 1),
        "cos_seq": lay(cos, 64), "sin_seq": lay(sin, 64),
        "tri": np.triu(np.ones((128, 128), np.float32), 1),
        "iota32": np.tile(np.arange(32, dtype=np.float32), (128, 1)),
        "thr8": np.tile(np.arange(8, dtype=np.float32) * 512.0, (128, 1)),
    }
    maps = []
    for c in range(8):
        b, hf = c // 2, c % 2
        m = dict(shared)
        m["xseq"] = x[b]
        m["xown"] = np.ascontiguousarray(x[b, hf * OWN:(hf + 1) * OWN])
        m["mem"] = mem[b]
        m["cos_own"] = lay(cos[hf * OWN:(hf + 1) * OWN], 32)
        m["sin_own"] = lay(sin[hf * OWN:(hf + 1) * OWN], 32)
        maps.append(m)
    return maps


def kernel(**inputs):
    nc = build()
    maps = make_in_maps(inputs)
    res = run_bass_kernel_spmd(nc, maps, core_ids=list(range(8)))
    outp = np.empty((4, SEQ, D), np.float32)
    for c in range(8):
        b, hf = c // 2, c % 2
        outp[b, hf * OWN:(hf + 1) * OWN] = np.asarray(res.results[c]["out"], dtype=np.float32)
    return outp
```

```python
import contextlib
import os as _os0
import numpy as np
import concourse.bass as bass
import concourse.mybir as mybir
from concourse.bass_utils import run_bass_kernel_spmd

F32 = mybir.dt.float32
BF16 = mybir.dt.bfloat16
AF = mybir.ActivationFunctionType
ALU = mybir.AluOpType
AX = mybir.AxisListType

D = 1024
SEQ = 8192
OWN = 4096
NMEM = 256
EPS = 1e-6
NE = 16
BIG = 1.0e30


class Buf:
    __slots__ = ("name", "w", "r", "psum")

    def __init__(self, name="", psum=False):
        self.name = name
        self.psum = psum
        self.w = None
        self.r = {}


class Eng:
    def __init__(self, name):
        self.name = name
        self.sem = None
        self.count = 0
        self.seen = {}
        self.prog = []
        self.chsems = []
        self.dma_i = 0


class Prog:
    NCH = int(_os0.environ.get('NCH', 8))

    def __init__(self, nc, stack):
        self.nc = nc
        self.E = {}
        for n in ("pe", "act", "dve", "pool", "sp"):
            e = Eng(n)
            e.sem = stack.enter_context(nc.semaphore("sem_" + n))
            self.E[n] = e
        for n in ("sp", "pool", "act"):
            e = self.E[n]
            for i in range(self.NCH):
                e.chsems.append(stack.enter_context(nc.semaphore("ch_%s_%d" % (n, i))))
        self.chan_issued = {}
        import os
        self.limit = int(os.environ.get("OPLIMIT", 10 ** 9))
        self.nops = 0

    def _need(self, need, ev):
        sem, val, src = ev
        k = id(sem)
        if k not in need or need[k][1] < val:
            need[k] = (sem, val, src)

    def _deps(self, eng, reads, writes, extra=()):
        E = self.E[eng]
        need = {}
        for b in reads:
            if b.w is not None:
                self._need(need, b.w)
        for b in writes:
            if b.w is not None:
                self._need(need, b.w)
            for k, (sem, val, src) in b.r.items():
                self._need(need, (sem, val, src))
        for ev in extra:
            self._need(need, ev)
        for k, (sem, val, src) in need.items():
            if src == "pe" and eng == "pe":
                continue
            if E.seen.get(k, 0) >= val:
                continue
            E.seen[k] = val
            E.prog.append(("wait", sem, val))

    def _commit(self, ev, reads, writes):
        sem, val, src = ev
        for b in writes:
            b.w = ev
            b.r = {}
        for b in reads:
            if b not in writes:
                b.r[id(sem)] = (sem, val, src)

    def op(self, eng, fns, reads=(), writes=()):
        if not isinstance(fns, (list, tuple)):
            fns = [fns]
        self.nops += 1
        if self.nops > self.limit:
            return
        pr = [b for b in reads if b.psum]
        if pr:
            reads = [b for b in reads if not b.psum]
            writes = list(writes) + [b for b in pr if b not in writes]
        E = self.E[eng]
        self._deps(eng, reads, writes)
        E.count += 1
        for f in fns[:-1]:
            E.prog.append(("raw", f))
        E.prog.append(("op", fns[-1], E.sem))
        self._commit((E.sem, E.count, eng), reads, writes)

    def dma(self, q, fn, reads=(), writes=()):
        self.nops += 1
        if self.nops > self.limit:
            return
        E = self.E[q]
        ch = E.dma_i % self.NCH
        rnd = E.dma_i // self.NCH
        E.dma_i += 1
        chsem = E.chsems[ch]
        extra = []
        if rnd > 0:
            extra.append((chsem, 16 * rnd, "dma"))
        self._deps(q, reads, writes, extra)
        E.prog.append(("dma", fn, chsem))
        self.chan_issued[id(chsem)] = (chsem, 16 * (rnd + 1))
        self._commit((chsem, 16 * (rnd + 1), "dma"), reads, writes)

    def dma_bg(self, q, fn, sem, total, buf):
        E = self.E[q]
        E.prog.append(("dma", fn, sem))
        buf.w = (sem, total, "dma")

    def barrier(self):
        for n, E in self.E.items():
            for m, F in self.E.items():
                if m == n or F.count == 0:
                    continue
                k = id(F.sem)
                if E.seen.get(k, 0) < F.count:
                    E.seen[k] = F.count
                    E.prog.append(("wait", F.sem, F.count))
            for k, (sem, val) in self.chan_issued.items():
                if E.seen.get(k, 0) < val:
                    E.seen[k] = val
                    E.prog.append(("wait", sem, val))

    def flush(self):
        self.barrier()
        nc = self.nc
        progs = {n: E.prog for n, E in self.E.items()}
        for E in self.E.values():
            E.prog = []

        def replay(eng, prog):
            for it in prog:
                if it[0] == "wait":
                    eng.wait_ge(it[1], it[2])
                elif it[0] == "raw":
                    it[1](eng)
                elif it[0] == "op":
                    it[1](eng).then_inc(it[2], 1)
                else:
                    it[1](eng).then_inc(it[2], 16)

        with nc.Block() as block:
            @block.tensor
            def _(e):
                replay(e, progs["pe"])

            @block.scalar
            def _(e):
                replay(e, progs["act"])

            @block.vector
            def _(e):
                replay(e, progs["dve"])

            @block.gpsimd
            def _(e):
                replay(e, progs["pool"])

            @block.sync
            def _(e):
                replay(e, progs["sp"])


class Rot:
    def __init__(self, items):
        self.items = items
        self.i = 0

    def next(self):
        it = self.items[self.i % len(self.items)]
        self.i += 1
        return it


class Pool:
    def __init__(self, items):
        self.free = list(items)

    def acquire(self):
        return self.free.pop(0) if self.free else None

    def release(self, it):
        self.free.append(it)


def build(dbg=False):
    nc = bass.Bass("TRN2", target_bir_lowering=False)

    def din(name, shape):
        return nc.dram_tensor(name, list(shape), F32, kind="ExternalInput").ap()

    xseq = din("xseq", [SEQ, D])
    xown = din("xown", [OWN, D])
    memd = din("mem", [NMEM, D])
    cos_seq = din("cos_seq", [128, 64, 32])
    sin_seq = din("sin_seq", [128, 64, 32])
    cos_own = din("cos_own", [128, 32, 32])
    sin_own = din("sin_own", [128, 32, 32])
    identd = din("ident", [128, 128])
    g_mix = din("g_mix", [D])
    w_in = din("w_in", [D, 3840])
    g_q = din("g_q", [64])
    g_k = din("g_k", [64])
    w_att_out = din("w_att_out", [512, D])
    sgu_ln_g = din("sgu_ln_g", [512])
    sgu_ln_b = din("sgu_ln_b", [512])
    w_s = din("w_s", [4, 128, 128])
    b_s = din("b_s", [4, 128])
    w_sgu_out = din("w_sgu_out", [512, D])
    w_out = din("w_out", [D, D])
    g_cross = din("g_cross", [D])
    g_mem = din("g_mem", [D])
    w_cq = din("w_cq", [D, 512])
    w_ckv = din("w_ckv", [D, D])
    w_co = din("w_co", [512, D])
    g_moe = din("g_moe", [D])
    w_rg = din("w_rg", [D, 4])
    b_rg = din("b_rg", [4])
    w_re = din("w_re", [D, 16])
    b_re = din("b_re", [16])
    wcat = din("wcat", [NE * 128, 12288])
    pidxd = din("pidx", [128, 1])
    wcat_bf = nc.dram_tensor("wcat_bf", [NE * 128, 12288], BF16, kind="Internal").ap()
    g_final = din("g_final", [D])
    out = nc.dram_tensor("out", [OWN, D], F32, kind="ExternalOutput").ap()
    yatt_d = nc.dram_tensor("yatt_scr", [8, 128, 8, 512], F32, kind="Internal").ap()
    x2_d = nc.dram_tensor("x2_scr", [OWN, D], F32, kind="Internal").ap()

    def wv(w, c0, c1):
        return w.rearrange("(c p) n -> p c n", p=128)[:, :, c0:c1]

    def gT_view(g):
        return g.rearrange("(c p) -> p c", p=128)

    with contextlib.ExitStack() as top:
        P = Prog(nc, top)
        bgsem = top.enter_context(nc.semaphore("bgsem"))
        B_wbf = Buf("wcat_bf")
        print("sbuf bytes remaining at start:", nc.sbuf_bytes_remaining)

        def sb(stack, name, shape, dt=F32):
            return stack.enter_context(nc.sbuf_tensor("s_" + name, list(shape), dt))

        def ps(stack, name, shape, dt=F32):
            return stack.enter_context(nc.psum_tensor("p_" + name, list(shape), dt))

        ident = sb(top, "ident", [128, 128], BF16)
        kcT = sb(top, "kcT", [128, 4, NMEM], BF16)
        Vc = sb(top, "Vc", [128, 2, 512], BF16)
        gmixT = sb(top, "gmixT", [128, 8])
        attn_stack = contextlib.ExitStack()
        kT_all = sb(attn_stack, "kT_all", [128, SEQ], BF16)
        V_all = sb(attn_stack, "V_all", [128, 64, 2, 65], BF16)
        B_const = Buf("const")

        P.dma("pool", lambda e: e.dma_start(out=ident[:], in_=identd[:, :]), [], [B_const])
        P.dma("sp", lambda e: e.dma_start(out=gmixT[:], in_=gT_view(g_mix), allow_slow_non_contiguous=True), [], [B_const])
        P.op("pool", lambda e: e.memset(V_all[:], 1.0), [], [B_const])

        def run_pipeline(gens, depth):
            it = iter(gens)
            active = []
            more = True
            while True:
                if more and len(active) < depth:
                    try:
                        active.append(next(it))
                    except StopIteration:
                        more = False
                if not active:
                    break
                for g in list(active):
                    try:
                        next(g)
                    except StopIteration:
                        active.remove(g)

        def drain(g):
            for _ in g:
                pass

        def acq(pool):
            while True:
                it = pool.acquire()
                if it is not None:
                    return it
                yield

        def bfview(bank_ap):
            return bank_ap.bitcast(BF16).rearrange("p (c t) -> p c t", c=8)

        def norm_T_gen(st, xt_ap, xbuf, gT, out_ap3, outbuf, tps, tpsb, pool=None, tok=None):
            ss, b, _unused = st["sets"].next()
            xb, bxb = st["xb"].next()
            P.op("act", lambda e: e.activation(out=xb[:], in_=xt_ap, func=AF.Square, scale=1.0 / 32,
                                               accum_out=ss[:, 0:1]), [xbuf], [b[0], bxb])
            yield
            P.op("dve", lambda e: e.tensor_scalar(out=ss[:, 1:2], in0=ss[:, 0:1], scalar1=EPS, scalar2=None,
                                                  op0=ALU.add), [b[0]], [b[1]])
            yield
            P.op("act", lambda e: e.activation(out=ss[:, 2:3], in_=ss[:, 1:2], func=AF.Ln), [b[1]], [b[2]])
            yield
            P.op("act", lambda e: e.activation(out=ss[:, 3:4], in_=ss[:, 2:3], func=AF.Exp, scale=-0.5), [b[2]], [b[3]])
            yield
            P.op("act", lambda e: e.activation(out=xb[:], in_=xt_ap, func=AF.Copy, scale=ss[:, 3:4]), [xbuf, b[3]], [bxb])
            yield
            if tok is not None:
                P.op("dve", lambda e: e.tensor_tensor(out=tok[0], in0=xb[:], in1=tok[1], op=ALU.mult), [bxb, tok[2]], [tok[3]])
                yield
            held = None
            if pool is not None:
                while held is None:
                    held = pool.acquire()
                    if held is None:
                        yield
                tps, tpsb = bfview(held[0][:]), held[1]
            P.op("pe", [(lambda e, c=c: e.transpose(tps[:, c, :], xb[:, c * 128:(c + 1) * 128], ident[:])) for c in range(8)],
                 [bxb], [tpsb])
            yield
            P.op("dve", lambda e: e.tensor_tensor(out=out_ap3, in0=tps, in1=gT[:, 0:8].unsqueeze(2).broadcast_to([128, 8, 128]),
                                                  op=ALU.mult), [tpsb], [outbuf])
            if held is not None:
                pool.release(held)
            yield

        def norm_T(st, xt_ap, xbuf, gT, out_ap3, outbuf, tps, tpsb):
            drain(norm_T_gen(st, xt_ap, xbuf, gT, out_ap3, outbuf, tps[:], tpsb))

        def norm_state(stack, pfx, nsets=2, nxb=2):
            st = {}
            st["sets"] = Rot([(sb(stack, pfx + "ss%d" % i, [128, 4]), [Buf("ss") for _ in range(4)], None) for i in range(nsets)])
            st["xb"] = Rot([(sb(stack, pfx + "xb%d" % i, [128, 1024], BF16), Buf("xb")) for i in range(nxb)])
            return st

        def head_norm_rope_gen(ws, src_ps, bsrc, H, g_bc, cosb, sinb, dst, bdst, t):
            if isinstance(ws, Pool):
                w = yield from acq(ws)
            else:
                w = ws.next()
            n = H * 64
            src3 = src_ps.rearrange("p (h d) -> p h d", h=H)
            P.op("act", lambda e: e.activation(out=w["sq"][:, 0:n], in_=src_ps, func=AF.Square, scale=0.125), [bsrc], [w["bsq"]])
            yield
            P.op("dve", lambda e: e.tensor_reduce(out=w["hs"][:, 0:H], in_=w["sq"][:, 0:n].rearrange("p (h d) -> p h d", h=H),
                                                  axis=AX.X, op=ALU.add), [w["bsq"]], [w["bhs"]])
            yield
            P.op("dve", lambda e: e.tensor_scalar(out=w["hs"][:, 8:8 + H], in0=w["hs"][:, 0:H], scalar1=EPS, scalar2=None, op0=ALU.add),
                 [w["bhs"]], [w["bhs1"]])
            yield
            P.op("act", lambda e: e.activation(out=w["hs"][:, 16:16 + H], in_=w["hs"][:, 8:8 + H], func=AF.Ln), [w["bhs1"]], [w["bhs2"]])
            yield
            P.op("act", lambda e: e.activation(out=w["hs"][:, 24:24 + H], in_=w["hs"][:, 16:16 + H], func=AF.Exp, scale=-0.5),
                 [w["bhs2"]], [w["bhs3"]])
            yield
            qn3 = w["qn"][:, 0:n].rearrange("p (h d) -> p h d", h=H)
            P.op("dve", lambda e: e.tensor_tensor(out=qn3, in0=src3, in1=w["hs"][:, 24:24 + H].unsqueeze(2).broadcast_to([128, H, 64]),
                                                  op=ALU.mult), [bsrc, w["bhs3"]], [w["bqn"]])
            yield
            P.op("dve", lambda e: e.tensor_tensor(out=qn3, in0=qn3, in1=g_bc[:, :].unsqueeze(1).broadcast_to([128, H, 64]),
                                                  op=ALU.mult), [w["bqn"]], [w["bqn"]])
            yield
            qn4 = w["qn"][:, 0:n].rearrange("p (h i t) -> p h i t", h=H, t=2)
            A4 = w["A"][:, 0:n].rearrange("p (h i t) -> p h i t", h=H, t=2)
            B4 = w["B"][:, 0:n].rearrange("p (h i t) -> p h i t", h=H, t=2)
            c4 = cosb[:, t, :].unsqueeze(1).unsqueeze(3).broadcast_to([128, H, 32, 2])
            s4 = sinb[:, t, :].unsqueeze(1).unsqueeze(3).broadcast_to([128, H, 32, 2])
            P.op("dve", lambda e: e.tensor_tensor(out=A4, in0=qn4, in1=c4, op=ALU.mult), [w["bqn"]], [w["bA"]])
            yield
            P.op("dve", lambda e: e.tensor_tensor(out=B4, in0=qn4, in1=s4, op=ALU.mult), [w["bqn"]], [w["bB"]])
            yield
            P.op("dve", lambda e: e.tensor_tensor(out=dst[:, :, :, 0], in0=A4[:, :, :, 0], in1=B4[:, :, :, 1], op=ALU.subtract),
                 [w["bA"], w["bB"]], [bdst])
            yield
            P.op("dve", lambda e: e.tensor_tensor(out=dst[:, :, :, 1], in0=B4[:, :, :, 0], in1=A4[:, :, :, 1], op=ALU.add),
                 [w["bA"], w["bB"]], [bdst])
            if isinstance(ws, Pool):
                ws.release(w)
            yield

        def rope_state(stack, pfx, n, nsets=1):
            sets = []
            for i in range(nsets):
                w = {}
                for k in ("sq", "qn", "A", "B"):
                    w[k] = sb(stack, "%s%s%d" % (pfx, k, i), [128, n])
                w["hs"] = sb(stack, "%shs%d" % (pfx, i), [128, 32])
                for k in ("bsq", "bhs", "bhs1", "bhs2", "bhs3", "bqn", "bA", "bB"):
                    w[k] = Buf(k)
                sets.append(w)
            return Rot(sets)

        with contextlib.ExitStack() as ph:
            wkv = sb(ph, "wkv", [128, 8, 256], BF16)
            cosb = sb(ph, "cosb", [128, 64, 32])
            sinb = sb(ph, "sinb", [128, 64, 32])
            gk_bc = sb(ph, "gk_bc", [128, 64])
            P.dma("pool", lambda e: e.dma_start(out=wkv[:], in_=wv(w_in, 512, 768)), [], [B_const])
            P.dma("sp", lambda e: e.dma_start(out=cosb[:], in_=cos_seq[:, :, :]), [], [B_const])
            P.dma("sp", lambda e: e.dma_start(out=sinb[:], in_=sin_seq[:, :, :]), [], [B_const])
            P.dma("sp", lambda e: e.dma_start(out=gk_bc[:], in_=g_k.partition_broadcast(128)), [], [B_const])
            NP1 = 8
            P.barrier()
            st = norm_state(ph, "p1", nsets=NP1, nxb=NP1)
            rws = rope_state(ph, "p1r", 128, nsets=NP1)
            xts = Rot([(sb(ph, "p1x%d" % i, [128, 1024]), Buf("x")) for i in range(NP1)])
            hTs = Rot([(sb(ph, "p1h%d" % i, [128, 8, 128], BF16), Buf("h")) for i in range(NP1)])
            krs = Rot([(sb(ph, "p1kr%d" % i, [128, 2, 32, 2], BF16), Buf("kr")) for i in range(NP1)])
            banks = Pool([(ps(ph, "p1b%d" % i, [128, 512]), Buf("bank", True)) for i in range(8)])

            import os as _os

            def p1_tile(t):
                xt, bx = xts.next()
                P.dma("sp", lambda e: e.dma_start(out=xt[:], in_=xseq[t * 128:(t + 1) * 128, :]), [], [bx])
                yield
                hT, bh = hTs.next()
                yield from norm_T_gen(st, xt[:], bx, gmixT, hT[:], bh, None, None, pool=banks)
                hkv = yield from acq(banks)
                kv, bkv = hkv
                P.op("pe", [(lambda e, c=c: e.matmul(kv[:, 0:256], hT[:, c, :], wkv[:, c, :], start=(c == 0), stop=(c == 7)))
                            for c in range(8)], [bh, B_const], [bkv])
                yield
                P.op("act", lambda e: e.activation(out=V_all[:, t, :, 0:64], in_=kv[:, 128:256].rearrange("p (h d) -> p h d", h=2),
                                                   func=AF.Copy), [bkv], [])
                yield
                kr, bkr = krs.next()
                yield from head_norm_rope_gen(rws, kv[:, 0:128], bkv, 2, gk_bc, cosb, sinb, kr, bkr, t)
                banks.release(hkv)
                hkt = yield from acq(banks)
                kt, bkt = hkt
                ktv = kt[:].bitcast(BF16)
                P.op("pe", lambda e: e.transpose(ktv[:, 0:128], kr[:].rearrange("p h i t -> p (h i t)"), ident[:]), [bkr], [bkt])
                yield
                P.op("act", lambda e: e.activation(out=kT_all[:, t * 128:(t + 1) * 128], in_=ktv[:, 0:128], func=AF.Copy), [bkt], [])
                banks.release(hkt)
                yield

            run_pipeline((p1_tile(t) for t in range(int(_os.environ.get('NT1', 64)))), int(_os.environ.get('DEPTH1', 8)))
            P.flush()

        if dbg == 1:
            P.limit = 10 ** 9
            P.limit = 10 ** 9
            print("nops", P.nops)
            dk = nc.dram_tensor("dbg_kT", [128, SEQ], BF16, kind="ExternalOutput").ap()
            dv = nc.dram_tensor("dbg_V", [128, 64 * 2 * 65], BF16, kind="ExternalOutput").ap()
            P.dma("sp", lambda e: e.dma_start(out=dk[:, :], in_=kT_all[:]), [], [])
            P.dma("sp", lambda e: e.dma_start(out=dv[:, :], in_=V_all[:].rearrange("p a b c -> p (a b c)")), [], [])
            P.flush()
            attn_stack.close()
            return nc

        with contextlib.ExitStack() as ph:
            wq = sb(ph, "wq", [128, 8, 512], BF16)
            wao = sb(ph, "wao", [64, 8, 1024], BF16)
            cosb = sb(ph, "cosb2", [128, 32, 32])
            sinb = sb(ph, "sinb2", [128, 32, 32])
            gq_bc = sb(ph, "gq_bc", [128, 64])
            ones65 = sb(ph, "ones65", [65, 64])
            B_c2 = Buf("c2")
            for hd_ in range(8):
                pos_ = (hd_ % 4) * 2 + hd_ // 4
                P.dma("pool", lambda e, hd_=hd_, pos_=pos_: e.dma_start(out=wq[:, :, pos_ * 64:(pos_ + 1) * 64], in_=wv(w_in, hd_ * 64, hd_ * 64 + 64)), [], [B_c2])
            P.dma("pool", lambda e: e.dma_start(out=wao[:], in_=w_att_out.rearrange("(h p) n -> p h n", p=64)), [], [B_c2])
            P.dma("sp", lambda e: e.dma_start(out=cosb[:], in_=cos_own[:, :, :]), [], [B_c2])
            P.dma("sp", lambda e: e.dma_start(out=sinb[:], in_=sin_own[:, :, :]), [], [B_c2])
            P.dma("sp", lambda e: e.dma_start(out=gq_bc[:], in_=g_q.partition_broadcast(128)), [], [B_c2])
            P.op("dve", lambda e: e.tensor_scalar(out=gq_bc[:], in0=gq_bc[:], scalar1=0.125, scalar2=None, op0=ALU.mult), [B_c2], [B_c2])
            P.op("pool", lambda e: e.memset(ones65[:], 1.0), [], [B_c2])
            P.barrier()
            st = norm_state(ph, "p2", nsets=4, nxb=4)
            rws = Pool(rope_state(ph, "p2r", 512, nsets=3).items)
            xts = Rot([(sb(ph, "p2x%d" % i, [128, 1024]), Buf("x")) for i in range(4)])
            hTs = Rot([(sb(ph, "p2h%d" % i, [128, 8, 128], BF16), Buf("h")) for i in range(4)])
            qrs = Rot([(sb(ph, "p2qr%d" % i, [128, 8, 32, 2], BF16), Buf("qr")) for i in range(4)])
            Ps = Rot([(sb(ph, "p2P%d" % i, [128, 2, 512], BF16), Buf("P")) for i in range(3)])
            OnTs = Rot([(sb(ph, "p2OnT%d" % i, [64, 8, 512], BF16), [Buf("OnT") for _ in range(8)]) for i in range(2)])
            ysbs = Rot([(sb(ph, "p2ysb%d" % i, [128, 4, 512]), [Buf("ysb") for _ in range(4)]) for i in range(1)])
            Sps = Rot([(ps(ph, "p2S%d" % i, [128, 2, 512]), Buf("S", True)) for i in range(3)])
            Ops, bO = ps(ph, "p2O", [128, 2, 512]), [Buf("O0", True), Buf("O1", True)]
            LA = int(_os0.environ.get('LA', 2))

            sbank = Pool([(Sps.items[i][0][:, bnk, :], Buf("sbank", True)) for i in range(3) for bnk in range(2)])
            Osbs = Rot([(sb(ph, "p2Osc%d" % i, [65, 512]), Buf("Osb")) for i in range(4)])
            rrs = Rot([(sb(ph, "p2rrc%d" % i, [65, 512]), Buf("rr")) for i in range(4)])

            qT_all = sb(ph, "qT_all", [128, 4, OWN], BF16)
            bqTt = [Buf("qTt") for _ in range(32)]
            print("sbuf bytes remaining in 2a:", nc.sbuf_bytes_remaining)

            def q_tile(tile):
                xt, bx = xts.next()
                P.dma("sp", lambda e: e.dma_start(out=xt[:], in_=xown[tile * 128:(tile + 1) * 128, :]), [], [bx])
                yield
                hT, bh = hTs.next()
                yield from norm_T_gen(st, xt[:], bx, gmixT, hT[:], bh, None, None, pool=sbank)
                hq = yield from acq(sbank)
                qps, bqp = hq
                P.op("pe", [(lambda e, c=c: e.matmul(qps, hT[:, c, :], wq[:, c, :], start=(c == 0), stop=(c == 7)))
                            for c in range(8)], [bh, B_c2], [bqp])
                yield
                qr, bqr = qrs.next()
                yield from head_norm_rope_gen(rws, qps, bqp, 8, gq_bc, cosb, sinb, qr, bqr, tile)
                sbank.release(hq)
                ht = yield from acq(sbank)
                tpv = bfview(ht[0])
                qr3 = qr[:].rearrange("p h i t -> p (h i t)")
                P.op("pe", [(lambda e, g=g: e.transpose(tpv[:, g, :], qr3[:, g * 128:(g + 1) * 128], ident[:])) for g in range(4)],
                     [bqr], [ht[1]])
                yield
                P.op("act", lambda e: e.activation(out=qT_all[:, :, tile * 128:(tile + 1) * 128], in_=tpv[:, 0:4, :], func=AF.Copy),
                     [ht[1]], [bqTt[tile]])
                sbank.release(ht)
                yield

            spool = Pool(list(Sps.items))

            def epi_unit(item, bnk):
                (Osb, bOsb, rr, brr, OnT, bOn, head) = item
                hS = spool.acquire()
                assert hS is not None
                mS, bmS = hS
                P.op("pe", lambda e: e.matmul(mS[0:64, bnk, :], ones65[64:65, 0:64], rr[64:65, :], start=True, stop=True), [brr, B_c2], [bmS])
                P.op("dve", lambda e: e.tensor_tensor(out=OnT[:, head, :], in0=Osb[0:64, :], in1=mS[0:64, bnk, :], op=ALU.mult),
                     [bOsb, bmS], [bOn[head]])
                spool.release(hS)

            def y_unit(gi, dc, OnT, bOn, ysb, bys):
                hS = spool.acquire()
                assert hS is not None
                mS, bmS = hS
                P.op("pe", [(lambda e, h=h: e.matmul(mS[:, 0, :], wao[:, h, dc * 128:(dc + 1) * 128], OnT[:, h, :], start=(h == 0), stop=(h == 7)))
                            for h in range(8)], bOn + [B_c2], [bmS])
                P.op("dve", lambda e: e.tensor_copy(out=ysb[:, dc % 4, :], in_=mS[:, 0, :]), [bmS], [bys[dc % 4]])
                spool.release(hS)
                if dc % 4 == 3:
                    P.dma("sp", lambda e: e.dma_start(out=yatt_d[gi][:, dc - 3:dc + 1, :], in_=ysb[:]), bys, [])

            run_pipeline((q_tile(t) for t in range(32)), 4)
            P.barrier()
            for ex_ in range(NE):
                P.dma_bg("pool", lambda e, ex_=ex_: e.dma_start(out=wcat_bf[ex_ * 128:(ex_ + 1) * 128, :], in_=wcat[ex_ * 128:(ex_ + 1) * 128, :]), bgsem, 16 * NE, B_wbf)
            units = []
            for gi in range(8):
                OnT, bOn = OnTs.next()
                bqT = bqTt[gi * 4:gi * 4 + 4]
                for g in range(4):
                    pend = []

                    def try_issue(kt, g=g, gi=gi, bqT=bqT, pend=pend):
                        hS = spool.acquire()
                        if hS is None:
                            return False
                        S, bS = hS
                        P.op("pe", [lambda e: e.matmul(S[:, 0, :], kT_all[0:64, kt * 128:(kt + 1) * 128], qT_all[0:64, g, gi * 512:(gi + 1) * 512], start=True, stop=True),
                                    lambda e: e.matmul(S[:, 1, :], kT_all[64:128, kt * 128:(kt + 1) * 128], qT_all[64:128, g, gi * 512:(gi + 1) * 512], start=True, stop=True)],
                             bqT, [bS])
                        pend.append(hS)
                        return True
                    nxt = 0
                    for kt in range(64):
                        while nxt < 64 and nxt <= kt + LA and try_issue(nxt):
                            nxt += 1
                        hS = pend.pop(0)
                        S, bS = hS
                        Pt, bP = Ps.next()
                        P.op("act", lambda e, S=S, Pt=Pt: e.activation(out=Pt[:], in_=S[:], func=AF.Exp), [bS], [bP])
                        spool.release(hS)
                        if units and kt >= 3 and kt % 3 == 0:
                            units.pop(0)()
                        P.op("pe", [lambda e, Pt=Pt, kt=kt: e.matmul(Ops[0:65, 0, :], V_all[:, kt, 0, :], Pt[:, 0, :], start=(kt == 0), stop=(kt == 63)),
                                    lambda e, Pt=Pt, kt=kt: e.matmul(Ops[0:65, 1, :], V_all[:, kt, 1, :], Pt[:, 1, :], start=(kt == 0), stop=(kt == 63))],
                             [bP], bO)
                    for hh in range(2):
                        head = g + 4 * hh
                        Osb, bOsb = Osbs.next()
                        rr, brr = rrs.next()
                        P.op("act", lambda e, Osb=Osb, hh=hh: e.activation(out=Osb[:], in_=Ops[0:65, hh, :], func=AF.Copy), [bO[hh]], [bOsb])
                        P.op("dve", lambda e, Osb=Osb, rr=rr: e.reciprocal(out=rr[64:65, :], in_=Osb[64:65, :]), [bOsb], [brr])
                        item = (Osb, bOsb, rr, brr, OnT, bOn, head)
                        units.append(lambda item=item, hh=hh: epi_unit(item, hh))
                ysb, bys = ysbs.next()
                for dc in range(8):
                    units.append(lambda gi=gi, dc=dc, OnT=OnT, bOn=bOn, ysb=ysb, bys=bys: y_unit(gi, dc, OnT, bOn, ysb, bys))
            while units:
                units.pop(0)()
            P.flush()

        attn_stack.close()
        if dbg == 2:
            dy = nc.dram_tensor("dbg_yatt", [8, 128, 8, 512], F32, kind="ExternalOutput").ap()
            P.dma("sp", lambda e: e.dma_start(out=dy.rearrange("a p c t -> (a p) (c t)"), in_=yatt_d.rearrange("a p c t -> (a p) (c t)")), [], [])
            P.flush()
            return nc

        with contextlib.ExitStack() as ph:
            wckv = sb(ph, "wckv", [128, 8, 1024], BF16)
            gmemT = sb(ph, "gmemT", [128, 8])
            memT = sb(ph, "memT", [128, 8, 256], BF16)
            B_cm = Buf("cm")
            P.dma("pool", lambda e: e.dma_start(out=wckv[:], in_=wv(w_ckv, 0, 1024)), [], [B_cm])
            P.dma("sp", lambda e: e.dma_start(out=gmemT[:], in_=gT_view(g_mem), allow_slow_non_contiguous=True), [], [B_cm])
            st = norm_state(ph, "pm")
            xts = Rot([(sb(ph, "pmx%d" % i, [128, 1024]), Buf("x")) for i in range(2)])
            tp, btp = ps(ph, "pmtp", [128, 8, 128], BF16), Buf("tp", True)
            mps = Rot([(ps(ph, "pmps%d" % i, [128, 512]), Buf("mps", True)) for i in range(2)])
            bmemT = [Buf("memT0"), Buf("memT1")]
            for mt in range(2):
                xt, bx = xts.next()
                P.dma("sp", lambda e, xt=xt, mt=mt: e.dma_start(out=xt[:], in_=memd[mt * 128:(mt + 1) * 128, :]), [], [bx])
                norm_T(st, xt[:], bx, gmemT, memT[:, :, mt * 128:(mt + 1) * 128], bmemT[mt], tp, btp)
            for h in range(4):
                mp, bmp = mps.next()
                P.op("pe", [(lambda e, c=c, h=h, mp=mp: e.matmul(mp[:, 0:256], wckv[:, c, h * 128:(h + 1) * 128], memT[:, c, :], start=(c == 0), stop=(c == 7)))
                            for c in range(8)], bmemT + [B_cm], [bmp])
                P.op("act", lambda e, h=h, mp=mp: e.activation(out=kcT[:, h, :], in_=mp[:, 0:256], func=AF.Copy), [bmp], [])
            for mt in range(2):
                mp, bmp = mps.next()
                P.op("pe", [(lambda e, c=c, mt=mt, mp=mp: e.matmul(mp[:], memT[:, c, mt * 128:(mt + 1) * 128], wckv[:, c, 512:1024], start=(c == 0), stop=(c == 7)))
                            for c in range(8)], bmemT + [B_cm], [bmp])
                P.op("act", lambda e, mt=mt, mp=mp: e.activation(out=Vc[:, mt, :], in_=mp[:], func=AF.Copy), [bmp], [])
            P.flush()

        with contextlib.ExitStack() as ph:
            wsv = sb(ph, "wsv", [128, 8, 512], BF16)
            wsu = sb(ph, "wsu", [128, 8, 512], BF16)
            wga = sb(ph, "wga", [128, 8, 1024], BF16)
            wgb = sb(ph, "wgb", [128, 8, 1024], BF16)
            wso = sb(ph, "wso", [128, 4, 1024], BF16)
            wo = sb(ph, "wo", [128, 8, 1024], BF16)
            wcq = sb(ph, "wcq", [128, 8, 512], BF16)
            wco = sb(ph, "wco", [128, 4, 1024], BF16)
            gcrT = sb(ph, "gcrT", [128, 8])
            lng_bc = sb(ph, "lng_bc", [128, 512])
            lnb_bc = sb(ph, "lnb_bc", [128, 512])
            bs_bc = sb(ph, "bs_bc", [128, 4, 128])
            wsraw = sb(ph, "wsraw", [128, 4, 128], BF16)
            WsT = sb(ph, "WsT", [128, 4, 128], BF16)
            B_c = Buf("c2b")
            B_ws = Buf("ws")
            P.dma("pool", lambda e: e.dma_start(out=wsu[:], in_=wv(w_in, 768, 1280)), [], [B_c])
            P.dma("pool", lambda e: e.dma_start(out=wsv[:], in_=wv(w_in, 1280, 1792)), [], [B_c])
            P.dma("pool", lambda e: e.dma_start(out=wga[:], in_=wv(w_in, 1792, 2816)), [], [B_c])
            P.dma("pool", lambda e: e.dma_start(out=wgb[:], in_=wv(w_in, 2816, 3840)), [], [B_c])
            P.dma("pool", lambda e: e.dma_start(out=wso[:], in_=wv(w_sgu_out, 0, 1024)), [], [B_c])
            P.dma("pool", lambda e: e.dma_start(out=wo[:], in_=wv(w_out, 0, 1024)), [], [B_c])
            P.dma("pool", lambda e: e.dma_start(out=wcq[:], in_=wv(w_cq, 0, 512)), [], [B_c])
            P.dma("pool", lambda e: e.dma_start(out=wco[:], in_=wv(w_co, 0, 1024)), [], [B_c])
            P.dma("pool", lambda e: e.dma_start(out=wsraw[:], in_=w_s.rearrange("g p q -> p g q")), [], [B_ws])
            P.dma("sp", lambda e: e.dma_start(out=gcrT[:], in_=gT_view(g_cross), allow_slow_non_contiguous=True), [], [B_c])
            P.dma("sp", lambda e: e.dma_start(out=lng_bc[:], in_=sgu_ln_g.partition_broadcast(128)), [], [B_c])
            P.dma("sp", lambda e: e.dma_start(out=lnb_bc[:], in_=sgu_ln_b.partition_broadcast(128)), [], [B_c])
            P.dma("sp", lambda e: e.dma_start(out=bs_bc[:].rearrange("p g q -> p (g q)"), in_=b_s.rearrange("g q -> (g q)").partition_broadcast(128)), [], [B_c])
            P.barrier()
            st = norm_state(ph, "p3", nsets=4, nxb=4)
            bpool = Pool([(ps(ph, "p3g%d" % i, [128, 512]), Buf("g", True)) for i in range(8)])
            hb0 = bpool.acquire()
            tpb0 = bfview(hb0[0][:])
            P.op("pe", [(lambda e, g=g: e.transpose(tpb0[:, g, :], wsraw[:, g, :], ident[:])) for g in range(4)], [B_ws], [hb0[1]])
            P.op("act", lambda e: e.activation(out=WsT[:], in_=tpb0[:, 0:4, :], func=AF.Copy), [hb0[1]], [B_c])
            bpool.release(hb0)
            P.barrier()

            xg = [(sb(ph, "p3x%d" % i, [128, 1024]), Buf("x")) for i in range(4)]
            hTg, bhT = sb(ph, "p3hT", [128, 8, 512], BF16), [Buf("hT") for _ in range(4)]
            hcT, bhc = hTg, bhT
            NA = 3
            asets = Pool([dict(gv=sb(ph, "p3gv%d" % i, [128, 512]), vn=sb(ph, "p3vn%d" % i, [128, 512], BF16), lst=sb(ph, "p3lst%d" % i, [128, 16]),
                               bgv=Buf("gv"), bvn=Buf("vn"), bl=[Buf("l") for _ in range(8)]) for i in range(NA)])
            vmb, bvmb = sb(ph, "p3vmb", [128, 4, 512]), [Buf("vmb") for _ in range(4)]
            gus = Pool([(sb(ph, "p3gu%d" % i, [128, 512]), Buf("gu")) for i in range(2)])
            sguT, bsg = sb(ph, "p3sguT", [128, 4, 512], BF16), [Buf("sg") for _ in range(4)]
            csets = Pool([dict(ya=sb(ph, "p3ya%d" % i, [128, 512]), sa=sb(ph, "p3sa%d" % i, [128, 512]), sb=sb(ph, "p3sb%d" % i, [128, 512]),
                               bya=Buf("ya"), bsa=Buf("sa"), bsb=Buf("sb")) for i in range(2)])
            mrg, bmrg = sb(ph, "p3mrg", [128, 8, 512], BF16), [Buf("mrg") for _ in range(8)]
            qcT, bqc = sb(ph, "p3qcT", [128, 4, 512], BF16), [Buf("qc") for _ in range(4)]
            gsets = Pool([dict(cst=sb(ph, "p3cst%d" % i, [128, 16]), pc=sb(ph, "p3pc%d" % i, [128, 4, 256]), pn=sb(ph, "p3pn%d" % i, [128, 4, 256], BF16),
                               pnT=sb(ph, "p3pnT%d" % i, [128, 8, 128], BF16), ocT=sb(ph, "p3ocT%d" % i, [128, 4, 128], BF16),
                               bc=[Buf("c") for _ in range(4)], bpc=Buf("pc"), bpn=Buf("pn"), bpnT=Buf("pnT"), boc=Buf("oc")) for i in range(2)])
            print("sbuf bytes remaining in 2b:", nc.sbuf_bytes_remaining)

            def a_tile(gi, j):
                tile = gi * 4 + j
                xt, bx = xg[j]
                P.dma("sp", lambda e: e.dma_start(out=xt[:], in_=xown[tile * 128:(tile + 1) * 128, :]), [], [bx])
                yield
                yield from norm_T_gen(st, xt[:], bx, gmixT, hTg[:, :, j * 128:(j + 1) * 128], bhT[j], None, None, pool=bpool)
                A = yield from acq(asets)
                gv, vn, lst, bgv, bvn, bl = A["gv"], A["vn"], A["lst"], A["bgv"], A["bvn"], A["bl"]
                h0 = yield from acq(bpool)
                g0, bg0 = h0
                P.op("pe", [(lambda e, c=c: e.matmul(g0[:], hTg[:, c, j * 128:(j + 1) * 128], wsv[:, c, :], start=(c == 0), stop=(c == 7)))
                            for c in range(8)], [bhT[j], B_c], [bg0])
                yield
                P.op("act", lambda e: e.activation(out=gv[:], in_=g0[:], func=AF.Gelu_apprx_tanh), [bg0], [bgv])
                bpool.release(h0)
                yield
                P.op("dve", lambda e: e.tensor_reduce(out=lst[:, 0:1], in_=gv[:], axis=AX.X, op=ALU.add), [bgv], [bl[0]])
                yield
                P.op("act", lambda e: e.activation(out=vn[:], in_=gv[:], func=AF.Square, accum_out=lst[:, 1:2]), [bgv], [bvn, bl[1]])
                yield
                P.op("dve", lambda e: e.tensor_scalar(out=lst[:, 2:3], in0=lst[:, 0:1], scalar1=1.0 / 512, scalar2=None, op0=ALU.mult), [bl[0]], [bl[2]])
                yield
                P.op("dve", lambda e: e.tensor_tensor(out=lst[:, 3:4], in0=lst[:, 2:3], in1=lst[:, 2:3], op=ALU.mult), [bl[2]], [bl[3]])
                yield
                P.op("dve", lambda e: e.scalar_tensor_tensor(out=lst[:, 4:5], in0=lst[:, 1:2], scalar=1.0 / 512, in1=lst[:, 3:4], op0=ALU.mult, op1=ALU.subtract),
                     [bl[1], bl[3]], [bl[4]])
                yield
                P.op("dve", lambda e: e.tensor_scalar(out=lst[:, 5:6], in0=lst[:, 4:5], scalar1=EPS, scalar2=None, op0=ALU.add), [bl[4]], [bl[5]])
                yield
                P.op("act", lambda e: e.activation(out=lst[:, 6:7], in_=lst[:, 5:6], func=AF.Ln), [bl[5]], [bl[6]])
                yield
                P.op("act", lambda e: e.activation(out=lst[:, 7:8], in_=lst[:, 6:7], func=AF.Exp, scale=-0.5), [bl[6]], [bl[7]])
                yield
                P.op("dve", lambda e: e.tensor_scalar(out=gv[:], in0=gv[:], scalar1=lst[:, 2:3], scalar2=lst[:, 7:8], op0=ALU.subtract, op1=ALU.mult),
                     [bgv, bl[2], bl[7]], [bgv])
                yield
                P.op("dve", lambda e: e.tensor_tensor(out=gv[:], in0=gv[:], in1=lng_bc[:], op=ALU.mult), [bgv, B_c], [bgv])
                yield
                P.op("dve", lambda e: e.tensor_tensor(out=vn[:], in0=gv[:], in1=lnb_bc[:], op=ALU.add), [bgv, B_c], [bvn])
                yield
                h1 = yield from acq(bpool)
                g1, bg1 = h1
                P.op("pe", [(lambda e, g=g: e.matmul(g1[:, g * 128:(g + 1) * 128], vn[:, g * 128:(g + 1) * 128], WsT[:, g, :], start=True, stop=True))
                            for g in range(4)], [bvn, B_c], [bg1])
                yield
                P.op("dve", lambda e: e.tensor_tensor(out=vmb[:, :, j * 128:(j + 1) * 128], in0=g1[:].rearrange("p (g q) -> p g q", g=4),
                                                      in1=bs_bc[:], op=ALU.add), [bg1, B_c], [bvmb[j]])
                bpool.release(h1)
                asets.release(A)
                yield

            def b_chunk(g):
                h0 = yield from acq(bpool)
                g0, bg0 = h0
                P.op("pe", [(lambda e, c=c: e.matmul(g0[:], wsu[:, c, g * 128:(g + 1) * 128], hTg[:, c, :], start=(c == 0), stop=(c == 7)))
                            for c in range(8)], bhT + [B_c], [bg0])
                yield
                G = yield from acq(gus)
                gu, bgu = G
                P.op("act", lambda e: e.activation(out=gu[:], in_=g0[:], func=AF.Gelu_apprx_tanh), [bg0], [bgu])
                bpool.release(h0)
                yield
                P.op("dve", lambda e: e.tensor_tensor(out=sguT[:, g, :], in0=gu[:], in1=vmb[:, g, :], op=ALU.mult), [bgu] + bvmb, [bsg[g]])
                gus.release(G)
                yield

            def c_chunk(gi, dc):
                C = yield from acq(csets)
                ya, sa, sbt, bya, bsa, bsb = C["ya"], C["sa"], C["sb"], C["bya"], C["bsa"], C["bsb"]
                P.dma("sp", lambda e: e.dma_start(out=ya[:], in_=yatt_d[gi][:, dc, :]), [], [bya])
                yield
                h0 = yield from acq(bpool)
                ga_ps, bga = h0
                P.op("pe", [(lambda e, c=c: e.matmul(ga_ps[:], wga[:, c, dc * 128:(dc + 1) * 128], hTg[:, c, :], start=(c == 0), stop=(c == 7)))
                            for c in range(8)], bhT + [B_c], [bga])
                yield
                P.op("act", lambda e: e.activation(out=sa[:], in_=ga_ps[:], func=AF.Sigmoid), [bga], [bsa])
                bpool.release(h0)
                yield
                h1 = yield from acq(bpool)
                gb_ps, bgb = h1
                P.op("pe", [(lambda e, c=c: e.matmul(gb_ps[:], wgb[:, c, dc * 128:(dc + 1) * 128], hTg[:, c, :], start=(c == 0), stop=(c == 7)))
                            for c in range(8)], bhT + [B_c], [bgb])
                yield
                P.op("act", lambda e: e.activation(out=sbt[:], in_=gb_ps[:], func=AF.Sigmoid), [bgb], [bsb])
                bpool.release(h1)
                yield
                h2 = yield from acq(bpool)
                ys_ps, bysp = h2
                P.op("pe", [(lambda e, g=g: e.matmul(ys_ps[:], wso[:, g, dc * 128:(dc + 1) * 128], sguT[:, g, :], start=(g == 0), stop=(g == 3)))
                            for g in range(4)], bsg + [B_c], [bysp])
                yield
                P.op("dve", lambda e: e.tensor_tensor(out=sa[:], in0=sa[:], in1=ya[:], op=ALU.mult), [bsa, bya], [bsa])
                yield
                P.op("dve", lambda e: e.tensor_tensor(out=sbt[:], in0=sbt[:], in1=ys_ps[:], op=ALU.mult), [bsb, bysp], [bsb])
                bpool.release(h2)
                yield
                P.op("dve", lambda e: e.tensor_tensor(out=mrg[:, dc, :], in0=sa[:], in1=sbt[:], op=ALU.add), [bsa, bsb], [bmrg[dc]])
                csets.release(C)
                yield

            def d_part(j, nb):
                xt, bx = xg[j]
                h0 = yield from acq(bpool)
                o_ps, bo = h0
                P.op("pe", [(lambda e, dc=dc: e.matmul(o_ps[:], mrg[:, dc, j * 128:(j + 1) * 128], wo[:, dc, nb * 512:(nb + 1) * 512],
                                                      start=(dc == 0), stop=(dc == 7))) for dc in range(8)], bmrg + [B_c], [bo])
                yield
                P.op("dve", lambda e: e.tensor_tensor(out=xt[:, nb * 512:(nb + 1) * 512], in0=xt[:, nb * 512:(nb + 1) * 512], in1=o_ps[:], op=ALU.add),
                     [bo, bx], [bx])
                bpool.release(h0)
                yield

            def e_tile(j):
                xt, bx = xg[j]
                yield from norm_T_gen(st, xt[:], bx, gcrT, hcT[:, :, j * 128:(j + 1) * 128], bhc[j], None, None, pool=bpool)

            def f_head(h):
                h0 = yield from acq(bpool)
                q_ps, bq = h0
                P.op("pe", [(lambda e, c=c: e.matmul(q_ps[:], wcq[:, c, h * 128:(h + 1) * 128], hcT[:, c, :], start=(c == 0), stop=(c == 7)))
                            for c in range(8)], bhc + [B_c], [bq])
                yield
                P.op("act", lambda e: e.activation(out=qcT[:, h, :], in_=q_ps[:], func=AF.Copy, scale=float(128 ** -0.5)), [bq], [bqc[h]])
                bpool.release(h0)
                yield

            def g_tile(gi, j):
                tile = gi * 4 + j
                xt, bx = xg[j]
                Gs = yield from acq(gsets)
                cst, pc, pn, pnT, ocT = Gs["cst"], Gs["pc"], Gs["pn"], Gs["pnT"], Gs["ocT"]
                bc, bpc, bpn, bpnT, boc = Gs["bc"], Gs["bpc"], Gs["bpn"], Gs["bpnT"], Gs["boc"]
                hs0 = yield from acq(bpool)
                hs1 = yield from acq(bpool)
                scs = [hs0[0], hs1[0]]
                bscs = [hs0[1], hs1[1]]
                for half in range(2):
                    P.op("pe", [(lambda e, hh=hh, half=half: e.matmul(scs[half][:, hh * 256:(hh + 1) * 256], qcT[:, half * 2 + hh, j * 128:(j + 1) * 128], kcT[:, half * 2 + hh, :],
                                                                       start=True, stop=True)) for hh in range(2)], bqc, [bscs[half]])
                    yield
                for half in range(2):
                    P.op("dve", lambda e, half=half: e.tensor_reduce(out=cst[:, half * 2:half * 2 + 2], in_=scs[half][:].rearrange("p (h m) -> p h m", h=2), axis=AX.X, op=ALU.max),
                         [bscs[half]], [bc[0]])
                    yield
                P.op("dve", lambda e: e.tensor_scalar(out=cst[:, 4:8], in0=cst[:, 0:4], scalar1=-1.0, scalar2=None, op0=ALU.mult), [bc[0]], [bc[1]])
                yield
                for half in range(2):
                    P.op("act", [(lambda e, hh=hh, half=half: e.activation(out=pc[:, half * 2 + hh, :], in_=scs[half][:, hh * 256:(hh + 1) * 256], func=AF.Exp,
                                                                          bias=cst[:, 4 + half * 2 + hh:5 + half * 2 + hh], accum_out=cst[:, 8 + half * 2 + hh:9 + half * 2 + hh]))
                                 for hh in range(2)], [bscs[half], bc[1]], [bpc, bc[2]])
                    yield
                bpool.release(hs0)
                bpool.release(hs1)
                P.op("dve", lambda e: e.reciprocal(out=cst[:, 12:16], in_=cst[:, 8:12]), [bc[2]], [bc[3]])
                yield
                P.op("dve", lambda e: e.tensor_tensor(out=pn[:], in0=pc[:], in1=cst[:, 12:16].unsqueeze(2).broadcast_to([128, 4, 256]), op=ALU.mult),
                     [bpc, bc[3]], [bpn])
                yield
                ht = yield from acq(bpool)
                tpv = bfview(ht[0][:])
                P.op("pe", [(lambda e, k=k: e.transpose(tpv[:, k, :], pn[:, k // 2, (k % 2) * 128:(k % 2 + 1) * 128], ident[:])) for k in range(8)], [bpn], [ht[1]])
                yield
                P.op("act", lambda e: e.activation(out=pnT[:], in_=tpv, func=AF.Copy), [ht[1]], [bpnT])
                bpool.release(ht)
                yield
                ho = yield from acq(bpool)
                oc_ps, bocp = ho
                fl = []
                for h in range(4):
                    for mt in range(2):
                        fl.append(lambda e, h=h, mt=mt: e.matmul(oc_ps[:, h * 128:(h + 1) * 128], Vc[:, mt, h * 128:(h + 1) * 128], pnT[:, h * 2 + mt, :],
                                                                 start=(mt == 0), stop=(mt == 1)))
                P.op("pe", fl, [bpnT], [bocp])
                yield
                P.op("act", lambda e: e.activation(out=ocT[:], in_=oc_ps[:].rearrange("p (h t) -> p h t", h=4), func=AF.Copy), [bocp], [boc])
                bpool.release(ho)
                yield
                for nb in range(2):
                    hy = yield from acq(bpool)
                    y_ps, byp = hy
                    P.op("pe", [(lambda e, h=h, nb=nb, y_ps=y_ps: e.matmul(y_ps[:], ocT[:, h, :], wco[:, h, nb * 512:(nb + 1) * 512], start=(h == 0), stop=(h == 3)))
                                for h in range(4)], [boc, B_c], [byp])
                    yield
                    P.op("dve", lambda e, nb=nb, y_ps=y_ps: e.tensor_tensor(out=xt[:, nb * 512:(nb + 1) * 512], in0=xt[:, nb * 512:(nb + 1) * 512], in1=y_ps[:], op=ALU.add),
                         [byp, bx], [bx])
                    bpool.release(hy)
                    yield
                gsets.release(Gs)
                P.dma("sp", lambda e: e.dma_start(out=x2_d[tile * 128:(tile + 1) * 128, :], in_=xt[:]), [bx], [])
                yield

            run_pipeline((a_tile(0, j) for j in range(4)), 4)
            for gi in range(8):
                run_pipeline(iter([b_chunk(g) for g in range(4)] + [c_chunk(gi, dc) for dc in range(8)]), 4)
                de = []
                for j in range(4):
                    de += [d_part(j, 0), d_part(j, 1), e_tile(j)]
                run_pipeline(iter(de), 4)
                run_pipeline((f_head(h) for h in range(4)), 4)
                ga_ = [g_tile(gi, j) for j in range(4)]
                if gi + 1 < 8:
                    ga_ += [a_tile(gi + 1, j) for j in range(4)]
                run_pipeline(iter(ga_), 4)
            P.flush()

        if dbg == 3:
            dx = nc.dram_tensor("dbg_x2", [OWN, D], F32, kind="ExternalOutput").ap()
            P.dma("sp", lambda e: e.dma_start(out=dx[:, :], in_=x2_d[:, :]), [], [])
            P.flush()
            return nc

        NI = 32
        IT = 512
        NS = NI * IT
        hs_d = nc.dram_tensor("hs_scr", [NS, D], BF16, kind="Internal").ap()
        ys_d = nc.dram_tensor("ys_scr", [NS, D], F32, kind="Internal").ap()
        trid = din("tri", [128, 128])
        iotad = din("iota32", [128, 32])
        thrd = din("thr8", [128, 8])
        with contextlib.ExitStack() as ph:
            gmoT = sb(ph, "gmoT", [128, 8])
            wr = sb(ph, "wr", [128, 8, 20], BF16)
            br_bc = sb(ph, "br_bc", [128, 20])
            gfin_bc = sb(ph, "gfin_bc", [128, 1024])
            oh1 = sb(ph, "oh1", [128, 32, 16])
            oh2 = sb(ph, "oh2", [128, 32, 16])
            wts = sb(ph, "wts", [128, 32, 2])
            idx1 = sb(ph, "idx1", [128, 32], mybir.dt.int32)
            idx2 = sb(ph, "idx2", [128, 32], mybir.dt.int32)
            Ei = sb(ph, "Ei", [128, 32], mybir.dt.int32)
            Widx = sb(ph, "Widx", [128, 32], mybir.dt.int32)
            pidx = sb(ph, "pidx", [128, 1])
            P.dma("sp", lambda e: e.dma_start(out=pidx[:], in_=pidxd[:, :]), [], [])
            B_c = Buf("c3")
            P.dma("sp", lambda e: e.dma_start(out=gmoT[:], in_=gT_view(g_moe), allow_slow_non_contiguous=True), [], [B_c])
            P.dma("pool", lambda e: e.dma_start(out=wr[:, :, 0:4], in_=wv(w_rg, 0, 4)), [], [B_c])
            P.dma("pool", lambda e: e.dma_start(out=wr[:, :, 4:20], in_=wv(w_re, 0, 16)), [], [B_c])
            P.dma("sp", lambda e: e.dma_start(out=br_bc[:, 0:4], in_=b_rg.partition_broadcast(128)), [], [B_c])
            P.dma("sp", lambda e: e.dma_start(out=br_bc[:, 4:20], in_=b_re.partition_broadcast(128)), [], [B_c])
            P.dma("sp", lambda e: e.dma_start(out=gfin_bc[:], in_=g_final.partition_broadcast(128)), [], [B_c])
            allbanks = [(ps(ph, "p4g%d" % i, [128, 512]), Buf("g", True)) for i in range(8)]
            bpool = Pool(allbanks)
            gen = Rot(allbanks)

            with contextlib.ExitStack() as sp1:
                hm_all = sb(sp1, "hm_all", [128, 32, 1024], BF16)
                gmo_bc = sb(sp1, "gmo_bc", [128, 1024])
                tri = sb(sp1, "tri", [128, 128], BF16)
                onesb = sb(sp1, "onesb", [128, 128], BF16)
                iota32 = sb(sp1, "iota32", [128, 32])
                thr8 = sb(sp1, "thr8", [128, 8])
                Mb = sb(sp1, "Mb", [128, 32, 16], BF16)
                pos_all = sb(sp1, "pos_all", [128, 32, 16])
                base = sb(sp1, "base", [128, 16])
                P.dma("sp", lambda e: e.dma_start(out=gmo_bc[:], in_=g_moe.partition_broadcast(128)), [], [B_c])
                P.dma("pool", lambda e: e.dma_start(out=tri[:], in_=trid[:, :]), [], [B_c])
                P.dma("sp", lambda e: e.dma_start(out=iota32[:], in_=iotad[:, :]), [], [B_c])
                P.dma("sp", lambda e: e.dma_start(out=thr8[:], in_=thrd[:, :]), [], [B_c])
                P.op("pool", lambda e: e.memset(onesb[:], 1.0), [], [B_c])
                P.op("pool", lambda e: e.memset(base[:], 0.0), [], [B_c])
                P.barrier()
                NP3 = 6
                st = norm_state(sp1, "p4", nsets=NP3, nxb=NP3)
                xts = Rot([(sb(sp1, "p4x%d" % i, [128, 1024]), Buf("x")) for i in range(NP3)])
                hTt = Rot([(sb(sp1, "p4h%d" % i, [128, 8, 128], BF16), Buf("h")) for i in range(NP3)])
                rts = Rot([(sb(sp1, "rt%d" % i, [128, 160]), [Buf("rt") for _ in range(24)]) for i in range(NP3)])
                boh = [Buf("oh%d" % i) for i in range(32)]
                bhm = [Buf("hm%d" % i) for i in range(32)]

                def moe_tile(i):
                    xt, bx = xts.next()
                    P.dma("sp", lambda e: e.dma_start(out=xt[:], in_=x2_d[i * 128:(i + 1) * 128, :]), [], [bx])
                    yield
                    hT, bh = hTt.next()
                    yield from norm_T_gen(st, xt[:], bx, gmoT, hT[:], bh, None, None, pool=bpool, tok=(hm_all[:, i, :], gmo_bc[:], B_c, bhm[i]))
                    hb = yield from acq(bpool)
                    r_ps, brp = hb
                    P.op("pe", [(lambda e, c=c: e.matmul(r_ps[:, 0:20], hT[:, c, :], wr[:, c, :], start=(c == 0), stop=(c == 7)))
                                for c in range(8)], [bh, B_c], [brp])
                    yield
                    R, brt = rts.next()
                    o1 = oh1[:, i, :]
                    o2 = oh2[:, i, :]
                    P.op("dve", lambda e: e.tensor_tensor(out=R[:, 0:20], in0=r_ps[:, 0:20], in1=br_bc[:], op=ALU.add), [brp, B_c], [brt[0]])
                    bpool.release(hb)
                    yield
                    P.op("dve", lambda e: e.tensor_reduce(out=R[:, 20:21], in_=R[:, 0:4], axis=AX.X, op=ALU.max), [brt[0]], [brt[1]])
                    yield
                    P.op("dve", lambda e: e.tensor_scalar(out=R[:, 21:22], in0=R[:, 20:21], scalar1=-1.0, scalar2=None, op0=ALU.mult), [brt[1]], [brt[2]])
                    yield
                    P.op("act", lambda e: e.activation(out=R[:, 22:26], in_=R[:, 0:4], func=AF.Exp, bias=R[:, 21:22], accum_out=R[:, 26:27]), [brt[0], brt[2]], [brt[3]])
                    yield
                    P.op("dve", lambda e: e.reciprocal(out=R[:, 27:28], in_=R[:, 26:27]), [brt[3]], [brt[4]])
                    yield
                    P.op("dve", lambda e: e.tensor_scalar(out=R[:, 28:32], in0=R[:, 0:4], scalar1=R[:, 20:21], scalar2=None, op0=ALU.is_ge), [brt[0], brt[1]], [brt[5]])
                    yield
                    P.op("dve", lambda e: e.tensor_scalar(out=R[:, 32:36], in0=R[:, 28:32], scalar1=BIG, scalar2=-BIG, op0=ALU.mult, op1=ALU.add), [brt[5]], [brt[6]])
                    yield
                    P.op("dve", lambda e: e.tensor_tensor(out=R[:, 40:56].rearrange("p (g k) -> p g k", g=4), in0=R[:, 4:20].rearrange("p (g k) -> p g k", g=4),
                                                          in1=R[:, 32:36].unsqueeze(2).broadcast_to([128, 4, 4]), op=ALU.add), [brt[0], brt[6]], [brt[7]])
                    yield
                    P.op("dve", lambda e: e.tensor_reduce(out=R[:, 56:57], in_=R[:, 40:56], axis=AX.X, op=ALU.max), [brt[7]], [brt[8]])
                    yield
                    P.op("dve", lambda e: e.tensor_scalar(out=o1, in0=R[:, 40:56], scalar1=R[:, 56:57], scalar2=None, op0=ALU.is_ge), [brt[7], brt[8]], [brt[9]])
                    yield
                    P.op("dve", lambda e: e.scalar_tensor_tensor(out=R[:, 80:96], in0=o1, scalar=-BIG, in1=R[:, 40:56], op0=ALU.mult, op1=ALU.add),
                         [brt[9], brt[7]], [brt[10]])
                    yield
                    P.op("dve", lambda e: e.tensor_reduce(out=R[:, 96:97], in_=R[:, 80:96], axis=AX.X, op=ALU.max), [brt[10]], [brt[11]])
                    yield
                    P.op("dve", lambda e: e.tensor_scalar(out=o2, in0=R[:, 80:96], scalar1=R[:, 96:97], scalar2=None, op0=ALU.is_ge), [brt[10], brt[11]], [brt[12]])
                    yield
                    P.op("dve", lambda e: e.tensor_tensor(out=R[:, 116:117], in0=R[:, 96:97], in1=R[:, 56:57], op=ALU.subtract), [brt[11], brt[8]], [brt[13]])
                    yield
                    P.op("act", lambda e: e.activation(out=R[:, 117:118], in_=R[:, 116:117], func=AF.Exp), [brt[13]], [brt[14]])
                    yield
                    P.op("dve", lambda e: e.tensor_scalar(out=R[:, 118:119], in0=R[:, 117:118], scalar1=1.0, scalar2=None, op0=ALU.add), [brt[14]], [brt[15]])
                    yield
                    P.op("dve", lambda e: e.reciprocal(out=R[:, 119:120], in_=R[:, 118:119]), [brt[15]], [brt[16]])
                    yield
                    P.op("dve", lambda e: e.tensor_tensor(out=wts[:, i, 0:1], in0=R[:, 119:120], in1=R[:, 27:28], op=ALU.mult), [brt[16], brt[4]], [brt[17]])
                    yield
                    P.op("dve", lambda e: e.tensor_tensor(out=wts[:, i, 1:2], in0=wts[:, i, 0:1], in1=R[:, 117:118], op=ALU.mult), [brt[17], brt[14]], [brt[18]])
                    yield
                    P.op("dve", lambda e: e.tensor_tensor(out=Mb[:, i, :], in0=o1, in1=o2, op=ALU.add), [brt[9], brt[12]], [boh[i]])
                    yield

                run_pipeline((moe_tile(i) for i in range(32)), NP3)
                P.barrier()
                prefb = Rot([bpool.acquire(), bpool.acquire()])
                bbase = Buf("base")
                bpos = Buf("pos")
                for i in range(32):
                    pb, bpb = prefb.next()
                    P.op("pe", [lambda e, pb=pb, i=i: e.matmul(pb[:, 0:16], tri[:], Mb[:, i, :], start=True, stop=True),
                                lambda e, pb=pb, i=i: e.matmul(pb[:, 16:32], onesb[:], Mb[:, i, :], start=True, stop=True)], [], [bpb])
                    P.op("dve", lambda e, pb=pb, i=i: e.tensor_tensor(out=pos_all[:, i, :], in0=pb[:, 0:16], in1=base[:], op=ALU.add), [bpb, bbase], [bpos])
                    P.op("dve", lambda e, pb=pb: e.tensor_tensor(out=base[:], in0=pb[:, 16:32], in1=base[:], op=ALU.add), [bpb, bbase], [bbase])
                for it_ in prefb.items:
                    bpool.release(it_)
                P.barrier()
                sm = sb(sp1, "sm", [128, 256])
                big = sb(sp1, "bigt", [128, 32, 16])
                big2 = sb(sp1, "bigt2", [128, 32, 16])
                sf = sb(sp1, "sf", [128, 64])
                bs_ = Buf("sm")

                def S1(fn, eng="dve"):
                    P.op(eng, fn, [bs_], [bs_])
                S1(lambda e: e.tensor_tensor(out=sm[:, 0:128].rearrange("p (a k) -> p a k", k=8), in0=base[:].unsqueeze(2).broadcast_to([128, 16, 8]),
                                             in1=thr8[:].unsqueeze(1).broadcast_to([128, 16, 8]), op=ALU.is_gt))
                S1(lambda e: e.tensor_reduce(out=sm[:, 128:144], in_=sm[:, 0:128].rearrange("p (a k) -> p a k", k=8), axis=AX.X, op=ALU.add))
                S1(lambda e: e.tensor_copy(out=sm[:, 144:160], in_=sm[:, 128:144]))
                cur, oth = 144, 160
                for k in (1, 2, 4, 8):
                    S1(lambda e, cur=cur, oth=oth, k=k: e.tensor_copy(out=sm[:, oth:oth + k], in_=sm[:, cur:cur + k]))
                    S1(lambda e, cur=cur, oth=oth, k=k: e.tensor_tensor(out=sm[:, oth + k:oth + 16], in0=sm[:, cur + k:cur + 16], in1=sm[:, cur:cur + 16 - k], op=ALU.add))
                    cur, oth = oth, cur
                S1(lambda e, cur=cur: e.tensor_copy(out=sm[:, 176:192], in_=sm[:, cur:cur + 16]))
                S1(lambda e: e.tensor_tensor(out=sm[:, 192:208], in0=sm[:, 176:192], in1=sm[:, 128:144], op=ALU.subtract))
                S1(lambda e: e.tensor_scalar(out=sm[:, 192:208], in0=sm[:, 192:208], scalar1=float(IT), scalar2=None, op0=ALU.mult))
                S1(lambda e: e.memset(sm[:, 208:240], 0.0))
                for ex in range(NE):
                    S1(lambda e, ex=ex: e.scalar_tensor_tensor(out=sm[:, 208:240], in0=iota32[:], scalar=sm[:, 176 + ex:177 + ex], in1=sm[:, 208:240], op0=ALU.is_ge, op1=ALU.add))
                S1(lambda e: e.tensor_scalar(out=sm[:, 208:240], in0=sm[:, 208:240], scalar1=float(NE - 1), scalar2=None, op0=ALU.min))
                S1(lambda e: e.tensor_copy(out=Ei[:], in_=sm[:, 208:240]))
                S1(lambda e: e.tensor_scalar(out=sf[:, 0:32], in0=sm[:, 208:240], scalar1=128.0, scalar2=pidx[:, 0:1], op0=ALU.mult, op1=ALU.add))
                S1(lambda e: e.tensor_copy(out=Widx[:], in_=sf[:, 0:32]))
                S1(lambda e: e.tensor_tensor(out=big[:], in0=pos_all[:], in1=sm[:, 192:208].unsqueeze(1).broadcast_to([128, 32, 16]), op=ALU.add))
                S1(lambda e: e.tensor_tensor(out=big2[:], in0=big[:], in1=oh1[:], op=ALU.mult))
                S1(lambda e: e.tensor_reduce(out=sf[:, 0:32], in_=big2[:], axis=AX.X, op=ALU.add))
                S1(lambda e: e.tensor_tensor(out=big2[:], in0=big[:], in1=oh2[:], op=ALU.mult))
                S1(lambda e: e.tensor_reduce(out=sf[:, 32:64], in_=big2[:], axis=AX.X, op=ALU.add))
                S1(lambda e: e.tensor_copy(out=idx1[:], in_=sf[:, 0:32]))
                S1(lambda e: e.tensor_copy(out=idx2[:], in_=sf[:, 32:64]))
                P.barrier()
                for i in range(int(_os0.environ.get("NSCAT", 32))):
                    for ixt in (idx1, idx2):
                        P.dma("pool", lambda e, i=i, ixt=ixt: e.indirect_dma_start(out=hs_d[:, :], out_offset=bass.IndirectOffsetOnAxis(ap=ixt[:, i:i + 1], axis=0),
                                                                                   in_=hm_all[:, i, :], in_offset=None), [], [])
                P.barrier()

            if dbg == 4:
                d1 = nc.dram_tensor("dbg_idx", [128, 64], mybir.dt.int32, kind="ExternalOutput").ap()
                d2 = nc.dram_tensor("dbg_E", [128, 32], mybir.dt.int32, kind="ExternalOutput").ap()
                d3 = nc.dram_tensor("dbg_wts", [128, 64], F32, kind="ExternalOutput").ap()
                P.dma("sp", lambda e: e.dma_start(out=d1[:, 0:32], in_=idx1[:]), [], [])
                P.dma("sp", lambda e: e.dma_start(out=d1[:, 32:64], in_=idx2[:]), [], [])
                P.dma("sp", lambda e: e.dma_start(out=d2[:, :], in_=Ei[:]), [], [])
                P.dma("sp", lambda e: e.dma_start(out=d3[:, :], in_=wts[:].rearrange("p a b -> p (a b)")), [], [])
                P.flush()
                return nc

            with contextlib.ExitStack() as sp2:
                Wb = [(sb(sp2, "Wb%d" % i, [128, 12288], BF16), Buf("Wb")) for i in range(2)]
                hss = Rot([(sb(sp2, "hs%d" % i, [128, 4, 1024], BF16), Buf("hs")) for i in range(2)])
                hsTs = Rot([(sb(sp2, "hsT%d" % i, [128, 8, 512], BF16), [Buf("hsT") for _ in range(4)]) for i in range(2)])
                sgs = Rot([(sb(sp2, "sg%d" % i, [128, 512]), Buf("sg")) for i in range(2)])
                heTs = Rot([(sb(sp2, "heT%d" % i, [128, 4, 512], BF16), [Buf("he") for _ in range(4)]) for i in range(2)])
                yss = Rot([(sb(sp2, "ys%d" % i, [128, 1024]), Buf("ys")) for i in range(3)])

                def load_w(w):
                    wb, bwb = Wb[w % 2]
                    P.dma("pool", lambda e: e.indirect_dma_start(out=wb[:, :], out_offset=None, in_=wcat_bf[:, :],
                                                                 in_offset=bass.IndirectOffsetOnAxis(ap=Widx[:, w:w + 1], axis=0)), [B_wbf], [bwb])

                def item_T(w):
                    hs, bhs = hss.next()
                    P.dma("sp", lambda e, hs=hs, w=w: e.dma_start(out=hs[:], in_=hs_d[w * IT:(w + 1) * IT, :].rearrange("(s p) d -> p s d", p=128)), [], [bhs])
                    hsT, bhsT = hsTs.next()
                    for sub in range(4):
                        tb, btb = gen.next()
                        tpv = bfview(tb[:])
                        P.op("pe", [(lambda e, c=c, sub=sub, tpv=tpv, hs=hs: e.transpose(tpv[:, c, :], hs[:, sub, c * 128:(c + 1) * 128], ident[:])) for c in range(8)],
                             [bhs], [btb])
                        if sub % 2 == 0:
                            P.op("act", lambda e, sub=sub, tpv=tpv, hsT=hsT: e.activation(out=hsT[:, :, sub * 128:(sub + 1) * 128], in_=tpv, func=AF.Copy), [btb], [bhsT[sub]])
                        else:
                            P.op("dve", lambda e, sub=sub, tpv=tpv, hsT=hsT: e.tensor_copy(out=hsT[:, :, sub * 128:(sub + 1) * 128], in_=tpv), [btb], [bhsT[sub]])
                    return hsT, bhsT

                load_w(0)
                nxtT = item_T(0)
                for w in range(NI):
                    if w + 1 < NI:
                        load_w(w + 1)
                    wb, bwb = Wb[w % 2]
                    wg = wb[:, 0:4096].rearrange("p (c f) -> p c f", c=8)
                    wu = wb[:, 4096:8192].rearrange("p (c f) -> p c f", c=8)
                    wd = wb[:, 8192:12288].rearrange("p (c f) -> p c f", c=4)
                    bwg = bwu = bwd = bwb
                    hsT, bhsT = nxtT
                    heT, bhe = heTs.next()
                    for fc in range(4):
                        g_ps, bgp = gen.next()
                        u_ps, bup = gen.next()
                        P.op("pe", [(lambda e, c=c, fc=fc, g_ps=g_ps, wg=wg, hsT=hsT: e.matmul(g_ps[:], wg[:, c, fc * 128:(fc + 1) * 128], hsT[:, c, :],
                                                                                          start=(c == 0), stop=(c == 7))) for c in range(8)], bhsT + [bwg], [bgp])
                        P.op("pe", [(lambda e, c=c, fc=fc, u_ps=u_ps, wu=wu, hsT=hsT: e.matmul(u_ps[:], wu[:, c, fc * 128:(fc + 1) * 128], hsT[:, c, :],
                                                                                          start=(c == 0), stop=(c == 7))) for c in range(8)], bhsT + [bwu], [bup])
                        sg, bsg_ = sgs.next()
                        P.op("act", lambda e, sg=sg, g_ps=g_ps: e.activation(out=sg[:], in_=g_ps[:], func=AF.Silu), [bgp], [bsg_])
                        P.op("dve", lambda e, sg=sg, u_ps=u_ps, heT=heT, fc=fc: e.tensor_tensor(out=heT[:, fc, :], in0=sg[:], in1=u_ps[:], op=ALU.mult),
                             [bsg_, bup], [bhe[fc]])
                    if w + 1 < NI:
                        nxtT = item_T(w + 1)
                    for sub in range(4):
                        ys, bys = yss.next()
                        for nb in range(2):
                            y_ps, byp = gen.next()
                            P.op("pe", [(lambda e, fc=fc, sub=sub, nb=nb, y_ps=y_ps, heT=heT, wd=wd: e.matmul(y_ps[:], heT[:, fc, sub * 128:(sub + 1) * 128], wd[:, fc, nb * 512:(nb + 1) * 512],
                                                                                                             start=(fc == 0), stop=(fc == 3))) for fc in range(4)], bhe + [bwd], [byp])
                            if nb == 0:
                                P.op("act", lambda e, ys=ys, y_ps=y_ps: e.activation(out=ys[:, 0:512], in_=y_ps[:], func=AF.Copy), [byp], [bys])
                            else:
                                P.op("dve", lambda e, ys=ys, y_ps=y_ps: e.tensor_copy(out=ys[:, 512:1024], in_=y_ps[:]), [byp], [bys])
                        P.dma("sp", lambda e, ys=ys, w=w, sub=sub: e.dma_start(out=ys_d[w * IT + sub * 128:w * IT + (sub + 1) * 128, :], in_=ys[:]), [bys], [])
                P.barrier()

            with contextlib.ExitStack() as sp3:
                NC3 = 3
                st = norm_state(sp3, "p5", nsets=4, nxb=1)
                xts = Rot([(sb(sp3, "p5x%d" % i, [128, 1024]), Buf("x")) for i in range(NC3)])
                y0s = Rot([(sb(sp3, "p5y0%d" % i, [128, 1024]), Buf("y0")) for i in range(NC3)])
                y1s = Rot([(sb(sp3, "p5y1%d" % i, [128, 1024]), Buf("y1")) for i in range(NC3)])
                ots = Rot([(sb(sp3, "p5o%d" % i, [128, 1024]), Buf("ot")) for i in range(NC3)])

                def fin_tile(i):
                    xt, bx = xts.next()
                    y0, by0 = y0s.next()
                    y1, by1 = y1s.next()
                    P.dma("sp", lambda e: e.dma_start(out=xt[:], in_=x2_d[i * 128:(i + 1) * 128, :]), [], [bx])
                    yield
                    P.dma("pool", lambda e: e.indirect_dma_start(out=y0[:, :], out_offset=None, in_=ys_d[:, :],
                                                                 in_offset=bass.IndirectOffsetOnAxis(ap=idx1[:, i:i + 1], axis=0)), [], [by0])
                    yield
                    P.dma("pool", lambda e: e.indirect_dma_start(out=y1[:, :], out_offset=None, in_=ys_d[:, :],
                                                                 in_offset=bass.IndirectOffsetOnAxis(ap=idx2[:, i:i + 1], axis=0)), [], [by1])
                    yield
                    P.op("dve", lambda e: e.scalar_tensor_tensor(out=xt[:], in0=y0[:], scalar=wts[:, i, 0:1], in1=xt[:], op0=ALU.mult, op1=ALU.add), [by0, bx], [bx])
                    yield
                    P.op("dve", lambda e: e.scalar_tensor_tensor(out=xt[:], in0=y1[:], scalar=wts[:, i, 1:2], in1=xt[:], op0=ALU.mult, op1=ALU.add), [by1, bx], [bx])
                    yield
                    fss, fb, _u = st["sets"].next()
                    ot, bot = ots.next()
                    P.op("act", lambda e: e.activation(out=ot[:], in_=xt[:], func=AF.Square, scale=1.0 / 32, accum_out=fss[:, 0:1]), [bx], [fb[0], bot])
                    yield
                    P.op("dve", lambda e: e.tensor_scalar(out=fss[:, 1:2], in0=fss[:, 0:1], scalar1=EPS, scalar2=None, op0=ALU.add), [fb[0]], [fb[1]])
                    yield
                    P.op("act", lambda e: e.activation(out=fss[:, 2:3], in_=fss[:, 1:2], func=AF.Ln), [fb[1]], [fb[2]])
                    yield
                    P.op("act", lambda e: e.activation(out=fss[:, 3:4], in_=fss[:, 2:3], func=AF.Exp, scale=-0.5), [fb[2]], [fb[3]])
                    yield
                    P.op("dve", lambda e: e.scalar_tensor_tensor(out=ot[:], in0=xt[:], scalar=fss[:, 3:4], in1=gfin_bc[:], op0=ALU.mult, op1=ALU.mult),
                         [bx, fb[3], B_c], [bot])
                    yield
                    P.dma("sp", lambda e: e.dma_start(out=out[i * 128:(i + 1) * 128, :], in_=ot[:]), [bot], [])
                    yield

                run_pipeline((fin_tile(i) for i in range(32)), NC3)
                P.barrier()
            P.flush()
    return nc


def _rope_tables():
    rows = SEQ // 64
    row_ids = np.repeat(np.arange(rows), 64).astype(np.float32)
    col_ids = np.tile(np.arange(64), rows).astype(np.float32)
    inv = (10000.0 ** (-np.arange(16, dtype=np.float32) / 16)).astype(np.float32)
    ang = np.concatenate([row_ids[:, None] * inv[None, :], col_ids[:, None] * inv[None, :]], axis=-1).astype(np.float32)
    return np.cos(ang).astype(np.float32), np.sin(ang).astype(np.float32)


def _wcat(wg, wu, wd):
    g = wg.reshape(NE, 8, 128, 512).transpose(0, 2, 1, 3).reshape(NE, 128, 4096)
    u = wu.reshape(NE, 8, 128, 512).transpose(0, 2, 1, 3).reshape(NE, 128, 4096)
    d_ = wd.reshape(NE, 4, 128, 1024).transpose(0, 2, 1, 3).reshape(NE, 128, 4096)
    return np.ascontiguousarray(np.concatenate([g, u, d_], axis=2).reshape(NE * 128, 12288))


def make_in_maps(inputs):
    f = lambda a: np.ascontiguousarray(np.asarray(a, dtype=np.float32))
    x = f(inputs["x"])
    mem = f(inputs["mem"])
    cos, sin = _rope_tables()

    def lay(t, n):
        return np.ascontiguousarray(t.reshape(n, 128, 32).transpose(1, 0, 2))

    shared = {
        "ident": np.eye(128, dtype=np.float32),
        "g_mix": f(inputs["g_mix"][0]), "w_in": f(inputs["w_in"][0]), "g_q": f(inputs["g_q"][0]), "g_k": f(inputs["g_k"][0]),
        "w_att_out": f(inputs["w_att_out"][0]), "sgu_ln_g": f(inputs["sgu_ln_g"][0]), "sgu_ln_b": f(inputs["sgu_ln_b"][0]),
        "w_s": f(inputs["w_s"][0]), "b_s": f(inputs["b_s"][0]), "w_sgu_out": f(inputs["w_sgu_out"][0]), "w_out": f(inputs["w_out"][0]),
        "g_cross": f(inputs["g_cross"][0]), "g_mem": f(inputs["g_mem"][0]), "w_cq": f(inputs["w_cq"][0]), "w_ckv": f(inputs["w_ckv"][0]),
        "w_co": f(inputs["w_co"][0]), "g_moe": f(inputs["g_moe"][0]), "w_rg": f(inputs["w_rg"][0]), "b_rg": f(inputs["b_rg"][0]),
        "w_re": f(inputs["w_re"][0]), "b_re": f(inputs["b_re"][0]), "g_final": f(inputs["g_final"]),
        "wcat": _wcat(f(inputs["w_gate"][0]), f(inputs["w_up"][0]), f(inputs["w_down"][0])),
        "pidx": np.arange(128, dtype=np.float32).reshape(128, 1),
        "cos_seq": lay(cos, 64), "sin_seq": lay(sin, 64),
        "tri": np.triu(np.ones((128, 128), np.float32), 1),
        "iota32": np.tile(np.arange(32, dtype=np.float32), (128, 1)),
        "thr8": np.tile(np.arange(8, dtype=np.float32) * 512.0, (128, 1)),
    }
    maps = []
    for c in range(8):
        b, hf = c // 2, c % 2
        m = dict(shared)
        m["xseq"] = x[b]
        m["xown"] = np.ascontiguousarray(x[b, hf * OWN:(hf + 1) * OWN])
        m["mem"] = mem[b]
        m["cos_own"] = lay(cos[hf * OWN:(hf + 1) * OWN], 32)
        m["sin_own"] = lay(sin[hf * OWN:(hf + 1) * OWN], 32)
        maps.append(m)
    return maps


def kernel(**inputs):
    nc = build()
    maps = make_in_maps(inputs)
    res = run_bass_kernel_spmd(nc, maps, core_ids=list(range(8)))
    outp = np.empty((4, SEQ, D), np.float32)
    for c in range(8):
        b, hf = c // 2, c % 2
        outp[b, hf * OWN:(hf + 1) * OWN] = np.asarray(res.results[c]["out"], dtype=np.float32)
    return outp
```

```python
import contextlib
import os as _os0
import numpy as np
import concourse.bass as bass
import concourse.mybir as mybir
from concourse.bass_utils import run_bass_kernel_spmd

F32 = mybir.dt.float32
BF16 = mybir.dt.bfloat16
AF = mybir.ActivationFunctionType
ALU = mybir.AluOpType
AX = mybir.AxisListType

D = 1024
SEQ = 8192
OWN = 4096
NMEM = 256
EPS = 1e-6
NE = 16
BIG = 1.0e30


class Buf:
    __slots__ = ("name", "w", "r", "psum")

    def __init__(self, name="", psum=False):
        self.name = name
        self.psum = psum
        self.w = None
        self.r = {}


class Eng:
    def __init__(self, name):
        self.name = name
        self.sem = None
        self.count = 0
        self.seen = {}
        self.prog = []
        self.chsems = []
        self.dma_i = 0


class Prog:
    NCH = int(_os0.environ.get('NCH', 8))

    def __init__(self, nc, stack):
        self.nc = nc
        self.E = {}
        for n in ("pe", "act", "dve", "pool", "sp"):
            e = Eng(n)
            e.sem = stack.enter_context(nc.semaphore("sem_" + n))
            self.E[n] = e
        for n in ("sp", "pool", "act"):
            e = self.E[n]
            for i in range(self.NCH):
                e.chsems.append(stack.enter_context(nc.semaphore("ch_%s_%d" % (n, i))))
        self.chan_issued = {}
        import os
        self.limit = int(os.environ.get("OPLIMIT", 10 ** 9))
        self.nops = 0

    def _need(self, need, ev):
        sem, val, src = ev
        k = id(sem)
        if k not in need or need[k][1] < val:
            need[k] = (sem, val, src)

    def _deps(self, eng, reads, writes, extra=()):
        E = self.E[eng]
        need = {}
        for b in reads:
            if b.w is not None:
                self._need(need, b.w)
        for b in writes:
            if b.w is not None:
                self._need(need, b.w)
            for k, (sem, val, src) in b.r.items():
                self._need(need, (sem, val, src))
        for ev in extra:
            self._need(need, ev)
        for k, (sem, val, src) in need.items():
            if src == "pe" and eng == "pe":
                continue
            if E.seen.get(k, 0) >= val:
                continue
            E.seen[k] = val
            E.prog.append(("wait", sem, val))

    def _commit(self, ev, reads, writes):
        sem, val, src = ev
        for b in writes:
            b.w = ev
            b.r = {}
        for b in reads:
            if b not in writes:
                b.r[id(sem)] = (sem, val, src)

    def op(self, eng, fns, reads=(), writes=()):
        if not isinstance(fns, (list, tuple)):
            fns = [fns]
        self.nops += 1
        if self.nops > self.limit:
            return
        pr = [b for b in reads if b.psum]
        if pr:
            reads = [b for b in reads if not b.psum]
            writes = list(writes) + [b for b in pr if b not in writes]
        E = self.E[eng]
        self._deps(eng, reads, writes)
        E.count += 1
        for f in fns[:-1]:
            E.prog.append(("raw", f))
        E.prog.append(("op", fns[-1], E.sem))
        self._commit((E.sem, E.count, eng), reads, writes)

    def dma(self, q, fn, reads=(), writes=()):
        self.nops += 1
        if self.nops > self.limit:
            return
        E = self.E[q]
        ch = E.dma_i % self.NCH
        rnd = E.dma_i // self.NCH
        E.dma_i += 1
        chsem = E.chsems[ch]
        extra = []
        if rnd > 0:
            extra.append((chsem, 16 * rnd, "dma"))
        self._deps(q, reads, writes, extra)
        E.prog.append(("dma", fn, chsem))
        self.chan_issued[id(chsem)] = (chsem, 16 * (rnd + 1))
        self._commit((chsem, 16 * (rnd + 1), "dma"), reads, writes)

    def dma_bg(self, q, fn, sem, total, buf):
        E = self.E[q]
        E.prog.append(("dma", fn, sem))
        buf.w = (sem, total, "dma")

    def barrier(self):
        for n, E in self.E.items():
            for m, F in self.E.items():
                if m == n or F.count == 0:
                    continue
                k = id(F.sem)
                if E.seen.get(k, 0) < F.count:
                    E.seen[k] = F.count
                    E.prog.append(("wait", F.sem, F.count))
            for k, (sem, val) in self.chan_issued.items():
                if E.seen.get(k, 0) < val:
                    E.seen[k] = val
                    E.prog.append(("wait", sem, val))

    def flush(self):
        self.barrier()
        nc = self.nc
        progs = {n: E.prog for n, E in self.E.items()}
        for E in self.E.values():
            E.prog = []

        def replay(eng, prog):
            for it in prog:
                if it[0] == "wait":
                    eng.wait_ge(it[1], it[2])
                elif it[0] == "raw":
                    it[1](eng)
                elif it[0] == "op":
                    it[1](eng).then_inc(it[2], 1)
                else:
                    it[1](eng).then_inc(it[2], 16)

        with nc.Block() as block:
            @block.tensor
            def _(e):
                replay(e, progs["pe"])

            @block.scalar
            def _(e):
                replay(e, progs["act"])

            @block.vector
            def _(e):
                replay(e, progs["dve"])

            @block.gpsimd
            def _(e):
                replay(e, progs["pool"])

            @block.sync
            def _(e):
                replay(e, progs["sp"])


class Rot:
    def __init__(self, items):
        self.items = items
        self.i = 0

    def next(self):
        it = self.items[self.i % len(self.items)]
        self.i += 1
        return it


class Pool:
    def __init__(self, items):
        self.free = list(items)

    def acquire(self):
        return self.free.pop(0) if self.free else None

    def release(self, it):
        self.free.append(it)


def build(dbg=False):
    nc = bass.Bass("TRN2", target_bir_lowering=False)

    def din(name, shape):
        return nc.dram_tensor(name, list(shape), F32, kind="ExternalInput").ap()

    xseq = din("xseq", [SEQ, D])
    xown = din("xown", [OWN, D])
    memd = din("mem", [NMEM, D])
    cos_seq = din("cos_seq", [128, 64, 32])
    sin_seq = din("sin_seq", [128, 64, 32])
    cos_own = din("cos_own", [128, 32, 32])
    sin_own = din("sin_own", [128, 32, 32])
    identd = din("ident", [128, 128])
    g_mix = din("g_mix", [D])
    w_in = din("w_in", [D, 3840])
    g_q = din("g_q", [64])
    g_k = din("g_k", [64])
    w_att_out = din("w_att_out", [512, D])
    sgu_ln_g = din("sgu_ln_g", [512])
    sgu_ln_b = din("sgu_ln_b", [512])
    w_s = din("w_s", [4, 128, 128])
    b_s = din("b_s", [4, 128])
    w_sgu_out = din("w_sgu_out", [512, D])
    w_out = din("w_out", [D, D])
    g_cross = din("g_cross", [D])
    g_mem = din("g_mem", [D])
    w_cq = din("w_cq", [D, 512])
    w_ckv = din("w_ckv", [D, D])
    w_co = din("w_co", [512, D])
    g_moe = din("g_moe", [D])
    w_rg = din("w_rg", [D, 4])
    b_rg = din("b_rg", [4])
    w_re = din("w_re", [D, 16])
    b_re = din("b_re", [16])
    wcat = din("wcat", [NE * 128, 12288])
    pidxd = din("pidx", [128, 1])
    wcat_bf = nc.dram_tensor("wcat_bf", [NE * 128, 12288], BF16, kind="Internal").ap()
    g_final = din("g_final", [D])
    out = nc.dram_tensor("out", [OWN, D], F32, kind="ExternalOutput").ap()
    yatt_d = nc.dram_tensor("yatt_scr", [8, 128, 8, 512], F32, kind="Internal").ap()
    x2_d = nc.dram_tensor("x2_scr", [OWN, D], F32, kind="Internal").ap()

    def wv(w, c0, c1):
        return w.rearrange("(c p) n -> p c n", p=128)[:, :, c0:c1]

    def gT_view(g):
        return g.rearrange("(c p) -> p c", p=128)

    with contextlib.ExitStack() as top:
        P = Prog(nc, top)
        bgsem = top.enter_context(nc.semaphore("bgsem"))
        B_wbf = Buf("wcat_bf")
        print("sbuf bytes remaining at start:", nc.sbuf_bytes_remaining)

        def sb(stack, name, shape, dt=F32):
            return stack.enter_context(nc.sbuf_tensor("s_" + name, list(shape), dt))

        def ps(stack, name, shape, dt=F32):
            return stack.enter_context(nc.psum_tensor("p_" + name, list(shape), dt))

        ident = sb(top, "ident", [128, 128], BF16)
        kcT = sb(top, "kcT", [128, 4, NMEM], BF16)
        Vc = sb(top, "Vc", [128, 2, 512], BF16)
        gmixT = sb(top, "gmixT", [128, 8])
        attn_stack = contextlib.ExitStack()
        kT_all = sb(attn_stack, "kT_all", [128, SEQ], BF16)
        V_all = sb(attn_stack, "V_all", [128, 64, 2, 65], BF16)
        B_const = Buf("const")

        P.dma("pool", lambda e: e.dma_start(out=ident[:], in_=identd[:, :]), [], [B_const])
        P.dma("sp", lambda e: e.dma_start(out=gmixT[:], in_=gT_view(g_mix), allow_slow_non_contiguous=True), [], [B_const])
        P.op("pool", lambda e: e.memset(V_all[:], 1.0), [], [B_const])

        def run_pipeline(gens, depth):
            it = iter(gens)
            active = []
            more = True
            while True:
                if more and len(active) < depth:
                    try:
                        active.append(next(it))
                    except StopIteration:
                        more = False
                if not active:
                    break
                for g in list(active):
                    try:
                        next(g)
                    except StopIteration:
                        active.remove(g)

        def drain(g):
            for _ in g:
                pass

        def acq(pool):
            while True:
                it = pool.acquire()
                if it is not None:
                    return it
                yield

        def bfview(bank_ap):
            return bank_ap.bitcast(BF16).rearrange("p (c t) -> p c t", c=8)

        def norm_T_gen(st, xt_ap, xbuf, gT, out_ap3, outbuf, tps, tpsb, pool=None, tok=None):
            ss, b, _unused = st["sets"].next()
            xb, bxb = st["xb"].next()
            P.op("act", lambda e: e.activation(out=xb[:], in_=xt_ap, func=AF.Square, scale=1.0 / 32,
                                               accum_out=ss[:, 0:1]), [xbuf], [b[0], bxb])
            yield
            P.op("dve", lambda e: e.tensor_scalar(out=ss[:, 1:2], in0=ss[:, 0:1], scalar1=EPS, scalar2=None,
                                                  op0=ALU.add), [b[0]], [b[1]])
            yield
            P.op("act", lambda e: e.activation(out=ss[:, 2:3], in_=ss[:, 1:2], func=AF.Ln), [b[1]], [b[2]])
            yield
            P.op("act", lambda e: e.activation(out=ss[:, 3:4], in_=ss[:, 2:3], func=AF.Exp, scale=-0.5), [b[2]], [b[3]])
            yield
            P.op("act", lambda e: e.activation(out=xb[:], in_=xt_ap, func=AF.Copy, scale=ss[:, 3:4]), [xbuf, b[3]], [bxb])
            yield
            if tok is not None:
                P.op("dve", lambda e: e.tensor_tensor(out=tok[0], in0=xb[:], in1=tok[1], op=ALU.mult), [bxb, tok[2]], [tok[3]])
                yield
            held = None
            if pool is not None:
                while held is None:
                    held = pool.acquire()
                    if held is None:
                        yield
                tps, tpsb = bfview(held[0][:]), held[1]
            P.op("pe", [(lambda e, c=c: e.transpose(tps[:, c, :], xb[:, c * 128:(c + 1) * 128], ident[:])) for c in range(8)],
                 [bxb], [tpsb])
            yield
            P.op("dve", lambda e: e.tensor_tensor(out=out_ap3, in0=tps, in1=gT[:, 0:8].unsqueeze(2).broadcast_to([128, 8, 128]),
                                                  op=ALU.mult), [tpsb], [outbuf])
            if held is not None:
                pool.release(held)
            yield

        def norm_T(st, xt_ap, xbuf, gT, out_ap3, outbuf, tps, tpsb):
            drain(norm_T_gen(st, xt_ap, xbuf, gT, out_ap3, outbuf, tps[:], tpsb))

        def norm_state(stack, pfx, nsets=2, nxb=2):
            st = {}
            st["sets"] = Rot([(sb(stack, pfx + "ss%d" % i, [128, 4]), [Buf("ss") for _ in range(4)], None) for i in range(nsets)])
            st["xb"] = Rot([(sb(stack, pfx + "xb%d" % i, [128, 1024], BF16), Buf("xb")) for i in range(nxb)])
            return st

        def head_norm_rope_gen(ws, src_ps, bsrc, H, g_bc, cosb, sinb, dst, bdst, t):
            if isinstance(ws, Pool):
                w = yield from acq(ws)
            else:
                w = ws.next()
            n = H * 64
            src3 = src_ps.rearrange("p (h d) -> p h d", h=H)
            P.op("act", lambda e: e.activation(out=w["sq"][:, 0:n], in_=src_ps, func=AF.Square, scale=0.125), [bsrc], [w["bsq"]])
            yield
            P.op("dve", lambda e: e.tensor_reduce(out=w["hs"][:, 0:H], in_=w["sq"][:, 0:n].rearrange("p (h d) -> p h d", h=H),
                                                  axis=AX.X, op=ALU.add), [w["bsq"]], [w["bhs"]])
            yield
            P.op("dve", lambda e: e.tensor_scalar(out=w["hs"][:, 8:8 + H], in0=w["hs"][:, 0:H], scalar1=EPS, scalar2=None, op0=ALU.add),
                 [w["bhs"]], [w["bhs1"]])
            yield
            P.op("act", lambda e: e.activation(out=w["hs"][:, 16:16 + H], in_=w["hs"][:, 8:8 + H], func=AF.Ln), [w["bhs1"]], [w["bhs2"]])
            yield
            P.op("act", lambda e: e.activation(out=w["hs"][:, 24:24 + H], in_=w["hs"][:, 16:16 + H], func=AF.Exp, scale=-0.5),
                 [w["bhs2"]], [w["bhs3"]])
            yield
            qn3 = w["qn"][:, 0:n].rearrange("p (h d) -> p h d", h=H)
            P.op("dve", lambda e: e.tensor_tensor(out=qn3, in0=src3, in1=w["hs"][:, 24:24 + H].unsqueeze(2).broadcast_to([128, H, 64]),
                                                  op=ALU.mult), [bsrc, w["bhs3"]], [w["bqn"]])
            yield
            P.op("dve", lambda e: e.tensor_tensor(out=qn3, in0=qn3, in1=g_bc[:, :].unsqueeze(1).broadcast_to([128, H, 64]),
                                                  op=ALU.mult), [w["bqn"]], [w["bqn"]])
            yield
            qn4 = w["qn"][:, 0:n].rearrange("p (h i t) -> p h i t", h=H, t=2)
            A4 = w["A"][:, 0:n].rearrange("p (h i t) -> p h i t", h=H, t=2)
            B4 = w["B"][:, 0:n].rearrange("p (h i t) -> p h i t", h=H, t=2)
            c4 = cosb[:, t, :].unsqueeze(1).unsqueeze(3).broadcast_to([128, H, 32, 2])
            s4 = sinb[:, t, :].unsqueeze(1).unsqueeze(3).broadcast_to([128, H, 32, 2])
            P.op("dve", lambda e: e.tensor_tensor(out=A4, in0=qn4, in1=c4, op=ALU.mult), [w["bqn"]], [w["bA"]])
            yield
            P.op("dve", lambda e: e.tensor_tensor(out=B4, in0=qn4, in1=s4, op=ALU.mult), [w["bqn"]], [w["bB"]])
            yield
            P.op("dve", lambda e: e.tensor_tensor(out=dst[:, :, :, 0], in0=A4[:, :, :, 0], in1=B4[:, :, :, 1], op=ALU.subtract),
                 [w["bA"], w["bB"]], [bdst])
            yield
            P.op("dve", lambda e: e.tensor_tensor(out=dst[:, :, :, 1], in0=B4[:, :, :, 0], in1=A4[:, :, :, 1], op=ALU.add),
                 [w["bA"], w["bB"]], [bdst])
            if isinstance(ws, Pool):
                ws.release(w)
            yield

        def rope_state(stack, pfx, n, nsets=1):
            sets = []
            for i in range(nsets):
                w = {}
                for k in ("sq", "qn", "A", "B"):
                    w[k] = sb(stack, "%s%s%d" % (pfx, k, i), [128, n])
                w["hs"] = sb(stack, "%shs%d" % (pfx, i), [128, 32])
                for k in ("bsq", "bhs", "bhs1", "bhs2", "bhs3", "bqn", "bA", "bB"):
                    w[k] = Buf(k)
                sets.append(w)
            return Rot(sets)

        with contextlib.ExitStack() as ph:
            wkv = sb(ph, "wkv", [128, 8, 256], BF16)
            cosb = sb(ph, "cosb", [128, 64, 32])
            sinb = sb(ph, "sinb", [128, 64, 32])
            gk_bc = sb(ph, "gk_bc", [128, 64])
            P.dma("pool", lambda e: e.dma_start(out=wkv[:], in_=wv(w_in, 512, 768)), [], [B_const])
            P.dma("sp", lambda e: e.dma_start(out=cosb[:], in_=cos_seq[:, :, :]), [], [B_const])
            P.dma("sp", lambda e: e.dma_start(out=sinb[:], in_=sin_seq[:, :, :]), [], [B_const])
            P.dma("sp", lambda e: e.dma_start(out=gk_bc[:], in_=g_k.partition_broadcast(128)), [], [B_const])
            NP1 = 8
            P.barrier()
            st = norm_state(ph, "p1", nsets=NP1, nxb=NP1)
            rws = rope_state(ph, "p1r", 128, nsets=NP1)
            xts = Rot([(sb(ph, "p1x%d" % i, [128, 1024]), Buf("x")) for i in range(NP1)])
            hTs = Rot([(sb(ph, "p1h%d" % i, [128, 8, 128], BF16), Buf("h")) for i in range(NP1)])
            krs = Rot([(sb(ph, "p1kr%d" % i, [128, 2, 32, 2], BF16), Buf("kr")) for i in range(NP1)])
            banks = Pool([(ps(ph, "p1b%d" % i, [128, 512]), Buf("bank", True)) for i in range(8)])

            import os as _os

            def p1_tile(t):
                xt, bx = xts.next()
                P.dma("sp", lambda e: e.dma_start(out=xt[:], in_=xseq[t * 128:(t + 1) * 128, :]), [], [bx])
                yield
                hT, bh = hTs.next()
                yield from norm_T_gen(st, xt[:], bx, gmixT, hT[:], bh, None, None, pool=banks)
                hkv = yield from acq(banks)
                kv, bkv = hkv
                P.op("pe", [(lambda e, c=c: e.matmul(kv[:, 0:256], hT[:, c, :], wkv[:, c, :], start=(c == 0), stop=(c == 7)))
                            for c in range(8)], [bh, B_const], [bkv])
                yield
                P.op("act", lambda e: e.activation(out=V_all[:, t, :, 0:64], in_=kv[:, 128:256].rearrange("p (h d) -> p h d", h=2),
                                                   func=AF.Copy), [bkv], [])
                yield
                kr, bkr = krs.next()
                yield from head_norm_rope_gen(rws, kv[:, 0:128], bkv, 2, gk_bc, cosb, sinb, kr, bkr, t)
                banks.release(hkv)
                hkt = yield from acq(banks)
                kt, bkt = hkt
                ktv = kt[:].bitcast(BF16)
                P.op("pe", lambda e: e.transpose(ktv[:, 0:128], kr[:].rearrange("p h i t -> p (h i t)"), ident[:]), [bkr], [bkt])
                yield
                P.op("act", lambda e: e.activation(out=kT_all[:, t * 128:(t + 1) * 128], in_=ktv[:, 0:128], func=AF.Copy), [bkt], [])
                banks.release(hkt)
                yield

            run_pipeline((p1_tile(t) for t in range(int(_os.environ.get('NT1', 64)))), int(_os.environ.get('DEPTH1', 8)))
            P.flush()

        if dbg == 1:
            P.limit = 10 ** 9
            P.limit = 10 ** 9
            print("nops", P.nops)
            dk = nc.dram_tensor("dbg_kT", [128, SEQ], BF16, kind="ExternalOutput").ap()
            dv = nc.dram_tensor("dbg_V", [128, 64 * 2 * 65], BF16, kind="ExternalOutput").ap()
            P.dma("sp", lambda e: e.dma_start(out=dk[:, :], in_=kT_all[:]), [], [])
            P.dma("sp", lambda e: e.dma_start(out=dv[:, :], in_=V_all[:].rearrange("p a b c -> p (a b c)")), [], [])
            P.flush()
            attn_stack.close()
            return nc

        with contextlib.ExitStack() as ph:
            wq = sb(ph, "wq", [128, 8, 512], BF16)
            wao = sb(ph, "wao", [64, 8, 1024], BF16)
            cosb = sb(ph, "cosb2", [128, 32, 32])
            sinb = sb(ph, "sinb2", [128, 32, 32])
            gq_bc = sb(ph, "gq_bc", [128, 64])
            ones65 = sb(ph, "ones65", [65, 64])
            B_c2 = Buf("c2")
            for hd_ in range(8):
                pos_ = (hd_ % 4) * 2 + hd_ // 4
                P.dma("pool", lambda e, hd_=hd_, pos_=pos_: e.dma_start(out=wq[:, :, pos_ * 64:(pos_ + 1) * 64], in_=wv(w_in, hd_ * 64, hd_ * 64 + 64)), [], [B_c2])
            P.dma("pool", lambda e: e.dma_start(out=wao[:], in_=w_att_out.rearrange("(h p) n -> p h n", p=64)), [], [B_c2])
            P.dma("sp", lambda e: e.dma_start(out=cosb[:], in_=cos_own[:, :, :]), [], [B_c2])
            P.dma("sp", lambda e: e.dma_start(out=sinb[:], in_=sin_own[:, :, :]), [], [B_c2])
            P.dma("sp", lambda e: e.dma_start(out=gq_bc[:], in_=g_q.partition_broadcast(128)), [], [B_c2])
            P.op("dve", lambda e: e.tensor_scalar(out=gq_bc[:], in0=gq_bc[:], scalar1=0.125, scalar2=None, op0=ALU.mult), [B_c2], [B_c2])
            P.op("pool", lambda e: e.memset(ones65[:], 1.0), [], [B_c2])
            P.barrier()
            st = norm_state(ph, "p2", nsets=4, nxb=4)
            rws = Pool(rope_state(ph, "p2r", 512, nsets=3).items)
            xts = Rot([(sb(ph, "p2x%d" % i, [128, 1024]), Buf("x")) for i in range(4)])
            hTs = Rot([(sb(ph, "p2h%d" % i, [128, 8, 128], BF16), Buf("h")) for i in range(4)])
            qrs = Rot([(sb(ph, "p2qr%d" % i, [128, 8, 32, 2], BF16), Buf("qr")) for i in range(4)])
            Ps = Rot([(sb(ph, "p2P%d" % i, [128, 2, 512], BF16), Buf("P")) for i in range(3)])
            OnTs = Rot([(sb(ph, "p2OnT%d" % i, [64, 8, 512], BF16), [Buf("OnT") for _ in range(8)]) for i in range(2)])
            ysbs = Rot([(sb(ph, "p2ysb%d" % i, [128, 4, 512]), [Buf("ysb") for _ in range(4)]) for i in range(1)])
            Sps = Rot([(ps(ph, "p2S%d" % i, [128, 2, 512]), Buf("S", True)) for i in range(3)])
            Ops, bO = ps(ph, "p2O", [128, 2, 512]), [Buf("O0", True), Buf("O1", True)]
            LA = int(_os0.environ.get('LA', 2))

            sbank = Pool([(Sps.items[i][0][:, bnk, :], Buf("sbank", True)) for i in range(3) for bnk in range(2)])
            Osbs = Rot([(sb(ph, "p2Osc%d" % i, [65, 512]), Buf("Osb")) for i in range(4)])
            rrs = Rot([(sb(ph, "p2rrc%d" % i, [65, 512]), Buf("rr")) for i in range(4)])

            qT_all = sb(ph, "qT_all", [128, 4, OWN], BF16)
            bqTt = [Buf("qTt") for _ in range(32)]
            print("sbuf bytes remaining in 2a:", nc.sbuf_bytes_remaining)

            def q_tile(tile):
                xt, bx = xts.next()
                P.dma("sp", lambda e: e.dma_start(out=xt[:], in_=xown[tile * 128:(tile + 1) * 128, :]), [], [bx])
                yield
                hT, bh = hTs.next()
                yield from norm_T_gen(st, xt[:], bx, gmixT, hT[:], bh, None, None, pool=sbank)
                hq = yield from acq(sbank)
                qps, bqp = hq
                P.op("pe", [(lambda e, c=c: e.matmul(qps, hT[:, c, :], wq[:, c, :], start=(c == 0), stop=(c == 7)))
                            for c in range(8)], [bh, B_c2], [bqp])
                yield
                qr, bqr = qrs.next()
                yield from head_norm_rope_gen(rws, qps, bqp, 8, gq_bc, cosb, sinb, qr, bqr, tile)
                sbank.release(hq)
                ht = yield from acq(sbank)
                tpv = bfview(ht[0])
                qr3 = qr[:].rearrange("p h i t -> p (h i t)")
                P.op("pe", [(lambda e, g=g: e.transpose(tpv[:, g, :], qr3[:, g * 128:(g + 1) * 128], ident[:])) for g in range(4)],
                     [bqr], [ht[1]])
                yield
                P.op("act", lambda e: e.activation(out=qT_all[:, :, tile * 128:(tile + 1) * 128], in_=tpv[:, 0:4, :], func=AF.Copy),
                     [ht[1]], [bqTt[tile]])
                sbank.release(ht)
                yield

            spool = Pool(list(Sps.items))

            def epi_unit(item, bnk):
                (Osb, bOsb, rr, brr, OnT, bOn, head) = item
                hS = spool.acquire()
                assert hS is not None
                mS, bmS = hS
                P.op("pe", lambda e: e.matmul(mS[0:64, bnk, :], ones65[64:65, 0:64], rr[64:65, :], start=True, stop=True), [brr, B_c2], [bmS])
                P.op("dve", lambda e: e.tensor_tensor(out=OnT[:, head, :], in0=Osb[0:64, :], in1=mS[0:64, bnk, :], op=ALU.mult),
                     [bOsb, bmS], [bOn[head]])
                spool.release(hS)

            def y_unit(gi, dc, OnT, bOn, ysb, bys):
                hS = spool.acquire()
                assert hS is not None
                mS, bmS = hS
                P.op("pe", [(lambda e, h=h: e.matmul(mS[:, 0, :], wao[:, h, dc * 128:(dc + 1) * 128], OnT[:, h, :], start=(h == 0), stop=(h == 7)))
                            for h in range(8)], bOn + [B_c2], [bmS])
                P.op("dve", lambda e: e.tensor_copy(out=ysb[:, dc % 4, :], in_=mS[:, 0, :]), [bmS], [bys[dc % 4]])
                spool.release(hS)
                if dc % 4 == 3:
                    P.dma("sp", lambda e: e.dma_start(out=yatt_d[gi][:, dc - 3:dc + 1, :], in_=ysb[:]), bys, [])

            run_pipeline((q_tile(t) for t in range(32)), 4)
            P.barrier()
            for ex_ in range(NE):
                P.dma_bg("pool", lambda e, ex_=ex_: e.dma_start(out=wcat_bf[ex_ * 128:(ex_ + 1) * 128, :], in_=wcat[ex_ * 128:(ex_ + 1) * 128, :]), bgsem, 16 * NE, B_wbf)
            units = []
            for gi in range(8):
                OnT, bOn = OnTs.next()
                bqT = bqTt[gi * 4:gi * 4 + 4]
                for g in range(4):
                    pend = []

                    def try_issue(kt, g=g, gi=gi, bqT=bqT, pend=pend):
                        hS = spool.acquire()
                        if hS is None:
                            return False
                        S, bS = hS
                        P.op("pe", [lambda e: e.matmul(S[:, 0, :], kT_all[0:64, kt * 128:(kt + 1) * 128], qT_all[0:64, g, gi * 512:(gi + 1) * 512], start=True, stop=True),
                                    lambda e: e.matmul(S[:, 1, :], kT_all[64:128, kt * 128:(kt + 1) * 128], qT_all[64:128, g, gi * 512:(gi + 1) * 512], start=True, stop=True)],
                             bqT, [bS])
                        pend.append(hS)
                        return True
                    nxt = 0
                    for kt in range(64):
                        while nxt < 64 and nxt <= kt + LA and try_issue(nxt):
                            nxt += 1
                        hS = pend.pop(0)
                        S, bS = hS
                        Pt, bP = Ps.next()
                        P.op("act", lambda e, S=S, Pt=Pt: e.activation(out=Pt[:], in_=S[:], func=AF.Exp), [bS], [bP])
                        spool.release(hS)
                        if units and kt >= 3 and kt % 3 == 0:
                            units.pop(0)()
                        P.op("pe", [lambda e, Pt=Pt, kt=kt: e.matmul(Ops[0:65, 0, :], V_all[:, kt, 0, :], Pt[:, 0, :], start=(kt == 0), stop=(kt == 63)),
                                    lambda e, Pt=Pt, kt=kt: e.matmul(Ops[0:65, 1, :], V_all[:, kt, 1, :], Pt[:, 1, :], start=(kt == 0), stop=(kt == 63))],
                             [bP], bO)
                    for hh in range(2):
                        head = g + 4 * hh
                        Osb, bOsb = Osbs.next()
                        rr, brr = rrs.next()
                        P.op("act", lambda e, Osb=Osb, hh=hh: e.activation(out=Osb[:], in_=Ops[0:65, hh, :], func=AF.Copy), [bO[hh]], [bOsb])
                        P.op("dve", lambda e, Osb=Osb, rr=rr: e.reciprocal(out=rr[64:65, :], in_=Osb[64:65, :]), [bOsb], [brr])
                        item = (Osb, bOsb, rr, brr, OnT, bOn, head)
                        units.append(lambda item=item, hh=hh: epi_unit(item, hh))
                ysb, bys = ysbs.next()
                for dc in range(8):
                    units.append(lambda gi=gi, dc=dc, OnT=OnT, bOn=bOn, ysb=ysb, bys=bys: y_unit(gi, dc, OnT, bOn, ysb, bys))
            while units:
                units.pop(0)()
            P.flush()

        attn_stack.close()
        if dbg == 2:
            dy = nc.dram_tensor("dbg_yatt", [8, 128, 8, 512], F32, kind="ExternalOutput").ap()
            P.dma("sp", lambda e: e.dma_start(out=dy.rearrange("a p c t -> (a p) (c t)"), in_=yatt_d.rearrange("a p c t -> (a p) (c t)")), [], [])
            P.flush()
            return nc

        with contextlib.ExitStack() as ph:
            wckv = sb(ph, "wckv", [128, 8, 1024], BF16)
            gmemT = sb(ph, "gmemT", [128, 8])
            memT = sb(ph, "memT", [128, 8, 256], BF16)
            B_cm = Buf("cm")
            P.dma("pool", lambda e: e.dma_start(out=wckv[:], in_=wv(w_ckv, 0, 1024)), [], [B_cm])
            P.dma("sp", lambda e: e.dma_start(out=gmemT[:], in_=gT_view(g_mem), allow_slow_non_contiguous=True), [], [B_cm])
            st = norm_state(ph, "pm")
            xts = Rot([(sb(ph, "pmx%d" % i, [128, 1024]), Buf("x")) for i in range(2)])
            tp, btp = ps(ph, "pmtp", [128, 8, 128], BF16), Buf("tp", True)
            mps = Rot([(ps(ph, "pmps%d" % i, [128, 512]), Buf("mps", True)) for i in range(2)])
            bmemT = [Buf("memT0"), Buf("memT1")]
            for mt in range(2):
                xt, bx = xts.next()
                P.dma("sp", lambda e, xt=xt, mt=mt: e.dma_start(out=xt[:], in_=memd[mt * 128:(mt + 1) * 128, :]), [], [bx])
                norm_T(st, xt[:], bx, gmemT, memT[:, :, mt * 128:(mt + 1) * 128], bmemT[mt], tp, btp)
            for h in range(4):
                mp, bmp = mps.next()
                P.op("pe", [(lambda e, c=c, h=h, mp=mp: e.matmul(mp[:, 0:256], wckv[:, c, h * 128:(h + 1) * 128], memT[:, c, :], start=(c == 0), stop=(c == 7)))
                            for c in range(8)], bmemT + [B_cm], [bmp])
                P.op("act", lambda e, h=h, mp=mp: e.activation(out=kcT[:, h, :], in_=mp[:, 0:256], func=AF.Copy), [bmp], [])
            for mt in range(2):
                mp, bmp = mps.next()
                P.op("pe", [(lambda e, c=c, mt=mt, mp=mp: e.matmul(mp[:], memT[:, c, mt * 128:(mt + 1) * 128], wckv[:, c, 512:1024], start=(c == 0), stop=(c == 7)))
                            for c in range(8)], bmemT + [B_cm], [bmp])
                P.op("act", lambda e, mt=mt, mp=mp: e.activation(out=Vc[:, mt, :], in_=mp[:], func=AF.Copy), [bmp], [])
            P.flush()

        with contextlib.ExitStack() as ph:
            wsv = sb(ph, "wsv", [128, 8, 512], BF16)
            wsu = sb(ph, "wsu", [128, 8, 512], BF16)
            wga = sb(ph, "wga", [128, 8, 1024], BF16)
            wgb = sb(ph, "wgb", [128, 8, 1024], BF16)
            wso = sb(ph, "wso", [128, 4, 1024], BF16)
            wo = sb(ph, "wo", [128, 8, 1024], BF16)
            wcq = sb(ph, "wcq", [128, 8, 512], BF16)
            wco = sb(ph, "wco", [128, 4, 1024], BF16)
            gcrT = sb(ph, "gcrT", [128, 8])
            lng_bc = sb(ph, "lng_bc", [128, 512])
            lnb_bc = sb(ph, "lnb_bc", [128, 512])
            bs_bc = sb(ph, "bs_bc", [128, 4, 128])
            wsraw = sb(ph, "wsraw", [128, 4, 128], BF16)
            WsT = sb(ph, "WsT", [128, 4, 128], BF16)
            B_c = Buf("c2b")
            B_ws = Buf("ws")
            P.dma("pool", lambda e: e.dma_start(out=wsu[:], in_=wv(w_in, 768, 1280)), [], [B_c])
            P.dma("pool", lambda e: e.dma_start(out=wsv[:], in_=wv(w_in, 1280, 1792)), [], [B_c])
            P.dma("pool", lambda e: e.dma_start(out=wga[:], in_=wv(w_in, 1792, 2816)), [], [B_c])
            P.dma("pool", lambda e: e.dma_start(out=wgb[:], in_=wv(w_in, 2816, 3840)), [], [B_c])
            P.dma("pool", lambda e: e.dma_start(out=wso[:], in_=wv(w_sgu_out, 0, 1024)), [], [B_c])
            P.dma("pool", lambda e: e.dma_start(out=wo[:], in_=wv(w_out, 0, 1024)), [], [B_c])
            P.dma("pool", lambda e: e.dma_start(out=wcq[:], in_=wv(w_cq, 0, 512)), [], [B_c])
            P.dma("pool", lambda e: e.dma_start(out=wco[:], in_=wv(w_co, 0, 1024)), [], [B_c])
            P.dma("pool", lambda e: e.dma_start(out=wsraw[:], in_=w_s.rearrange("g p q -> p g q")), [], [B_ws])
            P.dma("sp", lambda e: e.dma_start(out=gcrT[:], in_=gT_view(g_cross), allow_slow_non_contiguous=True), [], [B_c])
            P.dma("sp", lambda e: e.dma_start(out=lng_bc[:], in_=sgu_ln_g.partition_broadcast(128)), [], [B_c])
            P.dma("sp", lambda e: e.dma_start(out=lnb_bc[:], in_=sgu_ln_b.partition_broadcast(128)), [], [B_c])
            P.dma("sp", lambda e: e.dma_start(out=bs_bc[:].rearrange("p g q -> p (g q)"), in_=b_s.rearrange("g q -> (g q)").partition_broadcast(128)), [], [B_c])
            P.barrier()
            st = norm_state(ph, "p3", nsets=4, nxb=4)
            bpool = Pool([(ps(ph, "p3g%d" % i, [128, 512]), Buf("g", True)) for i in range(8)])
            hb0 = bpool.acquire()
            tpb0 = bfview(hb0[0][:])
            P.op("pe", [(lambda e, g=g: e.transpose(tpb0[:, g, :], wsraw[:, g, :], ident[:])) for g in range(4)], [B_ws], [hb0[1]])
            P.op("act", lambda e: e.activation(out=WsT[:], in_=tpb0[:, 0:4, :], func=AF.Copy), [hb0[1]], [B_c])
            bpool.release(hb0)
            P.barrier()

            xg = [(sb(ph, "p3x%d" % i, [128, 1024]), Buf("x")) for i in range(4)]
            hTg, bhT = sb(ph, "p3hT", [128, 8, 512], BF16), [Buf("hT") for _ in range(4)]
            hcT, bhc = hTg, bhT
            NA = 3
            asets = Pool([dict(gv=sb(ph, "p3gv%d" % i, [128, 512]), vn=sb(ph, "p3vn%d" % i, [128, 512], BF16), lst=sb(ph, "p3lst%d" % i, [128, 16]),
                               bgv=Buf("gv"), bvn=Buf("vn"), bl=[Buf("l") for _ in range(8)]) for i in range(NA)])
            vmb, bvmb = sb(ph, "p3vmb", [128, 4, 512]), [Buf("vmb") for _ in range(4)]
            gus = Pool([(sb(ph, "p3gu%d" % i, [128, 512]), Buf("gu")) for i in range(2)])
            sguT, bsg = sb(ph, "p3sguT", [128, 4, 512], BF16), [Buf("sg") for _ in range(4)]
            csets = Pool([dict(ya=sb(ph, "p3ya%d" % i, [128, 512]), sa=sb(ph, "p3sa%d" % i, [128, 512]), sb=sb(ph, "p3sb%d" % i, [128, 512]),
                               bya=Buf("ya"), bsa=Buf("sa"), bsb=Buf("sb")) for i in range(2)])
            mrg, bmrg = sb(ph, "p3mrg", [128, 8, 512], BF16), [Buf("mrg") for _ in range(8)]
            qcT, bqc = sb(ph, "p3qcT", [128, 4, 512], BF16), [Buf("qc") for _ in range(4)]
            gsets = Pool([dict(cst=sb(ph, "p3cst%d" % i, [128, 16]), pc=sb(ph, "p3pc%d" % i, [128, 4, 256]), pn=sb(ph, "p3pn%d" % i, [128, 4, 256], BF16),
                               pnT=sb(ph, "p3pnT%d" % i, [128, 8, 128], BF16), ocT=sb(ph, "p3ocT%d" % i, [128, 4, 128], BF16),
                               bc=[Buf("c") for _ in range(4)], bpc=Buf("pc"), bpn=Buf("pn"), bpnT=Buf("pnT"), boc=Buf("oc")) for i in range(2)])
            print("sbuf bytes remaining in 2b:", nc.sbuf_bytes_remaining)

            def a_tile(gi, j):
                tile = gi * 4 + j
                xt, bx = xg[j]
                P.dma("sp", lambda e: e.dma_start(out=xt[:], in_=xown[tile * 128:(tile + 1) * 128, :]), [], [bx])
                yield
                yield from norm_T_gen(st, xt[:], bx, gmixT, hTg[:, :, j * 128:(j + 1) * 128], bhT[j], None, None, pool=bpool)
                A = yield from acq(asets)
                gv, vn, lst, bgv, bvn, bl = A["gv"], A["vn"], A["lst"], A["bgv"], A["bvn"], A["bl"]
                h0 = yield from acq(bpool)
                g0, bg0 = h0
                P.op("pe", [(lambda e, c=c: e.matmul(g0[:], hTg[:, c, j * 128:(j + 1) * 128], wsv[:, c, :], start=(c == 0), stop=(c == 7)))
                            for c in range(8)], [bhT[j], B_c], [bg0])
                yield
                P.op("act", lambda e: e.activation(out=gv[:], in_=g0[:], func=AF.Gelu_apprx_tanh), [bg0], [bgv])
                bpool.release(h0)
                yield
                P.op("dve", lambda e: e.tensor_reduce(out=lst[:, 0:1], in_=gv[:], axis=AX.X, op=ALU.add), [bgv], [bl[0]])
                yield
                P.op("act", lambda e: e.activation(out=vn[:], in_=gv[:], func=AF.Square, accum_out=lst[:, 1:2]), [bgv], [bvn, bl[1]])
                yield
                P.op("dve", lambda e: e.tensor_scalar(out=lst[:, 2:3], in0=lst[:, 0:1], scalar1=1.0 / 512, scalar2=None, op0=ALU.mult), [bl[0]], [bl[2]])
                yield
                P.op("dve", lambda e: e.tensor_tensor(out=lst[:, 3:4], in0=lst[:, 2:3], in1=lst[:, 2:3], op=ALU.mult), [bl[2]], [bl[3]])
                yield
                P.op("dve", lambda e: e.scalar_tensor_tensor(out=lst[:, 4:5], in0=lst[:, 1:2], scalar=1.0 / 512, in1=lst[:, 3:4], op0=ALU.mult, op1=ALU.subtract),
                     [bl[1], bl[3]], [bl[4]])
                yield
                P.op("dve", lambda e: e.tensor_scalar(out=lst[:, 5:6], in0=lst[:, 4:5], scalar1=EPS, scalar2=None, op0=ALU.add), [bl[4]], [bl[5]])
                yield
                P.op("act", lambda e: e.activation(out=lst[:, 6:7], in_=lst[:, 5:6], func=AF.Ln), [bl[5]], [bl[6]])
                yield
                P.op("act", lambda e: e.activation(out=lst[:, 7:8], in_=lst[:, 6:7], func=AF.Exp, scale=-0.5), [bl[6]], [bl[7]])
                yield
                P.op("dve", lambda e: e.tensor_scalar(out=gv[:], in0=gv[:], scalar1=lst[:, 2:3], scalar2=lst[:, 7:8], op0=ALU.subtract, op1=ALU.mult),
                     [bgv, bl[2], bl[7]], [bgv])
                yield
                P.op("dve", lambda e: e.tensor_tensor(out=gv[:], in0=gv[:], in1=lng_bc[:], op=ALU.mult), [bgv, B_c], [bgv])
                yield
                P.op("dve", lambda e: e.tensor_tensor(out=vn[:], in0=gv[:], in1=lnb_bc[:], op=ALU.add), [bgv, B_c], [bvn])
                yield
                h1 = yield from acq(bpool)
                g1, bg1 = h1
                P.op("pe", [(lambda e, g=g: e.matmul(g1[:, g * 128:(g + 1) * 128], vn[:, g * 128:(g + 1) * 128], WsT[:, g, :], start=True, stop=True))
                            for g in range(4)], [bvn, B_c], [bg1])
                yield
                P.op("dve", lambda e: e.tensor_tensor(out=vmb[:, :, j * 128:(j + 1) * 128], in0=g1[:].rearrange("p (g q) -> p g q", g=4),
                                                      in1=bs_bc[:], op=ALU.add), [bg1, B_c], [bvmb[j]])
                bpool.release(h1)
                asets.release(A)
                yield

            def b_chunk(g):
                h0 = yield from acq(bpool)
                g0, bg0 = h0
                P.op("pe", [(lambda e, c=c: e.matmul(g0[:], wsu[:, c, g * 128:(g + 1) * 128], hTg[:, c, :], start=(c == 0), stop=(c == 7)))
                            for c in range(8)], bhT + [B_c], [bg0])
                yield
                G = yield from acq(gus)
                gu, bgu = G
                P.op("act", lambda e: e.activation(out=gu[:], in_=g0[:], func=AF.Gelu_apprx_tanh), [bg0], [bgu])
                bpool.release(h0)
                yield
                P.op("dve", lambda e: e.tensor_tensor(out=sguT[:, g, :], in0=gu[:], in1=vmb[:, g, :], op=ALU.mult), [bgu] + bvmb, [bsg[g]])
                gus.release(G)
                yield

            def c_chunk(gi, dc):
                C = yield from acq(csets)
                ya, sa, sbt, bya, bsa, bsb = C["ya"], C["sa"], C["sb"], C["bya"], C["bsa"], C["bsb"]
                P.dma("sp", lambda e: e.dma_start(out=ya[:], in_=yatt_d[gi][:, dc, :]), [], [bya])
                yield
                h0 = yield from acq(bpool)
                ga_ps, bga = h0
                P.op("pe", [(lambda e, c=c: e.matmul(ga_ps[:], wga[:, c, dc * 128:(dc + 1) * 128], hTg[:, c, :], start=(c == 0), stop=(c == 7)))
                            for c in range(8)], bhT + [B_c], [bga])
                yield
                P.op("act", lambda e: e.activation(out=sa[:], in_=ga_ps[:], func=AF.Sigmoid), [bga], [bsa])
                bpool.release(h0)
                yield
                h1 = yield from acq(bpool)
                gb_ps, bgb = h1
                P.op("pe", [(lambda e, c=c: e.matmul(gb_ps[:], wgb[:, c, dc * 128:(dc + 1) * 128], hTg[:, c, :], start=(c == 0), stop=(c == 7)))
                            for c in range(8)], bhT + [B_c], [bgb])
                yield
                P.op("act", lambda e: e.activation(out=sbt[:], in_=gb_ps[:], func=AF.Sigmoid), [bgb], [bsb])
                bpool.release(h1)
                yield
                h2 = yield from acq(bpool)
                ys_ps, bysp = h2
                P.op("pe", [(lambda e, g=g: e.matmul(ys_ps[:], wso[:, g, dc * 128:(dc + 1) * 128], sguT[:, g, :], start=(g == 0), stop=(g == 3)))
                            for g in range(4)], bsg + [B_c], [bysp])
                yield
                P.op("dve", lambda e: e.tensor_tensor(out=sa[:], in0=sa[:], in1=ya[:], op=ALU.mult), [bsa, bya], [bsa])
                yield
                P.op("dve", lambda e: e.tensor_tensor(out=sbt[:], in0=sbt[:], in1=ys_ps[:], op=ALU.mult), [bsb, bysp], [bsb])
                bpool.release(h2)
                yield
                P.op("dve", lambda e: e.tensor_tensor(out=mrg[:, dc, :], in0=sa[:], in1=sbt[:], op=ALU.add), [bsa, bsb], [bmrg[dc]])
                csets.release(C)
                yield

            def d_part(j, nb):
                xt, bx = xg[j]
                h0 = yield from acq(bpool)
                o_ps, bo = h0
                P.op("pe", [(lambda e, dc=dc: e.matmul(o_ps[:], mrg[:, dc, j * 128:(j + 1) * 128], wo[:, dc, nb * 512:(nb + 1) * 512],
                                                      start=(dc == 0), stop=(dc == 7))) for dc in range(8)], bmrg + [B_c], [bo])
                yield
                P.op("dve", lambda e: e.tensor_tensor(out=xt[:, nb * 512:(nb + 1) * 512], in0=xt[:, nb * 512:(nb + 1) * 512], in1=o_ps[:], op=ALU.add),
                     [bo, bx], [bx])
                bpool.release(h0)
                yield

            def e_tile(j):
                xt, bx = xg[j]
                yield from norm_T_gen(st, xt[:], bx, gcrT, hcT[:, :, j * 128:(j + 1) * 128], bhc[j], None, None, pool=bpool)

            def f_head(h):
                h0 = yield from acq(bpool)
                q_ps, bq = h0
                P.op("pe", [(lambda e, c=c: e.matmul(q_ps[:], wcq[:, c, h * 128:(h + 1) * 128], hcT[:, c, :], start=(c == 0), stop=(c == 7)))
                            for c in range(8)], bhc + [B_c], [bq])
                yield
                P.op("act", lambda e: e.activation(out=qcT[:, h, :], in_=q_ps[:], func=AF.Copy, scale=float(128 ** -0.5)), [bq], [bqc[h]])
                bpool.release(h0)
                yield

            def g_tile(gi, j):
                tile = gi * 4 + j
                xt, bx = xg[j]
                Gs = yield from acq(gsets)
                cst, pc, pn, pnT, ocT = Gs["cst"], Gs["pc"], Gs["pn"], Gs["pnT"], Gs["ocT"]
                bc, bpc, bpn, bpnT, boc = Gs["bc"], Gs["bpc"], Gs["bpn"], Gs["bpnT"], Gs["boc"]
                hs0 = yield from acq(bpool)
                hs1 = yield from acq(bpool)
                scs = [hs0[0], hs1[0]]
                bscs = [hs0[1], hs1[1]]
                for half in range(2):
                    P.op("pe", [(lambda e, hh=hh, half=half: e.matmul(scs[half][:, hh * 256:(hh + 1) * 256], qcT[:, half * 2 + hh, j * 128:(j + 1) * 128], kcT[:, half * 2 + hh, :],
                                                                       start=True, stop=True)) for hh in range(2)], bqc, [bscs[half]])
                    yield
                for half in range(2):
                    P.op("dve", lambda e, half=half: e.tensor_reduce(out=cst[:, half * 2:half * 2 + 2], in_=scs[half][:].rearrange("p (h m) -> p h m", h=2), axis=AX.X, op=ALU.max),
                         [bscs[half]], [bc[0]])
                    yield
                P.op("dve", lambda e: e.tensor_scalar(out=cst[:, 4:8], in0=cst[:, 0:4], scalar1=-1.0, scalar2=None, op0=ALU.mult), [bc[0]], [bc[1]])
                yield
                for half in range(2):
                    P.op("act", [(lambda e, hh=hh, half=half: e.activation(out=pc[:, half * 2 + hh, :], in_=scs[half][:, hh * 256:(hh + 1) * 256], func=AF.Exp,
                                                                          bias=cst[:, 4 + half * 2 + hh:5 + half * 2 + hh], accum_out=cst[:, 8 + half * 2 + hh:9 + half * 2 + hh]))
                                 for hh in range(2)], [bscs[half], bc[1]], [bpc, bc[2]])
                    yield
                bpool.release(hs0)
                bpool.release(hs1)
                P.op("dve", lambda e: e.reciprocal(out=cst[:, 12:16], in_=cst[:, 8:12]), [bc[2]], [bc[3]])
                yield
                P.op("dve", lambda e: e.tensor_tensor(out=pn[:], in0=pc[:], in1=cst[:, 12:16].unsqueeze(2).broadcast_to([128, 4, 256]), op=ALU.mult),
                     [bpc, bc[3]], [bpn])
                yield
                ht = yield from acq(bpool)
                tpv = bfview(ht[0][:])
                P.op("pe", [(lambda e, k=k: e.transpose(tpv[:, k, :], pn[:, k // 2, (k % 2) * 128:(k % 2 + 1) * 128], ident[:])) for k in range(8)], [bpn], [ht[1]])
                yield
                P.op("act", lambda e: e.activation(out=pnT[:], in_=tpv, func=AF.Copy), [ht[1]], [bpnT])
                bpool.release(ht)
                yield
                ho = yield from acq(bpool)
                oc_ps, bocp = ho
                fl = []
                for h in range(4):
                    for mt in range(2):
                        fl.append(lambda e, h=h, mt=mt: e.matmul(oc_ps[:, h * 128:(h + 1) * 128], Vc[:, mt, h * 128:(h + 1) * 128], pnT[:, h * 2 + mt, :],
                                                                 start=(mt == 0), stop=(mt == 1)))
                P.op("pe", fl, [bpnT], [bocp])
                yield
                P.op("act", lambda e: e.activation(out=ocT[:], in_=oc_ps[:].rearrange("p (h t) -> p h t", h=4), func=AF.Copy), [bocp], [boc])
                bpool.release(ho)
                yield
                for nb in range(2):
                    hy = yield from acq(bpool)
                    y_ps, byp = hy
                    P.op("pe", [(lambda e, h=h, nb=nb, y_ps=y_ps: e.matmul(y_ps[:], ocT[:, h, :], wco[:, h, nb * 512:(nb + 1) * 512], start=(h == 0), stop=(h == 3)))
                                for h in range(4)], [boc, B_c], [byp])
                    yield
                    P.op("dve", lambda e, nb=nb, y_ps=y_ps: e.tensor_tensor(out=xt[:, nb * 512:(nb + 1) * 512], in0=xt[:, nb * 512:(nb + 1) * 512], in1=y_ps[:], op=ALU.add),
                         [byp, bx], [bx])
                    bpool.release(hy)
                    yield
                gsets.release(Gs)
                P.dma("sp", lambda e: e.dma_start(out=x2_d[tile * 128:(tile + 1) * 128, :], in_=xt[:]), [bx], [])
                yield

            run_pipeline((a_tile(0, j) for j in range(4)), 4)
            for gi in range(8):
                run_pipeline(iter([b_chunk(g) for g in range(4)] + [c_chunk(gi, dc) for dc in range(8)]), 4)
                de = []
                for j in range(4):
                    de += [d_part(j, 0), d_part(j, 1), e_tile(j)]
                run_pipeline(iter(de), 4)
                run_pipeline((f_head(h) for h in range(4)), 4)
                ga_ = [g_tile(gi, j) for j in range(4)]
                if gi + 1 < 8:
                    ga_ += [a_tile(gi + 1, j) for j in range(4)]
                run_pipeline(iter(ga_), 4)
            P.flush()

        if dbg == 3:
            dx = nc.dram_tensor("dbg_x2", [OWN, D], F32, kind="ExternalOutput").ap()
            P.dma("sp", lambda e: e.dma_start(out=dx[:, :], in_=x2_d[:, :]), [], [])
            P.flush()
            return nc

        NI = 32
        IT = 512
        NS = NI * IT
        hs_d = nc.dram_tensor("hs_scr", [NS, D], BF16, kind="Internal").ap()
        ys_d = nc.dram_tensor("ys_scr", [NS, D], F32, kind="Internal").ap()
        trid = din("tri", [128, 128])
        iotad = din("iota32", [128, 32])
        thrd = din("thr8", [128, 8])
        with contextlib.ExitStack() as ph:
            gmoT = sb(ph, "gmoT", [128, 8])
            wr = sb(ph, "wr", [128, 8, 20], BF16)
            br_bc = sb(ph, "br_bc", [128, 20])
            gfin_bc = sb(ph, "gfin_bc", [128, 1024])
            oh1 = sb(ph, "oh1", [128, 32, 16])
            oh2 = sb(ph, "oh2", [128, 32, 16])
            wts = sb(ph, "wts", [128, 32, 2])
            idx1 = sb(ph, "idx1", [128, 32], mybir.dt.int32)
            idx2 = sb(ph, "idx2", [128, 32], mybir.dt.int32)
            Ei = sb(ph, "Ei", [128, 32], mybir.dt.int32)
            Widx = sb(ph, "Widx", [128, 32], mybir.dt.int32)
            pidx = sb(ph, "pidx", [128, 1])
            P.dma("sp", lambda e: e.dma_start(out=pidx[:], in_=pidxd[:, :]), [], [])
            B_c = Buf("c3")
            P.dma("sp", lambda e: e.dma_start(out=gmoT[:], in_=gT_view(g_moe), allow_slow_non_contiguous=True), [], [B_c])
            P.dma("pool", lambda e: e.dma_start(out=wr[:, :, 0:4], in_=wv(w_rg, 0, 4)), [], [B_c])
            P.dma("pool", lambda e: e.dma_start(out=wr[:, :, 4:20], in_=wv(w_re, 0, 16)), [], [B_c])
            P.dma("sp", lambda e: e.dma_start(out=br_bc[:, 0:4], in_=b_rg.partition_broadcast(128)), [], [B_c])
            P.dma("sp", lambda e: e.dma_start(out=br_bc[:, 4:20], in_=b_re.partition_broadcast(128)), [], [B_c])
            P.dma("sp", lambda e: e.dma_start(out=gfin_bc[:], in_=g_final.partition_broadcast(128)), [], [B_c])
            allbanks = [(ps(ph, "p4g%d" % i, [128, 512]), Buf("g", True)) for i in range(8)]
            bpool = Pool(allbanks)
            gen = Rot(allbanks)

            with contextlib.ExitStack() as sp1:
                hm_all = sb(sp1, "hm_all", [128, 32, 1024], BF16)
                gmo_bc = sb(sp1, "gmo_bc", [128, 1024])
                tri = sb(sp1, "tri", [128, 128], BF16)
                onesb = sb(sp1, "onesb", [128, 128], BF16)
                iota32 = sb(sp1, "iota32", [128, 32])
                thr8 = sb(sp1, "thr8", [128, 8])
                Mb = sb(sp1, "Mb", [128, 32, 16], BF16)
                pos_all = sb(sp1, "pos_all", [128, 32, 16])
                base = sb(sp1, "base", [128, 16])
                P.dma("sp", lambda e: e.dma_start(out=gmo_bc[:], in_=g_moe.partition_broadcast(128)), [], [B_c])
                P.dma("pool", lambda e: e.dma_start(out=tri[:], in_=trid[:, :]), [], [B_c])
                P.dma("sp", lambda e: e.dma_start(out=iota32[:], in_=iotad[:, :]), [], [B_c])
                P.dma("sp", lambda e: e.dma_start(out=thr8[:], in_=thrd[:, :]), [], [B_c])
                P.op("pool", lambda e: e.memset(onesb[:], 1.0), [], [B_c])
                P.op("pool", lambda e: e.memset(base[:], 0.0), [], [B_c])
                P.barrier()
                NP3 = 8
                st = norm_state(sp1, "p4", nsets=NP3, nxb=NP3)
                xts = Rot([(sb(sp1, "p4x%d" % i, [128, 1024]), Buf("x")) for i in range(NP3)])
                hTt = Rot([(sb(sp1, "p4h%d" % i, [128, 8, 128], BF16), Buf("h")) for i in range(NP3)])
                rts = Rot([(sb(sp1, "rt%d" % i, [128, 160]), [Buf("rt") for _ in range(24)]) for i in range(NP3)])
                boh = [Buf("oh%d" % i) for i in range(32)]
                bhm = [Buf("hm%d" % i) for i in range(32)]

                def moe_tile(i):
                    xt, bx = xts.next()
                    P.dma("sp", lambda e: e.dma_start(out=xt[:], in_=x2_d[i * 128:(i + 1) * 128, :]), [], [bx])
                    yield
                    hT, bh = hTt.next()
                    yield from norm_T_gen(st, xt[:], bx, gmoT, hT[:], bh, None, None, pool=bpool, tok=(hm_all[:, i, :], gmo_bc[:], B_c, bhm[i]))
                    hb = yield from acq(bpool)
                    r_ps, brp = hb
                    P.op("pe", [(lambda e, c=c: e.matmul(r_ps[:, 0:20], hT[:, c, :], wr[:, c, :], start=(c == 0), stop=(c == 7)))
                                for c in range(8)], [bh, B_c], [brp])
                    yield
                    R, brt = rts.next()
                    o1 = oh1[:, i, :]
                    o2 = oh2[:, i, :]
                    P.op("dve", lambda e: e.tensor_tensor(out=R[:, 0:20], in0=r_ps[:, 0:20], in1=br_bc[:], op=ALU.add), [brp, B_c], [brt[0]])
                    bpool.release(hb)
                    yield
                    P.op("dve", lambda e: e.tensor_reduce(out=R[:, 20:21], in_=R[:, 0:4], axis=AX.X, op=ALU.max), [brt[0]], [brt[1]])
                    yield
                    P.op("dve", lambda e: e.tensor_scalar(out=R[:, 21:22], in0=R[:, 20:21], scalar1=-1.0, scalar2=None, op0=ALU.mult), [brt[1]], [brt[2]])
                    yield
                    P.op("act", lambda e: e.activation(out=R[:, 22:26], in_=R[:, 0:4], func=AF.Exp, bias=R[:, 21:22], accum_out=R[:, 26:27]), [brt[0], brt[2]], [brt[3]])
                    yield
                    P.op("dve", lambda e: e.reciprocal(out=R[:, 27:28], in_=R[:, 26:27]), [brt[3]], [brt[4]])
                    yield
                    P.op("dve", lambda e: e.tensor_scalar(out=R[:, 28:32], in0=R[:, 0:4], scalar1=R[:, 20:21], scalar2=None, op0=ALU.is_ge), [brt[0], brt[1]], [brt[5]])
                    yield
                    P.op("dve", lambda e: e.tensor_scalar(out=R[:, 32:36], in0=R[:, 28:32], scalar1=BIG, scalar2=-BIG, op0=ALU.mult, op1=ALU.add), [brt[5]], [brt[6]])
                    yield
                    P.op("dve", lambda e: e.tensor_tensor(out=R[:, 40:56].rearrange("p (g k) -> p g k", g=4), in0=R[:, 4:20].rearrange("p (g k) -> p g k", g=4),
                                                          in1=R[:, 32:36].unsqueeze(2).broadcast_to([128, 4, 4]), op=ALU.add), [brt[0], brt[6]], [brt[7]])
                    yield
                    P.op("dve", lambda e: e.tensor_reduce(out=R[:, 56:57], in_=R[:, 40:56], axis=AX.X, op=ALU.max), [brt[7]], [brt[8]])
                    yield
                    P.op("dve", lambda e: e.tensor_scalar(out=o1, in0=R[:, 40:56], scalar1=R[:, 56:57], scalar2=None, op0=ALU.is_ge), [brt[7], brt[8]], [brt[9]])
                    yield
                    P.op("dve", lambda e: e.scalar_tensor_tensor(out=R[:, 80:96], in0=o1, scalar=-BIG, in1=R[:, 40:56], op0=ALU.mult, op1=ALU.add),
                         [brt[9], brt[7]], [brt[10]])
                    yield
                    P.op("dve", lambda e: e.tensor_reduce(out=R[:, 96:97], in_=R[:, 80:96], axis=AX.X, op=ALU.max), [brt[10]], [brt[11]])
                    yield
                    P.op("dve", lambda e: e.tensor_scalar(out=o2, in0=R[:, 80:96], scalar1=R[:, 96:97], scalar2=None, op0=ALU.is_ge), [brt[10], brt[11]], [brt[12]])
                    yield
                    P.op("dve", lambda e: e.tensor_tensor(out=R[:, 116:117], in0=R[:, 96:97], in1=R[:, 56:57], op=ALU.subtract), [brt[11], brt[8]], [brt[13]])
                    yield
                    P.op("act", lambda e: e.activation(out=R[:, 117:118], in_=R[:, 116:117], func=AF.Exp), [brt[13]], [brt[14]])
                    yield
                    P.op("dve", lambda e: e.tensor_scalar(out=R[:, 118:119], in0=R[:, 117:118], scalar1=1.0, scalar2=None, op0=ALU.add), [brt[14]], [brt[15]])
                    yield
                    P.op("dve", lambda e: e.reciprocal(out=R[:, 119:120], in_=R[:, 118:119]), [brt[15]], [brt[16]])
                    yield
                    P.op("dve", lambda e: e.tensor_tensor(out=wts[:, i, 0:1], in0=R[:, 119:120], in1=R[:, 27:28], op=ALU.mult), [brt[16], brt[4]], [brt[17]])
                    yield
                    P.op("dve", lambda e: e.tensor_tensor(out=wts[:, i, 1:2], in0=wts[:, i, 0:1], in1=R[:, 117:118], op=ALU.mult), [brt[17], brt[14]], [brt[18]])
                    yield
                    P.op("dve", lambda e: e.tensor_tensor(out=Mb[:, i, :], in0=o1, in1=o2, op=ALU.add), [brt[9], brt[12]], [boh[i]])
                    yield

                run_pipeline((moe_tile(i) for i in range(32)), NP3)
                P.barrier()
                prefb = Rot([bpool.acquire(), bpool.acquire()])
                bbase = Buf("base")
                bpos = Buf("pos")
                for i in range(32):
                    pb, bpb = prefb.next()
                    P.op("pe", [lambda e, pb=pb, i=i: e.matmul(pb[:, 0:16], tri[:], Mb[:, i, :], start=True, stop=True),
                                lambda e, pb=pb, i=i: e.matmul(pb[:, 16:32], onesb[:], Mb[:, i, :], start=True, stop=True)], [], [bpb])
                    P.op("dve", lambda e, pb=pb, i=i: e.tensor_tensor(out=pos_all[:, i, :], in0=pb[:, 0:16], in1=base[:], op=ALU.add), [bpb, bbase], [bpos])
                    P.op("dve", lambda e, pb=pb: e.tensor_tensor(out=base[:], in0=pb[:, 16:32], in1=base[:], op=ALU.add), [bpb, bbase], [bbase])
                for it_ in prefb.items:
                    bpool.release(it_)
                P.barrier()
                sm = sb(sp1, "sm", [128, 256])
                big = sb(sp1, "bigt", [128, 32, 16])
                big2 = sb(sp1, "bigt2", [128, 32, 16])
                sf = sb(sp1, "sf", [128, 64])
                bs_ = Buf("sm")

                def S1(fn, eng="dve"):
                    P.op(eng, fn, [bs_], [bs_])
                S1(lambda e: e.tensor_tensor(out=sm[:, 0:128].rearrange("p (a k) -> p a k", k=8), in0=base[:].unsqueeze(2).broadcast_to([128, 16, 8]),
                                             in1=thr8[:].unsqueeze(1).broadcast_to([128, 16, 8]), op=ALU.is_gt))
                S1(lambda e: e.tensor_reduce(out=sm[:, 128:144], in_=sm[:, 0:128].rearrange("p (a k) -> p a k", k=8), axis=AX.X, op=ALU.add))
                S1(lambda e: e.tensor_copy(out=sm[:, 144:160], in_=sm[:, 128:144]))
                cur, oth = 144, 160
                for k in (1, 2, 4, 8):
                    S1(lambda e, cur=cur, oth=oth, k=k: e.tensor_copy(out=sm[:, oth:oth + k], in_=sm[:, cur:cur + k]))
                    S1(lambda e, cur=cur, oth=oth, k=k: e.tensor_tensor(out=sm[:, oth + k:oth + 16], in0=sm[:, cur + k:cur + 16], in1=sm[:, cur:cur + 16 - k], op=ALU.add))
                    cur, oth = oth, cur
                S1(lambda e, cur=cur: e.tensor_copy(out=sm[:, 176:192], in_=sm[:, cur:cur + 16]))
                S1(lambda e: e.tensor_tensor(out=sm[:, 192:208], in0=sm[:, 176:192], in1=sm[:, 128:144], op=ALU.subtract))
                S1(lambda e: e.tensor_scalar(out=sm[:, 192:208], in0=sm[:, 192:208], scalar1=float(IT), scalar2=None, op0=ALU.mult))
                S1(lambda e: e.memset(sm[:, 208:240], 0.0))
                for ex in range(NE):
                    S1(lambda e, ex=ex: e.scalar_tensor_tensor(out=sm[:, 208:240], in0=iota32[:], scalar=sm[:, 176 + ex:177 + ex], in1=sm[:, 208:240], op0=ALU.is_ge, op1=ALU.add))
                S1(lambda e: e.tensor_scalar(out=sm[:, 208:240], in0=sm[:, 208:240], scalar1=float(NE - 1), scalar2=None, op0=ALU.min))
                S1(lambda e: e.tensor_copy(out=Ei[:], in_=sm[:, 208:240]))
                S1(lambda e: e.tensor_scalar(out=sf[:, 0:32], in0=sm[:, 208:240], scalar1=128.0, scalar2=pidx[:, 0:1], op0=ALU.mult, op1=ALU.add))
                S1(lambda e: e.tensor_copy(out=Widx[:], in_=sf[:, 0:32]))
                S1(lambda e: e.tensor_tensor(out=big[:], in0=pos_all[:], in1=sm[:, 192:208].unsqueeze(1).broadcast_to([128, 32, 16]), op=ALU.add))
                S1(lambda e: e.tensor_tensor(out=big2[:], in0=big[:], in1=oh1[:], op=ALU.mult))
                S1(lambda e: e.tensor_reduce(out=sf[:, 0:32], in_=big2[:], axis=AX.X, op=ALU.add))
                S1(lambda e: e.tensor_tensor(out=big2[:], in0=big[:], in1=oh2[:], op=ALU.mult))
                S1(lambda e: e.tensor_reduce(out=sf[:, 32:64], in_=big2[:], axis=AX.X, op=ALU.add))
                S1(lambda e: e.tensor_copy(out=idx1[:], in_=sf[:, 0:32]))
                S1(lambda e: e.tensor_copy(out=idx2[:], in_=sf[:, 32:64]))
                P.barrier()
                for i in range(int(_os0.environ.get("NSCAT", 32))):
                    for ixt in (idx1, idx2):
                        P.dma("pool", lambda e, i=i, ixt=ixt: e.indirect_dma_start(out=hs_d[:, :], out_offset=bass.IndirectOffsetOnAxis(ap=ixt[:, i:i + 1], axis=0),
                                                                                   in_=hm_all[:, i, :], in_offset=None), [], [])
                P.barrier()

            if dbg == 4:
                d1 = nc.dram_tensor("dbg_idx", [128, 64], mybir.dt.int32, kind="ExternalOutput").ap()
                d2 = nc.dram_tensor("dbg_E", [128, 32], mybir.dt.int32, kind="ExternalOutput").ap()
                d3 = nc.dram_tensor("dbg_wts", [128, 64], F32, kind="ExternalOutput").ap()
                P.dma("sp", lambda e: e.dma_start(out=d1[:, 0:32], in_=idx1[:]), [], [])
                P.dma("sp", lambda e: e.dma_start(out=d1[:, 32:64], in_=idx2[:]), [], [])
                P.dma("sp", lambda e: e.dma_start(out=d2[:, :], in_=Ei[:]), [], [])
                P.dma("sp", lambda e: e.dma_start(out=d3[:, :], in_=wts[:].rearrange("p a b -> p (a b)")), [], [])
                P.flush()
                return nc

            with contextlib.ExitStack() as sp2:
                Wb = [(sb(sp2, "Wb%d" % i, [128, 12288], BF16), Buf("Wb")) for i in range(2)]
                hss = Rot([(sb(sp2, "hs%d" % i, [128, 4, 1024], BF16), Buf("hs")) for i in range(2)])
                hsTs = Rot([(sb(sp2, "hsT%d" % i, [128, 8, 512], BF16), [Buf("hsT") for _ in range(4)]) for i in range(2)])
                sgs = Rot([(sb(sp2, "sg%d" % i, [128, 512]), Buf("sg")) for i in range(2)])
                heTs = Rot([(sb(sp2, "heT%d" % i, [128, 4, 512], BF16), [Buf("he") for _ in range(4)]) for i in range(2)])
                yss = Rot([(sb(sp2, "ys%d" % i, [128, 1024]), Buf("ys")) for i in range(3)])

                def load_w(w):
                    wb, bwb = Wb[w % 2]
                    P.dma("pool", lambda e: e.indirect_dma_start(out=wb[:, :], out_offset=None, in_=wcat_bf[:, :],
                                                                 in_offset=bass.IndirectOffsetOnAxis(ap=Widx[:, w:w + 1], axis=0)), [B_wbf], [bwb])

                def item_T(w):
                    hs, bhs = hss.next()
                    P.dma("sp", lambda e, hs=hs, w=w: e.dma_start(out=hs[:], in_=hs_d[w * IT:(w + 1) * IT, :].rearrange("(s p) d -> p s d", p=128)), [], [bhs])
                    hsT, bhsT = hsTs.next()
                    for sub in range(4):
                        tb, btb = gen.next()
                        tpv = bfview(tb[:])
                        P.op("pe", [(lambda e, c=c, sub=sub, tpv=tpv, hs=hs: e.transpose(tpv[:, c, :], hs[:, sub, c * 128:(c + 1) * 128], ident[:])) for c in range(8)],
                             [bhs], [btb])
                        if sub % 2 == 0:
                            P.op("act", lambda e, sub=sub, tpv=tpv, hsT=hsT: e.activation(out=hsT[:, :, sub * 128:(sub + 1) * 128], in_=tpv, func=AF.Copy), [btb], [bhsT[sub]])
                        else:
                            P.op("dve", lambda e, sub=sub, tpv=tpv, hsT=hsT: e.tensor_copy(out=hsT[:, :, sub * 128:(sub + 1) * 128], in_=tpv), [btb], [bhsT[sub]])
                    return hsT, bhsT

                load_w(0)
                nxtT = item_T(0)
                for w in range(NI):
                    if w + 1 < NI:
                        load_w(w + 1)
                    wb, bwb = Wb[w % 2]
                    wg = wb[:, 0:4096].rearrange("p (c f) -> p c f", c=8)
                    wu = wb[:, 4096:8192].rearrange("p (c f) -> p c f", c=8)
                    wd = wb[:, 8192:12288].rearrange("p (c f) -> p c f", c=4)
                    bwg = bwu = bwd = bwb
                    hsT, bhsT = nxtT
                    heT, bhe = heTs.next()
                    for fc in range(4):
                        g_ps, bgp = gen.next()
                        u_ps, bup = gen.next()
                        P.op("pe", [(lambda e, c=c, fc=fc, g_ps=g_ps, wg=wg, hsT=hsT: e.matmul(g_ps[:], wg[:, c, fc * 128:(fc + 1) * 128], hsT[:, c, :],
                                                                                          start=(c == 0), stop=(c == 7))) for c in range(8)], bhsT + [bwg], [bgp])
                        P.op("pe", [(lambda e, c=c, fc=fc, u_ps=u_ps, wu=wu, hsT=hsT: e.matmul(u_ps[:], wu[:, c, fc * 128:(fc + 1) * 128], hsT[:, c, :],
                                                                                          start=(c == 0), stop=(c == 7))) for c in range(8)], bhsT + [bwu], [bup])
                        sg, bsg_ = sgs.next()
                        P.op("act", lambda e, sg=sg, g_ps=g_ps: e.activation(out=sg[:], in_=g_ps[:], func=AF.Silu), [bgp], [bsg_])
                        P.op("dve", lambda e, sg=sg, u_ps=u_ps, heT=heT, fc=fc: e.tensor_tensor(out=heT[:, fc, :], in0=sg[:], in1=u_ps[:], op=ALU.mult),
                             [bsg_, bup], [bhe[fc]])
                    if w + 1 < NI:
                        nxtT = item_T(w + 1)
                    for sub in range(4):
                        ys, bys = yss.next()
                        for nb in range(2):
                            y_ps, byp = gen.next()
                            P.op("pe", [(lambda e, fc=fc, sub=sub, nb=nb, y_ps=y_ps, heT=heT, wd=wd: e.matmul(y_ps[:], heT[:, fc, sub * 128:(sub + 1) * 128], wd[:, fc, nb * 512:(nb + 1) * 512],
                                                                                                             start=(fc == 0), stop=(fc == 3))) for fc in range(4)], bhe + [bwd], [byp])
                            if nb == 0:
                                P.op("act", lambda e, ys=ys, y_ps=y_ps: e.activation(out=ys[:, 0:512], in_=y_ps[:], func=AF.Copy), [byp], [bys])
                            else:
                                P.op("dve", lambda e, ys=ys, y_ps=y_ps: e.tensor_copy(out=ys[:, 512:1024], in_=y_ps[:]), [byp], [bys])
                        P.dma("sp", lambda e, ys=ys, w=w, sub=sub: e.dma_start(out=ys_d[w * IT + sub * 128:w * IT + (sub + 1) * 128, :], in_=ys[:]), [bys], [])
                P.barrier()

            with contextlib.ExitStack() as sp3:
                NC3 = 4
                st = norm_state(sp3, "p5", nsets=4, nxb=1)
                xts = Rot([(sb(sp3, "p5x%d" % i, [128, 1024]), Buf("x")) for i in range(NC3)])
                y0s = Rot([(sb(sp3, "p5y0%d" % i, [128, 1024]), Buf("y0")) for i in range(NC3)])
                y1s = Rot([(sb(sp3, "p5y1%d" % i, [128, 1024]), Buf("y1")) for i in range(NC3)])
                ots = Rot([(sb(sp3, "p5o%d" % i, [128, 1024]), Buf("ot")) for i in range(NC3)])

                def fin_tile(i):
                    xt, bx = xts.next()
                    y0, by0 = y0s.next()
                    y1, by1 = y1s.next()
                    P.dma("sp", lambda e: e.dma_start(out=xt[:], in_=x2_d[i * 128:(i + 1) * 128, :]), [], [bx])
                    yield
                    P.dma("pool", lambda e: e.indirect_dma_start(out=y0[:, :], out_offset=None, in_=ys_d[:, :],
                                                                 in_offset=bass.IndirectOffsetOnAxis(ap=idx1[:, i:i + 1], axis=0)), [], [by0])
                    yield
                    P.dma("pool", lambda e: e.indirect_dma_start(out=y1[:, :], out_offset=None, in_=ys_d[:, :],
                                                                 in_offset=bass.IndirectOffsetOnAxis(ap=idx2[:, i:i + 1], axis=0)), [], [by1])
                    yield
                    P.op("dve", lambda e: e.scalar_tensor_tensor(out=xt[:], in0=y0[:], scalar=wts[:, i, 0:1], in1=xt[:], op0=ALU.mult, op1=ALU.add), [by0, bx], [bx])
                    yield
                    P.op("dve", lambda e: e.scalar_tensor_tensor(out=xt[:], in0=y1[:], scalar=wts[:, i, 1:2], in1=xt[:], op0=ALU.mult, op1=ALU.add), [by1, bx], [bx])
                    yield
                    fss, fb, _u = st["sets"].next()
                    ot, bot = ots.next()
                    P.op("act", lambda e: e.activation(out=ot[:], in_=xt[:], func=AF.Square, scale=1.0 / 32, accum_out=fss[:, 0:1]), [bx], [fb[0], bot])
                    yield
                    P.op("dve", lambda e: e.tensor_scalar(out=fss[:, 1:2], in0=fss[:, 0:1], scalar1=EPS, scalar2=None, op0=ALU.add), [fb[0]], [fb[1]])
                    yield
                    P.op("act", lambda e: e.activation(out=fss[:, 2:3], in_=fss[:, 1:2], func=AF.Ln), [fb[1]], [fb[2]])
                    yield
                    P.op("act", lambda e: e.activation(out=fss[:, 3:4], in_=fss[:, 2:3], func=AF.Exp, scale=-0.5), [fb[2]], [fb[3]])
                    yield
                    P.op("dve", lambda e: e.scalar_tensor_tensor(out=ot[:], in0=xt[:], scalar=fss[:, 3:4], in1=gfin_bc[:], op0=ALU.mult, op1=ALU.mult),
                         [bx, fb[3], B_c], [bot])
                    yield
                    P.dma("sp", lambda e: e.dma_start(out=out[i * 128:(i + 1) * 128, :], in_=ot[:]), [bot], [])
                    yield

                run_pipeline((fin_tile(i) for i in range(32)), NC3)
                P.barrier()
            P.flush()
    return nc


def _rope_tables():
    rows = SEQ // 64
    row_ids = np.repeat(np.arange(rows), 64).astype(np.float32)
    col_ids = np.tile(np.arange(64), rows).astype(np.float32)
    inv = (10000.0 ** (-np.arange(16, dtype=np.float32) / 16)).astype(np.float32)
    ang = np.concatenate([row_ids[:, None] * inv[None, :], col_ids[:, None] * inv[None, :]], axis=-1).astype(np.float32)
    return np.cos(ang).astype(np.float32), np.sin(ang).astype(np.float32)


def _wcat(wg, wu, wd):
    g = wg.reshape(NE, 8, 128, 512).transpose(0, 2, 1, 3).reshape(NE, 128, 4096)
    u = wu.reshape(NE, 8, 128, 512).transpose(0, 2, 1, 3).reshape(NE, 128, 4096)
    d_ = wd.reshape(NE, 4, 128, 1024).transpose(0, 2, 1, 3).reshape(NE, 128, 4096)
    return np.ascontiguousarray(np.concatenate([g, u, d_], axis=2).reshape(NE * 128, 12288))


def make_in_maps(inputs):
    f = lambda a: np.ascontiguousarray(np.asarray(a, dtype=np.float32))
    x = f(inputs["x"])
    mem = f(inputs["mem"])
    cos, sin = _rope_tables()

    def lay(t, n):
        return np.ascontiguousarray(t.reshape(n, 128, 32).transpose(1, 0, 2))

    shared = {
        "ident": np.eye(128, dtype=np.float32),
        "g_mix": f(inputs["g_mix"][0]), "w_in": f(inputs["w_in"][0]), "g_q": f(inputs["g_q"][0]), "g_k": f(inputs["g_k"][0]),
        "w_att_out": f(inputs["w_att_out"][0]), "sgu_ln_g": f(inputs["sgu_ln_g"][0]), "sgu_ln_b": f(inputs["sgu_ln_b"][0]),
        "w_s": f(inputs["w_s"][0]), "b_s": f(inputs["b_s"][0]), "w_sgu_out": f(inputs["w_sgu_out"][0]), "w_out": f(inputs["w_out"][0]),
        "g_cross": f(inputs["g_cross"][0]), "g_mem": f(inputs["g_mem"][0]), "w_cq": f(inputs["w_cq"][0]), "w_ckv": f(inputs["w_ckv"][0]),
        "w_co": f(inputs["w_co"][0]), "g_moe": f(inputs["g_moe"][0]), "w_rg": f(inputs["w_rg"][0]), "b_rg": f(inputs["b_rg"][0]),
        "w_re": f(inputs["w_re"][0]), "b_re": f(inputs["b_re"][0]), "g_final": f(inputs["g_final"]),
        "wcat": _wcat(f(inputs["w_gate"][0]), f(inputs["w_up"][0]), f(inputs["w_down"][0])),
        "pidx": np.arange(128, dtype=np.float32).reshape(128, 1),
        "cos_seq": lay(cos, 64), "sin_seq": lay(sin, 64),
        "tri": np.triu(np.ones((128, 128), np.float32), 1),
        "iota32": np.tile(np.arange(32, dtype=np.float32), (128, 1)),
        "thr8": np.tile(np.arange(8, dtype=np.float32) * 512.0, (128, 1)),
    }
    maps = []
    for c in range(8):
        b, hf = c // 2, c % 2
        m = dict(shared)
        m["xseq"] = x[b]
        m["xown"] = np.ascontiguousarray(x[b, hf * OWN:(hf + 1) * OWN])
        m["mem"] = mem[b]
        m["cos_own"] = lay(cos[hf * OWN:(hf + 1) * OWN], 32)
        m["sin_own"] = lay(sin[hf * OWN:(hf + 1) * OWN], 32)
        maps.append(m)
    return maps


def kernel(**inputs):
    nc = build()
    maps = make_in_maps(inputs)
    res = run_bass_kernel_spmd(nc, maps, core_ids=list(range(8)))
    outp = np.empty((4, SEQ, D), np.float32)
    for c in range(8):
        b, hf = c // 2, c % 2
        outp[b, hf * OWN:(hf + 1) * OWN] = np.asarray(res.results[c]["out"], dtype=np.float32)
    return outp
```
